# Optimizing a Trainium2 kernel written in Bass

```python
import jax, jax.numpy as jnp
from jax import lax
import numpy as np

D_MODEL = 1024
BATCH = 8
SEQ = 4096
DEPTH = 1

D_CONV = D_MODEL // 2
CONV_WIDTH = 31
GROUP_DIM = 64
N_HEADS = 8
N_KV_HEADS = 2
HEAD_DIM = 64
D_ATTN = N_HEADS * HEAD_DIM
D_KV = N_KV_HEADS * HEAD_DIM
WINDOW = 128
BLOCK = 128
D_IN = 2 * D_CONV + D_ATTN + 2 * D_KV

N_GROUPS = 4
EXPERTS_PER_GROUP = 8
N_EXPERTS = N_GROUPS * EXPERTS_PER_GROUP
TOP_K = 2
D_EXPERT = D_MODEL // 4

ALPHA = (2.0 * DEPTH) ** 0.25
BETA = (8.0 * DEPTH) ** -0.25
EPS = 1e-5
NEG_INF = -1e30

kernel_name = "hymba_conformer_swa_sink_hmoe_deepnorm_adaln"


def layer_norm(x, g, b):
    xf = x.astype(jnp.float32)
    mu = jnp.mean(xf, axis=-1, keepdims=True)
    var = jnp.mean(jnp.square(xf - mu), axis=-1, keepdims=True)
    y = (xf - mu) * lax.rsqrt(var + EPS)
    return (y * g.astype(jnp.float32) + b.astype(jnp.float32)).astype(x.dtype)


def group_rms_norm(y, g):
    shp = y.shape
    yf = y.astype(jnp.float32).reshape(shp[:-1] + (shp[-1] // GROUP_DIM, GROUP_DIM))
    yf = yf * lax.rsqrt(jnp.mean(jnp.square(yf), axis=-1, keepdims=True) + EPS)
    return (yf.reshape(shp) * g.astype(jnp.float32)).astype(y.dtype)


def conformer_conv(u_a, u_b, conv_w, conv_b, ln_g, ln_b):
    v = u_a * jax.nn.sigmoid(u_b)
    y = lax.conv_general_dilated(
        v, conv_w[:, None, :].astype(v.dtype), window_strides=(1,),
        padding=[(CONV_WIDTH - 1, 0)],
        dimension_numbers=("NWC", "WIO", "NWC"),
        feature_group_count=D_CONV) + conv_b
    y = layer_norm(y, ln_g, ln_b)
    return jax.nn.silu(y)


def sliding_window_sink_attention(q, k, v, sinks):
    B, S, H, Dh = q.shape
    KV = k.shape[2]
    G = H // KV
    NB = S // BLOCK
    qb = q.reshape(B, NB, BLOCK, KV, G, Dh)
    pad = jnp.zeros((B, BLOCK, KV, Dh), k.dtype)
    kp = jnp.concatenate([pad, k], axis=1).reshape(B, NB + 1, BLOCK, KV, Dh)
    vp = jnp.concatenate([pad, v], axis=1).reshape(B, NB + 1, BLOCK, KV, Dh)
    kb = jnp.concatenate([kp[:, :-1], kp[:, 1:]], axis=2)
    vb = jnp.concatenate([vp[:, :-1], vp[:, 1:]], axis=2)
    s = jnp.einsum("bnqkgd,bnskd->bnkgqs", qb, kb).astype(jnp.float32) * (Dh ** -0.5)
    qi = jnp.arange(BLOCK)[:, None]
    si = jnp.arange(2 * BLOCK)[None, :]
    diff = qi + BLOCK - si
    band = (diff >= 0) & (diff < WINDOW)
    key_pos = jnp.arange(NB)[:, None, None] * BLOCK - BLOCK + si[None]
    valid = band[None] & (key_pos >= 0)
    s = jnp.where(valid[None, :, None, None], s, NEG_INF)
    sink = jnp.broadcast_to(sinks.astype(jnp.float32).reshape(1, 1, KV, G, 1, 1),
                            s.shape[:-1] + (1,))
    p = jax.nn.softmax(jnp.concatenate([s, sink], axis=-1), axis=-1)[..., :-1]
    o = jnp.einsum("bnkgqs,bnskd->bnqkgd", p.astype(v.dtype), vb)
    return o.reshape(B, S, H * Dh)


def hierarchical_moe(h, w_rg, b_rg, w_re, b_re, w_gate, w_up, w_down):
    B, S, D = h.shape
    t = h.reshape(-1, D)
    T = t.shape[0]
    p_group = jax.nn.softmax((t @ w_rg + b_rg).astype(jnp.float32), axis=-1)
    p_top, g_idx = lax.top_k(p_group, 1)
    e_logits = (t @ w_re + b_re).astype(jnp.float32).reshape(T, N_GROUPS, EXPERTS_PER_GROUP)
    sel = jnp.take_along_axis(e_logits, g_idx[:, :, None], axis=1)[:, 0]
    p_exp = jax.nn.softmax(sel, axis=-1)
    w_top, e_idx = lax.top_k(p_exp, TOP_K)
    w_top = w_top / jnp.sum(w_top, axis=-1, keepdims=True) * p_top
    eid = g_idx * EXPERTS_PER_GROUP + e_idx
    gates = jnp.sum(jax.nn.one_hot(eid, N_EXPERTS, dtype=jnp.float32) * w_top[..., None],
                    axis=1).astype(t.dtype)
    y = jnp.zeros_like(t)
    for e in range(N_EXPERTS):
        a = jax.nn.silu(t @ w_gate[e]) * (t @ w_up[e])
        y = y + gates[:, e:e + 1] * (a @ w_down[e])
    return y.reshape(B, S, D)


def setup_inputs(seed: int = 0) -> dict:
    key = jax.random.key(seed)
    ks = jax.random.split(key, 32)
    L, D = DEPTH, D_MODEL
    nrm = lambda k, shp, s: jax.random.normal(k, shp, jnp.float32) * s
    col_scale = jnp.concatenate([
        jnp.full((2 * D_CONV,), BETA), jnp.ones((D_ATTN + D_KV,)), jnp.full((D_KV,), BETA)])
    return {
        "x": nrm(ks[0], (BATCH, SEQ, D), 1.0),
        "c": nrm(ks[1], (BATCH, D), 1.0),
        "w_ada": nrm(ks[2], (L, D, 6 * D), 0.5 * D ** -0.5),
        "b_ada": nrm(ks[3], (L, 6 * D), 0.02),
        "w_in": nrm(ks[4], (L, D, D_IN), D ** -0.5) * col_scale,
        "b_in": nrm(ks[5], (L, D_IN), 0.02),
        "conv_w": nrm(ks[6], (L, CONV_WIDTH, D_CONV), CONV_WIDTH ** -0.5),
        "conv_b": nrm(ks[7], (L, D_CONV), 0.02),
        "conv_ln_g": 1.0 + nrm(ks[8], (L, D_CONV), 0.02),
        "conv_ln_b": nrm(ks[9], (L, D_CONV), 0.02),
        "conv_out_g": 1.0 + nrm(ks[10], (L, D_CONV), 0.02),
        "sinks": nrm(ks[11], (L, N_HEADS), 0.5),
        "attn_out_g": 1.0 + nrm(ks[12], (L, D_ATTN), 0.02),
        "w_out": nrm(ks[13], (L, D_CONV + D_ATTN, D), (D_CONV + D_ATTN) ** -0.5 * BETA),
        "b_out": nrm(ks[14], (L, D), 0.02),
        "ln1_g": 1.0 + nrm(ks[15], (L, D), 0.02),
        "ln1_b": nrm(ks[16], (L, D), 0.02),
        "w_router_group": nrm(ks[17], (L, D, N_GROUPS), D ** -0.5),
        "b_router_group": nrm(ks[18], (L, N_GROUPS), 0.01),
        "w_router_expert": nrm(ks[19], (L, D, N_EXPERTS), D ** -0.5),
        "b_router_expert": nrm(ks[20], (L, N_EXPERTS), 0.01),
        "w_gate": nrm(ks[21], (L, N_EXPERTS, D, D_EXPERT), D ** -0.5 * BETA),
        "w_up": nrm(ks[22], (L, N_EXPERTS, D, D_EXPERT), D ** -0.5 * BETA),
        "w_down": nrm(ks[23], (L, N_EXPERTS, D_EXPERT, D), D_EXPERT ** -0.5 * BETA),
        "ln2_g": 1.0 + nrm(ks[24], (L, D), 0.02),
        "ln2_b": nrm(ks[25], (L, D), 0.02),
    }


def reference(x, c, w_ada, b_ada, w_in, b_in, conv_w, conv_b, conv_ln_g, conv_ln_b,
              conv_out_g, sinks, attn_out_g, w_out, b_out, ln1_g, ln1_b,
              w_router_group, b_router_group, w_router_expert, b_router_expert,
              w_gate, w_up, w_down, ln2_g, ln2_b):
    B, S, _ = x.shape
    c_act = jax.nn.silu(c)
    for l in range(DEPTH):
        mod = c_act @ w_ada[l] + b_ada[l]
        sh1, sc1, g1, sh2, sc2, g2 = [m[:, None, :] for m in jnp.split(mod, 6, axis=-1)]

        h = x * (1.0 + sc1) + sh1
        proj = h @ w_in[l] + b_in[l]
        u_a, u_b, q, k, v = jnp.split(
            proj, [D_CONV, 2 * D_CONV, 2 * D_CONV + D_ATTN, 2 * D_CONV + D_ATTN + D_KV], axis=-1)
        y_conv = conformer_conv(u_a, u_b, conv_w[l], conv_b[l], conv_ln_g[l], conv_ln_b[l])
        y_conv = group_rms_norm(y_conv, conv_out_g[l])
        y_attn = sliding_window_sink_attention(
            q.reshape(B, S, N_HEADS, HEAD_DIM), k.reshape(B, S, N_KV_HEADS, HEAD_DIM),
            v.reshape(B, S, N_KV_HEADS, HEAD_DIM), sinks[l])
        y_attn = group_rms_norm(y_attn, attn_out_g[l])
        mix = jnp.concatenate([y_conv, y_attn], axis=-1) @ w_out[l] + b_out[l]
        x = layer_norm(ALPHA * x + g1 * mix, ln1_g[l], ln1_b[l])

        h2 = x * (1.0 + sc2) + sh2
        ffn = hierarchical_moe(h2, w_router_group[l], b_router_group[l], w_router_expert[l],
                               b_router_expert[l], w_gate[l], w_up[l], w_down[l])
        x = layer_norm(ALPHA * x + g2 * ffn, ln2_g[l], ln2_b[l])
    return x
```

```python
import os
import numpy as np
import concourse.bass as bass
import concourse.mybir as mybir
from concourse.bass_utils import run_bass_kernel_spmd

F32 = mybir.dt.float32
BF16 = mybir.dt.bfloat16
I32 = mybir.dt.int32
ALU = mybir.AluOpType
AF = mybir.ActivationFunctionType
AX = mybir.AxisListType

D = 1024
S = 4096
NT = S // 128
NMT = S // 512
DIN = 1792
NE = 32
DE = 256
ALPHA = 2.0 ** 0.25
EPS = 1e-5
NEG = -30000.0
T = 384
NS = (2 * S + NE * (T - 1) + T - 1) // T
NSUB = T // 128
HORDER = [0, 4, 1, 5, 2, 6, 3, 7]

C_C = 0
C_BIN = 8
C_CW = 21
C_CB = 145
C_LG = 149
C_LB = 153
C_OG = 157
C_PID = 161
NCOL = 162
R_BADA = 0
R_BV = 6144
R_SINK = 6272
R_AOG = 6280
R_BOUT = 6792
R_L1G = 7816
R_L1B = 8840
R_L2G = 9864
R_L2B = 10888
R_BR = 11912
R_SLOT = 11948
NROW = 12012
K_ID = 0
K_U = 128
K_BD = 256
K_M0 = 384
K_M1 = 896
NK = 1408


class Prog:
    def __init__(self, nc, sems):
        self.nc = nc
        self.ops = []
        self.last_w = {}
        self.readers = {}
        self.eng_sem = {e: sems[i] for i, e in enumerate(["pe", "act", "dve", "pool"])}
        rest = sems[4:]
        n_sp = (len(rest) * 5) // 10
        n_pool = (len(rest) * 4) // 10
        self.dma_pool = {"sp": rest[:n_sp], "pool": rest[n_sp:n_sp + n_pool], "act": rest[n_sp + n_pool:]}

    def op(self, eng, fn, r=(), w=(), dma=False):
        i = len(self.ops)
        w = list(w) + [h for h in r if h.startswith("ps") and h not in w]
        raw, oth = set(), set()
        for h in r:
            if h in self.last_w:
                raw.add(self.last_w[h])
        for h in w:
            if h in self.last_w:
                oth.add(self.last_w[h])
            for j in self.readers.get(h, ()):
                oth.add(j)
        for h in w:
            self.last_w[h] = i
            self.readers[h] = []
        for h in r:
            self.readers.setdefault(h, []).append(i)
        deps = []
        for j in sorted(raw | oth):
            p = self.ops[j]
            if j == i:
                continue
            if (not p["dma"]) and p["eng"] == eng:
                if eng == "pe" or j not in raw:
                    continue
            deps.append(j)
        self.ops.append(dict(eng=eng, fn=fn, deps=deps, dma=dma, sig=False))
        for j in deps:
            self.ops[j]["sig"] = True
        return i

    def fence(self, engs=("pe", "act", "dve", "pool", "sp")):
        hs = list(self.last_w.keys())
        for e in engs:
            self.op(e, None, r=hs, w=["_fence_" + e])

    def emit(self):
        nc = self.nc
        ticket = {e: 0 for e in self.eng_sem}
        dma_next = {q: 0 for q in self.dma_pool}
        dma_uses = {}
        for o in self.ops:
            if o["dma"]:
                q = o["eng"]
                pool = self.dma_pool[q]
                sem = pool[dma_next[q] % len(pool)]
                dma_next[q] += 1
                u = dma_uses.get(id(sem), 0)
                o["pre"] = (sem, 16 * u)
                dma_uses[id(sem)] = u + 1
                o["ev"] = (sem, 16 * (u + 1))
            elif o["sig"] and o["fn"] is not None:
                ticket[o["eng"]] += 1
                o["ev"] = (self.eng_sem[o["eng"]], ticket[o["eng"]])
        ops = self.ops

        def run(engname, eobj):
            waited = {}

            def wait(sem, val):
                if val <= 0:
                    return
                if waited.get(id(sem), 0) >= val:
                    return
                eobj.wait_ge(sem, val)
                waited[id(sem)] = val

            for o in ops:
                if o["eng"] != engname:
                    continue
                for j in o["deps"]:
                    ev = ops[j].get("ev")
                    if ev is not None:
                        wait(*ev)
                if o["fn"] is None:
                    continue
                if o["dma"]:
                    wait(*o["pre"])
                    ins = o["fn"](eobj)
                    ins.then_inc(o["ev"][0], 16)
                else:
                    ins = o["fn"](eobj)
                    if o["sig"]:
                        ins.then_inc(o["ev"][0], 1)

        with nc.Block() as block:
            @block.tensor
            def _(e):
                run("pe", e)

            @block.scalar
            def _(e):
                run("act", e)

            @block.vector
            def _(e):
                run("dve", e)

            @block.gpsimd
            def _(e):
                run("pool", e)

            @block.sync
            def _(e):
                run("sp", e)


class _Stop(Exception):
    pass


class Arena:
    def __init__(self, t, nbytes):
        self.t = t
        self.nbytes = nbytes
        self.off = 0

    def alloc(self, shape, dt):
        esz = 4 if dt in (F32, I32) else 2
        n = int(np.prod(shape)) * esz
        n = (n + 63) // 64 * 64
        assert self.off + n <= self.nbytes, ("arena overflow", self.off, n, self.nbytes)
        v = self.t[:, self.off // 4:(self.off + n) // 4]
        self.off += n
        if dt != F32:
            v = v.bitcast(dt)
        v = v[:, 0:int(np.prod(shape))]
        if len(shape) == 2:
            return v.rearrange("p (a b) -> p a b", a=shape[0])
        if len(shape) == 3:
            return v.rearrange("p (a b c) -> p a b c", a=shape[0], b=shape[1])
        return v


def build_program(dbg=None):
    nc = bass.Bass("TRN2", target_bir_lowering=False)
    try:
        return _build_program(nc, dbg)
    except _Stop:
        return nc


def _build_program(nc, dbg=None):
    dr = {}

    def din(name, shape, dt=F32):
        dr[name] = nc.dram_tensor(name, list(shape), dt, kind="ExternalInput").ap()
        return dr[name]

    x_d = din("x", [S, D])
    cols_d = din("cols", [128, NCOL])
    rows_d = din("rows", [1, NROW])
    cst_d = din("cst", [128, NK])
    wada_d = din("w_ada", [D, 6 * D])
    win_d = din("w_in", [D, DIN])
    wout_d = din("w_out", [D, D])
    wr_d = din("w_r", [D, 36])
    wg_d = din("wg", [NE * 128, 2048])
    wu_d = din("wu", [NE * 128, 2048])
    wd_d = din("wd", [NE * 128, 2048])
    out_d = nc.dram_tensor("out", [S, D], F32, kind="ExternalOutput").ap()
    if _CACHE.get("p2only"):
        z_d = din("z_in", [S, D])
    else:
        z_d = nc.dram_tensor("z_scr", [S, D], F32, kind="Internal").ap()
    xs_d = nc.dram_tensor("xs_scr", [NS * T, D], BF16, kind="Internal").ap()
    ys_d = nc.dram_tensor("ys_scr", [NS * T, D], F32, kind="Internal").ap()
    dbg_d = None
    if dbg:
        dbg_d = nc.dram_tensor("dbg", list(dbg["shape"]), F32, kind="ExternalOutput").ap()

    import contextlib
    with contextlib.ExitStack() as st:
        ARENA_BYTES = 206 * 1024
        arena_t = st.enter_context(nc.sbuf_tensor("arena", [128, ARENA_BYTES // 4], F32))
        ps = [st.enter_context(nc.psum_tensor(f"ps{i}", [128, 512], F32)) for i in range(6)]
        psb = [st.enter_context(nc.psum_tensor(f"psb{i}", [128, 1024], BF16)) for i in range(2)]
        sems = [st.enter_context(nc.semaphore(f"s{i}")) for i in range(_CACHE.get("nsem", 48))]
        P = Prog(nc, sems)
        A = Arena(arena_t, ARENA_BYTES)
        psn = [0]

        def nps():
            i = psn[0] % 6
            psn[0] += 1
            return ps[i], f"ps{i}"

        cols = A.alloc([NCOL], F32)
        cst = A.alloc([NK], F32)
        identf = cst[:, K_ID:K_ID + 128]
        identb = A.alloc([128], BF16)
        onesb = A.alloc([128], BF16)
        ones512 = A.alloc([128], BF16)
        bdb = A.alloc([128], BF16)
        Ub = A.alloc([128], BF16)
        maskT = A.alloc([2, 512], BF16)
        modc = A.alloc([32], F32)
        gbc = A.alloc([4, D], F32)
        EPSC = A.alloc([1], F32)
        mark_persist = A.off

        P.op("sp", lambda e: e.dma_start(out=cols, in_=cols_d), w=["cols"], dma=True)
        P.op("sp", lambda e: e.dma_start(out=cst, in_=cst_d), w=["cst"], dma=True)
        P.op("dve", lambda e: e.tensor_copy(out=identb, in_=identf), r=["cst"], w=["identb"])
        P.op("dve", lambda e: e.memset(onesb, 1.0), w=["onesb"])
        P.op("dve", lambda e: e.memset(EPSC, EPS), w=["epsc"])
        P.op("dve", lambda e: e.memset(ones512, 1.0 / 512.0), w=["ones512"])
        P.op("dve", lambda e: e.tensor_copy(out=bdb, in_=cst[:, K_BD:K_BD + 128]), r=["cst"], w=["bdb"])
        P.op("dve", lambda e: e.tensor_copy(out=Ub, in_=cst[:, K_U:K_U + 128]), r=["cst"], w=["Ub"])
        P.op("dve", lambda e: e.tensor_copy(out=maskT.rearrange("p a b -> p (a b)"), in_=cst[:, K_M0:K_M0 + 1024]),
             r=["cst"], w=["maskT"])

        ph0 = A.off
        cact = A.alloc([8], F32)
        cbc = A.alloc([8, 128], BF16)
        wab = [A.alloc([8, 512], BF16) for _ in range(2)]
        badab = A.alloc([512], F32)
        modb = A.alloc([4 * D], F32)
        P.op("act", lambda e: e.activation(out=cact, in_=cols[:, C_C:C_C + 8], func=AF.Silu), r=["cols"], w=["cact"])
        for c in range(8):
            P.op("dve", lambda e, c=c: e.tensor_scalar(out=cbc[:, c, :], in0=onesb, scalar1=cact[:, c:c + 1],
                                                       scalar2=None, op0=ALU.mult),
                 r=["cact", "onesb"], w=["cbc"])
        for blk in range(12):
            wb = wab[blk % 2]
            hw = f"wab{blk % 2}"
            P.op("pool", lambda e, blk=blk, wb=wb: e.dma_start(
                out=wb, in_=wada_d[:, blk * 512:(blk + 1) * 512].rearrange("(c p) n -> p c n", p=128)),
                w=[hw], dma=True)
            P.op("sp", lambda e, blk=blk: e.dma_start(
                out=badab, in_=rows_d[0:1, R_BADA + blk * 512:R_BADA + (blk + 1) * 512].partition_broadcast(128)),
                w=["badab"], dma=True)
            pt, hp = nps()
            for c in range(8):
                P.op("pe", lambda e, c=c, pt=pt, wb=wb: e.matmul(pt[:], lhsT=cbc[:, c, :], rhs=wb[:, c, :],
                                                               start=(c == 0), stop=(c == 7)),
                     r=["cbc", hw], w=[hp])
            if blk < 4:
                dst = modb[:, blk * 512:(blk + 1) * 512]
                hd = f"modb{blk}"
            else:
                gi = (blk - 4) // 2
                dst = gbc[:, gi, ((blk - 4) % 2) * 512:((blk - 4) % 2 + 1) * 512]
                hd = f"gbc{gi}_{blk % 2}"
            P.op("dve", lambda e, pt=pt, dst=dst: e.tensor_tensor(out=dst, in0=pt[:], in1=badab, op=ALU.add),
                 r=[hp, "badab"], w=[hd])
        srcs = []
        for c in range(8):
            srcs.append((modb[:, c * 128:(c + 1) * 128], f"modb{c // 4}", c, 0.0))
        for c in range(8):
            srcs.append((modb[:, 1024 + c * 128:1024 + (c + 1) * 128], f"modb{2 + c // 4}", 8 + c, 1.0))
        for c in range(8):
            srcs.append((gbc[:, 1, c * 128:(c + 1) * 128], f"gbc1_{c // 4}", 16 + c, 0.0))
        for c in range(8):
            srcs.append((gbc[:, 2, c * 128:(c + 1) * 128], f"gbc2_{c // 4}", 24 + c, 1.0))
        for (src, hs, col, add) in srcs:
            pt, hp = nps()
            P.op("pe", lambda e, pt=pt, src=src: e.transpose(out=pt[:, 0:128], in_=src, identity=identf),
                 r=[hs, "cst"], w=[hp])
            P.op("dve", lambda e, pt=pt, col=col, add=add: e.tensor_scalar(
                out=modc[:, col:col + 1], in0=pt[:, 0:1], scalar1=add, scalar2=None, op0=ALU.add),
                r=[hp], w=["modc"])
        P.fence()
        if _CACHE.get("stop") == 0:
            P.op("sp", lambda e: e.dma_start(out=dbg_d[0:128, 0:32], in_=modc), r=["modc"], w=["dbg"], dma=True)
            P.op("sp", lambda e: e.dma_start(out=dbg_d[128:256, :], in_=gbc[:, 0, :]), r=["gbc0_0", "gbc0_1"], w=["dbg2"], dma=True)
            P.fence()
            P.emit()
            return nc
        A.off = ph0

        win = A.alloc([8, DIN], BF16)
        modc2 = A.alloc([32], F32)
        P.op("dve", lambda e: e.tensor_copy(out=modc2, in_=modc), r=["modc"], w=["modc2"])
        wout = A.alloc([8, D], BF16)
        diag = A.alloc([124, 128], BF16)
        xt = A.alloc([4, D], BF16)
        hT = A.alloc([8, 512], BF16)
        vr = [A.alloc([4, 542], BF16) for _ in range(2)]
        kr = [[A.alloc([640], BF16) for _ in range(2)] for _ in range(2)]
        va = [A.alloc([5, 130], BF16) for _ in range(2)]
        qT = A.alloc([4, 512], BF16)
        sig = A.alloc([512], F32)
        ybf = A.alloc([4, 512], BF16)
        y2bf = A.alloc([4, 512], BF16)
        mean_sb = A.alloc([512], F32)
        m2_sb = A.alloc([512], F32)
        zc = m2_sb
        rstd_sb = A.alloc([512], F32)
        nmr_sb = A.alloc([512], F32)
        sc_ = A.alloc([512], F32)
        s2bf = A.alloc([512], BF16)
        r2_sb = sig
        ycT = A.alloc([8, 512], BF16)
        mx = A.alloc([8], F32)
        mxb = A.alloc([8], BF16)
        nmx = A.alloc([8], F32)
        dcat = A.alloc([2, 512], BF16)
        ET = A.alloc([4, 512], BF16)
        es_t = A.alloc([8], F32)
        den = A.alloc([8], F32)
        osb = A.alloc([8, 64], F32)
        osq = A.alloc([8, 64], F32)
        ssq = A.alloc([8], F32)
        yat = A.alloc([512], BF16)
        rows_sb = A.alloc([128 + 8 + 512 + D], F32)
        gb = A.alloc([D], F32)
        xr = A.alloc([D], F32)
        rr = A.alloc([D], F32)
        zz = A.alloc([D], F32)
        bnst = A.alloc([12], F32)
        bnag = A.alloc([4], F32)
        print("phase1 arena bytes", A.off)

        bv_bc = rows_sb[:, 0:128]
        sink_bc = rows_sb[:, 128:136]
        aog_bc = rows_sb[:, 136:648]
        bout_bc = rows_sb[:, 648:648 + D]
        P.op("sp", lambda e: e.dma_start(out=rows_sb, in_=rows_d[0:1, R_BV:R_BV + 128 + 8 + 512 + D].partition_broadcast(128)),
             w=["rows_sb"], dma=True)
        for c in range(8):
            P.op("pool", lambda e, c=c: e.dma_start(out=win[:, c, :], in_=win_d[c * 128:(c + 1) * 128, :]), w=[f"win{c}"], dma=True)
            P.op("pool", lambda e, c=c: e.dma_start(out=wout[:, c, :], in_=wout_d[c * 128:(c + 1) * 128, :]), w=[f"wout{c}"], dma=True)
        for c in range(8):
            P.op("dve", lambda e, c=c: e.tensor_tensor(out=wout[:, c, :], in0=wout[:, c, :], in1=gbc[:, 0, :], op=ALU.mult),
                 r=[f"wout{c}", "gbc0_0", "gbc0_1"], w=[f"wout{c}"])
        P.op("dve", lambda e: e.tensor_tensor(out=gb, in0=bout_bc, in1=gbc[:, 0, :], op=ALU.mult),
             r=["rows_sb", "gbc0_0", "gbc0_1"], w=["gb"])
        WINH = [f"win{c}" for c in range(8)]
        WOUTH = [f"wout{c}" for c in range(8)]
        SK = _CACHE.get("skip", set())
        for c in range(4 if "diag" not in SK else 0):
            P.op("dve", lambda e, c=c: e.tensor_tensor(
                out=diag[:, c * 31:(c + 1) * 31, :], in0=identb.unsqueeze(1).to_broadcast([128, 31, 128]),
                in1=cols[:, C_CW + c * 31:C_CW + (c + 1) * 31].unsqueeze(2).to_broadcast([128, 31, 128]), op=ALU.mult),
                r=["identb", "cols"], w=["diag"])
        if "memset" not in SK:
            P.op("pool", lambda e: e.memset(vr[0], 0.0), w=["vr0"])
            P.op("pool", lambda e: e.memset(vr[1], 0.0), w=["vr1"])
        for par in range(2 if "memset" not in SK else 0):
            for g in range(2):
                P.op("pool", lambda e, par=par, g=g: e.memset(kr[par][g], 0.0), w=[f"kr{par}"])
            P.op("pool", lambda e, par=par: e.memset(va[par], 0.0), w=[f"va{par}"])
            P.op("dve", lambda e, par=par: e.memset(va[par][:, 1:, 64:65], 1.0), r=[f"va{par}"], w=[f"va{par}"])
            P.op("dve", lambda e, par=par: e.memset(va[par][:, 1:, 129:130], 1.0), r=[f"va{par}"], w=[f"va{par}"])

        if _CACHE.get("stop") == 1:
            P.fence()
            P.op("sp", lambda e: e.dma_start(out=dbg_d[0:128, 0:512], in_=gb[:, 0:512]), r=["gb"], w=["dbg"], dma=True)
            P.fence()
            P.emit()
            return nc
        for mt in range(0 if _CACHE.get("p2only") else _CACHE.get('nmt_run', NMT)):
            t0 = mt * 512
            vcur, vprev = vr[mt % 2], vr[(mt + 1) % 2]
            hv, hvp = f"vr{mt % 2}", f"vr{(mt + 1) % 2}"
            kT, kTp = kr[mt % 2], kr[(mt + 1) % 2]
            hk, hkp = f"kr{mt % 2}", f"kr{(mt + 1) % 2}"
            vaug, vaugp = va[mt % 2], va[(mt + 1) % 2]
            hva, hvap = f"va{mt % 2}", f"va{(mt + 1) % 2}"
            P.op("pool", lambda e, t0=t0: e.dma_start(out=xt, in_=x_d[t0:t0 + 512, :].rearrange("(s p) d -> p s d", p=128)),
                 w=["xt"], dma=True)
            for c in range(8):
                ptx = psb[c % 2][:, 0:512]
                hp = f"psb{c % 2}"
                for s in range(4 if "notr" not in SK else 0):
                    P.op("pe", lambda e, ptx=ptx, s=s, c=c: e.transpose(
                        out=ptx[:, s * 128:(s + 1) * 128], in_=xt[:, s, c * 128:(c + 1) * 128], identity=identb),
                        r=["xt", "identb"], w=[hp])
                if "noact" not in SK:
                    P.op("act", lambda e, ptx=ptx, c=c: e.activation(
                        out=hT[:, c, :], in_=ptx[:, 0:512], func=AF.Identity, bias=modc2[:, c:c + 1], scale=(1.0 if "fscale" in SK else modc2[:, 8 + c:9 + c])),
                        r=[hp, "modc2"], w=["hT"])
            def stop_at(k):
                if _CACHE.get("stop") == k:
                    P.fence()
                    P.op("sp", lambda e: e.dma_start(out=dbg_d[0:128, 0:512], in_=gb[:, 0:512]), r=["gb"], w=["dbg"], dma=True)
                    P.fence()
                    P.emit()
                    raise _Stop()
            stop_at(2)
            if mt > 0:
                P.op("pool", lambda e, vcur=vcur, vprev=vprev: e.tensor_copy(out=vcur[:, :, 0:30], in_=vprev[:, :, 512:542]),
                     r=[hvp], w=[hv])
                for g in range(2):
                    P.op("pool", lambda e, kT=kT, kTp=kTp, g=g: e.tensor_copy(out=kT[g][:, 0:128], in_=kTp[g][:, 512:640]),
                         r=[hkp], w=[hk])
                P.op("pool", lambda e, vaug=vaug, vaugp=vaugp: e.tensor_copy(out=vaug[:, 0, :], in_=vaugp[:, 4, :]),
                     r=[hvap], w=[hva])
            for c in range(4):
                pb, hpb = nps()
                for k in range(8):
                    P.op("pe", lambda e, pb=pb, k=k, c=c: e.matmul(
                        pb[:], lhsT=win[:, k, 512 + c * 128:512 + (c + 1) * 128], rhs=hT[:, k, :],
                        start=(k == 0), stop=(k == 7)), r=WINH + ["hT"], w=[hpb])
                pa, hpa = nps()
                for k in range(8):
                    P.op("pe", lambda e, pa=pa, k=k, c=c: e.matmul(
                        pa[:], lhsT=win[:, k, c * 128:(c + 1) * 128], rhs=hT[:, k, :],
                        start=(k == 0), stop=(k == 7)), r=WINH + ["hT"], w=[hpa])
                P.op("act", lambda e, pb=pb, c=c: e.activation(
                    out=sig, in_=pb[:], func=AF.Sigmoid, bias=cols[:, C_BIN + 4 + c:C_BIN + 5 + c], scale=1.0),
                    r=[hpb, "cols"], w=["sig"])
                P.op("dve", lambda e, pa=pa, c=c, vcur=vcur: e.scalar_tensor_tensor(
                    out=vcur[:, c, 30:542], in0=pa[:], scalar=cols[:, C_BIN + c:C_BIN + c + 1], in1=sig,
                    op0=ALU.add, op1=ALU.mult), r=[hpa, "sig", "cols"], w=[hv])
            for i in range(4):
                pq, hpq = nps()
                for k in range(8):
                    P.op("pe", lambda e, pq=pq, k=k, i=i: e.matmul(
                        pq[:], lhsT=win[:, k, 1024 + i * 128:1024 + (i + 1) * 128], rhs=hT[:, k, :],
                        start=(k == 0), stop=(k == 7)), r=WINH + ["hT"], w=[hpq])
                P.op("dve", lambda e, pq=pq, i=i: e.tensor_scalar(
                    out=qT[:, i, :], in0=pq[:], scalar1=cols[:, C_BIN + 8 + i:C_BIN + 9 + i], scalar2=0.125,
                    op0=ALU.add, op1=ALU.mult), r=[hpq, "cols"], w=["qT"])
            pk, hpk = nps()
            for k in range(8):
                P.op("pe", lambda e, pk=pk, k=k: e.matmul(
                    pk[:], lhsT=win[:, k, 1536:1664], rhs=hT[:, k, :], start=(k == 0), stop=(k == 7)),
                    r=WINH + ["hT"], w=[hpk])
            for g in range(2):
                P.op("act", lambda e, pk=pk, g=g, kT=kT: e.activation(
                    out=kT[g][g * 64:(g + 1) * 64, 128:640], in_=pk[g * 64:(g + 1) * 64, :],
                    func=AF.Identity, bias=cols[g * 64:(g + 1) * 64, C_BIN + 12:C_BIN + 13], scale=1.0),
                    r=[hpk, "cols"], w=[hk])
            pv, hpv = nps()
            for s in range(4):
                for k in range(8):
                    P.op("pe", lambda e, pv=pv, s=s, k=k: e.matmul(
                        pv[:, s * 128:(s + 1) * 128], lhsT=hT[:, k, s * 128:(s + 1) * 128], rhs=win[:, k, 1664:1792],
                        start=(k == 0), stop=(k == 7)), r=WINH + ["hT"], w=[hpv])
            for s in range(4):
                blk = s + 1
                P.op("dve", lambda e, pv=pv, s=s, blk=blk, vaug=vaug: e.tensor_tensor(
                    out=vaug[:, blk, :].rearrange("p (g d) -> p g d", g=2)[:, :, 0:64],
                    in0=pv[:, s * 128:(s + 1) * 128].rearrange("p (g d) -> p g d", g=2),
                    in1=bv_bc.rearrange("p (g d) -> p g d", g=2), op=ALU.add),
                    r=[hpv, "rows_sb"], w=[hva])
            stop_at(3)
            for c in range(4):
                py, hpy = nps()
                for j in range(31):
                    P.op("pe", lambda e, py=py, c=c, j=j, vcur=vcur: e.matmul(
                        py[:], lhsT=diag[:, c * 31 + j, :], rhs=vcur[:, c, j:j + 512], start=(j == 0), stop=(j == 30)),
                        r=["diag", hv], w=[hpy])
                P.op("act", lambda e, py=py, c=c: e.activation(
                    out=ybf[:, c, :], in_=py[:], func=AF.Identity, bias=cols[:, C_CB + c:C_CB + c + 1], scale=1.0),
                    r=[hpy, "cols"], w=["ybf"])
                P.op("act", lambda e, py=py, c=c: e.activation(
                    out=y2bf[:, c, :], in_=py[:], func=AF.Square, bias=cols[:, C_CB + c:C_CB + c + 1], scale=1.0),
                    r=[hpy, "cols"], w=["y2bf"])
            pm, hpm = nps()
            for c in range(4):
                P.op("pe", lambda e, pm=pm, c=c: e.matmul(pm[:], lhsT=ones512, rhs=ybf[:, c, :], start=(c == 0), stop=(c == 3)),
                     r=["ones512", "ybf"], w=[hpm])
            pe2, hpe2 = nps()
            for c in range(4):
                P.op("pe", lambda e, pe2=pe2, c=c: e.matmul(pe2[:], lhsT=ones512, rhs=y2bf[:, c, :], start=(c == 0), stop=(c == 3)),
                     r=["ones512", "y2bf"], w=[hpe2])
            P.op("act", lambda e, pm=pm: e.activation(out=mean_sb, in_=pm[:], func=AF.Identity), r=[hpm], w=["mean_sb"])
            P.op("dve", lambda e: e.tensor_tensor(out=m2_sb, in0=mean_sb, in1=mean_sb, op=ALU.mult), r=["mean_sb"], w=["m2_sb"])
            P.op("dve", lambda e, pe2=pe2: e.tensor_tensor(out=m2_sb, in0=pe2[:], in1=m2_sb, op=ALU.subtract),
                 r=[hpe2, "m2_sb"], w=["m2_sb"])
            P.op("act", lambda e: e.activation(out=rstd_sb, in_=m2_sb, func=AF.Sqrt, bias=EPSC, scale=1.0), r=["m2_sb", "epsc"], w=["rstd_sb"])
            P.op("dve", lambda e: e.reciprocal(out=rstd_sb, in_=rstd_sb), r=["rstd_sb"], w=["rstd_sb"])
            P.op("dve", lambda e: e.scalar_tensor_tensor(out=nmr_sb, in0=mean_sb, scalar=-1.0, in1=rstd_sb, op0=ALU.mult, op1=ALU.mult),
                 r=["mean_sb", "rstd_sb"], w=["nmr_sb"])
            for c in range(4):
                P.op("dve", lambda e, c=c: e.tensor_tensor(out=zc, in0=ybf[:, c, :], in1=rstd_sb, op=ALU.mult),
                     r=["ybf", "rstd_sb"], w=["m2_sb"])
                P.op("dve", lambda e: e.tensor_tensor(out=zc, in0=zc, in1=nmr_sb, op=ALU.add), r=["m2_sb", "nmr_sb"], w=["m2_sb"])
                P.op("act", lambda e, c=c: e.activation(out=sc_, in_=zc, func=AF.Silu, bias=cols[:, C_LB + c:C_LB + c + 1],
                                                        scale=cols[:, C_LG + c:C_LG + c + 1]), r=["m2_sb", "cols"], w=["sc_"])
                P.op("act", lambda e: e.activation(out=s2bf, in_=sc_, func=AF.Square), r=["sc_"], w=["s2bf"])
                pr, hpr = nps()
                P.op("pe", lambda e, pr=pr: e.matmul(pr[:], lhsT=bdb, rhs=s2bf, start=True, stop=True), r=["bdb", "s2bf"], w=[hpr])
                P.op("act", lambda e, pr=pr: e.activation(out=r2_sb, in_=pr[:], func=AF.Sqrt, bias=EPSC, scale=1.0), r=[hpr, "epsc"], w=["sig"])
                P.op("dve", lambda e: e.reciprocal(out=r2_sb, in_=r2_sb), r=["sig"], w=["sig"])
                P.op("dve", lambda e, c=c: e.scalar_tensor_tensor(out=ycT[:, c, :], in0=sc_, scalar=cols[:, C_OG + c:C_OG + c + 1],
                                                                  in1=r2_sb, op0=ALU.mult, op1=ALU.mult),
                     r=["sc_", "sig", "cols"], w=["ycT"])
            stop_at(4)
            for s in range(4):
                n = mt * 4 + s
                qs = slice(s * 128, (s + 1) * 128)
                for i in range(4):
                    pS, hpS = nps()
                    for g in range(2):
                        P.op("pe", lambda e, pS=pS, i=i, g=g, s=s, qs=qs, kT=kT: e.matmul(
                            pS[:, g * 256:(g + 1) * 256], lhsT=qT[:, i, qs], rhs=kT[g][:, s * 128:s * 128 + 256],
                            start=True, stop=True), r=["qT", hk], w=[hpS])
                    P.op("dve", lambda e, pS=pS, i=i: e.tensor_reduce(
                        out=mx[:, 2 * i:2 * i + 2], in_=pS[:].rearrange("p (g k) -> p g k", g=2), axis=AX.X, op=ALU.max),
                        r=[hpS], w=["mx"])
                P.op("dve", lambda e: e.tensor_copy(out=mxb, in_=mx), r=["mx"], w=["mxb"])
                P.op("dve", lambda e: e.tensor_scalar(out=nmx, in0=mxb, scalar1=-1.0, scalar2=None, op0=ALU.mult), r=["mxb"], w=["nmx"])
                for g in range(2):
                    P.op("dve", lambda e, g=g: e.tensor_tensor(
                        out=dcat[:, g, :].rearrange("p (i q) -> p i q", i=4), in0=identb.unsqueeze(1).to_broadcast([128, 4, 128]),
                        in1=nmx.rearrange("p (i g) -> p i g", g=2)[:, :, g:g + 1].to_broadcast([128, 4, 128]), op=ALU.mult),
                        r=["identb", "nmx"], w=["dcat"])
                khs = [1] if n == 0 else [0, 1]
                for g in range(2):
                    for kh in khs:
                        pT, hpT = nps()
                        kc = slice(s * 128 + kh * 128, s * 128 + kh * 128 + 128)
                        P.op("pe", lambda e, pT=pT, g=g, kc=kc, qs=qs, kT=kT: e.matmul(
                            pT[:].rearrange("p (i q) -> p i q", i=4), lhsT=kT[g][:, kc], rhs=qT[:, :, qs], start=True, stop=False),
                            r=[hk, "qT"], w=[hpT])
                        P.op("pe", lambda e, pT=pT, g=g: e.matmul(pT[:], lhsT=onesb, rhs=dcat[:, g, :], start=False, stop=False),
                             r=["onesb", "dcat"], w=[hpT])
                        P.op("pe", lambda e, pT=pT, kh=kh: e.matmul(pT[:], lhsT=identb, rhs=maskT[:, kh, :], start=False, stop=True),
                             r=["identb", "maskT"], w=[hpT])
                        P.op("act", lambda e, pT=pT, g=g, kh=kh: e.activation(out=ET[:, g * 2 + kh, :], in_=pT[:], func=AF.Exp),
                             r=[hpT], w=[f"ET{g}{kh}"])
                pos_ = []
                for g in range(2):
                    po, hpo = nps()
                    pos_.append((po, hpo))
                    for i in range(4):
                        for kh in khs:
                            P.op("pe", lambda e, po=po, g=g, i=i, kh=kh, s=s, khs=khs, vaug=vaug: e.matmul(
                                po[:, i * 65:(i + 1) * 65], lhsT=ET[:, g * 2 + kh, i * 128:(i + 1) * 128],
                                rhs=vaug[:, s + kh, g * 65:(g + 1) * 65], start=(kh == khs[0]), stop=(kh == 1)),
                                r=[f"ET{g}{kh}", hva], w=[hpo])
                P.op("dve", lambda e: e.tensor_tensor(out=es_t, in0=sink_bc, in1=nmx, op=ALU.add), r=["rows_sb", "nmx"], w=["es_t"])
                P.op("act", lambda e: e.activation(out=es_t, in_=es_t, func=AF.Exp), r=["es_t"], w=["es_t"])
                for g in range(2):
                    po, hpo = pos_[g]
                    P.op("dve", lambda e, po=po, g=g: e.tensor_tensor(
                        out=den.rearrange("p (i g) -> p i g", g=2)[:, :, g:g + 1],
                        in0=po[:, 0:260].rearrange("p (i d) -> p i d", d=65)[:, :, 64:65],
                        in1=es_t.rearrange("p (i g) -> p i g", g=2)[:, :, g:g + 1], op=ALU.add),
                        r=[hpo, "es_t"], w=["den"])
                    P.op("act", lambda e, po=po, g=g: e.activation(
                        out=osb.rearrange("p (i g) d -> p i g d", g=2)[:, :, g, :],
                        in_=po[:, 0:260].rearrange("p (i d) -> p i d", d=65)[:, :, 0:64], func=AF.Identity),
                        r=[hpo], w=["osb"])
                P.op("dve", lambda e: e.reciprocal(out=den, in_=den), r=["den"], w=["den"])
                P.op("dve", lambda e: e.tensor_tensor(out=osb, in0=osb, in1=den.unsqueeze(2).to_broadcast([128, 8, 64]), op=ALU.mult),
                     r=["osb", "den"], w=["osb"])
                P.op("act", lambda e: e.activation(out=osq, in_=osb, func=AF.Square), r=["osb"], w=["osq"])
                P.op("dve", lambda e: e.tensor_reduce(out=ssq, in_=osq, axis=AX.X, op=ALU.add), r=["osq"], w=["ssq"])
                P.op("act", lambda e: e.activation(out=ssq, in_=ssq, func=AF.Sqrt, bias=EPSC, scale=1.0 / 64.0), r=["ssq", "epsc"], w=["ssq"])
                P.op("dve", lambda e: e.reciprocal(out=ssq, in_=ssq), r=["ssq"], w=["ssq"])
                P.op("dve", lambda e: e.tensor_tensor(out=osb, in0=osb, in1=ssq.unsqueeze(2).to_broadcast([128, 8, 64]), op=ALU.mult),
                     r=["osb", "ssq"], w=["osb"])
                P.op("dve", lambda e: e.tensor_tensor(out=yat, in0=osb.rearrange("p h d -> p (h d)"), in1=aog_bc, op=ALU.mult),
                     r=["osb", "rows_sb"], w=["yat"])
                ptb = psb[s % 2][:, 0:512]
                hptr = f"psb{s % 2}"
                for i in range(4):
                    P.op("pe", lambda e, ptb=ptb, i=i: e.transpose(out=ptb[:, i * 128:(i + 1) * 128], in_=yat[:, i * 128:(i + 1) * 128],
                                                                  identity=identb), r=["yat", "identb"], w=[hptr])
                P.op("act", lambda e, ptb=ptb, qs=qs: e.activation(
                    out=ycT[:, 4:8, qs], in_=ptb[:, 0:512].rearrange("p (i q) -> p i q", i=4), func=AF.Identity),
                    r=[hptr], w=["ycT"])
            stop_at(5)
            for s in range(4):
                tt = mt * 4 + s
                P.op("sp", lambda e, tt=tt: e.dma_start(out=xr, in_=x_d[tt * 128:(tt + 1) * 128, :]), w=["xr"], dma=True)
                P.op("pool", lambda e: e.tensor_scalar(out=xr, in0=xr, scalar1=ALPHA, scalar2=None, op0=ALU.mult), r=["xr"], w=["xr"])
                P.op("pool", lambda e: e.tensor_tensor(out=xr, in0=xr, in1=gb, op=ALU.add), r=["xr", "gb"], w=["xr"])
                for h in range(2):
                    po, hpo = nps()
                    for c in range(8):
                        P.op("pe", lambda e, po=po, c=c, s=s, h=h: e.matmul(
                            po[:], lhsT=ycT[:, c, s * 128:(s + 1) * 128], rhs=wout[:, c, h * 512:(h + 1) * 512],
                            start=(c == 0), stop=(c == 7)), r=["ycT"] + WOUTH, w=[hpo])
                    P.op("dve", lambda e, po=po, h=h: e.tensor_tensor(out=rr[:, h * 512:(h + 1) * 512], in0=po[:],
                                                                      in1=xr[:, h * 512:(h + 1) * 512], op=ALU.add),
                         r=[hpo, "xr"], w=["rr"])
                for h in range(2):
                    P.op("dve", lambda e, h=h: e.bn_stats(out=bnst[:, h * 6:(h + 1) * 6], in_=rr[:, h * 512:(h + 1) * 512]),
                         r=["rr"], w=["bnst"])
                P.op("dve", lambda e: e.bn_aggr(out=bnag[:, 0:2], in_=bnst), r=["bnst"], w=["bnag"])
                P.op("act", lambda e: e.activation(out=bnag[:, 2:3], in_=bnag[:, 1:2], func=AF.Sqrt, bias=EPSC, scale=1.0),
                     r=["bnag", "epsc"], w=["bnag2"])
                P.op("dve", lambda e: e.reciprocal(out=bnag[:, 2:3], in_=bnag[:, 2:3]), r=["bnag2"], w=["bnag2"])
                P.op("dve", lambda e: e.scalar_tensor_tensor(out=bnag[:, 3:4], in0=bnag[:, 0:1], scalar=-1.0, in1=bnag[:, 2:3],
                                                             op0=ALU.mult, op1=ALU.mult), r=["bnag", "bnag2"], w=["bnag3"])
                P.op("act", lambda e: e.activation(out=zz, in_=rr, func=AF.Identity, bias=bnag[:, 3:4], scale=bnag[:, 2:3]),
                     r=["rr", "bnag2", "bnag3"], w=["zz"])
                P.op("sp", lambda e, tt=tt: e.dma_start(out=z_d[tt * 128:(tt + 1) * 128, :], in_=zz), r=["zz"], w=[f"z_d{tt}"], dma=True)

        P.fence()
        if dbg and dbg["what"] == "z":
            nr = _CACHE.get('nmt_run', NMT) * 512
            P.op("sp", lambda e: e.dma_start(out=dbg_d[0:nr, :], in_=z_d[0:nr, :]), r=[f"z_d{i}" for i in range(nr // 128)], w=["dbg"], dma=True)
            P.fence()
            P.emit()
            return nc

        try:
            build_phase2(nc, P, A, ps, nps, dr, out_d, z_d, xs_d, ys_d, dbg, dbg_d, mark_persist,
                     dict(cols=cols, cst=cst, identf=identf, identb=identb, onesb=onesb, Ub=Ub, modc=modc, gbc=gbc,
                              EPSC=EPSC, psb=psb))
        except _StopEmit:
            pass
        P.fence()
        P.emit()
    return nc


def build_phase2(nc, P, A, ps, nps, dr, out_d, z_d, xs_d, ys_d, dbg, dbg_d, mark, K):
    cols, identf, identb, onesb, Ub, gbc, EPSC, psb = (K[k] for k in ("cols", "identf", "identb", "onesb", "Ub", "gbc", "EPSC", "psb"))
    rows_d, wr_d, wg_d, wu_d, wd_d = dr["rows"], dr["w_r"], dr["wg"], dr["wu"], dr["wd"]
    A.off = mark
    lnr = A.alloc([4, D], F32)
    misc = A.alloc([36 + 64], F32)
    A2 = A.alloc([D], F32)
    B2 = A.alloc([D], F32)
    GA = A.alloc([D], F32)
    BA = A.alloc([D], F32)
    wr = A.alloc([8, 36], F32)
    pos_i = A.alloc([2, NT], I32)
    wts = A.alloc([2, NT], F32)
    widx_i = A.alloc([64], I32)
    mark2 = A.off
    br_bc = misc[:, 0:36]
    slot_bc = misc[:, 36:36 + NS]
    P.op("sp", lambda e: e.dma_start(out=lnr.rearrange("p a d -> p (a d)"), in_=rows_d[0:1, R_L1G:R_L1G + 4 * D].partition_broadcast(128)),
         w=["lnr"], dma=True)
    P.op("sp", lambda e: e.dma_start(out=misc, in_=rows_d[0:1, R_BR:R_BR + 100].partition_broadcast(128)), w=["misc"], dma=True)
    P.op("sp", lambda e: e.dma_start(out=wr, in_=wr_d.rearrange("(c p) n -> p c n", p=128)), w=["wr"], dma=True)
    G2H = ["gbc1_0", "gbc1_1", "gbc2_0", "gbc2_1", "gbc3_0", "gbc3_1"]
    P.op("dve", lambda e: e.scalar_tensor_tensor(out=A2, in0=gbc[:, 2, :], scalar=1.0, in1=lnr[:, 0, :], op0=ALU.add, op1=ALU.mult),
         r=["lnr"] + G2H, w=["A2"])
    P.op("dve", lambda e: e.scalar_tensor_tensor(out=B2, in0=gbc[:, 2, :], scalar=1.0, in1=lnr[:, 1, :], op0=ALU.add, op1=ALU.mult),
         r=["lnr"] + G2H, w=["B2"])
    P.op("dve", lambda e: e.tensor_tensor(out=B2, in0=B2, in1=gbc[:, 1, :], op=ALU.add), r=["B2"] + G2H, w=["B2"])
    P.op("dve", lambda e: e.tensor_scalar(out=GA, in0=lnr[:, 0, :], scalar1=ALPHA, scalar2=None, op0=ALU.mult), r=["lnr"], w=["GA"])
    P.op("dve", lambda e: e.tensor_scalar(out=BA, in0=lnr[:, 1, :], scalar1=ALPHA, scalar2=None, op0=ALU.mult), r=["lnr"], w=["BA"])

    h2b = A.alloc([NT, D], BF16)
    logits = A.alloc([NT, 36], F32)
    zt = [A.alloc([D], F32) for _ in range(2)]
    h2f = A.alloc([D], F32)
    h2T = A.alloc([8, 128], F32)
    for j in range(NT):
        z_, hz = zt[j % 2], f"zt{j % 2}"
        P.op("sp", lambda e, j=j, z_=z_: e.dma_start(out=z_, in_=z_d[j * 128:(j + 1) * 128, :]), r=[f"z_d{j}"], w=[hz], dma=True)
        P.op("dve", lambda e, z_=z_: e.tensor_tensor(out=h2f, in0=z_, in1=A2, op=ALU.mult), r=[hz, "A2"], w=["h2f"])
        P.op("dve", lambda e: e.tensor_tensor(out=h2f, in0=h2f, in1=B2, op=ALU.add), r=["h2f", "B2"], w=["h2f"])
        P.op("act", lambda e, j=j: e.activation(out=h2b[:, j, :], in_=h2f, func=AF.Identity), r=["h2f"], w=[f"h2b{j}"])
        for hh in range(2):
            pt, hp = nps()
            for c4 in range(4):
                c = hh * 4 + c4
                P.op("pe", lambda e, pt=pt, c=c, c4=c4: e.transpose(out=pt[:, c4 * 128:(c4 + 1) * 128],
                                                                  in_=h2f[:, c * 128:(c + 1) * 128], identity=identf),
                     r=["h2f", "cst"], w=[hp])
            if hh == 0:
                P.op("act", lambda e, pt=pt, hh=hh: e.activation(out=h2T[:, hh * 4:(hh + 1) * 4, :].rearrange("p c t -> p (c t)"),
                                                                 in_=pt[:], func=AF.Identity), r=[hp], w=["h2T"])
            else:
                P.op("dve", lambda e, pt=pt, hh=hh: e.tensor_copy(out=h2T[:, hh * 4:(hh + 1) * 4, :].rearrange("p c t -> p (c t)"),
                                                                  in_=pt[:]), r=[hp], w=["h2T"])
        pl, hpl = nps()
        for c in range(8):
            P.op("pe", lambda e, pl=pl, c=c: e.matmul(pl[:, 0:36], lhsT=h2T[:, c, :], rhs=wr[:, c, :], start=(c == 0), stop=(c == 7)),
                 r=["h2T", "wr"], w=[hpl])
        P.op("dve", lambda e, pl=pl, j=j: e.tensor_tensor(out=logits[:, j, :], in0=pl[:, 0:36], in1=br_bc, op=ALU.add),
             r=[hpl, "misc"], w=["logits"])

    def T3(n):
        return A.alloc([NT, n], F32)
    gmax = A.alloc([NT], F32)
    og = T3(4)
    eg = T3(4)
    sgm = A.alloc([NT], F32)
    ptop = A.alloc([NT], F32)
    tmp4 = A.alloc([NT, 4, 8], F32)
    sel = T3(8)
    sel2 = T3(8)
    m1 = A.alloc([NT], F32)
    m2 = A.alloc([NT], F32)
    o1 = T3(8)
    o2 = T3(8)
    e2 = A.alloc([NT], F32)
    r12 = A.alloc([NT], F32)
    O1 = A.alloc([NT, 4, 8], F32)
    O2 = A.alloc([NT, 4, 8], F32)
    Obf = A.alloc([NT * 32], BF16)
    totA = A.alloc([NT, 32], F32)
    totB = A.alloc([NT, 32], F32)
    tot0 = A.alloc([NT, 32], F32)
    base = A.alloc([NT, 32], F32)
    cnt = A.alloc([32], F32)
    cmpc = A.alloc([32, 24], F32)
    pcnt = A.alloc([32], F32)
    oeA = A.alloc([32], F32)
    oeB = A.alloc([32], F32)
    offs = A.alloc([32], F32)
    cmps = A.alloc([NS, 32], F32)
    esl = A.alloc([64], F32)
    used = A.alloc([64], F32)
    posf = A.alloc([2, NT], F32)

    LG = logits[:, :, 0:4]
    LE4 = logits[:, :, 4:36].rearrange("p j (g e) -> p j g e", g=4)

    def dv(fn, r, w):
        P.op("dve", fn, r=r, w=w)

    dv(lambda e: e.tensor_reduce(out=gmax, in_=LG, axis=AX.X, op=ALU.max), ["logits"], ["gmax"])
    dv(lambda e: e.tensor_tensor(out=og, in0=LG, in1=gmax.unsqueeze(2).to_broadcast([128, NT, 4]), op=ALU.is_equal), ["logits", "gmax"], ["og"])
    dv(lambda e: e.tensor_tensor(out=eg, in0=LG, in1=gmax.unsqueeze(2).to_broadcast([128, NT, 4]), op=ALU.subtract), ["logits", "gmax"], ["eg"])
    P.op("act", lambda e: e.activation(out=eg, in_=eg, func=AF.Exp), r=["eg"], w=["eg"])
    dv(lambda e: e.tensor_reduce(out=sgm, in_=eg, axis=AX.X, op=ALU.add), ["eg"], ["sgm"])
    dv(lambda e: e.reciprocal(out=ptop, in_=sgm), ["sgm"], ["ptop"])
    dv(lambda e: e.tensor_tensor(out=tmp4, in0=LE4, in1=og.unsqueeze(3).to_broadcast([128, NT, 4, 8]), op=ALU.mult), ["logits", "og"], ["tmp4"])
    dv(lambda e: e.tensor_reduce(out=sel, in_=tmp4.rearrange("p j g e -> p j e g"), axis=AX.X, op=ALU.add), ["tmp4"], ["sel"])
    dv(lambda e: e.tensor_reduce(out=m1, in_=sel, axis=AX.X, op=ALU.max), ["sel"], ["m1"])
    dv(lambda e: e.tensor_tensor(out=o1, in0=sel, in1=m1.unsqueeze(2).to_broadcast([128, NT, 8]), op=ALU.is_equal), ["sel", "m1"], ["o1"])
    dv(lambda e: e.scalar_tensor_tensor(out=sel2.rearrange("p j e -> p (j e)"), in0=o1.rearrange("p j e -> p (j e)"), scalar=-1.0e9,
                                        in1=sel.rearrange("p j e -> p (j e)"), op0=ALU.mult, op1=ALU.add), ["o1", "sel"], ["sel2"])
    dv(lambda e: e.tensor_reduce(out=m2, in_=sel2, axis=AX.X, op=ALU.max), ["sel2"], ["m2"])
    dv(lambda e: e.tensor_tensor(out=o2, in0=sel2, in1=m2.unsqueeze(2).to_broadcast([128, NT, 8]), op=ALU.is_equal), ["sel2", "m2"], ["o2"])
    dv(lambda e: e.tensor_tensor(out=e2, in0=m2, in1=m1, op=ALU.subtract), ["m1", "m2"], ["e2"])
    P.op("act", lambda e: e.activation(out=e2, in_=e2, func=AF.Exp), r=["e2"], w=["e2"])
    dv(lambda e: e.tensor_scalar(out=r12, in0=e2, scalar1=1.0, scalar2=None, op0=ALU.add), ["e2"], ["r12"])
    dv(lambda e: e.reciprocal(out=r12, in_=r12), ["r12"], ["r12"])
    dv(lambda e: e.tensor_tensor(out=wts[:, 0, :], in0=r12, in1=ptop, op=ALU.mult), ["r12", "ptop"], ["wts"])
    dv(lambda e: e.tensor_tensor(out=wts[:, 1, :], in0=wts[:, 0, :], in1=e2, op=ALU.mult), ["wts", "e2"], ["wts"])
    dv(lambda e: e.tensor_tensor(out=O1, in0=og.unsqueeze(3).to_broadcast([128, NT, 4, 8]),
                                 in1=o1.unsqueeze(2).to_broadcast([128, NT, 4, 8]), op=ALU.mult), ["og", "o1"], ["O1"])
    dv(lambda e: e.tensor_tensor(out=O2, in0=og.unsqueeze(3).to_broadcast([128, NT, 4, 8]),
                                 in1=o2.unsqueeze(2).to_broadcast([128, NT, 4, 8]), op=ALU.mult), ["og", "o2"], ["O2"])
    O1f = O1.rearrange("p j g e -> p (j g e)")
    O2f = O2.rearrange("p j g e -> p (j g e)")
    dv(lambda e: e.tensor_tensor(out=Obf, in0=O1f, in1=O2f, op=ALU.add), ["O1", "O2"], ["Obf"])
    pcs, pts = [], []
    for h in range(2):
        pc_, hpc = nps()
        P.op("pe", lambda e, pc_=pc_, h=h: e.matmul(pc_[:], lhsT=Ub, rhs=Obf[:, h * 512:(h + 1) * 512], start=True, stop=True),
             r=["Ub", "Obf"], w=[hpc])
        pcs.append((pc_, hpc))
        pt_, hpt = nps()
        P.op("pe", lambda e, pt_=pt_, h=h: e.matmul(pt_[:], lhsT=onesb, rhs=Obf[:, h * 512:(h + 1) * 512], start=True, stop=True),
             r=["onesb", "Obf"], w=[hpt])
        pts.append((pt_, hpt))
    tot0f = tot0.rearrange("p j e -> p (j e)")
    for h in range(2):
        dv(lambda e, h=h: e.tensor_copy(out=tot0f[:, h * 512:(h + 1) * 512], in_=pts[h][0][:]), [pts[h][1]], ["tot0"])
    cur, hc = tot0, "tot0"
    for i_, sft in enumerate((1, 2, 4, 8, 16)):
        nxt, hn = (totA, "totA") if i_ % 2 == 0 else (totB, "totB")
        dv(lambda e, cur=cur, nxt=nxt, sft=sft: e.tensor_tensor(out=nxt[:, sft:, :], in0=cur[:, sft:, :], in1=cur[:, :NT - sft, :], op=ALU.add),
           [hc], [hn])
        dv(lambda e, cur=cur, nxt=nxt, sft=sft: e.tensor_copy(out=nxt[:, :sft, :], in_=cur[:, :sft, :]), [hc, hn], [hn])
        cur, hc = nxt, hn
    incl, hincl = cur, hc
    dv(lambda e: e.tensor_copy(out=cnt, in_=incl[:, NT - 1, :]), [hincl], ["cnt"])
    dv(lambda e: e.tensor_tensor(out=cmpc, in0=cnt.unsqueeze(2).to_broadcast([128, 32, 24]),
                                 in1=slot_bc[:, 0:24].unsqueeze(1).to_broadcast([128, 32, 24]), op=ALU.is_gt), ["cnt", "misc"], ["cmpc"])
    dv(lambda e: e.tensor_reduce(out=pcnt, in_=cmpc, axis=AX.X, op=ALU.add), ["cmpc"], ["pcnt"])
    dv(lambda e: e.tensor_scalar(out=pcnt, in0=pcnt, scalar1=float(T), scalar2=None, op0=ALU.mult), ["pcnt"], ["pcnt"])
    cur, hc = pcnt, "pcnt"
    for i_, sft in enumerate((1, 2, 4, 8, 16)):
        nxt, hn = (oeA, "oeA") if i_ % 2 == 0 else (oeB, "oeB")
        dv(lambda e, cur=cur, nxt=nxt, sft=sft: e.tensor_tensor(out=nxt[:, sft:], in0=cur[:, sft:], in1=cur[:, :32 - sft], op=ALU.add), [hc], [hn])
        dv(lambda e, cur=cur, nxt=nxt, sft=sft: e.tensor_copy(out=nxt[:, :sft], in_=cur[:, :sft]), [hc, hn], [hn])
        cur, hc = nxt, hn
    oend, hoend = cur, hc
    dv(lambda e: e.tensor_tensor(out=offs, in0=oend, in1=pcnt, op=ALU.subtract), [hoend, "pcnt"], ["offs"])
    dv(lambda e: e.tensor_tensor(out=base, in0=incl, in1=tot0, op=ALU.subtract), [hincl, "tot0"], ["base"])
    dv(lambda e: e.tensor_tensor(out=base, in0=base, in1=offs.unsqueeze(1).to_broadcast([128, NT, 32]), op=ALU.add), ["base", "offs"], ["base"])
    basef = base.rearrange("p j e -> p (j e)")
    for h in range(2):
        dv(lambda e, h=h: e.tensor_tensor(out=basef[:, h * 512:(h + 1) * 512], in0=pcs[h][0][:], in1=basef[:, h * 512:(h + 1) * 512], op=ALU.add),
           [pcs[h][1], "base"], ["base"])
    for k, (Ok, hO) in enumerate(((O1, "O1"), (O2, "O2"))):
        dv(lambda e, Ok=Ok: e.tensor_tensor(out=Ok.rearrange("p j g e -> p (j g e)"), in0=Ok.rearrange("p j g e -> p (j g e)"), in1=basef, op=ALU.mult),
           [hO, "base"], [hO])
        dv(lambda e, Ok=Ok, k=k: e.tensor_reduce(out=posf[:, k, :], in_=Ok.rearrange("p j g e -> p j (g e)"), axis=AX.X, op=ALU.add), [hO], ["posf"])
    dv(lambda e: e.tensor_copy(out=pos_i, in_=posf), ["posf"], ["pos_i"])
    dv(lambda e: e.tensor_tensor(out=cmps, in0=oend.unsqueeze(1).to_broadcast([128, NS, 32]),
                                 in1=slot_bc.unsqueeze(2).to_broadcast([128, NS, 32]), op=ALU.is_le), [hoend, "misc"], ["cmps"])
    dv(lambda e: e.tensor_reduce(out=esl[:, 0:NS], in_=cmps, axis=AX.X, op=ALU.add), ["cmps"], ["esl"])
    dv(lambda e: e.tensor_scalar(out=esl[:, 0:NS], in0=esl[:, 0:NS], scalar1=float(NE - 1), scalar2=128.0, op0=ALU.min, op1=ALU.mult), ["esl"], ["esl"])
    dv(lambda e: e.tensor_scalar(out=used[:, 0:NS], in0=slot_bc, scalar1=oend[:, 31:32], scalar2=None, op0=ALU.is_lt), ["misc", hoend], ["used"])
    dv(lambda e: e.tensor_scalar(out=used[:, 0:NS], in0=used[:, 0:NS], scalar1=-1.0e6, scalar2=1.0e6, op0=ALU.mult, op1=ALU.add), ["used"], ["used"])
    dv(lambda e: e.tensor_tensor(out=esl[:, 0:NS], in0=esl[:, 0:NS], in1=used[:, 0:NS], op=ALU.add), ["esl", "used"], ["esl"])
    dv(lambda e: e.tensor_scalar(out=esl[:, 0:NS], in0=esl[:, 0:NS], scalar1=cols[:, C_PID:C_PID + 1], scalar2=None, op0=ALU.add), ["esl", "cols"], ["esl"])
    dv(lambda e: e.tensor_copy(out=widx_i[:, 0:NS], in_=esl[:, 0:NS]), ["esl"], ["widx_i"])

    if dbg and dbg["what"] == "route":
        P.fence()
        P.op("sp", lambda e: e.dma_start(out=dbg_d[0:128, 0:NT * 36], in_=logits.rearrange("p j n -> p (j n)")), r=["logits"], w=["dbg0"], dma=True)
        P.op("sp", lambda e: e.dma_start(out=dbg_d[128:256, 0:2 * NT], in_=posf.rearrange("p k j -> p (k j)")), r=["posf"], w=["dbg1"], dma=True)
        P.op("sp", lambda e: e.dma_start(out=dbg_d[256:384, 0:2 * NT], in_=wts.rearrange("p k j -> p (k j)")), r=["wts"], w=["dbg2"], dma=True)
        P.op("sp", lambda e: e.dma_start(out=dbg_d[384:512, 0:NS], in_=esl[:, 0:NS]), r=["esl"], w=["dbg3"], dma=True)
        P.fence()
        raise _StopEmit()

    for j in range(NT):
        for k in range(2):
            P.op("pool", lambda e, j=j, k=k: e.indirect_dma_start(
                out=xs_d[:, :], out_offset=bass.IndirectOffsetOnAxis(ap=pos_i[:, k, j:j + 1], axis=0),
                in_=h2b[:, j, :], in_offset=None), r=[f"h2b{j}", "pos_i"], w=[f"xs_{j}_{k}"], dma=True)
    P.fence()

    A.off = mark2
    wgs = [A.alloc([2048], BF16) for _ in range(2)]
    wus = [A.alloc([2048], BF16) for _ in range(2)]
    wds = [A.alloc([2048], BF16) for _ in range(2)]
    xtok = [A.alloc([NSUB, D], BF16) for _ in range(2)]
    XT = [A.alloc([8, T], BF16) for _ in range(2)]
    sgs = [A.alloc([T], F32) for _ in range(2)]
    aT = [A.alloc([2, T], BF16) for _ in range(2)]
    yo = [A.alloc([D], F32) for _ in range(2)]
    _bc = {}

    def get_bc(e):
        if "v" not in _bc:
            reg = e.alloc_register("bcreg")
            e.reg_mov(reg, NE * 128 - 1)
            _bc["v"] = e.snap(reg, donate=True)
        return _bc["v"]

    def issue_loads(s):
        b = s % 2
        for (wsb, wdr, hn) in ((wgs[b], wg_d, f"wg{b}"), (wus[b], wu_d, f"wu{b}"), (wds[b], wd_d, f"wd{b}")):
            P.op("pool", lambda e, wsb=wsb, wdr=wdr, s=s: e.indirect_dma_start(
                out=wsb, out_offset=None, in_=wdr[:, :],
                in_offset=bass.IndirectOffsetOnAxis(ap=widx_i[:, s:s + 1], axis=0),
                bounds_check=get_bc(e), oob_is_err=False), r=["widx_i"], w=[hn], dma=True)
        for st in range(NSUB):
            r0 = s * T + st * 128
            P.op("sp", lambda e, b=b, st=st, r0=r0: e.dma_start(out=xtok[b][:, st, :], in_=xs_d[r0:r0 + 128, :]), w=[f"xtok{b}_{st}"], dma=True)

    issue_loads(0)
    for s in range(NS):
        b = s % 2
        if s + 1 < NS:
            issue_loads(s + 1)
        for st in range(NSUB):
            pb_ = psb[(s * NSUB + st) % 2]
            hpb = f"psb{(s * NSUB + st) % 2}"
            for c in range(8):
                P.op("pe", lambda e, pb_=pb_, b=b, st=st, c=c: e.transpose(out=pb_[:, c * 128:(c + 1) * 128],
                                                                       in_=xtok[b][:, st, c * 128:(c + 1) * 128], identity=identb),
                     r=[f"xtok{b}_{st}", "identb"], w=[hpb])
            eng = "act" if st % 2 == 0 else "dve"
            if eng == "act":
                P.op("act", lambda e, pb_=pb_, b=b, st=st: e.activation(out=XT[b][:, :, st * 128:(st + 1) * 128],
                                                                        in_=pb_[:].rearrange("p (c t) -> p c t", c=8), func=AF.Identity),
                     r=[hpb], w=[f"XT{b}"])
            else:
                P.op("dve", lambda e, pb_=pb_, b=b, st=st: e.tensor_copy(out=XT[b][:, :, st * 128:(st + 1) * 128],
                                                                         in_=pb_[:].rearrange("p (c t) -> p c t", c=8)),
                     r=[hpb], w=[f"XT{b}"])
        for fch in range(2):
            pg, hpg = nps()
            for c in range(8):
                P.op("pe", lambda e, pg=pg, b=b, c=c, fch=fch: e.matmul(
                    pg[:, 0:T], lhsT=wgs[b][:, c * 256 + fch * 128:c * 256 + (fch + 1) * 128], rhs=XT[b][:, c, :],
                    start=(c == 0), stop=(c == 7)), r=[f"wg{b}", f"XT{b}"], w=[hpg])
            pu, hpu = nps()
            for c in range(8):
                P.op("pe", lambda e, pu=pu, b=b, c=c, fch=fch: e.matmul(
                    pu[:, 0:T], lhsT=wus[b][:, c * 256 + fch * 128:c * 256 + (fch + 1) * 128], rhs=XT[b][:, c, :],
                    start=(c == 0), stop=(c == 7)), r=[f"wu{b}", f"XT{b}"], w=[hpu])
            P.op("act", lambda e, pg=pg, b=b: e.activation(out=sgs[b], in_=pg[:, 0:T], func=AF.Silu), r=[hpg], w=[f"sgs{b}"])
            P.op("dve", lambda e, pu=pu, b=b, fch=fch: e.tensor_tensor(out=aT[b][:, fch, :], in0=pu[:, 0:T], in1=sgs[b], op=ALU.mult),
                 r=[hpu, f"sgs{b}"], w=[f"aT{b}"])
        for st in range(NSUB):
            yb = (s * NSUB + st) % 2
            for half in range(2):
                po, hpo = nps()
                for fch in range(2):
                    P.op("pe", lambda e, po=po, b=b, st=st, fch=fch, half=half: e.matmul(
                        po[:], lhsT=aT[b][:, fch, st * 128:(st + 1) * 128],
                        rhs=wds[b][:, fch * 1024 + half * 512:fch * 1024 + (half + 1) * 512], start=(fch == 0), stop=(fch == 1)),
                        r=[f"aT{b}", f"wd{b}"], w=[hpo])
                if half == 0:
                    P.op("act", lambda e, po=po, yb=yb: e.activation(out=yo[yb][:, 0:512], in_=po[:], func=AF.Identity), r=[hpo], w=[f"yo{yb}a"])
                else:
                    P.op("dve", lambda e, po=po, yb=yb: e.tensor_copy(out=yo[yb][:, 512:1024], in_=po[:]), r=[hpo], w=[f"yo{yb}b"])
            r0 = s * T + st * 128
            P.op("sp", lambda e, yb=yb, r0=r0: e.dma_start(out=ys_d[r0:r0 + 128, :], in_=yo[yb]), r=[f"yo{yb}a", f"yo{yb}b"],
                 w=[f"ys_{s}_{st}"], dma=True)
    P.fence()

    A.off = mark2
    Y1 = [A.alloc([D], F32) for _ in range(2)]
    Y2 = [A.alloc([D], F32) for _ in range(2)]
    zf = [A.alloc([D], F32) for _ in range(2)]
    t1 = [A.alloc([D], F32) for _ in range(2)]
    ff = A.alloc([D], F32)
    r2 = A.alloc([D], F32)
    ob = [A.alloc([D], F32) for _ in range(2)]
    bn2 = A.alloc([12], F32)
    ag2 = A.alloc([4], F32)
    for j in range(NT):
        b = j % 2
        P.op("pool", lambda e, j=j, b=b: e.indirect_dma_start(
            out=Y1[b], out_offset=None, in_=ys_d[:, :], in_offset=bass.IndirectOffsetOnAxis(ap=pos_i[:, 0, j:j + 1], axis=0)),
            r=["pos_i"], w=[f"Y1{b}"], dma=True)
        P.op("pool", lambda e, j=j, b=b: e.indirect_dma_start(
            out=Y2[b], out_offset=None, in_=ys_d[:, :], in_offset=bass.IndirectOffsetOnAxis(ap=pos_i[:, 1, j:j + 1], axis=0)),
            r=["pos_i"], w=[f"Y2{b}"], dma=True)
        P.op("sp", lambda e, j=j, b=b: e.dma_start(out=zf[b], in_=z_d[j * 128:(j + 1) * 128, :]), r=[f"z_d{j}"], w=[f"zf{b}"], dma=True)
        P.op("pool", lambda e, b=b: e.tensor_tensor(out=t1[b], in0=zf[b], in1=GA, op=ALU.mult), r=[f"zf{b}", "GA"], w=[f"t1{b}"])
        P.op("pool", lambda e, b=b: e.tensor_tensor(out=t1[b], in0=t1[b], in1=BA, op=ALU.add), r=[f"t1{b}", "BA"], w=[f"t1{b}"])
        P.op("act", lambda e, j=j, b=b: e.activation(out=ff, in_=Y1[b], func=AF.Identity, scale=wts[:, 0, j:j + 1]), r=[f"Y1{b}", "wts"], w=["ff"])
        P.op("dve", lambda e, j=j, b=b: e.scalar_tensor_tensor(out=ff, in0=Y2[b], scalar=wts[:, 1, j:j + 1], in1=ff, op0=ALU.mult, op1=ALU.add),
             r=[f"Y2{b}", "wts", "ff"], w=["ff"])
        P.op("dve", lambda e: e.tensor_tensor(out=ff, in0=ff, in1=gbc[:, 3, :], op=ALU.mult), r=["ff", "gbc3_0", "gbc3_1"], w=["ff"])
        P.op("dve", lambda e, b=b: e.tensor_tensor(out=r2, in0=ff, in1=t1[b], op=ALU.add), r=["ff", f"t1{b}"], w=["r2"])
        for h in range(2):
            P.op("dve", lambda e, h=h: e.bn_stats(out=bn2[:, h * 6:(h + 1) * 6], in_=r2[:, h * 512:(h + 1) * 512]), r=["r2"], w=["bn2"])
        P.op("dve", lambda e: e.bn_aggr(out=ag2[:, 0:2], in_=bn2), r=["bn2"], w=["ag2"])
        P.op("act", lambda e: e.activation(out=ag2[:, 2:3], in_=ag2[:, 1:2], func=AF.Sqrt, bias=EPSC, scale=1.0), r=["ag2", "epsc"], w=["ag2b"])
        P.op("dve", lambda e: e.reciprocal(out=ag2[:, 2:3], in_=ag2[:, 2:3]), r=["ag2b"], w=["ag2b"])
        P.op("dve", lambda e: e.scalar_tensor_tensor(out=ag2[:, 3:4], in0=ag2[:, 0:1], scalar=-1.0, in1=ag2[:, 2:3], op0=ALU.mult, op1=ALU.mult),
             r=["ag2", "ag2b"], w=["ag2c"])
        P.op("act", lambda e, b=b: e.activation(out=ob[b], in_=r2, func=AF.Identity, bias=ag2[:, 3:4], scale=ag2[:, 2:3]),
             r=["r2", "ag2b", "ag2c"], w=[f"ob{b}"])
        P.op("pool", lambda e, b=b: e.tensor_tensor(out=ob[b], in0=ob[b], in1=lnr[:, 2, :], op=ALU.mult), r=[f"ob{b}", "lnr"], w=[f"ob{b}"])
        P.op("pool", lambda e, b=b: e.tensor_tensor(out=ob[b], in0=ob[b], in1=lnr[:, 3, :], op=ALU.add), r=[f"ob{b}", "lnr"], w=[f"ob{b}"])
        P.op("sp", lambda e, j=j, b=b: e.dma_start(out=out_d[j * 128:(j + 1) * 128, :], in_=ob[b]), r=[f"ob{b}"], w=[f"out{j}"], dma=True)


class _StopEmit(Exception):
    pass


def _host_prep(inp, b):
    f = np.float32
    L = 0
    cols = np.zeros((128, NCOL), f)
    cols[:, C_C:C_C + 8] = inp["c"][b].reshape(8, 128).T
    w_in = inp["w_in"][L]
    b_in = inp["b_in"][L]
    qcols = np.concatenate([np.arange(1024 + h * 64, 1024 + (h + 1) * 64) for h in HORDER])
    perm = np.concatenate([np.arange(0, 1024), qcols, np.arange(1536, 1792)])
    w_in_p = np.ascontiguousarray(w_in[:, perm])
    b_in_p = b_in[perm]
    cols[:, C_BIN:C_BIN + 13] = b_in_p[:1664].reshape(13, 128).T
    cols[:, C_CW:C_CW + 124] = inp["conv_w"][L].T.reshape(4, 128, 31).transpose(1, 0, 2).reshape(128, 124)
    cols[:, C_CB:C_CB + 4] = inp["conv_b"][L].reshape(4, 128).T
    cols[:, C_LG:C_LG + 4] = inp["conv_ln_g"][L].reshape(4, 128).T
    cols[:, C_LB:C_LB + 4] = inp["conv_ln_b"][L].reshape(4, 128).T
    cols[:, C_OG:C_OG + 4] = inp["conv_out_g"][L].reshape(4, 128).T
    cols[:, C_PID] = np.arange(128)
    rows = np.zeros((1, NROW), f)
    rows[0, R_BADA:R_BADA + 6144] = inp["b_ada"][L]
    rows[0, R_BV:R_BV + 128] = b_in[1664:1792]
    rows[0, R_SINK:R_SINK + 8] = inp["sinks"][L][HORDER]
    rows[0, R_AOG:R_AOG + 512] = inp["attn_out_g"][L].reshape(8, 64)[HORDER].reshape(-1)
    rows[0, R_BOUT:R_BOUT + D] = inp["b_out"][L]
    rows[0, R_L1G:R_L1G + D] = inp["ln1_g"][L]
    rows[0, R_L1B:R_L1B + D] = inp["ln1_b"][L]
    rows[0, R_L2G:R_L2G + D] = inp["ln2_g"][L]
    rows[0, R_L2B:R_L2B + D] = inp["ln2_b"][L]
    rows[0, R_BR:R_BR + 4] = inp["b_router_group"][L]
    rows[0, R_BR + 4:R_BR + 36] = inp["b_router_expert"][L]
    rows[0, R_SLOT:R_SLOT + NS] = np.arange(NS) * T
    w_out = inp["w_out"][L]
    arows = np.concatenate([np.arange(512 + h * 64, 512 + (h + 1) * 64) for h in HORDER])
    w_out_p = np.ascontiguousarray(np.concatenate([w_out[:512], w_out[arows]], axis=0))
    w_r = np.ascontiguousarray(np.concatenate([inp["w_router_group"][L], inp["w_router_expert"][L]], axis=1))
    return dict(cols=cols, rows=rows, w_in=w_in_p, w_out=w_out_p, w_r=w_r)


def _consts():
    f = np.float32
    cst = np.zeros((128, NK), f)
    cst[:, K_ID:K_ID + 128] = np.eye(128)
    p = np.arange(128)
    cst[:, K_U:K_U + 128] = (p[:, None] < p[None, :])
    cst[:, K_BD:K_BD + 128] = ((p[:, None] // 64) == (p[None, :] // 64)) / 64.0
    m0 = np.where(p[:, None] > p[None, :], 0.0, NEG)
    m1 = np.where(p[:, None] <= p[None, :], 0.0, NEG)
    cst[:, K_M0:K_M0 + 512] = np.tile(m0, (1, 4))
    cst[:, K_M1:K_M1 + 512] = np.tile(m1, (1, 4))
    return cst


def _expert_layout(inp):
    L = 0
    wg = np.ascontiguousarray(inp["w_gate"][L].reshape(NE, 8, 128, DE).transpose(0, 2, 1, 3)).reshape(NE * 128, 2048)
    wu = np.ascontiguousarray(inp["w_up"][L].reshape(NE, 8, 128, DE).transpose(0, 2, 1, 3)).reshape(NE * 128, 2048)
    wd = np.ascontiguousarray(inp["w_down"][L].reshape(NE, 2, 128, D).transpose(0, 2, 1, 3)).reshape(NE * 128, 2048)
    return wg, wu, wd


_CACHE = {}


def kernel(**inputs):
    inp = {k: np.asarray(v) for k, v in inputs.items()}
    dbg = _CACHE.get("dbg")
    nc = build_program(dbg)
    cst = _consts()
    wg, wu, wd = _expert_layout(inp)
    w_ada = np.ascontiguousarray(inp["w_ada"][0])
    in_maps = []
    ncores = _CACHE.get("ncores", 8)
    for b in range(ncores):
        hp = _host_prep(inp, b)
        m = dict(x=np.ascontiguousarray(inp["x"][b]), cols=hp["cols"], rows=hp["rows"], cst=cst, w_ada=w_ada,
                 w_in=hp["w_in"], w_out=hp["w_out"], w_r=hp["w_r"], wg=wg, wu=wu, wd=wd)
        if _CACHE.get("p2only"):
            m["z_in"] = _CACHE["z_in"]
        in_maps.append(m)
    if _CACHE.get("trace"):
        res = run_bass_kernel_spmd(nc, in_maps, core_ids=list(range(ncores)), trace=True)
        print("EXEC_NS", res.exec_time_ns)
    else:
        res = run_bass_kernel_spmd(nc, in_maps, core_ids=list(range(ncores)))
    if dbg:
        return [np.asarray(r["dbg"]) for r in res.results]
    out = np.stack([np.asarray(r["out"]) for r in res.results], axis=0).astype(np.float32)
    return out
```

```python
import os
import numpy as np
import concourse.bass as bass
import concourse.mybir as mybir
from concourse.bass_utils import run_bass_kernel_spmd

F32 = mybir.dt.float32
BF16 = mybir.dt.bfloat16
I32 = mybir.dt.int32
ALU = mybir.AluOpType
AF = mybir.ActivationFunctionType
AX = mybir.AxisListType

D = 1024
S = 4096
NT = S // 128
NMT = S // 512
DIN = 1792
NE = 32
DE = 256
ALPHA = 2.0 ** 0.25
EPS = 1e-5
NEG = -30000.0
T = 384
NS = (2 * S + NE * (T - 1) + T - 1) // T
NSUB = T // 128
HORDER = [0, 4, 1, 5, 2, 6, 3, 7]

C_C = 0
C_BIN = 8
C_CW = 21
C_CB = 145
C_LG = 149
C_LB = 153
C_OG = 157
C_PID = 161
NCOL = 162
R_BADA = 0
R_BV = 6144
R_SINK = 6272
R_AOG = 6280
R_BOUT = 6792
R_L1G = 7816
R_L1B = 8840
R_L2G = 9864
R_L2B = 10888
R_BR = 11912
R_SLOT = 11948
NROW = 12012
K_ID = 0
K_U = 128
K_BD = 256
K_M0 = 384
K_M1 = 896
NK = 1408


class Prog:
    def __init__(self, nc, sems):
        self.nc = nc
        self.ops = []
        self.last_w = {}
        self.readers = {}
        self.eng_sem = {e: sems[i] for i, e in enumerate(["pe", "act", "dve", "pool"])}
        rest = sems[4:]
        n_sp = (len(rest) * 5) // 10
        n_pool = (len(rest) * 4) // 10
        self.dma_pool = {"sp": rest[:n_sp], "pool": rest[n_sp:n_sp + n_pool], "act": rest[n_sp + n_pool:]}
        self._rec = None

    def rec(self):
        assert self._rec is None
        self._rec = []

    def end(self):
        l = self._rec
        self._rec = None
        return l

    def play(self, lst):
        assert self._rec is None
        for o in lst:
            self.op(*o)

    def op(self, eng, fn, r=(), w=(), dma=False):
        if self._rec is not None:
            self._rec.append((eng, fn, list(r), list(w), dma))
            return None
        i = len(self.ops)
        w = list(w) + [h for h in r if h.startswith("ps") and h not in w]
        raw, oth = set(), set()
        for h in r:
            if h in self.last_w:
                raw.add(self.last_w[h])
        for h in w:
            if h in self.last_w:
                oth.add(self.last_w[h])
            for j in self.readers.get(h, ()):
                oth.add(j)
        for h in w:
            self.last_w[h] = i
            self.readers[h] = []
        for h in r:
            self.readers.setdefault(h, []).append(i)
        deps = []
        for j in sorted(raw | oth):
            p = self.ops[j]
            if j == i:
                continue
            if (not p["dma"]) and p["eng"] == eng:
                if eng == "pe" or j not in raw:
                    continue
            deps.append(j)
        self.ops.append(dict(eng=eng, fn=fn, deps=deps, dma=dma, sig=False))
        for j in deps:
            self.ops[j]["sig"] = True
        return i

    def fence(self, engs=("pe", "act", "dve", "pool", "sp")):
        hs = list(self.last_w.keys())
        for e in engs:
            self.op(e, None, r=hs, w=["_fence_" + e])

    def emit(self):
        nc = self.nc
        ticket = {e: 0 for e in self.eng_sem}
        dma_next = {q: 0 for q in self.dma_pool}
        dma_uses = {}
        for o in self.ops:
            if o["dma"]:
                q = o["eng"]
                pool = self.dma_pool[q]
                sem = pool[dma_next[q] % len(pool)]
                dma_next[q] += 1
                u = dma_uses.get(id(sem), 0)
                o["pre"] = (sem, 16 * u)
                dma_uses[id(sem)] = u + 1
                o["ev"] = (sem, 16 * (u + 1))
            elif o["sig"] and o["fn"] is not None:
                ticket[o["eng"]] += 1
                o["ev"] = (self.eng_sem[o["eng"]], ticket[o["eng"]])
        ops = self.ops

        def run(engname, eobj):
            waited = {}

            def wait(sem, val):
                if val <= 0:
                    return
                if waited.get(id(sem), 0) >= val:
                    return
                eobj.wait_ge(sem, val)
                waited[id(sem)] = val

            for o in ops:
                if o["eng"] != engname:
                    continue
                for j in o["deps"]:
                    ev = ops[j].get("ev")
                    if ev is not None:
                        wait(*ev)
                if o["fn"] is None:
                    continue
                if o["dma"]:
                    wait(*o["pre"])
                    ins = o["fn"](eobj)
                    ins.then_inc(o["ev"][0], 16)
                else:
                    ins = o["fn"](eobj)
                    if o["sig"]:
                        ins.then_inc(o["ev"][0], 1)

        with nc.Block() as block:
            @block.tensor
            def _(e):
                run("pe", e)

            @block.scalar
            def _(e):
                run("act", e)

            @block.vector
            def _(e):
                run("dve", e)

            @block.gpsimd
            def _(e):
                run("pool", e)

            @block.sync
            def _(e):
                run("sp", e)


class _Stop(Exception):
    pass


def merge(*lists):
    lists = [l for l in lists if l]
    out = []
    idx = [0] * len(lists)
    total = sum(len(l) for l in lists)
    while len(out) < total:
        best, bv = None, None
        for k, l in enumerate(lists):
            if idx[k] < len(l):
                v = (idx[k] + 0.5) / len(l)
                if bv is None or v < bv:
                    best, bv = k, v
        out.append(lists[best][idx[best]])
        idx[best] += 1
    return out


class Arena:
    def __init__(self, t, nbytes):
        self.t = t
        self.nbytes = nbytes
        self.off = 0

    def alloc(self, shape, dt):
        esz = 4 if dt in (F32, I32) else 2
        n = int(np.prod(shape)) * esz
        n = (n + 63) // 64 * 64
        assert self.off + n <= self.nbytes, ("arena overflow", self.off, n, self.nbytes)
        v = self.t[:, self.off // 4:(self.off + n) // 4]
        self.off += n
        if dt != F32:
            v = v.bitcast(dt)
        v = v[:, 0:int(np.prod(shape))]
        if len(shape) == 2:
            return v.rearrange("p (a b) -> p a b", a=shape[0])
        if len(shape) == 3:
            return v.rearrange("p (a b c) -> p a b c", a=shape[0], b=shape[1])
        return v


def build_program(dbg=None):
    nc = bass.Bass("TRN2", target_bir_lowering=False)
    try:
        return _build_program(nc, dbg)
    except _Stop:
        return nc


def _build_program(nc, dbg=None):
    dr = {}

    def din(name, shape, dt=F32):
        dr[name] = nc.dram_tensor(name, list(shape), dt, kind="ExternalInput").ap()
        return dr[name]

    x_d = din("x", [S, D])
    cols_d = din("cols", [128, NCOL])
    rows_d = din("rows", [1, NROW])
    cst_d = din("cst", [128, NK])
    wada_d = din("w_ada", [D, 6 * D])
    win_d = din("w_in", [D, DIN])
    wout_d = din("w_out", [D, D])
    wr_d = din("w_r", [D, 36])
    wg_d = din("wg", [NE * 128, 2048])
    wu_d = din("wu", [NE * 128, 2048])
    wd_d = din("wd", [NE * 128, 2048])
    out_d = nc.dram_tensor("out", [S, D], F32, kind="ExternalOutput").ap()
    if _CACHE.get("p2only"):
        z_d = din("z_in", [S, D])
    else:
        z_d = nc.dram_tensor("z_scr", [S, D], F32, kind="Internal").ap()
    xs_d = nc.dram_tensor("xs_scr", [NS * T, D], BF16, kind="Internal").ap()
    ys_d = nc.dram_tensor("ys_scr", [NS * T, D], F32, kind="Internal").ap()
    dbg_d = None
    if dbg:
        dbg_d = nc.dram_tensor("dbg", list(dbg["shape"]), F32, kind="ExternalOutput").ap()

    import contextlib
    with contextlib.ExitStack() as st:
        ARENA_BYTES = 206 * 1024
        arena_t = st.enter_context(nc.sbuf_tensor("arena", [128, ARENA_BYTES // 4], F32))
        ps = [st.enter_context(nc.psum_tensor(f"ps{i}", [128, 512], F32)) for i in range(6)]
        psb = [st.enter_context(nc.psum_tensor(f"psb{i}", [128, 1024], BF16)) for i in range(2)]
        sems = [st.enter_context(nc.semaphore(f"s{i}")) for i in range(_CACHE.get("nsem", 48))]
        P = Prog(nc, sems)
        A = Arena(arena_t, ARENA_BYTES)
        psn = [0]

        def nps():
            i = psn[0] % 6
            psn[0] += 1
            return ps[i], f"ps{i}"

        cols = A.alloc([NCOL], F32)
        cst = A.alloc([NK], F32)
        identf = cst[:, K_ID:K_ID + 128]
        identb = A.alloc([128], BF16)
        onesb = A.alloc([128], BF16)
        ones512 = A.alloc([128], BF16)
        bdb = A.alloc([128], BF16)
        Ub = A.alloc([128], BF16)
        maskT = A.alloc([2, 512], BF16)
        modc = A.alloc([32], F32)
        gbc = A.alloc([4, D], F32)
        EPSC = A.alloc([1], F32)
        mark_persist = A.off

        P.op("sp", lambda e: e.dma_start(out=cols, in_=cols_d), w=["cols"], dma=True)
        P.op("sp", lambda e: e.dma_start(out=cst, in_=cst_d), w=["cst"], dma=True)
        P.op("dve", lambda e: e.tensor_copy(out=identb, in_=identf), r=["cst"], w=["identb"])
        P.op("dve", lambda e: e.memset(onesb, 1.0), w=["onesb"])
        P.op("dve", lambda e: e.memset(EPSC, EPS), w=["epsc"])
        P.op("dve", lambda e: e.memset(ones512, 1.0 / 512.0), w=["ones512"])
        P.op("dve", lambda e: e.tensor_copy(out=bdb, in_=cst[:, K_BD:K_BD + 128]), r=["cst"], w=["bdb"])
        P.op("dve", lambda e: e.tensor_copy(out=Ub, in_=cst[:, K_U:K_U + 128]), r=["cst"], w=["Ub"])
        P.op("dve", lambda e: e.tensor_copy(out=maskT.rearrange("p a b -> p (a b)"), in_=cst[:, K_M0:K_M0 + 1024]),
             r=["cst"], w=["maskT"])

        ph0 = A.off
        cact = A.alloc([8], F32)
        cbc = A.alloc([8, 128], BF16)
        wab = [A.alloc([8, 512], BF16) for _ in range(2)]
        badab = A.alloc([512], F32)
        modb = A.alloc([4 * D], F32)
        P.op("act", lambda e: e.activation(out=cact, in_=cols[:, C_C:C_C + 8], func=AF.Silu), r=["cols"], w=["cact"])
        for c in range(8):
            P.op("dve", lambda e, c=c: e.tensor_scalar(out=cbc[:, c, :], in0=onesb, scalar1=cact[:, c:c + 1],
                                                       scalar2=None, op0=ALU.mult),
                 r=["cact", "onesb"], w=["cbc"])
        for blk in range(12):
            wb = wab[blk % 2]
            hw = f"wab{blk % 2}"
            P.op("pool", lambda e, blk=blk, wb=wb: e.dma_start(
                out=wb, in_=wada_d[:, blk * 512:(blk + 1) * 512].rearrange("(c p) n -> p c n", p=128)),
                w=[hw], dma=True)
            P.op("sp", lambda e, blk=blk: e.dma_start(
                out=badab, in_=rows_d[0:1, R_BADA + blk * 512:R_BADA + (blk + 1) * 512].partition_broadcast(128)),
                w=["badab"], dma=True)
            pt, hp = nps()
            for c in range(8):
                P.op("pe", lambda e, c=c, pt=pt, wb=wb: e.matmul(pt[:], lhsT=cbc[:, c, :], rhs=wb[:, c, :],
                                                               start=(c == 0), stop=(c == 7)),
                     r=["cbc", hw], w=[hp])
            if blk < 4:
                dst = modb[:, blk * 512:(blk + 1) * 512]
                hd = f"modb{blk}"
            else:
                gi = (blk - 4) // 2
                dst = gbc[:, gi, ((blk - 4) % 2) * 512:((blk - 4) % 2 + 1) * 512]
                hd = f"gbc{gi}_{blk % 2}"
            P.op("dve", lambda e, pt=pt, dst=dst: e.tensor_tensor(out=dst, in0=pt[:], in1=badab, op=ALU.add),
                 r=[hp, "badab"], w=[hd])
        srcs = []
        for c in range(8):
            srcs.append((modb[:, c * 128:(c + 1) * 128], f"modb{c // 4}", c, 0.0))
        for c in range(8):
            srcs.append((modb[:, 1024 + c * 128:1024 + (c + 1) * 128], f"modb{2 + c // 4}", 8 + c, 1.0))
        for c in range(8):
            srcs.append((gbc[:, 1, c * 128:(c + 1) * 128], f"gbc1_{c // 4}", 16 + c, 0.0))
        for c in range(8):
            srcs.append((gbc[:, 2, c * 128:(c + 1) * 128], f"gbc2_{c // 4}", 24 + c, 1.0))
        for (src, hs, col, add) in srcs:
            pt, hp = nps()
            P.op("pe", lambda e, pt=pt, src=src: e.transpose(out=pt[:, 0:128], in_=src, identity=identf),
                 r=[hs, "cst"], w=[hp])
            P.op("dve", lambda e, pt=pt, col=col, add=add: e.tensor_scalar(
                out=modc[:, col:col + 1], in0=pt[:, 0:1], scalar1=add, scalar2=None, op0=ALU.add),
                r=[hp], w=["modc"])
        P.fence()
        if _CACHE.get("stop") == 0:
            P.op("sp", lambda e: e.dma_start(out=dbg_d[0:128, 0:32], in_=modc), r=["modc"], w=["dbg"], dma=True)
            P.op("sp", lambda e: e.dma_start(out=dbg_d[128:256, :], in_=gbc[:, 0, :]), r=["gbc0_0", "gbc0_1"], w=["dbg2"], dma=True)
            P.fence()
            P.emit()
            return nc
        A.off = ph0

        win = A.alloc([8, DIN], BF16)
        modc2 = A.alloc([32], F32)
        P.op("dve", lambda e: e.tensor_copy(out=modc2, in_=modc), r=["modc"], w=["modc2"])
        wout = A.alloc([8, D], BF16)
        diag = A.alloc([124, 128], BF16)
        xt = A.alloc([4, D], BF16)
        hT = A.alloc([8, 512], BF16)
        vr = [A.alloc([4, 542], BF16) for _ in range(2)]
        kr = [[A.alloc([640], BF16) for _ in range(2)] for _ in range(2)]
        va = [A.alloc([5, 130], BF16) for _ in range(2)]
        qT = A.alloc([4, 512], BF16)
        sig = A.alloc([512], F32)
        ybf = A.alloc([4, 512], BF16)
        y2bf = A.alloc([4, 512], BF16)
        mean_sb = A.alloc([512], F32)
        m2_sb = A.alloc([512], F32)
        zc = m2_sb
        rstd_sb = A.alloc([512], F32)
        nmr_sb = A.alloc([512], F32)
        sc_ = A.alloc([512], F32)
        s2bf = A.alloc([512], BF16)
        r2_sb = sig
        ycT = A.alloc([8, 512], BF16)
        mx = A.alloc([8], F32)
        mxb = A.alloc([8], BF16)
        nmx = A.alloc([8], F32)
        dcat = A.alloc([2, 512], BF16)
        ET = A.alloc([4, 512], BF16)
        es_t = A.alloc([8], F32)
        den = A.alloc([8], F32)
        osb = A.alloc([8, 64], F32)
        osq = A.alloc([8, 64], F32)
        ssq = A.alloc([8], F32)
        yat = A.alloc([512], BF16)
        rows_sb = A.alloc([128 + 8 + 512 + D], F32)
        gb = A.alloc([D], F32)
        xr = A.alloc([D], F32)
        rr = A.alloc([D], F32)
        zz = A.alloc([D], F32)
        bnst = A.alloc([12], F32)
        bnag = A.alloc([4], F32)
        print("phase1 arena bytes", A.off)

        bv_bc = rows_sb[:, 0:128]
        sink_bc = rows_sb[:, 128:136]
        aog_bc = rows_sb[:, 136:648]
        bout_bc = rows_sb[:, 648:648 + D]
        P.op("sp", lambda e: e.dma_start(out=rows_sb, in_=rows_d[0:1, R_BV:R_BV + 128 + 8 + 512 + D].partition_broadcast(128)),
             w=["rows_sb"], dma=True)
        for c in range(8):
            P.op("pool", lambda e, c=c: e.dma_start(out=win[:, c, :], in_=win_d[c * 128:(c + 1) * 128, :]), w=[f"win{c}"], dma=True)
            P.op("pool", lambda e, c=c: e.dma_start(out=wout[:, c, :], in_=wout_d[c * 128:(c + 1) * 128, :]), w=[f"wout{c}"], dma=True)
        for c in range(8):
            P.op("dve", lambda e, c=c: e.tensor_tensor(out=wout[:, c, :], in0=wout[:, c, :], in1=gbc[:, 0, :], op=ALU.mult),
                 r=[f"wout{c}", "gbc0_0", "gbc0_1"], w=[f"wout{c}"])
        P.op("dve", lambda e: e.tensor_tensor(out=gb, in0=bout_bc, in1=gbc[:, 0, :], op=ALU.mult),
             r=["rows_sb", "gbc0_0", "gbc0_1"], w=["gb"])
        WINH = [f"win{c}" for c in range(8)]
        WOUTH = [f"wout{c}" for c in range(8)]
        SK = _CACHE.get("skip", set())
        for c in range(4 if "diag" not in SK else 0):
            P.op("dve", lambda e, c=c: e.tensor_tensor(
                out=diag[:, c * 31:(c + 1) * 31, :], in0=identb.unsqueeze(1).to_broadcast([128, 31, 128]),
                in1=cols[:, C_CW + c * 31:C_CW + (c + 1) * 31].unsqueeze(2).to_broadcast([128, 31, 128]), op=ALU.mult),
                r=["identb", "cols"], w=["diag"])
        if "memset" not in SK:
            P.op("pool", lambda e: e.memset(vr[0], 0.0), w=["vr0"])
            P.op("pool", lambda e: e.memset(vr[1], 0.0), w=["vr1"])
        for par in range(2 if "memset" not in SK else 0):
            for g in range(2):
                P.op("pool", lambda e, par=par, g=g: e.memset(kr[par][g], 0.0), w=[f"kr{par}"])
            P.op("pool", lambda e, par=par: e.memset(va[par], 0.0), w=[f"va{par}"])
            P.op("dve", lambda e, par=par: e.memset(va[par][:, 1:, 64:65], 1.0), r=[f"va{par}"], w=[f"va{par}"])
            P.op("dve", lambda e, par=par: e.memset(va[par][:, 1:, 129:130], 1.0), r=[f"va{par}"], w=[f"va{par}"])

        if _CACHE.get("stop") == 1:
            P.fence()
            P.op("sp", lambda e: e.dma_start(out=dbg_d[0:128, 0:512], in_=gb[:, 0:512]), r=["gb"], w=["dbg"], dma=True)
            P.fence()
            P.emit()
            return nc
        for mt in range(0 if _CACHE.get("p2only") else _CACHE.get('nmt_run', NMT)):
            t0 = mt * 512
            vcur, vprev = vr[mt % 2], vr[(mt + 1) % 2]
            hv, hvp = f"vr{mt % 2}", f"vr{(mt + 1) % 2}"
            kT, kTp = kr[mt % 2], kr[(mt + 1) % 2]
            hk, hkp = f"kr{mt % 2}", f"kr{(mt + 1) % 2}"
            vaug, vaugp = va[mt % 2], va[(mt + 1) % 2]
            hva, hvap = f"va{mt % 2}", f"va{(mt + 1) % 2}"
            P.op("pool", lambda e, t0=t0: e.dma_start(out=xt, in_=x_d[t0:t0 + 512, :].rearrange("(s p) d -> p s d", p=128)),
                 w=["xt"], dma=True)
            for c in range(8):
                ptx = psb[c % 2][:, 0:512]
                hp = f"psb{c % 2}"
                for s in range(4 if "notr" not in SK else 0):
                    P.op("pe", lambda e, ptx=ptx, s=s, c=c: e.transpose(
                        out=ptx[:, s * 128:(s + 1) * 128], in_=xt[:, s, c * 128:(c + 1) * 128], identity=identb),
                        r=["xt", "identb"], w=[hp])
                if "noact" not in SK:
                    P.op("act", lambda e, ptx=ptx, c=c: e.activation(
                        out=hT[:, c, :], in_=ptx[:, 0:512], func=AF.Identity, bias=modc2[:, c:c + 1], scale=(1.0 if "fscale" in SK else modc2[:, 8 + c:9 + c])),
                        r=[hp, "modc2"], w=["hT"])
            def stop_at(k):
                if _CACHE.get("stop") == k:
                    P.fence()
                    P.op("sp", lambda e: e.dma_start(out=dbg_d[0:128, 0:512], in_=gb[:, 0:512]), r=["gb"], w=["dbg"], dma=True)
                    P.fence()
                    P.emit()
                    raise _Stop()
            stop_at(2)
            if mt > 0:
                P.op("pool", lambda e, vcur=vcur, vprev=vprev: e.tensor_copy(out=vcur[:, :, 0:30], in_=vprev[:, :, 512:542]),
                     r=[hvp], w=[hv])
                for g in range(2):
                    P.op("pool", lambda e, kT=kT, kTp=kTp, g=g: e.tensor_copy(out=kT[g][:, 0:128], in_=kTp[g][:, 512:640]),
                         r=[hkp], w=[hk])
                P.op("pool", lambda e, vaug=vaug, vaugp=vaugp: e.tensor_copy(out=vaug[:, 0, :], in_=vaugp[:, 4, :]),
                     r=[hvap], w=[hva])
            for c in range(4):
                pb, hpb = nps()
                for k in range(8):
                    P.op("pe", lambda e, pb=pb, k=k, c=c: e.matmul(
                        pb[:], lhsT=win[:, k, 512 + c * 128:512 + (c + 1) * 128], rhs=hT[:, k, :],
                        start=(k == 0), stop=(k == 7)), r=WINH + ["hT"], w=[hpb])
                pa, hpa = nps()
                for k in range(8):
                    P.op("pe", lambda e, pa=pa, k=k, c=c: e.matmul(
                        pa[:], lhsT=win[:, k, c * 128:(c + 1) * 128], rhs=hT[:, k, :],
                        start=(k == 0), stop=(k == 7)), r=WINH + ["hT"], w=[hpa])
                P.op("act", lambda e, pb=pb, c=c: e.activation(
                    out=sig, in_=pb[:], func=AF.Sigmoid, bias=cols[:, C_BIN + 4 + c:C_BIN + 5 + c], scale=1.0),
                    r=[hpb, "cols"], w=["sig"])
                P.op("dve", lambda e, pa=pa, c=c, vcur=vcur: e.scalar_tensor_tensor(
                    out=vcur[:, c, 30:542], in0=pa[:], scalar=cols[:, C_BIN + c:C_BIN + c + 1], in1=sig,
                    op0=ALU.add, op1=ALU.mult), r=[hpa, "sig", "cols"], w=[hv])
            for i in range(4):
                pq, hpq = nps()
                for k in range(8):
                    P.op("pe", lambda e, pq=pq, k=k, i=i: e.matmul(
                        pq[:], lhsT=win[:, k, 1024 + i * 128:1024 + (i + 1) * 128], rhs=hT[:, k, :],
                        start=(k == 0), stop=(k == 7)), r=WINH + ["hT"], w=[hpq])
                P.op("dve", lambda e, pq=pq, i=i: e.tensor_scalar(
                    out=qT[:, i, :], in0=pq[:], scalar1=cols[:, C_BIN + 8 + i:C_BIN + 9 + i], scalar2=0.125,
                    op0=ALU.add, op1=ALU.mult), r=[hpq, "cols"], w=["qT"])
            pk, hpk = nps()
            for k in range(8):
                P.op("pe", lambda e, pk=pk, k=k: e.matmul(
                    pk[:], lhsT=win[:, k, 1536:1664], rhs=hT[:, k, :], start=(k == 0), stop=(k == 7)),
                    r=WINH + ["hT"], w=[hpk])
            for g in range(2):
                P.op("act", lambda e, pk=pk, g=g, kT=kT: e.activation(
                    out=kT[g][g * 64:(g + 1) * 64, 128:640], in_=pk[g * 64:(g + 1) * 64, :],
                    func=AF.Identity, bias=cols[g * 64:(g + 1) * 64, C_BIN + 12:C_BIN + 13], scale=1.0),
                    r=[hpk, "cols"], w=[hk])
            pv, hpv = nps()
            for s in range(4):
                for k in range(8):
                    P.op("pe", lambda e, pv=pv, s=s, k=k: e.matmul(
                        pv[:, s * 128:(s + 1) * 128], lhsT=hT[:, k, s * 128:(s + 1) * 128], rhs=win[:, k, 1664:1792],
                        start=(k == 0), stop=(k == 7)), r=WINH + ["hT"], w=[hpv])
            for s in range(4):
                blk = s + 1
                P.op("dve", lambda e, pv=pv, s=s, blk=blk, vaug=vaug: e.tensor_tensor(
                    out=vaug[:, blk, :].rearrange("p (g d) -> p g d", g=2)[:, :, 0:64],
                    in0=pv[:, s * 128:(s + 1) * 128].rearrange("p (g d) -> p g d", g=2),
                    in1=bv_bc.rearrange("p (g d) -> p g d", g=2), op=ALU.add),
                    r=[hpv, "rows_sb"], w=[hva])
            stop_at(3)
            for c in range(4):
                py, hpy = nps()
                for j in range(31):
                    P.op("pe", lambda e, py=py, c=c, j=j, vcur=vcur: e.matmul(
                        py[:], lhsT=diag[:, c * 31 + j, :], rhs=vcur[:, c, j:j + 512], start=(j == 0), stop=(j == 30)),
                        r=["diag", hv], w=[hpy])
                P.op("act", lambda e, py=py, c=c: e.activation(
                    out=ybf[:, c, :], in_=py[:], func=AF.Identity, bias=cols[:, C_CB + c:C_CB + c + 1], scale=1.0),
                    r=[hpy, "cols"], w=["ybf"])
                P.op("act", lambda e, py=py, c=c: e.activation(
                    out=y2bf[:, c, :], in_=py[:], func=AF.Square, bias=cols[:, C_CB + c:C_CB + c + 1], scale=1.0),
                    r=[hpy, "cols"], w=["y2bf"])
            pm, hpm = nps()
            for c in range(4):
                P.op("pe", lambda e, pm=pm, c=c: e.matmul(pm[:], lhsT=ones512, rhs=ybf[:, c, :], start=(c == 0), stop=(c == 3)),
                     r=["ones512", "ybf"], w=[hpm])
            pe2, hpe2 = nps()
            for c in range(4):
                P.op("pe", lambda e, pe2=pe2, c=c: e.matmul(pe2[:], lhsT=ones512, rhs=y2bf[:, c, :], start=(c == 0), stop=(c == 3)),
                     r=["ones512", "y2bf"], w=[hpe2])
            P.op("act", lambda e, pm=pm: e.activation(out=mean_sb, in_=pm[:], func=AF.Identity), r=[hpm], w=["mean_sb"])
            P.op("dve", lambda e: e.tensor_tensor(out=m2_sb, in0=mean_sb, in1=mean_sb, op=ALU.mult), r=["mean_sb"], w=["m2_sb"])
            P.op("dve", lambda e, pe2=pe2: e.tensor_tensor(out=m2_sb, in0=pe2[:], in1=m2_sb, op=ALU.subtract),
                 r=[hpe2, "m2_sb"], w=["m2_sb"])
            P.op("act", lambda e: e.activation(out=rstd_sb, in_=m2_sb, func=AF.Sqrt, bias=EPSC, scale=1.0), r=["m2_sb", "epsc"], w=["rstd_sb"])
            P.op("dve", lambda e: e.reciprocal(out=rstd_sb, in_=rstd_sb), r=["rstd_sb"], w=["rstd_sb"])
            P.op("dve", lambda e: e.scalar_tensor_tensor(out=nmr_sb, in0=mean_sb, scalar=-1.0, in1=rstd_sb, op0=ALU.mult, op1=ALU.mult),
                 r=["mean_sb", "rstd_sb"], w=["nmr_sb"])
            for c in range(4):
                P.op("dve", lambda e, c=c: e.tensor_tensor(out=zc, in0=ybf[:, c, :], in1=rstd_sb, op=ALU.mult),
                     r=["ybf", "rstd_sb"], w=["m2_sb"])
                P.op("dve", lambda e: e.tensor_tensor(out=zc, in0=zc, in1=nmr_sb, op=ALU.add), r=["m2_sb", "nmr_sb"], w=["m2_sb"])
                P.op("act", lambda e, c=c: e.activation(out=sc_, in_=zc, func=AF.Silu, bias=cols[:, C_LB + c:C_LB + c + 1],
                                                        scale=cols[:, C_LG + c:C_LG + c + 1]), r=["m2_sb", "cols"], w=["sc_"])
                P.op("act", lambda e: e.activation(out=s2bf, in_=sc_, func=AF.Square), r=["sc_"], w=["s2bf"])
                pr, hpr = nps()
                P.op("pe", lambda e, pr=pr: e.matmul(pr[:], lhsT=bdb, rhs=s2bf, start=True, stop=True), r=["bdb", "s2bf"], w=[hpr])
                P.op("act", lambda e, pr=pr: e.activation(out=r2_sb, in_=pr[:], func=AF.Sqrt, bias=EPSC, scale=1.0), r=[hpr, "epsc"], w=["sig"])
                P.op("dve", lambda e: e.reciprocal(out=r2_sb, in_=r2_sb), r=["sig"], w=["sig"])
                P.op("dve", lambda e, c=c: e.scalar_tensor_tensor(out=ycT[:, c, :], in0=sc_, scalar=cols[:, C_OG + c:C_OG + c + 1],
                                                                  in1=r2_sb, op0=ALU.mult, op1=ALU.mult),
                     r=["sc_", "sig", "cols"], w=["ycT"])
            stop_at(4)
            for s in range(4):
                n = mt * 4 + s
                qs = slice(s * 128, (s + 1) * 128)
                for i in range(4):
                    pS, hpS = nps()
                    for g in range(2):
                        P.op("pe", lambda e, pS=pS, i=i, g=g, s=s, qs=qs, kT=kT: e.matmul(
                            pS[:, g * 256:(g + 1) * 256], lhsT=qT[:, i, qs], rhs=kT[g][:, s * 128:s * 128 + 256],
                            start=True, stop=True), r=["qT", hk], w=[hpS])
                    P.op("dve", lambda e, pS=pS, i=i: e.tensor_reduce(
                        out=mx[:, 2 * i:2 * i + 2], in_=pS[:].rearrange("p (g k) -> p g k", g=2), axis=AX.X, op=ALU.max),
                        r=[hpS], w=["mx"])
                P.op("dve", lambda e: e.tensor_copy(out=mxb, in_=mx), r=["mx"], w=["mxb"])
                P.op("dve", lambda e: e.tensor_scalar(out=nmx, in0=mxb, scalar1=-1.0, scalar2=None, op0=ALU.mult), r=["mxb"], w=["nmx"])
                for g in range(2):
                    P.op("dve", lambda e, g=g: e.tensor_tensor(
                        out=dcat[:, g, :].rearrange("p (i q) -> p i q", i=4), in0=identb.unsqueeze(1).to_broadcast([128, 4, 128]),
                        in1=nmx.rearrange("p (i g) -> p i g", g=2)[:, :, g:g + 1].to_broadcast([128, 4, 128]), op=ALU.mult),
                        r=["identb", "nmx"], w=["dcat"])
                khs = [1] if n == 0 else [0, 1]
                for g in range(2):
                    for kh in khs:
                        pT, hpT = nps()
                        kc = slice(s * 128 + kh * 128, s * 128 + kh * 128 + 128)
                        P.op("pe", lambda e, pT=pT, g=g, kc=kc, qs=qs, kT=kT: e.matmul(
                            pT[:].rearrange("p (i q) -> p i q", i=4), lhsT=kT[g][:, kc], rhs=qT[:, :, qs], start=True, stop=False),
                            r=[hk, "qT"], w=[hpT])
                        P.op("pe", lambda e, pT=pT, g=g: e.matmul(pT[:], lhsT=onesb, rhs=dcat[:, g, :], start=False, stop=False),
                             r=["onesb", "dcat"], w=[hpT])
                        P.op("pe", lambda e, pT=pT, kh=kh: e.matmul(pT[:], lhsT=identb, rhs=maskT[:, kh, :], start=False, stop=True),
                             r=["identb", "maskT"], w=[hpT])
                        P.op("act", lambda e, pT=pT, g=g, kh=kh: e.activation(out=ET[:, g * 2 + kh, :], in_=pT[:], func=AF.Exp),
                             r=[hpT], w=[f"ET{g}{kh}"])
                pos_ = []
                for g in range(2):
                    po, hpo = nps()
                    pos_.append((po, hpo))
                    for i in range(4):
                        for kh in khs:
                            P.op("pe", lambda e, po=po, g=g, i=i, kh=kh, s=s, khs=khs, vaug=vaug: e.matmul(
                                po[:, i * 65:(i + 1) * 65], lhsT=ET[:, g * 2 + kh, i * 128:(i + 1) * 128],
                                rhs=vaug[:, s + kh, g * 65:(g + 1) * 65], start=(kh == khs[0]), stop=(kh == 1)),
                                r=[f"ET{g}{kh}", hva], w=[hpo])
                P.op("dve", lambda e: e.tensor_tensor(out=es_t, in0=sink_bc, in1=nmx, op=ALU.add), r=["rows_sb", "nmx"], w=["es_t"])
                P.op("act", lambda e: e.activation(out=es_t, in_=es_t, func=AF.Exp), r=["es_t"], w=["es_t"])
                for g in range(2):
                    po, hpo = pos_[g]
                    P.op("dve", lambda e, po=po, g=g: e.tensor_tensor(
                        out=den.rearrange("p (i g) -> p i g", g=2)[:, :, g:g + 1],
                        in0=po[:, 0:260].rearrange("p (i d) -> p i d", d=65)[:, :, 64:65],
                        in1=es_t.rearrange("p (i g) -> p i g", g=2)[:, :, g:g + 1], op=ALU.add),
                        r=[hpo, "es_t"], w=["den"])
                    P.op("act", lambda e, po=po, g=g: e.activation(
                        out=osb.rearrange("p (i g) d -> p i g d", g=2)[:, :, g, :],
                        in_=po[:, 0:260].rearrange("p (i d) -> p i d", d=65)[:, :, 0:64], func=AF.Identity),
                        r=[hpo], w=["osb"])
                P.op("dve", lambda e: e.reciprocal(out=den, in_=den), r=["den"], w=["den"])
                P.op("dve", lambda e: e.tensor_tensor(out=osb, in0=osb, in1=den.unsqueeze(2).to_broadcast([128, 8, 64]), op=ALU.mult),
                     r=["osb", "den"], w=["osb"])
                P.op("act", lambda e: e.activation(out=osq, in_=osb, func=AF.Square), r=["osb"], w=["osq"])
                P.op("dve", lambda e: e.tensor_reduce(out=ssq, in_=osq, axis=AX.X, op=ALU.add), r=["osq"], w=["ssq"])
                P.op("act", lambda e: e.activation(out=ssq, in_=ssq, func=AF.Sqrt, bias=EPSC, scale=1.0 / 64.0), r=["ssq", "epsc"], w=["ssq"])
                P.op("dve", lambda e: e.reciprocal(out=ssq, in_=ssq), r=["ssq"], w=["ssq"])
                P.op("dve", lambda e: e.tensor_tensor(out=osb, in0=osb, in1=ssq.unsqueeze(2).to_broadcast([128, 8, 64]), op=ALU.mult),
                     r=["osb", "ssq"], w=["osb"])
                P.op("dve", lambda e: e.tensor_tensor(out=yat, in0=osb.rearrange("p h d -> p (h d)"), in1=aog_bc, op=ALU.mult),
                     r=["osb", "rows_sb"], w=["yat"])
                ptb = psb[s % 2][:, 0:512]
                hptr = f"psb{s % 2}"
                for i in range(4):
                    P.op("pe", lambda e, ptb=ptb, i=i: e.transpose(out=ptb[:, i * 128:(i + 1) * 128], in_=yat[:, i * 128:(i + 1) * 128],
                                                                  identity=identb), r=["yat", "identb"], w=[hptr])
                P.op("act", lambda e, ptb=ptb, qs=qs: e.activation(
                    out=ycT[:, 4:8, qs], in_=ptb[:, 0:512].rearrange("p (i q) -> p i q", i=4), func=AF.Identity),
                    r=[hptr], w=["ycT"])
            stop_at(5)
            for s in range(4):
                tt = mt * 4 + s
                P.op("sp", lambda e, tt=tt: e.dma_start(out=xr, in_=x_d[tt * 128:(tt + 1) * 128, :]), w=["xr"], dma=True)
                P.op("pool", lambda e: e.tensor_scalar(out=xr, in0=xr, scalar1=ALPHA, scalar2=None, op0=ALU.mult), r=["xr"], w=["xr"])
                P.op("pool", lambda e: e.tensor_tensor(out=xr, in0=xr, in1=gb, op=ALU.add), r=["xr", "gb"], w=["xr"])
                for h in range(2):
                    po, hpo = nps()
                    for c in range(8):
                        P.op("pe", lambda e, po=po, c=c, s=s, h=h: e.matmul(
                            po[:], lhsT=ycT[:, c, s * 128:(s + 1) * 128], rhs=wout[:, c, h * 512:(h + 1) * 512],
                            start=(c == 0), stop=(c == 7)), r=["ycT"] + WOUTH, w=[hpo])
                    P.op("dve", lambda e, po=po, h=h: e.tensor_tensor(out=rr[:, h * 512:(h + 1) * 512], in0=po[:],
                                                                      in1=xr[:, h * 512:(h + 1) * 512], op=ALU.add),
                         r=[hpo, "xr"], w=["rr"])
                for h in range(2):
                    P.op("dve", lambda e, h=h: e.bn_stats(out=bnst[:, h * 6:(h + 1) * 6], in_=rr[:, h * 512:(h + 1) * 512]),
                         r=["rr"], w=["bnst"])
                P.op("dve", lambda e: e.bn_aggr(out=bnag[:, 0:2], in_=bnst), r=["bnst"], w=["bnag"])
                P.op("act", lambda e: e.activation(out=bnag[:, 2:3], in_=bnag[:, 1:2], func=AF.Sqrt, bias=EPSC, scale=1.0),
                     r=["bnag", "epsc"], w=["bnag2"])
                P.op("dve", lambda e: e.reciprocal(out=bnag[:, 2:3], in_=bnag[:, 2:3]), r=["bnag2"], w=["bnag2"])
                P.op("dve", lambda e: e.scalar_tensor_tensor(out=bnag[:, 3:4], in0=bnag[:, 0:1], scalar=-1.0, in1=bnag[:, 2:3],
                                                             op0=ALU.mult, op1=ALU.mult), r=["bnag", "bnag2"], w=["bnag3"])
                P.op("act", lambda e: e.activation(out=zz, in_=rr, func=AF.Identity, bias=bnag[:, 3:4], scale=bnag[:, 2:3]),
                     r=["rr", "bnag2", "bnag3"], w=["zz"])
                P.op("sp", lambda e, tt=tt: e.dma_start(out=z_d[tt * 128:(tt + 1) * 128, :], in_=zz), r=["zz"], w=[f"z_d{tt}"], dma=True)

        P.fence()
        if dbg and dbg["what"] == "z":
            nr = _CACHE.get('nmt_run', NMT) * 512
            P.op("sp", lambda e: e.dma_start(out=dbg_d[0:nr, :], in_=z_d[0:nr, :]), r=[f"z_d{i}" for i in range(nr // 128)], w=["dbg"], dma=True)
            P.fence()
            P.emit()
            return nc

        try:
            build_phase2(nc, P, A, ps, nps, dr, out_d, z_d, xs_d, ys_d, dbg, dbg_d, mark_persist,
                     dict(cols=cols, cst=cst, identf=identf, identb=identb, onesb=onesb, Ub=Ub, modc=modc, gbc=gbc,
                              EPSC=EPSC, psb=psb))
        except _StopEmit:
            pass
        P.fence()
        P.emit()
    return nc


def build_phase2(nc, P, A, ps, nps, dr, out_d, z_d, xs_d, ys_d, dbg, dbg_d, mark, K):
    cols, identf, identb, onesb, Ub, gbc, EPSC, psb = (K[k] for k in ("cols", "identf", "identb", "onesb", "Ub", "gbc", "EPSC", "psb"))
    rows_d, wr_d, wg_d, wu_d, wd_d = dr["rows"], dr["w_r"], dr["wg"], dr["wu"], dr["wd"]
    A.off = mark
    lnr = A.alloc([4, D], F32)
    misc = A.alloc([36 + 64], F32)
    A2 = A.alloc([D], F32)
    B2 = A.alloc([D], F32)
    GA = A.alloc([D], F32)
    BA = A.alloc([D], F32)
    wr = A.alloc([8, 36], F32)
    pos_i = A.alloc([2, NT], I32)
    wts = A.alloc([2, NT], F32)
    widx_i = A.alloc([64], I32)
    mark2 = A.off
    br_bc = misc[:, 0:36]
    slot_bc = misc[:, 36:36 + NS]
    P.op("sp", lambda e: e.dma_start(out=lnr.rearrange("p a d -> p (a d)"), in_=rows_d[0:1, R_L1G:R_L1G + 4 * D].partition_broadcast(128)),
         w=["lnr"], dma=True)
    P.op("sp", lambda e: e.dma_start(out=misc, in_=rows_d[0:1, R_BR:R_BR + 100].partition_broadcast(128)), w=["misc"], dma=True)
    P.op("sp", lambda e: e.dma_start(out=wr, in_=wr_d.rearrange("(c p) n -> p c n", p=128)), w=["wr"], dma=True)
    G2H = ["gbc1_0", "gbc1_1", "gbc2_0", "gbc2_1", "gbc3_0", "gbc3_1"]
    P.op("dve", lambda e: e.scalar_tensor_tensor(out=A2, in0=gbc[:, 2, :], scalar=1.0, in1=lnr[:, 0, :], op0=ALU.add, op1=ALU.mult),
         r=["lnr"] + G2H, w=["A2"])
    P.op("dve", lambda e: e.scalar_tensor_tensor(out=B2, in0=gbc[:, 2, :], scalar=1.0, in1=lnr[:, 1, :], op0=ALU.add, op1=ALU.mult),
         r=["lnr"] + G2H, w=["B2"])
    P.op("dve", lambda e: e.tensor_tensor(out=B2, in0=B2, in1=gbc[:, 1, :], op=ALU.add), r=["B2"] + G2H, w=["B2"])
    P.op("dve", lambda e: e.tensor_scalar(out=GA, in0=lnr[:, 0, :], scalar1=ALPHA, scalar2=None, op0=ALU.mult), r=["lnr"], w=["GA"])
    P.op("dve", lambda e: e.tensor_scalar(out=BA, in0=lnr[:, 1, :], scalar1=ALPHA, scalar2=None, op0=ALU.mult), r=["lnr"], w=["BA"])

    def mk_nps(ids):
        st_ = [0]

        def f():
            i = ids[st_[0] % len(ids)]
            st_[0] += 1
            return ps[i], f"ps{i}"
        return f

    t1_d = nc.dram_tensor("t1_scr", [S, D], F32, kind="Internal").ap()
    h2b = A.alloc([NT, D], BF16)
    logits = A.alloc([NT, 36], F32)
    mark2a = A.off
    zt = [A.alloc([D], F32) for _ in range(2)]
    h2f = [A.alloc([D], F32) for _ in range(2)]
    h2T = [A.alloc([8, 128], F32) for _ in range(2)]
    t1s = [A.alloc([D], F32) for _ in range(2)]
    nps2a = [mk_nps([0, 1, 2]), mk_nps([3, 4, 5])]

    def chain2a(j):
        b = j % 2
        mynps = nps2a[b]
        z_, hz = zt[b], f"zt{b}"
        hf, hhf = h2f[b], f"h2f{b}"
        hT_, hhT = h2T[b], f"h2T{b}"
        t1_, ht1 = t1s[b], f"t1s{b}"
        P.rec()
        P.op("sp", lambda e: e.dma_start(out=z_, in_=z_d[j * 128:(j + 1) * 128, :]), r=[f"z_d{j}"], w=[hz], dma=True)
        P.op("dve", lambda e: e.tensor_tensor(out=hf, in0=z_, in1=A2, op=ALU.mult), r=[hz, "A2"], w=[hhf])
        P.op("dve", lambda e: e.tensor_tensor(out=hf, in0=hf, in1=B2, op=ALU.add), r=[hhf, "B2"], w=[hhf])
        P.op("pool", lambda e: e.tensor_tensor(out=t1_, in0=z_, in1=GA, op=ALU.mult), r=[hz, "GA"], w=[ht1])
        P.op("pool", lambda e: e.tensor_tensor(out=t1_, in0=t1_, in1=BA, op=ALU.add), r=[ht1, "BA"], w=[ht1])
        P.op("sp", lambda e: e.dma_start(out=t1_d[j * 128:(j + 1) * 128, :], in_=t1_), r=[ht1], w=[f"t1_d{j}"], dma=True)
        P.op("act", lambda e: e.activation(out=h2b[:, j, :], in_=hf, func=AF.Identity), r=[hhf], w=[f"h2b{j}"])
        for hh in range(2):
            pt, hp = mynps()
            for c4 in range(4):
                c = hh * 4 + c4
                P.op("pe", lambda e, pt=pt, c=c, c4=c4: e.transpose(out=pt[:, c4 * 128:(c4 + 1) * 128],
                                                                  in_=hf[:, c * 128:(c + 1) * 128], identity=identf),
                     r=[hhf, "cst"], w=[hp])
            if hh == 0:
                P.op("act", lambda e, pt=pt: e.activation(out=hT_[:, 0:4, :].rearrange("p c t -> p (c t)"),
                                                          in_=pt[:], func=AF.Identity), r=[hp], w=[hhT + "a"])
            else:
                P.op("dve", lambda e, pt=pt: e.tensor_copy(out=hT_[:, 4:8, :].rearrange("p c t -> p (c t)"),
                                                           in_=pt[:]), r=[hp], w=[hhT + "b"])
        pl, hpl = mynps()
        for c in range(8):
            P.op("pe", lambda e, pl=pl, c=c: e.matmul(pl[:, 0:36], lhsT=hT_[:, c, :], rhs=wr[:, c, :], start=(c == 0), stop=(c == 7)),
                 r=[hhT + "a", hhT + "b", "wr"], w=[hpl])
        P.op("dve", lambda e, pl=pl: e.tensor_tensor(out=logits[:, j, :], in0=pl[:, 0:36], in1=br_bc, op=ALU.add),
             r=[hpl, "misc"], w=["logits"])
        return P.end()

    for j in range(0, NT, 2):
        P.play(merge(chain2a(j), chain2a(j + 1)))
    P.fence()
    A.off = mark2a

    def T3(n):
        return A.alloc([NT, n], F32)
    gmax = A.alloc([NT], F32)
    og = T3(4)
    eg = T3(4)
    sgm = A.alloc([NT], F32)
    ptop = A.alloc([NT], F32)
    tmp4 = A.alloc([NT, 4, 8], F32)
    sel = T3(8)
    sel2 = T3(8)
    m1 = A.alloc([NT], F32)
    m2 = A.alloc([NT], F32)
    o1 = T3(8)
    o2 = T3(8)
    e2 = A.alloc([NT], F32)
    r12 = A.alloc([NT], F32)
    O1 = A.alloc([NT, 4, 8], F32)
    O2 = A.alloc([NT, 4, 8], F32)
    Obf = A.alloc([NT * 32], BF16)
    totA = A.alloc([NT, 32], F32)
    totB = A.alloc([NT, 32], F32)
    tot0 = A.alloc([NT, 32], F32)
    base = A.alloc([NT, 32], F32)
    cnt = A.alloc([32], F32)
    cmpc = A.alloc([32, 24], F32)
    pcnt = A.alloc([32], F32)
    oeA = A.alloc([32], F32)
    oeB = A.alloc([32], F32)
    offs = A.alloc([32], F32)
    cmps = A.alloc([NS, 32], F32)
    esl = A.alloc([64], F32)
    used = A.alloc([64], F32)
    posf = A.alloc([2, NT], F32)

    LG = logits[:, :, 0:4]
    LE4 = logits[:, :, 4:36].rearrange("p j (g e) -> p j g e", g=4)

    def dv(fn, r, w):
        P.op("dve", fn, r=r, w=w)

    dv(lambda e: e.tensor_reduce(out=gmax, in_=LG, axis=AX.X, op=ALU.max), ["logits"], ["gmax"])
    dv(lambda e: e.tensor_tensor(out=og, in0=LG, in1=gmax.unsqueeze(2).to_broadcast([128, NT, 4]), op=ALU.is_equal), ["logits", "gmax"], ["og"])
    dv(lambda e: e.tensor_tensor(out=eg, in0=LG, in1=gmax.unsqueeze(2).to_broadcast([128, NT, 4]), op=ALU.subtract), ["logits", "gmax"], ["eg"])
    P.op("act", lambda e: e.activation(out=eg, in_=eg, func=AF.Exp), r=["eg"], w=["eg"])
    dv(lambda e: e.tensor_reduce(out=sgm, in_=eg, axis=AX.X, op=ALU.add), ["eg"], ["sgm"])
    dv(lambda e: e.reciprocal(out=ptop, in_=sgm), ["sgm"], ["ptop"])
    dv(lambda e: e.tensor_tensor(out=tmp4, in0=LE4, in1=og.unsqueeze(3).to_broadcast([128, NT, 4, 8]), op=ALU.mult), ["logits", "og"], ["tmp4"])
    dv(lambda e: e.tensor_reduce(out=sel, in_=tmp4.rearrange("p j g e -> p j e g"), axis=AX.X, op=ALU.add), ["tmp4"], ["sel"])
    dv(lambda e: e.tensor_reduce(out=m1, in_=sel, axis=AX.X, op=ALU.max), ["sel"], ["m1"])
    dv(lambda e: e.tensor_tensor(out=o1, in0=sel, in1=m1.unsqueeze(2).to_broadcast([128, NT, 8]), op=ALU.is_equal), ["sel", "m1"], ["o1"])
    dv(lambda e: e.scalar_tensor_tensor(out=sel2.rearrange("p j e -> p (j e)"), in0=o1.rearrange("p j e -> p (j e)"), scalar=-1.0e9,
                                        in1=sel.rearrange("p j e -> p (j e)"), op0=ALU.mult, op1=ALU.add), ["o1", "sel"], ["sel2"])
    dv(lambda e: e.tensor_reduce(out=m2, in_=sel2, axis=AX.X, op=ALU.max), ["sel2"], ["m2"])
    dv(lambda e: e.tensor_tensor(out=o2, in0=sel2, in1=m2.unsqueeze(2).to_broadcast([128, NT, 8]), op=ALU.is_equal), ["sel2", "m2"], ["o2"])
    dv(lambda e: e.tensor_tensor(out=e2, in0=m2, in1=m1, op=ALU.subtract), ["m1", "m2"], ["e2"])
    P.op("act", lambda e: e.activation(out=e2, in_=e2, func=AF.Exp), r=["e2"], w=["e2"])
    dv(lambda e: e.tensor_scalar(out=r12, in0=e2, scalar1=1.0, scalar2=None, op0=ALU.add), ["e2"], ["r12"])
    dv(lambda e: e.reciprocal(out=r12, in_=r12), ["r12"], ["r12"])
    dv(lambda e: e.tensor_tensor(out=wts[:, 0, :], in0=r12, in1=ptop, op=ALU.mult), ["r12", "ptop"], ["wts"])
    dv(lambda e: e.tensor_tensor(out=wts[:, 1, :], in0=wts[:, 0, :], in1=e2, op=ALU.mult), ["wts", "e2"], ["wts"])
    dv(lambda e: e.tensor_tensor(out=O1, in0=og.unsqueeze(3).to_broadcast([128, NT, 4, 8]),
                                 in1=o1.unsqueeze(2).to_broadcast([128, NT, 4, 8]), op=ALU.mult), ["og", "o1"], ["O1"])
    dv(lambda e: e.tensor_tensor(out=O2, in0=og.unsqueeze(3).to_broadcast([128, NT, 4, 8]),
                                 in1=o2.unsqueeze(2).to_broadcast([128, NT, 4, 8]), op=ALU.mult), ["og", "o2"], ["O2"])
    O1f = O1.rearrange("p j g e -> p (j g e)")
    O2f = O2.rearrange("p j g e -> p (j g e)")
    dv(lambda e: e.tensor_tensor(out=Obf, in0=O1f, in1=O2f, op=ALU.add), ["O1", "O2"], ["Obf"])
    pcs, pts = [], []
    for h in range(2):
        pc_, hpc = nps()
        P.op("pe", lambda e, pc_=pc_, h=h: e.matmul(pc_[:], lhsT=Ub, rhs=Obf[:, h * 512:(h + 1) * 512], start=True, stop=True),
             r=["Ub", "Obf"], w=[hpc])
        pcs.append((pc_, hpc))
        pt_, hpt = nps()
        P.op("pe", lambda e, pt_=pt_, h=h: e.matmul(pt_[:], lhsT=onesb, rhs=Obf[:, h * 512:(h + 1) * 512], start=True, stop=True),
             r=["onesb", "Obf"], w=[hpt])
        pts.append((pt_, hpt))
    tot0f = tot0.rearrange("p j e -> p (j e)")
    for h in range(2):
        dv(lambda e, h=h: e.tensor_copy(out=tot0f[:, h * 512:(h + 1) * 512], in_=pts[h][0][:]), [pts[h][1]], ["tot0"])
    cur, hc = tot0, "tot0"
    for i_, sft in enumerate((1, 2, 4, 8, 16)):
        nxt, hn = (totA, "totA") if i_ % 2 == 0 else (totB, "totB")
        dv(lambda e, cur=cur, nxt=nxt, sft=sft: e.tensor_tensor(out=nxt[:, sft:, :], in0=cur[:, sft:, :], in1=cur[:, :NT - sft, :], op=ALU.add),
           [hc], [hn])
        dv(lambda e, cur=cur, nxt=nxt, sft=sft: e.tensor_copy(out=nxt[:, :sft, :], in_=cur[:, :sft, :]), [hc, hn], [hn])
        cur, hc = nxt, hn
    incl, hincl = cur, hc
    dv(lambda e: e.tensor_copy(out=cnt, in_=incl[:, NT - 1, :]), [hincl], ["cnt"])
    dv(lambda e: e.tensor_tensor(out=cmpc, in0=cnt.unsqueeze(2).to_broadcast([128, 32, 24]),
                                 in1=slot_bc[:, 0:24].unsqueeze(1).to_broadcast([128, 32, 24]), op=ALU.is_gt), ["cnt", "misc"], ["cmpc"])
    dv(lambda e: e.tensor_reduce(out=pcnt, in_=cmpc, axis=AX.X, op=ALU.add), ["cmpc"], ["pcnt"])
    dv(lambda e: e.tensor_scalar(out=pcnt, in0=pcnt, scalar1=float(T), scalar2=None, op0=ALU.mult), ["pcnt"], ["pcnt"])
    cur, hc = pcnt, "pcnt"
    for i_, sft in enumerate((1, 2, 4, 8, 16)):
        nxt, hn = (oeA, "oeA") if i_ % 2 == 0 else (oeB, "oeB")
        dv(lambda e, cur=cur, nxt=nxt, sft=sft: e.tensor_tensor(out=nxt[:, sft:], in0=cur[:, sft:], in1=cur[:, :32 - sft], op=ALU.add), [hc], [hn])
        dv(lambda e, cur=cur, nxt=nxt, sft=sft: e.tensor_copy(out=nxt[:, :sft], in_=cur[:, :sft]), [hc, hn], [hn])
        cur, hc = nxt, hn
    oend, hoend = cur, hc
    dv(lambda e: e.tensor_tensor(out=offs, in0=oend, in1=pcnt, op=ALU.subtract), [hoend, "pcnt"], ["offs"])
    dv(lambda e: e.tensor_tensor(out=base, in0=incl, in1=tot0, op=ALU.subtract), [hincl, "tot0"], ["base"])
    dv(lambda e: e.tensor_tensor(out=base, in0=base, in1=offs.unsqueeze(1).to_broadcast([128, NT, 32]), op=ALU.add), ["base", "offs"], ["base"])
    basef = base.rearrange("p j e -> p (j e)")
    for h in range(2):
        dv(lambda e, h=h: e.tensor_tensor(out=basef[:, h * 512:(h + 1) * 512], in0=pcs[h][0][:], in1=basef[:, h * 512:(h + 1) * 512], op=ALU.add),
           [pcs[h][1], "base"], ["base"])
    for k, (Ok, hO) in enumerate(((O1, "O1"), (O2, "O2"))):
        dv(lambda e, Ok=Ok: e.tensor_tensor(out=Ok.rearrange("p j g e -> p (j g e)"), in0=Ok.rearrange("p j g e -> p (j g e)"), in1=basef, op=ALU.mult),
           [hO, "base"], [hO])
        dv(lambda e, Ok=Ok, k=k: e.tensor_reduce(out=posf[:, k, :], in_=Ok.rearrange("p j g e -> p j (g e)"), axis=AX.X, op=ALU.add), [hO], ["posf"])
    dv(lambda e: e.tensor_copy(out=pos_i, in_=posf), ["posf"], ["pos_i"])
    dv(lambda e: e.tensor_tensor(out=cmps, in0=oend.unsqueeze(1).to_broadcast([128, NS, 32]),
                                 in1=slot_bc.unsqueeze(2).to_broadcast([128, NS, 32]), op=ALU.is_le), [hoend, "misc"], ["cmps"])
    dv(lambda e: e.tensor_reduce(out=esl[:, 0:NS], in_=cmps, axis=AX.X, op=ALU.add), ["cmps"], ["esl"])
    dv(lambda e: e.tensor_scalar(out=esl[:, 0:NS], in0=esl[:, 0:NS], scalar1=float(NE - 1), scalar2=128.0, op0=ALU.min, op1=ALU.mult), ["esl"], ["esl"])
    dv(lambda e: e.tensor_scalar(out=used[:, 0:NS], in0=slot_bc, scalar1=oend[:, 31:32], scalar2=None, op0=ALU.is_lt), ["misc", hoend], ["used"])
    dv(lambda e: e.tensor_scalar(out=used[:, 0:NS], in0=used[:, 0:NS], scalar1=-1.0e6, scalar2=1.0e6, op0=ALU.mult, op1=ALU.add), ["used"], ["used"])
    dv(lambda e: e.tensor_tensor(out=esl[:, 0:NS], in0=esl[:, 0:NS], in1=used[:, 0:NS], op=ALU.add), ["esl", "used"], ["esl"])
    dv(lambda e: e.tensor_scalar(out=esl[:, 0:NS], in0=esl[:, 0:NS], scalar1=cols[:, C_PID:C_PID + 1], scalar2=None, op0=ALU.add), ["esl", "cols"], ["esl"])
    dv(lambda e: e.tensor_copy(out=widx_i[:, 0:NS], in_=esl[:, 0:NS]), ["esl"], ["widx_i"])

    if dbg and dbg["what"] == "route":
        P.fence()
        P.op("sp", lambda e: e.dma_start(out=dbg_d[0:128, 0:NT * 36], in_=logits.rearrange("p j n -> p (j n)")), r=["logits"], w=["dbg0"], dma=True)
        P.op("sp", lambda e: e.dma_start(out=dbg_d[128:256, 0:2 * NT], in_=posf.rearrange("p k j -> p (k j)")), r=["posf"], w=["dbg1"], dma=True)
        P.op("sp", lambda e: e.dma_start(out=dbg_d[256:384, 0:2 * NT], in_=wts.rearrange("p k j -> p (k j)")), r=["wts"], w=["dbg2"], dma=True)
        P.op("sp", lambda e: e.dma_start(out=dbg_d[384:512, 0:NS], in_=esl[:, 0:NS]), r=["esl"], w=["dbg3"], dma=True)
        P.fence()
        raise _StopEmit()

    for j in range(NT):
        for k in range(2):
            P.op("pool", lambda e, j=j, k=k: e.indirect_dma_start(
                out=xs_d[:, :], out_offset=bass.IndirectOffsetOnAxis(ap=pos_i[:, k, j:j + 1], axis=0),
                in_=h2b[:, j, :], in_offset=None), r=[f"h2b{j}", "pos_i"], w=[f"xs_{j}_{k}"], dma=True)
    P.fence()

    A.off = mark2
    NBW = 4
    wgs = [A.alloc([2048], BF16) for _ in range(NBW)]
    wus = [A.alloc([2048], BF16) for _ in range(NBW)]
    wds = [A.alloc([2048], BF16) for _ in range(NBW)]
    xtok = [A.alloc([NSUB, D], BF16) for _ in range(NBW)]
    XT = [A.alloc([8, T], BF16) for _ in range(2)]
    sgs = [A.alloc([T], F32) for _ in range(2)]
    aT = [A.alloc([2, T], BF16) for _ in range(2)]
    NYO = 3
    yo = [A.alloc([D], F32) for _ in range(NYO)]
    npsB = mk_nps([0, 1, 2])
    npsC = mk_nps([3, 4, 5])
    _bc = {}

    def get_bc(e):
        if "v" not in _bc:
            reg = e.alloc_register("bcreg")
            e.reg_mov(reg, NE * 128 - 1)
            _bc["v"] = e.snap(reg, donate=True)
        return _bc["v"]

    def load_w(s, which):
        bw = s % NBW
        for (wsb, wdr, hn) in which(bw):
            P.op("pool", lambda e, wsb=wsb, wdr=wdr, s=s: e.indirect_dma_start(
                out=wsb, out_offset=None, in_=wdr[:, :],
                in_offset=bass.IndirectOffsetOnAxis(ap=widx_i[:, s:s + 1], axis=0),
                bounds_check=get_bc(e), oob_is_err=False), r=["widx_i"], w=[hn], dma=True)

    def w_gu(bw):
        return ((wgs[bw], wg_d, f"wg{bw}"), (wus[bw], wu_d, f"wu{bw}"))

    def w_d(bw):
        return ((wds[bw], wd_d, f"wd{bw}"),)

    def load_x(s):
        bw = s % NBW
        for st in range(NSUB):
            r0 = s * T + st * 128
            P.op("sp", lambda e, bw=bw, st=st, r0=r0: e.dma_start(out=xtok[bw][:, st, :], in_=xs_d[r0:r0 + 128, :]),
                 w=[f"xtok{bw}_{st}"], dma=True)

    def stageA(s):
        b, bw = s % 2, s % NBW
        P.rec()
        for st in range(NSUB):
            k = (s * NSUB + st) % 2
            pb_, hpb = psb[k], f"psb{k}"
            for c in range(8):
                P.op("pe", lambda e, pb_=pb_, st=st, c=c: e.transpose(out=pb_[:, c * 128:(c + 1) * 128],
                                                                     in_=xtok[bw][:, st, c * 128:(c + 1) * 128], identity=identb),
                     r=[f"xtok{bw}_{st}", "identb"], w=[hpb])
            if (s * NSUB + st) % 2 == 0:
                P.op("act", lambda e, pb_=pb_, st=st: e.activation(out=XT[b][:, :, st * 128:(st + 1) * 128],
                                                                   in_=pb_[:].rearrange("p (c t) -> p c t", c=8), func=AF.Identity),
                     r=[hpb], w=[f"XT{b}_{st}"])
            else:
                P.op("dve", lambda e, pb_=pb_, st=st: e.tensor_copy(out=XT[b][:, :, st * 128:(st + 1) * 128],
                                                                    in_=pb_[:].rearrange("p (c t) -> p c t", c=8)),
                     r=[hpb], w=[f"XT{b}_{st}"])
        return P.end()

    def stageB(s):
        b, bw = s % 2, s % NBW
        XH = [f"XT{b}_{st}" for st in range(NSUB)]
        P.rec()
        for fch in range(2):
            pg, hpg = npsB()
            for c in range(8):
                P.op("pe", lambda e, pg=pg, c=c, fch=fch: e.matmul(
                    pg[:, 0:T], lhsT=wgs[bw][:, c * 256 + fch * 128:c * 256 + (fch + 1) * 128], rhs=XT[b][:, c, :],
                    start=(c == 0), stop=(c == 7)), r=[f"wg{bw}"] + XH, w=[hpg])
            P.op("act", lambda e, pg=pg: e.activation(out=sgs[b], in_=pg[:, 0:T], func=AF.Silu), r=[hpg], w=[f"sgs{b}"])
            pu, hpu = npsB()
            for c in range(8):
                P.op("pe", lambda e, pu=pu, c=c, fch=fch: e.matmul(
                    pu[:, 0:T], lhsT=wus[bw][:, c * 256 + fch * 128:c * 256 + (fch + 1) * 128], rhs=XT[b][:, c, :],
                    start=(c == 0), stop=(c == 7)), r=[f"wu{bw}"] + XH, w=[hpu])
            P.op("dve", lambda e, pu=pu, fch=fch: e.tensor_tensor(out=aT[b][:, fch, :], in0=pu[:, 0:T], in1=sgs[b], op=ALU.mult),
                 r=[hpu, f"sgs{b}"], w=[f"aT{b}_{fch}"])
        return P.end()

    def stageC(s):
        b, bw = s % 2, s % NBW
        P.rec()
        for st in range(NSUB):
            yb = (s * NSUB + st) % NYO
            for half in range(2):
                po, hpo = npsC()
                for fch in range(2):
                    P.op("pe", lambda e, po=po, st=st, fch=fch, half=half: e.matmul(
                        po[:], lhsT=aT[b][:, fch, st * 128:(st + 1) * 128],
                        rhs=wds[bw][:, fch * 1024 + half * 512:fch * 1024 + (half + 1) * 512], start=(fch == 0), stop=(fch == 1)),
                        r=[f"aT{b}_0", f"aT{b}_1", f"wd{bw}"], w=[hpo])
                if half == 0:
                    P.op("act", lambda e, po=po, yb=yb: e.activation(out=yo[yb][:, 0:512], in_=po[:], func=AF.Identity), r=[hpo], w=[f"yo{yb}a"])
                else:
                    P.op("dve", lambda e, po=po, yb=yb: e.tensor_copy(out=yo[yb][:, 512:1024], in_=po[:]), r=[hpo], w=[f"yo{yb}b"])
            r0 = s * T + st * 128
            P.op("sp", lambda e, yb=yb, r0=r0: e.dma_start(out=ys_d[r0:r0 + 128, :], in_=yo[yb]), r=[f"yo{yb}a", f"yo{yb}b"],
                 w=[f"ys_{s}_{st}"], dma=True)
        return P.end()

    for s in range(min(NBW, NS)):
        load_x(s)
        load_w(s, w_gu)
        load_w(s, w_d)
    for i in range(NS + 2):
        lists = []
        if i < NS:
            lists.append(stageA(i))
        if 0 <= i - 1 < NS:
            lists.append(stageB(i - 1))
        if 0 <= i - 2 < NS:
            lists.append(stageC(i - 2))
        P.play(merge(*lists))
        if i + NBW < NS:
            load_x(i + NBW)
        if i - 1 >= 0 and i - 1 + NBW < NS:
            load_w(i - 1 + NBW, w_gu)
        if i - 2 >= 0 and i - 2 + NBW < NS:
            load_w(i - 2 + NBW, w_d)
    P.fence()

    A.off = mark2
    Y1 = [A.alloc([D], F32) for _ in range(2)]
    Y2 = [A.alloc([D], F32) for _ in range(2)]
    t1 = [A.alloc([D], F32) for _ in range(2)]
    ff = [A.alloc([D], F32) for _ in range(2)]
    r2 = [A.alloc([D], F32) for _ in range(2)]
    qq = [A.alloc([D], F32) for _ in range(2)]
    ob = [A.alloc([D], F32) for _ in range(2)]
    bn2 = [A.alloc([12], F32) for _ in range(2)]
    ag2 = [A.alloc([4], F32) for _ in range(2)]
    G2H_ = ["gbc3_0", "gbc3_1"]

    def chain2f(j):
        b = j % 2
        P.rec()
        P.op("pool", lambda e: e.indirect_dma_start(
            out=Y1[b], out_offset=None, in_=ys_d[:, :], in_offset=bass.IndirectOffsetOnAxis(ap=pos_i[:, 0, j:j + 1], axis=0)),
            r=["pos_i"], w=[f"Y1{b}"], dma=True)
        P.op("pool", lambda e: e.indirect_dma_start(
            out=Y2[b], out_offset=None, in_=ys_d[:, :], in_offset=bass.IndirectOffsetOnAxis(ap=pos_i[:, 1, j:j + 1], axis=0)),
            r=["pos_i"], w=[f"Y2{b}"], dma=True)
        P.op("sp", lambda e: e.dma_start(out=t1[b], in_=t1_d[j * 128:(j + 1) * 128, :]), r=[f"t1_d{j}"], w=[f"t1{b}"], dma=True)
        P.op("act", lambda e: e.activation(out=ff[b], in_=Y1[b], func=AF.Identity, scale=wts[:, 0, j:j + 1]), r=[f"Y1{b}", "wts"], w=[f"ff{b}"])
        P.op("dve", lambda e: e.scalar_tensor_tensor(out=ff[b], in0=Y2[b], scalar=wts[:, 1, j:j + 1], in1=ff[b], op0=ALU.mult, op1=ALU.add),
             r=[f"Y2{b}", "wts", f"ff{b}"], w=[f"ff{b}"])
        P.op("pool", lambda e: e.tensor_tensor(out=ff[b], in0=ff[b], in1=gbc[:, 3, :], op=ALU.mult), r=[f"ff{b}"] + G2H_, w=[f"ff{b}"])
        P.op("dve", lambda e: e.tensor_tensor(out=r2[b], in0=ff[b], in1=t1[b], op=ALU.add), r=[f"ff{b}", f"t1{b}"], w=[f"r2{b}"])
        for h in range(2):
            P.op("dve", lambda e, h=h: e.bn_stats(out=bn2[b][:, h * 6:(h + 1) * 6], in_=r2[b][:, h * 512:(h + 1) * 512]), r=[f"r2{b}"], w=[f"bn2{b}"])
        P.op("dve", lambda e: e.bn_aggr(out=ag2[b][:, 0:2], in_=bn2[b]), r=[f"bn2{b}"], w=[f"ag2{b}"])
        P.op("act", lambda e: e.activation(out=ag2[b][:, 2:3], in_=ag2[b][:, 1:2], func=AF.Sqrt, bias=EPSC, scale=1.0), r=[f"ag2{b}", "epsc"], w=[f"ag2b{b}"])
        P.op("dve", lambda e: e.reciprocal(out=ag2[b][:, 2:3], in_=ag2[b][:, 2:3]), r=[f"ag2b{b}"], w=[f"ag2b{b}"])
        P.op("dve", lambda e: e.scalar_tensor_tensor(out=ag2[b][:, 3:4], in0=ag2[b][:, 0:1], scalar=-1.0, in1=ag2[b][:, 2:3], op0=ALU.mult, op1=ALU.mult),
             r=[f"ag2{b}", f"ag2b{b}"], w=[f"ag2c{b}"])
        P.op("act", lambda e: e.activation(out=qq[b], in_=r2[b], func=AF.Identity, bias=ag2[b][:, 3:4], scale=ag2[b][:, 2:3]),
             r=[f"r2{b}", f"ag2b{b}", f"ag2c{b}"], w=[f"qq{b}"])
        P.op("pool", lambda e: e.tensor_tensor(out=qq[b], in0=qq[b], in1=lnr[:, 2, :], op=ALU.mult), r=[f"qq{b}", "lnr"], w=[f"qq{b}"])
        P.op("dve", lambda e: e.tensor_tensor(out=ob[b], in0=qq[b], in1=lnr[:, 3, :], op=ALU.add), r=[f"qq{b}", "lnr"], w=[f"ob{b}"])
        P.op("sp", lambda e: e.dma_start(out=out_d[j * 128:(j + 1) * 128, :], in_=ob[b]), r=[f"ob{b}"], w=[f"out{j}"], dma=True)
        return P.end()

    for j in range(0, NT, 2):
        P.play(merge(chain2f(j), chain2f(j + 1)))


class _StopEmit(Exception):
    pass


def _host_prep(inp, b):
    f = np.float32
    L = 0
    cols = np.zeros((128, NCOL), f)
    cols[:, C_C:C_C + 8] = inp["c"][b].reshape(8, 128).T
    w_in = inp["w_in"][L]
    b_in = inp["b_in"][L]
    qcols = np.concatenate([np.arange(1024 + h * 64, 1024 + (h + 1) * 64) for h in HORDER])
    perm = np.concatenate([np.arange(0, 1024), qcols, np.arange(1536, 1792)])
    w_in_p = np.ascontiguousarray(w_in[:, perm])
    b_in_p = b_in[perm]
    cols[:, C_BIN:C_BIN + 13] = b_in_p[:1664].reshape(13, 128).T
    cols[:, C_CW:C_CW + 124] = inp["conv_w"][L].T.reshape(4, 128, 31).transpose(1, 0, 2).reshape(128, 124)
    cols[:, C_CB:C_CB + 4] = inp["conv_b"][L].reshape(4, 128).T
    cols[:, C_LG:C_LG + 4] = inp["conv_ln_g"][L].reshape(4, 128).T
    cols[:, C_LB:C_LB + 4] = inp["conv_ln_b"][L].reshape(4, 128).T
    cols[:, C_OG:C_OG + 4] = inp["conv_out_g"][L].reshape(4, 128).T
    cols[:, C_PID] = np.arange(128)
    rows = np.zeros((1, NROW), f)
    rows[0, R_BADA:R_BADA + 6144] = inp["b_ada"][L]
    rows[0, R_BV:R_BV + 128] = b_in[1664:1792]
    rows[0, R_SINK:R_SINK + 8] = inp["sinks"][L][HORDER]
    rows[0, R_AOG:R_AOG + 512] = inp["attn_out_g"][L].reshape(8, 64)[HORDER].reshape(-1)
    rows[0, R_BOUT:R_BOUT + D] = inp["b_out"][L]
    rows[0, R_L1G:R_L1G + D] = inp["ln1_g"][L]
    rows[0, R_L1B:R_L1B + D] = inp["ln1_b"][L]
    rows[0, R_L2G:R_L2G + D] = inp["ln2_g"][L]
    rows[0, R_L2B:R_L2B + D] = inp["ln2_b"][L]
    rows[0, R_BR:R_BR + 4] = inp["b_router_group"][L]
    rows[0, R_BR + 4:R_BR + 36] = inp["b_router_expert"][L]
    rows[0, R_SLOT:R_SLOT + NS] = np.arange(NS) * T
    w_out = inp["w_out"][L]
    arows = np.concatenate([np.arange(512 + h * 64, 512 + (h + 1) * 64) for h in HORDER])
    w_out_p = np.ascontiguousarray(np.concatenate([w_out[:512], w_out[arows]], axis=0))
    w_r = np.ascontiguousarray(np.concatenate([inp["w_router_group"][L], inp["w_router_expert"][L]], axis=1))
    return dict(cols=cols, rows=rows, w_in=w_in_p, w_out=w_out_p, w_r=w_r)


def _consts():
    f = np.float32
    cst = np.zeros((128, NK), f)
    cst[:, K_ID:K_ID + 128] = np.eye(128)
    p = np.arange(128)
    cst[:, K_U:K_U + 128] = (p[:, None] < p[None, :])
    cst[:, K_BD:K_BD + 128] = ((p[:, None] // 64) == (p[None, :] // 64)) / 64.0
    m0 = np.where(p[:, None] > p[None, :], 0.0, NEG)
    m1 = np.where(p[:, None] <= p[None, :], 0.0, NEG)
    cst[:, K_M0:K_M0 + 512] = np.tile(m0, (1, 4))
    cst[:, K_M1:K_M1 + 512] = np.tile(m1, (1, 4))
    return cst


def _expert_layout(inp):
    L = 0
    wg = np.ascontiguousarray(inp["w_gate"][L].reshape(NE, 8, 128, DE).transpose(0, 2, 1, 3)).reshape(NE * 128, 2048)
    wu = np.ascontiguousarray(inp["w_up"][L].reshape(NE, 8, 128, DE).transpose(0, 2, 1, 3)).reshape(NE * 128, 2048)
    wd = np.ascontiguousarray(inp["w_down"][L].reshape(NE, 2, 128, D).transpose(0, 2, 1, 3)).reshape(NE * 128, 2048)
    return wg, wu, wd


_CACHE = {}


def kernel(**inputs):
    inp = {k: np.asarray(v) for k, v in inputs.items()}
    dbg = _CACHE.get("dbg")
    nc = build_program(dbg)
    cst = _consts()
    wg, wu, wd = _expert_layout(inp)
    w_ada = np.ascontiguousarray(inp["w_ada"][0])
    in_maps = []
    ncores = _CACHE.get("ncores", 8)
    for b in range(ncores):
        hp = _host_prep(inp, b)
        m = dict(x=np.ascontiguousarray(inp["x"][b]), cols=hp["cols"], rows=hp["rows"], cst=cst, w_ada=w_ada,
                 w_in=hp["w_in"], w_out=hp["w_out"], w_r=hp["w_r"], wg=wg, wu=wu, wd=wd)
        if _CACHE.get("p2only"):
            m["z_in"] = _CACHE["z_in"]
        in_maps.append(m)
    if _CACHE.get("trace"):
        res = run_bass_kernel_spmd(nc, in_maps, core_ids=list(range(ncores)), trace=True)
        print("EXEC_NS", res.exec_time_ns)
    else:
        res = run_bass_kernel_spmd(nc, in_maps, core_ids=list(range(ncores)))
    if dbg:
        return [np.asarray(r["dbg"]) for r in res.results]
    out = np.stack([np.asarray(r["out"]) for r in res.results], axis=0).astype(np.float32)
    return out
```

```python
import os
import numpy as np
import concourse.bass as bass
import concourse.mybir as mybir
from concourse.bass_utils import run_bass_kernel_spmd

F32 = mybir.dt.float32
BF16 = mybir.dt.bfloat16
I32 = mybir.dt.int32
ALU = mybir.AluOpType
AF = mybir.ActivationFunctionType
AX = mybir.AxisListType

D = 1024
S = 4096
NT = S // 128
NMT = S // 512
DIN = 1792
NE = 32
DE = 256
ALPHA = 2.0 ** 0.25
EPS = 1e-5
NEG = -30000.0
T = 384
NS = (2 * S + NE * (T - 1) + T - 1) // T
NSUB = T // 128
HORDER = [0, 4, 1, 5, 2, 6, 3, 7]

C_C = 0
C_BIN = 8
C_CW = 21
C_CB = 145
C_LG = 149
C_LB = 153
C_OG = 157
C_PID = 161
NCOL = 162
R_BADA = 0
R_BV = 6144
R_SINK = 6272
R_AOG = 6280
R_BOUT = 6792
R_L1G = 7816
R_L1B = 8840
R_L2G = 9864
R_L2B = 10888
R_BR = 11912
R_SLOT = 11948
NROW = 12012
K_ID = 0
K_U = 128
K_BD = 256
K_M0 = 384
K_M1 = 896
NK = 1408


class Prog:
    def __init__(self, nc, sems):
        self.nc = nc
        self.ops = []
        self.last_w = {}
        self.readers = {}
        self.eng_sem = {e: sems[i] for i, e in enumerate(["pe", "act", "dve", "pool"])}
        rest = sems[4:]
        n_sp = (len(rest) * 5) // 10
        n_pool = (len(rest) * 4) // 10
        self.dma_pool = {"sp": rest[:n_sp], "pool": rest[n_sp:n_sp + n_pool], "act": rest[n_sp + n_pool:]}
        self._rec = None

    def rec(self):
        assert self._rec is None
        self._rec = []

    def end(self):
        l = self._rec
        self._rec = None
        return l

    def play(self, lst):
        assert self._rec is None
        for o in lst:
            self.op(*o)

    def op(self, eng, fn, r=(), w=(), dma=False):
        if self._rec is not None:
            self._rec.append((eng, fn, list(r), list(w), dma))
            return None
        i = len(self.ops)
        w = list(w) + [h for h in r if h.startswith("ps") and h not in w]
        raw, oth = set(), set()
        for h in r:
            if h in self.last_w:
                raw.add(self.last_w[h])
        for h in w:
            if h in self.last_w:
                oth.add(self.last_w[h])
            for j in self.readers.get(h, ()):
                oth.add(j)
        for h in w:
            self.last_w[h] = i
            self.readers[h] = []
        for h in r:
            self.readers.setdefault(h, []).append(i)
        deps = []
        for j in sorted(raw | oth):
            p = self.ops[j]
            if j == i:
                continue
            if (not p["dma"]) and p["eng"] == eng:
                if eng == "pe" or j not in raw:
                    continue
            deps.append(j)
        self.ops.append(dict(eng=eng, fn=fn, deps=deps, dma=dma, sig=False))
        for j in deps:
            self.ops[j]["sig"] = True
        return i

    def fence(self, engs=("pe", "act", "dve", "pool", "sp")):
        hs = list(self.last_w.keys())
        for e in engs:
            self.op(e, None, r=hs, w=["_fence_" + e])

    def emit(self):
        nc = self.nc
        ticket = {e: 0 for e in self.eng_sem}
        dma_next = {q: 0 for q in self.dma_pool}
        dma_uses = {}
        for o in self.ops:
            if o["dma"]:
                q = o["eng"]
                pool = self.dma_pool[q]
                sem = pool[dma_next[q] % len(pool)]
                dma_next[q] += 1
                u = dma_uses.get(id(sem), 0)
                o["pre"] = (sem, 16 * u)
                dma_uses[id(sem)] = u + 1
                o["ev"] = (sem, 16 * (u + 1))
            elif o["sig"] and o["fn"] is not None:
                ticket[o["eng"]] += 1
                o["ev"] = (self.eng_sem[o["eng"]], ticket[o["eng"]])
        ops = self.ops

        def run(engname, eobj):
            waited = {}

            def wait(sem, val):
                if val <= 0:
                    return
                if waited.get(id(sem), 0) >= val:
                    return
                eobj.wait_ge(sem, val)
                waited[id(sem)] = val

            for o in ops:
                if o["eng"] != engname:
                    continue
                for j in o["deps"]:
                    ev = ops[j].get("ev")
                    if ev is not None:
                        wait(*ev)
                if o["fn"] is None:
                    continue
                if o["dma"]:
                    wait(*o["pre"])
                    ins = o["fn"](eobj)
                    ins.then_inc(o["ev"][0], 16)
                else:
                    ins = o["fn"](eobj)
                    if o["sig"]:
                        ins.then_inc(o["ev"][0], 1)

        with nc.Block() as block:
            @block.tensor
            def _(e):
                run("pe", e)

            @block.scalar
            def _(e):
                run("act", e)

            @block.vector
            def _(e):
                run("dve", e)

            @block.gpsimd
            def _(e):
                run("pool", e)

            @block.sync
            def _(e):
                run("sp", e)


class _Stop(Exception):
    pass


def merge(*lists):
    lists = [l for l in lists if l]
    out = []
    idx = [0] * len(lists)
    total = sum(len(l) for l in lists)
    while len(out) < total:
        best, bv = None, None
        for k, l in enumerate(lists):
            if idx[k] < len(l):
                v = (idx[k] + 0.5) / len(l)
                if bv is None or v < bv:
                    best, bv = k, v
        out.append(lists[best][idx[best]])
        idx[best] += 1
    return out


class Arena:
    def __init__(self, t, nbytes):
        self.t = t
        self.nbytes = nbytes
        self.off = 0

    def alloc(self, shape, dt):
        esz = 4 if dt in (F32, I32) else 2
        n = int(np.prod(shape)) * esz
        n = (n + 63) // 64 * 64
        assert self.off + n <= self.nbytes, ("arena overflow", self.off, n, self.nbytes)
        v = self.t[:, self.off // 4:(self.off + n) // 4]
        self.off += n
        if dt != F32:
            v = v.bitcast(dt)
        v = v[:, 0:int(np.prod(shape))]
        if len(shape) == 2:
            return v.rearrange("p (a b) -> p a b", a=shape[0])
        if len(shape) == 3:
            return v.rearrange("p (a b c) -> p a b c", a=shape[0], b=shape[1])
        return v


def build_program(dbg=None):
    nc = bass.Bass("TRN2", target_bir_lowering=False)
    try:
        return _build_program(nc, dbg)
    except _Stop:
        return nc


def _build_program(nc, dbg=None):
    dr = {}

    def din(name, shape, dt=F32):
        dr[name] = nc.dram_tensor(name, list(shape), dt, kind="ExternalInput").ap()
        return dr[name]

    x_d = din("x", [S, D])
    cols_d = din("cols", [128, NCOL])
    rows_d = din("rows", [1, NROW])
    cst_d = din("cst", [128, NK])
    wada_d = din("w_ada", [D, 6 * D])
    win_d = din("w_in", [D, DIN])
    wout_d = din("w_out", [D, D])
    wr_d = din("w_r", [D, 36])
    wg_d = din("wg", [NE * 128, 2048])
    wu_d = din("wu", [NE * 128, 2048])
    wd_d = din("wd", [NE * 128, 2048])
    out_d = nc.dram_tensor("out", [S, D], F32, kind="ExternalOutput").ap()
    if _CACHE.get("p2only"):
        z_d = din("z_in", [S, D])
    else:
        z_d = nc.dram_tensor("z_scr", [S, D], F32, kind="Internal").ap()
    xs_d = nc.dram_tensor("xs_scr", [NS * T, D], BF16, kind="Internal").ap()
    ys_d = nc.dram_tensor("ys_scr", [NS * T, D], F32, kind="Internal").ap()
    dbg_d = None
    if dbg:
        dbg_d = nc.dram_tensor("dbg", list(dbg["shape"]), F32, kind="ExternalOutput").ap()

    import contextlib
    with contextlib.ExitStack() as st:
        ARENA_BYTES = 206 * 1024
        arena_t = st.enter_context(nc.sbuf_tensor("arena", [128, ARENA_BYTES // 4], F32))
        ps = [st.enter_context(nc.psum_tensor(f"ps{i}", [128, 512], F32)) for i in range(6)]
        psb = [st.enter_context(nc.psum_tensor(f"psb{i}", [128, 1024], BF16)) for i in range(2)]
        sems = [st.enter_context(nc.semaphore(f"s{i}")) for i in range(_CACHE.get("nsem", 48))]
        P = Prog(nc, sems)
        A = Arena(arena_t, ARENA_BYTES)
        psn = [0]

        def nps():
            i = psn[0] % 6
            psn[0] += 1
            return ps[i], f"ps{i}"

        cols = A.alloc([NCOL], F32)
        identf = A.alloc([128], F32)
        identb = A.alloc([128], BF16)
        onesb = A.alloc([128], BF16)
        ones512 = A.alloc([128], BF16)
        bdb = A.alloc([128], BF16)
        Ub = A.alloc([128], BF16)
        maskT = A.alloc([2, 512], BF16)
        modc = A.alloc([32], F32)
        g1bc = A.alloc([D], F32)
        gbrow = A.alloc([D], BF16)
        EPSC = A.alloc([1], F32)
        EPSC2 = A.alloc([1], F32)
        mark_persist = A.off
        cst = A.alloc([NK], F32)
        gbc = A.alloc([4, D], F32)
        mod_d = nc.dram_tensor("mod_scr", [1, 3 * D], F32, kind="Internal").ap()

        P.op("sp", lambda e: e.dma_start(out=cols, in_=cols_d), w=["cols"], dma=True)
        P.op("sp", lambda e: e.dma_start(out=cst, in_=cst_d), w=["cst0"], dma=True)
        P.op("sp", lambda e: e.dma_start(out=identf, in_=cst_d[:, K_ID:K_ID + 128]), w=["cst"], dma=True)
        P.op("dve", lambda e: e.tensor_copy(out=identb, in_=identf), r=["cst"], w=["identb"])
        P.op("dve", lambda e: e.memset(onesb, 1.0), w=["onesb"])
        P.op("dve", lambda e: e.memset(EPSC, EPS), w=["epsc"])
        P.op("dve", lambda e: e.memset(EPSC2, EPS / (ALPHA * ALPHA)), w=["epsc2"])
        P.op("dve", lambda e: e.memset(ones512, 1.0 / 512.0), w=["ones512"])
        P.op("dve", lambda e: e.tensor_copy(out=bdb, in_=cst[:, K_BD:K_BD + 128]), r=["cst0"], w=["bdb"])
        P.op("dve", lambda e: e.tensor_copy(out=Ub, in_=cst[:, K_U:K_U + 128]), r=["cst0"], w=["Ub"])
        P.op("dve", lambda e: e.tensor_copy(out=maskT.rearrange("p a b -> p (a b)"), in_=cst[:, K_M0:K_M0 + 1024]),
             r=["cst0"], w=["maskT"])

        ph0 = A.off
        cact = A.alloc([8], F32)
        cbc = A.alloc([8, 128], BF16)
        wab = [A.alloc([8, 512], BF16) for _ in range(2)]
        badab = A.alloc([512], F32)
        modb = A.alloc([4 * D], F32)
        P.op("act", lambda e: e.activation(out=cact, in_=cols[:, C_C:C_C + 8], func=AF.Silu), r=["cols"], w=["cact"])
        for c in range(8):
            P.op("dve", lambda e, c=c: e.tensor_scalar(out=cbc[:, c, :], in0=onesb, scalar1=cact[:, c:c + 1],
                                                       scalar2=None, op0=ALU.mult),
                 r=["cact", "onesb"], w=["cbc"])
        for blk in range(12):
            wb = wab[blk % 2]
            hw = f"wab{blk % 2}"
            P.op("pool", lambda e, blk=blk, wb=wb: e.dma_start(
                out=wb, in_=wada_d[:, blk * 512:(blk + 1) * 512].rearrange("(c p) n -> p c n", p=128)),
                w=[hw], dma=True)
            P.op("sp", lambda e, blk=blk: e.dma_start(
                out=badab, in_=rows_d[0:1, R_BADA + blk * 512:R_BADA + (blk + 1) * 512].partition_broadcast(128)),
                w=["badab"], dma=True)
            pt, hp = nps()
            for c in range(8):
                P.op("pe", lambda e, c=c, pt=pt, wb=wb: e.matmul(pt[:], lhsT=cbc[:, c, :], rhs=wb[:, c, :],
                                                               start=(c == 0), stop=(c == 7)),
                     r=["cbc", hw], w=[hp])
            if blk < 4:
                dst = modb[:, blk * 512:(blk + 1) * 512]
                hd = f"modb{blk}"
            else:
                gi = (blk - 4) // 2
                dst = gbc[:, gi, ((blk - 4) % 2) * 512:((blk - 4) % 2 + 1) * 512]
                hd = f"gbc{gi}_{blk % 2}"
            P.op("dve", lambda e, pt=pt, dst=dst: e.tensor_tensor(out=dst, in0=pt[:], in1=badab, op=ALU.add),
                 r=[hp, "badab"], w=[hd])
        srcs = []
        for c in range(8):
            srcs.append((modb[:, c * 128:(c + 1) * 128], f"modb{c // 4}", c, 0.0))
        for c in range(8):
            srcs.append((modb[:, 1024 + c * 128:1024 + (c + 1) * 128], f"modb{2 + c // 4}", 8 + c, 1.0))
        for c in range(8):
            srcs.append((gbc[:, 1, c * 128:(c + 1) * 128], f"gbc1_{c // 4}", 16 + c, 0.0))
        for c in range(8):
            srcs.append((gbc[:, 2, c * 128:(c + 1) * 128], f"gbc2_{c // 4}", 24 + c, 1.0))
        for (src, hs, col, add) in srcs:
            pt, hp = nps()
            P.op("pe", lambda e, pt=pt, src=src: e.transpose(out=pt[:, 0:128], in_=src, identity=identf),
                 r=[hs, "cst"], w=[hp])
            P.op("dve", lambda e, pt=pt, col=col, add=add: e.tensor_scalar(
                out=modc[:, col:col + 1], in0=pt[:, 0:1], scalar1=add, scalar2=None, op0=ALU.add),
                r=[hp], w=["modc"])
        G_ALL = [f"gbc{gi}_{h}" for gi in range(4) for h in range(2)]
        P.op("sp", lambda e: e.dma_start(out=mod_d, in_=gbc[0:1, 1:4, :].rearrange("p a d -> p (a d)")), r=G_ALL, w=["mod_d"], dma=True)
        P.op("dve", lambda e: e.tensor_scalar(out=g1bc, in0=gbc[:, 0, :], scalar1=1.0 / ALPHA, scalar2=None, op0=ALU.mult), r=G_ALL, w=["g1bc"])
        boutb = modb[:, 0:D]
        P.op("sp", lambda e: e.dma_start(out=boutb, in_=rows_d[0:1, R_BOUT:R_BOUT + D].partition_broadcast(128)),
             w=["modb0", "modb1"], dma=True)
        P.op("dve", lambda e: e.tensor_tensor(out=boutb, in0=boutb, in1=g1bc, op=ALU.mult), r=["modb0", "modb1", "g1bc"], w=["modb0", "modb1"])
        P.op("dve", lambda e: e.tensor_copy(out=gbrow, in_=boutb), r=["modb0", "modb1"], w=["gbrow"])
        P.fence()
        if _CACHE.get("stop") == 0:
            P.op("sp", lambda e: e.dma_start(out=dbg_d[0:128, 0:32], in_=modc), r=["modc"], w=["dbg"], dma=True)
            P.op("sp", lambda e: e.dma_start(out=dbg_d[128:256, :], in_=gbc[:, 0, :]), r=["gbc0_0", "gbc0_1"], w=["dbg2"], dma=True)
            P.fence()
            P.emit()
            return nc
        A.off = mark_persist

        win = A.alloc([8, DIN], BF16)
        modc2 = A.alloc([32], F32)
        P.op("dve", lambda e: e.tensor_copy(out=modc2, in_=modc), r=["modc"], w=["modc2"])
        wout = A.alloc([8, D], BF16)
        diag = A.alloc([124, 128], BF16)
        xt = A.alloc([4, D], BF16)
        hT = A.alloc([8, 512], BF16)
        vr = [A.alloc([4, 542], BF16) for _ in range(2)]
        kr = [[A.alloc([640], BF16) for _ in range(2)] for _ in range(2)]
        va = [A.alloc([5, 130], BF16) for _ in range(2)]
        qT = A.alloc([4, 512], BF16)
        sig = A.alloc([512], F32)
        ybf = A.alloc([4, 512], BF16)
        y2bf = A.alloc([4, 512], BF16)
        mean_sb = A.alloc([512], F32)
        m2_sb = A.alloc([512], F32)
        rstd_sb = A.alloc([512], F32)
        nmr_sb = A.alloc([512], F32)
        zc2 = [A.alloc([512], F32) for _ in range(2)]
        sc2_ = [A.alloc([512], F32) for _ in range(2)]
        s2bf2 = [A.alloc([512], BF16) for _ in range(2)]
        r2c2 = [A.alloc([512], F32) for _ in range(2)]
        ycT = A.alloc([8, 512], BF16)
        mx2 = [A.alloc([8], F32) for _ in range(2)]
        mxb2 = [A.alloc([8], BF16) for _ in range(2)]
        nmx2 = [A.alloc([8], F32) for _ in range(2)]
        dcat2 = [A.alloc([2, 512], BF16) for _ in range(2)]
        ET2 = [A.alloc([4, 512], BF16) for _ in range(2)]
        es_t2 = [A.alloc([8], F32) for _ in range(2)]
        den2 = [A.alloc([8], F32) for _ in range(2)]
        osb2 = [A.alloc([8, 64], F32) for _ in range(2)]
        osq2 = [A.alloc([8, 64], F32) for _ in range(2)]
        ssq2 = [A.alloc([8], F32) for _ in range(2)]
        yat2 = [A.alloc([512], BF16) for _ in range(2)]
        rows_sb = A.alloc([128 + 8 + 512], F32)
        xr2 = [A.alloc([D], F32) for _ in range(2)]
        rr2 = [A.alloc([D], F32) for _ in range(2)]
        bnst2 = [A.alloc([12], F32) for _ in range(2)]
        bnag2 = [A.alloc([4], F32) for _ in range(2)]
        print("phase1 arena bytes", A.off)

        def mk_nps(ids):
            st_ = [0]

            def f():
                i = ids[st_[0] % len(ids)]
                st_[0] += 1
                return ps[i], f"ps{i}"
            return f

        bv_bc = rows_sb[:, 0:128]
        sink_bc = rows_sb[:, 128:136]
        aog_bc = rows_sb[:, 136:648]
        P.op("sp", lambda e: e.dma_start(out=rows_sb, in_=rows_d[0:1, R_BV:R_BV + 128 + 8 + 512].partition_broadcast(128)),
             w=["rows_sb"], dma=True)
        for c in range(8):
            P.op("pool", lambda e, c=c: e.dma_start(out=win[:, c, :], in_=win_d[c * 128:(c + 1) * 128, :]), w=[f"win{c}"], dma=True)
            P.op("pool", lambda e, c=c: e.dma_start(out=wout[:, c, :], in_=wout_d[c * 128:(c + 1) * 128, :]), w=[f"wout{c}"], dma=True)
        for c in range(8):
            P.op("dve", lambda e, c=c: e.tensor_tensor(out=wout[:, c, :], in0=wout[:, c, :], in1=g1bc, op=ALU.mult),
                 r=[f"wout{c}", "g1bc"], w=[f"wout{c}"])
        WINH = [f"win{c}" for c in range(8)]
        WOUTH = [f"wout{c}" for c in range(8)]
        SK = _CACHE.get("skip", set())
        for c in range(4 if "diag" not in SK else 0):
            P.op("dve", lambda e, c=c: e.tensor_tensor(
                out=diag[:, c * 31:(c + 1) * 31, :], in0=identb.unsqueeze(1).to_broadcast([128, 31, 128]),
                in1=cols[:, C_CW + c * 31:C_CW + (c + 1) * 31].unsqueeze(2).to_broadcast([128, 31, 128]), op=ALU.mult),
                r=["identb", "cols"], w=["diag"])
        if "memset" not in SK:
            P.op("pool", lambda e: e.memset(vr[0], 0.0), w=["vr0"])
            P.op("pool", lambda e: e.memset(vr[1], 0.0), w=["vr1"])
        for par in range(2 if "memset" not in SK else 0):
            for g in range(2):
                P.op("pool", lambda e, par=par, g=g: e.memset(kr[par][g], 0.0), w=[f"kr{par}"])
            P.op("pool", lambda e, par=par: e.memset(va[par], 0.0), w=[f"va{par}"])
            P.op("dve", lambda e, par=par: e.memset(va[par][:, 1:, 64:65], 1.0), r=[f"va{par}"], w=[f"va{par}"])
            P.op("dve", lambda e, par=par: e.memset(va[par][:, 1:, 129:130], 1.0), r=[f"va{par}"], w=[f"va{par}"])

        if _CACHE.get("stop") == 1:
            P.fence()
            P.op("sp", lambda e: e.dma_start(out=dbg_d[0:128, 0:512], in_=g1bc[:, 0:512]), r=["g1bc"], w=["dbg"], dma=True)
            P.fence()
            P.emit()
            return nc
        for mt in range(0 if _CACHE.get("p2only") else _CACHE.get('nmt_run', NMT)):
            t0 = mt * 512
            vcur, vprev = vr[mt % 2], vr[(mt + 1) % 2]
            hv, hvp = f"vr{mt % 2}", f"vr{(mt + 1) % 2}"
            kT, kTp = kr[mt % 2], kr[(mt + 1) % 2]
            hk, hkp = f"kr{mt % 2}", f"kr{(mt + 1) % 2}"
            vaug, vaugp = va[mt % 2], va[(mt + 1) % 2]
            hva, hvap = f"va{mt % 2}", f"va{(mt + 1) % 2}"
            P.op("pool", lambda e, t0=t0: e.dma_start(out=xt, in_=x_d[t0:t0 + 512, :].rearrange("(s p) d -> p s d", p=128)),
                 w=["xt"], dma=True)
            for c in range(8):
                ptx = psb[c % 2][:, 0:512]
                hp = f"psb{c % 2}"
                for s in range(4 if "notr" not in SK else 0):
                    P.op("pe", lambda e, ptx=ptx, s=s, c=c: e.transpose(
                        out=ptx[:, s * 128:(s + 1) * 128], in_=xt[:, s, c * 128:(c + 1) * 128], identity=identb),
                        r=["xt", "identb"], w=[hp])
                if "noact" not in SK:
                    P.op("act", lambda e, ptx=ptx, c=c: e.activation(
                        out=hT[:, c, :], in_=ptx[:, 0:512], func=AF.Identity, bias=modc2[:, c:c + 1], scale=(1.0 if "fscale" in SK else modc2[:, 8 + c:9 + c])),
                        r=[hp, "modc2"], w=["hT"])
            def stop_at(k):
                if _CACHE.get("stop") == k:
                    P.fence()
                    P.op("sp", lambda e: e.dma_start(out=dbg_d[0:128, 0:512], in_=g1bc[:, 0:512]), r=["g1bc"], w=["dbg"], dma=True)
                    P.fence()
                    P.emit()
                    raise _Stop()
            stop_at(2)
            if mt > 0:
                P.op("pool", lambda e, vcur=vcur, vprev=vprev: e.tensor_copy(out=vcur[:, :, 0:30], in_=vprev[:, :, 512:542]),
                     r=[hvp], w=[hv])
                for g in range(2):
                    P.op("pool", lambda e, kT=kT, kTp=kTp, g=g: e.tensor_copy(out=kT[g][:, 0:128], in_=kTp[g][:, 512:640]),
                         r=[hkp], w=[hk])
                P.op("pool", lambda e, vaug=vaug, vaugp=vaugp: e.tensor_copy(out=vaug[:, 0, :], in_=vaugp[:, 4, :]),
                     r=[hvap], w=[hva])
            for c in range(4):
                pb, hpb = nps()
                for k in range(8):
                    P.op("pe", lambda e, pb=pb, k=k, c=c: e.matmul(
                        pb[:], lhsT=win[:, k, 512 + c * 128:512 + (c + 1) * 128], rhs=hT[:, k, :],
                        start=(k == 0), stop=(k == 7)), r=WINH + ["hT"], w=[hpb])
                pa, hpa = nps()
                for k in range(8):
                    P.op("pe", lambda e, pa=pa, k=k, c=c: e.matmul(
                        pa[:], lhsT=win[:, k, c * 128:(c + 1) * 128], rhs=hT[:, k, :],
                        start=(k == 0), stop=(k == 7)), r=WINH + ["hT"], w=[hpa])
                P.op("act", lambda e, pb=pb, c=c: e.activation(
                    out=sig, in_=pb[:], func=AF.Sigmoid, bias=cols[:, C_BIN + 4 + c:C_BIN + 5 + c], scale=1.0),
                    r=[hpb, "cols"], w=["sig"])
                P.op("dve", lambda e, pa=pa, c=c, vcur=vcur: e.scalar_tensor_tensor(
                    out=vcur[:, c, 30:542], in0=pa[:], scalar=cols[:, C_BIN + c:C_BIN + c + 1], in1=sig,
                    op0=ALU.add, op1=ALU.mult), r=[hpa, "sig", "cols"], w=[hv])
            for i in range(4):
                pq, hpq = nps()
                for k in range(8):
                    P.op("pe", lambda e, pq=pq, k=k, i=i: e.matmul(
                        pq[:], lhsT=win[:, k, 1024 + i * 128:1024 + (i + 1) * 128], rhs=hT[:, k, :],
                        start=(k == 0), stop=(k == 7)), r=WINH + ["hT"], w=[hpq])
                P.op("dve", lambda e, pq=pq, i=i: e.tensor_scalar(
                    out=qT[:, i, :], in0=pq[:], scalar1=cols[:, C_BIN + 8 + i:C_BIN + 9 + i], scalar2=0.125,
                    op0=ALU.add, op1=ALU.mult), r=[hpq, "cols"], w=["qT"])
            pk, hpk = nps()
            for k in range(8):
                P.op("pe", lambda e, pk=pk, k=k: e.matmul(
                    pk[:], lhsT=win[:, k, 1536:1664], rhs=hT[:, k, :], start=(k == 0), stop=(k == 7)),
                    r=WINH + ["hT"], w=[hpk])
            for g in range(2):
                P.op("act", lambda e, pk=pk, g=g, kT=kT: e.activation(
                    out=kT[g][g * 64:(g + 1) * 64, 128:640], in_=pk[g * 64:(g + 1) * 64, :],
                    func=AF.Identity, bias=cols[g * 64:(g + 1) * 64, C_BIN + 12:C_BIN + 13], scale=1.0),
                    r=[hpk, "cols"], w=[hk])
            pv, hpv = nps()
            for s in range(4):
                for k in range(8):
                    P.op("pe", lambda e, pv=pv, s=s, k=k: e.matmul(
                        pv[:, s * 128:(s + 1) * 128], lhsT=hT[:, k, s * 128:(s + 1) * 128], rhs=win[:, k, 1664:1792],
                        start=(k == 0), stop=(k == 7)), r=WINH + ["hT"], w=[hpv])
            for s in range(4):
                blk = s + 1
                P.op("dve", lambda e, pv=pv, s=s, blk=blk, vaug=vaug: e.tensor_tensor(
                    out=vaug[:, blk, :].rearrange("p (g d) -> p g d", g=2)[:, :, 0:64],
                    in0=pv[:, s * 128:(s + 1) * 128].rearrange("p (g d) -> p g d", g=2),
                    in1=bv_bc.rearrange("p (g d) -> p g d", g=2), op=ALU.add),
                    r=[hpv, "rows_sb"], w=[hva])
            stop_at(3)
            for c in range(4):
                py, hpy = nps()
                for j in range(31):
                    P.op("pe", lambda e, py=py, c=c, j=j, vcur=vcur: e.matmul(
                        py[:], lhsT=diag[:, c * 31 + j, :], rhs=vcur[:, c, j:j + 512], start=(j == 0), stop=(j == 30)),
                        r=["diag", hv], w=[hpy])
                P.op("act", lambda e, py=py, c=c: e.activation(
                    out=ybf[:, c, :], in_=py[:], func=AF.Identity, bias=cols[:, C_CB + c:C_CB + c + 1], scale=1.0),
                    r=[hpy, "cols"], w=["ybf"])
                P.op("act", lambda e, py=py, c=c: e.activation(
                    out=y2bf[:, c, :], in_=py[:], func=AF.Square, bias=cols[:, C_CB + c:C_CB + c + 1], scale=1.0),
                    r=[hpy, "cols"], w=["y2bf"])
            pm, hpm = nps()
            for c in range(4):
                P.op("pe", lambda e, pm=pm, c=c: e.matmul(pm[:], lhsT=ones512, rhs=ybf[:, c, :], start=(c == 0), stop=(c == 3)),
                     r=["ones512", "ybf"], w=[hpm])
            pe2, hpe2 = nps()
            for c in range(4):
                P.op("pe", lambda e, pe2=pe2, c=c: e.matmul(pe2[:], lhsT=ones512, rhs=y2bf[:, c, :], start=(c == 0), stop=(c == 3)),
                     r=["ones512", "y2bf"], w=[hpe2])
            P.op("act", lambda e, pm=pm: e.activation(out=mean_sb, in_=pm[:], func=AF.Identity), r=[hpm], w=["mean_sb"])
            P.op("dve", lambda e: e.tensor_tensor(out=m2_sb, in0=mean_sb, in1=mean_sb, op=ALU.mult), r=["mean_sb"], w=["m2_sb"])
            P.op("dve", lambda e, pe2=pe2: e.tensor_tensor(out=m2_sb, in0=pe2[:], in1=m2_sb, op=ALU.subtract),
                 r=[hpe2, "m2_sb"], w=["m2_sb"])
            P.op("act", lambda e: e.activation(out=rstd_sb, in_=m2_sb, func=AF.Sqrt, bias=EPSC, scale=1.0), r=["m2_sb", "epsc"], w=["rstd_sb"])
            P.op("dve", lambda e: e.reciprocal(out=rstd_sb, in_=rstd_sb), r=["rstd_sb"], w=["rstd_sb"])
            P.op("dve", lambda e: e.scalar_tensor_tensor(out=nmr_sb, in0=mean_sb, scalar=-1.0, in1=rstd_sb, op0=ALU.mult, op1=ALU.mult),
                 r=["mean_sb", "rstd_sb"], w=["nmr_sb"])
            nps_cl = [mk_nps([0]), mk_nps([1])]
            nps_at = [mk_nps([2, 3]), mk_nps([4, 5])]
            nps_ep = [mk_nps([0, 1, 2]), mk_nps([3, 4, 5])]

            def convln_chain(c):
                k = c % 2
                zc, sc_, s2bf, r2c = zc2[k], sc2_[k], s2bf2[k], r2c2[k]
                P.rec()
                P.op("dve", lambda e: e.tensor_tensor(out=zc, in0=ybf[:, c, :], in1=rstd_sb, op=ALU.mult),
                     r=["ybf", "rstd_sb"], w=[f"zc{k}"])
                P.op("dve", lambda e: e.tensor_tensor(out=zc, in0=zc, in1=nmr_sb, op=ALU.add), r=[f"zc{k}", "nmr_sb"], w=[f"zc{k}"])
                P.op("act", lambda e: e.activation(out=sc_, in_=zc, func=AF.Silu, bias=cols[:, C_LB + c:C_LB + c + 1],
                                                   scale=cols[:, C_LG + c:C_LG + c + 1]), r=[f"zc{k}", "cols"], w=[f"sc{k}"])
                P.op("act", lambda e: e.activation(out=s2bf, in_=sc_, func=AF.Square), r=[f"sc{k}"], w=[f"s2bf{k}"])
                pr, hpr = nps_cl[k]()
                P.op("pe", lambda e: e.matmul(pr[:], lhsT=bdb, rhs=s2bf, start=True, stop=True), r=["bdb", f"s2bf{k}"], w=[hpr])
                P.op("act", lambda e: e.activation(out=r2c, in_=pr[:], func=AF.Sqrt, bias=EPSC, scale=1.0), r=[hpr, "epsc"], w=[f"r2c{k}"])
                P.op("dve", lambda e: e.reciprocal(out=r2c, in_=r2c), r=[f"r2c{k}"], w=[f"r2c{k}"])
                P.op("dve", lambda e: e.scalar_tensor_tensor(out=ycT[:, c, :], in0=sc_, scalar=cols[:, C_OG + c:C_OG + c + 1],
                                                             in1=r2c, op0=ALU.mult, op1=ALU.mult),
                     r=[f"sc{k}", f"r2c{k}", "cols"], w=[f"ycTc{c}"])
                return P.end()

            def attn_chain(s):
                k = s % 2
                mx, mxb, nmx, dcat, ET = mx2[k], mxb2[k], nmx2[k], dcat2[k], ET2[k]
                es_t, den, osb, osq, ssq, yat = es_t2[k], den2[k], osb2[k], osq2[k], ssq2[k], yat2[k]
                mynps = nps_at[k]
                kT_l, vaug_l = kT, vaug
                n = mt * 4 + s
                qs = slice(s * 128, (s + 1) * 128)
                P.rec()
                for i in range(4):
                    pS, hpS = mynps()
                    for g in range(2):
                        P.op("pe", lambda e, pS=pS, i=i, g=g: e.matmul(
                            pS[:, g * 256:(g + 1) * 256], lhsT=qT[:, i, qs], rhs=kT_l[g][:, s * 128:s * 128 + 256],
                            start=True, stop=True), r=["qT", hk], w=[hpS])
                    P.op("dve", lambda e, pS=pS, i=i: e.tensor_reduce(
                        out=mx[:, 2 * i:2 * i + 2], in_=pS[:].rearrange("p (g k) -> p g k", g=2), axis=AX.X, op=ALU.max),
                        r=[hpS], w=[f"mx{k}"])
                P.op("dve", lambda e: e.tensor_copy(out=mxb, in_=mx), r=[f"mx{k}"], w=[f"mxb{k}"])
                P.op("dve", lambda e: e.tensor_scalar(out=nmx, in0=mxb, scalar1=-1.0, scalar2=None, op0=ALU.mult), r=[f"mxb{k}"], w=[f"nmx{k}"])
                for g in range(2):
                    P.op("dve", lambda e, g=g: e.tensor_tensor(
                        out=dcat[:, g, :].rearrange("p (i q) -> p i q", i=4), in0=identb.unsqueeze(1).to_broadcast([128, 4, 128]),
                        in1=nmx.rearrange("p (i g) -> p i g", g=2)[:, :, g:g + 1].to_broadcast([128, 4, 128]), op=ALU.mult),
                        r=["identb", f"nmx{k}"], w=[f"dcat{k}_{g}"])
                khs = [1] if n == 0 else [0, 1]
                for g in range(2):
                    for kh in khs:
                        pT, hpT = mynps()
                        kc = slice(s * 128 + kh * 128, s * 128 + kh * 128 + 128)
                        P.op("pe", lambda e, pT=pT, g=g, kc=kc: e.matmul(
                            pT[:].rearrange("p (i q) -> p i q", i=4), lhsT=kT_l[g][:, kc], rhs=qT[:, :, qs], start=True, stop=False),
                            r=[hk, "qT"], w=[hpT])
                        P.op("pe", lambda e, pT=pT, g=g: e.matmul(pT[:], lhsT=onesb, rhs=dcat[:, g, :], start=False, stop=False),
                             r=["onesb", f"dcat{k}_{g}"], w=[hpT])
                        P.op("pe", lambda e, pT=pT, kh=kh: e.matmul(pT[:], lhsT=identb, rhs=maskT[:, kh, :], start=False, stop=True),
                             r=["identb", "maskT"], w=[hpT])
                        P.op("act", lambda e, pT=pT, g=g, kh=kh: e.activation(out=ET[:, g * 2 + kh, :], in_=pT[:], func=AF.Exp),
                             r=[hpT], w=[f"ET{k}_{g}{kh}"])
                P.op("dve", lambda e: e.tensor_tensor(out=es_t, in0=sink_bc, in1=nmx, op=ALU.add), r=["rows_sb", f"nmx{k}"], w=[f"es_t{k}"])
                P.op("act", lambda e: e.activation(out=es_t, in_=es_t, func=AF.Exp), r=[f"es_t{k}"], w=[f"es_t{k}"])
                for g in range(2):
                    po, hpo = mynps()
                    for i in range(4):
                        for kh in khs:
                            P.op("pe", lambda e, po=po, g=g, i=i, kh=kh: e.matmul(
                                po[:, i * 65:(i + 1) * 65], lhsT=ET[:, g * 2 + kh, i * 128:(i + 1) * 128],
                                rhs=vaug_l[:, s + kh, g * 65:(g + 1) * 65], start=(kh == khs[0]), stop=(kh == 1)),
                                r=[f"ET{k}_{g}{kh}", hva], w=[hpo])
                    P.op("dve", lambda e, po=po, g=g: e.tensor_tensor(
                        out=den.rearrange("p (i g) -> p i g", g=2)[:, :, g:g + 1],
                        in0=po[:, 0:260].rearrange("p (i d) -> p i d", d=65)[:, :, 64:65],
                        in1=es_t.rearrange("p (i g) -> p i g", g=2)[:, :, g:g + 1], op=ALU.add),
                        r=[hpo, f"es_t{k}"], w=[f"den{k}_{g}"])
                    P.op("act", lambda e, po=po, g=g: e.activation(
                        out=osb.rearrange("p (i g) d -> p i g d", g=2)[:, :, g, :],
                        in_=po[:, 0:260].rearrange("p (i d) -> p i d", d=65)[:, :, 0:64], func=AF.Identity),
                        r=[hpo], w=[f"osb{k}_{g}"])
                DH = [f"den{k}_0", f"den{k}_1"]
                OH = [f"osb{k}_0", f"osb{k}_1"]
                P.op("dve", lambda e: e.reciprocal(out=den, in_=den), r=DH, w=DH)
                P.op("dve", lambda e: e.tensor_tensor(out=osb, in0=osb, in1=den.unsqueeze(2).to_broadcast([128, 8, 64]), op=ALU.mult),
                     r=OH + DH, w=OH)
                P.op("act", lambda e: e.activation(out=osq, in_=osb, func=AF.Square), r=OH, w=[f"osq{k}"])
                P.op("dve", lambda e: e.tensor_reduce(out=ssq, in_=osq, axis=AX.X, op=ALU.add), r=[f"osq{k}"], w=[f"ssq{k}"])
                P.op("act", lambda e: e.activation(out=ssq, in_=ssq, func=AF.Sqrt, bias=EPSC, scale=1.0 / 64.0), r=[f"ssq{k}", "epsc"], w=[f"ssq{k}"])
                P.op("dve", lambda e: e.reciprocal(out=ssq, in_=ssq), r=[f"ssq{k}"], w=[f"ssq{k}"])
                P.op("dve", lambda e: e.tensor_tensor(out=osb, in0=osb, in1=ssq.unsqueeze(2).to_broadcast([128, 8, 64]), op=ALU.mult),
                     r=OH + [f"ssq{k}"], w=OH)
                P.op("dve", lambda e: e.tensor_tensor(out=yat, in0=osb.rearrange("p h d -> p (h d)"), in1=aog_bc, op=ALU.mult),
                     r=OH + ["rows_sb"], w=[f"yat{k}"])
                ptb = psb[k][:, 0:512]
                hptr = f"psb{k}"
                for i in range(4):
                    P.op("pe", lambda e, i=i: e.transpose(out=ptb[:, i * 128:(i + 1) * 128], in_=yat[:, i * 128:(i + 1) * 128],
                                                         identity=identb), r=[f"yat{k}", "identb"], w=[hptr])
                P.op("act", lambda e: e.activation(
                    out=ycT[:, 4:8, qs], in_=ptb[:, 0:512].rearrange("p (i q) -> p i q", i=4), func=AF.Identity),
                    r=[hptr], w=[f"ycTa{s}"])
                return P.end()

            def xr_load(s):
                k = s % 2
                tt = mt * 4 + s
                P.op("sp", lambda e: e.dma_start(out=xr2[k], in_=x_d[tt * 128:(tt + 1) * 128, :]), w=[f"xr{k}"], dma=True)

            def epi_chain(s, with_load):
                k = s % 2
                tt = mt * 4 + s
                xr, rr, bnst, bnag = xr2[k], rr2[k], bnst2[k], bnag2[k]
                mynps = nps_ep[k]
                YH = [f"ycTc{c}" for c in range(4)] + [f"ycTa{s}"]
                P.rec()
                if with_load:
                    xr_load(s)
                for h in range(2):
                    po, hpo = mynps()
                    for c in range(8):
                        P.op("pe", lambda e, po=po, c=c, h=h: e.matmul(
                            po[:], lhsT=ycT[:, c, s * 128:(s + 1) * 128], rhs=wout[:, c, h * 512:(h + 1) * 512],
                            start=(c == 0), stop=False), r=YH + WOUTH, w=[hpo])
                    P.op("pe", lambda e, po=po, h=h: e.matmul(
                        po[:], lhsT=onesb[0:1, :], rhs=gbrow[0:1, h * 512:(h + 1) * 512], start=False, stop=True),
                        r=["onesb", "gbrow"], w=[hpo])
                    P.op("dve", lambda e, po=po, h=h: e.tensor_tensor(out=rr[:, h * 512:(h + 1) * 512], in0=po[:],
                                                                      in1=xr[:, h * 512:(h + 1) * 512], op=ALU.add),
                         r=[hpo, f"xr{k}"], w=[f"rr{k}"])
                for h in range(2):
                    P.op("dve", lambda e, h=h: e.bn_stats(out=bnst[:, h * 6:(h + 1) * 6], in_=rr[:, h * 512:(h + 1) * 512]),
                         r=[f"rr{k}"], w=[f"bnst{k}"])
                P.op("dve", lambda e: e.bn_aggr(out=bnag[:, 0:2], in_=bnst), r=[f"bnst{k}"], w=[f"bnag{k}"])
                P.op("act", lambda e: e.activation(out=bnag[:, 2:3], in_=bnag[:, 1:2], func=AF.Sqrt, bias=EPSC2, scale=1.0),
                     r=[f"bnag{k}", "epsc2"], w=[f"bnagb{k}"])
                P.op("dve", lambda e: e.reciprocal(out=bnag[:, 2:3], in_=bnag[:, 2:3]), r=[f"bnagb{k}"], w=[f"bnagb{k}"])
                P.op("dve", lambda e: e.scalar_tensor_tensor(out=bnag[:, 3:4], in0=bnag[:, 0:1], scalar=-1.0, in1=bnag[:, 2:3],
                                                             op0=ALU.mult, op1=ALU.mult), r=[f"bnag{k}", f"bnagb{k}"], w=[f"bnagc{k}"])
                P.op("act", lambda e: e.activation(out=rr, in_=rr, func=AF.Identity, bias=bnag[:, 3:4], scale=bnag[:, 2:3]),
                     r=[f"rr{k}", f"bnagb{k}", f"bnagc{k}"], w=[f"rr{k}"])
                P.op("sp", lambda e: e.dma_start(out=z_d[tt * 128:(tt + 1) * 128, :], in_=rr), r=[f"rr{k}"], w=[f"z_d{tt}"], dma=True)
                return P.end()

            xr_load(0)
            xr_load(1)
            P.play(merge(convln_chain(0), convln_chain(1), attn_chain(0), attn_chain(1)))
            P.play(merge(convln_chain(2), convln_chain(3), attn_chain(2), attn_chain(3)))
            P.play(merge(epi_chain(0, False), epi_chain(1, False)))
            P.play(merge(epi_chain(2, True), epi_chain(3, True)))

        P.fence()
        if dbg and dbg["what"] == "z":
            nr = _CACHE.get('nmt_run', NMT) * 512
            P.op("sp", lambda e: e.dma_start(out=dbg_d[0:nr, :], in_=z_d[0:nr, :]), r=[f"z_d{i}" for i in range(nr // 128)], w=["dbg"], dma=True)
            P.fence()
            P.emit()
            return nc

        try:
            build_phase2(nc, P, A, ps, nps, dr, out_d, z_d, xs_d, ys_d, dbg, dbg_d, mark_persist,
                     dict(cols=cols, identf=identf, identb=identb, onesb=onesb, Ub=Ub, modc=modc, mod_d=mod_d,
                              EPSC=EPSC, psb=psb))
        except _StopEmit:
            pass
        P.fence()
        P.emit()
    return nc


def build_phase2(nc, P, A, ps, nps, dr, out_d, z_d, xs_d, ys_d, dbg, dbg_d, mark, K):
    cols, identf, identb, onesb, Ub, mod_d, EPSC, psb = (K[k] for k in ("cols", "identf", "identb", "onesb", "Ub", "mod_d", "EPSC", "psb"))
    rows_d, wr_d, wg_d, wu_d, wd_d = dr["rows"], dr["w_r"], dr["wg"], dr["wu"], dr["wd"]
    A.off = mark
    gbc = A.alloc([4, D], F32)
    P.op("sp", lambda e: e.dma_start(out=gbc[:, 1:4, :].rearrange("p a d -> p (a d)"), in_=mod_d[0:1, :].partition_broadcast(128)),
         r=["mod_d"], w=["gbc1_0", "gbc1_1", "gbc2_0", "gbc2_1", "gbc3_0", "gbc3_1"], dma=True)
    lnr = A.alloc([4, D], F32)
    misc = A.alloc([36 + 64], F32)
    A2 = A.alloc([D], F32)
    B2 = A.alloc([D], F32)
    GA = A.alloc([D], F32)
    BA = A.alloc([D], F32)
    wr = A.alloc([8, 36], F32)
    pos_i = A.alloc([2, NT], I32)
    wts = A.alloc([2, NT], F32)
    widx_i = A.alloc([64], I32)
    mark2 = A.off
    br_bc = misc[:, 0:36]
    slot_bc = misc[:, 36:36 + NS]
    P.op("sp", lambda e: e.dma_start(out=lnr.rearrange("p a d -> p (a d)"), in_=rows_d[0:1, R_L1G:R_L1G + 4 * D].partition_broadcast(128)),
         w=["lnr"], dma=True)
    P.op("sp", lambda e: e.dma_start(out=misc, in_=rows_d[0:1, R_BR:R_BR + 100].partition_broadcast(128)), w=["misc"], dma=True)
    P.op("sp", lambda e: e.dma_start(out=wr, in_=wr_d.rearrange("(c p) n -> p c n", p=128)), w=["wr"], dma=True)
    G2H = ["gbc1_0", "gbc1_1", "gbc2_0", "gbc2_1", "gbc3_0", "gbc3_1"]
    P.op("dve", lambda e: e.scalar_tensor_tensor(out=A2, in0=gbc[:, 2, :], scalar=1.0, in1=lnr[:, 0, :], op0=ALU.add, op1=ALU.mult),
         r=["lnr"] + G2H, w=["A2"])
    P.op("dve", lambda e: e.scalar_tensor_tensor(out=B2, in0=gbc[:, 2, :], scalar=1.0, in1=lnr[:, 1, :], op0=ALU.add, op1=ALU.mult),
         r=["lnr"] + G2H, w=["B2"])
    P.op("dve", lambda e: e.tensor_tensor(out=B2, in0=B2, in1=gbc[:, 1, :], op=ALU.add), r=["B2"] + G2H, w=["B2"])
    P.op("dve", lambda e: e.tensor_scalar(out=GA, in0=lnr[:, 0, :], scalar1=ALPHA, scalar2=None, op0=ALU.mult), r=["lnr"], w=["GA"])
    P.op("dve", lambda e: e.tensor_scalar(out=BA, in0=lnr[:, 1, :], scalar1=ALPHA, scalar2=None, op0=ALU.mult), r=["lnr"], w=["BA"])

    def mk_nps(ids):
        st_ = [0]

        def f():
            i = ids[st_[0] % len(ids)]
            st_[0] += 1
            return ps[i], f"ps{i}"
        return f

    t1_d = nc.dram_tensor("t1_scr", [S, D], F32, kind="Internal").ap()
    h2b = A.alloc([NT, D], BF16)
    logits = A.alloc([NT, 36], F32)
    mark2a = A.off
    zt = [A.alloc([D], F32) for _ in range(2)]
    h2f = [A.alloc([D], F32) for _ in range(2)]
    h2T = [A.alloc([8, 128], F32) for _ in range(2)]
    t1s = [A.alloc([D], F32) for _ in range(2)]
    nps2a = [mk_nps([0, 1, 2]), mk_nps([3, 4, 5])]

    def chain2a(j):
        b = j % 2
        mynps = nps2a[b]
        z_, hz = zt[b], f"zt{b}"
        hf, hhf = h2f[b], f"h2f{b}"
        hT_, hhT = h2T[b], f"h2T{b}"
        t1_, ht1 = t1s[b], f"t1s{b}"
        P.rec()
        P.op("sp", lambda e: e.dma_start(out=z_, in_=z_d[j * 128:(j + 1) * 128, :]), r=[f"z_d{j}"], w=[hz], dma=True)
        P.op("dve", lambda e: e.tensor_tensor(out=hf, in0=z_, in1=A2, op=ALU.mult), r=[hz, "A2"], w=[hhf])
        P.op("dve", lambda e: e.tensor_tensor(out=hf, in0=hf, in1=B2, op=ALU.add), r=[hhf, "B2"], w=[hhf])
        P.op("pool", lambda e: e.tensor_tensor(out=t1_, in0=z_, in1=GA, op=ALU.mult), r=[hz, "GA"], w=[ht1])
        P.op("pool", lambda e: e.tensor_tensor(out=t1_, in0=t1_, in1=BA, op=ALU.add), r=[ht1, "BA"], w=[ht1])
        P.op("sp", lambda e: e.dma_start(out=t1_d[j * 128:(j + 1) * 128, :], in_=t1_), r=[ht1], w=[f"t1_d{j}"], dma=True)
        P.op("act", lambda e: e.activation(out=h2b[:, j, :], in_=hf, func=AF.Identity), r=[hhf], w=[f"h2b{j}"])
        for hh in range(2):
            pt, hp = mynps()
            for c4 in range(4):
                c = hh * 4 + c4
                P.op("pe", lambda e, pt=pt, c=c, c4=c4: e.transpose(out=pt[:, c4 * 128:(c4 + 1) * 128],
                                                                  in_=hf[:, c * 128:(c + 1) * 128], identity=identf),
                     r=[hhf, "cst"], w=[hp])
            if hh == 0:
                P.op("act", lambda e, pt=pt: e.activation(out=hT_[:, 0:4, :].rearrange("p c t -> p (c t)"),
                                                          in_=pt[:], func=AF.Identity), r=[hp], w=[hhT + "a"])
            else:
                P.op("dve", lambda e, pt=pt: e.tensor_copy(out=hT_[:, 4:8, :].rearrange("p c t -> p (c t)"),
                                                           in_=pt[:]), r=[hp], w=[hhT + "b"])
        pl, hpl = mynps()
        for c in range(8):
            P.op("pe", lambda e, pl=pl, c=c: e.matmul(pl[:, 0:36], lhsT=hT_[:, c, :], rhs=wr[:, c, :], start=(c == 0), stop=(c == 7)),
                 r=[hhT + "a", hhT + "b", "wr"], w=[hpl])
        P.op("dve", lambda e, pl=pl: e.tensor_tensor(out=logits[:, j, :], in0=pl[:, 0:36], in1=br_bc, op=ALU.add),
             r=[hpl, "misc"], w=["logits"])
        return P.end()

    for j in range(0, NT, 2):
        P.play(merge(chain2a(j), chain2a(j + 1)))
    P.fence()
    A.off = mark2a

    def T3(n):
        return A.alloc([NT, n], F32)
    gmax = A.alloc([NT], F32)
    og = T3(4)
    eg = T3(4)
    sgm = A.alloc([NT], F32)
    ptop = A.alloc([NT], F32)
    tmp4 = A.alloc([NT, 4, 8], F32)
    sel = T3(8)
    sel2 = T3(8)
    m1 = A.alloc([NT], F32)
    m2 = A.alloc([NT], F32)
    o1 = T3(8)
    o2 = T3(8)
    e2 = A.alloc([NT], F32)
    r12 = A.alloc([NT], F32)
    O1 = A.alloc([NT, 4, 8], F32)
    O2 = A.alloc([NT, 4, 8], F32)
    Obf = A.alloc([NT * 32], BF16)
    totA = A.alloc([NT, 32], F32)
    totB = A.alloc([NT, 32], F32)
    tot0 = A.alloc([NT, 32], F32)
    base = A.alloc([NT, 32], F32)
    cnt = A.alloc([32], F32)
    cmpc = A.alloc([32, 24], F32)
    pcnt = A.alloc([32], F32)
    oeA = A.alloc([32], F32)
    oeB = A.alloc([32], F32)
    offs = A.alloc([32], F32)
    cmps = A.alloc([NS, 32], F32)
    esl = A.alloc([64], F32)
    used = A.alloc([64], F32)
    posf = A.alloc([2, NT], F32)

    LG = logits[:, :, 0:4]
    LE4 = logits[:, :, 4:36].rearrange("p j (g e) -> p j g e", g=4)

    def dv(fn, r, w):
        P.op("dve", fn, r=r, w=w)

    dv(lambda e: e.tensor_reduce(out=gmax, in_=LG, axis=AX.X, op=ALU.max), ["logits"], ["gmax"])
    dv(lambda e: e.tensor_tensor(out=og, in0=LG, in1=gmax.unsqueeze(2).to_broadcast([128, NT, 4]), op=ALU.is_equal), ["logits", "gmax"], ["og"])
    dv(lambda e: e.tensor_tensor(out=eg, in0=LG, in1=gmax.unsqueeze(2).to_broadcast([128, NT, 4]), op=ALU.subtract), ["logits", "gmax"], ["eg"])
    P.op("act", lambda e: e.activation(out=eg, in_=eg, func=AF.Exp), r=["eg"], w=["eg"])
    dv(lambda e: e.tensor_reduce(out=sgm, in_=eg, axis=AX.X, op=ALU.add), ["eg"], ["sgm"])
    dv(lambda e: e.reciprocal(out=ptop, in_=sgm), ["sgm"], ["ptop"])
    dv(lambda e: e.tensor_tensor(out=tmp4, in0=LE4, in1=og.unsqueeze(3).to_broadcast([128, NT, 4, 8]), op=ALU.mult), ["logits", "og"], ["tmp4"])
    dv(lambda e: e.tensor_reduce(out=sel, in_=tmp4.rearrange("p j g e -> p j e g"), axis=AX.X, op=ALU.add), ["tmp4"], ["sel"])
    dv(lambda e: e.tensor_reduce(out=m1, in_=sel, axis=AX.X, op=ALU.max), ["sel"], ["m1"])
    dv(lambda e: e.tensor_tensor(out=o1, in0=sel, in1=m1.unsqueeze(2).to_broadcast([128, NT, 8]), op=ALU.is_equal), ["sel", "m1"], ["o1"])
    dv(lambda e: e.scalar_tensor_tensor(out=sel2.rearrange("p j e -> p (j e)"), in0=o1.rearrange("p j e -> p (j e)"), scalar=-1.0e9,
                                        in1=sel.rearrange("p j e -> p (j e)"), op0=ALU.mult, op1=ALU.add), ["o1", "sel"], ["sel2"])
    dv(lambda e: e.tensor_reduce(out=m2, in_=sel2, axis=AX.X, op=ALU.max), ["sel2"], ["m2"])
    dv(lambda e: e.tensor_tensor(out=o2, in0=sel2, in1=m2.unsqueeze(2).to_broadcast([128, NT, 8]), op=ALU.is_equal), ["sel2", "m2"], ["o2"])
    dv(lambda e: e.tensor_tensor(out=e2, in0=m2, in1=m1, op=ALU.subtract), ["m1", "m2"], ["e2"])
    P.op("act", lambda e: e.activation(out=e2, in_=e2, func=AF.Exp), r=["e2"], w=["e2"])
    dv(lambda e: e.tensor_scalar(out=r12, in0=e2, scalar1=1.0, scalar2=None, op0=ALU.add), ["e2"], ["r12"])
    dv(lambda e: e.reciprocal(out=r12, in_=r12), ["r12"], ["r12"])
    dv(lambda e: e.tensor_tensor(out=wts[:, 0, :], in0=r12, in1=ptop, op=ALU.mult), ["r12", "ptop"], ["wts"])
    dv(lambda e: e.tensor_tensor(out=wts[:, 1, :], in0=wts[:, 0, :], in1=e2, op=ALU.mult), ["wts", "e2"], ["wts"])
    dv(lambda e: e.tensor_tensor(out=O1, in0=og.unsqueeze(3).to_broadcast([128, NT, 4, 8]),
                                 in1=o1.unsqueeze(2).to_broadcast([128, NT, 4, 8]), op=ALU.mult), ["og", "o1"], ["O1"])
    dv(lambda e: e.tensor_tensor(out=O2, in0=og.unsqueeze(3).to_broadcast([128, NT, 4, 8]),
                                 in1=o2.unsqueeze(2).to_broadcast([128, NT, 4, 8]), op=ALU.mult), ["og", "o2"], ["O2"])
    O1f = O1.rearrange("p j g e -> p (j g e)")
    O2f = O2.rearrange("p j g e -> p (j g e)")
    dv(lambda e: e.tensor_tensor(out=Obf, in0=O1f, in1=O2f, op=ALU.add), ["O1", "O2"], ["Obf"])
    pcs, pts = [], []
    for h in range(2):
        pc_, hpc = nps()
        P.op("pe", lambda e, pc_=pc_, h=h: e.matmul(pc_[:], lhsT=Ub, rhs=Obf[:, h * 512:(h + 1) * 512], start=True, stop=True),
             r=["Ub", "Obf"], w=[hpc])
        pcs.append((pc_, hpc))
        pt_, hpt = nps()
        P.op("pe", lambda e, pt_=pt_, h=h: e.matmul(pt_[:], lhsT=onesb, rhs=Obf[:, h * 512:(h + 1) * 512], start=True, stop=True),
             r=["onesb", "Obf"], w=[hpt])
        pts.append((pt_, hpt))
    tot0f = tot0.rearrange("p j e -> p (j e)")
    for h in range(2):
        dv(lambda e, h=h: e.tensor_copy(out=tot0f[:, h * 512:(h + 1) * 512], in_=pts[h][0][:]), [pts[h][1]], ["tot0"])
    cur, hc = tot0, "tot0"
    for i_, sft in enumerate((1, 2, 4, 8, 16)):
        nxt, hn = (totA, "totA") if i_ % 2 == 0 else (totB, "totB")
        dv(lambda e, cur=cur, nxt=nxt, sft=sft: e.tensor_tensor(out=nxt[:, sft:, :], in0=cur[:, sft:, :], in1=cur[:, :NT - sft, :], op=ALU.add),
           [hc], [hn])
        dv(lambda e, cur=cur, nxt=nxt, sft=sft: e.tensor_copy(out=nxt[:, :sft, :], in_=cur[:, :sft, :]), [hc, hn], [hn])
        cur, hc = nxt, hn
    incl, hincl = cur, hc
    dv(lambda e: e.tensor_copy(out=cnt, in_=incl[:, NT - 1, :]), [hincl], ["cnt"])
    dv(lambda e: e.tensor_tensor(out=cmpc, in0=cnt.unsqueeze(2).to_broadcast([128, 32, 24]),
                                 in1=slot_bc[:, 0:24].unsqueeze(1).to_broadcast([128, 32, 24]), op=ALU.is_gt), ["cnt", "misc"], ["cmpc"])
    dv(lambda e: e.tensor_reduce(out=pcnt, in_=cmpc, axis=AX.X, op=ALU.add), ["cmpc"], ["pcnt"])
    dv(lambda e: e.tensor_scalar(out=pcnt, in0=pcnt, scalar1=float(T), scalar2=None, op0=ALU.mult), ["pcnt"], ["pcnt"])
    cur, hc = pcnt, "pcnt"
    for i_, sft in enumerate((1, 2, 4, 8, 16)):
        nxt, hn = (oeA, "oeA") if i_ % 2 == 0 else (oeB, "oeB")
        dv(lambda e, cur=cur, nxt=nxt, sft=sft: e.tensor_tensor(out=nxt[:, sft:], in0=cur[:, sft:], in1=cur[:, :32 - sft], op=ALU.add), [hc], [hn])
        dv(lambda e, cur=cur, nxt=nxt, sft=sft: e.tensor_copy(out=nxt[:, :sft], in_=cur[:, :sft]), [hc, hn], [hn])
        cur, hc = nxt, hn
    oend, hoend = cur, hc
    dv(lambda e: e.tensor_tensor(out=offs, in0=oend, in1=pcnt, op=ALU.subtract), [hoend, "pcnt"], ["offs"])
    dv(lambda e: e.tensor_tensor(out=base, in0=incl, in1=tot0, op=ALU.subtract), [hincl, "tot0"], ["base"])
    dv(lambda e: e.tensor_tensor(out=base, in0=base, in1=offs.unsqueeze(1).to_broadcast([128, NT, 32]), op=ALU.add), ["base", "offs"], ["base"])
    basef = base.rearrange("p j e -> p (j e)")
    for h in range(2):
        dv(lambda e, h=h: e.tensor_tensor(out=basef[:, h * 512:(h + 1) * 512], in0=pcs[h][0][:], in1=basef[:, h * 512:(h + 1) * 512], op=ALU.add),
           [pcs[h][1], "base"], ["base"])
    for k, (Ok, hO) in enumerate(((O1, "O1"), (O2, "O2"))):
        dv(lambda e, Ok=Ok: e.tensor_tensor(out=Ok.rearrange("p j g e -> p (j g e)"), in0=Ok.rearrange("p j g e -> p (j g e)"), in1=basef, op=ALU.mult),
           [hO, "base"], [hO])
        dv(lambda e, Ok=Ok, k=k: e.tensor_reduce(out=posf[:, k, :], in_=Ok.rearrange("p j g e -> p j (g e)"), axis=AX.X, op=ALU.add), [hO], ["posf"])
    dv(lambda e: e.tensor_copy(out=pos_i, in_=posf), ["posf"], ["pos_i"])
    dv(lambda e: e.tensor_tensor(out=cmps, in0=oend.unsqueeze(1).to_broadcast([128, NS, 32]),
                                 in1=slot_bc.unsqueeze(2).to_broadcast([128, NS, 32]), op=ALU.is_le), [hoend, "misc"], ["cmps"])
    dv(lambda e: e.tensor_reduce(out=esl[:, 0:NS], in_=cmps, axis=AX.X, op=ALU.add), ["cmps"], ["esl"])
    dv(lambda e: e.tensor_scalar(out=esl[:, 0:NS], in0=esl[:, 0:NS], scalar1=float(NE - 1), scalar2=128.0, op0=ALU.min, op1=ALU.mult), ["esl"], ["esl"])
    dv(lambda e: e.tensor_scalar(out=used[:, 0:NS], in0=slot_bc, scalar1=oend[:, 31:32], scalar2=None, op0=ALU.is_lt), ["misc", hoend], ["used"])
    dv(lambda e: e.tensor_scalar(out=used[:, 0:NS], in0=used[:, 0:NS], scalar1=-1.0e6, scalar2=1.0e6, op0=ALU.mult, op1=ALU.add), ["used"], ["used"])
    dv(lambda e: e.tensor_tensor(out=esl[:, 0:NS], in0=esl[:, 0:NS], in1=used[:, 0:NS], op=ALU.add), ["esl", "used"], ["esl"])
    dv(lambda e: e.tensor_scalar(out=esl[:, 0:NS], in0=esl[:, 0:NS], scalar1=cols[:, C_PID:C_PID + 1], scalar2=None, op0=ALU.add), ["esl", "cols"], ["esl"])
    dv(lambda e: e.tensor_copy(out=widx_i[:, 0:NS], in_=esl[:, 0:NS]), ["esl"], ["widx_i"])

    if dbg and dbg["what"] == "route":
        P.fence()
        P.op("sp", lambda e: e.dma_start(out=dbg_d[0:128, 0:NT * 36], in_=logits.rearrange("p j n -> p (j n)")), r=["logits"], w=["dbg0"], dma=True)
        P.op("sp", lambda e: e.dma_start(out=dbg_d[128:256, 0:2 * NT], in_=posf.rearrange("p k j -> p (k j)")), r=["posf"], w=["dbg1"], dma=True)
        P.op("sp", lambda e: e.dma_start(out=dbg_d[256:384, 0:2 * NT], in_=wts.rearrange("p k j -> p (k j)")), r=["wts"], w=["dbg2"], dma=True)
        P.op("sp", lambda e: e.dma_start(out=dbg_d[384:512, 0:NS], in_=esl[:, 0:NS]), r=["esl"], w=["dbg3"], dma=True)
        P.fence()
        raise _StopEmit()

    for j in range(NT):
        for k in range(2):
            P.op("pool", lambda e, j=j, k=k: e.indirect_dma_start(
                out=xs_d[:, :], out_offset=bass.IndirectOffsetOnAxis(ap=pos_i[:, k, j:j + 1], axis=0),
                in_=h2b[:, j, :], in_offset=None), r=[f"h2b{j}", "pos_i"], w=[f"xs_{j}_{k}"], dma=True)
    P.fence()

    A.off = mark2
    NBW = 4
    wgs = [A.alloc([2048], BF16) for _ in range(NBW)]
    wus = [A.alloc([2048], BF16) for _ in range(NBW)]
    wds = [A.alloc([2048], BF16) for _ in range(NBW)]
    xtok = [A.alloc([NSUB, D], BF16) for _ in range(NBW)]
    XT = [A.alloc([8, T], BF16) for _ in range(2)]
    sgs = [A.alloc([T], F32) for _ in range(2)]
    aT = [A.alloc([2, T], BF16) for _ in range(2)]
    NYO = 3
    yo = [A.alloc([D], F32) for _ in range(NYO)]
    npsB = mk_nps([0, 1, 2])
    npsC = mk_nps([3, 4, 5])
    _bc = {}

    def get_bc(e):
        if "v" not in _bc:
            reg = e.alloc_register("bcreg")
            e.reg_mov(reg, NE * 128 - 1)
            _bc["v"] = e.snap(reg, donate=True)
        return _bc["v"]

    def load_w(s, which):
        bw = s % NBW
        for (wsb, wdr, hn) in which(bw):
            P.op("pool", lambda e, wsb=wsb, wdr=wdr, s=s: e.indirect_dma_start(
                out=wsb, out_offset=None, in_=wdr[:, :],
                in_offset=bass.IndirectOffsetOnAxis(ap=widx_i[:, s:s + 1], axis=0),
                bounds_check=get_bc(e), oob_is_err=False), r=["widx_i"], w=[hn], dma=True)

    def w_gu(bw):
        return ((wgs[bw], wg_d, f"wg{bw}"), (wus[bw], wu_d, f"wu{bw}"))

    def w_d(bw):
        return ((wds[bw], wd_d, f"wd{bw}"),)

    def load_x(s):
        bw = s % NBW
        for st in range(NSUB):
            r0 = s * T + st * 128
            P.op("sp", lambda e, bw=bw, st=st, r0=r0: e.dma_start(out=xtok[bw][:, st, :], in_=xs_d[r0:r0 + 128, :]),
                 w=[f"xtok{bw}_{st}"], dma=True)

    def stageA(s):
        b, bw = s % 2, s % NBW
        P.rec()
        for st in range(NSUB):
            k = (s * NSUB + st) % 2
            pb_, hpb = psb[k], f"psb{k}"
            for c in range(8):
                P.op("pe", lambda e, pb_=pb_, st=st, c=c: e.transpose(out=pb_[:, c * 128:(c + 1) * 128],
                                                                     in_=xtok[bw][:, st, c * 128:(c + 1) * 128], identity=identb),
                     r=[f"xtok{bw}_{st}", "identb"], w=[hpb])
            if (s * NSUB + st) % 2 == 0:
                P.op("act", lambda e, pb_=pb_, st=st: e.activation(out=XT[b][:, :, st * 128:(st + 1) * 128],
                                                                   in_=pb_[:].rearrange("p (c t) -> p c t", c=8), func=AF.Identity),
                     r=[hpb], w=[f"XT{b}_{st}"])
            else:
                P.op("dve", lambda e, pb_=pb_, st=st: e.tensor_copy(out=XT[b][:, :, st * 128:(st + 1) * 128],
                                                                    in_=pb_[:].rearrange("p (c t) -> p c t", c=8)),
                     r=[hpb], w=[f"XT{b}_{st}"])
        return P.end()

    def stageB(s):
        b, bw = s % 2, s % NBW
        XH = [f"XT{b}_{st}" for st in range(NSUB)]
        P.rec()
        for fch in range(2):
            pg, hpg = npsB()
            for c in range(8):
                P.op("pe", lambda e, pg=pg, c=c, fch=fch: e.matmul(
                    pg[:, 0:T], lhsT=wgs[bw][:, c * 256 + fch * 128:c * 256 + (fch + 1) * 128], rhs=XT[b][:, c, :],
                    start=(c == 0), stop=(c == 7)), r=[f"wg{bw}"] + XH, w=[hpg])
            P.op("act", lambda e, pg=pg: e.activation(out=sgs[b], in_=pg[:, 0:T], func=AF.Silu), r=[hpg], w=[f"sgs{b}"])
            pu, hpu = npsB()
            for c in range(8):
                P.op("pe", lambda e, pu=pu, c=c, fch=fch: e.matmul(
                    pu[:, 0:T], lhsT=wus[bw][:, c * 256 + fch * 128:c * 256 + (fch + 1) * 128], rhs=XT[b][:, c, :],
                    start=(c == 0), stop=(c == 7)), r=[f"wu{bw}"] + XH, w=[hpu])
            P.op("dve", lambda e, pu=pu, fch=fch: e.tensor_tensor(out=aT[b][:, fch, :], in0=pu[:, 0:T], in1=sgs[b], op=ALU.mult),
                 r=[hpu, f"sgs{b}"], w=[f"aT{b}_{fch}"])
        return P.end()

    def stageC(s):
        b, bw = s % 2, s % NBW
        P.rec()
        for st in range(NSUB):
            yb = (s * NSUB + st) % NYO
            for half in range(2):
                po, hpo = npsC()
                for fch in range(2):
                    P.op("pe", lambda e, po=po, st=st, fch=fch, half=half: e.matmul(
                        po[:], lhsT=aT[b][:, fch, st * 128:(st + 1) * 128],
                        rhs=wds[bw][:, fch * 1024 + half * 512:fch * 1024 + (half + 1) * 512], start=(fch == 0), stop=(fch == 1)),
                        r=[f"aT{b}_0", f"aT{b}_1", f"wd{bw}"], w=[hpo])
                if half == 0:
                    P.op("act", lambda e, po=po, yb=yb: e.activation(out=yo[yb][:, 0:512], in_=po[:], func=AF.Identity), r=[hpo], w=[f"yo{yb}a"])
                else:
                    P.op("dve", lambda e, po=po, yb=yb: e.tensor_copy(out=yo[yb][:, 512:1024], in_=po[:]), r=[hpo], w=[f"yo{yb}b"])
            r0 = s * T + st * 128
            P.op("sp", lambda e, yb=yb, r0=r0: e.dma_start(out=ys_d[r0:r0 + 128, :], in_=yo[yb]), r=[f"yo{yb}a", f"yo{yb}b"],
                 w=[f"ys_{s}_{st}"], dma=True)
        return P.end()

    for s in range(min(NBW, NS)):
        load_x(s)
        load_w(s, w_gu)
        load_w(s, w_d)
    for i in range(NS + 2):
        lists = []
        if i < NS:
            lists.append(stageA(i))
        if 0 <= i - 1 < NS:
            lists.append(stageB(i - 1))
        if 0 <= i - 2 < NS:
            lists.append(stageC(i - 2))
        P.play(merge(*lists))
        if i + NBW < NS:
            load_x(i + NBW)
        if i - 1 >= 0 and i - 1 + NBW < NS:
            load_w(i - 1 + NBW, w_gu)
        if i - 2 >= 0 and i - 2 + NBW < NS:
            load_w(i - 2 + NBW, w_d)
    P.fence()

    A.off = mark2
    Y1 = [A.alloc([D], F32) for _ in range(2)]
    Y2 = [A.alloc([D], F32) for _ in range(2)]
    t1 = [A.alloc([D], F32) for _ in range(2)]
    ff = [A.alloc([D], F32) for _ in range(2)]
    r2 = [A.alloc([D], F32) for _ in range(2)]
    qq = [A.alloc([D], F32) for _ in range(2)]
    ob = [A.alloc([D], F32) for _ in range(2)]
    bn2 = [A.alloc([12], F32) for _ in range(2)]
    ag2 = [A.alloc([4], F32) for _ in range(2)]
    G2H_ = ["gbc3_0", "gbc3_1"]

    def chain2f(j):
        b = j % 2
        P.rec()
        P.op("pool", lambda e: e.indirect_dma_start(
            out=Y1[b], out_offset=None, in_=ys_d[:, :], in_offset=bass.IndirectOffsetOnAxis(ap=pos_i[:, 0, j:j + 1], axis=0)),
            r=["pos_i"], w=[f"Y1{b}"], dma=True)
        P.op("pool", lambda e: e.indirect_dma_start(
            out=Y2[b], out_offset=None, in_=ys_d[:, :], in_offset=bass.IndirectOffsetOnAxis(ap=pos_i[:, 1, j:j + 1], axis=0)),
            r=["pos_i"], w=[f"Y2{b}"], dma=True)
        P.op("sp", lambda e: e.dma_start(out=t1[b], in_=t1_d[j * 128:(j + 1) * 128, :]), r=[f"t1_d{j}"], w=[f"t1{b}"], dma=True)
        P.op("act", lambda e: e.activation(out=ff[b], in_=Y1[b], func=AF.Identity, scale=wts[:, 0, j:j + 1]), r=[f"Y1{b}", "wts"], w=[f"ff{b}"])
        P.op("dve", lambda e: e.scalar_tensor_tensor(out=ff[b], in0=Y2[b], scalar=wts[:, 1, j:j + 1], in1=ff[b], op0=ALU.mult, op1=ALU.add),
             r=[f"Y2{b}", "wts", f"ff{b}"], w=[f"ff{b}"])
        P.op("pool", lambda e: e.tensor_tensor(out=ff[b], in0=ff[b], in1=gbc[:, 3, :], op=ALU.mult), r=[f"ff{b}"] + G2H_, w=[f"ff{b}"])
        P.op("dve", lambda e: e.tensor_tensor(out=r2[b], in0=ff[b], in1=t1[b], op=ALU.add), r=[f"ff{b}", f"t1{b}"], w=[f"r2{b}"])
        for h in range(2):
            P.op("dve", lambda e, h=h: e.bn_stats(out=bn2[b][:, h * 6:(h + 1) * 6], in_=r2[b][:, h * 512:(h + 1) * 512]), r=[f"r2{b}"], w=[f"bn2{b}"])
        P.op("dve", lambda e: e.bn_aggr(out=ag2[b][:, 0:2], in_=bn2[b]), r=[f"bn2{b}"], w=[f"ag2{b}"])
        P.op("act", lambda e: e.activation(out=ag2[b][:, 2:3], in_=ag2[b][:, 1:2], func=AF.Sqrt, bias=EPSC, scale=1.0), r=[f"ag2{b}", "epsc"], w=[f"ag2b{b}"])
        P.op("dve", lambda e: e.reciprocal(out=ag2[b][:, 2:3], in_=ag2[b][:, 2:3]), r=[f"ag2b{b}"], w=[f"ag2b{b}"])
        P.op("dve", lambda e: e.scalar_tensor_tensor(out=ag2[b][:, 3:4], in0=ag2[b][:, 0:1], scalar=-1.0, in1=ag2[b][:, 2:3], op0=ALU.mult, op1=ALU.mult),
             r=[f"ag2{b}", f"ag2b{b}"], w=[f"ag2c{b}"])
        P.op("act", lambda e: e.activation(out=qq[b], in_=r2[b], func=AF.Identity, bias=ag2[b][:, 3:4], scale=ag2[b][:, 2:3]),
             r=[f"r2{b}", f"ag2b{b}", f"ag2c{b}"], w=[f"qq{b}"])
        P.op("pool", lambda e: e.tensor_tensor(out=qq[b], in0=qq[b], in1=lnr[:, 2, :], op=ALU.mult), r=[f"qq{b}", "lnr"], w=[f"qq{b}"])
        P.op("dve", lambda e: e.tensor_tensor(out=ob[b], in0=qq[b], in1=lnr[:, 3, :], op=ALU.add), r=[f"qq{b}", "lnr"], w=[f"ob{b}"])
        P.op("sp", lambda e: e.dma_start(out=out_d[j * 128:(j + 1) * 128, :], in_=ob[b]), r=[f"ob{b}"], w=[f"out{j}"], dma=True)
        return P.end()

    for j in range(0, NT, 2):
        P.play(merge(chain2f(j), chain2f(j + 1)))


class _StopEmit(Exception):
    pass


def _host_prep(inp, b):
    f = np.float32
    L = 0
    cols = np.zeros((128, NCOL), f)
    cols[:, C_C:C_C + 8] = inp["c"][b].reshape(8, 128).T
    w_in = inp["w_in"][L]
    b_in = inp["b_in"][L]
    qcols = np.concatenate([np.arange(1024 + h * 64, 1024 + (h + 1) * 64) for h in HORDER])
    perm = np.concatenate([np.arange(0, 1024), qcols, np.arange(1536, 1792)])
    w_in_p = np.ascontiguousarray(w_in[:, perm])
    b_in_p = b_in[perm]
    cols[:, C_BIN:C_BIN + 13] = b_in_p[:1664].reshape(13, 128).T
    cols[:, C_CW:C_CW + 124] = inp["conv_w"][L].T.reshape(4, 128, 31).transpose(1, 0, 2).reshape(128, 124)
    cols[:, C_CB:C_CB + 4] = inp["conv_b"][L].reshape(4, 128).T
    cols[:, C_LG:C_LG + 4] = inp["conv_ln_g"][L].reshape(4, 128).T
    cols[:, C_LB:C_LB + 4] = inp["conv_ln_b"][L].reshape(4, 128).T
    cols[:, C_OG:C_OG + 4] = inp["conv_out_g"][L].reshape(4, 128).T
    cols[:, C_PID] = np.arange(128)
    rows = np.zeros((1, NROW), f)
    rows[0, R_BADA:R_BADA + 6144] = inp["b_ada"][L]
    rows[0, R_BV:R_BV + 128] = b_in[1664:1792]
    rows[0, R_SINK:R_SINK + 8] = inp["sinks"][L][HORDER]
    rows[0, R_AOG:R_AOG + 512] = inp["attn_out_g"][L].reshape(8, 64)[HORDER].reshape(-1)
    rows[0, R_BOUT:R_BOUT + D] = inp["b_out"][L]
    rows[0, R_L1G:R_L1G + D] = inp["ln1_g"][L]
    rows[0, R_L1B:R_L1B + D] = inp["ln1_b"][L]
    rows[0, R_L2G:R_L2G + D] = inp["ln2_g"][L]
    rows[0, R_L2B:R_L2B + D] = inp["ln2_b"][L]
    rows[0, R_BR:R_BR + 4] = inp["b_router_group"][L]
    rows[0, R_BR + 4:R_BR + 36] = inp["b_router_expert"][L]
    rows[0, R_SLOT:R_SLOT + NS] = np.arange(NS) * T
    w_out = inp["w_out"][L]
    arows = np.concatenate([np.arange(512 + h * 64, 512 + (h + 1) * 64) for h in HORDER])
    w_out_p = np.ascontiguousarray(np.concatenate([w_out[:512], w_out[arows]], axis=0))
    w_r = np.ascontiguousarray(np.concatenate([inp["w_router_group"][L], inp["w_router_expert"][L]], axis=1))
    return dict(cols=cols, rows=rows, w_in=w_in_p, w_out=w_out_p, w_r=w_r)


def _consts():
    f = np.float32
    cst = np.zeros((128, NK), f)
    cst[:, K_ID:K_ID + 128] = np.eye(128)
    p = np.arange(128)
    cst[:, K_U:K_U + 128] = (p[:, None] < p[None, :])
    cst[:, K_BD:K_BD + 128] = ((p[:, None] // 64) == (p[None, :] // 64)) / 64.0
    m0 = np.where(p[:, None] > p[None, :], 0.0, NEG)
    m1 = np.where(p[:, None] <= p[None, :], 0.0, NEG)
    cst[:, K_M0:K_M0 + 512] = np.tile(m0, (1, 4))
    cst[:, K_M1:K_M1 + 512] = np.tile(m1, (1, 4))
    return cst


def _expert_layout(inp):
    L = 0
    wg = np.ascontiguousarray(inp["w_gate"][L].reshape(NE, 8, 128, DE).transpose(0, 2, 1, 3)).reshape(NE * 128, 2048)
    wu = np.ascontiguousarray(inp["w_up"][L].reshape(NE, 8, 128, DE).transpose(0, 2, 1, 3)).reshape(NE * 128, 2048)
    wd = np.ascontiguousarray(inp["w_down"][L].reshape(NE, 2, 128, D).transpose(0, 2, 1, 3)).reshape(NE * 128, 2048)
    return wg, wu, wd


_CACHE = {}


def kernel(**inputs):
    inp = {k: np.asarray(v) for k, v in inputs.items()}
    dbg = _CACHE.get("dbg")
    nc = build_program(dbg)
    cst = _consts()
    wg, wu, wd = _expert_layout(inp)
    w_ada = np.ascontiguousarray(inp["w_ada"][0])
    in_maps = []
    ncores = _CACHE.get("ncores", 8)
    for b in range(ncores):
        hp = _host_prep(inp, b)
        m = dict(x=np.ascontiguousarray(inp["x"][b]), cols=hp["cols"], rows=hp["rows"], cst=cst, w_ada=w_ada,
                 w_in=hp["w_in"], w_out=hp["w_out"], w_r=hp["w_r"], wg=wg, wu=wu, wd=wd)
        if _CACHE.get("p2only"):
            m["z_in"] = _CACHE["z_in"]
        in_maps.append(m)
    if _CACHE.get("trace"):
        res = run_bass_kernel_spmd(nc, in_maps, core_ids=list(range(ncores)), trace=True)
        print("EXEC_NS", res.exec_time_ns)
    else:
        res = run_bass_kernel_spmd(nc, in_maps, core_ids=list(range(ncores)))
    if dbg:
        return [np.asarray(r["dbg"]) for r in res.results]
    out = np.stack([np.asarray(r["out"]) for r in res.results], axis=0).astype(np.float32)
    return out
```

```python
import os
import numpy as np
import concourse.bass as bass
import concourse.mybir as mybir
from concourse.bass_utils import run_bass_kernel_spmd

F32 = mybir.dt.float32
BF16 = mybir.dt.bfloat16
I32 = mybir.dt.int32
ALU = mybir.AluOpType
AF = mybir.ActivationFunctionType
AX = mybir.AxisListType

D = 1024
S = 4096
NT = S // 128
NMT = S // 512
DIN = 1792
NE = 32
DE = 256
ALPHA = 2.0 ** 0.25
EPS = 1e-5
NEG = -30000.0
T = 384
NS = (2 * S + NE * (T - 1) + T - 1) // T
NSUB = T // 128
HORDER = [0, 4, 1, 5, 2, 6, 3, 7]

C_C = 0
C_BIN = 8
C_CW = 21
C_CB = 145
C_LG = 149
C_LB = 153
C_OG = 157
C_PID = 161
NCOL = 162
R_BADA = 0
R_BV = 6144
R_SINK = 6272
R_AOG = 6280
R_BOUT = 6792
R_L1G = 7816
R_L1B = 8840
R_L2G = 9864
R_L2B = 10888
R_BR = 11912
R_SLOT = 11948
NROW = 12012
K_ID = 0
K_U = 128
K_BD = 256
K_M0 = 384
K_M1 = 896
NK = 1408


class Prog:
    def __init__(self, nc, sems):
        self.nc = nc
        self.ops = []
        self.last_w = {}
        self.readers = {}
        self.eng_sem = {e: sems[i] for i, e in enumerate(["pe", "act", "dve", "pool"])}
        rest = sems[4:]
        n_sp = (len(rest) * 5) // 10
        n_pool = (len(rest) * 4) // 10
        self.dma_pool = {"sp": rest[:n_sp], "pool": rest[n_sp:n_sp + n_pool], "act": rest[n_sp + n_pool:]}
        self._rec = None

    def rec(self):
        assert self._rec is None
        self._rec = []

    def end(self):
        l = self._rec
        self._rec = None
        return l

    def play(self, lst):
        assert self._rec is None
        for o in lst:
            self.op(*o)

    def op(self, eng, fn, r=(), w=(), dma=False):
        if self._rec is not None:
            self._rec.append((eng, fn, list(r), list(w), dma))
            return None
        i = len(self.ops)
        w = list(w) + [h for h in r if h.startswith("ps") and h not in w]
        raw, oth = set(), set()
        for h in r:
            if h in self.last_w:
                raw.add(self.last_w[h])
        for h in w:
            if h in self.last_w:
                oth.add(self.last_w[h])
            for j in self.readers.get(h, ()):
                oth.add(j)
        for h in w:
            self.last_w[h] = i
            self.readers[h] = []
        for h in r:
            self.readers.setdefault(h, []).append(i)
        deps = []
        for j in sorted(raw | oth):
            p = self.ops[j]
            if j == i:
                continue
            if (not p["dma"]) and p["eng"] == eng:
                if eng == "pe" or j not in raw:
                    continue
            deps.append(j)
        self.ops.append(dict(eng=eng, fn=fn, deps=deps, dma=dma, sig=False))
        for j in deps:
            self.ops[j]["sig"] = True
        return i

    def fence(self, engs=("pe", "act", "dve", "pool", "sp")):
        hs = list(self.last_w.keys())
        for e in engs:
            self.op(e, None, r=hs, w=["_fence_" + e])

    def emit(self):
        nc = self.nc
        ticket = {e: 0 for e in self.eng_sem}
        dma_next = {q: 0 for q in self.dma_pool}
        dma_uses = {}
        for o in self.ops:
            if o["dma"]:
                q = o["eng"]
                pool = self.dma_pool[q]
                sem = pool[dma_next[q] % len(pool)]
                dma_next[q] += 1
                u = dma_uses.get(id(sem), 0)
                o["pre"] = (sem, 16 * u)
                dma_uses[id(sem)] = u + 1
                o["ev"] = (sem, 16 * (u + 1))
            elif o["sig"] and o["fn"] is not None:
                ticket[o["eng"]] += 1
                o["ev"] = (self.eng_sem[o["eng"]], ticket[o["eng"]])
        ops = self.ops

        def run(engname, eobj):
            waited = {}

            def wait(sem, val):
                if val <= 0:
                    return
                if waited.get(id(sem), 0) >= val:
                    return
                eobj.wait_ge(sem, val)
                waited[id(sem)] = val

            for o in ops:
                if o["eng"] != engname:
                    continue
                for j in o["deps"]:
                    ev = ops[j].get("ev")
                    if ev is not None:
                        wait(*ev)
                if o["fn"] is None:
                    continue
                if o["dma"]:
                    wait(*o["pre"])
                    ins = o["fn"](eobj)
                    ins.then_inc(o["ev"][0], 16)
                else:
                    ins = o["fn"](eobj)
                    if o["sig"]:
                        ins.then_inc(o["ev"][0], 1)

        with nc.Block() as block:
            @block.tensor
            def _(e):
                run("pe", e)

            @block.scalar
            def _(e):
                run("act", e)

            @block.vector
            def _(e):
                run("dve", e)

            @block.gpsimd
            def _(e):
                run("pool", e)

            @block.sync
            def _(e):
                run("sp", e)


class _Stop(Exception):
    pass


def merge(*lists):
    lists = [l for l in lists if l]
    out = []
    idx = [0] * len(lists)
    total = sum(len(l) for l in lists)
    while len(out) < total:
        best, bv = None, None
        for k, l in enumerate(lists):
            if idx[k] < len(l):
                v = (idx[k] + 0.5) / len(l)
                if bv is None or v < bv:
                    best, bv = k, v
        out.append(lists[best][idx[best]])
        idx[best] += 1
    return out


class Arena:
    def __init__(self, t, nbytes):
        self.t = t
        self.nbytes = nbytes
        self.off = 0

    def alloc(self, shape, dt):
        esz = 4 if dt in (F32, I32) else 2
        n = int(np.prod(shape)) * esz
        n = (n + 63) // 64 * 64
        assert self.off + n <= self.nbytes, ("arena overflow", self.off, n, self.nbytes)
        v = self.t[:, self.off // 4:(self.off + n) // 4]
        self.off += n
        if dt != F32:
            v = v.bitcast(dt)
        v = v[:, 0:int(np.prod(shape))]
        if len(shape) == 2:
            return v.rearrange("p (a b) -> p a b", a=shape[0])
        if len(shape) == 3:
            return v.rearrange("p (a b c) -> p a b c", a=shape[0], b=shape[1])
        return v


def build_program(dbg=None):
    nc = bass.Bass("TRN2", target_bir_lowering=False)
    try:
        return _build_program(nc, dbg)
    except _Stop:
        return nc


def _build_program(nc, dbg=None):
    dr = {}

    def din(name, shape, dt=F32):
        dr[name] = nc.dram_tensor(name, list(shape), dt, kind="ExternalInput").ap()
        return dr[name]

    x_d = din("x", [S, D])
    cols_d = din("cols", [128, NCOL])
    rows_d = din("rows", [1, NROW])
    cst_d = din("cst", [128, NK])
    wada_d = din("w_ada", [D, 6 * D])
    win_d = din("w_in", [D, DIN])
    wout_d = din("w_out", [D, D])
    wr_d = din("w_r", [D, 36])
    wg_d = din("wg", [NE * 128, 2048])
    wu_d = din("wu", [NE * 128, 2048])
    wd_d = din("wd", [NE * 128, 2048])
    out_d = nc.dram_tensor("out", [S, D], F32, kind="ExternalOutput").ap()
    if _CACHE.get("p2only"):
        z_d = din("z_in", [S, D])
    else:
        z_d = nc.dram_tensor("z_scr", [S, D], F32, kind="Internal").ap()
    xs_d = nc.dram_tensor("xs_scr", [NS * T, D], BF16, kind="Internal").ap()
    ys_d = nc.dram_tensor("ys_scr", [NS * T, D], F32, kind="Internal").ap()
    dbg_d = None
    if dbg:
        dbg_d = nc.dram_tensor("dbg", list(dbg["shape"]), F32, kind="ExternalOutput").ap()

    import contextlib
    with contextlib.ExitStack() as st:
        ARENA_BYTES = 206 * 1024
        arena_t = st.enter_context(nc.sbuf_tensor("arena", [128, ARENA_BYTES // 4], F32))
        ps = [st.enter_context(nc.psum_tensor(f"ps{i}", [128, 512], F32)) for i in range(6)]
        psb = [st.enter_context(nc.psum_tensor(f"psb{i}", [128, 1024], BF16)) for i in range(2)]
        sems = [st.enter_context(nc.semaphore(f"s{i}")) for i in range(_CACHE.get("nsem", 48))]
        P = Prog(nc, sems)
        A = Arena(arena_t, ARENA_BYTES)
        psn = [0]

        def nps():
            i = psn[0] % 6
            psn[0] += 1
            return ps[i], f"ps{i}"

        cols = A.alloc([NCOL], F32)
        identf = A.alloc([128], F32)
        identb = A.alloc([128], BF16)
        onesb = A.alloc([128], BF16)
        ones512 = A.alloc([128], BF16)
        bdb = A.alloc([128], BF16)
        Ub = A.alloc([128], BF16)
        maskT = A.alloc([2, 512], BF16)
        modc = A.alloc([32], F32)
        g1bc = A.alloc([D], F32)
        gbrow = A.alloc([D], BF16)
        EPSC = A.alloc([1], F32)
        EPSC2 = A.alloc([1], F32)
        mark_persist = A.off
        cst = A.alloc([NK], F32)
        gbc = A.alloc([4, D], F32)
        mod_d = nc.dram_tensor("mod_scr", [1, 3 * D], F32, kind="Internal").ap()

        P.op("sp", lambda e: e.dma_start(out=cols, in_=cols_d), w=["cols"], dma=True)
        P.op("sp", lambda e: e.dma_start(out=cst, in_=cst_d), w=["cst0"], dma=True)
        P.op("sp", lambda e: e.dma_start(out=identf, in_=cst_d[:, K_ID:K_ID + 128]), w=["cst"], dma=True)
        P.op("dve", lambda e: e.tensor_copy(out=identb, in_=identf), r=["cst"], w=["identb"])
        P.op("dve", lambda e: e.memset(onesb, 1.0), w=["onesb"])
        P.op("dve", lambda e: e.memset(EPSC, EPS), w=["epsc"])
        P.op("dve", lambda e: e.memset(EPSC2, EPS / (ALPHA * ALPHA)), w=["epsc2"])
        P.op("dve", lambda e: e.memset(ones512, 1.0 / 512.0), w=["ones512"])
        P.op("dve", lambda e: e.tensor_copy(out=bdb, in_=cst[:, K_BD:K_BD + 128]), r=["cst0"], w=["bdb"])
        P.op("dve", lambda e: e.tensor_copy(out=Ub, in_=cst[:, K_U:K_U + 128]), r=["cst0"], w=["Ub"])
        P.op("dve", lambda e: e.tensor_copy(out=maskT.rearrange("p a b -> p (a b)"), in_=cst[:, K_M0:K_M0 + 1024]),
             r=["cst0"], w=["maskT"])

        ph0 = A.off
        cact = A.alloc([8], F32)
        cbc = A.alloc([8, 128], BF16)
        wab = [A.alloc([8, 512], BF16) for _ in range(2)]
        badab = A.alloc([512], F32)
        modb = A.alloc([4 * D], F32)
        P.op("act", lambda e: e.activation(out=cact, in_=cols[:, C_C:C_C + 8], func=AF.Silu), r=["cols"], w=["cact"])
        for c in range(8):
            P.op("dve", lambda e, c=c: e.tensor_scalar(out=cbc[:, c, :], in0=onesb, scalar1=cact[:, c:c + 1],
                                                       scalar2=None, op0=ALU.mult),
                 r=["cact", "onesb"], w=["cbc"])
        for blk in range(12):
            wb = wab[blk % 2]
            hw = f"wab{blk % 2}"
            P.op("pool", lambda e, blk=blk, wb=wb: e.dma_start(
                out=wb, in_=wada_d[:, blk * 512:(blk + 1) * 512].rearrange("(c p) n -> p c n", p=128)),
                w=[hw], dma=True)
            P.op("sp", lambda e, blk=blk: e.dma_start(
                out=badab, in_=rows_d[0:1, R_BADA + blk * 512:R_BADA + (blk + 1) * 512].partition_broadcast(128)),
                w=["badab"], dma=True)
            pt, hp = nps()
            for c in range(8):
                P.op("pe", lambda e, c=c, pt=pt, wb=wb: e.matmul(pt[:], lhsT=cbc[:, c, :], rhs=wb[:, c, :],
                                                               start=(c == 0), stop=(c == 7)),
                     r=["cbc", hw], w=[hp])
            if blk < 4:
                dst = modb[:, blk * 512:(blk + 1) * 512]
                hd = f"modb{blk}"
            else:
                gi = (blk - 4) // 2
                dst = gbc[:, gi, ((blk - 4) % 2) * 512:((blk - 4) % 2 + 1) * 512]
                hd = f"gbc{gi}_{blk % 2}"
            P.op("dve", lambda e, pt=pt, dst=dst: e.tensor_tensor(out=dst, in0=pt[:], in1=badab, op=ALU.add),
                 r=[hp, "badab"], w=[hd])
        srcs = []
        for c in range(8):
            srcs.append((modb[:, c * 128:(c + 1) * 128], f"modb{c // 4}", c, 0.0))
        for c in range(8):
            srcs.append((modb[:, 1024 + c * 128:1024 + (c + 1) * 128], f"modb{2 + c // 4}", 8 + c, 1.0))
        for c in range(8):
            srcs.append((gbc[:, 1, c * 128:(c + 1) * 128], f"gbc1_{c // 4}", 16 + c, 0.0))
        for c in range(8):
            srcs.append((gbc[:, 2, c * 128:(c + 1) * 128], f"gbc2_{c // 4}", 24 + c, 1.0))
        for (src, hs, col, add) in srcs:
            pt, hp = nps()
            P.op("pe", lambda e, pt=pt, src=src: e.transpose(out=pt[:, 0:128], in_=src, identity=identf),
                 r=[hs, "cst"], w=[hp])
            P.op("dve", lambda e, pt=pt, col=col, add=add: e.tensor_scalar(
                out=modc[:, col:col + 1], in0=pt[:, 0:1], scalar1=add, scalar2=None, op0=ALU.add),
                r=[hp], w=["modc"])
        G_ALL = [f"gbc{gi}_{h}" for gi in range(4) for h in range(2)]
        P.op("sp", lambda e: e.dma_start(out=mod_d, in_=gbc[0:1, 1:4, :].rearrange("p a d -> p (a d)")), r=G_ALL, w=["mod_d"], dma=True)
        P.op("dve", lambda e: e.tensor_scalar(out=g1bc, in0=gbc[:, 0, :], scalar1=1.0 / ALPHA, scalar2=None, op0=ALU.mult), r=G_ALL, w=["g1bc"])
        boutb = modb[:, 0:D]
        P.op("sp", lambda e: e.dma_start(out=boutb, in_=rows_d[0:1, R_BOUT:R_BOUT + D].partition_broadcast(128)),
             w=["modb0", "modb1"], dma=True)
        P.op("dve", lambda e: e.tensor_tensor(out=boutb, in0=boutb, in1=g1bc, op=ALU.mult), r=["modb0", "modb1", "g1bc"], w=["modb0", "modb1"])
        P.op("dve", lambda e: e.tensor_copy(out=gbrow, in_=boutb), r=["modb0", "modb1"], w=["gbrow"])
        P.fence()
        if _CACHE.get("stop") == 0:
            P.op("sp", lambda e: e.dma_start(out=dbg_d[0:128, 0:32], in_=modc), r=["modc"], w=["dbg"], dma=True)
            P.op("sp", lambda e: e.dma_start(out=dbg_d[128:256, :], in_=gbc[:, 0, :]), r=["gbc0_0", "gbc0_1"], w=["dbg2"], dma=True)
            P.fence()
            P.emit()
            return nc
        A.off = mark_persist

        win = A.alloc([8, DIN], BF16)
        modc2 = A.alloc([32], F32)
        P.op("dve", lambda e: e.tensor_copy(out=modc2, in_=modc), r=["modc"], w=["modc2"])
        wout = A.alloc([8, D], BF16)
        diag = A.alloc([124, 128], BF16)
        xt = A.alloc([4, D], BF16)
        hT = A.alloc([8, 512], BF16)
        vr = [A.alloc([4, 542], BF16) for _ in range(2)]
        kr = [[A.alloc([640], BF16) for _ in range(2)] for _ in range(2)]
        va = [A.alloc([5, 130], BF16) for _ in range(2)]
        qT = A.alloc([4, 512], BF16)
        sig = A.alloc([512], F32)
        ybf = A.alloc([4, 512], BF16)
        y2bf = A.alloc([4, 512], BF16)
        mean_sb = A.alloc([512], F32)
        m2_sb = A.alloc([512], F32)
        rstd_sb = A.alloc([512], F32)
        nmr_sb = A.alloc([512], F32)
        zc2 = [A.alloc([512], F32) for _ in range(2)]
        sc2_ = [A.alloc([512], F32) for _ in range(2)]
        s2bf2 = [A.alloc([512], BF16) for _ in range(2)]
        r2c2 = [A.alloc([512], F32) for _ in range(2)]
        ycT = A.alloc([8, 512], BF16)
        mx2 = [A.alloc([8], F32) for _ in range(2)]
        mxb2 = [A.alloc([8], BF16) for _ in range(2)]
        nmx2 = [A.alloc([8], F32) for _ in range(2)]
        dcat2 = [A.alloc([2, 512], BF16) for _ in range(2)]
        ET2 = [A.alloc([4, 512], BF16) for _ in range(2)]
        es_t2 = [A.alloc([8], F32) for _ in range(2)]
        den2 = [A.alloc([8], F32) for _ in range(2)]
        osb2 = [A.alloc([8, 64], F32) for _ in range(2)]
        osq2 = [A.alloc([8, 64], F32) for _ in range(2)]
        ssq2 = [A.alloc([8], F32) for _ in range(2)]
        yat2 = [A.alloc([512], BF16) for _ in range(2)]
        rows_sb = A.alloc([128 + 8 + 512], F32)
        xr2 = [A.alloc([D], F32) for _ in range(2)]
        rr2 = [A.alloc([D], F32) for _ in range(2)]
        bnst2 = [A.alloc([12], F32) for _ in range(2)]
        bnag2 = [A.alloc([4], F32) for _ in range(2)]
        print("phase1 arena bytes", A.off)

        def mk_nps(ids):
            st_ = [0]

            def f():
                i = ids[st_[0] % len(ids)]
                st_[0] += 1
                return ps[i], f"ps{i}"
            return f

        bv_bc = rows_sb[:, 0:128]
        sink_bc = rows_sb[:, 128:136]
        aog_bc = rows_sb[:, 136:648]
        P.op("sp", lambda e: e.dma_start(out=rows_sb, in_=rows_d[0:1, R_BV:R_BV + 128 + 8 + 512].partition_broadcast(128)),
             w=["rows_sb"], dma=True)
        for c in range(8):
            P.op("pool", lambda e, c=c: e.dma_start(out=win[:, c, :], in_=win_d[c * 128:(c + 1) * 128, :]), w=[f"win{c}"], dma=True)
            P.op("pool", lambda e, c=c: e.dma_start(out=wout[:, c, :], in_=wout_d[c * 128:(c + 1) * 128, :]), w=[f"wout{c}"], dma=True)
        for c in range(8):
            P.op("dve", lambda e, c=c: e.tensor_tensor(out=wout[:, c, :], in0=wout[:, c, :], in1=g1bc, op=ALU.mult),
                 r=[f"wout{c}", "g1bc"], w=[f"wout{c}"])
        WINH = [f"win{c}" for c in range(8)]
        WOUTH = [f"wout{c}" for c in range(8)]
        SK = _CACHE.get("skip", set())
        for c in range(4 if "diag" not in SK else 0):
            P.op("dve", lambda e, c=c: e.tensor_tensor(
                out=diag[:, c * 31:(c + 1) * 31, :], in0=identb.unsqueeze(1).to_broadcast([128, 31, 128]),
                in1=cols[:, C_CW + c * 31:C_CW + (c + 1) * 31].unsqueeze(2).to_broadcast([128, 31, 128]), op=ALU.mult),
                r=["identb", "cols"], w=["diag"])
        if "memset" not in SK:
            P.op("pool", lambda e: e.memset(vr[0], 0.0), w=["vr0"])
            P.op("pool", lambda e: e.memset(vr[1], 0.0), w=["vr1"])
        for par in range(2 if "memset" not in SK else 0):
            for g in range(2):
                P.op("pool", lambda e, par=par, g=g: e.memset(kr[par][g], 0.0), w=[f"kr{par}"])
            P.op("pool", lambda e, par=par: e.memset(va[par], 0.0), w=[f"va{par}"])
            P.op("dve", lambda e, par=par: e.memset(va[par][:, 1:, 64:65], 1.0), r=[f"va{par}"], w=[f"va{par}"])
            P.op("dve", lambda e, par=par: e.memset(va[par][:, 1:, 129:130], 1.0), r=[f"va{par}"], w=[f"va{par}"])

        if _CACHE.get("stop") == 1:
            P.fence()
            P.op("sp", lambda e: e.dma_start(out=dbg_d[0:128, 0:512], in_=g1bc[:, 0:512]), r=["g1bc"], w=["dbg"], dma=True)
            P.fence()
            P.emit()
            return nc
        for mt in range(0 if _CACHE.get("p2only") else _CACHE.get('nmt_run', NMT)):
            t0 = mt * 512
            vcur, vprev = vr[mt % 2], vr[(mt + 1) % 2]
            hv, hvp = f"vr{mt % 2}", f"vr{(mt + 1) % 2}"
            kT, kTp = kr[mt % 2], kr[(mt + 1) % 2]
            hk, hkp = f"kr{mt % 2}", f"kr{(mt + 1) % 2}"
            vaug, vaugp = va[mt % 2], va[(mt + 1) % 2]
            hva, hvap = f"va{mt % 2}", f"va{(mt + 1) % 2}"
            P.op("pool", lambda e, t0=t0: e.dma_start(out=xt, in_=x_d[t0:t0 + 512, :].rearrange("(s p) d -> p s d", p=128)),
                 w=["xt"], dma=True)
            for c in range(8):
                ptx = psb[c % 2][:, 0:512]
                hp = f"psb{c % 2}"
                for s in range(4 if "notr" not in SK else 0):
                    P.op("pe", lambda e, ptx=ptx, s=s, c=c: e.transpose(
                        out=ptx[:, s * 128:(s + 1) * 128], in_=xt[:, s, c * 128:(c + 1) * 128], identity=identb),
                        r=["xt", "identb"], w=[hp])
                if "noact" not in SK:
                    P.op("act", lambda e, ptx=ptx, c=c: e.activation(
                        out=hT[:, c, :], in_=ptx[:, 0:512], func=AF.Identity, bias=modc2[:, c:c + 1], scale=(1.0 if "fscale" in SK else modc2[:, 8 + c:9 + c])),
                        r=[hp, "modc2"], w=["hT"])
            def stop_at(k):
                if _CACHE.get("stop") == k:
                    P.fence()
                    P.op("sp", lambda e: e.dma_start(out=dbg_d[0:128, 0:512], in_=g1bc[:, 0:512]), r=["g1bc"], w=["dbg"], dma=True)
                    P.fence()
                    P.emit()
                    raise _Stop()
            stop_at(2)
            if mt > 0:
                P.op("pool", lambda e, vcur=vcur, vprev=vprev: e.tensor_copy(out=vcur[:, :, 0:30], in_=vprev[:, :, 512:542]),
                     r=[hvp], w=[hv])
                for g in range(2):
                    P.op("pool", lambda e, kT=kT, kTp=kTp, g=g: e.tensor_copy(out=kT[g][:, 0:128], in_=kTp[g][:, 512:640]),
                         r=[hkp], w=[hk])
                P.op("pool", lambda e, vaug=vaug, vaugp=vaugp: e.tensor_copy(out=vaug[:, 0, :], in_=vaugp[:, 4, :]),
                     r=[hvap], w=[hva])
            for c in range(4):
                pb, hpb = nps()
                for k in range(8):
                    P.op("pe", lambda e, pb=pb, k=k, c=c: e.matmul(
                        pb[:], lhsT=win[:, k, 512 + c * 128:512 + (c + 1) * 128], rhs=hT[:, k, :],
                        start=(k == 0), stop=(k == 7)), r=WINH + ["hT"], w=[hpb])
                pa, hpa = nps()
                for k in range(8):
                    P.op("pe", lambda e, pa=pa, k=k, c=c: e.matmul(
                        pa[:], lhsT=win[:, k, c * 128:(c + 1) * 128], rhs=hT[:, k, :],
                        start=(k == 0), stop=(k == 7)), r=WINH + ["hT"], w=[hpa])
                P.op("act", lambda e, pb=pb, c=c: e.activation(
                    out=sig, in_=pb[:], func=AF.Sigmoid, bias=cols[:, C_BIN + 4 + c:C_BIN + 5 + c], scale=1.0),
                    r=[hpb, "cols"], w=["sig"])
                P.op("dve", lambda e, pa=pa, c=c, vcur=vcur: e.scalar_tensor_tensor(
                    out=vcur[:, c, 30:542], in0=pa[:], scalar=cols[:, C_BIN + c:C_BIN + c + 1], in1=sig,
                    op0=ALU.add, op1=ALU.mult), r=[hpa, "sig", "cols"], w=[hv])
            for i in range(4):
                pq, hpq = nps()
                for k in range(8):
                    P.op("pe", lambda e, pq=pq, k=k, i=i: e.matmul(
                        pq[:], lhsT=win[:, k, 1024 + i * 128:1024 + (i + 1) * 128], rhs=hT[:, k, :],
                        start=(k == 0), stop=(k == 7)), r=WINH + ["hT"], w=[hpq])
                P.op("dve", lambda e, pq=pq, i=i: e.tensor_scalar(
                    out=qT[:, i, :], in0=pq[:], scalar1=cols[:, C_BIN + 8 + i:C_BIN + 9 + i], scalar2=0.125,
                    op0=ALU.add, op1=ALU.mult), r=[hpq, "cols"], w=["qT"])
            pk, hpk = nps()
            for k in range(8):
                P.op("pe", lambda e, pk=pk, k=k: e.matmul(
                    pk[:], lhsT=win[:, k, 1536:1664], rhs=hT[:, k, :], start=(k == 0), stop=(k == 7)),
                    r=WINH + ["hT"], w=[hpk])
            for g in range(2):
                P.op("act", lambda e, pk=pk, g=g, kT=kT: e.activation(
                    out=kT[g][g * 64:(g + 1) * 64, 128:640], in_=pk[g * 64:(g + 1) * 64, :],
                    func=AF.Identity, bias=cols[g * 64:(g + 1) * 64, C_BIN + 12:C_BIN + 13], scale=1.0),
                    r=[hpk, "cols"], w=[hk])
            pv, hpv = nps()
            for s in range(4):
                for k in range(8):
                    P.op("pe", lambda e, pv=pv, s=s, k=k: e.matmul(
                        pv[:, s * 128:(s + 1) * 128], lhsT=hT[:, k, s * 128:(s + 1) * 128], rhs=win[:, k, 1664:1792],
                        start=(k == 0), stop=(k == 7)), r=WINH + ["hT"], w=[hpv])
            for s in range(4):
                blk = s + 1
                P.op("dve", lambda e, pv=pv, s=s, blk=blk, vaug=vaug: e.tensor_tensor(
                    out=vaug[:, blk, :].rearrange("p (g d) -> p g d", g=2)[:, :, 0:64],
                    in0=pv[:, s * 128:(s + 1) * 128].rearrange("p (g d) -> p g d", g=2),
                    in1=bv_bc.rearrange("p (g d) -> p g d", g=2), op=ALU.add),
                    r=[hpv, "rows_sb"], w=[hva])
            stop_at(3)
            for c in range(4):
                py, hpy = nps()
                for j in range(31):
                    P.op("pe", lambda e, py=py, c=c, j=j, vcur=vcur: e.matmul(
                        py[:], lhsT=diag[:, c * 31 + j, :], rhs=vcur[:, c, j:j + 512], start=(j == 0), stop=(j == 30)),
                        r=["diag", hv], w=[hpy])
                P.op("act", lambda e, py=py, c=c: e.activation(
                    out=ybf[:, c, :], in_=py[:], func=AF.Identity, bias=cols[:, C_CB + c:C_CB + c + 1], scale=1.0),
                    r=[hpy, "cols"], w=["ybf"])
                P.op("act", lambda e, py=py, c=c: e.activation(
                    out=y2bf[:, c, :], in_=py[:], func=AF.Square, bias=cols[:, C_CB + c:C_CB + c + 1], scale=1.0),
                    r=[hpy, "cols"], w=["y2bf"])
            pm, hpm = nps()
            for c in range(4):
                P.op("pe", lambda e, pm=pm, c=c: e.matmul(pm[:], lhsT=ones512, rhs=ybf[:, c, :], start=(c == 0), stop=(c == 3)),
                     r=["ones512", "ybf"], w=[hpm])
            pe2, hpe2 = nps()
            for c in range(4):
                P.op("pe", lambda e, pe2=pe2, c=c: e.matmul(pe2[:], lhsT=ones512, rhs=y2bf[:, c, :], start=(c == 0), stop=(c == 3)),
                     r=["ones512", "y2bf"], w=[hpe2])
            P.op("act", lambda e, pm=pm: e.activation(out=mean_sb, in_=pm[:], func=AF.Identity), r=[hpm], w=["mean_sb"])
            P.op("dve", lambda e: e.tensor_tensor(out=m2_sb, in0=mean_sb, in1=mean_sb, op=ALU.mult), r=["mean_sb"], w=["m2_sb"])
            P.op("dve", lambda e, pe2=pe2: e.tensor_tensor(out=m2_sb, in0=pe2[:], in1=m2_sb, op=ALU.subtract),
                 r=[hpe2, "m2_sb"], w=["m2_sb"])
            P.op("act", lambda e: e.activation(out=rstd_sb, in_=m2_sb, func=AF.Sqrt, bias=EPSC, scale=1.0), r=["m2_sb", "epsc"], w=["rstd_sb"])
            P.op("dve", lambda e: e.reciprocal(out=rstd_sb, in_=rstd_sb), r=["rstd_sb"], w=["rstd_sb"])
            P.op("dve", lambda e: e.scalar_tensor_tensor(out=nmr_sb, in0=mean_sb, scalar=-1.0, in1=rstd_sb, op0=ALU.mult, op1=ALU.mult),
                 r=["mean_sb", "rstd_sb"], w=["nmr_sb"])
            nps_cl = [mk_nps([0]), mk_nps([1])]
            nps_at = [mk_nps([2, 3]), mk_nps([4, 5])]
            nps_ep = [mk_nps([0, 1, 2]), mk_nps([3, 4, 5])]

            def convln_chain(c):
                k = c % 2
                zc, sc_, s2bf, r2c = zc2[k], sc2_[k], s2bf2[k], r2c2[k]
                P.rec()
                P.op("dve", lambda e: e.tensor_tensor(out=zc, in0=ybf[:, c, :], in1=rstd_sb, op=ALU.mult),
                     r=["ybf", "rstd_sb"], w=[f"zc{k}"])
                P.op("dve", lambda e: e.tensor_tensor(out=zc, in0=zc, in1=nmr_sb, op=ALU.add), r=[f"zc{k}", "nmr_sb"], w=[f"zc{k}"])
                P.op("act", lambda e: e.activation(out=sc_, in_=zc, func=AF.Silu, bias=cols[:, C_LB + c:C_LB + c + 1],
                                                   scale=cols[:, C_LG + c:C_LG + c + 1]), r=[f"zc{k}", "cols"], w=[f"sc{k}"])
                P.op("act", lambda e: e.activation(out=s2bf, in_=sc_, func=AF.Square), r=[f"sc{k}"], w=[f"s2bf{k}"])
                pr, hpr = nps_cl[k]()
                P.op("pe", lambda e: e.matmul(pr[:], lhsT=bdb, rhs=s2bf, start=True, stop=True), r=["bdb", f"s2bf{k}"], w=[hpr])
                P.op("act", lambda e: e.activation(out=r2c, in_=pr[:], func=AF.Sqrt, bias=EPSC, scale=1.0), r=[hpr, "epsc"], w=[f"r2c{k}"])
                P.op("dve", lambda e: e.reciprocal(out=r2c, in_=r2c), r=[f"r2c{k}"], w=[f"r2c{k}"])
                P.op("dve", lambda e: e.scalar_tensor_tensor(out=ycT[:, c, :], in0=sc_, scalar=cols[:, C_OG + c:C_OG + c + 1],
                                                             in1=r2c, op0=ALU.mult, op1=ALU.mult),
                     r=[f"sc{k}", f"r2c{k}", "cols"], w=[f"ycTc{c}"])
                return P.end()

            def attn_chain(s):
                k = s % 2
                mx, mxb, nmx, dcat, ET = mx2[k], mxb2[k], nmx2[k], dcat2[k], ET2[k]
                es_t, den, osb, osq, ssq, yat = es_t2[k], den2[k], osb2[k], osq2[k], ssq2[k], yat2[k]
                mynps = nps_at[k]
                kT_l, vaug_l = kT, vaug
                n = mt * 4 + s
                qs = slice(s * 128, (s + 1) * 128)
                P.rec()
                for i in range(4):
                    pS, hpS = mynps()
                    for g in range(2):
                        P.op("pe", lambda e, pS=pS, i=i, g=g: e.matmul(
                            pS[:, g * 256:(g + 1) * 256], lhsT=qT[:, i, qs], rhs=kT_l[g][:, s * 128:s * 128 + 256],
                            start=True, stop=True), r=["qT", hk], w=[hpS])
                    P.op("dve", lambda e, pS=pS, i=i: e.tensor_reduce(
                        out=mx[:, 2 * i:2 * i + 2], in_=pS[:].rearrange("p (g k) -> p g k", g=2), axis=AX.X, op=ALU.max),
                        r=[hpS], w=[f"mx{k}"])
                P.op("dve", lambda e: e.tensor_copy(out=mxb, in_=mx), r=[f"mx{k}"], w=[f"mxb{k}"])
                P.op("dve", lambda e: e.tensor_scalar(out=nmx, in0=mxb, scalar1=-1.0, scalar2=None, op0=ALU.mult), r=[f"mxb{k}"], w=[f"nmx{k}"])
                for g in range(2):
                    P.op("dve", lambda e, g=g: e.tensor_tensor(
                        out=dcat[:, g, :].rearrange("p (i q) -> p i q", i=4), in0=identb.unsqueeze(1).to_broadcast([128, 4, 128]),
                        in1=nmx.rearrange("p (i g) -> p i g", g=2)[:, :, g:g + 1].to_broadcast([128, 4, 128]), op=ALU.mult),
                        r=["identb", f"nmx{k}"], w=[f"dcat{k}_{g}"])
                khs = [1] if n == 0 else [0, 1]
                for g in range(2):
                    for kh in khs:
                        pT, hpT = mynps()
                        kc = slice(s * 128 + kh * 128, s * 128 + kh * 128 + 128)
                        P.op("pe", lambda e, pT=pT, g=g, kc=kc: e.matmul(
                            pT[:].rearrange("p (i q) -> p i q", i=4), lhsT=kT_l[g][:, kc], rhs=qT[:, :, qs], start=True, stop=False),
                            r=[hk, "qT"], w=[hpT])
                        P.op("pe", lambda e, pT=pT, g=g: e.matmul(pT[:], lhsT=onesb, rhs=dcat[:, g, :], start=False, stop=False),
                             r=["onesb", f"dcat{k}_{g}"], w=[hpT])
                        P.op("pe", lambda e, pT=pT, kh=kh: e.matmul(pT[:], lhsT=identb, rhs=maskT[:, kh, :], start=False, stop=True),
                             r=["identb", "maskT"], w=[hpT])
                        P.op("act", lambda e, pT=pT, g=g, kh=kh: e.activation(out=ET[:, g * 2 + kh, :], in_=pT[:], func=AF.Exp),
                             r=[hpT], w=[f"ET{k}_{g}{kh}"])
                P.op("dve", lambda e: e.tensor_tensor(out=es_t, in0=sink_bc, in1=nmx, op=ALU.add), r=["rows_sb", f"nmx{k}"], w=[f"es_t{k}"])
                P.op("act", lambda e: e.activation(out=es_t, in_=es_t, func=AF.Exp), r=[f"es_t{k}"], w=[f"es_t{k}"])
                for g in range(2):
                    po, hpo = mynps()
                    for i in range(4):
                        for kh in khs:
                            P.op("pe", lambda e, po=po, g=g, i=i, kh=kh: e.matmul(
                                po[:, i * 65:(i + 1) * 65], lhsT=ET[:, g * 2 + kh, i * 128:(i + 1) * 128],
                                rhs=vaug_l[:, s + kh, g * 65:(g + 1) * 65], start=(kh == khs[0]), stop=(kh == 1)),
                                r=[f"ET{k}_{g}{kh}", hva], w=[hpo])
                    P.op("dve", lambda e, po=po, g=g: e.tensor_tensor(
                        out=den.rearrange("p (i g) -> p i g", g=2)[:, :, g:g + 1],
                        in0=po[:, 0:260].rearrange("p (i d) -> p i d", d=65)[:, :, 64:65],
                        in1=es_t.rearrange("p (i g) -> p i g", g=2)[:, :, g:g + 1], op=ALU.add),
                        r=[hpo, f"es_t{k}"], w=[f"den{k}_{g}"])
                    P.op("act", lambda e, po=po, g=g: e.activation(
                        out=osb.rearrange("p (i g) d -> p i g d", g=2)[:, :, g, :],
                        in_=po[:, 0:260].rearrange("p (i d) -> p i d", d=65)[:, :, 0:64], func=AF.Identity),
                        r=[hpo], w=[f"osb{k}_{g}"])
                DH = [f"den{k}_0", f"den{k}_1"]
                OH = [f"osb{k}_0", f"osb{k}_1"]
                P.op("dve", lambda e: e.reciprocal(out=den, in_=den), r=DH, w=DH)
                P.op("dve", lambda e: e.tensor_tensor(out=osb, in0=osb, in1=den.unsqueeze(2).to_broadcast([128, 8, 64]), op=ALU.mult),
                     r=OH + DH, w=OH)
                P.op("act", lambda e: e.activation(out=osq, in_=osb, func=AF.Square), r=OH, w=[f"osq{k}"])
                P.op("dve", lambda e: e.tensor_reduce(out=ssq, in_=osq, axis=AX.X, op=ALU.add), r=[f"osq{k}"], w=[f"ssq{k}"])
                P.op("act", lambda e: e.activation(out=ssq, in_=ssq, func=AF.Sqrt, bias=EPSC, scale=1.0 / 64.0), r=[f"ssq{k}", "epsc"], w=[f"ssq{k}"])
                P.op("dve", lambda e: e.reciprocal(out=ssq, in_=ssq), r=[f"ssq{k}"], w=[f"ssq{k}"])
                P.op("dve", lambda e: e.tensor_tensor(out=osb, in0=osb, in1=ssq.unsqueeze(2).to_broadcast([128, 8, 64]), op=ALU.mult),
                     r=OH + [f"ssq{k}"], w=OH)
                P.op("dve", lambda e: e.tensor_tensor(out=yat, in0=osb.rearrange("p h d -> p (h d)"), in1=aog_bc, op=ALU.mult),
                     r=OH + ["rows_sb"], w=[f"yat{k}"])
                ptb = psb[k][:, 0:512]
                hptr = f"psb{k}"
                for i in range(4):
                    P.op("pe", lambda e, i=i: e.transpose(out=ptb[:, i * 128:(i + 1) * 128], in_=yat[:, i * 128:(i + 1) * 128],
                                                         identity=identb), r=[f"yat{k}", "identb"], w=[hptr])
                P.op("act", lambda e: e.activation(
                    out=ycT[:, 4:8, qs], in_=ptb[:, 0:512].rearrange("p (i q) -> p i q", i=4), func=AF.Identity),
                    r=[hptr], w=[f"ycTa{s}"])
                return P.end()

            def xr_load(s):
                k = s % 2
                tt = mt * 4 + s
                P.op("sp", lambda e: e.dma_start(out=xr2[k], in_=x_d[tt * 128:(tt + 1) * 128, :]), w=[f"xr{k}"], dma=True)

            def epi_chain(s, with_load):
                k = s % 2
                tt = mt * 4 + s
                xr, rr, bnst, bnag = xr2[k], rr2[k], bnst2[k], bnag2[k]
                mynps = nps_ep[k]
                YH = [f"ycTc{c}" for c in range(4)] + [f"ycTa{s}"]
                P.rec()
                if with_load:
                    xr_load(s)
                for h in range(2):
                    po, hpo = mynps()
                    for c in range(8):
                        P.op("pe", lambda e, po=po, c=c, h=h: e.matmul(
                            po[:], lhsT=ycT[:, c, s * 128:(s + 1) * 128], rhs=wout[:, c, h * 512:(h + 1) * 512],
                            start=(c == 0), stop=False), r=YH + WOUTH, w=[hpo])
                    P.op("pe", lambda e, po=po, h=h: e.matmul(
                        po[:], lhsT=onesb[0:1, :], rhs=gbrow[0:1, h * 512:(h + 1) * 512], start=False, stop=True),
                        r=["onesb", "gbrow"], w=[hpo])
                    P.op("dve", lambda e, po=po, h=h: e.tensor_tensor(out=rr[:, h * 512:(h + 1) * 512], in0=po[:],
                                                                      in1=xr[:, h * 512:(h + 1) * 512], op=ALU.add),
                         r=[hpo, f"xr{k}"], w=[f"rr{k}"])
                for h in range(2):
                    P.op("dve", lambda e, h=h: e.bn_stats(out=bnst[:, h * 6:(h + 1) * 6], in_=rr[:, h * 512:(h + 1) * 512]),
                         r=[f"rr{k}"], w=[f"bnst{k}"])
                P.op("dve", lambda e: e.bn_aggr(out=bnag[:, 0:2], in_=bnst), r=[f"bnst{k}"], w=[f"bnag{k}"])
                P.op("act", lambda e: e.activation(out=bnag[:, 2:3], in_=bnag[:, 1:2], func=AF.Sqrt, bias=EPSC2, scale=1.0),
                     r=[f"bnag{k}", "epsc2"], w=[f"bnagb{k}"])
                P.op("dve", lambda e: e.reciprocal(out=bnag[:, 2:3], in_=bnag[:, 2:3]), r=[f"bnagb{k}"], w=[f"bnagb{k}"])
                P.op("dve", lambda e: e.scalar_tensor_tensor(out=bnag[:, 3:4], in0=bnag[:, 0:1], scalar=-1.0, in1=bnag[:, 2:3],
                                                             op0=ALU.mult, op1=ALU.mult), r=[f"bnag{k}", f"bnagb{k}"], w=[f"bnagc{k}"])
                P.op("act", lambda e: e.activation(out=rr, in_=rr, func=AF.Identity, bias=bnag[:, 3:4], scale=bnag[:, 2:3]),
                     r=[f"rr{k}", f"bnagb{k}", f"bnagc{k}"], w=[f"rr{k}"])
                P.op("sp", lambda e: e.dma_start(out=z_d[tt * 128:(tt + 1) * 128, :], in_=rr), r=[f"rr{k}"], w=[f"z_d{tt}"], dma=True)
                return P.end()

            xr_load(0)
            xr_load(1)
            P.play(merge(convln_chain(0), convln_chain(1), attn_chain(0), attn_chain(1)))
            P.play(merge(convln_chain(2), convln_chain(3), attn_chain(2), attn_chain(3)))
            P.play(merge(epi_chain(0, False), epi_chain(1, False)))
            P.play(merge(epi_chain(2, True), epi_chain(3, True)))

        P.fence()
        if dbg and dbg["what"] == "z":
            nr = _CACHE.get('nmt_run', NMT) * 512
            P.op("sp", lambda e: e.dma_start(out=dbg_d[0:nr, :], in_=z_d[0:nr, :]), r=[f"z_d{i}" for i in range(nr // 128)], w=["dbg"], dma=True)
            P.fence()
            P.emit()
            return nc

        try:
            build_phase2(nc, P, A, ps, nps, dr, out_d, z_d, xs_d, ys_d, dbg, dbg_d, mark_persist,
                     dict(cols=cols, identf=identf, identb=identb, onesb=onesb, Ub=Ub, modc=modc, mod_d=mod_d,
                              EPSC=EPSC, psb=psb))
        except _StopEmit:
            pass
        P.fence()
        P.emit()
    return nc


def build_phase2(nc, P, A, ps, nps, dr, out_d, z_d, xs_d, ys_d, dbg, dbg_d, mark, K):
    cols, identf, identb, onesb, Ub, mod_d, EPSC, psb = (K[k] for k in ("cols", "identf", "identb", "onesb", "Ub", "mod_d", "EPSC", "psb"))
    rows_d, wr_d, wg_d, wu_d, wd_d = dr["rows"], dr["w_r"], dr["wg"], dr["wu"], dr["wd"]
    A.off = mark
    gbc = A.alloc([4, D], F32)
    P.op("sp", lambda e: e.dma_start(out=gbc[:, 1:4, :].rearrange("p a d -> p (a d)"), in_=mod_d[0:1, :].partition_broadcast(128)),
         r=["mod_d"], w=["gbc1_0", "gbc1_1", "gbc2_0", "gbc2_1", "gbc3_0", "gbc3_1"], dma=True)
    lnr = A.alloc([4, D], F32)
    misc = A.alloc([36 + 64], F32)
    A2 = A.alloc([D], F32)
    B2 = A.alloc([D], F32)
    GA = A.alloc([D], F32)
    BA = A.alloc([D], F32)
    wr = A.alloc([8, 36], F32)
    pos_i = A.alloc([2, NT], I32)
    wts = A.alloc([2, NT], F32)
    widx_i = A.alloc([64], I32)
    mark2 = A.off
    br_bc = misc[:, 0:36]
    slot_bc = misc[:, 36:36 + NS]
    P.op("sp", lambda e: e.dma_start(out=lnr.rearrange("p a d -> p (a d)"), in_=rows_d[0:1, R_L1G:R_L1G + 4 * D].partition_broadcast(128)),
         w=["lnr"], dma=True)
    P.op("sp", lambda e: e.dma_start(out=misc, in_=rows_d[0:1, R_BR:R_BR + 100].partition_broadcast(128)), w=["misc"], dma=True)
    P.op("sp", lambda e: e.dma_start(out=wr, in_=wr_d.rearrange("(c p) n -> p c n", p=128)), w=["wr"], dma=True)
    G2H = ["gbc1_0", "gbc1_1", "gbc2_0", "gbc2_1", "gbc3_0", "gbc3_1"]
    P.op("dve", lambda e: e.scalar_tensor_tensor(out=A2, in0=gbc[:, 2, :], scalar=1.0, in1=lnr[:, 0, :], op0=ALU.add, op1=ALU.mult),
         r=["lnr"] + G2H, w=["A2"])
    P.op("dve", lambda e: e.scalar_tensor_tensor(out=B2, in0=gbc[:, 2, :], scalar=1.0, in1=lnr[:, 1, :], op0=ALU.add, op1=ALU.mult),
         r=["lnr"] + G2H, w=["B2"])
    P.op("dve", lambda e: e.tensor_tensor(out=B2, in0=B2, in1=gbc[:, 1, :], op=ALU.add), r=["B2"] + G2H, w=["B2"])
    P.op("dve", lambda e: e.tensor_scalar(out=GA, in0=lnr[:, 0, :], scalar1=ALPHA, scalar2=None, op0=ALU.mult), r=["lnr"], w=["GA"])
    P.op("dve", lambda e: e.tensor_scalar(out=BA, in0=lnr[:, 1, :], scalar1=ALPHA, scalar2=None, op0=ALU.mult), r=["lnr"], w=["BA"])

    def mk_nps(ids):
        st_ = [0]

        def f():
            i = ids[st_[0] % len(ids)]
            st_[0] += 1
            return ps[i], f"ps{i}"
        return f

    t1_d = nc.dram_tensor("t1_scr", [S, D], F32, kind="Internal").ap()
    h2b = A.alloc([NT, D], BF16)
    logits = A.alloc([NT, 36], F32)
    mark2a = A.off
    zt = [A.alloc([D], F32) for _ in range(2)]
    h2f = [A.alloc([D], F32) for _ in range(2)]
    h2T = [A.alloc([8, 128], F32) for _ in range(2)]
    t1s = [A.alloc([D], F32) for _ in range(2)]
    nps2a = [mk_nps([0, 1, 2]), mk_nps([3, 4, 5])]

    def chain2a(j):
        b = j % 2
        mynps = nps2a[b]
        z_, hz = zt[b], f"zt{b}"
        hf, hhf = h2f[b], f"h2f{b}"
        hT_, hhT = h2T[b], f"h2T{b}"
        t1_, ht1 = t1s[b], f"t1s{b}"
        P.rec()
        P.op("sp", lambda e: e.dma_start(out=z_, in_=z_d[j * 128:(j + 1) * 128, :]), r=[f"z_d{j}"], w=[hz], dma=True)
        P.op("dve", lambda e: e.tensor_tensor(out=hf, in0=z_, in1=A2, op=ALU.mult), r=[hz, "A2"], w=[hhf])
        P.op("dve", lambda e: e.tensor_tensor(out=hf, in0=hf, in1=B2, op=ALU.add), r=[hhf, "B2"], w=[hhf])
        P.op("pool", lambda e: e.tensor_tensor(out=t1_, in0=z_, in1=GA, op=ALU.mult), r=[hz, "GA"], w=[ht1])
        P.op("dve", lambda e: e.tensor_tensor(out=t1_, in0=t1_, in1=BA, op=ALU.add), r=[ht1, "BA"], w=[ht1])
        P.op("sp", lambda e: e.dma_start(out=t1_d[j * 128:(j + 1) * 128, :], in_=t1_), r=[ht1], w=[f"t1_d{j}"], dma=True)
        P.op("act", lambda e: e.activation(out=h2b[:, j, :], in_=hf, func=AF.Identity), r=[hhf], w=[f"h2b{j}"])
        for hh in range(2):
            pt, hp = mynps()
            for c4 in range(4):
                c = hh * 4 + c4
                P.op("pe", lambda e, pt=pt, c=c, c4=c4: e.transpose(out=pt[:, c4 * 128:(c4 + 1) * 128],
                                                                  in_=hf[:, c * 128:(c + 1) * 128], identity=identf),
                     r=[hhf, "cst"], w=[hp])
            if hh == 0:
                P.op("act", lambda e, pt=pt: e.activation(out=hT_[:, 0:4, :].rearrange("p c t -> p (c t)"),
                                                          in_=pt[:], func=AF.Identity), r=[hp], w=[hhT + "a"])
            else:
                P.op("act", lambda e, pt=pt: e.activation(out=hT_[:, 4:8, :].rearrange("p c t -> p (c t)"),
                                                          in_=pt[:], func=AF.Identity), r=[hp], w=[hhT + "b"])
        pl, hpl = mynps()
        for c in range(8):
            P.op("pe", lambda e, pl=pl, c=c: e.matmul(pl[:, 0:36], lhsT=hT_[:, c, :], rhs=wr[:, c, :], start=(c == 0), stop=(c == 7)),
                 r=[hhT + "a", hhT + "b", "wr"], w=[hpl])
        P.op("dve", lambda e, pl=pl: e.tensor_tensor(out=logits[:, j, :], in0=pl[:, 0:36], in1=br_bc, op=ALU.add),
             r=[hpl, "misc"], w=["logits"])
        return P.end()

    for j in range(0, NT, 2):
        P.play(merge(chain2a(j), chain2a(j + 1)))
    P.fence()
    A.off = mark2a

    def T3(n):
        return A.alloc([NT, n], F32)
    gmax = A.alloc([NT], F32)
    og = T3(4)
    eg = T3(4)
    sgm = A.alloc([NT], F32)
    ptop = A.alloc([NT], F32)
    tmp4 = A.alloc([NT, 4, 8], F32)
    sel = T3(8)
    sel2 = T3(8)
    m1 = A.alloc([NT], F32)
    m2 = A.alloc([NT], F32)
    o1 = T3(8)
    o2 = T3(8)
    e2 = A.alloc([NT], F32)
    r12 = A.alloc([NT], F32)
    O1 = A.alloc([NT, 4, 8], F32)
    O2 = A.alloc([NT, 4, 8], F32)
    Obf = A.alloc([NT * 32], BF16)
    totA = A.alloc([NT, 32], F32)
    totB = A.alloc([NT, 32], F32)
    tot0 = A.alloc([NT, 32], F32)
    base = A.alloc([NT, 32], F32)
    cnt = A.alloc([32], F32)
    cmpc = A.alloc([32, 24], F32)
    pcnt = A.alloc([32], F32)
    oeA = A.alloc([32], F32)
    oeB = A.alloc([32], F32)
    offs = A.alloc([32], F32)
    cmps = A.alloc([NS, 32], F32)
    esl = A.alloc([64], F32)
    used = A.alloc([64], F32)
    posf = A.alloc([2, NT], F32)

    LG = logits[:, :, 0:4]
    LE4 = logits[:, :, 4:36].rearrange("p j (g e) -> p j g e", g=4)

    def dv(fn, r, w):
        P.op("dve", fn, r=r, w=w)

    dv(lambda e: e.tensor_reduce(out=gmax, in_=LG, axis=AX.X, op=ALU.max), ["logits"], ["gmax"])
    dv(lambda e: e.tensor_tensor(out=og, in0=LG, in1=gmax.unsqueeze(2).to_broadcast([128, NT, 4]), op=ALU.is_equal), ["logits", "gmax"], ["og"])
    dv(lambda e: e.tensor_tensor(out=eg, in0=LG, in1=gmax.unsqueeze(2).to_broadcast([128, NT, 4]), op=ALU.subtract), ["logits", "gmax"], ["eg"])
    P.op("act", lambda e: e.activation(out=eg, in_=eg, func=AF.Exp), r=["eg"], w=["eg"])
    dv(lambda e: e.tensor_reduce(out=sgm, in_=eg, axis=AX.X, op=ALU.add), ["eg"], ["sgm"])
    dv(lambda e: e.reciprocal(out=ptop, in_=sgm), ["sgm"], ["ptop"])
    dv(lambda e: e.tensor_tensor(out=tmp4, in0=LE4, in1=og.unsqueeze(3).to_broadcast([128, NT, 4, 8]), op=ALU.mult), ["logits", "og"], ["tmp4"])
    dv(lambda e: e.tensor_reduce(out=sel, in_=tmp4.rearrange("p j g e -> p j e g"), axis=AX.X, op=ALU.add), ["tmp4"], ["sel"])
    dv(lambda e: e.tensor_reduce(out=m1, in_=sel, axis=AX.X, op=ALU.max), ["sel"], ["m1"])
    dv(lambda e: e.tensor_tensor(out=o1, in0=sel, in1=m1.unsqueeze(2).to_broadcast([128, NT, 8]), op=ALU.is_equal), ["sel", "m1"], ["o1"])
    dv(lambda e: e.scalar_tensor_tensor(out=sel2.rearrange("p j e -> p (j e)"), in0=o1.rearrange("p j e -> p (j e)"), scalar=-1.0e9,
                                        in1=sel.rearrange("p j e -> p (j e)"), op0=ALU.mult, op1=ALU.add), ["o1", "sel"], ["sel2"])
    dv(lambda e: e.tensor_reduce(out=m2, in_=sel2, axis=AX.X, op=ALU.max), ["sel2"], ["m2"])
    dv(lambda e: e.tensor_tensor(out=o2, in0=sel2, in1=m2.unsqueeze(2).to_broadcast([128, NT, 8]), op=ALU.is_equal), ["sel2", "m2"], ["o2"])
    dv(lambda e: e.tensor_tensor(out=e2, in0=m2, in1=m1, op=ALU.subtract), ["m1", "m2"], ["e2"])
    P.op("act", lambda e: e.activation(out=e2, in_=e2, func=AF.Exp), r=["e2"], w=["e2"])
    dv(lambda e: e.tensor_scalar(out=r12, in0=e2, scalar1=1.0, scalar2=None, op0=ALU.add), ["e2"], ["r12"])
    dv(lambda e: e.reciprocal(out=r12, in_=r12), ["r12"], ["r12"])
    dv(lambda e: e.tensor_tensor(out=wts[:, 0, :], in0=r12, in1=ptop, op=ALU.mult), ["r12", "ptop"], ["wts"])
    dv(lambda e: e.tensor_tensor(out=wts[:, 1, :], in0=wts[:, 0, :], in1=e2, op=ALU.mult), ["wts", "e2"], ["wts"])
    dv(lambda e: e.tensor_tensor(out=O1, in0=og.unsqueeze(3).to_broadcast([128, NT, 4, 8]),
                                 in1=o1.unsqueeze(2).to_broadcast([128, NT, 4, 8]), op=ALU.mult), ["og", "o1"], ["O1"])
    dv(lambda e: e.tensor_tensor(out=O2, in0=og.unsqueeze(3).to_broadcast([128, NT, 4, 8]),
                                 in1=o2.unsqueeze(2).to_broadcast([128, NT, 4, 8]), op=ALU.mult), ["og", "o2"], ["O2"])
    O1f = O1.rearrange("p j g e -> p (j g e)")
    O2f = O2.rearrange("p j g e -> p (j g e)")
    dv(lambda e: e.tensor_tensor(out=Obf, in0=O1f, in1=O2f, op=ALU.add), ["O1", "O2"], ["Obf"])
    pcs, pts = [], []
    for h in range(2):
        pc_, hpc = nps()
        P.op("pe", lambda e, pc_=pc_, h=h: e.matmul(pc_[:], lhsT=Ub, rhs=Obf[:, h * 512:(h + 1) * 512], start=True, stop=True),
             r=["Ub", "Obf"], w=[hpc])
        pcs.append((pc_, hpc))
        pt_, hpt = nps()
        P.op("pe", lambda e, pt_=pt_, h=h: e.matmul(pt_[:], lhsT=onesb, rhs=Obf[:, h * 512:(h + 1) * 512], start=True, stop=True),
             r=["onesb", "Obf"], w=[hpt])
        pts.append((pt_, hpt))
    tot0f = tot0.rearrange("p j e -> p (j e)")
    for h in range(2):
        dv(lambda e, h=h: e.tensor_copy(out=tot0f[:, h * 512:(h + 1) * 512], in_=pts[h][0][:]), [pts[h][1]], ["tot0"])
    cur, hc = tot0, "tot0"
    for i_, sft in enumerate((1, 2, 4, 8, 16)):
        nxt, hn = (totA, "totA") if i_ % 2 == 0 else (totB, "totB")
        dv(lambda e, cur=cur, nxt=nxt, sft=sft: e.tensor_tensor(out=nxt[:, sft:, :], in0=cur[:, sft:, :], in1=cur[:, :NT - sft, :], op=ALU.add),
           [hc], [hn])
        dv(lambda e, cur=cur, nxt=nxt, sft=sft: e.tensor_copy(out=nxt[:, :sft, :], in_=cur[:, :sft, :]), [hc, hn], [hn])
        cur, hc = nxt, hn
    incl, hincl = cur, hc
    dv(lambda e: e.tensor_copy(out=cnt, in_=incl[:, NT - 1, :]), [hincl], ["cnt"])
    dv(lambda e: e.tensor_tensor(out=cmpc, in0=cnt.unsqueeze(2).to_broadcast([128, 32, 24]),
                                 in1=slot_bc[:, 0:24].unsqueeze(1).to_broadcast([128, 32, 24]), op=ALU.is_gt), ["cnt", "misc"], ["cmpc"])
    dv(lambda e: e.tensor_reduce(out=pcnt, in_=cmpc, axis=AX.X, op=ALU.add), ["cmpc"], ["pcnt"])
    dv(lambda e: e.tensor_scalar(out=pcnt, in0=pcnt, scalar1=float(T), scalar2=None, op0=ALU.mult), ["pcnt"], ["pcnt"])
    cur, hc = pcnt, "pcnt"
    for i_, sft in enumerate((1, 2, 4, 8, 16)):
        nxt, hn = (oeA, "oeA") if i_ % 2 == 0 else (oeB, "oeB")
        dv(lambda e, cur=cur, nxt=nxt, sft=sft: e.tensor_tensor(out=nxt[:, sft:], in0=cur[:, sft:], in1=cur[:, :32 - sft], op=ALU.add), [hc], [hn])
        dv(lambda e, cur=cur, nxt=nxt, sft=sft: e.tensor_copy(out=nxt[:, :sft], in_=cur[:, :sft]), [hc, hn], [hn])
        cur, hc = nxt, hn
    oend, hoend = cur, hc
    dv(lambda e: e.tensor_tensor(out=offs, in0=oend, in1=pcnt, op=ALU.subtract), [hoend, "pcnt"], ["offs"])
    dv(lambda e: e.tensor_tensor(out=base, in0=incl, in1=tot0, op=ALU.subtract), [hincl, "tot0"], ["base"])
    dv(lambda e: e.tensor_tensor(out=base, in0=base, in1=offs.unsqueeze(1).to_broadcast([128, NT, 32]), op=ALU.add), ["base", "offs"], ["base"])
    basef = base.rearrange("p j e -> p (j e)")
    for h in range(2):
        dv(lambda e, h=h: e.tensor_tensor(out=basef[:, h * 512:(h + 1) * 512], in0=pcs[h][0][:], in1=basef[:, h * 512:(h + 1) * 512], op=ALU.add),
           [pcs[h][1], "base"], ["base"])
    for k, (Ok, hO) in enumerate(((O1, "O1"), (O2, "O2"))):
        dv(lambda e, Ok=Ok: e.tensor_tensor(out=Ok.rearrange("p j g e -> p (j g e)"), in0=Ok.rearrange("p j g e -> p (j g e)"), in1=basef, op=ALU.mult),
           [hO, "base"], [hO])
        dv(lambda e, Ok=Ok, k=k: e.tensor_reduce(out=posf[:, k, :], in_=Ok.rearrange("p j g e -> p j (g e)"), axis=AX.X, op=ALU.add), [hO], ["posf"])
    dv(lambda e: e.tensor_copy(out=pos_i, in_=posf), ["posf"], ["pos_i"])
    dv(lambda e: e.tensor_tensor(out=cmps, in0=oend.unsqueeze(1).to_broadcast([128, NS, 32]),
                                 in1=slot_bc.unsqueeze(2).to_broadcast([128, NS, 32]), op=ALU.is_le), [hoend, "misc"], ["cmps"])
    dv(lambda e: e.tensor_reduce(out=esl[:, 0:NS], in_=cmps, axis=AX.X, op=ALU.add), ["cmps"], ["esl"])
    dv(lambda e: e.tensor_scalar(out=esl[:, 0:NS], in0=esl[:, 0:NS], scalar1=float(NE - 1), scalar2=128.0, op0=ALU.min, op1=ALU.mult), ["esl"], ["esl"])
    dv(lambda e: e.tensor_scalar(out=used[:, 0:NS], in0=slot_bc, scalar1=oend[:, 31:32], scalar2=None, op0=ALU.is_lt), ["misc", hoend], ["used"])
    dv(lambda e: e.tensor_scalar(out=used[:, 0:NS], in0=used[:, 0:NS], scalar1=-1.0e6, scalar2=1.0e6, op0=ALU.mult, op1=ALU.add), ["used"], ["used"])
    dv(lambda e: e.tensor_tensor(out=esl[:, 0:NS], in0=esl[:, 0:NS], in1=used[:, 0:NS], op=ALU.add), ["esl", "used"], ["esl"])
    dv(lambda e: e.tensor_scalar(out=esl[:, 0:NS], in0=esl[:, 0:NS], scalar1=cols[:, C_PID:C_PID + 1], scalar2=None, op0=ALU.add), ["esl", "cols"], ["esl"])
    dv(lambda e: e.tensor_copy(out=widx_i[:, 0:NS], in_=esl[:, 0:NS]), ["esl"], ["widx_i"])

    if dbg and dbg["what"] == "route":
        P.fence()
        P.op("sp", lambda e: e.dma_start(out=dbg_d[0:128, 0:NT * 36], in_=logits.rearrange("p j n -> p (j n)")), r=["logits"], w=["dbg0"], dma=True)
        P.op("sp", lambda e: e.dma_start(out=dbg_d[128:256, 0:2 * NT], in_=posf.rearrange("p k j -> p (k j)")), r=["posf"], w=["dbg1"], dma=True)
        P.op("sp", lambda e: e.dma_start(out=dbg_d[256:384, 0:2 * NT], in_=wts.rearrange("p k j -> p (k j)")), r=["wts"], w=["dbg2"], dma=True)
        P.op("sp", lambda e: e.dma_start(out=dbg_d[384:512, 0:NS], in_=esl[:, 0:NS]), r=["esl"], w=["dbg3"], dma=True)
        P.fence()
        raise _StopEmit()

    for j in range(NT):
        for k in range(2):
            P.op("pool", lambda e, j=j, k=k: e.indirect_dma_start(
                out=xs_d[:, :], out_offset=bass.IndirectOffsetOnAxis(ap=pos_i[:, k, j:j + 1], axis=0),
                in_=h2b[:, j, :], in_offset=None), r=[f"h2b{j}", "pos_i"], w=[f"xs_{j}_{k}"], dma=True)
    P.fence()

    A.off = mark2
    NBW = 4
    wgs = [A.alloc([2048], BF16) for _ in range(NBW)]
    wus = [A.alloc([2048], BF16) for _ in range(NBW)]
    wds = [A.alloc([2048], BF16) for _ in range(NBW)]
    xtok = [A.alloc([NSUB, D], BF16) for _ in range(NBW)]
    XT = [A.alloc([8, T], BF16) for _ in range(2)]
    sgs = [A.alloc([T], F32) for _ in range(2)]
    aT = [A.alloc([2, T], BF16) for _ in range(2)]
    NYO = 3
    yo = [A.alloc([D], F32) for _ in range(NYO)]
    npsB = mk_nps([0, 1, 2])
    npsC = mk_nps([3, 4, 5])
    _bc = {}

    def get_bc(e):
        if "v" not in _bc:
            reg = e.alloc_register("bcreg")
            e.reg_mov(reg, NE * 128 - 1)
            _bc["v"] = e.snap(reg, donate=True)
        return _bc["v"]

    def load_w(s, which):
        bw = s % NBW
        for (wsb, wdr, hn) in which(bw):
            P.op("pool", lambda e, wsb=wsb, wdr=wdr, s=s: e.indirect_dma_start(
                out=wsb, out_offset=None, in_=wdr[:, :],
                in_offset=bass.IndirectOffsetOnAxis(ap=widx_i[:, s:s + 1], axis=0),
                bounds_check=get_bc(e), oob_is_err=False), r=["widx_i"], w=[hn], dma=True)

    def w_gu(bw):
        return ((wgs[bw], wg_d, f"wg{bw}"), (wus[bw], wu_d, f"wu{bw}"))

    def w_d(bw):
        return ((wds[bw], wd_d, f"wd{bw}"),)

    def load_x(s):
        bw = s % NBW
        for st in range(NSUB):
            r0 = s * T + st * 128
            P.op("sp", lambda e, bw=bw, st=st, r0=r0: e.dma_start(out=xtok[bw][:, st, :], in_=xs_d[r0:r0 + 128, :]),
                 w=[f"xtok{bw}_{st}"], dma=True)

    def stageA(s):
        b, bw = s % 2, s % NBW
        P.rec()
        for st in range(NSUB):
            k = (s * NSUB + st) % 2
            pb_, hpb = psb[k], f"psb{k}"
            for c in range(8):
                P.op("pe", lambda e, pb_=pb_, st=st, c=c: e.transpose(out=pb_[:, c * 128:(c + 1) * 128],
                                                                     in_=xtok[bw][:, st, c * 128:(c + 1) * 128], identity=identb),
                     r=[f"xtok{bw}_{st}", "identb"], w=[hpb])
            if True:
                P.op("act", lambda e, pb_=pb_, st=st: e.activation(out=XT[b][:, :, st * 128:(st + 1) * 128],
                                                                   in_=pb_[:].rearrange("p (c t) -> p c t", c=8), func=AF.Identity),
                     r=[hpb], w=[f"XT{b}_{st}"])
            else:
                P.op("dve", lambda e, pb_=pb_, st=st: e.tensor_copy(out=XT[b][:, :, st * 128:(st + 1) * 128],
                                                                    in_=pb_[:].rearrange("p (c t) -> p c t", c=8)),
                     r=[hpb], w=[f"XT{b}_{st}"])
        return P.end()

    def stageB(s):
        b, bw = s % 2, s % NBW
        XH = [f"XT{b}_{st}" for st in range(NSUB)]
        P.rec()
        for fch in range(2):
            pg, hpg = npsB()
            for c in range(8):
                P.op("pe", lambda e, pg=pg, c=c, fch=fch: e.matmul(
                    pg[:, 0:T], lhsT=wgs[bw][:, c * 256 + fch * 128:c * 256 + (fch + 1) * 128], rhs=XT[b][:, c, :],
                    start=(c == 0), stop=(c == 7)), r=[f"wg{bw}"] + XH, w=[hpg])
            P.op("act", lambda e, pg=pg: e.activation(out=sgs[b], in_=pg[:, 0:T], func=AF.Silu), r=[hpg], w=[f"sgs{b}"])
            pu, hpu = npsB()
            for c in range(8):
                P.op("pe", lambda e, pu=pu, c=c, fch=fch: e.matmul(
                    pu[:, 0:T], lhsT=wus[bw][:, c * 256 + fch * 128:c * 256 + (fch + 1) * 128], rhs=XT[b][:, c, :],
                    start=(c == 0), stop=(c == 7)), r=[f"wu{bw}"] + XH, w=[hpu])
            P.op("dve", lambda e, pu=pu, fch=fch: e.tensor_tensor(out=aT[b][:, fch, :], in0=pu[:, 0:T], in1=sgs[b], op=ALU.mult),
                 r=[hpu, f"sgs{b}"], w=[f"aT{b}_{fch}"])
        return P.end()

    def stageC(s):
        b, bw = s % 2, s % NBW
        P.rec()
        for st in range(NSUB):
            yb = (s * NSUB + st) % NYO
            for half in range(2):
                po, hpo = npsC()
                for fch in range(2):
                    P.op("pe", lambda e, po=po, st=st, fch=fch, half=half: e.matmul(
                        po[:], lhsT=aT[b][:, fch, st * 128:(st + 1) * 128],
                        rhs=wds[bw][:, fch * 1024 + half * 512:fch * 1024 + (half + 1) * 512], start=(fch == 0), stop=(fch == 1)),
                        r=[f"aT{b}_0", f"aT{b}_1", f"wd{bw}"], w=[hpo])
                P.op("dve", lambda e, po=po, yb=yb, half=half: e.tensor_tensor(
                    out=yo[yb][:, half * 512:(half + 1) * 512], in0=po[:], in1=gbc[:, 3, half * 512:(half + 1) * 512], op=ALU.mult),
                    r=[hpo, "gbc3_0", "gbc3_1"], w=[f"yo{yb}{'ab'[half]}"])
            r0 = s * T + st * 128
            P.op("sp", lambda e, yb=yb, r0=r0: e.dma_start(out=ys_d[r0:r0 + 128, :], in_=yo[yb]), r=[f"yo{yb}a", f"yo{yb}b"],
                 w=[f"ys_{s}_{st}"], dma=True)
        return P.end()

    for s in range(min(NBW, NS)):
        load_x(s)
        load_w(s, w_gu)
        load_w(s, w_d)
    for i in range(NS + 2):
        lists = []
        if i < NS:
            lists.append(stageA(i))
        if 0 <= i - 1 < NS:
            lists.append(stageB(i - 1))
        if 0 <= i - 2 < NS:
            lists.append(stageC(i - 2))
        P.play(merge(*lists))
        if i + NBW < NS:
            load_x(i + NBW)
        if i - 1 >= 0 and i - 1 + NBW < NS:
            load_w(i - 1 + NBW, w_gu)
        if i - 2 >= 0 and i - 2 + NBW < NS:
            load_w(i - 2 + NBW, w_d)
    P.fence()

    A.off = mark2
    NB2F = 4
    Y1 = [A.alloc([D], F32) for _ in range(NB2F)]
    Y2 = [A.alloc([D], F32) for _ in range(NB2F)]
    t1 = [A.alloc([D], F32) for _ in range(NB2F)]
    ff = [A.alloc([D], F32) for _ in range(2)]
    r2 = [A.alloc([D], F32) for _ in range(2)]
    qq = [A.alloc([D], F32) for _ in range(2)]
    ob = [A.alloc([D], F32) for _ in range(2)]
    bn2 = [A.alloc([12], F32) for _ in range(2)]
    ag2 = [A.alloc([4], F32) for _ in range(2)]

    def loads2f(j):
        q = j % NB2F
        P.op("pool", lambda e: e.indirect_dma_start(
            out=Y1[q], out_offset=None, in_=ys_d[:, :], in_offset=bass.IndirectOffsetOnAxis(ap=pos_i[:, 0, j:j + 1], axis=0)),
            r=["pos_i"], w=[f"Y1{q}"], dma=True)
        P.op("pool", lambda e: e.indirect_dma_start(
            out=Y2[q], out_offset=None, in_=ys_d[:, :], in_offset=bass.IndirectOffsetOnAxis(ap=pos_i[:, 1, j:j + 1], axis=0)),
            r=["pos_i"], w=[f"Y2{q}"], dma=True)
        P.op("sp", lambda e: e.dma_start(out=t1[q], in_=t1_d[j * 128:(j + 1) * 128, :]), r=[f"t1_d{j}"], w=[f"t1{q}"], dma=True)

    def chain2f(j):
        b = j % 2
        q = j % NB2F
        P.rec()
        P.op("act", lambda e: e.activation(out=ff[b], in_=Y1[q], func=AF.Identity, scale=wts[:, 0, j:j + 1]), r=[f"Y1{q}", "wts"], w=[f"ff{b}"])
        P.op("dve", lambda e: e.scalar_tensor_tensor(out=ff[b], in0=Y2[q], scalar=wts[:, 1, j:j + 1], in1=ff[b], op0=ALU.mult, op1=ALU.add),
             r=[f"Y2{q}", "wts", f"ff{b}"], w=[f"ff{b}"])
        P.op("dve", lambda e: e.tensor_tensor(out=r2[b], in0=ff[b], in1=t1[q], op=ALU.add), r=[f"ff{b}", f"t1{q}"], w=[f"r2{b}"])
        for h in range(2):
            P.op("dve", lambda e, h=h: e.bn_stats(out=bn2[b][:, h * 6:(h + 1) * 6], in_=r2[b][:, h * 512:(h + 1) * 512]), r=[f"r2{b}"], w=[f"bn2{b}"])
        P.op("dve", lambda e: e.bn_aggr(out=ag2[b][:, 0:2], in_=bn2[b]), r=[f"bn2{b}"], w=[f"ag2{b}"])
        P.op("act", lambda e: e.activation(out=ag2[b][:, 2:3], in_=ag2[b][:, 1:2], func=AF.Sqrt, bias=EPSC, scale=1.0), r=[f"ag2{b}", "epsc"], w=[f"ag2b{b}"])
        P.op("dve", lambda e: e.reciprocal(out=ag2[b][:, 2:3], in_=ag2[b][:, 2:3]), r=[f"ag2b{b}"], w=[f"ag2b{b}"])
        P.op("dve", lambda e: e.scalar_tensor_tensor(out=ag2[b][:, 3:4], in0=ag2[b][:, 0:1], scalar=-1.0, in1=ag2[b][:, 2:3], op0=ALU.mult, op1=ALU.mult),
             r=[f"ag2{b}", f"ag2b{b}"], w=[f"ag2c{b}"])
        P.op("act", lambda e: e.activation(out=qq[b], in_=r2[b], func=AF.Identity, bias=ag2[b][:, 3:4], scale=ag2[b][:, 2:3]),
             r=[f"r2{b}", f"ag2b{b}", f"ag2c{b}"], w=[f"qq{b}"])
        P.op("pool", lambda e: e.tensor_tensor(out=qq[b], in0=qq[b], in1=lnr[:, 2, :], op=ALU.mult), r=[f"qq{b}", "lnr"], w=[f"qq{b}"])
        P.op("dve", lambda e: e.tensor_tensor(out=ob[b], in0=qq[b], in1=lnr[:, 3, :], op=ALU.add), r=[f"qq{b}", "lnr"], w=[f"ob{b}"])
        P.op("sp", lambda e: e.dma_start(out=out_d[j * 128:(j + 1) * 128, :], in_=ob[b]), r=[f"ob{b}"], w=[f"out{j}"], dma=True)
        return P.end()

    loads2f(0)
    loads2f(1)
    for j in range(0, NT, 2):
        if j + 2 < NT:
            loads2f(j + 2)
            loads2f(j + 3)
        P.play(merge(chain2f(j), chain2f(j + 1)))


class _StopEmit(Exception):
    pass


def _host_prep(inp, b):
    f = np.float32
    L = 0
    cols = np.zeros((128, NCOL), f)
    cols[:, C_C:C_C + 8] = inp["c"][b].reshape(8, 128).T
    w_in = inp["w_in"][L]
    b_in = inp["b_in"][L]
    qcols = np.concatenate([np.arange(1024 + h * 64, 1024 + (h + 1) * 64) for h in HORDER])
    perm = np.concatenate([np.arange(0, 1024), qcols, np.arange(1536, 1792)])
    w_in_p = np.ascontiguousarray(w_in[:, perm])
    b_in_p = b_in[perm]
    cols[:, C_BIN:C_BIN + 13] = b_in_p[:1664].reshape(13, 128).T
    cols[:, C_CW:C_CW + 124] = inp["conv_w"][L].T.reshape(4, 128, 31).transpose(1, 0, 2).reshape(128, 124)
    cols[:, C_CB:C_CB + 4] = inp["conv_b"][L].reshape(4, 128).T
    cols[:, C_LG:C_LG + 4] = inp["conv_ln_g"][L].reshape(4, 128).T
    cols[:, C_LB:C_LB + 4] = inp["conv_ln_b"][L].reshape(4, 128).T
    cols[:, C_OG:C_OG + 4] = inp["conv_out_g"][L].reshape(4, 128).T
    cols[:, C_PID] = np.arange(128)
    rows = np.zeros((1, NROW), f)
    rows[0, R_BADA:R_BADA + 6144] = inp["b_ada"][L]
    rows[0, R_BV:R_BV + 128] = b_in[1664:1792]
    rows[0, R_SINK:R_SINK + 8] = inp["sinks"][L][HORDER]
    rows[0, R_AOG:R_AOG + 512] = inp["attn_out_g"][L].reshape(8, 64)[HORDER].reshape(-1)
    rows[0, R_BOUT:R_BOUT + D] = inp["b_out"][L]
    rows[0, R_L1G:R_L1G + D] = inp["ln1_g"][L]
    rows[0, R_L1B:R_L1B + D] = inp["ln1_b"][L]
    rows[0, R_L2G:R_L2G + D] = inp["ln2_g"][L]
    rows[0, R_L2B:R_L2B + D] = inp["ln2_b"][L]
    rows[0, R_BR:R_BR + 4] = inp["b_router_group"][L]
    rows[0, R_BR + 4:R_BR + 36] = inp["b_router_expert"][L]
    rows[0, R_SLOT:R_SLOT + NS] = np.arange(NS) * T
    w_out = inp["w_out"][L]
    arows = np.concatenate([np.arange(512 + h * 64, 512 + (h + 1) * 64) for h in HORDER])
    w_out_p = np.ascontiguousarray(np.concatenate([w_out[:512], w_out[arows]], axis=0))
    w_r = np.ascontiguousarray(np.concatenate([inp["w_router_group"][L], inp["w_router_expert"][L]], axis=1))
    return dict(cols=cols, rows=rows, w_in=w_in_p, w_out=w_out_p, w_r=w_r)


def _consts():
    f = np.float32
    cst = np.zeros((128, NK), f)
    cst[:, K_ID:K_ID + 128] = np.eye(128)
    p = np.arange(128)
    cst[:, K_U:K_U + 128] = (p[:, None] < p[None, :])
    cst[:, K_BD:K_BD + 128] = ((p[:, None] // 64) == (p[None, :] // 64)) / 64.0
    m0 = np.where(p[:, None] > p[None, :], 0.0, NEG)
    m1 = np.where(p[:, None] <= p[None, :], 0.0, NEG)
    cst[:, K_M0:K_M0 + 512] = np.tile(m0, (1, 4))
    cst[:, K_M1:K_M1 + 512] = np.tile(m1, (1, 4))
    return cst


def _expert_layout(inp):
    L = 0
    wg = np.ascontiguousarray(inp["w_gate"][L].reshape(NE, 8, 128, DE).transpose(0, 2, 1, 3)).reshape(NE * 128, 2048)
    wu = np.ascontiguousarray(inp["w_up"][L].reshape(NE, 8, 128, DE).transpose(0, 2, 1, 3)).reshape(NE * 128, 2048)
    wd = np.ascontiguousarray(inp["w_down"][L].reshape(NE, 2, 128, D).transpose(0, 2, 1, 3)).reshape(NE * 128, 2048)
    return wg, wu, wd


_CACHE = {}


def kernel(**inputs):
    inp = {k: np.asarray(v) for k, v in inputs.items()}
    dbg = _CACHE.get("dbg")
    nc = build_program(dbg)
    cst = _consts()
    wg, wu, wd = _expert_layout(inp)
    w_ada = np.ascontiguousarray(inp["w_ada"][0])
    in_maps = []
    ncores = _CACHE.get("ncores", 8)
    for b in range(ncores):
        hp = _host_prep(inp, b)
        m = dict(x=np.ascontiguousarray(inp["x"][b]), cols=hp["cols"], rows=hp["rows"], cst=cst, w_ada=w_ada,
                 w_in=hp["w_in"], w_out=hp["w_out"], w_r=hp["w_r"], wg=wg, wu=wu, wd=wd)
        if _CACHE.get("p2only"):
            m["z_in"] = _CACHE["z_in"]
        in_maps.append(m)
    if _CACHE.get("trace"):
        res = run_bass_kernel_spmd(nc, in_maps, core_ids=list(range(ncores)), trace=True)
        print("EXEC_NS", res.exec_time_ns)
    else:
        res = run_bass_kernel_spmd(nc, in_maps, core_ids=list(range(ncores)))
    if dbg:
        return [np.asarray(r["dbg"]) for r in res.results]
    out = np.stack([np.asarray(r["out"]) for r in res.results], axis=0).astype(np.float32)
    return out
```

```python
import os
import numpy as np
import concourse.bass as bass
import concourse.mybir as mybir
from concourse.bass_utils import run_bass_kernel_spmd

F32 = mybir.dt.float32
BF16 = mybir.dt.bfloat16
I32 = mybir.dt.int32
ALU = mybir.AluOpType
AF = mybir.ActivationFunctionType
AX = mybir.AxisListType

D = 1024
S = 4096
NT = S // 128
NMT = S // 512
DIN = 1792
NE = 32
DE = 256
ALPHA = 2.0 ** 0.25
EPS = 1e-5
NEG = -30000.0
T = 384
NS = (2 * S + NE * (T - 1) + T - 1) // T
NSUB = T // 128
HORDER = [0, 4, 1, 5, 2, 6, 3, 7]

C_C = 0
C_BIN = 8
C_CW = 21
C_CB = 145
C_LG = 149
C_LB = 153
C_OG = 157
C_PID = 161
NCOL = 162
R_BADA = 0
R_BV = 6144
R_SINK = 6272
R_AOG = 6280
R_BOUT = 6792
R_L1G = 7816
R_L1B = 8840
R_L2G = 9864
R_L2B = 10888
R_BR = 11912
R_SLOT = 11948
NROW = 12012
K_ID = 0
K_U = 128
K_BD = 256
K_M0 = 384
K_M1 = 896
NK = 1408


class Prog:
    def __init__(self, nc, sems):
        self.nc = nc
        self.ops = []
        self.last_w = {}
        self.readers = {}
        self.eng_sem = {e: sems[i] for i, e in enumerate(["pe", "act", "dve", "pool"])}
        rest = sems[4:]
        n_sp = (len(rest) * 5) // 10
        n_pool = (len(rest) * 4) // 10
        self.dma_pool = {"sp": rest[:n_sp], "pool": rest[n_sp:n_sp + n_pool], "act": rest[n_sp + n_pool:]}
        self._rec = None

    def rec(self):
        assert self._rec is None
        self._rec = []

    def end(self):
        l = self._rec
        self._rec = None
        return l

    def play(self, lst):
        assert self._rec is None
        for o in lst:
            self.op(*o)

    def op(self, eng, fn, r=(), w=(), dma=False):
        if self._rec is not None:
            self._rec.append((eng, fn, list(r), list(w), dma))
            return None
        i = len(self.ops)
        w = list(w) + [h for h in r if h.startswith("ps") and h not in w]
        raw, oth = set(), set()
        for h in r:
            if h in self.last_w:
                raw.add(self.last_w[h])
        for h in w:
            if h in self.last_w:
                oth.add(self.last_w[h])
            for j in self.readers.get(h, ()):
                oth.add(j)
        for h in w:
            self.last_w[h] = i
            self.readers[h] = []
        for h in r:
            self.readers.setdefault(h, []).append(i)
        deps = []
        for j in sorted(raw | oth):
            p = self.ops[j]
            if j == i:
                continue
            if (not p["dma"]) and p["eng"] == eng:
                if eng == "pe" or j not in raw:
                    continue
            deps.append(j)
        self.ops.append(dict(eng=eng, fn=fn, deps=deps, dma=dma, sig=False))
        for j in deps:
            self.ops[j]["sig"] = True
        return i

    def fence(self, engs=("pe", "act", "dve", "pool", "sp")):
        hs = list(self.last_w.keys())
        for e in engs:
            self.op(e, None, r=hs, w=["_fence_" + e])

    def emit(self):
        nc = self.nc
        ticket = {e: 0 for e in self.eng_sem}
        dma_next = {q: 0 for q in self.dma_pool}
        dma_uses = {}
        for o in self.ops:
            if o["dma"]:
                q = o["eng"]
                pool = self.dma_pool[q]
                sem = pool[dma_next[q] % len(pool)]
                dma_next[q] += 1
                u = dma_uses.get(id(sem), 0)
                o["pre"] = (sem, 16 * u)
                dma_uses[id(sem)] = u + 1
                o["ev"] = (sem, 16 * (u + 1))
            elif o["sig"] and o["fn"] is not None:
                ticket[o["eng"]] += 1
                o["ev"] = (self.eng_sem[o["eng"]], ticket[o["eng"]])
        ops = self.ops

        def run(engname, eobj):
            waited = {}

            def wait(sem, val):
                if val <= 0:
                    return
                if waited.get(id(sem), 0) >= val:
                    return
                eobj.wait_ge(sem, val)
                waited[id(sem)] = val

            for o in ops:
                if o["eng"] != engname:
                    continue
                for j in o["deps"]:
                    ev = ops[j].get("ev")
                    if ev is not None:
                        wait(*ev)
                if o["fn"] is None:
                    continue
                if o["dma"]:
                    wait(*o["pre"])
                    ins = o["fn"](eobj)
                    ins.then_inc(o["ev"][0], 16)
                else:
                    ins = o["fn"](eobj)
                    if o["sig"]:
                        ins.then_inc(o["ev"][0], 1)

        with nc.Block() as block:
            @block.tensor
            def _(e):
                run("pe", e)

            @block.scalar
            def _(e):
                run("act", e)

            @block.vector
            def _(e):
                run("dve", e)

            @block.gpsimd
            def _(e):
                run("pool", e)

            @block.sync
            def _(e):
                run("sp", e)


class _Stop(Exception):
    pass


def merge(*lists):
    lists = [l for l in lists if l]
    out = []
    idx = [0] * len(lists)
    total = sum(len(l) for l in lists)
    while len(out) < total:
        best, bv = None, None
        for k, l in enumerate(lists):
            if idx[k] < len(l):
                v = (idx[k] + 0.5) / len(l)
                if bv is None or v < bv:
                    best, bv = k, v
        out.append(lists[best][idx[best]])
        idx[best] += 1
    return out


class Arena:
    def __init__(self, t, nbytes):
        self.t = t
        self.nbytes = nbytes
        self.off = 0

    def alloc(self, shape, dt):
        esz = 4 if dt in (F32, I32) else 2
        n = int(np.prod(shape)) * esz
        n = (n + 63) // 64 * 64
        assert self.off + n <= self.nbytes, ("arena overflow", self.off, n, self.nbytes)
        v = self.t[:, self.off // 4:(self.off + n) // 4]
        self.off += n
        if dt != F32:
            v = v.bitcast(dt)
        v = v[:, 0:int(np.prod(shape))]
        if len(shape) == 2:
            return v.rearrange("p (a b) -> p a b", a=shape[0])
        if len(shape) == 3:
            return v.rearrange("p (a b c) -> p a b c", a=shape[0], b=shape[1])
        return v


def build_program(dbg=None):
    nc = bass.Bass("TRN2", target_bir_lowering=False)
    try:
        return _build_program(nc, dbg)
    except _Stop:
        return nc


def _build_program(nc, dbg=None):
    dr = {}

    def din(name, shape, dt=F32):
        dr[name] = nc.dram_tensor(name, list(shape), dt, kind="ExternalInput").ap()
        return dr[name]

    x_d = din("x", [S, D])
    cols_d = din("cols", [128, NCOL])
    rows_d = din("rows", [1, NROW])
    cst_d = din("cst", [128, NK])
    wada_d = din("w_ada", [D, 6 * D])
    win_d = din("w_in", [D, DIN])
    wout_d = din("w_out", [D, D])
    wr_d = din("w_r", [D, 36])
    wg_d = din("wg", [NE * 128, 2048])
    wu_d = din("wu", [NE * 128, 2048])
    wd_d = din("wd", [NE * 128, 2048])
    out_d = nc.dram_tensor("out", [S, D], F32, kind="ExternalOutput").ap()
    if _CACHE.get("p2only"):
        z_d = din("z_in", [S, D])
    else:
        z_d = nc.dram_tensor("z_scr", [S, D], F32, kind="Internal").ap()
    xs_d = nc.dram_tensor("xs_scr", [NS * T, D], BF16, kind="Internal").ap()
    ys_d = nc.dram_tensor("ys_scr", [NS * T, D], F32, kind="Internal").ap()
    dbg_d = None
    if dbg:
        dbg_d = nc.dram_tensor("dbg", list(dbg["shape"]), F32, kind="ExternalOutput").ap()

    import contextlib
    with contextlib.ExitStack() as st:
        ARENA_BYTES = 206 * 1024
        arena_t = st.enter_context(nc.sbuf_tensor("arena", [128, ARENA_BYTES // 4], F32))
        ps = [st.enter_context(nc.psum_tensor(f"ps{i}", [128, 512], F32)) for i in range(6)]
        psb = [st.enter_context(nc.psum_tensor(f"psb{i}", [128, 1024], BF16)) for i in range(2)]
        sems = [st.enter_context(nc.semaphore(f"s{i}")) for i in range(_CACHE.get("nsem", 48))]
        P = Prog(nc, sems)
        A = Arena(arena_t, ARENA_BYTES)
        psn = [0]

        def nps():
            i = psn[0] % 6
            psn[0] += 1
            return ps[i], f"ps{i}"

        cols = A.alloc([NCOL], F32)
        identf = A.alloc([128], F32)
        identb = A.alloc([128], BF16)
        onesb = A.alloc([128], BF16)
        ones512 = A.alloc([128], BF16)
        bdb = A.alloc([128], BF16)
        Ub = A.alloc([128], BF16)
        maskT = A.alloc([2, 512], BF16)
        modc = A.alloc([32], F32)
        g1bc = A.alloc([D], F32)
        gbrow = A.alloc([D], BF16)
        EPSC = A.alloc([1], F32)
        EPSC2 = A.alloc([1], F32)
        mark_persist = A.off
        cst = A.alloc([NK], F32)
        gbc = A.alloc([4, D], F32)
        mod_d = nc.dram_tensor("mod_scr", [1, 3 * D], F32, kind="Internal").ap()

        P.op("sp", lambda e: e.dma_start(out=cols, in_=cols_d), w=["cols"], dma=True)
        P.op("sp", lambda e: e.dma_start(out=cst, in_=cst_d), w=["cst0"], dma=True)
        P.op("sp", lambda e: e.dma_start(out=identf, in_=cst_d[:, K_ID:K_ID + 128]), w=["cst"], dma=True)
        P.op("dve", lambda e: e.tensor_copy(out=identb, in_=identf), r=["cst"], w=["identb"])
        P.op("dve", lambda e: e.memset(onesb, 1.0), w=["onesb"])
        P.op("dve", lambda e: e.memset(EPSC, EPS), w=["epsc"])
        P.op("dve", lambda e: e.memset(EPSC2, EPS / (ALPHA * ALPHA)), w=["epsc2"])
        P.op("dve", lambda e: e.memset(ones512, 1.0 / 512.0), w=["ones512"])
        P.op("dve", lambda e: e.tensor_copy(out=bdb, in_=cst[:, K_BD:K_BD + 128]), r=["cst0"], w=["bdb"])
        P.op("dve", lambda e: e.tensor_copy(out=Ub, in_=cst[:, K_U:K_U + 128]), r=["cst0"], w=["Ub"])
        P.op("dve", lambda e: e.tensor_copy(out=maskT.rearrange("p a b -> p (a b)"), in_=cst[:, K_M0:K_M0 + 1024]),
             r=["cst0"], w=["maskT"])

        ph0 = A.off
        cact = A.alloc([8], F32)
        cbc = A.alloc([8, 128], BF16)
        wab = [A.alloc([8, 512], BF16) for _ in range(2)]
        badab = A.alloc([512], F32)
        modb = A.alloc([4 * D], F32)
        P.op("act", lambda e: e.activation(out=cact, in_=cols[:, C_C:C_C + 8], func=AF.Silu), r=["cols"], w=["cact"])
        for c in range(8):
            P.op("dve", lambda e, c=c: e.tensor_scalar(out=cbc[:, c, :], in0=onesb, scalar1=cact[:, c:c + 1],
                                                       scalar2=None, op0=ALU.mult),
                 r=["cact", "onesb"], w=["cbc"])
        for blk in range(12):
            wb = wab[blk % 2]
            hw = f"wab{blk % 2}"
            P.op("pool", lambda e, blk=blk, wb=wb: e.dma_start(
                out=wb, in_=wada_d[:, blk * 512:(blk + 1) * 512].rearrange("(c p) n -> p c n", p=128)),
                w=[hw], dma=True)
            P.op("sp", lambda e, blk=blk: e.dma_start(
                out=badab, in_=rows_d[0:1, R_BADA + blk * 512:R_BADA + (blk + 1) * 512].partition_broadcast(128)),
                w=["badab"], dma=True)
            pt, hp = nps()
            for c in range(8):
                P.op("pe", lambda e, c=c, pt=pt, wb=wb: e.matmul(pt[:], lhsT=cbc[:, c, :], rhs=wb[:, c, :],
                                                               start=(c == 0), stop=(c == 7)),
                     r=["cbc", hw], w=[hp])
            if blk < 4:
                dst = modb[:, blk * 512:(blk + 1) * 512]
                hd = f"modb{blk}"
            else:
                gi = (blk - 4) // 2
                dst = gbc[:, gi, ((blk - 4) % 2) * 512:((blk - 4) % 2 + 1) * 512]
                hd = f"gbc{gi}_{blk % 2}"
            P.op("dve", lambda e, pt=pt, dst=dst: e.tensor_tensor(out=dst, in0=pt[:], in1=badab, op=ALU.add),
                 r=[hp, "badab"], w=[hd])
        srcs = []
        for c in range(8):
            srcs.append((modb[:, c * 128:(c + 1) * 128], f"modb{c // 4}", c, 0.0))
        for c in range(8):
            srcs.append((modb[:, 1024 + c * 128:1024 + (c + 1) * 128], f"modb{2 + c // 4}", 8 + c, 1.0))
        for c in range(8):
            srcs.append((gbc[:, 1, c * 128:(c + 1) * 128], f"gbc1_{c // 4}", 16 + c, 0.0))
        for c in range(8):
            srcs.append((gbc[:, 2, c * 128:(c + 1) * 128], f"gbc2_{c // 4}", 24 + c, 1.0))
        for (src, hs, col, add) in srcs:
            pt, hp = nps()
            P.op("pe", lambda e, pt=pt, src=src: e.transpose(out=pt[:, 0:128], in_=src, identity=identf),
                 r=[hs, "cst"], w=[hp])
            P.op("dve", lambda e, pt=pt, col=col, add=add: e.tensor_scalar(
                out=modc[:, col:col + 1], in0=pt[:, 0:1], scalar1=add, scalar2=None, op0=ALU.add),
                r=[hp], w=["modc"])
        G_ALL = [f"gbc{gi}_{h}" for gi in range(4) for h in range(2)]
        P.op("sp", lambda e: e.dma_start(out=mod_d, in_=gbc[0:1, 1:4, :].rearrange("p a d -> p (a d)")), r=G_ALL, w=["mod_d"], dma=True)
        P.op("dve", lambda e: e.tensor_scalar(out=g1bc, in0=gbc[:, 0, :], scalar1=1.0 / ALPHA, scalar2=None, op0=ALU.mult), r=G_ALL, w=["g1bc"])
        boutb = modb[:, 0:D]
        P.op("sp", lambda e: e.dma_start(out=boutb, in_=rows_d[0:1, R_BOUT:R_BOUT + D].partition_broadcast(128)),
             w=["modb0", "modb1"], dma=True)
        P.op("dve", lambda e: e.tensor_tensor(out=boutb, in0=boutb, in1=g1bc, op=ALU.mult), r=["modb0", "modb1", "g1bc"], w=["modb0", "modb1"])
        P.op("dve", lambda e: e.tensor_copy(out=gbrow, in_=boutb), r=["modb0", "modb1"], w=["gbrow"])
        P.fence()
        if _CACHE.get("stop") == 0:
            P.op("sp", lambda e: e.dma_start(out=dbg_d[0:128, 0:32], in_=modc), r=["modc"], w=["dbg"], dma=True)
            P.op("sp", lambda e: e.dma_start(out=dbg_d[128:256, :], in_=gbc[:, 0, :]), r=["gbc0_0", "gbc0_1"], w=["dbg2"], dma=True)
            P.fence()
            P.emit()
            return nc
        A.off = mark_persist

        win = A.alloc([8, DIN], BF16)
        modc2 = A.alloc([32], F32)
        P.op("dve", lambda e: e.tensor_copy(out=modc2, in_=modc), r=["modc"], w=["modc2"])
        wout = A.alloc([8, D], BF16)
        diag = A.alloc([124, 128], BF16)
        xt = A.alloc([4, D], BF16)
        hT = A.alloc([8, 512], BF16)
        vr = [A.alloc([4, 542], BF16) for _ in range(2)]
        kr = [[A.alloc([640], BF16) for _ in range(2)] for _ in range(2)]
        va = [A.alloc([5, 130], BF16) for _ in range(2)]
        qT2 = [A.alloc([4, 512], BF16) for _ in range(2)]
        sig = A.alloc([512], F32)
        ybf2 = [A.alloc([4, 512], BF16) for _ in range(2)]
        y2bf = A.alloc([4, 512], BF16)
        rstd2 = [A.alloc([512], F32) for _ in range(2)]
        nmr2 = [A.alloc([512], F32) for _ in range(2)]
        zc2 = [A.alloc([512], F32) for _ in range(2)]
        sc2_ = [A.alloc([512], F32) for _ in range(2)]
        s2bf2 = [A.alloc([512], BF16) for _ in range(2)]
        r2c2 = [A.alloc([512], F32) for _ in range(2)]
        ycT = A.alloc([8, 512], BF16)
        mx2 = [A.alloc([8], F32) for _ in range(2)]
        mxb2 = [A.alloc([8], BF16) for _ in range(2)]
        nmx2 = [A.alloc([8], F32) for _ in range(2)]
        dcat2 = [A.alloc([2, 512], BF16) for _ in range(2)]
        ET2 = [A.alloc([4, 512], BF16) for _ in range(2)]
        es_t2 = [A.alloc([8], F32) for _ in range(2)]
        den2 = [A.alloc([8], F32) for _ in range(2)]
        osb2 = [A.alloc([8, 64], F32) for _ in range(2)]
        osq2 = [A.alloc([8, 64], F32) for _ in range(2)]
        ssq2 = [A.alloc([8], F32) for _ in range(2)]
        yat2 = [A.alloc([512], BF16) for _ in range(2)]
        rows_sb = A.alloc([128 + 8 + 512], F32)
        xr2 = [A.alloc([D], F32) for _ in range(2)]
        bnst2 = [A.alloc([12], F32) for _ in range(2)]
        bnag2 = [A.alloc([4], F32) for _ in range(2)]
        print("phase1 arena bytes", A.off)

        def mk_nps(ids):
            st_ = [0]

            def f():
                i = ids[st_[0] % len(ids)]
                st_[0] += 1
                return ps[i], f"ps{i}"
            return f

        bv_bc = rows_sb[:, 0:128]
        sink_bc = rows_sb[:, 128:136]
        aog_bc = rows_sb[:, 136:648]
        P.op("sp", lambda e: e.dma_start(out=rows_sb, in_=rows_d[0:1, R_BV:R_BV + 128 + 8 + 512].partition_broadcast(128)),
             w=["rows_sb"], dma=True)
        for c in range(8):
            P.op("pool", lambda e, c=c: e.dma_start(out=win[:, c, :], in_=win_d[c * 128:(c + 1) * 128, :]), w=[f"win{c}"], dma=True)
            P.op("pool", lambda e, c=c: e.dma_start(out=wout[:, c, :], in_=wout_d[c * 128:(c + 1) * 128, :]), w=[f"wout{c}"], dma=True)
        for c in range(8):
            P.op("dve", lambda e, c=c: e.tensor_tensor(out=wout[:, c, :], in0=wout[:, c, :], in1=g1bc, op=ALU.mult),
                 r=[f"wout{c}", "g1bc"], w=[f"wout{c}"])
        WINH = [f"win{c}" for c in range(8)]
        WOUTH = [f"wout{c}" for c in range(8)]
        SK = _CACHE.get("skip", set())
        for c in range(4 if "diag" not in SK else 0):
            P.op("dve", lambda e, c=c: e.tensor_tensor(
                out=diag[:, c * 31:(c + 1) * 31, :], in0=identb.unsqueeze(1).to_broadcast([128, 31, 128]),
                in1=cols[:, C_CW + c * 31:C_CW + (c + 1) * 31].unsqueeze(2).to_broadcast([128, 31, 128]), op=ALU.mult),
                r=["identb", "cols"], w=["diag"])
        if "memset" not in SK:
            P.op("pool", lambda e: e.memset(vr[0], 0.0), w=["vr0"])
            P.op("pool", lambda e: e.memset(vr[1], 0.0), w=["vr1"])
        for par in range(2 if "memset" not in SK else 0):
            for g in range(2):
                P.op("pool", lambda e, par=par, g=g: e.memset(kr[par][g], 0.0), w=[f"kr{par}"])
            P.op("pool", lambda e, par=par: e.memset(va[par], 0.0), w=[f"va{par}"])
            P.op("dve", lambda e, par=par: e.memset(va[par][:, 1:, 64:65], 1.0), r=[f"va{par}"], w=[f"va{par}"])
            P.op("dve", lambda e, par=par: e.memset(va[par][:, 1:, 129:130], 1.0), r=[f"va{par}"], w=[f"va{par}"])

        if _CACHE.get("stop") == 1:
            P.fence()
            P.op("sp", lambda e: e.dma_start(out=dbg_d[0:128, 0:512], in_=g1bc[:, 0:512]), r=["g1bc"], w=["dbg"], dma=True)
            P.fence()
            P.emit()
            return nc
        nps_s1 = mk_nps([0, 1])
        psb1_f32 = psb[1][:, :].bitcast(F32)
        nps_at = [mk_nps([2, 3]), mk_nps([4, 5])]
        nps_ep = [mk_nps([2, 3]), mk_nps([4, 5])]

        def stage1(mt):
            par = mt % 2
            t0 = mt * 512
            vcur, vprev = vr[par], vr[1 - par]
            hv, hvp = f"vr{par}", f"vr{1 - par}"
            kT, kTp = kr[par], kr[1 - par]
            hk, hkp = f"kr{par}", f"kr{1 - par}"
            vaug, vaugp = va[par], va[1 - par]
            hva, hvap = f"va{par}", f"va{1 - par}"
            qT, hq = qT2[par], f"qT{par}"
            ybf, hy = ybf2[par], f"ybf{par}"
            rstd_sb, hrs = rstd2[par], f"rstd{par}"
            nmr_sb, hnm = nmr2[par], f"nmr{par}"
            nps = nps_s1
            P.rec()
            P.op("pool", lambda e: e.dma_start(out=xt, in_=x_d[t0:t0 + 512, :].rearrange("(s p) d -> p s d", p=128)),
                 w=["xt"], dma=True)
            for c in range(8):
                ptx = psb[0][:, 0:512]
                hp = "psb0"
                for s in range(4):
                    P.op("pe", lambda e, s=s, c=c: e.transpose(
                        out=ptx[:, s * 128:(s + 1) * 128], in_=xt[:, s, c * 128:(c + 1) * 128], identity=identb),
                        r=["xt", "identb"], w=[hp])
                P.op("act", lambda e, c=c: e.activation(
                    out=hT[:, c, :], in_=ptx[:, 0:512], func=AF.Identity, bias=modc2[:, c:c + 1], scale=modc2[:, 8 + c:9 + c]),
                    r=[hp, "modc2"], w=["hT"])
            if mt > 0:
                P.op("pool", lambda e: e.tensor_copy(out=vcur[:, :, 0:30], in_=vprev[:, :, 512:542]), r=[hvp], w=[hv])
                for g in range(2):
                    P.op("pool", lambda e, g=g: e.tensor_copy(out=kT[g][:, 0:128], in_=kTp[g][:, 512:640]), r=[hkp], w=[hk])
                P.op("pool", lambda e: e.tensor_copy(out=vaug[:, 0, :], in_=vaugp[:, 4, :]), r=[hvap], w=[hva])
            for c in range(4):
                pb, hpb = nps()
                for k in range(8):
                    P.op("pe", lambda e, pb=pb, k=k, c=c: e.matmul(
                        pb[:], lhsT=win[:, k, 512 + c * 128:512 + (c + 1) * 128], rhs=hT[:, k, :],
                        start=(k == 0), stop=(k == 7)), r=WINH + ["hT"], w=[hpb])
                P.op("act", lambda e, pb=pb, c=c: e.activation(
                    out=sig, in_=pb[:], func=AF.Sigmoid, bias=cols[:, C_BIN + 4 + c:C_BIN + 5 + c], scale=1.0),
                    r=[hpb, "cols"], w=["sig"])
                pa, hpa = nps()
                for k in range(8):
                    P.op("pe", lambda e, pa=pa, k=k, c=c: e.matmul(
                        pa[:], lhsT=win[:, k, c * 128:(c + 1) * 128], rhs=hT[:, k, :],
                        start=(k == 0), stop=(k == 7)), r=WINH + ["hT"], w=[hpa])
                P.op("dve", lambda e, pa=pa, c=c: e.scalar_tensor_tensor(
                    out=vcur[:, c, 30:542], in0=pa[:], scalar=cols[:, C_BIN + c:C_BIN + c + 1], in1=sig,
                    op0=ALU.add, op1=ALU.mult), r=[hpa, "sig", "cols"], w=[hv])
            for i in range(4):
                pq, hpq = nps()
                for k in range(8):
                    P.op("pe", lambda e, pq=pq, k=k, i=i: e.matmul(
                        pq[:], lhsT=win[:, k, 1024 + i * 128:1024 + (i + 1) * 128], rhs=hT[:, k, :],
                        start=(k == 0), stop=(k == 7)), r=WINH + ["hT"], w=[hpq])
                P.op("dve", lambda e, pq=pq, i=i: e.tensor_scalar(
                    out=qT[:, i, :], in0=pq[:], scalar1=cols[:, C_BIN + 8 + i:C_BIN + 9 + i], scalar2=0.125,
                    op0=ALU.add, op1=ALU.mult), r=[hpq, "cols"], w=[hq])
            pk, hpk = nps()
            for k in range(8):
                P.op("pe", lambda e, k=k: e.matmul(
                    pk[:], lhsT=win[:, k, 1536:1664], rhs=hT[:, k, :], start=(k == 0), stop=(k == 7)),
                    r=WINH + ["hT"], w=[hpk])
            for g in range(2):
                P.op("act", lambda e, g=g: e.activation(
                    out=kT[g][g * 64:(g + 1) * 64, 128:640], in_=pk[g * 64:(g + 1) * 64, :],
                    func=AF.Identity, bias=cols[g * 64:(g + 1) * 64, C_BIN + 12:C_BIN + 13], scale=1.0),
                    r=[hpk, "cols"], w=[hk])
            pv, hpv = nps()
            for s in range(4):
                for k in range(8):
                    P.op("pe", lambda e, s=s, k=k: e.matmul(
                        pv[:, s * 128:(s + 1) * 128], lhsT=hT[:, k, s * 128:(s + 1) * 128], rhs=win[:, k, 1664:1792],
                        start=(k == 0), stop=(k == 7)), r=WINH + ["hT"], w=[hpv])
            for s in range(4):
                blk = s + 1
                P.op("dve", lambda e, s=s, blk=blk: e.tensor_tensor(
                    out=vaug[:, blk, :].rearrange("p (g d) -> p g d", g=2)[:, :, 0:64],
                    in0=pv[:, s * 128:(s + 1) * 128].rearrange("p (g d) -> p g d", g=2),
                    in1=bv_bc.rearrange("p (g d) -> p g d", g=2), op=ALU.add),
                    r=[hpv, "rows_sb"], w=[hva])
            for c in range(4):
                py, hpy = nps()
                for j in range(31):
                    P.op("pe", lambda e, py=py, c=c, j=j: e.matmul(
                        py[:], lhsT=diag[:, c * 31 + j, :], rhs=vcur[:, c, j:j + 512], start=(j == 0), stop=(j == 30)),
                        r=["diag", hv], w=[hpy])
                P.op("act", lambda e, py=py, c=c: e.activation(
                    out=ybf[:, c, :], in_=py[:], func=AF.Identity, bias=cols[:, C_CB + c:C_CB + c + 1], scale=1.0),
                    r=[hpy, "cols"], w=[hy])
                P.op("act", lambda e, py=py, c=c: e.activation(
                    out=y2bf[:, c, :], in_=py[:], func=AF.Square, bias=cols[:, C_CB + c:C_CB + c + 1], scale=1.0),
                    r=[hpy, "cols"], w=["y2bf"])
            pm, hpm = nps()
            for c in range(4):
                P.op("pe", lambda e, c=c: e.matmul(pm[:], lhsT=ones512, rhs=ybf[:, c, :], start=(c == 0), stop=(c == 3)),
                     r=["ones512", hy], w=[hpm])
            pe2, hpe2 = nps()
            for c in range(4):
                P.op("pe", lambda e, c=c: e.matmul(pe2[:], lhsT=ones512, rhs=y2bf[:, c, :], start=(c == 0), stop=(c == 3)),
                     r=["ones512", "y2bf"], w=[hpe2])
            P.op("act", lambda e: e.activation(out=nmr_sb, in_=pm[:], func=AF.Identity), r=[hpm], w=[hnm])
            P.op("dve", lambda e: e.tensor_tensor(out=rstd_sb, in0=nmr_sb, in1=nmr_sb, op=ALU.mult), r=[hnm], w=[hrs])
            P.op("dve", lambda e: e.tensor_tensor(out=rstd_sb, in0=pe2[:], in1=rstd_sb, op=ALU.subtract), r=[hpe2, hrs], w=[hrs])
            P.op("act", lambda e: e.activation(out=rstd_sb, in_=rstd_sb, func=AF.Sqrt, bias=EPSC, scale=1.0), r=[hrs, "epsc"], w=[hrs])
            P.op("dve", lambda e: e.reciprocal(out=rstd_sb, in_=rstd_sb), r=[hrs], w=[hrs])
            P.op("dve", lambda e: e.scalar_tensor_tensor(out=nmr_sb, in0=nmr_sb, scalar=-1.0, in1=rstd_sb, op0=ALU.mult, op1=ALU.mult),
                 r=[hnm, hrs], w=[hnm])
            return P.end()

        def stage2(mt):
            par = mt % 2
            kT_l, vaug_l = kr[par], va[par]
            hk, hva = f"kr{par}", f"va{par}"
            qT, hq = qT2[par], f"qT{par}"
            ybf, hy = ybf2[par], f"ybf{par}"
            rstd_sb, hrs = rstd2[par], f"rstd{par}"
            nmr_sb, hnm = nmr2[par], f"nmr{par}"

            def convln_chain(c):
                k = c % 2
                zc, sc_, s2bf, r2c = zc2[k], sc2_[k], s2bf2[k], r2c2[k]
                P.rec()
                P.op("dve", lambda e: e.tensor_tensor(out=zc, in0=ybf[:, c, :], in1=rstd_sb, op=ALU.mult),
                     r=[hy, hrs], w=[f"zc{k}"])
                P.op("dve", lambda e: e.tensor_tensor(out=zc, in0=zc, in1=nmr_sb, op=ALU.add), r=[f"zc{k}", hnm], w=[f"zc{k}"])
                P.op("act", lambda e: e.activation(out=sc_, in_=zc, func=AF.Silu, bias=cols[:, C_LB + c:C_LB + c + 1],
                                                   scale=cols[:, C_LG + c:C_LG + c + 1]), r=[f"zc{k}", "cols"], w=[f"sc{k}"])
                P.op("act", lambda e: e.activation(out=s2bf, in_=sc_, func=AF.Square), r=[f"sc{k}"], w=[f"s2bf{k}"])
                pr, hpr = psb1_f32, "psb1"
                P.op("pe", lambda e: e.matmul(pr[:], lhsT=bdb, rhs=s2bf, start=True, stop=True), r=["bdb", f"s2bf{k}"], w=[hpr])
                P.op("act", lambda e: e.activation(out=r2c, in_=pr[:], func=AF.Sqrt, bias=EPSC, scale=1.0), r=[hpr, "epsc"], w=[f"r2c{k}"])
                P.op("dve", lambda e: e.reciprocal(out=r2c, in_=r2c), r=[f"r2c{k}"], w=[f"r2c{k}"])
                P.op("dve", lambda e: e.scalar_tensor_tensor(out=ycT[:, c, :], in0=sc_, scalar=cols[:, C_OG + c:C_OG + c + 1],
                                                             in1=r2c, op0=ALU.mult, op1=ALU.mult),
                     r=[f"sc{k}", f"r2c{k}", "cols"], w=[f"ycTc{c}"])
                return P.end()

            def attn_chain(s):
                k = s % 2
                mx, mxb, nmx, dcat, ET = mx2[k], mxb2[k], nmx2[k], dcat2[k], ET2[k]
                es_t, den, osb, osq, ssq, yat = es_t2[k], den2[k], osb2[k], osq2[k], ssq2[k], yat2[k]
                mynps = nps_at[k]
                n = mt * 4 + s
                qs = slice(s * 128, (s + 1) * 128)
                P.rec()
                for i in range(4):
                    pS, hpS = mynps()
                    for g in range(2):
                        P.op("pe", lambda e, pS=pS, i=i, g=g: e.matmul(
                            pS[:, g * 256:(g + 1) * 256], lhsT=qT[:, i, qs], rhs=kT_l[g][:, s * 128:s * 128 + 256],
                            start=True, stop=True), r=[hq, hk], w=[hpS])
                    P.op("dve", lambda e, pS=pS, i=i: e.tensor_reduce(
                        out=mx[:, 2 * i:2 * i + 2], in_=pS[:].rearrange("p (g k) -> p g k", g=2), axis=AX.X, op=ALU.max),
                        r=[hpS], w=[f"mx{k}"])
                P.op("dve", lambda e: e.tensor_copy(out=mxb, in_=mx), r=[f"mx{k}"], w=[f"mxb{k}"])
                P.op("dve", lambda e: e.tensor_scalar(out=nmx, in0=mxb, scalar1=-1.0, scalar2=None, op0=ALU.mult), r=[f"mxb{k}"], w=[f"nmx{k}"])
                for g in range(2):
                    P.op("dve", lambda e, g=g: e.tensor_tensor(
                        out=dcat[:, g, :].rearrange("p (i q) -> p i q", i=4), in0=identb.unsqueeze(1).to_broadcast([128, 4, 128]),
                        in1=nmx.rearrange("p (i g) -> p i g", g=2)[:, :, g:g + 1].to_broadcast([128, 4, 128]), op=ALU.mult),
                        r=["identb", f"nmx{k}"], w=[f"dcat{k}_{g}"])
                khs = [1] if n == 0 else [0, 1]
                for g in range(2):
                    for kh in khs:
                        pT, hpT = mynps()
                        kc = slice(s * 128 + kh * 128, s * 128 + kh * 128 + 128)
                        P.op("pe", lambda e, pT=pT, g=g, kc=kc: e.matmul(
                            pT[:].rearrange("p (i q) -> p i q", i=4), lhsT=kT_l[g][:, kc], rhs=qT[:, :, qs], start=True, stop=False),
                            r=[hk, hq], w=[hpT])
                        P.op("pe", lambda e, pT=pT, g=g: e.matmul(pT[:], lhsT=onesb, rhs=dcat[:, g, :], start=False, stop=False),
                             r=["onesb", f"dcat{k}_{g}"], w=[hpT])
                        P.op("pe", lambda e, pT=pT, kh=kh: e.matmul(pT[:], lhsT=identb, rhs=maskT[:, kh, :], start=False, stop=True),
                             r=["identb", "maskT"], w=[hpT])
                        P.op("act", lambda e, pT=pT, g=g, kh=kh: e.activation(out=ET[:, g * 2 + kh, :], in_=pT[:], func=AF.Exp),
                             r=[hpT], w=[f"ET{k}_{g}{kh}"])
                P.op("dve", lambda e: e.tensor_tensor(out=es_t, in0=sink_bc, in1=nmx, op=ALU.add), r=["rows_sb", f"nmx{k}"], w=[f"es_t{k}"])
                P.op("act", lambda e: e.activation(out=es_t, in_=es_t, func=AF.Exp), r=[f"es_t{k}"], w=[f"es_t{k}"])
                for g in range(2):
                    po, hpo = mynps()
                    for i in range(4):
                        for kh in khs:
                            P.op("pe", lambda e, po=po, g=g, i=i, kh=kh: e.matmul(
                                po[:, i * 65:(i + 1) * 65], lhsT=ET[:, g * 2 + kh, i * 128:(i + 1) * 128],
                                rhs=vaug_l[:, s + kh, g * 65:(g + 1) * 65], start=(kh == khs[0]), stop=(kh == 1)),
                                r=[f"ET{k}_{g}{kh}", hva], w=[hpo])
                    P.op("dve", lambda e, po=po, g=g: e.tensor_tensor(
                        out=den.rearrange("p (i g) -> p i g", g=2)[:, :, g:g + 1],
                        in0=po[:, 0:260].rearrange("p (i d) -> p i d", d=65)[:, :, 64:65],
                        in1=es_t.rearrange("p (i g) -> p i g", g=2)[:, :, g:g + 1], op=ALU.add),
                        r=[hpo, f"es_t{k}"], w=[f"den{k}_{g}"])
                    P.op("act", lambda e, po=po, g=g: e.activation(
                        out=osb.rearrange("p (i g) d -> p i g d", g=2)[:, :, g, :],
                        in_=po[:, 0:260].rearrange("p (i d) -> p i d", d=65)[:, :, 0:64], func=AF.Identity),
                        r=[hpo], w=[f"osb{k}_{g}"])
                DH = [f"den{k}_0", f"den{k}_1"]
                OH = [f"osb{k}_0", f"osb{k}_1"]
                P.op("dve", lambda e: e.reciprocal(out=den, in_=den), r=DH, w=DH)
                P.op("dve", lambda e: e.tensor_tensor(out=osb, in0=osb, in1=den.unsqueeze(2).to_broadcast([128, 8, 64]), op=ALU.mult),
                     r=OH + DH, w=OH)
                P.op("act", lambda e: e.activation(out=osq, in_=osb, func=AF.Square), r=OH, w=[f"osq{k}"])
                P.op("dve", lambda e: e.tensor_reduce(out=ssq, in_=osq, axis=AX.X, op=ALU.add), r=[f"osq{k}"], w=[f"ssq{k}"])
                P.op("act", lambda e: e.activation(out=ssq, in_=ssq, func=AF.Sqrt, bias=EPSC, scale=1.0 / 64.0), r=[f"ssq{k}", "epsc"], w=[f"ssq{k}"])
                P.op("dve", lambda e: e.reciprocal(out=ssq, in_=ssq), r=[f"ssq{k}"], w=[f"ssq{k}"])
                P.op("dve", lambda e: e.tensor_tensor(out=osb, in0=osb, in1=ssq.unsqueeze(2).to_broadcast([128, 8, 64]), op=ALU.mult),
                     r=OH + [f"ssq{k}"], w=OH)
                P.op("dve", lambda e: e.tensor_tensor(out=yat, in0=osb.rearrange("p h d -> p (h d)"), in1=aog_bc, op=ALU.mult),
                     r=OH + ["rows_sb"], w=[f"yat{k}"])
                ptb = ps[3 + 2 * k][:, :].bitcast(BF16)[:, 0:512]
                hptr = f"ps{3 + 2 * k}"
                for i in range(4):
                    P.op("pe", lambda e, i=i: e.transpose(out=ptb[:, i * 128:(i + 1) * 128], in_=yat[:, i * 128:(i + 1) * 128],
                                                         identity=identb), r=[f"yat{k}", "identb"], w=[hptr])
                P.op("act", lambda e: e.activation(
                    out=ycT[:, 4:8, qs], in_=ptb[:, 0:512].rearrange("p (i q) -> p i q", i=4), func=AF.Identity),
                    r=[hptr], w=[f"ycTa{s}"])
                return P.end()

            def xr_load(s):
                k = s % 2
                tt = mt * 4 + s
                P.op("sp", lambda e: e.dma_start(out=xr2[k], in_=x_d[tt * 128:(tt + 1) * 128, :]), w=[f"xr{k}"], dma=True)

            def epi_chain(s, with_load):
                k = s % 2
                tt = mt * 4 + s
                xr, bnst, bnag = xr2[k], bnst2[k], bnag2[k]
                rr = xr
                mynps = nps_ep[k]
                YH = [f"ycTc{c}" for c in range(4)] + [f"ycTa{s}"]
                P.rec()
                if with_load:
                    xr_load(s)
                for h in range(2):
                    po, hpo = mynps()
                    for c in range(8):
                        P.op("pe", lambda e, po=po, c=c, h=h: e.matmul(
                            po[:], lhsT=ycT[:, c, s * 128:(s + 1) * 128], rhs=wout[:, c, h * 512:(h + 1) * 512],
                            start=(c == 0), stop=False), r=YH + WOUTH, w=[hpo])
                    P.op("pe", lambda e, po=po, h=h: e.matmul(
                        po[:], lhsT=onesb[0:1, :], rhs=gbrow[0:1, h * 512:(h + 1) * 512], start=False, stop=True),
                        r=["onesb", "gbrow"], w=[hpo])
                    P.op("dve", lambda e, po=po, h=h: e.tensor_tensor(out=rr[:, h * 512:(h + 1) * 512], in0=po[:],
                                                                      in1=xr[:, h * 512:(h + 1) * 512], op=ALU.add),
                         r=[hpo, f"xr{k}"], w=[f"xr{k}"])
                for h in range(2):
                    P.op("dve", lambda e, h=h: e.bn_stats(out=bnst[:, h * 6:(h + 1) * 6], in_=rr[:, h * 512:(h + 1) * 512]),
                         r=[f"xr{k}"], w=[f"bnst{k}"])
                P.op("dve", lambda e: e.bn_aggr(out=bnag[:, 0:2], in_=bnst), r=[f"bnst{k}"], w=[f"bnag{k}"])
                P.op("act", lambda e: e.activation(out=bnag[:, 2:3], in_=bnag[:, 1:2], func=AF.Sqrt, bias=EPSC2, scale=1.0),
                     r=[f"bnag{k}", "epsc2"], w=[f"bnagb{k}"])
                P.op("dve", lambda e: e.reciprocal(out=bnag[:, 2:3], in_=bnag[:, 2:3]), r=[f"bnagb{k}"], w=[f"bnagb{k}"])
                P.op("dve", lambda e: e.scalar_tensor_tensor(out=bnag[:, 3:4], in0=bnag[:, 0:1], scalar=-1.0, in1=bnag[:, 2:3],
                                                             op0=ALU.mult, op1=ALU.mult), r=[f"bnag{k}", f"bnagb{k}"], w=[f"bnagc{k}"])
                P.op("act", lambda e: e.activation(out=rr, in_=rr, func=AF.Identity, bias=bnag[:, 3:4], scale=bnag[:, 2:3]),
                     r=[f"xr{k}", f"bnagb{k}", f"bnagc{k}"], w=[f"xr{k}"])
                P.op("sp", lambda e: e.dma_start(out=z_d[tt * 128:(tt + 1) * 128, :], in_=rr), r=[f"xr{k}"], w=[f"z_d{tt}"], dma=True)
                return P.end()


            out = []
            P.rec()
            xr_load(0)
            xr_load(1)
            out += P.end()
            out += merge(convln_chain(0) + convln_chain(1), attn_chain(0), attn_chain(1))
            out += merge(convln_chain(2) + convln_chain(3), attn_chain(2), attn_chain(3))
            out += merge(epi_chain(0, False), epi_chain(1, False))
            out += merge(epi_chain(2, True), epi_chain(3, True))
            return out

        P.play(stage1(0))
        for mt in range(NMT):
            nxt = stage1(mt + 1) if mt + 1 < NMT else []
            P.play(merge(stage2(mt), nxt))

        P.fence()
        if dbg and dbg["what"] == "z":
            nr = _CACHE.get('nmt_run', NMT) * 512
            P.op("sp", lambda e: e.dma_start(out=dbg_d[0:nr, :], in_=z_d[0:nr, :]), r=[f"z_d{i}" for i in range(nr // 128)], w=["dbg"], dma=True)
            P.fence()
            P.emit()
            return nc

        try:
            build_phase2(nc, P, A, ps, nps, dr, out_d, z_d, xs_d, ys_d, dbg, dbg_d, mark_persist,
                     dict(cols=cols, identf=identf, identb=identb, onesb=onesb, Ub=Ub, modc=modc, mod_d=mod_d,
                              EPSC=EPSC, psb=psb))
        except _StopEmit:
            pass
        P.fence()
        P.emit()
    return nc


def build_phase2(nc, P, A, ps, nps, dr, out_d, z_d, xs_d, ys_d, dbg, dbg_d, mark, K):
    cols, identf, identb, onesb, Ub, mod_d, EPSC, psb = (K[k] for k in ("cols", "identf", "identb", "onesb", "Ub", "mod_d", "EPSC", "psb"))
    rows_d, wr_d, wg_d, wu_d, wd_d = dr["rows"], dr["w_r"], dr["wg"], dr["wu"], dr["wd"]
    A.off = mark
    gbc = A.alloc([4, D], F32)
    P.op("sp", lambda e: e.dma_start(out=gbc[:, 1:4, :].rearrange("p a d -> p (a d)"), in_=mod_d[0:1, :].partition_broadcast(128)),
         r=["mod_d"], w=["gbc1_0", "gbc1_1", "gbc2_0", "gbc2_1", "gbc3_0", "gbc3_1"], dma=True)
    lnr = A.alloc([4, D], F32)
    misc = A.alloc([36 + 64], F32)
    A2 = A.alloc([D], F32)
    B2 = A.alloc([D], F32)
    GA = A.alloc([D], F32)
    BA = A.alloc([D], F32)
    wr = A.alloc([8, 36], F32)
    pos_i = A.alloc([2, NT], I32)
    wts = A.alloc([2, NT], F32)
    widx_i = A.alloc([64], I32)
    mark2 = A.off
    br_bc = misc[:, 0:36]
    slot_bc = misc[:, 36:36 + NS]
    P.op("sp", lambda e: e.dma_start(out=lnr.rearrange("p a d -> p (a d)"), in_=rows_d[0:1, R_L1G:R_L1G + 4 * D].partition_broadcast(128)),
         w=["lnr"], dma=True)
    P.op("sp", lambda e: e.dma_start(out=misc, in_=rows_d[0:1, R_BR:R_BR + 100].partition_broadcast(128)), w=["misc"], dma=True)
    P.op("sp", lambda e: e.dma_start(out=wr, in_=wr_d.rearrange("(c p) n -> p c n", p=128)), w=["wr"], dma=True)
    G2H = ["gbc1_0", "gbc1_1", "gbc2_0", "gbc2_1", "gbc3_0", "gbc3_1"]
    P.op("dve", lambda e: e.scalar_tensor_tensor(out=A2, in0=gbc[:, 2, :], scalar=1.0, in1=lnr[:, 0, :], op0=ALU.add, op1=ALU.mult),
         r=["lnr"] + G2H, w=["A2"])
    P.op("dve", lambda e: e.scalar_tensor_tensor(out=B2, in0=gbc[:, 2, :], scalar=1.0, in1=lnr[:, 1, :], op0=ALU.add, op1=ALU.mult),
         r=["lnr"] + G2H, w=["B2"])
    P.op("dve", lambda e: e.tensor_tensor(out=B2, in0=B2, in1=gbc[:, 1, :], op=ALU.add), r=["B2"] + G2H, w=["B2"])
    P.op("dve", lambda e: e.tensor_scalar(out=GA, in0=lnr[:, 0, :], scalar1=ALPHA, scalar2=None, op0=ALU.mult), r=["lnr"], w=["GA"])
    P.op("dve", lambda e: e.tensor_scalar(out=BA, in0=lnr[:, 1, :], scalar1=ALPHA, scalar2=None, op0=ALU.mult), r=["lnr"], w=["BA"])

    def mk_nps(ids):
        st_ = [0]

        def f():
            i = ids[st_[0] % len(ids)]
            st_[0] += 1
            return ps[i], f"ps{i}"
        return f

    t1_d = nc.dram_tensor("t1_scr", [S, D], F32, kind="Internal").ap()
    h2b = A.alloc([NT, D], BF16)
    logits = A.alloc([NT, 36], F32)
    mark2a = A.off
    zt = [A.alloc([D], F32) for _ in range(2)]
    h2f = [A.alloc([D], F32) for _ in range(2)]
    h2T = [A.alloc([8, 128], F32) for _ in range(2)]
    t1s = [A.alloc([D], F32) for _ in range(2)]
    nps2a = [mk_nps([0, 1, 2]), mk_nps([3, 4, 5])]

    def chain2a(j):
        b = j % 2
        mynps = nps2a[b]
        z_, hz = zt[b], f"zt{b}"
        hf, hhf = h2f[b], f"h2f{b}"
        hT_, hhT = h2T[b], f"h2T{b}"
        t1_, ht1 = t1s[b], f"t1s{b}"
        P.rec()
        P.op("sp", lambda e: e.dma_start(out=z_, in_=z_d[j * 128:(j + 1) * 128, :]), r=[f"z_d{j}"], w=[hz], dma=True)
        P.op("dve", lambda e: e.tensor_tensor(out=hf, in0=z_, in1=A2, op=ALU.mult), r=[hz, "A2"], w=[hhf])
        P.op("dve", lambda e: e.tensor_tensor(out=hf, in0=hf, in1=B2, op=ALU.add), r=[hhf, "B2"], w=[hhf])
        P.op("pool", lambda e: e.tensor_tensor(out=t1_, in0=z_, in1=GA, op=ALU.mult), r=[hz, "GA"], w=[ht1])
        P.op("dve", lambda e: e.tensor_tensor(out=t1_, in0=t1_, in1=BA, op=ALU.add), r=[ht1, "BA"], w=[ht1])
        P.op("sp", lambda e: e.dma_start(out=t1_d[j * 128:(j + 1) * 128, :], in_=t1_), r=[ht1], w=[f"t1_d{j}"], dma=True)
        P.op("act", lambda e: e.activation(out=h2b[:, j, :], in_=hf, func=AF.Identity), r=[hhf], w=[f"h2b{j}"])
        for hh in range(2):
            pt, hp = mynps()
            for c4 in range(4):
                c = hh * 4 + c4
                P.op("pe", lambda e, pt=pt, c=c, c4=c4: e.transpose(out=pt[:, c4 * 128:(c4 + 1) * 128],
                                                                  in_=hf[:, c * 128:(c + 1) * 128], identity=identf),
                     r=[hhf, "cst"], w=[hp])
            if hh == 0:
                P.op("act", lambda e, pt=pt: e.activation(out=hT_[:, 0:4, :].rearrange("p c t -> p (c t)"),
                                                          in_=pt[:], func=AF.Identity), r=[hp], w=[hhT + "a"])
            else:
                P.op("act", lambda e, pt=pt: e.activation(out=hT_[:, 4:8, :].rearrange("p c t -> p (c t)"),
                                                          in_=pt[:], func=AF.Identity), r=[hp], w=[hhT + "b"])
        pl, hpl = mynps()
        for c in range(8):
            P.op("pe", lambda e, pl=pl, c=c: e.matmul(pl[:, 0:36], lhsT=hT_[:, c, :], rhs=wr[:, c, :], start=(c == 0), stop=(c == 7)),
                 r=[hhT + "a", hhT + "b", "wr"], w=[hpl])
        P.op("dve", lambda e, pl=pl: e.tensor_tensor(out=logits[:, j, :], in0=pl[:, 0:36], in1=br_bc, op=ALU.add),
             r=[hpl, "misc"], w=["logits"])
        return P.end()

    for j in range(0, NT, 2):
        P.play(merge(chain2a(j), chain2a(j + 1)))
    P.fence()
    A.off = mark2a

    def T3(n):
        return A.alloc([NT, n], F32)
    gmax = A.alloc([NT], F32)
    og = T3(4)
    eg = T3(4)
    sgm = A.alloc([NT], F32)
    ptop = A.alloc([NT], F32)
    tmp4 = A.alloc([NT, 4, 8], F32)
    sel = T3(8)
    sel2 = T3(8)
    m1 = A.alloc([NT], F32)
    m2 = A.alloc([NT], F32)
    o1 = T3(8)
    o2 = T3(8)
    e2 = A.alloc([NT], F32)
    r12 = A.alloc([NT], F32)
    O1 = A.alloc([NT, 4, 8], F32)
    O2 = A.alloc([NT, 4, 8], F32)
    Obf = A.alloc([NT * 32], BF16)
    totA = A.alloc([NT, 32], F32)
    totB = A.alloc([NT, 32], F32)
    tot0 = A.alloc([NT, 32], F32)
    base = A.alloc([NT, 32], F32)
    cnt = A.alloc([32], F32)
    cmpc = A.alloc([32, 24], F32)
    pcnt = A.alloc([32], F32)
    oeA = A.alloc([32], F32)
    oeB = A.alloc([32], F32)
    offs = A.alloc([32], F32)
    cmps = A.alloc([NS, 32], F32)
    esl = A.alloc([64], F32)
    used = A.alloc([64], F32)
    posf = A.alloc([2, NT], F32)

    LG = logits[:, :, 0:4]
    LE4 = logits[:, :, 4:36].rearrange("p j (g e) -> p j g e", g=4)

    def dv(fn, r, w):
        P.op("dve", fn, r=r, w=w)

    dv(lambda e: e.tensor_reduce(out=gmax, in_=LG, axis=AX.X, op=ALU.max), ["logits"], ["gmax"])
    dv(lambda e: e.tensor_tensor(out=og, in0=LG, in1=gmax.unsqueeze(2).to_broadcast([128, NT, 4]), op=ALU.is_equal), ["logits", "gmax"], ["og"])
    dv(lambda e: e.tensor_tensor(out=eg, in0=LG, in1=gmax.unsqueeze(2).to_broadcast([128, NT, 4]), op=ALU.subtract), ["logits", "gmax"], ["eg"])
    P.op("act", lambda e: e.activation(out=eg, in_=eg, func=AF.Exp), r=["eg"], w=["eg"])
    dv(lambda e: e.tensor_reduce(out=sgm, in_=eg, axis=AX.X, op=ALU.add), ["eg"], ["sgm"])
    dv(lambda e: e.reciprocal(out=ptop, in_=sgm), ["sgm"], ["ptop"])
    dv(lambda e: e.tensor_tensor(out=tmp4, in0=LE4, in1=og.unsqueeze(3).to_broadcast([128, NT, 4, 8]), op=ALU.mult), ["logits", "og"], ["tmp4"])
    dv(lambda e: e.tensor_reduce(out=sel, in_=tmp4.rearrange("p j g e -> p j e g"), axis=AX.X, op=ALU.add), ["tmp4"], ["sel"])
    dv(lambda e: e.tensor_reduce(out=m1, in_=sel, axis=AX.X, op=ALU.max), ["sel"], ["m1"])
    dv(lambda e: e.tensor_tensor(out=o1, in0=sel, in1=m1.unsqueeze(2).to_broadcast([128, NT, 8]), op=ALU.is_equal), ["sel", "m1"], ["o1"])
    dv(lambda e: e.scalar_tensor_tensor(out=sel2.rearrange("p j e -> p (j e)"), in0=o1.rearrange("p j e -> p (j e)"), scalar=-1.0e9,
                                        in1=sel.rearrange("p j e -> p (j e)"), op0=ALU.mult, op1=ALU.add), ["o1", "sel"], ["sel2"])
    dv(lambda e: e.tensor_reduce(out=m2, in_=sel2, axis=AX.X, op=ALU.max), ["sel2"], ["m2"])
    dv(lambda e: e.tensor_tensor(out=o2, in0=sel2, in1=m2.unsqueeze(2).to_broadcast([128, NT, 8]), op=ALU.is_equal), ["sel2", "m2"], ["o2"])
    dv(lambda e: e.tensor_tensor(out=e2, in0=m2, in1=m1, op=ALU.subtract), ["m1", "m2"], ["e2"])
    P.op("act", lambda e: e.activation(out=e2, in_=e2, func=AF.Exp), r=["e2"], w=["e2"])
    dv(lambda e: e.tensor_scalar(out=r12, in0=e2, scalar1=1.0, scalar2=None, op0=ALU.add), ["e2"], ["r12"])
    dv(lambda e: e.reciprocal(out=r12, in_=r12), ["r12"], ["r12"])
    dv(lambda e: e.tensor_tensor(out=wts[:, 0, :], in0=r12, in1=ptop, op=ALU.mult), ["r12", "ptop"], ["wts"])
    dv(lambda e: e.tensor_tensor(out=wts[:, 1, :], in0=wts[:, 0, :], in1=e2, op=ALU.mult), ["wts", "e2"], ["wts"])
    dv(lambda e: e.tensor_tensor(out=O1, in0=og.unsqueeze(3).to_broadcast([128, NT, 4, 8]),
                                 in1=o1.unsqueeze(2).to_broadcast([128, NT, 4, 8]), op=ALU.mult), ["og", "o1"], ["O1"])
    dv(lambda e: e.tensor_tensor(out=O2, in0=og.unsqueeze(3).to_broadcast([128, NT, 4, 8]),
                                 in1=o2.unsqueeze(2).to_broadcast([128, NT, 4, 8]), op=ALU.mult), ["og", "o2"], ["O2"])
    O1f = O1.rearrange("p j g e -> p (j g e)")
    O2f = O2.rearrange("p j g e -> p (j g e)")
    dv(lambda e: e.tensor_tensor(out=Obf, in0=O1f, in1=O2f, op=ALU.add), ["O1", "O2"], ["Obf"])
    pcs, pts = [], []
    for h in range(2):
        pc_, hpc = nps()
        P.op("pe", lambda e, pc_=pc_, h=h: e.matmul(pc_[:], lhsT=Ub, rhs=Obf[:, h * 512:(h + 1) * 512], start=True, stop=True),
             r=["Ub", "Obf"], w=[hpc])
        pcs.append((pc_, hpc))
        pt_, hpt = nps()
        P.op("pe", lambda e, pt_=pt_, h=h: e.matmul(pt_[:], lhsT=onesb, rhs=Obf[:, h * 512:(h + 1) * 512], start=True, stop=True),
             r=["onesb", "Obf"], w=[hpt])
        pts.append((pt_, hpt))
    tot0f = tot0.rearrange("p j e -> p (j e)")
    for h in range(2):
        dv(lambda e, h=h: e.tensor_copy(out=tot0f[:, h * 512:(h + 1) * 512], in_=pts[h][0][:]), [pts[h][1]], ["tot0"])
    cur, hc = tot0, "tot0"
    for i_, sft in enumerate((1, 2, 4, 8, 16)):
        nxt, hn = (totA, "totA") if i_ % 2 == 0 else (totB, "totB")
        dv(lambda e, cur=cur, nxt=nxt, sft=sft: e.tensor_tensor(out=nxt[:, sft:, :], in0=cur[:, sft:, :], in1=cur[:, :NT - sft, :], op=ALU.add),
           [hc], [hn])
        dv(lambda e, cur=cur, nxt=nxt, sft=sft: e.tensor_copy(out=nxt[:, :sft, :], in_=cur[:, :sft, :]), [hc, hn], [hn])
        cur, hc = nxt, hn
    incl, hincl = cur, hc
    dv(lambda e: e.tensor_copy(out=cnt, in_=incl[:, NT - 1, :]), [hincl], ["cnt"])
    dv(lambda e: e.tensor_tensor(out=cmpc, in0=cnt.unsqueeze(2).to_broadcast([128, 32, 24]),
                                 in1=slot_bc[:, 0:24].unsqueeze(1).to_broadcast([128, 32, 24]), op=ALU.is_gt), ["cnt", "misc"], ["cmpc"])
    dv(lambda e: e.tensor_reduce(out=pcnt, in_=cmpc, axis=AX.X, op=ALU.add), ["cmpc"], ["pcnt"])
    dv(lambda e: e.tensor_scalar(out=pcnt, in0=pcnt, scalar1=float(T), scalar2=None, op0=ALU.mult), ["pcnt"], ["pcnt"])
    cur, hc = pcnt, "pcnt"
    for i_, sft in enumerate((1, 2, 4, 8, 16)):
        nxt, hn = (oeA, "oeA") if i_ % 2 == 0 else (oeB, "oeB")
        dv(lambda e, cur=cur, nxt=nxt, sft=sft: e.tensor_tensor(out=nxt[:, sft:], in0=cur[:, sft:], in1=cur[:, :32 - sft], op=ALU.add), [hc], [hn])
        dv(lambda e, cur=cur, nxt=nxt, sft=sft: e.tensor_copy(out=nxt[:, :sft], in_=cur[:, :sft]), [hc, hn], [hn])
        cur, hc = nxt, hn
    oend, hoend = cur, hc
    dv(lambda e: e.tensor_tensor(out=offs, in0=oend, in1=pcnt, op=ALU.subtract), [hoend, "pcnt"], ["offs"])
    dv(lambda e: e.tensor_tensor(out=base, in0=incl, in1=tot0, op=ALU.subtract), [hincl, "tot0"], ["base"])
    dv(lambda e: e.tensor_tensor(out=base, in0=base, in1=offs.unsqueeze(1).to_broadcast([128, NT, 32]), op=ALU.add), ["base", "offs"], ["base"])
    basef = base.rearrange("p j e -> p (j e)")
    for h in range(2):
        dv(lambda e, h=h: e.tensor_tensor(out=basef[:, h * 512:(h + 1) * 512], in0=pcs[h][0][:], in1=basef[:, h * 512:(h + 1) * 512], op=ALU.add),
           [pcs[h][1], "base"], ["base"])
    for k, (Ok, hO) in enumerate(((O1, "O1"), (O2, "O2"))):
        dv(lambda e, Ok=Ok: e.tensor_tensor(out=Ok.rearrange("p j g e -> p (j g e)"), in0=Ok.rearrange("p j g e -> p (j g e)"), in1=basef, op=ALU.mult),
           [hO, "base"], [hO])
        dv(lambda e, Ok=Ok, k=k: e.tensor_reduce(out=posf[:, k, :], in_=Ok.rearrange("p j g e -> p j (g e)"), axis=AX.X, op=ALU.add), [hO], ["posf"])
    dv(lambda e: e.tensor_copy(out=pos_i, in_=posf), ["posf"], ["pos_i"])
    dv(lambda e: e.tensor_tensor(out=cmps, in0=oend.unsqueeze(1).to_broadcast([128, NS, 32]),
                                 in1=slot_bc.unsqueeze(2).to_broadcast([128, NS, 32]), op=ALU.is_le), [hoend, "misc"], ["cmps"])
    dv(lambda e: e.tensor_reduce(out=esl[:, 0:NS], in_=cmps, axis=AX.X, op=ALU.add), ["cmps"], ["esl"])
    dv(lambda e: e.tensor_scalar(out=esl[:, 0:NS], in0=esl[:, 0:NS], scalar1=float(NE - 1), scalar2=128.0, op0=ALU.min, op1=ALU.mult), ["esl"], ["esl"])
    dv(lambda e: e.tensor_scalar(out=used[:, 0:NS], in0=slot_bc, scalar1=oend[:, 31:32], scalar2=None, op0=ALU.is_lt), ["misc", hoend], ["used"])
    dv(lambda e: e.tensor_scalar(out=used[:, 0:NS], in0=used[:, 0:NS], scalar1=-1.0e6, scalar2=1.0e6, op0=ALU.mult, op1=ALU.add), ["used"], ["used"])
    dv(lambda e: e.tensor_tensor(out=esl[:, 0:NS], in0=esl[:, 0:NS], in1=used[:, 0:NS], op=ALU.add), ["esl", "used"], ["esl"])
    dv(lambda e: e.tensor_scalar(out=esl[:, 0:NS], in0=esl[:, 0:NS], scalar1=cols[:, C_PID:C_PID + 1], scalar2=None, op0=ALU.add), ["esl", "cols"], ["esl"])
    dv(lambda e: e.tensor_copy(out=widx_i[:, 0:NS], in_=esl[:, 0:NS]), ["esl"], ["widx_i"])

    if dbg and dbg["what"] == "route":
        P.fence()
        P.op("sp", lambda e: e.dma_start(out=dbg_d[0:128, 0:NT * 36], in_=logits.rearrange("p j n -> p (j n)")), r=["logits"], w=["dbg0"], dma=True)
        P.op("sp", lambda e: e.dma_start(out=dbg_d[128:256, 0:2 * NT], in_=posf.rearrange("p k j -> p (k j)")), r=["posf"], w=["dbg1"], dma=True)
        P.op("sp", lambda e: e.dma_start(out=dbg_d[256:384, 0:2 * NT], in_=wts.rearrange("p k j -> p (k j)")), r=["wts"], w=["dbg2"], dma=True)
        P.op("sp", lambda e: e.dma_start(out=dbg_d[384:512, 0:NS], in_=esl[:, 0:NS]), r=["esl"], w=["dbg3"], dma=True)
        P.fence()
        raise _StopEmit()

    for j in range(NT):
        for k in range(2):
            P.op("pool", lambda e, j=j, k=k: e.indirect_dma_start(
                out=xs_d[:, :], out_offset=bass.IndirectOffsetOnAxis(ap=pos_i[:, k, j:j + 1], axis=0),
                in_=h2b[:, j, :], in_offset=None), r=[f"h2b{j}", "pos_i"], w=[f"xs_{j}_{k}"], dma=True)
    P.fence()

    A.off = mark2
    NBW = 4
    wgs = [A.alloc([2048], BF16) for _ in range(NBW)]
    wus = [A.alloc([2048], BF16) for _ in range(NBW)]
    wds = [A.alloc([2048], BF16) for _ in range(NBW)]
    xtok = [A.alloc([NSUB, D], BF16) for _ in range(NBW)]
    XT = [A.alloc([8, T], BF16) for _ in range(2)]
    sgs = [A.alloc([T], F32) for _ in range(2)]
    aT = [A.alloc([2, T], BF16) for _ in range(2)]
    NYO = 3
    yo = [A.alloc([D], F32) for _ in range(NYO)]
    npsB = mk_nps([0, 1, 2])
    npsC = mk_nps([3, 4, 5])
    _bc = {}

    def get_bc(e):
        if "v" not in _bc:
            reg = e.alloc_register("bcreg")
            e.reg_mov(reg, NE * 128 - 1)
            _bc["v"] = e.snap(reg, donate=True)
        return _bc["v"]

    def load_w(s, which):
        bw = s % NBW
        for (wsb, wdr, hn) in which(bw):
            P.op("pool", lambda e, wsb=wsb, wdr=wdr, s=s: e.indirect_dma_start(
                out=wsb, out_offset=None, in_=wdr[:, :],
                in_offset=bass.IndirectOffsetOnAxis(ap=widx_i[:, s:s + 1], axis=0),
                bounds_check=get_bc(e), oob_is_err=False), r=["widx_i"], w=[hn], dma=True)

    def w_gu(bw):
        return ((wgs[bw], wg_d, f"wg{bw}"), (wus[bw], wu_d, f"wu{bw}"))

    def w_d(bw):
        return ((wds[bw], wd_d, f"wd{bw}"),)

    def load_x(s):
        bw = s % NBW
        for st in range(NSUB):
            r0 = s * T + st * 128
            P.op("sp", lambda e, bw=bw, st=st, r0=r0: e.dma_start(out=xtok[bw][:, st, :], in_=xs_d[r0:r0 + 128, :]),
                 w=[f"xtok{bw}_{st}"], dma=True)

    def stageA(s):
        b, bw = s % 2, s % NBW
        P.rec()
        for st in range(NSUB):
            k = (s * NSUB + st) % 2
            pb_, hpb = psb[k], f"psb{k}"
            for c in range(8):
                P.op("pe", lambda e, pb_=pb_, st=st, c=c: e.transpose(out=pb_[:, c * 128:(c + 1) * 128],
                                                                     in_=xtok[bw][:, st, c * 128:(c + 1) * 128], identity=identb),
                     r=[f"xtok{bw}_{st}", "identb"], w=[hpb])
            if True:
                P.op("act", lambda e, pb_=pb_, st=st: e.activation(out=XT[b][:, :, st * 128:(st + 1) * 128],
                                                                   in_=pb_[:].rearrange("p (c t) -> p c t", c=8), func=AF.Identity),
                     r=[hpb], w=[f"XT{b}_{st}"])
            else:
                P.op("dve", lambda e, pb_=pb_, st=st: e.tensor_copy(out=XT[b][:, :, st * 128:(st + 1) * 128],
                                                                    in_=pb_[:].rearrange("p (c t) -> p c t", c=8)),
                     r=[hpb], w=[f"XT{b}_{st}"])
        return P.end()

    def stageB(s):
        b, bw = s % 2, s % NBW
        XH = [f"XT{b}_{st}" for st in range(NSUB)]
        P.rec()
        for fch in range(2):
            pg, hpg = npsB()
            for c in range(8):
                P.op("pe", lambda e, pg=pg, c=c, fch=fch: e.matmul(
                    pg[:, 0:T], lhsT=wgs[bw][:, c * 256 + fch * 128:c * 256 + (fch + 1) * 128], rhs=XT[b][:, c, :],
                    start=(c == 0), stop=(c == 7)), r=[f"wg{bw}"] + XH, w=[hpg])
            P.op("act", lambda e, pg=pg: e.activation(out=sgs[b], in_=pg[:, 0:T], func=AF.Silu), r=[hpg], w=[f"sgs{b}"])
            pu, hpu = npsB()
            for c in range(8):
                P.op("pe", lambda e, pu=pu, c=c, fch=fch: e.matmul(
                    pu[:, 0:T], lhsT=wus[bw][:, c * 256 + fch * 128:c * 256 + (fch + 1) * 128], rhs=XT[b][:, c, :],
                    start=(c == 0), stop=(c == 7)), r=[f"wu{bw}"] + XH, w=[hpu])
            P.op("dve", lambda e, pu=pu, fch=fch: e.tensor_tensor(out=aT[b][:, fch, :], in0=pu[:, 0:T], in1=sgs[b], op=ALU.mult),
                 r=[hpu, f"sgs{b}"], w=[f"aT{b}_{fch}"])
        return P.end()

    def stageC(s):
        b, bw = s % 2, s % NBW
        P.rec()
        for st in range(NSUB):
            yb = (s * NSUB + st) % NYO
            for half in range(2):
                po, hpo = npsC()
                for fch in range(2):
                    P.op("pe", lambda e, po=po, st=st, fch=fch, half=half: e.matmul(
                        po[:], lhsT=aT[b][:, fch, st * 128:(st + 1) * 128],
                        rhs=wds[bw][:, fch * 1024 + half * 512:fch * 1024 + (half + 1) * 512], start=(fch == 0), stop=(fch == 1)),
                        r=[f"aT{b}_0", f"aT{b}_1", f"wd{bw}"], w=[hpo])
                P.op("dve", lambda e, po=po, yb=yb, half=half: e.tensor_tensor(
                    out=yo[yb][:, half * 512:(half + 1) * 512], in0=po[:], in1=gbc[:, 3, half * 512:(half + 1) * 512], op=ALU.mult),
                    r=[hpo, "gbc3_0", "gbc3_1"], w=[f"yo{yb}{'ab'[half]}"])
            r0 = s * T + st * 128
            P.op("sp", lambda e, yb=yb, r0=r0: e.dma_start(out=ys_d[r0:r0 + 128, :], in_=yo[yb]), r=[f"yo{yb}a", f"yo{yb}b"],
                 w=[f"ys_{s}_{st}"], dma=True)
        return P.end()

    for s in range(min(NBW, NS)):
        load_x(s)
        load_w(s, w_gu)
        load_w(s, w_d)
    for i in range(NS + 2):
        lists = []
        if i < NS:
            lists.append(stageA(i))
        if 0 <= i - 1 < NS:
            lists.append(stageB(i - 1))
        if 0 <= i - 2 < NS:
            lists.append(stageC(i - 2))
        P.play(merge(*lists))
        if i + NBW < NS:
            load_x(i + NBW)
        if i - 1 >= 0 and i - 1 + NBW < NS:
            load_w(i - 1 + NBW, w_gu)
        if i - 2 >= 0 and i - 2 + NBW < NS:
            load_w(i - 2 + NBW, w_d)
    P.fence()

    A.off = mark2
    NB2F = 4
    Y1 = [A.alloc([D], F32) for _ in range(NB2F)]
    Y2 = [A.alloc([D], F32) for _ in range(NB2F)]
    t1 = [A.alloc([D], F32) for _ in range(NB2F)]
    ff = [A.alloc([D], F32) for _ in range(2)]
    r2 = [A.alloc([D], F32) for _ in range(2)]
    qq = [A.alloc([D], F32) for _ in range(2)]
    ob = [A.alloc([D], F32) for _ in range(2)]
    bn2 = [A.alloc([12], F32) for _ in range(2)]
    ag2 = [A.alloc([4], F32) for _ in range(2)]

    def loads2f(j):
        q = j % NB2F
        P.op("pool", lambda e: e.indirect_dma_start(
            out=Y1[q], out_offset=None, in_=ys_d[:, :], in_offset=bass.IndirectOffsetOnAxis(ap=pos_i[:, 0, j:j + 1], axis=0)),
            r=["pos_i"], w=[f"Y1{q}"], dma=True)
        P.op("pool", lambda e: e.indirect_dma_start(
            out=Y2[q], out_offset=None, in_=ys_d[:, :], in_offset=bass.IndirectOffsetOnAxis(ap=pos_i[:, 1, j:j + 1], axis=0)),
            r=["pos_i"], w=[f"Y2{q}"], dma=True)
        P.op("sp", lambda e: e.dma_start(out=t1[q], in_=t1_d[j * 128:(j + 1) * 128, :]), r=[f"t1_d{j}"], w=[f"t1{q}"], dma=True)

    def chain2f(j):
        b = j % 2
        q = j % NB2F
        P.rec()
        P.op("act", lambda e: e.activation(out=ff[b], in_=Y1[q], func=AF.Identity, scale=wts[:, 0, j:j + 1]), r=[f"Y1{q}", "wts"], w=[f"ff{b}"])
        P.op("dve", lambda e: e.scalar_tensor_tensor(out=ff[b], in0=Y2[q], scalar=wts[:, 1, j:j + 1], in1=ff[b], op0=ALU.mult, op1=ALU.add),
             r=[f"Y2{q}", "wts", f"ff{b}"], w=[f"ff{b}"])
        P.op("dve", lambda e: e.tensor_tensor(out=r2[b], in0=ff[b], in1=t1[q], op=ALU.add), r=[f"ff{b}", f"t1{q}"], w=[f"r2{b}"])
        for h in range(2):
            P.op("dve", lambda e, h=h: e.bn_stats(out=bn2[b][:, h * 6:(h + 1) * 6], in_=r2[b][:, h * 512:(h + 1) * 512]), r=[f"r2{b}"], w=[f"bn2{b}"])
        P.op("dve", lambda e: e.bn_aggr(out=ag2[b][:, 0:2], in_=bn2[b]), r=[f"bn2{b}"], w=[f"ag2{b}"])
        P.op("act", lambda e: e.activation(out=ag2[b][:, 2:3], in_=ag2[b][:, 1:2], func=AF.Sqrt, bias=EPSC, scale=1.0), r=[f"ag2{b}", "epsc"], w=[f"ag2b{b}"])
        P.op("dve", lambda e: e.reciprocal(out=ag2[b][:, 2:3], in_=ag2[b][:, 2:3]), r=[f"ag2b{b}"], w=[f"ag2b{b}"])
        P.op("dve", lambda e: e.scalar_tensor_tensor(out=ag2[b][:, 3:4], in0=ag2[b][:, 0:1], scalar=-1.0, in1=ag2[b][:, 2:3], op0=ALU.mult, op1=ALU.mult),
             r=[f"ag2{b}", f"ag2b{b}"], w=[f"ag2c{b}"])
        P.op("act", lambda e: e.activation(out=qq[b], in_=r2[b], func=AF.Identity, bias=ag2[b][:, 3:4], scale=ag2[b][:, 2:3]),
             r=[f"r2{b}", f"ag2b{b}", f"ag2c{b}"], w=[f"qq{b}"])
        P.op("pool", lambda e: e.tensor_tensor(out=qq[b], in0=qq[b], in1=lnr[:, 2, :], op=ALU.mult), r=[f"qq{b}", "lnr"], w=[f"qq{b}"])
        P.op("dve", lambda e: e.tensor_tensor(out=ob[b], in0=qq[b], in1=lnr[:, 3, :], op=ALU.add), r=[f"qq{b}", "lnr"], w=[f"ob{b}"])
        P.op("sp", lambda e: e.dma_start(out=out_d[j * 128:(j + 1) * 128, :], in_=ob[b]), r=[f"ob{b}"], w=[f"out{j}"], dma=True)
        return P.end()

    loads2f(0)
    loads2f(1)
    for j in range(0, NT, 2):
        if j + 2 < NT:
            loads2f(j + 2)
            loads2f(j + 3)
        P.play(merge(chain2f(j), chain2f(j + 1)))


class _StopEmit(Exception):
    pass


def _host_prep(inp, b):
    f = np.float32
    L = 0
    cols = np.zeros((128, NCOL), f)
    cols[:, C_C:C_C + 8] = inp["c"][b].reshape(8, 128).T
    w_in = inp["w_in"][L]
    b_in = inp["b_in"][L]
    qcols = np.concatenate([np.arange(1024 + h * 64, 1024 + (h + 1) * 64) for h in HORDER])
    perm = np.concatenate([np.arange(0, 1024), qcols, np.arange(1536, 1792)])
    w_in_p = np.ascontiguousarray(w_in[:, perm])
    b_in_p = b_in[perm]
    cols[:, C_BIN:C_BIN + 13] = b_in_p[:1664].reshape(13, 128).T
    cols[:, C_CW:C_CW + 124] = inp["conv_w"][L].T.reshape(4, 128, 31).transpose(1, 0, 2).reshape(128, 124)
    cols[:, C_CB:C_CB + 4] = inp["conv_b"][L].reshape(4, 128).T
    cols[:, C_LG:C_LG + 4] = inp["conv_ln_g"][L].reshape(4, 128).T
    cols[:, C_LB:C_LB + 4] = inp["conv_ln_b"][L].reshape(4, 128).T
    cols[:, C_OG:C_OG + 4] = inp["conv_out_g"][L].reshape(4, 128).T
    cols[:, C_PID] = np.arange(128)
    rows = np.zeros((1, NROW), f)
    rows[0, R_BADA:R_BADA + 6144] = inp["b_ada"][L]
    rows[0, R_BV:R_BV + 128] = b_in[1664:1792]
    rows[0, R_SINK:R_SINK + 8] = inp["sinks"][L][HORDER]
    rows[0, R_AOG:R_AOG + 512] = inp["attn_out_g"][L].reshape(8, 64)[HORDER].reshape(-1)
    rows[0, R_BOUT:R_BOUT + D] = inp["b_out"][L]
    rows[0, R_L1G:R_L1G + D] = inp["ln1_g"][L]
    rows[0, R_L1B:R_L1B + D] = inp["ln1_b"][L]
    rows[0, R_L2G:R_L2G + D] = inp["ln2_g"][L]
    rows[0, R_L2B:R_L2B + D] = inp["ln2_b"][L]
    rows[0, R_BR:R_BR + 4] = inp["b_router_group"][L]
    rows[0, R_BR + 4:R_BR + 36] = inp["b_router_expert"][L]
    rows[0, R_SLOT:R_SLOT + NS] = np.arange(NS) * T
    w_out = inp["w_out"][L]
    arows = np.concatenate([np.arange(512 + h * 64, 512 + (h + 1) * 64) for h in HORDER])
    w_out_p = np.ascontiguousarray(np.concatenate([w_out[:512], w_out[arows]], axis=0))
    w_r = np.ascontiguousarray(np.concatenate([inp["w_router_group"][L], inp["w_router_expert"][L]], axis=1))
    return dict(cols=cols, rows=rows, w_in=w_in_p, w_out=w_out_p, w_r=w_r)


def _consts():
    f = np.float32
    cst = np.zeros((128, NK), f)
    cst[:, K_ID:K_ID + 128] = np.eye(128)
    p = np.arange(128)
    cst[:, K_U:K_U + 128] = (p[:, None] < p[None, :])
    cst[:, K_BD:K_BD + 128] = ((p[:, None] // 64) == (p[None, :] // 64)) / 64.0
    m0 = np.where(p[:, None] > p[None, :], 0.0, NEG)
    m1 = np.where(p[:, None] <= p[None, :], 0.0, NEG)
    cst[:, K_M0:K_M0 + 512] = np.tile(m0, (1, 4))
    cst[:, K_M1:K_M1 + 512] = np.tile(m1, (1, 4))
    return cst


def _expert_layout(inp):
    L = 0
    wg = np.ascontiguousarray(inp["w_gate"][L].reshape(NE, 8, 128, DE).transpose(0, 2, 1, 3)).reshape(NE * 128, 2048)
    wu = np.ascontiguousarray(inp["w_up"][L].reshape(NE, 8, 128, DE).transpose(0, 2, 1, 3)).reshape(NE * 128, 2048)
    wd = np.ascontiguousarray(inp["w_down"][L].reshape(NE, 2, 128, D).transpose(0, 2, 1, 3)).reshape(NE * 128, 2048)
    return wg, wu, wd


_CACHE = {}


def kernel(**inputs):
    inp = {k: np.asarray(v) for k, v in inputs.items()}
    dbg = _CACHE.get("dbg")
    nc = build_program(dbg)
    cst = _consts()
    wg, wu, wd = _expert_layout(inp)
    w_ada = np.ascontiguousarray(inp["w_ada"][0])
    in_maps = []
    ncores = _CACHE.get("ncores", 8)
    for b in range(ncores):
        hp = _host_prep(inp, b)
        m = dict(x=np.ascontiguousarray(inp["x"][b]), cols=hp["cols"], rows=hp["rows"], cst=cst, w_ada=w_ada,
                 w_in=hp["w_in"], w_out=hp["w_out"], w_r=hp["w_r"], wg=wg, wu=wu, wd=wd)
        if _CACHE.get("p2only"):
            m["z_in"] = _CACHE["z_in"]
        in_maps.append(m)
    if _CACHE.get("trace"):
        res = run_bass_kernel_spmd(nc, in_maps, core_ids=list(range(ncores)), trace=True)
        print("EXEC_NS", res.exec_time_ns)
    else:
        res = run_bass_kernel_spmd(nc, in_maps, core_ids=list(range(ncores)))
    if dbg:
        return [np.asarray(r["dbg"]) for r in res.results]
    out = np.stack([np.asarray(r["out"]) for r in res.results], axis=0).astype(np.float32)
    return out
```

```python
import os
import numpy as np
import concourse.bass as bass
import concourse.mybir as mybir
from concourse.bass_utils import run_bass_kernel_spmd

F32 = mybir.dt.float32
BF16 = mybir.dt.bfloat16
I32 = mybir.dt.int32
ALU = mybir.AluOpType
AF = mybir.ActivationFunctionType
AX = mybir.AxisListType

D = 1024
S = 4096
NT = S // 128
NMT = S // 512
DIN = 1792
NE = 32
DE = 256
ALPHA = 2.0 ** 0.25
EPS = 1e-5
NEG = -30000.0
T = 384
NS = (2 * S + NE * (T - 1) + T - 1) // T
NSUB = T // 128
HORDER = [0, 4, 1, 5, 2, 6, 3, 7]

C_C = 0
C_BIN = 8
C_CW = 21
C_CB = 145
C_LG = 149
C_LB = 153
C_OG = 157
C_PID = 161
NCOL = 162
R_BADA = 0
R_BV = 6144
R_SINK = 6272
R_AOG = 6280
R_BOUT = 6792
R_L1G = 7816
R_L1B = 8840
R_L2G = 9864
R_L2B = 10888
R_BR = 11912
R_SLOT = 11948
NROW = 12012
K_ID = 0
K_U = 128
K_BD = 256
K_M0 = 384
K_M1 = 896
NK = 1408


class Prog:
    def __init__(self, nc, sems):
        self.nc = nc
        self.ops = []
        self.last_w = {}
        self.readers = {}
        self.eng_sem = {e: sems[i] for i, e in enumerate(["pe", "act", "dve", "pool"])}
        rest = sems[4:]
        n_sp = (len(rest) * 5) // 10
        n_pool = (len(rest) * 4) // 10
        self.dma_pool = {"sp": rest[:n_sp], "pool": rest[n_sp:n_sp + n_pool], "act": rest[n_sp + n_pool:]}
        self._rec = None

    def rec(self):
        assert self._rec is None
        self._rec = []

    def end(self):
        l = self._rec
        self._rec = None
        return l

    def play(self, lst):
        assert self._rec is None
        for o in lst:
            self.op(*o)

    def op(self, eng, fn, r=(), w=(), dma=False):
        if self._rec is not None:
            self._rec.append((eng, fn, list(r), list(w), dma))
            return None
        i = len(self.ops)
        w = list(w) + [h for h in r if h.startswith("ps") and h not in w]
        raw, oth = set(), set()
        for h in r:
            if h in self.last_w:
                raw.add(self.last_w[h])
        for h in w:
            if h in self.last_w:
                oth.add(self.last_w[h])
            for j in self.readers.get(h, ()):
                oth.add(j)
        for h in w:
            self.last_w[h] = i
            self.readers[h] = []
        for h in r:
            self.readers.setdefault(h, []).append(i)
        deps = []
        for j in sorted(raw | oth):
            p = self.ops[j]
            if j == i:
                continue
            if (not p["dma"]) and p["eng"] == eng:
                if eng == "pe" or j not in raw:
                    continue
            deps.append(j)
        self.ops.append(dict(eng=eng, fn=fn, deps=deps, dma=dma, sig=False))
        for j in deps:
            self.ops[j]["sig"] = True
        return i

    def fence(self, engs=("pe", "act", "dve", "pool", "sp")):
        hs = list(self.last_w.keys())
        for e in engs:
            self.op(e, None, r=hs, w=["_fence_" + e])

    def emit(self):
        nc = self.nc
        ticket = {e: 0 for e in self.eng_sem}
        dma_next = {q: 0 for q in self.dma_pool}
        dma_uses = {}
        for o in self.ops:
            if o["dma"]:
                q = o["eng"]
                pool = self.dma_pool[q]
                sem = pool[dma_next[q] % len(pool)]
                dma_next[q] += 1
                u = dma_uses.get(id(sem), 0)
                o["pre"] = (sem, 16 * u)
                dma_uses[id(sem)] = u + 1
                o["ev"] = (sem, 16 * (u + 1))
            elif o["sig"] and o["fn"] is not None:
                ticket[o["eng"]] += 1
                o["ev"] = (self.eng_sem[o["eng"]], ticket[o["eng"]])
        ops = self.ops

        def run(engname, eobj):
            waited = {}

            def wait(sem, val):
                if val <= 0:
                    return
                if waited.get(id(sem), 0) >= val:
                    return
                eobj.wait_ge(sem, val)
                waited[id(sem)] = val

            for o in ops:
                if o["eng"] != engname:
                    continue
                for j in o["deps"]:
                    ev = ops[j].get("ev")
                    if ev is not None:
                        wait(*ev)
                if o["fn"] is None:
                    continue
                if o["dma"]:
                    wait(*o["pre"])
                    ins = o["fn"](eobj)
                    ins.then_inc(o["ev"][0], 16)
                else:
                    ins = o["fn"](eobj)
                    if o["sig"]:
                        ins.then_inc(o["ev"][0], 1)

        with nc.Block() as block:
            @block.tensor
            def _(e):
                run("pe", e)

            @block.scalar
            def _(e):
                run("act", e)

            @block.vector
            def _(e):
                run("dve", e)

            @block.gpsimd
            def _(e):
                run("pool", e)

            @block.sync
            def _(e):
                run("sp", e)


class _Stop(Exception):
    pass


def merge(*lists):
    lists = [l for l in lists if l]
    out = []
    idx = [0] * len(lists)
    total = sum(len(l) for l in lists)
    while len(out) < total:
        best, bv = None, None
        for k, l in enumerate(lists):
            if idx[k] < len(l):
                v = (idx[k] + 0.5) / len(l)
                if bv is None or v < bv:
                    best, bv = k, v
        out.append(lists[best][idx[best]])
        idx[best] += 1
    return out


class Arena:
    def __init__(self, t, nbytes):
        self.t = t
        self.nbytes = nbytes
        self.off = 0

    def alloc(self, shape, dt):
        esz = 4 if dt in (F32, I32) else 2
        n = int(np.prod(shape)) * esz
        n = (n + 63) // 64 * 64
        assert self.off + n <= self.nbytes, ("arena overflow", self.off, n, self.nbytes)
        v = self.t[:, self.off // 4:(self.off + n) // 4]
        self.off += n
        if dt != F32:
            v = v.bitcast(dt)
        v = v[:, 0:int(np.prod(shape))]
        if len(shape) == 2:
            return v.rearrange("p (a b) -> p a b", a=shape[0])
        if len(shape) == 3:
            return v.rearrange("p (a b c) -> p a b c", a=shape[0], b=shape[1])
        return v


def build_program(dbg=None):
    nc = bass.Bass("TRN2", target_bir_lowering=False)
    try:
        return _build_program(nc, dbg)
    except _Stop:
        return nc


def _build_program(nc, dbg=None):
    dr = {}

    def din(name, shape, dt=F32):
        dr[name] = nc.dram_tensor(name, list(shape), dt, kind="ExternalInput").ap()
        return dr[name]

    x_d = din("x", [S, D])
    cols_d = din("cols", [128, NCOL])
    rows_d = din("rows", [1, NROW])
    cst_d = din("cst", [128, NK])
    wada_d = din("w_ada", [D, 6 * D])
    win_d = din("w_in", [D, DIN])
    wout_d = din("w_out", [D, D])
    wr_d = din("w_r", [D, 36])
    wg_d = din("wg", [NE * 128, 2048])
    wu_d = din("wu", [NE * 128, 2048])
    wd_d = din("wd", [NE * 128, 2048])
    out_d = nc.dram_tensor("out", [S, D], F32, kind="ExternalOutput").ap()
    if _CACHE.get("p2only"):
        z_d = din("z_in", [S, D])
    else:
        z_d = nc.dram_tensor("z_scr", [S, D], F32, kind="Internal").ap()
    xs_d = nc.dram_tensor("xs_scr", [NS * T, D], BF16, kind="Internal").ap()
    ys_d = nc.dram_tensor("ys_scr", [NS * T, D], F32, kind="Internal").ap()
    dbg_d = None
    if dbg:
        dbg_d = nc.dram_tensor("dbg", list(dbg["shape"]), F32, kind="ExternalOutput").ap()

    import contextlib
    with contextlib.ExitStack() as st:
        ARENA_BYTES = 206 * 1024
        arena_t = st.enter_context(nc.sbuf_tensor("arena", [128, ARENA_BYTES // 4], F32))
        ps = [st.enter_context(nc.psum_tensor(f"ps{i}", [128, 512], F32)) for i in range(6)]
        psb = [st.enter_context(nc.psum_tensor(f"psb{i}", [128, 1024], BF16)) for i in range(2)]
        sems = [st.enter_context(nc.semaphore(f"s{i}")) for i in range(_CACHE.get("nsem", 48))]
        P = Prog(nc, sems)
        A = Arena(arena_t, ARENA_BYTES)
        psn = [0]

        def nps():
            i = psn[0] % 6
            psn[0] += 1
            return ps[i], f"ps{i}"

        cols = A.alloc([NCOL], F32)
        identf = A.alloc([128], F32)
        identb = A.alloc([128], BF16)
        onesb = A.alloc([128], BF16)
        ones512 = A.alloc([128], BF16)
        bdb = A.alloc([128], BF16)
        Ub = A.alloc([128], BF16)
        maskT = A.alloc([2, 512], BF16)
        modc = A.alloc([32], F32)
        g1bc = A.alloc([D], F32)
        gbrow = A.alloc([D], BF16)
        EPSC = A.alloc([1], F32)
        EPSC2 = A.alloc([1], F32)
        mark_persist = A.off
        cst = A.alloc([NK], F32)
        gbc = A.alloc([4, D], F32)
        mod_d = nc.dram_tensor("mod_scr", [1, 3 * D], F32, kind="Internal").ap()

        P.op("sp", lambda e: e.dma_start(out=cols, in_=cols_d), w=["cols"], dma=True)
        P.op("sp", lambda e: e.dma_start(out=cst, in_=cst_d), w=["cst0"], dma=True)
        P.op("sp", lambda e: e.dma_start(out=identf, in_=cst_d[:, K_ID:K_ID + 128]), w=["cst"], dma=True)
        P.op("dve", lambda e: e.tensor_copy(out=identb, in_=identf), r=["cst"], w=["identb"])
        P.op("dve", lambda e: e.memset(onesb, 1.0), w=["onesb"])
        P.op("dve", lambda e: e.memset(EPSC, EPS), w=["epsc"])
        P.op("dve", lambda e: e.memset(EPSC2, EPS / (ALPHA * ALPHA)), w=["epsc2"])
        P.op("dve", lambda e: e.memset(ones512, 1.0 / 512.0), w=["ones512"])
        P.op("dve", lambda e: e.tensor_copy(out=bdb, in_=cst[:, K_BD:K_BD + 128]), r=["cst0"], w=["bdb"])
        P.op("dve", lambda e: e.tensor_copy(out=Ub, in_=cst[:, K_U:K_U + 128]), r=["cst0"], w=["Ub"])
        P.op("dve", lambda e: e.tensor_copy(out=maskT.rearrange("p a b -> p (a b)"), in_=cst[:, K_M0:K_M0 + 1024]),
             r=["cst0"], w=["maskT"])

        ph0 = A.off
        cact = A.alloc([8], F32)
        cbc = A.alloc([8, 128], BF16)
        wab = [A.alloc([8, 512], BF16) for _ in range(2)]
        badab = A.alloc([512], F32)
        modb = A.alloc([4 * D], F32)
        P.op("act", lambda e: e.activation(out=cact, in_=cols[:, C_C:C_C + 8], func=AF.Silu), r=["cols"], w=["cact"])
        for c in range(8):
            P.op("dve", lambda e, c=c: e.tensor_scalar(out=cbc[:, c, :], in0=onesb, scalar1=cact[:, c:c + 1],
                                                       scalar2=None, op0=ALU.mult),
                 r=["cact", "onesb"], w=["cbc"])
        for blk in range(12):
            wb = wab[blk % 2]
            hw = f"wab{blk % 2}"
            P.op("pool", lambda e, blk=blk, wb=wb: e.dma_start(
                out=wb, in_=wada_d[:, blk * 512:(blk + 1) * 512].rearrange("(c p) n -> p c n", p=128)),
                w=[hw], dma=True)
            P.op("sp", lambda e, blk=blk: e.dma_start(
                out=badab, in_=rows_d[0:1, R_BADA + blk * 512:R_BADA + (blk + 1) * 512].partition_broadcast(128)),
                w=["badab"], dma=True)
            pt, hp = nps()
            for c in range(8):
                P.op("pe", lambda e, c=c, pt=pt, wb=wb: e.matmul(pt[:], lhsT=cbc[:, c, :], rhs=wb[:, c, :],
                                                               start=(c == 0), stop=(c == 7)),
                     r=["cbc", hw], w=[hp])
            if blk < 4:
                dst = modb[:, blk * 512:(blk + 1) * 512]
                hd = f"modb{blk}"
            else:
                gi = (blk - 4) // 2
                dst = gbc[:, gi, ((blk - 4) % 2) * 512:((blk - 4) % 2 + 1) * 512]
                hd = f"gbc{gi}_{blk % 2}"
            P.op("dve", lambda e, pt=pt, dst=dst: e.tensor_tensor(out=dst, in0=pt[:], in1=badab, op=ALU.add),
                 r=[hp, "badab"], w=[hd])
        srcs = []
        for c in range(8):
            srcs.append((modb[:, c * 128:(c + 1) * 128], f"modb{c // 4}", c, 0.0))
        for c in range(8):
            srcs.append((modb[:, 1024 + c * 128:1024 + (c + 1) * 128], f"modb{2 + c // 4}", 8 + c, 1.0))
        for c in range(8):
            srcs.append((gbc[:, 1, c * 128:(c + 1) * 128], f"gbc1_{c // 4}", 16 + c, 0.0))
        for c in range(8):
            srcs.append((gbc[:, 2, c * 128:(c + 1) * 128], f"gbc2_{c // 4}", 24 + c, 1.0))
        for (src, hs, col, add) in srcs:
            pt, hp = nps()
            P.op("pe", lambda e, pt=pt, src=src: e.transpose(out=pt[:, 0:128], in_=src, identity=identf),
                 r=[hs, "cst"], w=[hp])
            P.op("dve", lambda e, pt=pt, col=col, add=add: e.tensor_scalar(
                out=modc[:, col:col + 1], in0=pt[:, 0:1], scalar1=add, scalar2=None, op0=ALU.add),
                r=[hp], w=["modc"])
        G_ALL = [f"gbc{gi}_{h}" for gi in range(4) for h in range(2)]
        P.op("sp", lambda e: e.dma_start(out=mod_d, in_=gbc[0:1, 1:4, :].rearrange("p a d -> p (a d)")), r=G_ALL, w=["mod_d"], dma=True)
        P.op("dve", lambda e: e.tensor_scalar(out=g1bc, in0=gbc[:, 0, :], scalar1=1.0 / ALPHA, scalar2=None, op0=ALU.mult), r=G_ALL, w=["g1bc"])
        boutb = modb[:, 0:D]
        P.op("sp", lambda e: e.dma_start(out=boutb, in_=rows_d[0:1, R_BOUT:R_BOUT + D].partition_broadcast(128)),
             w=["modb0", "modb1"], dma=True)
        P.op("dve", lambda e: e.tensor_tensor(out=boutb, in0=boutb, in1=g1bc, op=ALU.mult), r=["modb0", "modb1", "g1bc"], w=["modb0", "modb1"])
        P.op("dve", lambda e: e.tensor_copy(out=gbrow, in_=boutb), r=["modb0", "modb1"], w=["gbrow"])
        P.fence()
        if _CACHE.get("stop") == 0:
            P.op("sp", lambda e: e.dma_start(out=dbg_d[0:128, 0:32], in_=modc), r=["modc"], w=["dbg"], dma=True)
            P.op("sp", lambda e: e.dma_start(out=dbg_d[128:256, :], in_=gbc[:, 0, :]), r=["gbc0_0", "gbc0_1"], w=["dbg2"], dma=True)
            P.fence()
            P.emit()
            return nc
        A.off = mark_persist

        win = A.alloc([8, DIN], BF16)
        modc2 = A.alloc([32], F32)
        P.op("dve", lambda e: e.tensor_copy(out=modc2, in_=modc), r=["modc"], w=["modc2"])
        wout = A.alloc([8, D], BF16)
        diag = A.alloc([124, 128], BF16)
        xt = A.alloc([4, D], BF16)
        hT = A.alloc([8, 512], BF16)
        vr = [A.alloc([4, 542], BF16) for _ in range(2)]
        kr = [[A.alloc([640], BF16) for _ in range(2)] for _ in range(2)]
        va = [A.alloc([5, 130], BF16) for _ in range(2)]
        qT2 = [A.alloc([4, 512], BF16) for _ in range(2)]
        sig = A.alloc([512], F32)
        ybf2 = [A.alloc([4, 512], BF16) for _ in range(2)]
        y2bf = A.alloc([4, 512], BF16)
        rstd2 = [A.alloc([512], F32) for _ in range(2)]
        nmr2 = [A.alloc([512], F32) for _ in range(2)]
        zc2 = [A.alloc([512], F32) for _ in range(2)]
        sc2_ = [A.alloc([512], F32) for _ in range(2)]
        s2bf2 = [A.alloc([512], BF16) for _ in range(2)]
        r2c2 = [A.alloc([512], F32) for _ in range(2)]
        ycT = A.alloc([8, 512], BF16)
        mx2 = [A.alloc([8], F32) for _ in range(2)]
        mxb2 = [A.alloc([8], BF16) for _ in range(2)]
        nmx2 = [A.alloc([8], F32) for _ in range(2)]
        dcat2 = [A.alloc([2, 512], BF16) for _ in range(2)]
        ET2 = [A.alloc([4, 512], BF16) for _ in range(2)]
        es_t2 = [A.alloc([8], F32) for _ in range(2)]
        den2 = [A.alloc([8], F32) for _ in range(2)]
        osb2 = [A.alloc([8, 64], F32) for _ in range(2)]
        osq2 = [A.alloc([8, 64], F32) for _ in range(2)]
        ssq2 = [A.alloc([8], F32) for _ in range(2)]
        yat2 = [A.alloc([512], BF16) for _ in range(2)]
        rows_sb = A.alloc([128 + 8 + 512], F32)
        xr2 = [A.alloc([D], F32) for _ in range(2)]
        bnst2 = [A.alloc([12], F32) for _ in range(2)]
        bnag2 = [A.alloc([4], F32) for _ in range(2)]
        print("phase1 arena bytes", A.off)

        def mk_nps(ids):
            st_ = [0]

            def f():
                i = ids[st_[0] % len(ids)]
                st_[0] += 1
                return ps[i], f"ps{i}"
            return f

        bv_bc = rows_sb[:, 0:128]
        sink_bc = rows_sb[:, 128:136]
        aog_bc = rows_sb[:, 136:648]
        P.op("sp", lambda e: e.dma_start(out=rows_sb, in_=rows_d[0:1, R_BV:R_BV + 128 + 8 + 512].partition_broadcast(128)),
             w=["rows_sb"], dma=True)
        for c in range(8):
            P.op("pool", lambda e, c=c: e.dma_start(out=win[:, c, :], in_=win_d[c * 128:(c + 1) * 128, :]), w=[f"win{c}"], dma=True)
            P.op("pool", lambda e, c=c: e.dma_start(out=wout[:, c, :], in_=wout_d[c * 128:(c + 1) * 128, :]), w=[f"wout{c}"], dma=True)
        for c in range(8):
            P.op("dve", lambda e, c=c: e.tensor_tensor(out=wout[:, c, :], in0=wout[:, c, :], in1=g1bc, op=ALU.mult),
                 r=[f"wout{c}", "g1bc"], w=[f"wout{c}"])
        WINH = [f"win{c}" for c in range(8)]
        WOUTH = [f"wout{c}" for c in range(8)]
        SK = _CACHE.get("skip", set())
        for c in range(4 if "diag" not in SK else 0):
            P.op("dve", lambda e, c=c: e.tensor_tensor(
                out=diag[:, c * 31:(c + 1) * 31, :], in0=identb.unsqueeze(1).to_broadcast([128, 31, 128]),
                in1=cols[:, C_CW + c * 31:C_CW + (c + 1) * 31].unsqueeze(2).to_broadcast([128, 31, 128]), op=ALU.mult),
                r=["identb", "cols"], w=["diag"])
        if "memset" not in SK:
            P.op("pool", lambda e: e.memset(vr[0], 0.0), w=["vr0"])
            P.op("pool", lambda e: e.memset(vr[1], 0.0), w=["vr1"])
        for par in range(2 if "memset" not in SK else 0):
            for g in range(2):
                P.op("pool", lambda e, par=par, g=g: e.memset(kr[par][g], 0.0), w=[f"kr{par}"])
            P.op("pool", lambda e, par=par: e.memset(va[par], 0.0), w=[f"va{par}"])
            P.op("dve", lambda e, par=par: e.memset(va[par][:, 1:, 64:65], 1.0), r=[f"va{par}"], w=[f"va{par}"])
            P.op("dve", lambda e, par=par: e.memset(va[par][:, 1:, 129:130], 1.0), r=[f"va{par}"], w=[f"va{par}"])

        if _CACHE.get("stop") == 1:
            P.fence()
            P.op("sp", lambda e: e.dma_start(out=dbg_d[0:128, 0:512], in_=g1bc[:, 0:512]), r=["g1bc"], w=["dbg"], dma=True)
            P.fence()
            P.emit()
            return nc
        nps_s1 = mk_nps([0, 1])
        psb1_f32 = psb[1][:, :].bitcast(F32)
        nps_at = [mk_nps([2, 3]), mk_nps([4, 5])]
        nps_ep = [mk_nps([2, 3]), mk_nps([4, 5])]

        def stage1(mt):
            par = mt % 2
            t0 = mt * 512
            vcur, vprev = vr[par], vr[1 - par]
            hv, hvp = f"vr{par}", f"vr{1 - par}"
            kT, kTp = kr[par], kr[1 - par]
            hk, hkp = f"kr{par}", f"kr{1 - par}"
            vaug, vaugp = va[par], va[1 - par]
            hva, hvap = f"va{par}", f"va{1 - par}"
            qT, hq = qT2[par], f"qT{par}"
            ybf, hy = ybf2[par], f"ybf{par}"
            rstd_sb, hrs = rstd2[par], f"rstd{par}"
            nmr_sb, hnm = nmr2[par], f"nmr{par}"
            nps = nps_s1
            P.rec()
            P.op("pool", lambda e: e.dma_start(out=xt, in_=x_d[t0:t0 + 512, :].rearrange("(s p) d -> p s d", p=128)),
                 w=["xt"], dma=True)
            for c in range(8):
                ptx = psb[0][:, 0:512]
                hp = "psb0"
                for s in range(4):
                    P.op("pe", lambda e, s=s, c=c: e.transpose(
                        out=ptx[:, s * 128:(s + 1) * 128], in_=xt[:, s, c * 128:(c + 1) * 128], identity=identb),
                        r=["xt", "identb"], w=[hp])
                P.op("act", lambda e, c=c: e.activation(
                    out=hT[:, c, :], in_=ptx[:, 0:512], func=AF.Identity, bias=modc2[:, c:c + 1], scale=modc2[:, 8 + c:9 + c]),
                    r=[hp, "modc2"], w=["hT"])
            if mt > 0:
                P.op("pool", lambda e: e.tensor_copy(out=vcur[:, :, 0:30], in_=vprev[:, :, 512:542]), r=[hvp], w=[hv])
                for g in range(2):
                    P.op("pool", lambda e, g=g: e.tensor_copy(out=kT[g][:, 0:128], in_=kTp[g][:, 512:640]), r=[hkp], w=[hk])
                P.op("pool", lambda e: e.tensor_copy(out=vaug[:, 0, :], in_=vaugp[:, 4, :]), r=[hvap], w=[hva])
            for c in range(4):
                pb, hpb = nps()
                for k in range(8):
                    P.op("pe", lambda e, pb=pb, k=k, c=c: e.matmul(
                        pb[:], lhsT=win[:, k, 512 + c * 128:512 + (c + 1) * 128], rhs=hT[:, k, :],
                        start=(k == 0), stop=(k == 7)), r=WINH + ["hT"], w=[hpb])
                P.op("act", lambda e, pb=pb, c=c: e.activation(
                    out=sig, in_=pb[:], func=AF.Sigmoid, bias=cols[:, C_BIN + 4 + c:C_BIN + 5 + c], scale=1.0),
                    r=[hpb, "cols"], w=["sig"])
                pa, hpa = nps()
                for k in range(8):
                    P.op("pe", lambda e, pa=pa, k=k, c=c: e.matmul(
                        pa[:], lhsT=win[:, k, c * 128:(c + 1) * 128], rhs=hT[:, k, :],
                        start=(k == 0), stop=(k == 7)), r=WINH + ["hT"], w=[hpa])
                P.op("dve", lambda e, pa=pa, c=c: e.scalar_tensor_tensor(
                    out=vcur[:, c, 30:542], in0=pa[:], scalar=cols[:, C_BIN + c:C_BIN + c + 1], in1=sig,
                    op0=ALU.add, op1=ALU.mult), r=[hpa, "sig", "cols"], w=[hv])
            for i in range(4):
                pq, hpq = nps()
                for k in range(8):
                    P.op("pe", lambda e, pq=pq, k=k, i=i: e.matmul(
                        pq[:], lhsT=win[:, k, 1024 + i * 128:1024 + (i + 1) * 128], rhs=hT[:, k, :],
                        start=(k == 0), stop=(k == 7)), r=WINH + ["hT"], w=[hpq])
                P.op("dve", lambda e, pq=pq, i=i: e.tensor_scalar(
                    out=qT[:, i, :], in0=pq[:], scalar1=cols[:, C_BIN + 8 + i:C_BIN + 9 + i], scalar2=0.125,
                    op0=ALU.add, op1=ALU.mult), r=[hpq, "cols"], w=[hq])
            pk, hpk = nps()
            for k in range(8):
                P.op("pe", lambda e, k=k: e.matmul(
                    pk[:], lhsT=win[:, k, 1536:1664], rhs=hT[:, k, :], start=(k == 0), stop=(k == 7)),
                    r=WINH + ["hT"], w=[hpk])
            for g in range(2):
                P.op("act", lambda e, g=g: e.activation(
                    out=kT[g][g * 64:(g + 1) * 64, 128:640], in_=pk[g * 64:(g + 1) * 64, :],
                    func=AF.Identity, bias=cols[g * 64:(g + 1) * 64, C_BIN + 12:C_BIN + 13], scale=1.0),
                    r=[hpk, "cols"], w=[hk])
            pv, hpv = nps()
            for s in range(4):
                for k in range(8):
                    P.op("pe", lambda e, s=s, k=k: e.matmul(
                        pv[:, s * 128:(s + 1) * 128], lhsT=hT[:, k, s * 128:(s + 1) * 128], rhs=win[:, k, 1664:1792],
                        start=(k == 0), stop=(k == 7)), r=WINH + ["hT"], w=[hpv])
            for s in range(4):
                blk = s + 1
                P.op("dve", lambda e, s=s, blk=blk: e.tensor_tensor(
                    out=vaug[:, blk, :].rearrange("p (g d) -> p g d", g=2)[:, :, 0:64],
                    in0=pv[:, s * 128:(s + 1) * 128].rearrange("p (g d) -> p g d", g=2),
                    in1=bv_bc.rearrange("p (g d) -> p g d", g=2), op=ALU.add),
                    r=[hpv, "rows_sb"], w=[hva])
            for c in range(4):
                py, hpy = nps()
                for j in range(31):
                    P.op("pe", lambda e, py=py, c=c, j=j: e.matmul(
                        py[:], lhsT=diag[:, c * 31 + j, :], rhs=vcur[:, c, j:j + 512], start=(j == 0), stop=(j == 30)),
                        r=["diag", hv], w=[hpy])
                P.op("act", lambda e, py=py, c=c: e.activation(
                    out=ybf[:, c, :], in_=py[:], func=AF.Identity, bias=cols[:, C_CB + c:C_CB + c + 1], scale=1.0),
                    r=[hpy, "cols"], w=[hy])
                P.op("act", lambda e, py=py, c=c: e.activation(
                    out=y2bf[:, c, :], in_=py[:], func=AF.Square, bias=cols[:, C_CB + c:C_CB + c + 1], scale=1.0),
                    r=[hpy, "cols"], w=["y2bf"])
            pm, hpm = nps()
            for c in range(4):
                P.op("pe", lambda e, c=c: e.matmul(pm[:], lhsT=ones512, rhs=ybf[:, c, :], start=(c == 0), stop=(c == 3)),
                     r=["ones512", hy], w=[hpm])
            pe2, hpe2 = nps()
            for c in range(4):
                P.op("pe", lambda e, c=c: e.matmul(pe2[:], lhsT=ones512, rhs=y2bf[:, c, :], start=(c == 0), stop=(c == 3)),
                     r=["ones512", "y2bf"], w=[hpe2])
            P.op("act", lambda e: e.activation(out=nmr_sb, in_=pm[:], func=AF.Identity), r=[hpm], w=[hnm])
            P.op("dve", lambda e: e.tensor_tensor(out=rstd_sb, in0=nmr_sb, in1=nmr_sb, op=ALU.mult), r=[hnm], w=[hrs])
            P.op("dve", lambda e: e.tensor_tensor(out=rstd_sb, in0=pe2[:], in1=rstd_sb, op=ALU.subtract), r=[hpe2, hrs], w=[hrs])
            P.op("act", lambda e: e.activation(out=rstd_sb, in_=rstd_sb, func=AF.Sqrt, bias=EPSC, scale=1.0), r=[hrs, "epsc"], w=[hrs])
            P.op("dve", lambda e: e.reciprocal(out=rstd_sb, in_=rstd_sb), r=[hrs], w=[hrs])
            P.op("dve", lambda e: e.scalar_tensor_tensor(out=nmr_sb, in0=nmr_sb, scalar=-1.0, in1=rstd_sb, op0=ALU.mult, op1=ALU.mult),
                 r=[hnm, hrs], w=[hnm])
            return P.end()

        def stage2(mt):
            par = mt % 2
            kT_l, vaug_l = kr[par], va[par]
            hk, hva = f"kr{par}", f"va{par}"
            qT, hq = qT2[par], f"qT{par}"
            ybf, hy = ybf2[par], f"ybf{par}"
            rstd_sb, hrs = rstd2[par], f"rstd{par}"
            nmr_sb, hnm = nmr2[par], f"nmr{par}"

            def convln_chain(c):
                k = c % 2
                zc, sc_, s2bf, r2c = zc2[k], sc2_[k], s2bf2[k], r2c2[k]
                P.rec()
                P.op("dve", lambda e: e.tensor_tensor(out=zc, in0=ybf[:, c, :], in1=rstd_sb, op=ALU.mult),
                     r=[hy, hrs], w=[f"zc{k}"])
                P.op("dve", lambda e: e.tensor_tensor(out=zc, in0=zc, in1=nmr_sb, op=ALU.add), r=[f"zc{k}", hnm], w=[f"zc{k}"])
                P.op("act", lambda e: e.activation(out=sc_, in_=zc, func=AF.Silu, bias=cols[:, C_LB + c:C_LB + c + 1],
                                                   scale=cols[:, C_LG + c:C_LG + c + 1]), r=[f"zc{k}", "cols"], w=[f"sc{k}"])
                P.op("act", lambda e: e.activation(out=s2bf, in_=sc_, func=AF.Square), r=[f"sc{k}"], w=[f"s2bf{k}"])
                pr, hpr = psb1_f32, "psb1"
                P.op("pe", lambda e: e.matmul(pr[:], lhsT=bdb, rhs=s2bf, start=True, stop=True), r=["bdb", f"s2bf{k}"], w=[hpr])
                P.op("act", lambda e: e.activation(out=r2c, in_=pr[:], func=AF.Sqrt, bias=EPSC, scale=1.0), r=[hpr, "epsc"], w=[f"r2c{k}"])
                P.op("dve", lambda e: e.reciprocal(out=r2c, in_=r2c), r=[f"r2c{k}"], w=[f"r2c{k}"])
                P.op("dve", lambda e: e.scalar_tensor_tensor(out=ycT[:, c, :], in0=sc_, scalar=cols[:, C_OG + c:C_OG + c + 1],
                                                             in1=r2c, op0=ALU.mult, op1=ALU.mult),
                     r=[f"sc{k}", f"r2c{k}", "cols"], w=[f"ycTc{c}"])
                return P.end()

            def attn_chain(s):
                k = s % 2
                mx, mxb, nmx, dcat, ET = mx2[k], mxb2[k], nmx2[k], dcat2[k], ET2[k]
                es_t, den, osb, osq, ssq, yat = es_t2[k], den2[k], osb2[k], osq2[k], ssq2[k], yat2[k]
                mynps = nps_at[k]
                n = mt * 4 + s
                qs = slice(s * 128, (s + 1) * 128)
                P.rec()
                for i in range(4):
                    pS, hpS = mynps()
                    for g in range(2):
                        P.op("pe", lambda e, pS=pS, i=i, g=g: e.matmul(
                            pS[:, g * 256:(g + 1) * 256], lhsT=qT[:, i, qs], rhs=kT_l[g][:, s * 128:s * 128 + 256],
                            start=True, stop=True), r=[hq, hk], w=[hpS])
                    P.op("dve", lambda e, pS=pS, i=i: e.tensor_reduce(
                        out=mx[:, 2 * i:2 * i + 2], in_=pS[:].rearrange("p (g k) -> p g k", g=2), axis=AX.X, op=ALU.max),
                        r=[hpS], w=[f"mx{k}"])
                P.op("dve", lambda e: e.tensor_copy(out=mxb, in_=mx), r=[f"mx{k}"], w=[f"mxb{k}"])
                P.op("dve", lambda e: e.tensor_scalar(out=nmx, in0=mxb, scalar1=-1.0, scalar2=None, op0=ALU.mult), r=[f"mxb{k}"], w=[f"nmx{k}"])
                for g in range(2):
                    P.op("dve", lambda e, g=g: e.tensor_tensor(
                        out=dcat[:, g, :].rearrange("p (i q) -> p i q", i=4), in0=identb.unsqueeze(1).to_broadcast([128, 4, 128]),
                        in1=nmx.rearrange("p (i g) -> p i g", g=2)[:, :, g:g + 1].to_broadcast([128, 4, 128]), op=ALU.mult),
                        r=["identb", f"nmx{k}"], w=[f"dcat{k}_{g}"])
                khs = [1] if n == 0 else [0, 1]
                for g in range(2):
                    for kh in khs:
                        pT, hpT = mynps()
                        kc = slice(s * 128 + kh * 128, s * 128 + kh * 128 + 128)
                        P.op("pe", lambda e, pT=pT, g=g, kc=kc: e.matmul(
                            pT[:].rearrange("p (i q) -> p i q", i=4), lhsT=kT_l[g][:, kc], rhs=qT[:, :, qs], start=True, stop=False),
                            r=[hk, hq], w=[hpT])
                        P.op("pe", lambda e, pT=pT, g=g: e.matmul(pT[:], lhsT=onesb, rhs=dcat[:, g, :], start=False, stop=False),
                             r=["onesb", f"dcat{k}_{g}"], w=[hpT])
                        P.op("pe", lambda e, pT=pT, kh=kh: e.matmul(pT[:], lhsT=identb, rhs=maskT[:, kh, :], start=False, stop=True),
                             r=["identb", "maskT"], w=[hpT])
                        P.op("act", lambda e, pT=pT, g=g, kh=kh: e.activation(out=ET[:, g * 2 + kh, :], in_=pT[:], func=AF.Exp),
                             r=[hpT], w=[f"ET{k}_{g}{kh}"])
                P.op("dve", lambda e: e.tensor_tensor(out=es_t, in0=sink_bc, in1=nmx, op=ALU.add), r=["rows_sb", f"nmx{k}"], w=[f"es_t{k}"])
                P.op("act", lambda e: e.activation(out=es_t, in_=es_t, func=AF.Exp), r=[f"es_t{k}"], w=[f"es_t{k}"])
                for g in range(2):
                    po, hpo = mynps()
                    for i in range(4):
                        for kh in khs:
                            P.op("pe", lambda e, po=po, g=g, i=i, kh=kh: e.matmul(
                                po[:, i * 65:(i + 1) * 65], lhsT=ET[:, g * 2 + kh, i * 128:(i + 1) * 128],
                                rhs=vaug_l[:, s + kh, g * 65:(g + 1) * 65], start=(kh == khs[0]), stop=(kh == 1)),
                                r=[f"ET{k}_{g}{kh}", hva], w=[hpo])
                    P.op("dve", lambda e, po=po, g=g: e.tensor_tensor(
                        out=den.rearrange("p (i g) -> p i g", g=2)[:, :, g:g + 1],
                        in0=po[:, 0:260].rearrange("p (i d) -> p i d", d=65)[:, :, 64:65],
                        in1=es_t.rearrange("p (i g) -> p i g", g=2)[:, :, g:g + 1], op=ALU.add),
                        r=[hpo, f"es_t{k}"], w=[f"den{k}_{g}"])
                    P.op("act", lambda e, po=po, g=g: e.activation(
                        out=osb.rearrange("p (i g) d -> p i g d", g=2)[:, :, g, :],
                        in_=po[:, 0:260].rearrange("p (i d) -> p i d", d=65)[:, :, 0:64], func=AF.Identity),
                        r=[hpo], w=[f"osb{k}_{g}"])
                DH = [f"den{k}_0", f"den{k}_1"]
                OH = [f"osb{k}_0", f"osb{k}_1"]
                P.op("dve", lambda e: e.reciprocal(out=den, in_=den), r=DH, w=DH)
                P.op("dve", lambda e: e.tensor_tensor(out=osb, in0=osb, in1=den.unsqueeze(2).to_broadcast([128, 8, 64]), op=ALU.mult),
                     r=OH + DH, w=OH)
                P.op("act", lambda e: e.activation(out=osq, in_=osb, func=AF.Square), r=OH, w=[f"osq{k}"])
                P.op("dve", lambda e: e.tensor_reduce(out=ssq, in_=osq, axis=AX.X, op=ALU.add), r=[f"osq{k}"], w=[f"ssq{k}"])
                P.op("act", lambda e: e.activation(out=ssq, in_=ssq, func=AF.Sqrt, bias=EPSC, scale=1.0 / 64.0), r=[f"ssq{k}", "epsc"], w=[f"ssq{k}"])
                P.op("dve", lambda e: e.reciprocal(out=ssq, in_=ssq), r=[f"ssq{k}"], w=[f"ssq{k}"])
                P.op("dve", lambda e: e.tensor_tensor(out=osb, in0=osb, in1=ssq.unsqueeze(2).to_broadcast([128, 8, 64]), op=ALU.mult),
                     r=OH + [f"ssq{k}"], w=OH)
                P.op("dve", lambda e: e.tensor_tensor(out=yat, in0=osb.rearrange("p h d -> p (h d)"), in1=aog_bc, op=ALU.mult),
                     r=OH + ["rows_sb"], w=[f"yat{k}"])
                ptb = ps[3 + 2 * k][:, :].bitcast(BF16)[:, 0:512]
                hptr = f"ps{3 + 2 * k}"
                for i in range(4):
                    P.op("pe", lambda e, i=i: e.transpose(out=ptb[:, i * 128:(i + 1) * 128], in_=yat[:, i * 128:(i + 1) * 128],
                                                         identity=identb), r=[f"yat{k}", "identb"], w=[hptr])
                P.op("act", lambda e: e.activation(
                    out=ycT[:, 4:8, qs], in_=ptb[:, 0:512].rearrange("p (i q) -> p i q", i=4), func=AF.Identity),
                    r=[hptr], w=[f"ycTa{s}"])
                return P.end()

            def xr_load(s):
                k = s % 2
                tt = mt * 4 + s
                P.op("sp", lambda e: e.dma_start(out=xr2[k], in_=x_d[tt * 128:(tt + 1) * 128, :]), w=[f"xr{k}"], dma=True)

            def epi_chain(s, with_load):
                k = s % 2
                tt = mt * 4 + s
                xr, bnst, bnag = xr2[k], bnst2[k], bnag2[k]
                rr = xr
                mynps = nps_ep[k]
                YH = [f"ycTc{c}" for c in range(4)] + [f"ycTa{s}"]
                P.rec()
                if with_load:
                    xr_load(s)
                for h in range(2):
                    po, hpo = mynps()
                    for c in range(8):
                        P.op("pe", lambda e, po=po, c=c, h=h: e.matmul(
                            po[:], lhsT=ycT[:, c, s * 128:(s + 1) * 128], rhs=wout[:, c, h * 512:(h + 1) * 512],
                            start=(c == 0), stop=False), r=YH + WOUTH, w=[hpo])
                    P.op("pe", lambda e, po=po, h=h: e.matmul(
                        po[:], lhsT=onesb[0:1, :], rhs=gbrow[0:1, h * 512:(h + 1) * 512], start=False, stop=True),
                        r=["onesb", "gbrow"], w=[hpo])
                    P.op("dve", lambda e, po=po, h=h: e.tensor_tensor(out=rr[:, h * 512:(h + 1) * 512], in0=po[:],
                                                                      in1=xr[:, h * 512:(h + 1) * 512], op=ALU.add),
                         r=[hpo, f"xr{k}"], w=[f"xr{k}"])
                for h in range(2):
                    P.op("dve", lambda e, h=h: e.bn_stats(out=bnst[:, h * 6:(h + 1) * 6], in_=rr[:, h * 512:(h + 1) * 512]),
                         r=[f"xr{k}"], w=[f"bnst{k}"])
                P.op("dve", lambda e: e.bn_aggr(out=bnag[:, 0:2], in_=bnst), r=[f"bnst{k}"], w=[f"bnag{k}"])
                P.op("act", lambda e: e.activation(out=bnag[:, 2:3], in_=bnag[:, 1:2], func=AF.Sqrt, bias=EPSC2, scale=1.0),
                     r=[f"bnag{k}", "epsc2"], w=[f"bnagb{k}"])
                P.op("dve", lambda e: e.reciprocal(out=bnag[:, 2:3], in_=bnag[:, 2:3]), r=[f"bnagb{k}"], w=[f"bnagb{k}"])
                P.op("dve", lambda e: e.scalar_tensor_tensor(out=bnag[:, 3:4], in0=bnag[:, 0:1], scalar=-1.0, in1=bnag[:, 2:3],
                                                             op0=ALU.mult, op1=ALU.mult), r=[f"bnag{k}", f"bnagb{k}"], w=[f"bnagc{k}"])
                P.op("act", lambda e: e.activation(out=rr, in_=rr, func=AF.Identity, bias=bnag[:, 3:4], scale=bnag[:, 2:3]),
                     r=[f"xr{k}", f"bnagb{k}", f"bnagc{k}"], w=[f"xr{k}"])
                P.op("sp", lambda e: e.dma_start(out=z_d[tt * 128:(tt + 1) * 128, :], in_=rr), r=[f"xr{k}"], w=[f"z_d{tt}"], dma=True)
                return P.end()


            out = []
            P.rec()
            xr_load(0)
            xr_load(1)
            out += P.end()
            out += merge(convln_chain(0) + convln_chain(1), attn_chain(0), attn_chain(1))
            out += merge(convln_chain(2) + convln_chain(3), attn_chain(2), attn_chain(3))
            out += merge(epi_chain(0, False), epi_chain(1, False))
            out += merge(epi_chain(2, True), epi_chain(3, True))
            return out

        P.play(stage1(0))
        for mt in range(NMT):
            nxt = stage1(mt + 1) if mt + 1 < NMT else []
            P.play(merge(stage2(mt), nxt))

        P.fence()
        if dbg and dbg["what"] == "z":
            nr = _CACHE.get('nmt_run', NMT) * 512
            P.op("sp", lambda e: e.dma_start(out=dbg_d[0:nr, :], in_=z_d[0:nr, :]), r=[f"z_d{i}" for i in range(nr // 128)], w=["dbg"], dma=True)
            P.fence()
            P.emit()
            return nc

        try:
            build_phase2(nc, P, A, ps, nps, dr, out_d, z_d, xs_d, ys_d, dbg, dbg_d, mark_persist,
                     dict(cols=cols, identf=identf, identb=identb, onesb=onesb, Ub=Ub, modc=modc, mod_d=mod_d,
                              EPSC=EPSC, psb=psb))
        except _StopEmit:
            pass
        P.fence()
        P.emit()
    return nc


def build_phase2(nc, P, A, ps, nps, dr, out_d, z_d, xs_d, ys_d, dbg, dbg_d, mark, K):
    cols, identf, identb, onesb, Ub, mod_d, EPSC, psb = (K[k] for k in ("cols", "identf", "identb", "onesb", "Ub", "mod_d", "EPSC", "psb"))
    rows_d, wr_d, wg_d, wu_d, wd_d = dr["rows"], dr["w_r"], dr["wg"], dr["wu"], dr["wd"]
    A.off = mark
    gbc = A.alloc([4, D], F32)
    P.op("sp", lambda e: e.dma_start(out=gbc[:, 1:4, :].rearrange("p a d -> p (a d)"), in_=mod_d[0:1, :].partition_broadcast(128)),
         r=["mod_d"], w=["gbc1_0", "gbc1_1", "gbc2_0", "gbc2_1", "gbc3_0", "gbc3_1"], dma=True)
    lnr = A.alloc([4, D], F32)
    misc = A.alloc([36 + 64], F32)
    A2 = A.alloc([D], F32)
    B2 = A.alloc([D], F32)
    GA = A.alloc([D], F32)
    BA = A.alloc([D], F32)
    wr = A.alloc([8, 36], F32)
    pos_i = A.alloc([2, NT], I32)
    wts = A.alloc([2, NT], F32)
    widx_i = A.alloc([64], I32)
    mark2 = A.off
    br_bc = misc[:, 0:36]
    slot_bc = misc[:, 36:36 + NS]
    P.op("sp", lambda e: e.dma_start(out=lnr.rearrange("p a d -> p (a d)"), in_=rows_d[0:1, R_L1G:R_L1G + 4 * D].partition_broadcast(128)),
         w=["lnr"], dma=True)
    P.op("sp", lambda e: e.dma_start(out=misc, in_=rows_d[0:1, R_BR:R_BR + 100].partition_broadcast(128)), w=["misc"], dma=True)
    P.op("sp", lambda e: e.dma_start(out=wr, in_=wr_d.rearrange("(c p) n -> p c n", p=128)), w=["wr"], dma=True)
    G2H = ["gbc1_0", "gbc1_1", "gbc2_0", "gbc2_1", "gbc3_0", "gbc3_1"]
    P.op("dve", lambda e: e.scalar_tensor_tensor(out=A2, in0=gbc[:, 2, :], scalar=1.0, in1=lnr[:, 0, :], op0=ALU.add, op1=ALU.mult),
         r=["lnr"] + G2H, w=["A2"])
    P.op("dve", lambda e: e.scalar_tensor_tensor(out=B2, in0=gbc[:, 2, :], scalar=1.0, in1=lnr[:, 1, :], op0=ALU.add, op1=ALU.mult),
         r=["lnr"] + G2H, w=["B2"])
    P.op("dve", lambda e: e.tensor_tensor(out=B2, in0=B2, in1=gbc[:, 1, :], op=ALU.add), r=["B2"] + G2H, w=["B2"])
    P.op("dve", lambda e: e.tensor_scalar(out=GA, in0=lnr[:, 0, :], scalar1=ALPHA, scalar2=None, op0=ALU.mult), r=["lnr"], w=["GA"])
    P.op("dve", lambda e: e.tensor_scalar(out=BA, in0=lnr[:, 1, :], scalar1=ALPHA, scalar2=None, op0=ALU.mult), r=["lnr"], w=["BA"])

    def mk_nps(ids):
        st_ = [0]

        def f():
            i = ids[st_[0] % len(ids)]
            st_[0] += 1
            return ps[i], f"ps{i}"
        return f

    t1_d = nc.dram_tensor("t1_scr", [S, D], F32, kind="Internal").ap()
    h2b = A.alloc([NT, D], BF16)
    logits = A.alloc([NT, 36], F32)
    mark2a = A.off
    NB2A = 4
    zt = [A.alloc([D], F32) for _ in range(NB2A)]
    h2f = [A.alloc([D], F32) for _ in range(NB2A)]
    h2T = [A.alloc([8, 128], F32) for _ in range(NB2A)]
    t1s = [A.alloc([D], F32) for _ in range(NB2A)]
    nps2a_f = [mk_nps([0, 1]), mk_nps([2, 3])]
    nps2a_b = [mk_nps([4]), mk_nps([5])]

    def front2a(j):
        b = j % NB2A
        mynps = nps2a_f[j % 2]
        z_, hz = zt[b], f"zt{b}"
        hf, hhf = h2f[b], f"h2f{b}"
        hT_, hhT = h2T[b], f"h2T{b}"
        t1_, ht1 = t1s[b], f"t1s{b}"
        P.rec()
        P.op("sp", lambda e: e.dma_start(out=z_, in_=z_d[j * 128:(j + 1) * 128, :]), r=[f"z_d{j}"], w=[hz], dma=True)
        P.op("dve", lambda e: e.tensor_tensor(out=hf, in0=z_, in1=A2, op=ALU.mult), r=[hz, "A2"], w=[hhf])
        P.op("dve", lambda e: e.tensor_tensor(out=hf, in0=hf, in1=B2, op=ALU.add), r=[hhf, "B2"], w=[hhf])
        P.op("pool", lambda e: e.tensor_tensor(out=t1_, in0=z_, in1=GA, op=ALU.mult), r=[hz, "GA"], w=[ht1])
        P.op("dve", lambda e: e.tensor_tensor(out=t1_, in0=t1_, in1=BA, op=ALU.add), r=[ht1, "BA"], w=[ht1])
        P.op("sp", lambda e: e.dma_start(out=t1_d[j * 128:(j + 1) * 128, :], in_=t1_), r=[ht1], w=[f"t1_d{j}"], dma=True)
        P.op("act", lambda e: e.activation(out=h2b[:, j, :], in_=hf, func=AF.Identity), r=[hhf], w=[f"h2b{j}"])
        for hh in range(2):
            pt, hp = mynps()
            for c4 in range(4):
                c = hh * 4 + c4
                P.op("pe", lambda e, pt=pt, c=c, c4=c4: e.transpose(out=pt[:, c4 * 128:(c4 + 1) * 128],
                                                                  in_=hf[:, c * 128:(c + 1) * 128], identity=identf),
                     r=[hhf, "cst"], w=[hp])
            P.op("act", lambda e, pt=pt, hh=hh: e.activation(out=hT_[:, hh * 4:(hh + 1) * 4, :].rearrange("p c t -> p (c t)"),
                                                             in_=pt[:], func=AF.Identity), r=[hp], w=[hhT + "ab"[hh]])
        return P.end()

    def back2a(j):
        b = j % NB2A
        hT_, hhT = h2T[b], f"h2T{b}"
        P.rec()
        pl, hpl = nps2a_b[j % 2]()
        for c in range(8):
            P.op("pe", lambda e, c=c: e.matmul(pl[:, 0:36], lhsT=hT_[:, c, :], rhs=wr[:, c, :], start=(c == 0), stop=(c == 7)),
                 r=[hhT + "a", hhT + "b", "wr"], w=[hpl])
        P.op("dve", lambda e: e.tensor_tensor(out=logits[:, j, :], in0=pl[:, 0:36], in1=br_bc, op=ALU.add),
             r=[hpl, "misc"], w=["logits"])
        return P.end()

    NP2A = NT // 2
    P.play(merge(front2a(0), front2a(1)))
    for k in range(NP2A):
        blk = merge(back2a(2 * k), back2a(2 * k + 1))
        if k + 1 < NP2A:
            blk = merge(blk, merge(front2a(2 * k + 2), front2a(2 * k + 3)))
        P.play(blk)
    P.fence()
    A.off = mark2a

    def T3(n):
        return A.alloc([NT, n], F32)
    gmax = A.alloc([NT], F32)
    og = T3(4)
    eg = T3(4)
    sgm = A.alloc([NT], F32)
    ptop = A.alloc([NT], F32)
    tmp4 = A.alloc([NT, 4, 8], F32)
    sel = T3(8)
    sel2 = T3(8)
    m1 = A.alloc([NT], F32)
    m2 = A.alloc([NT], F32)
    o1 = T3(8)
    o2 = T3(8)
    e2 = A.alloc([NT], F32)
    r12 = A.alloc([NT], F32)
    O1 = A.alloc([NT, 4, 8], F32)
    O2 = A.alloc([NT, 4, 8], F32)
    Obf = A.alloc([NT * 32], BF16)
    totA = A.alloc([NT, 32], F32)
    totB = A.alloc([NT, 32], F32)
    tot0 = A.alloc([NT, 32], F32)
    base = A.alloc([NT, 32], F32)
    cnt = A.alloc([32], F32)
    cmpc = A.alloc([32, 24], F32)
    pcnt = A.alloc([32], F32)
    oeA = A.alloc([32], F32)
    oeB = A.alloc([32], F32)
    offs = A.alloc([32], F32)
    cmps = A.alloc([NS, 32], F32)
    esl = A.alloc([64], F32)
    used = A.alloc([64], F32)
    posf = A.alloc([2, NT], F32)

    LG = logits[:, :, 0:4]
    LE4 = logits[:, :, 4:36].rearrange("p j (g e) -> p j g e", g=4)

    def dv(fn, r, w):
        P.op("dve", fn, r=r, w=w)

    dv(lambda e: e.tensor_reduce(out=gmax, in_=LG, axis=AX.X, op=ALU.max), ["logits"], ["gmax"])
    dv(lambda e: e.tensor_tensor(out=og, in0=LG, in1=gmax.unsqueeze(2).to_broadcast([128, NT, 4]), op=ALU.is_equal), ["logits", "gmax"], ["og"])
    dv(lambda e: e.tensor_tensor(out=eg, in0=LG, in1=gmax.unsqueeze(2).to_broadcast([128, NT, 4]), op=ALU.subtract), ["logits", "gmax"], ["eg"])
    P.op("act", lambda e: e.activation(out=eg, in_=eg, func=AF.Exp), r=["eg"], w=["eg"])
    dv(lambda e: e.tensor_reduce(out=sgm, in_=eg, axis=AX.X, op=ALU.add), ["eg"], ["sgm"])
    dv(lambda e: e.reciprocal(out=ptop, in_=sgm), ["sgm"], ["ptop"])
    dv(lambda e: e.tensor_tensor(out=tmp4, in0=LE4, in1=og.unsqueeze(3).to_broadcast([128, NT, 4, 8]), op=ALU.mult), ["logits", "og"], ["tmp4"])
    dv(lambda e: e.tensor_reduce(out=sel, in_=tmp4.rearrange("p j g e -> p j e g"), axis=AX.X, op=ALU.add), ["tmp4"], ["sel"])
    dv(lambda e: e.tensor_reduce(out=m1, in_=sel, axis=AX.X, op=ALU.max), ["sel"], ["m1"])
    dv(lambda e: e.tensor_tensor(out=o1, in0=sel, in1=m1.unsqueeze(2).to_broadcast([128, NT, 8]), op=ALU.is_equal), ["sel", "m1"], ["o1"])
    dv(lambda e: e.scalar_tensor_tensor(out=sel2.rearrange("p j e -> p (j e)"), in0=o1.rearrange("p j e -> p (j e)"), scalar=-1.0e9,
                                        in1=sel.rearrange("p j e -> p (j e)"), op0=ALU.mult, op1=ALU.add), ["o1", "sel"], ["sel2"])
    dv(lambda e: e.tensor_reduce(out=m2, in_=sel2, axis=AX.X, op=ALU.max), ["sel2"], ["m2"])
    dv(lambda e: e.tensor_tensor(out=o2, in0=sel2, in1=m2.unsqueeze(2).to_broadcast([128, NT, 8]), op=ALU.is_equal), ["sel2", "m2"], ["o2"])
    dv(lambda e: e.tensor_tensor(out=e2, in0=m2, in1=m1, op=ALU.subtract), ["m1", "m2"], ["e2"])
    P.op("act", lambda e: e.activation(out=e2, in_=e2, func=AF.Exp), r=["e2"], w=["e2"])
    dv(lambda e: e.tensor_scalar(out=r12, in0=e2, scalar1=1.0, scalar2=None, op0=ALU.add), ["e2"], ["r12"])
    dv(lambda e: e.reciprocal(out=r12, in_=r12), ["r12"], ["r12"])
    dv(lambda e: e.tensor_tensor(out=wts[:, 0, :], in0=r12, in1=ptop, op=ALU.mult), ["r12", "ptop"], ["wts"])
    dv(lambda e: e.tensor_tensor(out=wts[:, 1, :], in0=wts[:, 0, :], in1=e2, op=ALU.mult), ["wts", "e2"], ["wts"])
    dv(lambda e: e.tensor_tensor(out=O1, in0=og.unsqueeze(3).to_broadcast([128, NT, 4, 8]),
                                 in1=o1.unsqueeze(2).to_broadcast([128, NT, 4, 8]), op=ALU.mult), ["og", "o1"], ["O1"])
    dv(lambda e: e.tensor_tensor(out=O2, in0=og.unsqueeze(3).to_broadcast([128, NT, 4, 8]),
                                 in1=o2.unsqueeze(2).to_broadcast([128, NT, 4, 8]), op=ALU.mult), ["og", "o2"], ["O2"])
    O1f = O1.rearrange("p j g e -> p (j g e)")
    O2f = O2.rearrange("p j g e -> p (j g e)")
    dv(lambda e: e.tensor_tensor(out=Obf, in0=O1f, in1=O2f, op=ALU.add), ["O1", "O2"], ["Obf"])
    pcs, pts = [], []
    for h in range(2):
        pc_, hpc = nps()
        P.op("pe", lambda e, pc_=pc_, h=h: e.matmul(pc_[:], lhsT=Ub, rhs=Obf[:, h * 512:(h + 1) * 512], start=True, stop=True),
             r=["Ub", "Obf"], w=[hpc])
        pcs.append((pc_, hpc))
        pt_, hpt = nps()
        P.op("pe", lambda e, pt_=pt_, h=h: e.matmul(pt_[:], lhsT=onesb, rhs=Obf[:, h * 512:(h + 1) * 512], start=True, stop=True),
             r=["onesb", "Obf"], w=[hpt])
        pts.append((pt_, hpt))
    tot0f = tot0.rearrange("p j e -> p (j e)")
    for h in range(2):
        dv(lambda e, h=h: e.tensor_copy(out=tot0f[:, h * 512:(h + 1) * 512], in_=pts[h][0][:]), [pts[h][1]], ["tot0"])
    cur, hc = tot0, "tot0"
    for i_, sft in enumerate((1, 2, 4, 8, 16)):
        nxt, hn = (totA, "totA") if i_ % 2 == 0 else (totB, "totB")
        dv(lambda e, cur=cur, nxt=nxt, sft=sft: e.tensor_tensor(out=nxt[:, sft:, :], in0=cur[:, sft:, :], in1=cur[:, :NT - sft, :], op=ALU.add),
           [hc], [hn])
        dv(lambda e, cur=cur, nxt=nxt, sft=sft: e.tensor_copy(out=nxt[:, :sft, :], in_=cur[:, :sft, :]), [hc, hn], [hn])
        cur, hc = nxt, hn
    incl, hincl = cur, hc
    dv(lambda e: e.tensor_copy(out=cnt, in_=incl[:, NT - 1, :]), [hincl], ["cnt"])
    dv(lambda e: e.tensor_tensor(out=cmpc, in0=cnt.unsqueeze(2).to_broadcast([128, 32, 24]),
                                 in1=slot_bc[:, 0:24].unsqueeze(1).to_broadcast([128, 32, 24]), op=ALU.is_gt), ["cnt", "misc"], ["cmpc"])
    dv(lambda e: e.tensor_reduce(out=pcnt, in_=cmpc, axis=AX.X, op=ALU.add), ["cmpc"], ["pcnt"])
    dv(lambda e: e.tensor_scalar(out=pcnt, in0=pcnt, scalar1=float(T), scalar2=None, op0=ALU.mult), ["pcnt"], ["pcnt"])
    cur, hc = pcnt, "pcnt"
    for i_, sft in enumerate((1, 2, 4, 8, 16)):
        nxt, hn = (oeA, "oeA") if i_ % 2 == 0 else (oeB, "oeB")
        dv(lambda e, cur=cur, nxt=nxt, sft=sft: e.tensor_tensor(out=nxt[:, sft:], in0=cur[:, sft:], in1=cur[:, :32 - sft], op=ALU.add), [hc], [hn])
        dv(lambda e, cur=cur, nxt=nxt, sft=sft: e.tensor_copy(out=nxt[:, :sft], in_=cur[:, :sft]), [hc, hn], [hn])
        cur, hc = nxt, hn
    oend, hoend = cur, hc
    dv(lambda e: e.tensor_tensor(out=offs, in0=oend, in1=pcnt, op=ALU.subtract), [hoend, "pcnt"], ["offs"])
    dv(lambda e: e.tensor_tensor(out=base, in0=incl, in1=tot0, op=ALU.subtract), [hincl, "tot0"], ["base"])
    dv(lambda e: e.tensor_tensor(out=base, in0=base, in1=offs.unsqueeze(1).to_broadcast([128, NT, 32]), op=ALU.add), ["base", "offs"], ["base"])
    basef = base.rearrange("p j e -> p (j e)")
    for h in range(2):
        dv(lambda e, h=h: e.tensor_tensor(out=basef[:, h * 512:(h + 1) * 512], in0=pcs[h][0][:], in1=basef[:, h * 512:(h + 1) * 512], op=ALU.add),
           [pcs[h][1], "base"], ["base"])
    for k, (Ok, hO) in enumerate(((O1, "O1"), (O2, "O2"))):
        dv(lambda e, Ok=Ok: e.tensor_tensor(out=Ok.rearrange("p j g e -> p (j g e)"), in0=Ok.rearrange("p j g e -> p (j g e)"), in1=basef, op=ALU.mult),
           [hO, "base"], [hO])
        dv(lambda e, Ok=Ok, k=k: e.tensor_reduce(out=posf[:, k, :], in_=Ok.rearrange("p j g e -> p j (g e)"), axis=AX.X, op=ALU.add), [hO], ["posf"])
    dv(lambda e: e.tensor_copy(out=pos_i, in_=posf), ["posf"], ["pos_i"])
    dv(lambda e: e.tensor_tensor(out=cmps, in0=oend.unsqueeze(1).to_broadcast([128, NS, 32]),
                                 in1=slot_bc.unsqueeze(2).to_broadcast([128, NS, 32]), op=ALU.is_le), [hoend, "misc"], ["cmps"])
    dv(lambda e: e.tensor_reduce(out=esl[:, 0:NS], in_=cmps, axis=AX.X, op=ALU.add), ["cmps"], ["esl"])
    dv(lambda e: e.tensor_scalar(out=esl[:, 0:NS], in0=esl[:, 0:NS], scalar1=float(NE - 1), scalar2=128.0, op0=ALU.min, op1=ALU.mult), ["esl"], ["esl"])
    dv(lambda e: e.tensor_scalar(out=used[:, 0:NS], in0=slot_bc, scalar1=oend[:, 31:32], scalar2=None, op0=ALU.is_lt), ["misc", hoend], ["used"])
    dv(lambda e: e.tensor_scalar(out=used[:, 0:NS], in0=used[:, 0:NS], scalar1=-1.0e6, scalar2=1.0e6, op0=ALU.mult, op1=ALU.add), ["used"], ["used"])
    dv(lambda e: e.tensor_tensor(out=esl[:, 0:NS], in0=esl[:, 0:NS], in1=used[:, 0:NS], op=ALU.add), ["esl", "used"], ["esl"])
    dv(lambda e: e.tensor_scalar(out=esl[:, 0:NS], in0=esl[:, 0:NS], scalar1=cols[:, C_PID:C_PID + 1], scalar2=None, op0=ALU.add), ["esl", "cols"], ["esl"])
    dv(lambda e: e.tensor_copy(out=widx_i[:, 0:NS], in_=esl[:, 0:NS]), ["esl"], ["widx_i"])

    if dbg and dbg["what"] == "route":
        P.fence()
        P.op("sp", lambda e: e.dma_start(out=dbg_d[0:128, 0:NT * 36], in_=logits.rearrange("p j n -> p (j n)")), r=["logits"], w=["dbg0"], dma=True)
        P.op("sp", lambda e: e.dma_start(out=dbg_d[128:256, 0:2 * NT], in_=posf.rearrange("p k j -> p (k j)")), r=["posf"], w=["dbg1"], dma=True)
        P.op("sp", lambda e: e.dma_start(out=dbg_d[256:384, 0:2 * NT], in_=wts.rearrange("p k j -> p (k j)")), r=["wts"], w=["dbg2"], dma=True)
        P.op("sp", lambda e: e.dma_start(out=dbg_d[384:512, 0:NS], in_=esl[:, 0:NS]), r=["esl"], w=["dbg3"], dma=True)
        P.fence()
        raise _StopEmit()

    for j in range(NT):
        for k in range(2):
            P.op("pool", lambda e, j=j, k=k: e.indirect_dma_start(
                out=xs_d[:, :], out_offset=bass.IndirectOffsetOnAxis(ap=pos_i[:, k, j:j + 1], axis=0),
                in_=h2b[:, j, :], in_offset=None), r=[f"h2b{j}", "pos_i"], w=[f"xs_{j}_{k}"], dma=True)
    P.fence()

    A.off = mark2
    NBW = 4
    wgs = [A.alloc([2048], BF16) for _ in range(NBW)]
    wus = [A.alloc([2048], BF16) for _ in range(NBW)]
    wds = [A.alloc([2048], BF16) for _ in range(NBW)]
    xtok = [A.alloc([NSUB, D], BF16) for _ in range(NBW)]
    XT = [A.alloc([8, T], BF16) for _ in range(2)]
    sgs = [A.alloc([T], F32) for _ in range(2)]
    aT = [A.alloc([2, T], BF16) for _ in range(2)]
    NYO = 3
    yo = [A.alloc([D], F32) for _ in range(NYO)]
    npsB = mk_nps([0, 1, 2])
    npsC = mk_nps([3, 4, 5])
    _bc = {}

    def get_bc(e):
        if "v" not in _bc:
            reg = e.alloc_register("bcreg")
            e.reg_mov(reg, NE * 128 - 1)
            _bc["v"] = e.snap(reg, donate=True)
        return _bc["v"]

    ORDER = []
    for q in range((NS + 1) // 2):
        ORDER.append(q)
        if NS - 1 - q > q:
            ORDER.append(NS - 1 - q)
    assert sorted(ORDER) == list(range(NS))

    def load_w(i, which):
        s = ORDER[i]
        bw = i % NBW
        for (wsb, wdr, hn) in which(bw):
            P.op("pool", lambda e, wsb=wsb, wdr=wdr, s=s: e.indirect_dma_start(
                out=wsb, out_offset=None, in_=wdr[:, :],
                in_offset=bass.IndirectOffsetOnAxis(ap=widx_i[:, s:s + 1], axis=0),
                bounds_check=get_bc(e), oob_is_err=False), r=["widx_i"], w=[hn], dma=True)

    def w_gu(bw):
        return ((wgs[bw], wg_d, f"wg{bw}"), (wus[bw], wu_d, f"wu{bw}"))

    def w_d(bw):
        return ((wds[bw], wd_d, f"wd{bw}"),)

    def load_x(i):
        s = ORDER[i]
        bw = i % NBW
        for st in range(NSUB):
            r0 = s * T + st * 128
            P.op("sp", lambda e, bw=bw, st=st, r0=r0: e.dma_start(out=xtok[bw][:, st, :], in_=xs_d[r0:r0 + 128, :]),
                 w=[f"xtok{bw}_{st}"], dma=True)

    def stageA(i):
        s = ORDER[i]
        b, bw = i % 2, i % NBW
        P.rec()
        for st in range(NSUB):
            k = (i * NSUB + st) % 2
            pb_, hpb = psb[k], f"psb{k}"
            for c in range(8):
                P.op("pe", lambda e, pb_=pb_, st=st, c=c: e.transpose(out=pb_[:, c * 128:(c + 1) * 128],
                                                                     in_=xtok[bw][:, st, c * 128:(c + 1) * 128], identity=identb),
                     r=[f"xtok{bw}_{st}", "identb"], w=[hpb])
            if True:
                P.op("act", lambda e, pb_=pb_, st=st: e.activation(out=XT[b][:, :, st * 128:(st + 1) * 128],
                                                                   in_=pb_[:].rearrange("p (c t) -> p c t", c=8), func=AF.Identity),
                     r=[hpb], w=[f"XT{b}_{st}"])
            else:
                P.op("dve", lambda e, pb_=pb_, st=st: e.tensor_copy(out=XT[b][:, :, st * 128:(st + 1) * 128],
                                                                    in_=pb_[:].rearrange("p (c t) -> p c t", c=8)),
                     r=[hpb], w=[f"XT{b}_{st}"])
        return P.end()

    def stageB(i):
        s = ORDER[i]
        b, bw = i % 2, i % NBW
        XH = [f"XT{b}_{st}" for st in range(NSUB)]
        P.rec()
        for fch in range(2):
            pg, hpg = npsB()
            for c in range(8):
                P.op("pe", lambda e, pg=pg, c=c, fch=fch: e.matmul(
                    pg[:, 0:T], lhsT=wgs[bw][:, c * 256 + fch * 128:c * 256 + (fch + 1) * 128], rhs=XT[b][:, c, :],
                    start=(c == 0), stop=(c == 7)), r=[f"wg{bw}"] + XH, w=[hpg])
            P.op("act", lambda e, pg=pg: e.activation(out=sgs[b], in_=pg[:, 0:T], func=AF.Silu), r=[hpg], w=[f"sgs{b}"])
            pu, hpu = npsB()
            for c in range(8):
                P.op("pe", lambda e, pu=pu, c=c, fch=fch: e.matmul(
                    pu[:, 0:T], lhsT=wus[bw][:, c * 256 + fch * 128:c * 256 + (fch + 1) * 128], rhs=XT[b][:, c, :],
                    start=(c == 0), stop=(c == 7)), r=[f"wu{bw}"] + XH, w=[hpu])
            P.op("dve", lambda e, pu=pu, fch=fch: e.tensor_tensor(out=aT[b][:, fch, :], in0=pu[:, 0:T], in1=sgs[b], op=ALU.mult),
                 r=[hpu, f"sgs{b}"], w=[f"aT{b}_{fch}"])
        return P.end()

    def stageC(i):
        s = ORDER[i]
        b, bw = i % 2, i % NBW
        P.rec()
        for st in range(NSUB):
            yb = (i * NSUB + st) % NYO
            for half in range(2):
                po, hpo = npsC()
                for fch in range(2):
                    P.op("pe", lambda e, po=po, st=st, fch=fch, half=half: e.matmul(
                        po[:], lhsT=aT[b][:, fch, st * 128:(st + 1) * 128],
                        rhs=wds[bw][:, fch * 1024 + half * 512:fch * 1024 + (half + 1) * 512], start=(fch == 0), stop=(fch == 1)),
                        r=[f"aT{b}_0", f"aT{b}_1", f"wd{bw}"], w=[hpo])
                P.op("dve", lambda e, po=po, yb=yb, half=half: e.tensor_tensor(
                    out=yo[yb][:, half * 512:(half + 1) * 512], in0=po[:], in1=gbc[:, 3, half * 512:(half + 1) * 512], op=ALU.mult),
                    r=[hpo, "gbc3_0", "gbc3_1"], w=[f"yo{yb}{'ab'[half]}"])
            r0 = s * T + st * 128
            P.op("sp", lambda e, yb=yb, r0=r0: e.dma_start(out=ys_d[r0:r0 + 128, :], in_=yo[yb]), r=[f"yo{yb}a", f"yo{yb}b"],
                 w=[f"ys_{s}_{st}"], dma=True)
        return P.end()

    for s in range(min(NBW, NS)):
        load_x(s)
        load_w(s, w_gu)
        load_w(s, w_d)
    for i in range(NS + 2):
        lists = []
        if i < NS:
            lists.append(stageA(i))
        if 0 <= i - 1 < NS:
            lists.append(stageB(i - 1))
        if 0 <= i - 2 < NS:
            lists.append(stageC(i - 2))
        P.play(merge(*lists))
        if i + NBW < NS:
            load_x(i + NBW)
        if i - 1 >= 0 and i - 1 + NBW < NS:
            load_w(i - 1 + NBW, w_gu)
        if i - 2 >= 0 and i - 2 + NBW < NS:
            load_w(i - 2 + NBW, w_d)
    P.fence()

    A.off = mark2
    NB2F = 4
    Y1 = [A.alloc([D], F32) for _ in range(NB2F)]
    Y2 = [A.alloc([D], F32) for _ in range(NB2F)]
    t1 = [A.alloc([D], F32) for _ in range(NB2F)]
    ff = [A.alloc([D], F32) for _ in range(2)]
    r2 = [A.alloc([D], F32) for _ in range(2)]
    qq = [A.alloc([D], F32) for _ in range(2)]
    ob = [A.alloc([D], F32) for _ in range(2)]
    bn2 = [A.alloc([12], F32) for _ in range(2)]
    ag2 = [A.alloc([4], F32) for _ in range(2)]

    def loads2f(j):
        q = j % NB2F
        P.op("pool", lambda e: e.indirect_dma_start(
            out=Y1[q], out_offset=None, in_=ys_d[:, :], in_offset=bass.IndirectOffsetOnAxis(ap=pos_i[:, 0, j:j + 1], axis=0)),
            r=["pos_i"], w=[f"Y1{q}"], dma=True)
        P.op("pool", lambda e: e.indirect_dma_start(
            out=Y2[q], out_offset=None, in_=ys_d[:, :], in_offset=bass.IndirectOffsetOnAxis(ap=pos_i[:, 1, j:j + 1], axis=0)),
            r=["pos_i"], w=[f"Y2{q}"], dma=True)
        P.op("sp", lambda e: e.dma_start(out=t1[q], in_=t1_d[j * 128:(j + 1) * 128, :]), r=[f"t1_d{j}"], w=[f"t1{q}"], dma=True)

    def chain2f(j):
        b = j % 2
        q = j % NB2F
        P.rec()
        P.op("act", lambda e: e.activation(out=ff[b], in_=Y1[q], func=AF.Identity, scale=wts[:, 0, j:j + 1]), r=[f"Y1{q}", "wts"], w=[f"ff{b}"])
        P.op("dve", lambda e: e.scalar_tensor_tensor(out=ff[b], in0=Y2[q], scalar=wts[:, 1, j:j + 1], in1=ff[b], op0=ALU.mult, op1=ALU.add),
             r=[f"Y2{q}", "wts", f"ff{b}"], w=[f"ff{b}"])
        P.op("dve", lambda e: e.tensor_tensor(out=r2[b], in0=ff[b], in1=t1[q], op=ALU.add), r=[f"ff{b}", f"t1{q}"], w=[f"r2{b}"])
        for h in range(2):
            P.op("dve", lambda e, h=h: e.bn_stats(out=bn2[b][:, h * 6:(h + 1) * 6], in_=r2[b][:, h * 512:(h + 1) * 512]), r=[f"r2{b}"], w=[f"bn2{b}"])
        P.op("dve", lambda e: e.bn_aggr(out=ag2[b][:, 0:2], in_=bn2[b]), r=[f"bn2{b}"], w=[f"ag2{b}"])
        P.op("act", lambda e: e.activation(out=ag2[b][:, 2:3], in_=ag2[b][:, 1:2], func=AF.Sqrt, bias=EPSC, scale=1.0), r=[f"ag2{b}", "epsc"], w=[f"ag2b{b}"])
        P.op("dve", lambda e: e.reciprocal(out=ag2[b][:, 2:3], in_=ag2[b][:, 2:3]), r=[f"ag2b{b}"], w=[f"ag2b{b}"])
        P.op("dve", lambda e: e.scalar_tensor_tensor(out=ag2[b][:, 3:4], in0=ag2[b][:, 0:1], scalar=-1.0, in1=ag2[b][:, 2:3], op0=ALU.mult, op1=ALU.mult),
             r=[f"ag2{b}", f"ag2b{b}"], w=[f"ag2c{b}"])
        P.op("act", lambda e: e.activation(out=qq[b], in_=r2[b], func=AF.Identity, bias=ag2[b][:, 3:4], scale=ag2[b][:, 2:3]),
             r=[f"r2{b}", f"ag2b{b}", f"ag2c{b}"], w=[f"qq{b}"])
        P.op("pool", lambda e: e.tensor_tensor(out=qq[b], in0=qq[b], in1=lnr[:, 2, :], op=ALU.mult), r=[f"qq{b}", "lnr"], w=[f"qq{b}"])
        P.op("dve", lambda e: e.tensor_tensor(out=ob[b], in0=qq[b], in1=lnr[:, 3, :], op=ALU.add), r=[f"qq{b}", "lnr"], w=[f"ob{b}"])
        P.op("sp", lambda e: e.dma_start(out=out_d[j * 128:(j + 1) * 128, :], in_=ob[b]), r=[f"ob{b}"], w=[f"out{j}"], dma=True)
        return P.end()

    loads2f(0)
    loads2f(1)
    for j in range(0, NT, 2):
        if j + 2 < NT:
            loads2f(j + 2)
            loads2f(j + 3)
        P.play(merge(chain2f(j), chain2f(j + 1)))


class _StopEmit(Exception):
    pass


def _host_prep(inp, b):
    f = np.float32
    L = 0
    cols = np.zeros((128, NCOL), f)
    cols[:, C_C:C_C + 8] = inp["c"][b].reshape(8, 128).T
    w_in = inp["w_in"][L]
    b_in = inp["b_in"][L]
    qcols = np.concatenate([np.arange(1024 + h * 64, 1024 + (h + 1) * 64) for h in HORDER])
    perm = np.concatenate([np.arange(0, 1024), qcols, np.arange(1536, 1792)])
    w_in_p = np.ascontiguousarray(w_in[:, perm])
    b_in_p = b_in[perm]
    cols[:, C_BIN:C_BIN + 13] = b_in_p[:1664].reshape(13, 128).T
    cols[:, C_CW:C_CW + 124] = inp["conv_w"][L].T.reshape(4, 128, 31).transpose(1, 0, 2).reshape(128, 124)
    cols[:, C_CB:C_CB + 4] = inp["conv_b"][L].reshape(4, 128).T
    cols[:, C_LG:C_LG + 4] = inp["conv_ln_g"][L].reshape(4, 128).T
    cols[:, C_LB:C_LB + 4] = inp["conv_ln_b"][L].reshape(4, 128).T
    cols[:, C_OG:C_OG + 4] = inp["conv_out_g"][L].reshape(4, 128).T
    cols[:, C_PID] = np.arange(128)
    rows = np.zeros((1, NROW), f)
    rows[0, R_BADA:R_BADA + 6144] = inp["b_ada"][L]
    rows[0, R_BV:R_BV + 128] = b_in[1664:1792]
    rows[0, R_SINK:R_SINK + 8] = inp["sinks"][L][HORDER]
    rows[0, R_AOG:R_AOG + 512] = inp["attn_out_g"][L].reshape(8, 64)[HORDER].reshape(-1)
    rows[0, R_BOUT:R_BOUT + D] = inp["b_out"][L]
    rows[0, R_L1G:R_L1G + D] = inp["ln1_g"][L]
    rows[0, R_L1B:R_L1B + D] = inp["ln1_b"][L]
    rows[0, R_L2G:R_L2G + D] = inp["ln2_g"][L]
    rows[0, R_L2B:R_L2B + D] = inp["ln2_b"][L]
    rows[0, R_BR:R_BR + 4] = inp["b_router_group"][L]
    rows[0, R_BR + 4:R_BR + 36] = inp["b_router_expert"][L]
    rows[0, R_SLOT:R_SLOT + NS] = np.arange(NS) * T
    w_out = inp["w_out"][L]
    arows = np.concatenate([np.arange(512 + h * 64, 512 + (h + 1) * 64) for h in HORDER])
    w_out_p = np.ascontiguousarray(np.concatenate([w_out[:512], w_out[arows]], axis=0))
    w_r = np.ascontiguousarray(np.concatenate([inp["w_router_group"][L], inp["w_router_expert"][L]], axis=1))
    return dict(cols=cols, rows=rows, w_in=w_in_p, w_out=w_out_p, w_r=w_r)


def _consts():
    f = np.float32
    cst = np.zeros((128, NK), f)
    cst[:, K_ID:K_ID + 128] = np.eye(128)
    p = np.arange(128)
    cst[:, K_U:K_U + 128] = (p[:, None] < p[None, :])
    cst[:, K_BD:K_BD + 128] = ((p[:, None] // 64) == (p[None, :] // 64)) / 64.0
    m0 = np.where(p[:, None] > p[None, :], 0.0, NEG)
    m1 = np.where(p[:, None] <= p[None, :], 0.0, NEG)
    cst[:, K_M0:K_M0 + 512] = np.tile(m0, (1, 4))
    cst[:, K_M1:K_M1 + 512] = np.tile(m1, (1, 4))
    return cst


def _expert_layout(inp):
    L = 0
    wg = np.ascontiguousarray(inp["w_gate"][L].reshape(NE, 8, 128, DE).transpose(0, 2, 1, 3)).reshape(NE * 128, 2048)
    wu = np.ascontiguousarray(inp["w_up"][L].reshape(NE, 8, 128, DE).transpose(0, 2, 1, 3)).reshape(NE * 128, 2048)
    wd = np.ascontiguousarray(inp["w_down"][L].reshape(NE, 2, 128, D).transpose(0, 2, 1, 3)).reshape(NE * 128, 2048)
    return wg, wu, wd


_CACHE = {}


def kernel(**inputs):
    inp = {k: np.asarray(v) for k, v in inputs.items()}
    dbg = _CACHE.get("dbg")
    nc = build_program(dbg)
    cst = _consts()
    wg, wu, wd = _expert_layout(inp)
    w_ada = np.ascontiguousarray(inp["w_ada"][0])
    in_maps = []
    ncores = _CACHE.get("ncores", 8)
    for b in range(ncores):
        hp = _host_prep(inp, b)
        m = dict(x=np.ascontiguousarray(inp["x"][b]), cols=hp["cols"], rows=hp["rows"], cst=cst, w_ada=w_ada,
                 w_in=hp["w_in"], w_out=hp["w_out"], w_r=hp["w_r"], wg=wg, wu=wu, wd=wd)
        if _CACHE.get("p2only"):
            m["z_in"] = _CACHE["z_in"]
        in_maps.append(m)
    if _CACHE.get("trace"):
        res = run_bass_kernel_spmd(nc, in_maps, core_ids=list(range(ncores)), trace=True)
        print("EXEC_NS", res.exec_time_ns)
    else:
        res = run_bass_kernel_spmd(nc, in_maps, core_ids=list(range(ncores)))
    if dbg:
        return [np.asarray(r["dbg"]) for r in res.results]
    out = np.stack([np.asarray(r["out"]) for r in res.results], axis=0).astype(np.float32)
    return out
```

```python
import os
import numpy as np
import concourse.bass as bass
import concourse.mybir as mybir
from concourse.bass_utils import run_bass_kernel_spmd

F32 = mybir.dt.float32
BF16 = mybir.dt.bfloat16
I32 = mybir.dt.int32
ALU = mybir.AluOpType
AF = mybir.ActivationFunctionType
AX = mybir.AxisListType

D = 1024
S = 4096
NT = S // 128
NMT = S // 512
DIN = 1792
NE = 32
DE = 256
ALPHA = 2.0 ** 0.25
EPS = 1e-5
NEG = -30000.0
T = 384
NS = (2 * S + NE * (T - 1) + T - 1) // T
NSUB = T // 128
HORDER = [0, 4, 1, 5, 2, 6, 3, 7]

C_C = 0
C_BIN = 8
C_CW = 21
C_CB = 145
C_LG = 149
C_LB = 153
C_OG = 157
C_PID = 161
NCOL = 162
R_BADA = 0
R_BV = 6144
R_SINK = 6272
R_AOG = 6280
R_BOUT = 6792
R_L1G = 7816
R_L1B = 8840
R_L2G = 9864
R_L2B = 10888
R_BR = 11912
R_SLOT = 11948
NROW = 12012
K_ID = 0
K_U = 128
K_BD = 256
K_M0 = 384
K_M1 = 896
NK = 1408


class Prog:
    def __init__(self, nc, sems):
        self.nc = nc
        self.ops = []
        self.last_w = {}
        self.readers = {}
        self.eng_sem = {e: sems[i] for i, e in enumerate(["pe", "act", "dve", "pool"])}
        rest = sems[4:]
        n_sp = (len(rest) * 5) // 10
        n_pool = (len(rest) * 4) // 10
        self.dma_pool = {"sp": rest[:n_sp], "pool": rest[n_sp:n_sp + n_pool], "act": rest[n_sp + n_pool:]}
        self._rec = None

    def rec(self):
        assert self._rec is None
        self._rec = []

    def end(self):
        l = self._rec
        self._rec = None
        return l

    def play(self, lst):
        assert self._rec is None
        for o in lst:
            self.op(*o)

    def op(self, eng, fn, r=(), w=(), dma=False):
        if self._rec is not None:
            self._rec.append((eng, fn, list(r), list(w), dma))
            return None
        i = len(self.ops)
        w = list(w) + [h for h in r if h.startswith("ps") and h not in w]
        raw, oth = set(), set()
        for h in r:
            if h in self.last_w:
                raw.add(self.last_w[h])
        for h in w:
            if h in self.last_w:
                oth.add(self.last_w[h])
            for j in self.readers.get(h, ()):
                oth.add(j)
        for h in w:
            self.last_w[h] = i
            self.readers[h] = []
        for h in r:
            self.readers.setdefault(h, []).append(i)
        deps = []
        for j in sorted(raw | oth):
            p = self.ops[j]
            if j == i:
                continue
            if (not p["dma"]) and p["eng"] == eng:
                if eng == "pe" or j not in raw:
                    continue
            deps.append(j)
        self.ops.append(dict(eng=eng, fn=fn, deps=deps, dma=dma, sig=False))
        for j in deps:
            self.ops[j]["sig"] = True
        return i

    def fence(self, engs=("pe", "act", "dve", "pool", "sp")):
        hs = list(self.last_w.keys())
        for e in engs:
            self.op(e, None, r=hs, w=["_fence_" + e])

    def emit(self):
        nc = self.nc
        ticket = {e: 0 for e in self.eng_sem}
        dma_next = {q: 0 for q in self.dma_pool}
        dma_uses = {}
        for o in self.ops:
            if o["dma"]:
                q = o["eng"]
                pool = self.dma_pool[q]
                sem = pool[dma_next[q] % len(pool)]
                dma_next[q] += 1
                u = dma_uses.get(id(sem), 0)
                o["pre"] = (sem, 16 * u)
                dma_uses[id(sem)] = u + 1
                o["ev"] = (sem, 16 * (u + 1))
            elif o["sig"] and o["fn"] is not None:
                ticket[o["eng"]] += 1
                o["ev"] = (self.eng_sem[o["eng"]], ticket[o["eng"]])
        ops = self.ops

        def run(engname, eobj):
            waited = {}

            def wait(sem, val):
                if val <= 0:
                    return
                if waited.get(id(sem), 0) >= val:
                    return
                eobj.wait_ge(sem, val)
                waited[id(sem)] = val

            for o in ops:
                if o["eng"] != engname:
                    continue
                for j in o["deps"]:
                    ev = ops[j].get("ev")
                    if ev is not None:
                        wait(*ev)
                if o["fn"] is None:
                    continue
                if o["dma"]:
                    wait(*o["pre"])
                    ins = o["fn"](eobj)
                    ins.then_inc(o["ev"][0], 16)
                else:
                    ins = o["fn"](eobj)
                    if o["sig"]:
                        ins.then_inc(o["ev"][0], 1)

        with nc.Block() as block:
            @block.tensor
            def _(e):
                run("pe", e)

            @block.scalar
            def _(e):
                run("act", e)

            @block.vector
            def _(e):
                run("dve", e)

            @block.gpsimd
            def _(e):
                run("pool", e)

            @block.sync
            def _(e):
                run("sp", e)


class _Stop(Exception):
    pass


def merge(*lists):
    lists = [l for l in lists if l]
    out = []
    idx = [0] * len(lists)
    total = sum(len(l) for l in lists)
    while len(out) < total:
        best, bv = None, None
        for k, l in enumerate(lists):
            if idx[k] < len(l):
                v = (idx[k] + 0.5) / len(l)
                if bv is None or v < bv:
                    best, bv = k, v
        out.append(lists[best][idx[best]])
        idx[best] += 1
    return out


class Arena:
    def __init__(self, t, nbytes):
        self.t = t
        self.nbytes = nbytes
        self.off = 0

    def alloc(self, shape, dt):
        esz = 4 if dt in (F32, I32) else 2
        n = int(np.prod(shape)) * esz
        n = (n + 63) // 64 * 64
        assert self.off + n <= self.nbytes, ("arena overflow", self.off, n, self.nbytes)
        v = self.t[:, self.off // 4:(self.off + n) // 4]
        self.off += n
        if dt != F32:
            v = v.bitcast(dt)
        v = v[:, 0:int(np.prod(shape))]
        if len(shape) == 2:
            return v.rearrange("p (a b) -> p a b", a=shape[0])
        if len(shape) == 3:
            return v.rearrange("p (a b c) -> p a b c", a=shape[0], b=shape[1])
        return v


def build_program(dbg=None):
    nc = bass.Bass("TRN2", target_bir_lowering=False)
    try:
        return _build_program(nc, dbg)
    except _Stop:
        return nc


def _build_program(nc, dbg=None):
    dr = {}

    def din(name, shape, dt=F32):
        dr[name] = nc.dram_tensor(name, list(shape), dt, kind="ExternalInput").ap()
        return dr[name]

    x_d = din("x", [S, D])
    cols_d = din("cols", [128, NCOL])
    rows_d = din("rows", [1, NROW])
    cst_d = din("cst", [128, NK])
    wada_d = din("w_ada", [D, 6 * D])
    win_d = din("w_in", [D, DIN])
    wout_d = din("w_out", [D, D])
    wr_d = din("w_r", [D, 36])
    wg_d = din("wg", [NE * 128, 2048])
    wu_d = din("wu", [NE * 128, 2048])
    wd_d = din("wd", [NE * 128, 2048])
    out_d = nc.dram_tensor("out", [S, D], F32, kind="ExternalOutput").ap()
    if _CACHE.get("p2only"):
        z_d = din("z_in", [S, D])
    else:
        z_d = nc.dram_tensor("z_scr", [S, D], F32, kind="Internal").ap()
    xs_d = nc.dram_tensor("xs_scr", [NS * T, D], BF16, kind="Internal").ap()
    ys_d = nc.dram_tensor("ys_scr", [NS * T, D], F32, kind="Internal").ap()
    dbg_d = None
    if dbg:
        dbg_d = nc.dram_tensor("dbg", list(dbg["shape"]), F32, kind="ExternalOutput").ap()

    import contextlib
    with contextlib.ExitStack() as st:
        ARENA_BYTES = 206 * 1024
        arena_t = st.enter_context(nc.sbuf_tensor("arena", [128, ARENA_BYTES // 4], F32))
        ps = [st.enter_context(nc.psum_tensor(f"ps{i}", [128, 512], F32)) for i in range(6)]
        psb = [st.enter_context(nc.psum_tensor(f"psb{i}", [128, 1024], BF16)) for i in range(2)]
        sems = [st.enter_context(nc.semaphore(f"s{i}")) for i in range(_CACHE.get("nsem", 48))]
        P = Prog(nc, sems)
        A = Arena(arena_t, ARENA_BYTES)
        psn = [0]

        def nps():
            i = psn[0] % 6
            psn[0] += 1
            return ps[i], f"ps{i}"

        cols = A.alloc([NCOL], F32)
        identf = A.alloc([128], F32)
        identb = A.alloc([128], BF16)
        onesb = A.alloc([128], BF16)
        ones512 = A.alloc([128], BF16)
        bdb = A.alloc([128], BF16)
        Ub = A.alloc([128], BF16)
        maskT = A.alloc([2, 512], BF16)
        modc = A.alloc([32], F32)
        g1bc = A.alloc([D], F32)
        gbrow = A.alloc([D], BF16)
        EPSC = A.alloc([1], F32)
        EPSC2 = A.alloc([1], F32)
        mark_persist = A.off
        cst = A.alloc([NK], F32)
        gbc = A.alloc([4, D], F32)
        mod_d = nc.dram_tensor("mod_scr", [1, 3 * D], F32, kind="Internal").ap()

        P.op("sp", lambda e: e.dma_start(out=cols, in_=cols_d), w=["cols"], dma=True)
        P.op("sp", lambda e: e.dma_start(out=cst, in_=cst_d), w=["cst0"], dma=True)
        P.op("sp", lambda e: e.dma_start(out=identf, in_=cst_d[:, K_ID:K_ID + 128]), w=["cst"], dma=True)
        P.op("dve", lambda e: e.tensor_copy(out=identb, in_=identf), r=["cst"], w=["identb"])
        P.op("dve", lambda e: e.memset(onesb, 1.0), w=["onesb"])
        P.op("dve", lambda e: e.memset(EPSC, EPS), w=["epsc"])
        P.op("dve", lambda e: e.memset(EPSC2, EPS / (ALPHA * ALPHA)), w=["epsc2"])
        P.op("dve", lambda e: e.memset(ones512, 1.0 / 512.0), w=["ones512"])
        P.op("dve", lambda e: e.tensor_copy(out=bdb, in_=cst[:, K_BD:K_BD + 128]), r=["cst0"], w=["bdb"])
        P.op("dve", lambda e: e.tensor_copy(out=Ub, in_=cst[:, K_U:K_U + 128]), r=["cst0"], w=["Ub"])
        P.op("dve", lambda e: e.tensor_copy(out=maskT.rearrange("p a b -> p (a b)"), in_=cst[:, K_M0:K_M0 + 1024]),
             r=["cst0"], w=["maskT"])

        ph0 = A.off
        cact = A.alloc([8], F32)
        cbc = A.alloc([8, 128], BF16)
        wab = [A.alloc([8, 512], BF16) for _ in range(2)]
        badab = A.alloc([512], F32)
        modb = A.alloc([4 * D], F32)
        P.op("act", lambda e: e.activation(out=cact, in_=cols[:, C_C:C_C + 8], func=AF.Silu), r=["cols"], w=["cact"])
        for c in range(8):
            P.op("dve", lambda e, c=c: e.tensor_scalar(out=cbc[:, c, :], in0=onesb, scalar1=cact[:, c:c + 1],
                                                       scalar2=None, op0=ALU.mult),
                 r=["cact", "onesb"], w=["cbc"])
        for blk in range(12):
            wb = wab[blk % 2]
            hw = f"wab{blk % 2}"
            P.op("pool", lambda e, blk=blk, wb=wb: e.dma_start(
                out=wb, in_=wada_d[:, blk * 512:(blk + 1) * 512].rearrange("(c p) n -> p c n", p=128)),
                w=[hw], dma=True)
            P.op("sp", lambda e, blk=blk: e.dma_start(
                out=badab, in_=rows_d[0:1, R_BADA + blk * 512:R_BADA + (blk + 1) * 512].partition_broadcast(128)),
                w=["badab"], dma=True)
            pt, hp = nps()
            for c in range(8):
                P.op("pe", lambda e, c=c, pt=pt, wb=wb: e.matmul(pt[:], lhsT=cbc[:, c, :], rhs=wb[:, c, :],
                                                               start=(c == 0), stop=(c == 7)),
                     r=["cbc", hw], w=[hp])
            if blk < 4:
                dst = modb[:, blk * 512:(blk + 1) * 512]
                hd = f"modb{blk}"
            else:
                gi = (blk - 4) // 2
                dst = gbc[:, gi, ((blk - 4) % 2) * 512:((blk - 4) % 2 + 1) * 512]
                hd = f"gbc{gi}_{blk % 2}"
            P.op("dve", lambda e, pt=pt, dst=dst: e.tensor_tensor(out=dst, in0=pt[:], in1=badab, op=ALU.add),
                 r=[hp, "badab"], w=[hd])
        srcs = []
        for c in range(8):
            srcs.append((modb[:, c * 128:(c + 1) * 128], f"modb{c // 4}", c, 0.0))
        for c in range(8):
            srcs.append((modb[:, 1024 + c * 128:1024 + (c + 1) * 128], f"modb{2 + c // 4}", 8 + c, 1.0))
        for c in range(8):
            srcs.append((gbc[:, 1, c * 128:(c + 1) * 128], f"gbc1_{c // 4}", 16 + c, 0.0))
        for c in range(8):
            srcs.append((gbc[:, 2, c * 128:(c + 1) * 128], f"gbc2_{c // 4}", 24 + c, 1.0))
        for (src, hs, col, add) in srcs:
            pt, hp = nps()
            P.op("pe", lambda e, pt=pt, src=src: e.transpose(out=pt[:, 0:128], in_=src, identity=identf),
                 r=[hs, "cst"], w=[hp])
            P.op("dve", lambda e, pt=pt, col=col, add=add: e.tensor_scalar(
                out=modc[:, col:col + 1], in0=pt[:, 0:1], scalar1=add, scalar2=None, op0=ALU.add),
                r=[hp], w=["modc"])
        G_ALL = [f"gbc{gi}_{h}" for gi in range(4) for h in range(2)]
        P.op("sp", lambda e: e.dma_start(out=mod_d, in_=gbc[0:1, 1:4, :].rearrange("p a d -> p (a d)")), r=G_ALL, w=["mod_d"], dma=True)
        P.op("dve", lambda e: e.tensor_scalar(out=g1bc, in0=gbc[:, 0, :], scalar1=1.0 / ALPHA, scalar2=None, op0=ALU.mult), r=G_ALL, w=["g1bc"])
        boutb = modb[:, 0:D]
        P.op("sp", lambda e: e.dma_start(out=boutb, in_=rows_d[0:1, R_BOUT:R_BOUT + D].partition_broadcast(128)),
             w=["modb0", "modb1"], dma=True)
        P.op("dve", lambda e: e.tensor_tensor(out=boutb, in0=boutb, in1=g1bc, op=ALU.mult), r=["modb0", "modb1", "g1bc"], w=["modb0", "modb1"])
        P.op("dve", lambda e: e.tensor_copy(out=gbrow, in_=boutb), r=["modb0", "modb1"], w=["gbrow"])
        P.fence()
        if _CACHE.get("stop") == 0:
            P.op("sp", lambda e: e.dma_start(out=dbg_d[0:128, 0:32], in_=modc), r=["modc"], w=["dbg"], dma=True)
            P.op("sp", lambda e: e.dma_start(out=dbg_d[128:256, :], in_=gbc[:, 0, :]), r=["gbc0_0", "gbc0_1"], w=["dbg2"], dma=True)
            P.fence()
            P.emit()
            return nc
        A.off = mark_persist

        win = A.alloc([8, DIN], BF16)
        modc2 = A.alloc([32], F32)
        P.op("dve", lambda e: e.tensor_copy(out=modc2, in_=modc), r=["modc"], w=["modc2"])
        wout = A.alloc([8, D], BF16)
        diag = A.alloc([124, 128], BF16)
        xt = A.alloc([4, D], BF16)
        hT = A.alloc([8, 512], BF16)
        vr = [A.alloc([4, 542], BF16) for _ in range(2)]
        kr = [[A.alloc([640], BF16) for _ in range(2)] for _ in range(2)]
        va = [A.alloc([5, 130], BF16) for _ in range(2)]
        qT2 = [A.alloc([4, 512], BF16) for _ in range(2)]
        sig = A.alloc([512], F32)
        ybf2 = [A.alloc([4, 512], BF16) for _ in range(2)]
        y2bf = A.alloc([4, 512], BF16)
        rstd2 = [A.alloc([512], F32) for _ in range(2)]
        nmr2 = [A.alloc([512], F32) for _ in range(2)]
        zc2 = [A.alloc([512], F32) for _ in range(2)]
        sc2_ = [A.alloc([512], F32) for _ in range(2)]
        s2bf2 = [A.alloc([512], BF16) for _ in range(2)]
        r2c2 = [A.alloc([512], F32) for _ in range(2)]
        ycT = A.alloc([8, 512], BF16)
        mx2 = [A.alloc([8], F32) for _ in range(2)]
        mxb2 = [A.alloc([8], BF16) for _ in range(2)]
        nmx2 = [A.alloc([8], F32) for _ in range(2)]
        dcat2 = [A.alloc([2, 512], BF16) for _ in range(2)]
        ET2 = [A.alloc([4, 512], BF16) for _ in range(2)]
        es_t2 = [A.alloc([8], F32) for _ in range(2)]
        den2 = [A.alloc([8], F32) for _ in range(2)]
        osb2 = [A.alloc([8, 64], F32) for _ in range(2)]
        osq2 = [A.alloc([8, 64], F32) for _ in range(2)]
        ssq2 = [A.alloc([8], F32) for _ in range(2)]
        yat2 = [A.alloc([512], BF16) for _ in range(2)]
        rows_sb = A.alloc([128 + 8 + 512], F32)
        xr2 = [A.alloc([D], F32) for _ in range(2)]
        bnst2 = [A.alloc([12], F32) for _ in range(2)]
        bnag2 = [A.alloc([4], F32) for _ in range(2)]
        print("phase1 arena bytes", A.off)

        def mk_nps(ids):
            st_ = [0]

            def f():
                i = ids[st_[0] % len(ids)]
                st_[0] += 1
                return ps[i], f"ps{i}"
            return f

        bv_bc = rows_sb[:, 0:128]
        sink_bc = rows_sb[:, 128:136]
        aog_bc = rows_sb[:, 136:648]
        P.op("sp", lambda e: e.dma_start(out=rows_sb, in_=rows_d[0:1, R_BV:R_BV + 128 + 8 + 512].partition_broadcast(128)),
             w=["rows_sb"], dma=True)
        for c in range(8):
            P.op("pool", lambda e, c=c: e.dma_start(out=win[:, c, :], in_=win_d[c * 128:(c + 1) * 128, :]), w=[f"win{c}"], dma=True)
            P.op("pool", lambda e, c=c: e.dma_start(out=wout[:, c, :], in_=wout_d[c * 128:(c + 1) * 128, :]), w=[f"wout{c}"], dma=True)
        for c in range(8):
            P.op("dve", lambda e, c=c: e.tensor_tensor(out=wout[:, c, :], in0=wout[:, c, :], in1=g1bc, op=ALU.mult),
                 r=[f"wout{c}", "g1bc"], w=[f"wout{c}"])
        WINH = [f"win{c}" for c in range(8)]
        WOUTH = [f"wout{c}" for c in range(8)]
        SK = _CACHE.get("skip", set())
        for c in range(4 if "diag" not in SK else 0):
            P.op("dve", lambda e, c=c: e.tensor_tensor(
                out=diag[:, c * 31:(c + 1) * 31, :], in0=identb.unsqueeze(1).to_broadcast([128, 31, 128]),
                in1=cols[:, C_CW + c * 31:C_CW + (c + 1) * 31].unsqueeze(2).to_broadcast([128, 31, 128]), op=ALU.mult),
                r=["identb", "cols"], w=["diag"])
        if "memset" not in SK:
            P.op("pool", lambda e: e.memset(vr[0], 0.0), w=["vr0"])
            P.op("pool", lambda e: e.memset(vr[1], 0.0), w=["vr1"])
        for par in range(2 if "memset" not in SK else 0):
            for g in range(2):
                P.op("pool", lambda e, par=par, g=g: e.memset(kr[par][g], 0.0), w=[f"kr{par}"])
            P.op("pool", lambda e, par=par: e.memset(va[par], 0.0), w=[f"va{par}"])
            P.op("dve", lambda e, par=par: e.memset(va[par][:, 1:, 64:65], 1.0), r=[f"va{par}"], w=[f"va{par}"])
            P.op("dve", lambda e, par=par: e.memset(va[par][:, 1:, 129:130], 1.0), r=[f"va{par}"], w=[f"va{par}"])

        if _CACHE.get("stop") == 1:
            P.fence()
            P.op("sp", lambda e: e.dma_start(out=dbg_d[0:128, 0:512], in_=g1bc[:, 0:512]), r=["g1bc"], w=["dbg"], dma=True)
            P.fence()
            P.emit()
            return nc
        nps_s1 = mk_nps([0, 1])
        psb1_f32 = psb[1][:, :].bitcast(F32)
        nps_at = [mk_nps([2, 3]), mk_nps([4, 5])]
        nps_ep = [mk_nps([2, 3]), mk_nps([4, 5])]

        def stage1(mt):
            par = mt % 2
            t0 = mt * 512
            vcur, vprev = vr[par], vr[1 - par]
            hv, hvp = f"vr{par}", f"vr{1 - par}"
            kT, kTp = kr[par], kr[1 - par]
            hk, hkp = f"kr{par}", f"kr{1 - par}"
            vaug, vaugp = va[par], va[1 - par]
            hva, hvap = f"va{par}", f"va{1 - par}"
            qT, hq = qT2[par], f"qT{par}"
            ybf, hy = ybf2[par], f"ybf{par}"
            rstd_sb, hrs = rstd2[par], f"rstd{par}"
            nmr_sb, hnm = nmr2[par], f"nmr{par}"
            nps = nps_s1
            P.rec()
            P.op("pool", lambda e: e.dma_start(out=xt, in_=x_d[t0:t0 + 512, :].rearrange("(s p) d -> p s d", p=128)),
                 w=["xt"], dma=True)
            for c in range(8):
                ptx = psb[0][:, 0:512]
                hp = "psb0"
                for s in range(4):
                    P.op("pe", lambda e, s=s, c=c: e.transpose(
                        out=ptx[:, s * 128:(s + 1) * 128], in_=xt[:, s, c * 128:(c + 1) * 128], identity=identb),
                        r=["xt", "identb"], w=[hp])
                P.op("act", lambda e, c=c: e.activation(
                    out=hT[:, c, :], in_=ptx[:, 0:512], func=AF.Identity, bias=modc2[:, c:c + 1], scale=modc2[:, 8 + c:9 + c]),
                    r=[hp, "modc2"], w=["hT"])
            if mt > 0:
                P.op("pool", lambda e: e.tensor_copy(out=vcur[:, :, 0:30], in_=vprev[:, :, 512:542]), r=[hvp], w=[hv])
                for g in range(2):
                    P.op("pool", lambda e, g=g: e.tensor_copy(out=kT[g][:, 0:128], in_=kTp[g][:, 512:640]), r=[hkp], w=[hk])
                P.op("pool", lambda e: e.tensor_copy(out=vaug[:, 0, :], in_=vaugp[:, 4, :]), r=[hvap], w=[hva])
            for c in range(4):
                pb, hpb = nps()
                for k in range(8):
                    P.op("pe", lambda e, pb=pb, k=k, c=c: e.matmul(
                        pb[:], lhsT=win[:, k, 512 + c * 128:512 + (c + 1) * 128], rhs=hT[:, k, :],
                        start=(k == 0), stop=(k == 7)), r=WINH + ["hT"], w=[hpb])
                P.op("act", lambda e, pb=pb, c=c: e.activation(
                    out=sig, in_=pb[:], func=AF.Sigmoid, bias=cols[:, C_BIN + 4 + c:C_BIN + 5 + c], scale=1.0),
                    r=[hpb, "cols"], w=["sig"])
                pa, hpa = nps()
                for k in range(8):
                    P.op("pe", lambda e, pa=pa, k=k, c=c: e.matmul(
                        pa[:], lhsT=win[:, k, c * 128:(c + 1) * 128], rhs=hT[:, k, :],
                        start=(k == 0), stop=(k == 7)), r=WINH + ["hT"], w=[hpa])
                P.op("dve", lambda e, pa=pa, c=c: e.scalar_tensor_tensor(
                    out=vcur[:, c, 30:542], in0=pa[:], scalar=cols[:, C_BIN + c:C_BIN + c + 1], in1=sig,
                    op0=ALU.add, op1=ALU.mult), r=[hpa, "sig", "cols"], w=[hv])
            for i in range(4):
                pq, hpq = nps()
                for k in range(8):
                    P.op("pe", lambda e, pq=pq, k=k, i=i: e.matmul(
                        pq[:], lhsT=win[:, k, 1024 + i * 128:1024 + (i + 1) * 128], rhs=hT[:, k, :],
                        start=(k == 0), stop=(k == 7)), r=WINH + ["hT"], w=[hpq])
                P.op("dve", lambda e, pq=pq, i=i: e.tensor_scalar(
                    out=qT[:, i, :], in0=pq[:], scalar1=cols[:, C_BIN + 8 + i:C_BIN + 9 + i], scalar2=0.125,
                    op0=ALU.add, op1=ALU.mult), r=[hpq, "cols"], w=[hq])
            pk, hpk = nps()
            for k in range(8):
                P.op("pe", lambda e, k=k: e.matmul(
                    pk[:], lhsT=win[:, k, 1536:1664], rhs=hT[:, k, :], start=(k == 0), stop=(k == 7)),
                    r=WINH + ["hT"], w=[hpk])
            for g in range(2):
                P.op("act", lambda e, g=g: e.activation(
                    out=kT[g][g * 64:(g + 1) * 64, 128:640], in_=pk[g * 64:(g + 1) * 64, :],
                    func=AF.Identity, bias=cols[g * 64:(g + 1) * 64, C_BIN + 12:C_BIN + 13], scale=1.0),
                    r=[hpk, "cols"], w=[hk])
            pv, hpv = nps()
            for s in range(4):
                for k in range(8):
                    P.op("pe", lambda e, s=s, k=k: e.matmul(
                        pv[:, s * 128:(s + 1) * 128], lhsT=hT[:, k, s * 128:(s + 1) * 128], rhs=win[:, k, 1664:1792],
                        start=(k == 0), stop=(k == 7)), r=WINH + ["hT"], w=[hpv])
            for s in range(4):
                blk = s + 1
                P.op("dve", lambda e, s=s, blk=blk: e.tensor_tensor(
                    out=vaug[:, blk, :].rearrange("p (g d) -> p g d", g=2)[:, :, 0:64],
                    in0=pv[:, s * 128:(s + 1) * 128].rearrange("p (g d) -> p g d", g=2),
                    in1=bv_bc.rearrange("p (g d) -> p g d", g=2), op=ALU.add),
                    r=[hpv, "rows_sb"], w=[hva])
            for c in range(4):
                py, hpy = nps()
                for j in range(31):
                    P.op("pe", lambda e, py=py, c=c, j=j: e.matmul(
                        py[:], lhsT=diag[:, c * 31 + j, :], rhs=vcur[:, c, j:j + 512], start=(j == 0), stop=(j == 30)),
                        r=["diag", hv], w=[hpy])
                P.op("act", lambda e, py=py, c=c: e.activation(
                    out=ybf[:, c, :], in_=py[:], func=AF.Identity, bias=cols[:, C_CB + c:C_CB + c + 1], scale=1.0),
                    r=[hpy, "cols"], w=[hy])
                P.op("act", lambda e, py=py, c=c: e.activation(
                    out=y2bf[:, c, :], in_=py[:], func=AF.Square, bias=cols[:, C_CB + c:C_CB + c + 1], scale=1.0),
                    r=[hpy, "cols"], w=["y2bf"])
            pm, hpm = nps()
            for c in range(4):
                P.op("pe", lambda e, c=c: e.matmul(pm[:], lhsT=ones512, rhs=ybf[:, c, :], start=(c == 0), stop=(c == 3)),
                     r=["ones512", hy], w=[hpm])
            pe2, hpe2 = nps()
            for c in range(4):
                P.op("pe", lambda e, c=c: e.matmul(pe2[:], lhsT=ones512, rhs=y2bf[:, c, :], start=(c == 0), stop=(c == 3)),
                     r=["ones512", "y2bf"], w=[hpe2])
            P.op("act", lambda e: e.activation(out=nmr_sb, in_=pm[:], func=AF.Identity), r=[hpm], w=[hnm])
            P.op("dve", lambda e: e.tensor_tensor(out=rstd_sb, in0=nmr_sb, in1=nmr_sb, op=ALU.mult), r=[hnm], w=[hrs])
            P.op("dve", lambda e: e.tensor_tensor(out=rstd_sb, in0=pe2[:], in1=rstd_sb, op=ALU.subtract), r=[hpe2, hrs], w=[hrs])
            P.op("act", lambda e: e.activation(out=rstd_sb, in_=rstd_sb, func=AF.Sqrt, bias=EPSC, scale=1.0), r=[hrs, "epsc"], w=[hrs])
            P.op("dve", lambda e: e.reciprocal(out=rstd_sb, in_=rstd_sb), r=[hrs], w=[hrs])
            P.op("dve", lambda e: e.scalar_tensor_tensor(out=nmr_sb, in0=nmr_sb, scalar=-1.0, in1=rstd_sb, op0=ALU.mult, op1=ALU.mult),
                 r=[hnm, hrs], w=[hnm])
            return P.end()

        def stage2(mt):
            par = mt % 2
            kT_l, vaug_l = kr[par], va[par]
            hk, hva = f"kr{par}", f"va{par}"
            qT, hq = qT2[par], f"qT{par}"
            ybf, hy = ybf2[par], f"ybf{par}"
            rstd_sb, hrs = rstd2[par], f"rstd{par}"
            nmr_sb, hnm = nmr2[par], f"nmr{par}"

            def convln_chain(c):
                k = c % 2
                zc, sc_, s2bf, r2c = zc2[k], sc2_[k], s2bf2[k], r2c2[k]
                P.rec()
                P.op("dve", lambda e: e.tensor_tensor(out=zc, in0=ybf[:, c, :], in1=rstd_sb, op=ALU.mult),
                     r=[hy, hrs], w=[f"zc{k}"])
                P.op("dve", lambda e: e.tensor_tensor(out=zc, in0=zc, in1=nmr_sb, op=ALU.add), r=[f"zc{k}", hnm], w=[f"zc{k}"])
                P.op("act", lambda e: e.activation(out=sc_, in_=zc, func=AF.Silu, bias=cols[:, C_LB + c:C_LB + c + 1],
                                                   scale=cols[:, C_LG + c:C_LG + c + 1]), r=[f"zc{k}", "cols"], w=[f"sc{k}"])
                P.op("act", lambda e: e.activation(out=s2bf, in_=sc_, func=AF.Square), r=[f"sc{k}"], w=[f"s2bf{k}"])
                pr, hpr = psb1_f32, "psb1"
                P.op("pe", lambda e: e.matmul(pr[:], lhsT=bdb, rhs=s2bf, start=True, stop=True), r=["bdb", f"s2bf{k}"], w=[hpr])
                P.op("act", lambda e: e.activation(out=r2c, in_=pr[:], func=AF.Sqrt, bias=EPSC, scale=1.0), r=[hpr, "epsc"], w=[f"r2c{k}"])
                P.op("dve", lambda e: e.reciprocal(out=r2c, in_=r2c), r=[f"r2c{k}"], w=[f"r2c{k}"])
                P.op("dve", lambda e: e.scalar_tensor_tensor(out=ycT[:, c, :], in0=sc_, scalar=cols[:, C_OG + c:C_OG + c + 1],
                                                             in1=r2c, op0=ALU.mult, op1=ALU.mult),
                     r=[f"sc{k}", f"r2c{k}", "cols"], w=[f"ycTc{c}"])
                return P.end()

            def attn_chain(s):
                k = s % 2
                mx, mxb, nmx, dcat, ET = mx2[k], mxb2[k], nmx2[k], dcat2[k], ET2[k]
                es_t, den, osb, osq, ssq, yat = es_t2[k], den2[k], osb2[k], osq2[k], ssq2[k], yat2[k]
                mynps = nps_at[k]
                n = mt * 4 + s
                qs = slice(s * 128, (s + 1) * 128)
                P.rec()
                for i in range(4):
                    pS, hpS = mynps()
                    for g in range(2):
                        P.op("pe", lambda e, pS=pS, i=i, g=g: e.matmul(
                            pS[:, g * 256:(g + 1) * 256], lhsT=qT[:, i, qs], rhs=kT_l[g][:, s * 128:s * 128 + 256],
                            start=True, stop=True), r=[hq, hk], w=[hpS])
                    P.op("dve", lambda e, pS=pS, i=i: e.tensor_reduce(
                        out=mx[:, 2 * i:2 * i + 2], in_=pS[:].rearrange("p (g k) -> p g k", g=2), axis=AX.X, op=ALU.max),
                        r=[hpS], w=[f"mx{k}"])
                P.op("dve", lambda e: e.tensor_copy(out=mxb, in_=mx), r=[f"mx{k}"], w=[f"mxb{k}"])
                P.op("dve", lambda e: e.tensor_scalar(out=nmx, in0=mxb, scalar1=-1.0, scalar2=None, op0=ALU.mult), r=[f"mxb{k}"], w=[f"nmx{k}"])
                for g in range(2):
                    P.op("dve", lambda e, g=g: e.tensor_tensor(
                        out=dcat[:, g, :].rearrange("p (i q) -> p i q", i=4), in0=identb.unsqueeze(1).to_broadcast([128, 4, 128]),
                        in1=nmx.rearrange("p (i g) -> p i g", g=2)[:, :, g:g + 1].to_broadcast([128, 4, 128]), op=ALU.mult),
                        r=["identb", f"nmx{k}"], w=[f"dcat{k}_{g}"])
                khs = [1] if n == 0 else [0, 1]
                for g in range(2):
                    for kh in khs:
                        pT, hpT = mynps()
                        kc = slice(s * 128 + kh * 128, s * 128 + kh * 128 + 128)
                        P.op("pe", lambda e, pT=pT, g=g, kc=kc: e.matmul(
                            pT[:].rearrange("p (i q) -> p i q", i=4), lhsT=kT_l[g][:, kc], rhs=qT[:, :, qs], start=True, stop=False),
                            r=[hk, hq], w=[hpT])
                        P.op("pe", lambda e, pT=pT, g=g: e.matmul(pT[:], lhsT=onesb, rhs=dcat[:, g, :], start=False, stop=False),
                             r=["onesb", f"dcat{k}_{g}"], w=[hpT])
                        P.op("pe", lambda e, pT=pT, kh=kh: e.matmul(pT[:], lhsT=identb, rhs=maskT[:, kh, :], start=False, stop=True),
                             r=["identb", "maskT"], w=[hpT])
                        P.op("act", lambda e, pT=pT, g=g, kh=kh: e.activation(out=ET[:, g * 2 + kh, :], in_=pT[:], func=AF.Exp),
                             r=[hpT], w=[f"ET{k}_{g}{kh}"])
                P.op("dve", lambda e: e.tensor_tensor(out=es_t, in0=sink_bc, in1=nmx, op=ALU.add), r=["rows_sb", f"nmx{k}"], w=[f"es_t{k}"])
                P.op("act", lambda e: e.activation(out=es_t, in_=es_t, func=AF.Exp), r=[f"es_t{k}"], w=[f"es_t{k}"])
                for g in range(2):
                    po, hpo = mynps()
                    for i in range(4):
                        for kh in khs:
                            P.op("pe", lambda e, po=po, g=g, i=i, kh=kh: e.matmul(
                                po[:, i * 65:(i + 1) * 65], lhsT=ET[:, g * 2 + kh, i * 128:(i + 1) * 128],
                                rhs=vaug_l[:, s + kh, g * 65:(g + 1) * 65], start=(kh == khs[0]), stop=(kh == 1)),
                                r=[f"ET{k}_{g}{kh}", hva], w=[hpo])
                    P.op("dve", lambda e, po=po, g=g: e.tensor_tensor(
                        out=den.rearrange("p (i g) -> p i g", g=2)[:, :, g:g + 1],
                        in0=po[:, 0:260].rearrange("p (i d) -> p i d", d=65)[:, :, 64:65],
                        in1=es_t.rearrange("p (i g) -> p i g", g=2)[:, :, g:g + 1], op=ALU.add),
                        r=[hpo, f"es_t{k}"], w=[f"den{k}_{g}"])
                    P.op("act", lambda e, po=po, g=g: e.activation(
                        out=osb.rearrange("p (i g) d -> p i g d", g=2)[:, :, g, :],
                        in_=po[:, 0:260].rearrange("p (i d) -> p i d", d=65)[:, :, 0:64], func=AF.Identity),
                        r=[hpo], w=[f"osb{k}_{g}"])
                DH = [f"den{k}_0", f"den{k}_1"]
                OH = [f"osb{k}_0", f"osb{k}_1"]
                P.op("dve", lambda e: e.reciprocal(out=den, in_=den), r=DH, w=DH)
                P.op("dve", lambda e: e.tensor_tensor(out=osb, in0=osb, in1=den.unsqueeze(2).to_broadcast([128, 8, 64]), op=ALU.mult),
                     r=OH + DH, w=OH)
                P.op("act", lambda e: e.activation(out=osq, in_=osb, func=AF.Square), r=OH, w=[f"osq{k}"])
                P.op("dve", lambda e: e.tensor_reduce(out=ssq, in_=osq, axis=AX.X, op=ALU.add), r=[f"osq{k}"], w=[f"ssq{k}"])
                P.op("act", lambda e: e.activation(out=ssq, in_=ssq, func=AF.Sqrt, bias=EPSC, scale=1.0 / 64.0), r=[f"ssq{k}", "epsc"], w=[f"ssq{k}"])
                P.op("dve", lambda e: e.reciprocal(out=ssq, in_=ssq), r=[f"ssq{k}"], w=[f"ssq{k}"])
                P.op("dve", lambda e: e.tensor_tensor(out=osb, in0=osb, in1=ssq.unsqueeze(2).to_broadcast([128, 8, 64]), op=ALU.mult),
                     r=OH + [f"ssq{k}"], w=OH)
                P.op("dve", lambda e: e.tensor_tensor(out=yat, in0=osb.rearrange("p h d -> p (h d)"), in1=aog_bc, op=ALU.mult),
                     r=OH + ["rows_sb"], w=[f"yat{k}"])
                ptb = ps[3 + 2 * k][:, :].bitcast(BF16)[:, 0:512]
                hptr = f"ps{3 + 2 * k}"
                for i in range(4):
                    P.op("pe", lambda e, i=i: e.transpose(out=ptb[:, i * 128:(i + 1) * 128], in_=yat[:, i * 128:(i + 1) * 128],
                                                         identity=identb), r=[f"yat{k}", "identb"], w=[hptr])
                P.op("act", lambda e: e.activation(
                    out=ycT[:, 4:8, qs], in_=ptb[:, 0:512].rearrange("p (i q) -> p i q", i=4), func=AF.Identity),
                    r=[hptr], w=[f"ycTa{s}"])
                return P.end()

            def xr_load(s):
                k = s % 2
                tt = mt * 4 + s
                P.op("sp", lambda e: e.dma_start(out=xr2[k], in_=x_d[tt * 128:(tt + 1) * 128, :]), w=[f"xr{k}"], dma=True)

            def epi_chain(s, with_load):
                k = s % 2
                tt = mt * 4 + s
                xr, bnst, bnag = xr2[k], bnst2[k], bnag2[k]
                rr = xr
                mynps = nps_ep[k]
                YH = [f"ycTc{c}" for c in range(4)] + [f"ycTa{s}"]
                P.rec()
                if with_load:
                    xr_load(s)
                for h in range(2):
                    po, hpo = mynps()
                    for c in range(8):
                        P.op("pe", lambda e, po=po, c=c, h=h: e.matmul(
                            po[:], lhsT=ycT[:, c, s * 128:(s + 1) * 128], rhs=wout[:, c, h * 512:(h + 1) * 512],
                            start=(c == 0), stop=False), r=YH + WOUTH, w=[hpo])
                    P.op("pe", lambda e, po=po, h=h: e.matmul(
                        po[:], lhsT=onesb[0:1, :], rhs=gbrow[0:1, h * 512:(h + 1) * 512], start=False, stop=True),
                        r=["onesb", "gbrow"], w=[hpo])
                    P.op("dve", lambda e, po=po, h=h: e.tensor_tensor(out=rr[:, h * 512:(h + 1) * 512], in0=po[:],
                                                                      in1=xr[:, h * 512:(h + 1) * 512], op=ALU.add),
                         r=[hpo, f"xr{k}"], w=[f"xr{k}"])
                for h in range(2):
                    P.op("dve", lambda e, h=h: e.bn_stats(out=bnst[:, h * 6:(h + 1) * 6], in_=rr[:, h * 512:(h + 1) * 512]),
                         r=[f"xr{k}"], w=[f"bnst{k}"])
                P.op("dve", lambda e: e.bn_aggr(out=bnag[:, 0:2], in_=bnst), r=[f"bnst{k}"], w=[f"bnag{k}"])
                P.op("act", lambda e: e.activation(out=bnag[:, 2:3], in_=bnag[:, 1:2], func=AF.Sqrt, bias=EPSC2, scale=1.0),
                     r=[f"bnag{k}", "epsc2"], w=[f"bnagb{k}"])
                P.op("dve", lambda e: e.reciprocal(out=bnag[:, 2:3], in_=bnag[:, 2:3]), r=[f"bnagb{k}"], w=[f"bnagb{k}"])
                P.op("dve", lambda e: e.scalar_tensor_tensor(out=bnag[:, 3:4], in0=bnag[:, 0:1], scalar=-1.0, in1=bnag[:, 2:3],
                                                             op0=ALU.mult, op1=ALU.mult), r=[f"bnag{k}", f"bnagb{k}"], w=[f"bnagc{k}"])
                P.op("act", lambda e: e.activation(out=rr, in_=rr, func=AF.Identity, bias=bnag[:, 3:4], scale=bnag[:, 2:3]),
                     r=[f"xr{k}", f"bnagb{k}", f"bnagc{k}"], w=[f"xr{k}"])
                P.op("sp", lambda e: e.dma_start(out=z_d[tt * 128:(tt + 1) * 128, :], in_=rr), r=[f"xr{k}"], w=[f"z_d{tt}"], dma=True)
                return P.end()


            out = []
            P.rec()
            xr_load(0)
            xr_load(1)
            out += P.end()
            out += merge(convln_chain(0) + convln_chain(1), attn_chain(0), attn_chain(1))
            out += merge(convln_chain(2) + convln_chain(3), attn_chain(2), attn_chain(3))
            out += merge(epi_chain(0, False), epi_chain(1, False))
            out += merge(epi_chain(2, True), epi_chain(3, True))
            return out

        P.play(stage1(0))
        for mt in range(NMT):
            nxt = stage1(mt + 1) if mt + 1 < NMT else []
            P.play(merge(stage2(mt), nxt))

        P.fence()
        if dbg and dbg["what"] == "z":
            nr = _CACHE.get('nmt_run', NMT) * 512
            P.op("sp", lambda e: e.dma_start(out=dbg_d[0:nr, :], in_=z_d[0:nr, :]), r=[f"z_d{i}" for i in range(nr // 128)], w=["dbg"], dma=True)
            P.fence()
            P.emit()
            return nc

        try:
            build_phase2(nc, P, A, ps, nps, dr, out_d, z_d, xs_d, ys_d, dbg, dbg_d, mark_persist,
                     dict(cols=cols, identf=identf, identb=identb, onesb=onesb, Ub=Ub, modc=modc, mod_d=mod_d,
                              EPSC=EPSC, psb=psb))
        except _StopEmit:
            pass
        P.fence()
        P.emit()
    return nc


def build_phase2(nc, P, A, ps, nps, dr, out_d, z_d, xs_d, ys_d, dbg, dbg_d, mark, K):
    cols, identf, identb, onesb, Ub, mod_d, EPSC, psb = (K[k] for k in ("cols", "identf", "identb", "onesb", "Ub", "mod_d", "EPSC", "psb"))
    rows_d, wr_d, wg_d, wu_d, wd_d = dr["rows"], dr["w_r"], dr["wg"], dr["wu"], dr["wd"]
    A.off = mark
    gbc = A.alloc([4, D], F32)
    P.op("sp", lambda e: e.dma_start(out=gbc[:, 1:4, :].rearrange("p a d -> p (a d)"), in_=mod_d[0:1, :].partition_broadcast(128)),
         r=["mod_d"], w=["gbc1_0", "gbc1_1", "gbc2_0", "gbc2_1", "gbc3_0", "gbc3_1"], dma=True)
    lnr = A.alloc([4, D], F32)
    misc = A.alloc([36 + 64], F32)
    A2 = A.alloc([D], F32)
    B2 = A.alloc([D], F32)
    GA = A.alloc([D], F32)
    BA = A.alloc([D], F32)
    wr = A.alloc([8, 36], F32)
    pos_i = A.alloc([2, NT], I32)
    wts = A.alloc([2, NT], F32)
    widx_i = A.alloc([64], I32)
    mark2 = A.off
    br_bc = misc[:, 0:36]
    slot_bc = misc[:, 36:36 + NS]
    P.op("sp", lambda e: e.dma_start(out=lnr.rearrange("p a d -> p (a d)"), in_=rows_d[0:1, R_L1G:R_L1G + 4 * D].partition_broadcast(128)),
         w=["lnr"], dma=True)
    P.op("sp", lambda e: e.dma_start(out=misc, in_=rows_d[0:1, R_BR:R_BR + 100].partition_broadcast(128)), w=["misc"], dma=True)
    P.op("sp", lambda e: e.dma_start(out=wr, in_=wr_d.rearrange("(c p) n -> p c n", p=128)), w=["wr"], dma=True)
    G2H = ["gbc1_0", "gbc1_1", "gbc2_0", "gbc2_1", "gbc3_0", "gbc3_1"]
    P.op("dve", lambda e: e.scalar_tensor_tensor(out=A2, in0=gbc[:, 2, :], scalar=1.0, in1=lnr[:, 0, :], op0=ALU.add, op1=ALU.mult),
         r=["lnr"] + G2H, w=["A2"])
    P.op("dve", lambda e: e.scalar_tensor_tensor(out=B2, in0=gbc[:, 2, :], scalar=1.0, in1=lnr[:, 1, :], op0=ALU.add, op1=ALU.mult),
         r=["lnr"] + G2H, w=["B2"])
    P.op("dve", lambda e: e.tensor_tensor(out=B2, in0=B2, in1=gbc[:, 1, :], op=ALU.add), r=["B2"] + G2H, w=["B2"])
    P.op("dve", lambda e: e.tensor_scalar(out=GA, in0=lnr[:, 0, :], scalar1=ALPHA, scalar2=None, op0=ALU.mult), r=["lnr"], w=["GA"])
    P.op("dve", lambda e: e.tensor_scalar(out=BA, in0=lnr[:, 1, :], scalar1=ALPHA, scalar2=None, op0=ALU.mult), r=["lnr"], w=["BA"])

    def mk_nps(ids):
        st_ = [0]

        def f():
            i = ids[st_[0] % len(ids)]
            st_[0] += 1
            return ps[i], f"ps{i}"
        return f

    t1_d = nc.dram_tensor("t1_scr", [S, D], F32, kind="Internal").ap()
    h2b = A.alloc([NT, D], BF16)
    logits = A.alloc([NT, 36], F32)
    mark2a = A.off
    NB2A = 4
    zt = [A.alloc([D], F32) for _ in range(NB2A)]
    h2f = [A.alloc([D], F32) for _ in range(NB2A)]
    h2T = [A.alloc([8, 128], F32) for _ in range(NB2A)]
    t1s = [A.alloc([D], F32) for _ in range(NB2A)]
    nps2a_f = [mk_nps([0, 1]), mk_nps([2, 3])]
    nps2a_b = [mk_nps([4]), mk_nps([5])]

    def front_ew(j):
        b = j % NB2A
        z_, hz = zt[b], f"zt{b}"
        hf, hhf = h2f[b], f"h2f{b}"
        t1_, ht1 = t1s[b], f"t1s{b}"
        P.rec()
        P.op("sp", lambda e: e.dma_start(out=z_, in_=z_d[j * 128:(j + 1) * 128, :]), r=[f"z_d{j}"], w=[hz], dma=True)
        P.op("dve", lambda e: e.tensor_tensor(out=hf, in0=z_, in1=A2, op=ALU.mult), r=[hz, "A2"], w=[hhf])
        P.op("dve", lambda e: e.tensor_tensor(out=hf, in0=hf, in1=B2, op=ALU.add), r=[hhf, "B2"], w=[hhf])
        P.op("pool", lambda e: e.tensor_tensor(out=t1_, in0=z_, in1=GA, op=ALU.mult), r=[hz, "GA"], w=[ht1])
        P.op("dve", lambda e: e.tensor_tensor(out=t1_, in0=t1_, in1=BA, op=ALU.add), r=[ht1, "BA"], w=[ht1])
        P.op("sp", lambda e: e.dma_start(out=t1_d[j * 128:(j + 1) * 128, :], in_=t1_), r=[ht1], w=[f"t1_d{j}"], dma=True)
        P.op("act", lambda e: e.activation(out=h2b[:, j, :], in_=hf, func=AF.Identity), r=[hhf], w=[f"h2b{j}"])
        return P.end()

    def front_tr(j):
        b = j % NB2A
        mynps = nps2a_f[j % 2]
        hf, hhf = h2f[b], f"h2f{b}"
        hT_, hhT = h2T[b], f"h2T{b}"
        P.rec()
        for hh in range(2):
            pt, hp = mynps()
            for c4 in range(4):
                c = hh * 4 + c4
                P.op("pe", lambda e, pt=pt, c=c, c4=c4: e.transpose(out=pt[:, c4 * 128:(c4 + 1) * 128],
                                                                  in_=hf[:, c * 128:(c + 1) * 128], identity=identf),
                     r=[hhf, "cst"], w=[hp])
            P.op("act", lambda e, pt=pt, hh=hh: e.activation(out=hT_[:, hh * 4:(hh + 1) * 4, :].rearrange("p c t -> p (c t)"),
                                                             in_=pt[:], func=AF.Identity), r=[hp], w=[hhT + "ab"[hh]])
        return P.end()

    def back2a(j):
        b = j % NB2A
        hT_, hhT = h2T[b], f"h2T{b}"
        P.rec()
        pl, hpl = nps2a_b[j % 2]()
        for c in range(8):
            P.op("pe", lambda e, c=c: e.matmul(pl[:, 0:36], lhsT=hT_[:, c, :], rhs=wr[:, c, :], start=(c == 0), stop=(c == 7)),
                 r=[hhT + "a", hhT + "b", "wr"], w=[hpl])
        P.op("dve", lambda e: e.tensor_tensor(out=logits[:, j, :], in0=pl[:, 0:36], in1=br_bc, op=ALU.add),
             r=[hpl, "misc"], w=["logits"])
        return P.end()

    NP2A = NT // 2
    P.play(front_ew(0) + front_ew(1) + merge(front_tr(0), front_tr(1)))
    for k in range(NP2A):
        blk = []
        if k + 1 < NP2A:
            blk += merge(front_ew(2 * k + 2), front_ew(2 * k + 3))
        bk = merge(back2a(2 * k), back2a(2 * k + 1))
        tail = [o for o in bk if o[0] == "dve"]
        blk += [o for o in bk if o[0] != "dve"]
        if k + 1 < NP2A:
            blk += merge(front_tr(2 * k + 2), front_tr(2 * k + 3))
        blk += tail
        P.play(blk)
    P.fence()
    A.off = mark2a

    def T3(n):
        return A.alloc([NT, n], F32)
    gmax = A.alloc([NT], F32)
    og = T3(4)
    eg = T3(4)
    sgm = A.alloc([NT], F32)
    ptop = A.alloc([NT], F32)
    tmp4 = A.alloc([NT, 4, 8], F32)
    sel = T3(8)
    sel2 = T3(8)
    m1 = A.alloc([NT], F32)
    m2 = A.alloc([NT], F32)
    o1 = T3(8)
    o2 = T3(8)
    e2 = A.alloc([NT], F32)
    r12 = A.alloc([NT], F32)
    O1 = A.alloc([NT, 4, 8], F32)
    O2 = A.alloc([NT, 4, 8], F32)
    Obf = A.alloc([NT * 32], BF16)
    totA = A.alloc([NT, 32], F32)
    totB = A.alloc([NT, 32], F32)
    tot0 = A.alloc([NT, 32], F32)
    base = A.alloc([NT, 32], F32)
    cnt = A.alloc([32], F32)
    cmpc = A.alloc([32, 24], F32)
    pcnt = A.alloc([32], F32)
    oeA = A.alloc([32], F32)
    oeB = A.alloc([32], F32)
    offs = A.alloc([32], F32)
    cmps = A.alloc([NS, 32], F32)
    esl = A.alloc([64], F32)
    used = A.alloc([64], F32)
    posf = A.alloc([2, NT], F32)

    LG = logits[:, :, 0:4]
    LE4 = logits[:, :, 4:36].rearrange("p j (g e) -> p j g e", g=4)

    def dv(fn, r, w):
        P.op("dve", fn, r=r, w=w)

    dv(lambda e: e.tensor_reduce(out=gmax, in_=LG, axis=AX.X, op=ALU.max), ["logits"], ["gmax"])
    dv(lambda e: e.tensor_tensor(out=og, in0=LG, in1=gmax.unsqueeze(2).to_broadcast([128, NT, 4]), op=ALU.is_equal), ["logits", "gmax"], ["og"])
    dv(lambda e: e.tensor_tensor(out=eg, in0=LG, in1=gmax.unsqueeze(2).to_broadcast([128, NT, 4]), op=ALU.subtract), ["logits", "gmax"], ["eg"])
    P.op("act", lambda e: e.activation(out=eg, in_=eg, func=AF.Exp), r=["eg"], w=["eg"])
    dv(lambda e: e.tensor_reduce(out=sgm, in_=eg, axis=AX.X, op=ALU.add), ["eg"], ["sgm"])
    dv(lambda e: e.reciprocal(out=ptop, in_=sgm), ["sgm"], ["ptop"])
    dv(lambda e: e.tensor_tensor(out=tmp4, in0=LE4, in1=og.unsqueeze(3).to_broadcast([128, NT, 4, 8]), op=ALU.mult), ["logits", "og"], ["tmp4"])
    dv(lambda e: e.tensor_reduce(out=sel, in_=tmp4.rearrange("p j g e -> p j e g"), axis=AX.X, op=ALU.add), ["tmp4"], ["sel"])
    dv(lambda e: e.tensor_reduce(out=m1, in_=sel, axis=AX.X, op=ALU.max), ["sel"], ["m1"])
    dv(lambda e: e.tensor_tensor(out=o1, in0=sel, in1=m1.unsqueeze(2).to_broadcast([128, NT, 8]), op=ALU.is_equal), ["sel", "m1"], ["o1"])
    dv(lambda e: e.scalar_tensor_tensor(out=sel2.rearrange("p j e -> p (j e)"), in0=o1.rearrange("p j e -> p (j e)"), scalar=-1.0e9,
                                        in1=sel.rearrange("p j e -> p (j e)"), op0=ALU.mult, op1=ALU.add), ["o1", "sel"], ["sel2"])
    dv(lambda e: e.tensor_reduce(out=m2, in_=sel2, axis=AX.X, op=ALU.max), ["sel2"], ["m2"])
    dv(lambda e: e.tensor_tensor(out=o2, in0=sel2, in1=m2.unsqueeze(2).to_broadcast([128, NT, 8]), op=ALU.is_equal), ["sel2", "m2"], ["o2"])
    dv(lambda e: e.tensor_tensor(out=e2, in0=m2, in1=m1, op=ALU.subtract), ["m1", "m2"], ["e2"])
    P.op("act", lambda e: e.activation(out=e2, in_=e2, func=AF.Exp), r=["e2"], w=["e2"])
    dv(lambda e: e.tensor_scalar(out=r12, in0=e2, scalar1=1.0, scalar2=None, op0=ALU.add), ["e2"], ["r12"])
    dv(lambda e: e.reciprocal(out=r12, in_=r12), ["r12"], ["r12"])
    dv(lambda e: e.tensor_tensor(out=wts[:, 0, :], in0=r12, in1=ptop, op=ALU.mult), ["r12", "ptop"], ["wts"])
    dv(lambda e: e.tensor_tensor(out=wts[:, 1, :], in0=wts[:, 0, :], in1=e2, op=ALU.mult), ["wts", "e2"], ["wts"])
    dv(lambda e: e.tensor_tensor(out=O1, in0=og.unsqueeze(3).to_broadcast([128, NT, 4, 8]),
                                 in1=o1.unsqueeze(2).to_broadcast([128, NT, 4, 8]), op=ALU.mult), ["og", "o1"], ["O1"])
    dv(lambda e: e.tensor_tensor(out=O2, in0=og.unsqueeze(3).to_broadcast([128, NT, 4, 8]),
                                 in1=o2.unsqueeze(2).to_broadcast([128, NT, 4, 8]), op=ALU.mult), ["og", "o2"], ["O2"])
    O1f = O1.rearrange("p j g e -> p (j g e)")
    O2f = O2.rearrange("p j g e -> p (j g e)")
    dv(lambda e: e.tensor_tensor(out=Obf, in0=O1f, in1=O2f, op=ALU.add), ["O1", "O2"], ["Obf"])
    pcs, pts = [], []
    for h in range(2):
        pc_, hpc = nps()
        P.op("pe", lambda e, pc_=pc_, h=h: e.matmul(pc_[:], lhsT=Ub, rhs=Obf[:, h * 512:(h + 1) * 512], start=True, stop=True),
             r=["Ub", "Obf"], w=[hpc])
        pcs.append((pc_, hpc))
        pt_, hpt = nps()
        P.op("pe", lambda e, pt_=pt_, h=h: e.matmul(pt_[:], lhsT=onesb, rhs=Obf[:, h * 512:(h + 1) * 512], start=True, stop=True),
             r=["onesb", "Obf"], w=[hpt])
        pts.append((pt_, hpt))
    tot0f = tot0.rearrange("p j e -> p (j e)")
    for h in range(2):
        dv(lambda e, h=h: e.tensor_copy(out=tot0f[:, h * 512:(h + 1) * 512], in_=pts[h][0][:]), [pts[h][1]], ["tot0"])
    cur, hc = tot0, "tot0"
    for i_, sft in enumerate((1, 2, 4, 8, 16)):
        nxt, hn = (totA, "totA") if i_ % 2 == 0 else (totB, "totB")
        dv(lambda e, cur=cur, nxt=nxt, sft=sft: e.tensor_tensor(out=nxt[:, sft:, :], in0=cur[:, sft:, :], in1=cur[:, :NT - sft, :], op=ALU.add),
           [hc], [hn])
        dv(lambda e, cur=cur, nxt=nxt, sft=sft: e.tensor_copy(out=nxt[:, :sft, :], in_=cur[:, :sft, :]), [hc, hn], [hn])
        cur, hc = nxt, hn
    incl, hincl = cur, hc
    dv(lambda e: e.tensor_copy(out=cnt, in_=incl[:, NT - 1, :]), [hincl], ["cnt"])
    dv(lambda e: e.tensor_tensor(out=cmpc, in0=cnt.unsqueeze(2).to_broadcast([128, 32, 24]),
                                 in1=slot_bc[:, 0:24].unsqueeze(1).to_broadcast([128, 32, 24]), op=ALU.is_gt), ["cnt", "misc"], ["cmpc"])
    dv(lambda e: e.tensor_reduce(out=pcnt, in_=cmpc, axis=AX.X, op=ALU.add), ["cmpc"], ["pcnt"])
    dv(lambda e: e.tensor_scalar(out=pcnt, in0=pcnt, scalar1=float(T), scalar2=None, op0=ALU.mult), ["pcnt"], ["pcnt"])
    cur, hc = pcnt, "pcnt"
    for i_, sft in enumerate((1, 2, 4, 8, 16)):
        nxt, hn = (oeA, "oeA") if i_ % 2 == 0 else (oeB, "oeB")
        dv(lambda e, cur=cur, nxt=nxt, sft=sft: e.tensor_tensor(out=nxt[:, sft:], in0=cur[:, sft:], in1=cur[:, :32 - sft], op=ALU.add), [hc], [hn])
        dv(lambda e, cur=cur, nxt=nxt, sft=sft: e.tensor_copy(out=nxt[:, :sft], in_=cur[:, :sft]), [hc, hn], [hn])
        cur, hc = nxt, hn
    oend, hoend = cur, hc
    dv(lambda e: e.tensor_tensor(out=offs, in0=oend, in1=pcnt, op=ALU.subtract), [hoend, "pcnt"], ["offs"])
    dv(lambda e: e.tensor_tensor(out=base, in0=incl, in1=tot0, op=ALU.subtract), [hincl, "tot0"], ["base"])
    dv(lambda e: e.tensor_tensor(out=base, in0=base, in1=offs.unsqueeze(1).to_broadcast([128, NT, 32]), op=ALU.add), ["base", "offs"], ["base"])
    basef = base.rearrange("p j e -> p (j e)")
    for h in range(2):
        dv(lambda e, h=h: e.tensor_tensor(out=basef[:, h * 512:(h + 1) * 512], in0=pcs[h][0][:], in1=basef[:, h * 512:(h + 1) * 512], op=ALU.add),
           [pcs[h][1], "base"], ["base"])
    for k, (Ok, hO) in enumerate(((O1, "O1"), (O2, "O2"))):
        dv(lambda e, Ok=Ok: e.tensor_tensor(out=Ok.rearrange("p j g e -> p (j g e)"), in0=Ok.rearrange("p j g e -> p (j g e)"), in1=basef, op=ALU.mult),
           [hO, "base"], [hO])
        dv(lambda e, Ok=Ok, k=k: e.tensor_reduce(out=posf[:, k, :], in_=Ok.rearrange("p j g e -> p j (g e)"), axis=AX.X, op=ALU.add), [hO], ["posf"])
    dv(lambda e: e.tensor_copy(out=pos_i, in_=posf), ["posf"], ["pos_i"])
    dv(lambda e: e.tensor_tensor(out=cmps, in0=oend.unsqueeze(1).to_broadcast([128, NS, 32]),
                                 in1=slot_bc.unsqueeze(2).to_broadcast([128, NS, 32]), op=ALU.is_le), [hoend, "misc"], ["cmps"])
    dv(lambda e: e.tensor_reduce(out=esl[:, 0:NS], in_=cmps, axis=AX.X, op=ALU.add), ["cmps"], ["esl"])
    dv(lambda e: e.tensor_scalar(out=esl[:, 0:NS], in0=esl[:, 0:NS], scalar1=float(NE - 1), scalar2=128.0, op0=ALU.min, op1=ALU.mult), ["esl"], ["esl"])
    dv(lambda e: e.tensor_scalar(out=used[:, 0:NS], in0=slot_bc, scalar1=oend[:, 31:32], scalar2=None, op0=ALU.is_lt), ["misc", hoend], ["used"])
    dv(lambda e: e.tensor_scalar(out=used[:, 0:NS], in0=used[:, 0:NS], scalar1=-1.0e6, scalar2=1.0e6, op0=ALU.mult, op1=ALU.add), ["used"], ["used"])
    dv(lambda e: e.tensor_tensor(out=esl[:, 0:NS], in0=esl[:, 0:NS], in1=used[:, 0:NS], op=ALU.add), ["esl", "used"], ["esl"])
    dv(lambda e: e.tensor_scalar(out=esl[:, 0:NS], in0=esl[:, 0:NS], scalar1=cols[:, C_PID:C_PID + 1], scalar2=None, op0=ALU.add), ["esl", "cols"], ["esl"])
    dv(lambda e: e.tensor_copy(out=widx_i[:, 0:NS], in_=esl[:, 0:NS]), ["esl"], ["widx_i"])

    if dbg and dbg["what"] == "route":
        P.fence()
        P.op("sp", lambda e: e.dma_start(out=dbg_d[0:128, 0:NT * 36], in_=logits.rearrange("p j n -> p (j n)")), r=["logits"], w=["dbg0"], dma=True)
        P.op("sp", lambda e: e.dma_start(out=dbg_d[128:256, 0:2 * NT], in_=posf.rearrange("p k j -> p (k j)")), r=["posf"], w=["dbg1"], dma=True)
        P.op("sp", lambda e: e.dma_start(out=dbg_d[256:384, 0:2 * NT], in_=wts.rearrange("p k j -> p (k j)")), r=["wts"], w=["dbg2"], dma=True)
        P.op("sp", lambda e: e.dma_start(out=dbg_d[384:512, 0:NS], in_=esl[:, 0:NS]), r=["esl"], w=["dbg3"], dma=True)
        P.fence()
        raise _StopEmit()

    for j in range(NT):
        for k in range(2):
            P.op("pool", lambda e, j=j, k=k: e.indirect_dma_start(
                out=xs_d[:, :], out_offset=bass.IndirectOffsetOnAxis(ap=pos_i[:, k, j:j + 1], axis=0),
                in_=h2b[:, j, :], in_offset=None), r=[f"h2b{j}", "pos_i"], w=[f"xs_{j}_{k}"], dma=True)
    P.fence()

    A.off = mark2
    NBW = 4
    wgs = [A.alloc([2048], BF16) for _ in range(NBW)]
    wus = [A.alloc([2048], BF16) for _ in range(NBW)]
    wds = [A.alloc([2048], BF16) for _ in range(NBW)]
    xtok = [A.alloc([NSUB, D], BF16) for _ in range(NBW)]
    XT = [A.alloc([8, T], BF16) for _ in range(2)]
    sgs = [A.alloc([T], F32) for _ in range(2)]
    aT = [A.alloc([2, T], BF16) for _ in range(2)]
    NYO = 3
    yo = [A.alloc([D], F32) for _ in range(NYO)]
    npsB = mk_nps([0, 1, 2])
    npsC = mk_nps([3, 4, 5])
    _bc = {}

    def get_bc(e):
        if "v" not in _bc:
            reg = e.alloc_register("bcreg")
            e.reg_mov(reg, NE * 128 - 1)
            _bc["v"] = e.snap(reg, donate=True)
        return _bc["v"]

    ORDER = []
    for q in range((NS + 1) // 2):
        ORDER.append(q)
        if NS - 1 - q > q:
            ORDER.append(NS - 1 - q)
    assert sorted(ORDER) == list(range(NS))

    def load_w(i, which):
        s = ORDER[i]
        bw = i % NBW
        for (wsb, wdr, hn) in which(bw):
            P.op("pool", lambda e, wsb=wsb, wdr=wdr, s=s: e.indirect_dma_start(
                out=wsb, out_offset=None, in_=wdr[:, :],
                in_offset=bass.IndirectOffsetOnAxis(ap=widx_i[:, s:s + 1], axis=0),
                bounds_check=get_bc(e), oob_is_err=False), r=["widx_i"], w=[hn], dma=True)

    def w_gu(bw):
        return ((wgs[bw], wg_d, f"wg{bw}"), (wus[bw], wu_d, f"wu{bw}"))

    def w_d(bw):
        return ((wds[bw], wd_d, f"wd{bw}"),)

    def load_x(i):
        s = ORDER[i]
        bw = i % NBW
        for st in range(NSUB):
            r0 = s * T + st * 128
            P.op("sp", lambda e, bw=bw, st=st, r0=r0: e.dma_start(out=xtok[bw][:, st, :], in_=xs_d[r0:r0 + 128, :]),
                 w=[f"xtok{bw}_{st}"], dma=True)

    def stageA(i):
        s = ORDER[i]
        b, bw = i % 2, i % NBW
        P.rec()
        for st in range(NSUB):
            k = (i * NSUB + st) % 2
            pb_, hpb = psb[k], f"psb{k}"
            for c in range(8):
                P.op("pe", lambda e, pb_=pb_, st=st, c=c: e.transpose(out=pb_[:, c * 128:(c + 1) * 128],
                                                                     in_=xtok[bw][:, st, c * 128:(c + 1) * 128], identity=identb),
                     r=[f"xtok{bw}_{st}", "identb"], w=[hpb])
            if True:
                P.op("act", lambda e, pb_=pb_, st=st: e.activation(out=XT[b][:, :, st * 128:(st + 1) * 128],
                                                                   in_=pb_[:].rearrange("p (c t) -> p c t", c=8), func=AF.Identity),
                     r=[hpb], w=[f"XT{b}_{st}"])
            else:
                P.op("dve", lambda e, pb_=pb_, st=st: e.tensor_copy(out=XT[b][:, :, st * 128:(st + 1) * 128],
                                                                    in_=pb_[:].rearrange("p (c t) -> p c t", c=8)),
                     r=[hpb], w=[f"XT{b}_{st}"])
        return P.end()

    def stageB(i):
        s = ORDER[i]
        b, bw = i % 2, i % NBW
        XH = [f"XT{b}_{st}" for st in range(NSUB)]
        P.rec()
        for fch in range(2):
            pg, hpg = npsB()
            for c in range(8):
                P.op("pe", lambda e, pg=pg, c=c, fch=fch: e.matmul(
                    pg[:, 0:T], lhsT=wgs[bw][:, c * 256 + fch * 128:c * 256 + (fch + 1) * 128], rhs=XT[b][:, c, :],
                    start=(c == 0), stop=(c == 7)), r=[f"wg{bw}"] + XH, w=[hpg])
            P.op("act", lambda e, pg=pg: e.activation(out=sgs[b], in_=pg[:, 0:T], func=AF.Silu), r=[hpg], w=[f"sgs{b}"])
            pu, hpu = npsB()
            for c in range(8):
                P.op("pe", lambda e, pu=pu, c=c, fch=fch: e.matmul(
                    pu[:, 0:T], lhsT=wus[bw][:, c * 256 + fch * 128:c * 256 + (fch + 1) * 128], rhs=XT[b][:, c, :],
                    start=(c == 0), stop=(c == 7)), r=[f"wu{bw}"] + XH, w=[hpu])
            P.op("dve", lambda e, pu=pu, fch=fch: e.tensor_tensor(out=aT[b][:, fch, :], in0=pu[:, 0:T], in1=sgs[b], op=ALU.mult),
                 r=[hpu, f"sgs{b}"], w=[f"aT{b}_{fch}"])
        return P.end()

    def stageC(i):
        s = ORDER[i]
        b, bw = i % 2, i % NBW
        P.rec()
        for st in range(NSUB):
            yb = (i * NSUB + st) % NYO
            for half in range(2):
                po, hpo = npsC()
                for fch in range(2):
                    P.op("pe", lambda e, po=po, st=st, fch=fch, half=half: e.matmul(
                        po[:], lhsT=aT[b][:, fch, st * 128:(st + 1) * 128],
                        rhs=wds[bw][:, fch * 1024 + half * 512:fch * 1024 + (half + 1) * 512], start=(fch == 0), stop=(fch == 1)),
                        r=[f"aT{b}_0", f"aT{b}_1", f"wd{bw}"], w=[hpo])
                P.op("dve", lambda e, po=po, yb=yb, half=half: e.tensor_tensor(
                    out=yo[yb][:, half * 512:(half + 1) * 512], in0=po[:], in1=gbc[:, 3, half * 512:(half + 1) * 512], op=ALU.mult),
                    r=[hpo, "gbc3_0", "gbc3_1"], w=[f"yo{yb}{'ab'[half]}"])
            r0 = s * T + st * 128
            P.op("sp", lambda e, yb=yb, r0=r0: e.dma_start(out=ys_d[r0:r0 + 128, :], in_=yo[yb]), r=[f"yo{yb}a", f"yo{yb}b"],
                 w=[f"ys_{s}_{st}"], dma=True)
        return P.end()

    for s in range(min(NBW, NS)):
        load_x(s)
        load_w(s, w_gu)
        load_w(s, w_d)
    for i in range(NS + 2):
        lists = []
        if i < NS:
            lists.append(stageA(i))
        if 0 <= i - 1 < NS:
            lists.append(stageB(i - 1))
        if 0 <= i - 2 < NS:
            lists.append(stageC(i - 2))
        P.play(merge(*lists))
        if i + NBW < NS:
            load_x(i + NBW)
        if i - 1 >= 0 and i - 1 + NBW < NS:
            load_w(i - 1 + NBW, w_gu)
        if i - 2 >= 0 and i - 2 + NBW < NS:
            load_w(i - 2 + NBW, w_d)
    P.fence()

    A.off = mark2
    NB2F = 4
    Y1 = [A.alloc([D], F32) for _ in range(NB2F)]
    Y2 = [A.alloc([D], F32) for _ in range(NB2F)]
    t1 = [A.alloc([D], F32) for _ in range(NB2F)]
    ff = [A.alloc([D], F32) for _ in range(2)]
    r2 = [A.alloc([D], F32) for _ in range(2)]
    qq = [A.alloc([D], F32) for _ in range(2)]
    ob = [A.alloc([D], F32) for _ in range(2)]
    bn2 = [A.alloc([12], F32) for _ in range(2)]
    ag2 = [A.alloc([4], F32) for _ in range(2)]

    def loads2f(j):
        q = j % NB2F
        P.op("pool", lambda e: e.indirect_dma_start(
            out=Y1[q], out_offset=None, in_=ys_d[:, :], in_offset=bass.IndirectOffsetOnAxis(ap=pos_i[:, 0, j:j + 1], axis=0)),
            r=["pos_i"], w=[f"Y1{q}"], dma=True)
        P.op("pool", lambda e: e.indirect_dma_start(
            out=Y2[q], out_offset=None, in_=ys_d[:, :], in_offset=bass.IndirectOffsetOnAxis(ap=pos_i[:, 1, j:j + 1], axis=0)),
            r=["pos_i"], w=[f"Y2{q}"], dma=True)
        P.op("sp", lambda e: e.dma_start(out=t1[q], in_=t1_d[j * 128:(j + 1) * 128, :]), r=[f"t1_d{j}"], w=[f"t1{q}"], dma=True)

    def chain2f(j):
        b = j % 2
        q = j % NB2F
        P.rec()
        P.op("act", lambda e: e.activation(out=ff[b], in_=Y1[q], func=AF.Identity, scale=wts[:, 0, j:j + 1]), r=[f"Y1{q}", "wts"], w=[f"ff{b}"])
        P.op("dve", lambda e: e.scalar_tensor_tensor(out=ff[b], in0=Y2[q], scalar=wts[:, 1, j:j + 1], in1=ff[b], op0=ALU.mult, op1=ALU.add),
             r=[f"Y2{q}", "wts", f"ff{b}"], w=[f"ff{b}"])
        P.op("dve", lambda e: e.tensor_tensor(out=r2[b], in0=ff[b], in1=t1[q], op=ALU.add), r=[f"ff{b}", f"t1{q}"], w=[f"r2{b}"])
        for h in range(2):
            P.op("dve", lambda e, h=h: e.bn_stats(out=bn2[b][:, h * 6:(h + 1) * 6], in_=r2[b][:, h * 512:(h + 1) * 512]), r=[f"r2{b}"], w=[f"bn2{b}"])
        P.op("dve", lambda e: e.bn_aggr(out=ag2[b][:, 0:2], in_=bn2[b]), r=[f"bn2{b}"], w=[f"ag2{b}"])
        P.op("act", lambda e: e.activation(out=ag2[b][:, 2:3], in_=ag2[b][:, 1:2], func=AF.Sqrt, bias=EPSC, scale=1.0), r=[f"ag2{b}", "epsc"], w=[f"ag2b{b}"])
        P.op("dve", lambda e: e.reciprocal(out=ag2[b][:, 2:3], in_=ag2[b][:, 2:3]), r=[f"ag2b{b}"], w=[f"ag2b{b}"])
        P.op("dve", lambda e: e.scalar_tensor_tensor(out=ag2[b][:, 3:4], in0=ag2[b][:, 0:1], scalar=-1.0, in1=ag2[b][:, 2:3], op0=ALU.mult, op1=ALU.mult),
             r=[f"ag2{b}", f"ag2b{b}"], w=[f"ag2c{b}"])
        P.op("act", lambda e: e.activation(out=qq[b], in_=r2[b], func=AF.Identity, bias=ag2[b][:, 3:4], scale=ag2[b][:, 2:3]),
             r=[f"r2{b}", f"ag2b{b}", f"ag2c{b}"], w=[f"qq{b}"])
        P.op("pool", lambda e: e.tensor_tensor(out=qq[b], in0=qq[b], in1=lnr[:, 2, :], op=ALU.mult), r=[f"qq{b}", "lnr"], w=[f"qq{b}"])
        P.op("dve", lambda e: e.tensor_tensor(out=ob[b], in0=qq[b], in1=lnr[:, 3, :], op=ALU.add), r=[f"qq{b}", "lnr"], w=[f"ob{b}"])
        P.op("sp", lambda e: e.dma_start(out=out_d[j * 128:(j + 1) * 128, :], in_=ob[b]), r=[f"ob{b}"], w=[f"out{j}"], dma=True)
        return P.end()

    loads2f(0)
    loads2f(1)
    for j in range(0, NT, 2):
        if j + 2 < NT:
            loads2f(j + 2)
            loads2f(j + 3)
        P.play(merge(chain2f(j), chain2f(j + 1)))


class _StopEmit(Exception):
    pass


def _host_prep(inp, b):
    f = np.float32
    L = 0
    cols = np.zeros((128, NCOL), f)
    cols[:, C_C:C_C + 8] = inp["c"][b].reshape(8, 128).T
    w_in = inp["w_in"][L]
    b_in = inp["b_in"][L]
    qcols = np.concatenate([np.arange(1024 + h * 64, 1024 + (h + 1) * 64) for h in HORDER])
    perm = np.concatenate([np.arange(0, 1024), qcols, np.arange(1536, 1792)])
    w_in_p = np.ascontiguousarray(w_in[:, perm])
    b_in_p = b_in[perm]
    cols[:, C_BIN:C_BIN + 13] = b_in_p[:1664].reshape(13, 128).T
    cols[:, C_CW:C_CW + 124] = inp["conv_w"][L].T.reshape(4, 128, 31).transpose(1, 0, 2).reshape(128, 124)
    cols[:, C_CB:C_CB + 4] = inp["conv_b"][L].reshape(4, 128).T
    cols[:, C_LG:C_LG + 4] = inp["conv_ln_g"][L].reshape(4, 128).T
    cols[:, C_LB:C_LB + 4] = inp["conv_ln_b"][L].reshape(4, 128).T
    cols[:, C_OG:C_OG + 4] = inp["conv_out_g"][L].reshape(4, 128).T
    cols[:, C_PID] = np.arange(128)
    rows = np.zeros((1, NROW), f)
    rows[0, R_BADA:R_BADA + 6144] = inp["b_ada"][L]
    rows[0, R_BV:R_BV + 128] = b_in[1664:1792]
    rows[0, R_SINK:R_SINK + 8] = inp["sinks"][L][HORDER]
    rows[0, R_AOG:R_AOG + 512] = inp["attn_out_g"][L].reshape(8, 64)[HORDER].reshape(-1)
    rows[0, R_BOUT:R_BOUT + D] = inp["b_out"][L]
    rows[0, R_L1G:R_L1G + D] = inp["ln1_g"][L]
    rows[0, R_L1B:R_L1B + D] = inp["ln1_b"][L]
    rows[0, R_L2G:R_L2G + D] = inp["ln2_g"][L]
    rows[0, R_L2B:R_L2B + D] = inp["ln2_b"][L]
    rows[0, R_BR:R_BR + 4] = inp["b_router_group"][L]
    rows[0, R_BR + 4:R_BR + 36] = inp["b_router_expert"][L]
    rows[0, R_SLOT:R_SLOT + NS] = np.arange(NS) * T
    w_out = inp["w_out"][L]
    arows = np.concatenate([np.arange(512 + h * 64, 512 + (h + 1) * 64) for h in HORDER])
    w_out_p = np.ascontiguousarray(np.concatenate([w_out[:512], w_out[arows]], axis=0))
    w_r = np.ascontiguousarray(np.concatenate([inp["w_router_group"][L], inp["w_router_expert"][L]], axis=1))
    return dict(cols=cols, rows=rows, w_in=w_in_p, w_out=w_out_p, w_r=w_r)


def _consts():
    f = np.float32
    cst = np.zeros((128, NK), f)
    cst[:, K_ID:K_ID + 128] = np.eye(128)
    p = np.arange(128)
    cst[:, K_U:K_U + 128] = (p[:, None] < p[None, :])
    cst[:, K_BD:K_BD + 128] = ((p[:, None] // 64) == (p[None, :] // 64)) / 64.0
    m0 = np.where(p[:, None] > p[None, :], 0.0, NEG)
    m1 = np.where(p[:, None] <= p[None, :], 0.0, NEG)
    cst[:, K_M0:K_M0 + 512] = np.tile(m0, (1, 4))
    cst[:, K_M1:K_M1 + 512] = np.tile(m1, (1, 4))
    return cst


def _expert_layout(inp):
    L = 0
    wg = np.ascontiguousarray(inp["w_gate"][L].reshape(NE, 8, 128, DE).transpose(0, 2, 1, 3)).reshape(NE * 128, 2048)
    wu = np.ascontiguousarray(inp["w_up"][L].reshape(NE, 8, 128, DE).transpose(0, 2, 1, 3)).reshape(NE * 128, 2048)
    wd = np.ascontiguousarray(inp["w_down"][L].reshape(NE, 2, 128, D).transpose(0, 2, 1, 3)).reshape(NE * 128, 2048)
    return wg, wu, wd


_CACHE = {}


def kernel(**inputs):
    inp = {k: np.asarray(v) for k, v in inputs.items()}
    dbg = _CACHE.get("dbg")
    nc = build_program(dbg)
    cst = _consts()
    wg, wu, wd = _expert_layout(inp)
    w_ada = np.ascontiguousarray(inp["w_ada"][0])
    in_maps = []
    ncores = _CACHE.get("ncores", 8)
    for b in range(ncores):
        hp = _host_prep(inp, b)
        m = dict(x=np.ascontiguousarray(inp["x"][b]), cols=hp["cols"], rows=hp["rows"], cst=cst, w_ada=w_ada,
                 w_in=hp["w_in"], w_out=hp["w_out"], w_r=hp["w_r"], wg=wg, wu=wu, wd=wd)
        if _CACHE.get("p2only"):
            m["z_in"] = _CACHE["z_in"]
        in_maps.append(m)
    if _CACHE.get("trace"):
        res = run_bass_kernel_spmd(nc, in_maps, core_ids=list(range(ncores)), trace=True)
        print("EXEC_NS", res.exec_time_ns)
    else:
        res = run_bass_kernel_spmd(nc, in_maps, core_ids=list(range(ncores)))
    if dbg:
        return [np.asarray(r["dbg"]) for r in res.results]
    out = np.stack([np.asarray(r["out"]) for r in res.results], axis=0).astype(np.float32)
    return out
```

```python
import os
import numpy as np
import concourse.bass as bass
import concourse.mybir as mybir
from concourse.bass_utils import run_bass_kernel_spmd

F32 = mybir.dt.float32
BF16 = mybir.dt.bfloat16
I32 = mybir.dt.int32
ALU = mybir.AluOpType
AF = mybir.ActivationFunctionType
AX = mybir.AxisListType

D = 1024
S = 4096
NT = S // 128
NMT = S // 512
DIN = 1792
NE = 32
DE = 256
ALPHA = 2.0 ** 0.25
EPS = 1e-5
NEG = -30000.0
T = 384
NS = (2 * S + NE * (T - 1) + T - 1) // T
NSUB = T // 128
HORDER = [0, 4, 1, 5, 2, 6, 3, 7]

C_C = 0
C_BIN = 8
C_CW = 21
C_CB = 145
C_LG = 149
C_LB = 153
C_OG = 157
C_PID = 161
NCOL = 162
R_BADA = 0
R_BV = 6144
R_SINK = 6272
R_AOG = 6280
R_BOUT = 6792
R_L1G = 7816
R_L1B = 8840
R_L2G = 9864
R_L2B = 10888
R_BR = 11912
R_SLOT = 11948
NROW = 12012
K_ID = 0
K_U = 128
K_BD = 256
K_M0 = 384
K_M1 = 896
NK = 1408


class Prog:
    def __init__(self, nc, sems):
        self.nc = nc
        self.ops = []
        self.last_w = {}
        self.readers = {}
        self.eng_sem = {e: sems[i] for i, e in enumerate(["pe", "act", "dve", "pool"])}
        rest = sems[4:]
        n_sp = (len(rest) * 5) // 10
        n_pool = (len(rest) * 4) // 10
        self.dma_pool = {"sp": rest[:n_sp], "pool": rest[n_sp:n_sp + n_pool], "act": rest[n_sp + n_pool:]}
        self._rec = None

    def rec(self):
        assert self._rec is None
        self._rec = []

    def end(self):
        l = self._rec
        self._rec = None
        return l

    def play(self, lst):
        assert self._rec is None
        for o in lst:
            self.op(*o)

    def op(self, eng, fn, r=(), w=(), dma=False):
        if self._rec is not None:
            self._rec.append((eng, fn, list(r), list(w), dma))
            return None
        i = len(self.ops)
        w = list(w) + [h for h in r if h.startswith("ps") and h not in w]
        raw, oth = set(), set()
        for h in r:
            if h in self.last_w:
                raw.add(self.last_w[h])
        for h in w:
            if h in self.last_w:
                oth.add(self.last_w[h])
            for j in self.readers.get(h, ()):
                oth.add(j)
        for h in w:
            self.last_w[h] = i
            self.readers[h] = []
        for h in r:
            self.readers.setdefault(h, []).append(i)
        deps = []
        for j in sorted(raw | oth):
            p = self.ops[j]
            if j == i:
                continue
            if (not p["dma"]) and p["eng"] == eng:
                if eng == "pe" or j not in raw:
                    continue
            deps.append(j)
        self.ops.append(dict(eng=eng, fn=fn, deps=deps, dma=dma, sig=False))
        for j in deps:
            self.ops[j]["sig"] = True
        return i

    def fence(self, engs=("pe", "act", "dve", "pool", "sp")):
        hs = list(self.last_w.keys())
        for e in engs:
            self.op(e, None, r=hs, w=["_fence_" + e])

    def emit(self):
        nc = self.nc
        ticket = {e: 0 for e in self.eng_sem}
        dma_next = {q: 0 for q in self.dma_pool}
        dma_uses = {}
        for o in self.ops:
            if o["dma"]:
                q = o["eng"]
                pool = self.dma_pool[q]
                sem = pool[dma_next[q] % len(pool)]
                dma_next[q] += 1
                u = dma_uses.get(id(sem), 0)
                o["pre"] = (sem, 16 * u)
                dma_uses[id(sem)] = u + 1
                o["ev"] = (sem, 16 * (u + 1))
            elif o["sig"] and o["fn"] is not None:
                ticket[o["eng"]] += 1
                o["ev"] = (self.eng_sem[o["eng"]], ticket[o["eng"]])
        ops = self.ops

        def run(engname, eobj):
            waited = {}

            def wait(sem, val):
                if val <= 0:
                    return
                if waited.get(id(sem), 0) >= val:
                    return
                eobj.wait_ge(sem, val)
                waited[id(sem)] = val

            for o in ops:
                if o["eng"] != engname:
                    continue
                for j in o["deps"]:
                    ev = ops[j].get("ev")
                    if ev is not None:
                        wait(*ev)
                if o["fn"] is None:
                    continue
                if o["dma"]:
                    wait(*o["pre"])
                    ins = o["fn"](eobj)
                    ins.then_inc(o["ev"][0], 16)
                else:
                    ins = o["fn"](eobj)
                    if o["sig"]:
                        ins.then_inc(o["ev"][0], 1)

        with nc.Block() as block:
            @block.tensor
            def _(e):
                run("pe", e)

            @block.scalar
            def _(e):
                run("act", e)

            @block.vector
            def _(e):
                run("dve", e)

            @block.gpsimd
            def _(e):
                run("pool", e)

            @block.sync
            def _(e):
                run("sp", e)


class _Stop(Exception):
    pass


def merge(*lists):
    lists = [l for l in lists if l]
    out = []
    idx = [0] * len(lists)
    total = sum(len(l) for l in lists)
    while len(out) < total:
        best, bv = None, None
        for k, l in enumerate(lists):
            if idx[k] < len(l):
                v = (idx[k] + 0.5) / len(l)
                if bv is None or v < bv:
                    best, bv = k, v
        out.append(lists[best][idx[best]])
        idx[best] += 1
    return out


class Arena:
    def __init__(self, t, nbytes):
        self.t = t
        self.nbytes = nbytes
        self.off = 0

    def alloc(self, shape, dt):
        esz = 4 if dt in (F32, I32) else 2
        n = int(np.prod(shape)) * esz
        n = (n + 63) // 64 * 64
        assert self.off + n <= self.nbytes, ("arena overflow", self.off, n, self.nbytes)
        v = self.t[:, self.off // 4:(self.off + n) // 4]
        self.off += n
        if dt != F32:
            v = v.bitcast(dt)
        v = v[:, 0:int(np.prod(shape))]
        if len(shape) == 2:
            return v.rearrange("p (a b) -> p a b", a=shape[0])
        if len(shape) == 3:
            return v.rearrange("p (a b c) -> p a b c", a=shape[0], b=shape[1])
        return v


def build_program(dbg=None):
    nc = bass.Bass("TRN2", target_bir_lowering=False)
    try:
        return _build_program(nc, dbg)
    except _Stop:
        return nc


def _build_program(nc, dbg=None):
    dr = {}

    def din(name, shape, dt=F32):
        dr[name] = nc.dram_tensor(name, list(shape), dt, kind="ExternalInput").ap()
        return dr[name]

    x_d = din("x", [S, D])
    cols_d = din("cols", [128, NCOL])
    rows_d = din("rows", [1, NROW])
    cst_d = din("cst", [128, NK])
    wada_d = din("w_ada", [D, 6 * D])
    win_d = din("w_in", [D, DIN])
    wout_d = din("w_out", [D, D])
    wr_d = din("w_r", [D, 36])
    wg_d = din("wg", [NE * 128, 2048])
    wu_d = din("wu", [NE * 128, 2048])
    wd_d = din("wd", [NE * 128, 2048])
    out_d = nc.dram_tensor("out", [S, D], F32, kind="ExternalOutput").ap()
    if _CACHE.get("p2only"):
        z_d = din("z_in", [S, D])
    else:
        z_d = nc.dram_tensor("z_scr", [S, D], F32, kind="Internal").ap()
    xs_d = nc.dram_tensor("xs_scr", [NS * T, D], BF16, kind="Internal").ap()
    ys_d = nc.dram_tensor("ys_scr", [NS * T, D], F32, kind="Internal").ap()
    dbg_d = None
    if dbg:
        dbg_d = nc.dram_tensor("dbg", list(dbg["shape"]), F32, kind="ExternalOutput").ap()

    import contextlib
    with contextlib.ExitStack() as st:
        ARENA_BYTES = 206 * 1024
        arena_t = st.enter_context(nc.sbuf_tensor("arena", [128, ARENA_BYTES // 4], F32))
        ps = [st.enter_context(nc.psum_tensor(f"ps{i}", [128, 512], F32)) for i in range(6)]
        psb = [st.enter_context(nc.psum_tensor(f"psb{i}", [128, 1024], BF16)) for i in range(2)]
        sems = [st.enter_context(nc.semaphore(f"s{i}")) for i in range(_CACHE.get("nsem", 48))]
        P = Prog(nc, sems)
        A = Arena(arena_t, ARENA_BYTES)
        psn = [0]

        def nps():
            i = psn[0] % 6
            psn[0] += 1
            return ps[i], f"ps{i}"

        cols = A.alloc([NCOL], F32)
        identf = A.alloc([128], F32)
        identb = A.alloc([128], BF16)
        onesb = A.alloc([128], BF16)
        ones512 = A.alloc([128], BF16)
        bdb = A.alloc([128], BF16)
        Ub = A.alloc([128], BF16)
        maskT = A.alloc([2, 512], BF16)
        modc = A.alloc([32], F32)
        g1bc = A.alloc([D], F32)
        gbrow = A.alloc([D], BF16)
        EPSC = A.alloc([1], F32)
        EPSC2 = A.alloc([1], F32)
        mark_persist = A.off
        cst = A.alloc([NK], F32)
        gbc = A.alloc([4, D], F32)
        mod_d = nc.dram_tensor("mod_scr", [1, 3 * D], F32, kind="Internal").ap()

        P.op("sp", lambda e: e.dma_start(out=cols, in_=cols_d), w=["cols"], dma=True)
        P.op("sp", lambda e: e.dma_start(out=cst, in_=cst_d), w=["cst0"], dma=True)
        P.op("sp", lambda e: e.dma_start(out=identf, in_=cst_d[:, K_ID:K_ID + 128]), w=["cst"], dma=True)
        P.op("dve", lambda e: e.tensor_copy(out=identb, in_=identf), r=["cst"], w=["identb"])
        P.op("dve", lambda e: e.memset(onesb, 1.0), w=["onesb"])
        P.op("dve", lambda e: e.memset(EPSC, EPS), w=["epsc"])
        P.op("dve", lambda e: e.memset(EPSC2, EPS / (ALPHA * ALPHA)), w=["epsc2"])
        P.op("dve", lambda e: e.memset(ones512, 1.0 / 512.0), w=["ones512"])
        P.op("dve", lambda e: e.tensor_copy(out=bdb, in_=cst[:, K_BD:K_BD + 128]), r=["cst0"], w=["bdb"])
        P.op("dve", lambda e: e.tensor_copy(out=Ub, in_=cst[:, K_U:K_U + 128]), r=["cst0"], w=["Ub"])
        P.op("dve", lambda e: e.tensor_copy(out=maskT.rearrange("p a b -> p (a b)"), in_=cst[:, K_M0:K_M0 + 1024]),
             r=["cst0"], w=["maskT"])

        ph0 = A.off
        cact = A.alloc([8], F32)
        cbc = A.alloc([8, 128], BF16)
        wab = [A.alloc([8, 512], BF16) for _ in range(2)]
        badab = A.alloc([512], F32)
        modb = A.alloc([4 * D], F32)
        P.op("act", lambda e: e.activation(out=cact, in_=cols[:, C_C:C_C + 8], func=AF.Silu), r=["cols"], w=["cact"])
        for c in range(8):
            P.op("dve", lambda e, c=c: e.tensor_scalar(out=cbc[:, c, :], in0=onesb, scalar1=cact[:, c:c + 1],
                                                       scalar2=None, op0=ALU.mult),
                 r=["cact", "onesb"], w=["cbc"])
        for blk in range(12):
            wb = wab[blk % 2]
            hw = f"wab{blk % 2}"
            P.op("pool", lambda e, blk=blk, wb=wb: e.dma_start(
                out=wb, in_=wada_d[:, blk * 512:(blk + 1) * 512].rearrange("(c p) n -> p c n", p=128)),
                w=[hw], dma=True)
            P.op("sp", lambda e, blk=blk: e.dma_start(
                out=badab, in_=rows_d[0:1, R_BADA + blk * 512:R_BADA + (blk + 1) * 512].partition_broadcast(128)),
                w=["badab"], dma=True)
            pt, hp = nps()
            for c in range(8):
                P.op("pe", lambda e, c=c, pt=pt, wb=wb: e.matmul(pt[:], lhsT=cbc[:, c, :], rhs=wb[:, c, :],
                                                               start=(c == 0), stop=(c == 7)),
                     r=["cbc", hw], w=[hp])
            if blk < 4:
                dst = modb[:, blk * 512:(blk + 1) * 512]
                hd = f"modb{blk}"
            else:
                gi = (blk - 4) // 2
                dst = gbc[:, gi, ((blk - 4) % 2) * 512:((blk - 4) % 2 + 1) * 512]
                hd = f"gbc{gi}_{blk % 2}"
            P.op("dve", lambda e, pt=pt, dst=dst: e.tensor_tensor(out=dst, in0=pt[:], in1=badab, op=ALU.add),
                 r=[hp, "badab"], w=[hd])
        srcs = []
        for c in range(8):
            srcs.append((modb[:, c * 128:(c + 1) * 128], f"modb{c // 4}", c, 0.0))
        for c in range(8):
            srcs.append((modb[:, 1024 + c * 128:1024 + (c + 1) * 128], f"modb{2 + c // 4}", 8 + c, 1.0))
        for c in range(8):
            srcs.append((gbc[:, 1, c * 128:(c + 1) * 128], f"gbc1_{c // 4}", 16 + c, 0.0))
        for c in range(8):
            srcs.append((gbc[:, 2, c * 128:(c + 1) * 128], f"gbc2_{c // 4}", 24 + c, 1.0))
        for (src, hs, col, add) in srcs:
            pt, hp = nps()
            P.op("pe", lambda e, pt=pt, src=src: e.transpose(out=pt[:, 0:128], in_=src, identity=identf),
                 r=[hs, "cst"], w=[hp])
            P.op("dve", lambda e, pt=pt, col=col, add=add: e.tensor_scalar(
                out=modc[:, col:col + 1], in0=pt[:, 0:1], scalar1=add, scalar2=None, op0=ALU.add),
                r=[hp], w=["modc"])
        G_ALL = [f"gbc{gi}_{h}" for gi in range(4) for h in range(2)]
        P.op("sp", lambda e: e.dma_start(out=mod_d, in_=gbc[0:1, 1:4, :].rearrange("p a d -> p (a d)")), r=G_ALL, w=["mod_d"], dma=True)
        P.op("dve", lambda e: e.tensor_scalar(out=g1bc, in0=gbc[:, 0, :], scalar1=1.0 / ALPHA, scalar2=None, op0=ALU.mult), r=G_ALL, w=["g1bc"])
        boutb = modb[:, 0:D]
        P.op("sp", lambda e: e.dma_start(out=boutb, in_=rows_d[0:1, R_BOUT:R_BOUT + D].partition_broadcast(128)),
             w=["modb0", "modb1"], dma=True)
        P.op("dve", lambda e: e.tensor_tensor(out=boutb, in0=boutb, in1=g1bc, op=ALU.mult), r=["modb0", "modb1", "g1bc"], w=["modb0", "modb1"])
        P.op("dve", lambda e: e.tensor_copy(out=gbrow, in_=boutb), r=["modb0", "modb1"], w=["gbrow"])
        P.fence()
        if _CACHE.get("stop") == 0:
            P.op("sp", lambda e: e.dma_start(out=dbg_d[0:128, 0:32], in_=modc), r=["modc"], w=["dbg"], dma=True)
            P.op("sp", lambda e: e.dma_start(out=dbg_d[128:256, :], in_=gbc[:, 0, :]), r=["gbc0_0", "gbc0_1"], w=["dbg2"], dma=True)
            P.fence()
            P.emit()
            return nc
        A.off = mark_persist

        win = A.alloc([8, DIN], BF16)
        modc2 = A.alloc([32], F32)
        P.op("dve", lambda e: e.tensor_copy(out=modc2, in_=modc), r=["modc"], w=["modc2"])
        wout = A.alloc([8, D], BF16)
        diag = A.alloc([124, 128], BF16)
        xt = A.alloc([4, D], BF16)
        hT = A.alloc([8, 512], BF16)
        vr = [A.alloc([4, 542], BF16) for _ in range(2)]
        kr = [[A.alloc([640], BF16) for _ in range(2)] for _ in range(2)]
        va = [A.alloc([5, 130], BF16) for _ in range(2)]
        qT2 = [A.alloc([4, 512], BF16) for _ in range(2)]
        sig = A.alloc([512], F32)
        ybf2 = [A.alloc([4, 512], BF16) for _ in range(2)]
        y2bf = A.alloc([4, 512], BF16)
        rstd2 = [A.alloc([512], F32) for _ in range(2)]
        nmr2 = [A.alloc([512], F32) for _ in range(2)]
        zc2 = [A.alloc([512], F32) for _ in range(2)]
        sc2_ = [A.alloc([512], F32) for _ in range(2)]
        s2bf2 = [A.alloc([512], BF16) for _ in range(2)]
        r2c2 = [A.alloc([512], F32) for _ in range(2)]
        ycT = A.alloc([8, 512], BF16)
        mx2 = [A.alloc([8], F32) for _ in range(2)]
        mxb2 = [A.alloc([8], BF16) for _ in range(2)]
        nmx2 = [A.alloc([8], F32) for _ in range(2)]
        dcat2 = [A.alloc([2, 512], BF16) for _ in range(2)]
        ET2 = [A.alloc([4, 512], BF16) for _ in range(2)]
        es_t2 = [A.alloc([8], F32) for _ in range(2)]
        den2 = [A.alloc([8], F32) for _ in range(2)]
        osb2 = [A.alloc([8, 64], F32) for _ in range(2)]
        osq2 = [A.alloc([8, 64], F32) for _ in range(2)]
        ssq2 = [A.alloc([8], F32) for _ in range(2)]
        yat2 = [A.alloc([512], BF16) for _ in range(2)]
        rows_sb = A.alloc([128 + 8 + 512], F32)
        xr2 = [A.alloc([D], F32) for _ in range(2)]
        bnst2 = [A.alloc([12], F32) for _ in range(2)]
        bnag2 = [A.alloc([4], F32) for _ in range(2)]
        print("phase1 arena bytes", A.off)

        def mk_nps(ids):
            st_ = [0]

            def f():
                i = ids[st_[0] % len(ids)]
                st_[0] += 1
                return ps[i], f"ps{i}"
            return f

        bv_bc = rows_sb[:, 0:128]
        sink_bc = rows_sb[:, 128:136]
        aog_bc = rows_sb[:, 136:648]
        P.op("sp", lambda e: e.dma_start(out=rows_sb, in_=rows_d[0:1, R_BV:R_BV + 128 + 8 + 512].partition_broadcast(128)),
             w=["rows_sb"], dma=True)
        for c in range(8):
            P.op("pool", lambda e, c=c: e.dma_start(out=win[:, c, :], in_=win_d[c * 128:(c + 1) * 128, :]), w=[f"win{c}"], dma=True)
            P.op("pool", lambda e, c=c: e.dma_start(out=wout[:, c, :], in_=wout_d[c * 128:(c + 1) * 128, :]), w=[f"wout{c}"], dma=True)
        for c in range(8):
            P.op("dve", lambda e, c=c: e.tensor_tensor(out=wout[:, c, :], in0=wout[:, c, :], in1=g1bc, op=ALU.mult),
                 r=[f"wout{c}", "g1bc"], w=[f"wout{c}"])
        WINH = [f"win{c}" for c in range(8)]
        WOUTH = [f"wout{c}" for c in range(8)]
        SK = _CACHE.get("skip", set())
        for c in range(4 if "diag" not in SK else 0):
            P.op("dve", lambda e, c=c: e.tensor_tensor(
                out=diag[:, c * 31:(c + 1) * 31, :], in0=identb.unsqueeze(1).to_broadcast([128, 31, 128]),
                in1=cols[:, C_CW + c * 31:C_CW + (c + 1) * 31].unsqueeze(2).to_broadcast([128, 31, 128]), op=ALU.mult),
                r=["identb", "cols"], w=["diag"])
        if "memset" not in SK:
            P.op("pool", lambda e: e.memset(vr[0], 0.0), w=["vr0"])
            P.op("pool", lambda e: e.memset(vr[1], 0.0), w=["vr1"])
        for par in range(2 if "memset" not in SK else 0):
            for g in range(2):
                P.op("pool", lambda e, par=par, g=g: e.memset(kr[par][g], 0.0), w=[f"kr{par}"])
            P.op("pool", lambda e, par=par: e.memset(va[par], 0.0), w=[f"va{par}"])
            P.op("dve", lambda e, par=par: e.memset(va[par][:, 1:, 64:65], 1.0), r=[f"va{par}"], w=[f"va{par}"])
            P.op("dve", lambda e, par=par: e.memset(va[par][:, 1:, 129:130], 1.0), r=[f"va{par}"], w=[f"va{par}"])

        if _CACHE.get("stop") == 1:
            P.fence()
            P.op("sp", lambda e: e.dma_start(out=dbg_d[0:128, 0:512], in_=g1bc[:, 0:512]), r=["g1bc"], w=["dbg"], dma=True)
            P.fence()
            P.emit()
            return nc
        nps_s1 = mk_nps([0, 1])
        psb1_f32 = psb[1][:, :].bitcast(F32)
        nps_at = [mk_nps([2, 3]), mk_nps([4, 5])]
        nps_ep = [mk_nps([2, 3]), mk_nps([4, 5])]

        def stage1(mt):
            par = mt % 2
            t0 = mt * 512
            vcur, vprev = vr[par], vr[1 - par]
            hv, hvp = f"vr{par}", f"vr{1 - par}"
            kT, kTp = kr[par], kr[1 - par]
            hk, hkp = f"kr{par}", f"kr{1 - par}"
            vaug, vaugp = va[par], va[1 - par]
            hva, hvap = f"va{par}", f"va{1 - par}"
            qT, hq = qT2[par], f"qT{par}"
            ybf, hy = ybf2[par], f"ybf{par}"
            rstd_sb, hrs = rstd2[par], f"rstd{par}"
            nmr_sb, hnm = nmr2[par], f"nmr{par}"
            nps = nps_s1
            P.rec()
            P.op("pool", lambda e: e.dma_start(out=xt, in_=x_d[t0:t0 + 512, :].rearrange("(s p) d -> p s d", p=128)),
                 w=["xt"], dma=True)
            for c in range(8):
                ptx = psb[0][:, 0:512]
                hp = "psb0"
                for s in range(4):
                    P.op("pe", lambda e, s=s, c=c: e.transpose(
                        out=ptx[:, s * 128:(s + 1) * 128], in_=xt[:, s, c * 128:(c + 1) * 128], identity=identb),
                        r=["xt", "identb"], w=[hp])
                P.op("act", lambda e, c=c: e.activation(
                    out=hT[:, c, :], in_=ptx[:, 0:512], func=AF.Identity, bias=modc2[:, c:c + 1], scale=modc2[:, 8 + c:9 + c]),
                    r=[hp, "modc2"], w=["hT"])
            if mt > 0:
                P.op("pool", lambda e: e.tensor_copy(out=vcur[:, :, 0:30], in_=vprev[:, :, 512:542]), r=[hvp], w=[hv])
                for g in range(2):
                    P.op("pool", lambda e, g=g: e.tensor_copy(out=kT[g][:, 0:128], in_=kTp[g][:, 512:640]), r=[hkp], w=[hk])
                P.op("pool", lambda e: e.tensor_copy(out=vaug[:, 0, :], in_=vaugp[:, 4, :]), r=[hvap], w=[hva])
            for c in range(4):
                pb, hpb = nps()
                for k in range(8):
                    P.op("pe", lambda e, pb=pb, k=k, c=c: e.matmul(
                        pb[:], lhsT=win[:, k, 512 + c * 128:512 + (c + 1) * 128], rhs=hT[:, k, :],
                        start=(k == 0), stop=(k == 7)), r=WINH + ["hT"], w=[hpb])
                P.op("act", lambda e, pb=pb, c=c: e.activation(
                    out=sig, in_=pb[:], func=AF.Sigmoid, bias=cols[:, C_BIN + 4 + c:C_BIN + 5 + c], scale=1.0),
                    r=[hpb, "cols"], w=["sig"])
                pa, hpa = nps()
                for k in range(8):
                    P.op("pe", lambda e, pa=pa, k=k, c=c: e.matmul(
                        pa[:], lhsT=win[:, k, c * 128:(c + 1) * 128], rhs=hT[:, k, :],
                        start=(k == 0), stop=(k == 7)), r=WINH + ["hT"], w=[hpa])
                P.op("dve", lambda e, pa=pa, c=c: e.scalar_tensor_tensor(
                    out=vcur[:, c, 30:542], in0=pa[:], scalar=cols[:, C_BIN + c:C_BIN + c + 1], in1=sig,
                    op0=ALU.add, op1=ALU.mult), r=[hpa, "sig", "cols"], w=[hv])
            for i in range(4):
                pq, hpq = nps()
                for k in range(8):
                    P.op("pe", lambda e, pq=pq, k=k, i=i: e.matmul(
                        pq[:], lhsT=win[:, k, 1024 + i * 128:1024 + (i + 1) * 128], rhs=hT[:, k, :],
                        start=(k == 0), stop=(k == 7)), r=WINH + ["hT"], w=[hpq])
                P.op("dve", lambda e, pq=pq, i=i: e.tensor_scalar(
                    out=qT[:, i, :], in0=pq[:], scalar1=cols[:, C_BIN + 8 + i:C_BIN + 9 + i], scalar2=0.125,
                    op0=ALU.add, op1=ALU.mult), r=[hpq, "cols"], w=[hq])
            pk, hpk = nps()
            for k in range(8):
                P.op("pe", lambda e, k=k: e.matmul(
                    pk[:], lhsT=win[:, k, 1536:1664], rhs=hT[:, k, :], start=(k == 0), stop=(k == 7)),
                    r=WINH + ["hT"], w=[hpk])
            for g in range(2):
                P.op("act", lambda e, g=g: e.activation(
                    out=kT[g][g * 64:(g + 1) * 64, 128:640], in_=pk[g * 64:(g + 1) * 64, :],
                    func=AF.Identity, bias=cols[g * 64:(g + 1) * 64, C_BIN + 12:C_BIN + 13], scale=1.0),
                    r=[hpk, "cols"], w=[hk])
            pv, hpv = nps()
            for s in range(4):
                for k in range(8):
                    P.op("pe", lambda e, s=s, k=k: e.matmul(
                        pv[:, s * 128:(s + 1) * 128], lhsT=hT[:, k, s * 128:(s + 1) * 128], rhs=win[:, k, 1664:1792],
                        start=(k == 0), stop=(k == 7)), r=WINH + ["hT"], w=[hpv])
            for s in range(4):
                blk = s + 1
                P.op("dve", lambda e, s=s, blk=blk: e.tensor_tensor(
                    out=vaug[:, blk, :].rearrange("p (g d) -> p g d", g=2)[:, :, 0:64],
                    in0=pv[:, s * 128:(s + 1) * 128].rearrange("p (g d) -> p g d", g=2),
                    in1=bv_bc.rearrange("p (g d) -> p g d", g=2), op=ALU.add),
                    r=[hpv, "rows_sb"], w=[hva])
            for c in range(4):
                py, hpy = nps()
                for j in range(31):
                    P.op("pe", lambda e, py=py, c=c, j=j: e.matmul(
                        py[:], lhsT=diag[:, c * 31 + j, :], rhs=vcur[:, c, j:j + 512], start=(j == 0), stop=(j == 30)),
                        r=["diag", hv], w=[hpy])
                P.op("act", lambda e, py=py, c=c: e.activation(
                    out=ybf[:, c, :], in_=py[:], func=AF.Identity, bias=cols[:, C_CB + c:C_CB + c + 1], scale=1.0),
                    r=[hpy, "cols"], w=[hy])
                P.op("act", lambda e, py=py, c=c: e.activation(
                    out=y2bf[:, c, :], in_=py[:], func=AF.Square, bias=cols[:, C_CB + c:C_CB + c + 1], scale=1.0),
                    r=[hpy, "cols"], w=["y2bf"])
            pm, hpm = nps()
            for c in range(4):
                P.op("pe", lambda e, c=c: e.matmul(pm[:], lhsT=ones512, rhs=ybf[:, c, :], start=(c == 0), stop=(c == 3)),
                     r=["ones512", hy], w=[hpm])
            pe2, hpe2 = nps()
            for c in range(4):
                P.op("pe", lambda e, c=c: e.matmul(pe2[:], lhsT=ones512, rhs=y2bf[:, c, :], start=(c == 0), stop=(c == 3)),
                     r=["ones512", "y2bf"], w=[hpe2])
            P.op("act", lambda e: e.activation(out=nmr_sb, in_=pm[:], func=AF.Identity), r=[hpm], w=[hnm])
            P.op("dve", lambda e: e.tensor_tensor(out=rstd_sb, in0=nmr_sb, in1=nmr_sb, op=ALU.mult), r=[hnm], w=[hrs])
            P.op("dve", lambda e: e.tensor_tensor(out=rstd_sb, in0=pe2[:], in1=rstd_sb, op=ALU.subtract), r=[hpe2, hrs], w=[hrs])
            P.op("act", lambda e: e.activation(out=rstd_sb, in_=rstd_sb, func=AF.Sqrt, bias=EPSC, scale=1.0), r=[hrs, "epsc"], w=[hrs])
            P.op("dve", lambda e: e.reciprocal(out=rstd_sb, in_=rstd_sb), r=[hrs], w=[hrs])
            P.op("dve", lambda e: e.scalar_tensor_tensor(out=nmr_sb, in0=nmr_sb, scalar=-1.0, in1=rstd_sb, op0=ALU.mult, op1=ALU.mult),
                 r=[hnm, hrs], w=[hnm])
            return P.end()

        def stage2(mt):
            par = mt % 2
            kT_l, vaug_l = kr[par], va[par]
            hk, hva = f"kr{par}", f"va{par}"
            qT, hq = qT2[par], f"qT{par}"
            ybf, hy = ybf2[par], f"ybf{par}"
            rstd_sb, hrs = rstd2[par], f"rstd{par}"
            nmr_sb, hnm = nmr2[par], f"nmr{par}"

            def convln_chain(c):
                k = c % 2
                zc, sc_, s2bf, r2c = zc2[k], sc2_[k], s2bf2[k], r2c2[k]
                P.rec()
                P.op("dve", lambda e: e.tensor_tensor(out=zc, in0=ybf[:, c, :], in1=rstd_sb, op=ALU.mult),
                     r=[hy, hrs], w=[f"zc{k}"])
                P.op("dve", lambda e: e.tensor_tensor(out=zc, in0=zc, in1=nmr_sb, op=ALU.add), r=[f"zc{k}", hnm], w=[f"zc{k}"])
                P.op("act", lambda e: e.activation(out=sc_, in_=zc, func=AF.Silu, bias=cols[:, C_LB + c:C_LB + c + 1],
                                                   scale=cols[:, C_LG + c:C_LG + c + 1]), r=[f"zc{k}", "cols"], w=[f"sc{k}"])
                P.op("act", lambda e: e.activation(out=s2bf, in_=sc_, func=AF.Square), r=[f"sc{k}"], w=[f"s2bf{k}"])
                pr, hpr = psb1_f32, "psb1"
                P.op("pe", lambda e: e.matmul(pr[:], lhsT=bdb, rhs=s2bf, start=True, stop=True), r=["bdb", f"s2bf{k}"], w=[hpr])
                P.op("act", lambda e: e.activation(out=r2c, in_=pr[:], func=AF.Sqrt, bias=EPSC, scale=1.0), r=[hpr, "epsc"], w=[f"r2c{k}"])
                P.op("dve", lambda e: e.reciprocal(out=r2c, in_=r2c), r=[f"r2c{k}"], w=[f"r2c{k}"])
                P.op("dve", lambda e: e.scalar_tensor_tensor(out=ycT[:, c, :], in0=sc_, scalar=cols[:, C_OG + c:C_OG + c + 1],
                                                             in1=r2c, op0=ALU.mult, op1=ALU.mult),
                     r=[f"sc{k}", f"r2c{k}", "cols"], w=[f"ycTc{c}"])
                return P.end()

            def attn_chain(s):
                k = s % 2
                mx, mxb, nmx, dcat, ET = mx2[k], mxb2[k], nmx2[k], dcat2[k], ET2[k]
                es_t, den, osb, osq, ssq, yat = es_t2[k], den2[k], osb2[k], osq2[k], ssq2[k], yat2[k]
                mynps = nps_at[k]
                n = mt * 4 + s
                qs = slice(s * 128, (s + 1) * 128)
                P.rec()
                for i in range(4):
                    pS, hpS = mynps()
                    for g in range(2):
                        P.op("pe", lambda e, pS=pS, i=i, g=g: e.matmul(
                            pS[:, g * 256:(g + 1) * 256], lhsT=qT[:, i, qs], rhs=kT_l[g][:, s * 128:s * 128 + 256],
                            start=True, stop=True), r=[hq, hk], w=[hpS])
                    P.op("dve", lambda e, pS=pS, i=i: e.tensor_reduce(
                        out=mx[:, 2 * i:2 * i + 2], in_=pS[:].rearrange("p (g k) -> p g k", g=2), axis=AX.X, op=ALU.max),
                        r=[hpS], w=[f"mx{k}"])
                P.op("dve", lambda e: e.tensor_copy(out=mxb, in_=mx), r=[f"mx{k}"], w=[f"mxb{k}"])
                P.op("dve", lambda e: e.tensor_scalar(out=nmx, in0=mxb, scalar1=-1.0, scalar2=None, op0=ALU.mult), r=[f"mxb{k}"], w=[f"nmx{k}"])
                for g in range(2):
                    P.op("dve", lambda e, g=g: e.tensor_tensor(
                        out=dcat[:, g, :].rearrange("p (i q) -> p i q", i=4), in0=identb.unsqueeze(1).to_broadcast([128, 4, 128]),
                        in1=nmx.rearrange("p (i g) -> p i g", g=2)[:, :, g:g + 1].to_broadcast([128, 4, 128]), op=ALU.mult),
                        r=["identb", f"nmx{k}"], w=[f"dcat{k}_{g}"])
                khs = [1] if n == 0 else [0, 1]
                for g in range(2):
                    for kh in khs:
                        pT, hpT = mynps()
                        kc = slice(s * 128 + kh * 128, s * 128 + kh * 128 + 128)
                        P.op("pe", lambda e, pT=pT, g=g, kc=kc: e.matmul(
                            pT[:].rearrange("p (i q) -> p i q", i=4), lhsT=kT_l[g][:, kc], rhs=qT[:, :, qs], start=True, stop=False),
                            r=[hk, hq], w=[hpT])
                        P.op("pe", lambda e, pT=pT, g=g: e.matmul(pT[:], lhsT=onesb, rhs=dcat[:, g, :], start=False, stop=False),
                             r=["onesb", f"dcat{k}_{g}"], w=[hpT])
                        P.op("pe", lambda e, pT=pT, kh=kh: e.matmul(pT[:], lhsT=identb, rhs=maskT[:, kh, :], start=False, stop=True),
                             r=["identb", "maskT"], w=[hpT])
                        P.op("act", lambda e, pT=pT, g=g, kh=kh: e.activation(out=ET[:, g * 2 + kh, :], in_=pT[:], func=AF.Exp),
                             r=[hpT], w=[f"ET{k}_{g}{kh}"])
                P.op("dve", lambda e: e.tensor_tensor(out=es_t, in0=sink_bc, in1=nmx, op=ALU.add), r=["rows_sb", f"nmx{k}"], w=[f"es_t{k}"])
                P.op("act", lambda e: e.activation(out=es_t, in_=es_t, func=AF.Exp), r=[f"es_t{k}"], w=[f"es_t{k}"])
                for g in range(2):
                    po, hpo = mynps()
                    for i in range(4):
                        for kh in khs:
                            P.op("pe", lambda e, po=po, g=g, i=i, kh=kh: e.matmul(
                                po[:, i * 65:(i + 1) * 65], lhsT=ET[:, g * 2 + kh, i * 128:(i + 1) * 128],
                                rhs=vaug_l[:, s + kh, g * 65:(g + 1) * 65], start=(kh == khs[0]), stop=(kh == 1)),
                                r=[f"ET{k}_{g}{kh}", hva], w=[hpo])
                    P.op("dve", lambda e, po=po, g=g: e.tensor_tensor(
                        out=den.rearrange("p (i g) -> p i g", g=2)[:, :, g:g + 1],
                        in0=po[:, 0:260].rearrange("p (i d) -> p i d", d=65)[:, :, 64:65],
                        in1=es_t.rearrange("p (i g) -> p i g", g=2)[:, :, g:g + 1], op=ALU.add),
                        r=[hpo, f"es_t{k}"], w=[f"den{k}_{g}"])
                    P.op("act", lambda e, po=po, g=g: e.activation(
                        out=osb.rearrange("p (i g) d -> p i g d", g=2)[:, :, g, :],
                        in_=po[:, 0:260].rearrange("p (i d) -> p i d", d=65)[:, :, 0:64], func=AF.Identity),
                        r=[hpo], w=[f"osb{k}_{g}"])
                DH = [f"den{k}_0", f"den{k}_1"]
                OH = [f"osb{k}_0", f"osb{k}_1"]
                P.op("dve", lambda e: e.reciprocal(out=den, in_=den), r=DH, w=DH)
                P.op("dve", lambda e: e.tensor_tensor(out=osb, in0=osb, in1=den.unsqueeze(2).to_broadcast([128, 8, 64]), op=ALU.mult),
                     r=OH + DH, w=OH)
                P.op("act", lambda e: e.activation(out=osq, in_=osb, func=AF.Square), r=OH, w=[f"osq{k}"])
                P.op("dve", lambda e: e.tensor_reduce(out=ssq, in_=osq, axis=AX.X, op=ALU.add), r=[f"osq{k}"], w=[f"ssq{k}"])
                P.op("act", lambda e: e.activation(out=ssq, in_=ssq, func=AF.Sqrt, bias=EPSC, scale=1.0 / 64.0), r=[f"ssq{k}", "epsc"], w=[f"ssq{k}"])
                P.op("dve", lambda e: e.reciprocal(out=ssq, in_=ssq), r=[f"ssq{k}"], w=[f"ssq{k}"])
                P.op("dve", lambda e: e.tensor_tensor(out=osb, in0=osb, in1=ssq.unsqueeze(2).to_broadcast([128, 8, 64]), op=ALU.mult),
                     r=OH + [f"ssq{k}"], w=OH)
                P.op("dve", lambda e: e.tensor_tensor(out=yat, in0=osb.rearrange("p h d -> p (h d)"), in1=aog_bc, op=ALU.mult),
                     r=OH + ["rows_sb"], w=[f"yat{k}"])
                ptb = ps[3 + 2 * k][:, :].bitcast(BF16)[:, 0:512]
                hptr = f"ps{3 + 2 * k}"
                for i in range(4):
                    P.op("pe", lambda e, i=i: e.transpose(out=ptb[:, i * 128:(i + 1) * 128], in_=yat[:, i * 128:(i + 1) * 128],
                                                         identity=identb), r=[f"yat{k}", "identb"], w=[hptr])
                P.op("act", lambda e: e.activation(
                    out=ycT[:, 4:8, qs], in_=ptb[:, 0:512].rearrange("p (i q) -> p i q", i=4), func=AF.Identity),
                    r=[hptr], w=[f"ycTa{s}"])
                return P.end()

            def xr_load(s):
                k = s % 2
                tt = mt * 4 + s
                P.op("sp", lambda e: e.dma_start(out=xr2[k], in_=x_d[tt * 128:(tt + 1) * 128, :]), w=[f"xr{k}"], dma=True)

            def epi_chain(s, with_load):
                k = s % 2
                tt = mt * 4 + s
                xr, bnst, bnag = xr2[k], bnst2[k], bnag2[k]
                rr = xr
                mynps = nps_ep[k]
                YH = [f"ycTc{c}" for c in range(4)] + [f"ycTa{s}"]
                P.rec()
                if with_load:
                    xr_load(s)
                for h in range(2):
                    po, hpo = mynps()
                    for c in range(8):
                        P.op("pe", lambda e, po=po, c=c, h=h: e.matmul(
                            po[:], lhsT=ycT[:, c, s * 128:(s + 1) * 128], rhs=wout[:, c, h * 512:(h + 1) * 512],
                            start=(c == 0), stop=False), r=YH + WOUTH, w=[hpo])
                    P.op("pe", lambda e, po=po, h=h: e.matmul(
                        po[:], lhsT=onesb[0:1, :], rhs=gbrow[0:1, h * 512:(h + 1) * 512], start=False, stop=True),
                        r=["onesb", "gbrow"], w=[hpo])
                    P.op("dve", lambda e, po=po, h=h: e.tensor_tensor(out=rr[:, h * 512:(h + 1) * 512], in0=po[:],
                                                                      in1=xr[:, h * 512:(h + 1) * 512], op=ALU.add),
                         r=[hpo, f"xr{k}"], w=[f"xr{k}"])
                for h in range(2):
                    P.op("dve", lambda e, h=h: e.bn_stats(out=bnst[:, h * 6:(h + 1) * 6], in_=rr[:, h * 512:(h + 1) * 512]),
                         r=[f"xr{k}"], w=[f"bnst{k}"])
                P.op("dve", lambda e: e.bn_aggr(out=bnag[:, 0:2], in_=bnst), r=[f"bnst{k}"], w=[f"bnag{k}"])
                P.op("act", lambda e: e.activation(out=bnag[:, 2:3], in_=bnag[:, 1:2], func=AF.Sqrt, bias=EPSC2, scale=1.0),
                     r=[f"bnag{k}", "epsc2"], w=[f"bnagb{k}"])
                P.op("dve", lambda e: e.reciprocal(out=bnag[:, 2:3], in_=bnag[:, 2:3]), r=[f"bnagb{k}"], w=[f"bnagb{k}"])
                P.op("dve", lambda e: e.scalar_tensor_tensor(out=bnag[:, 3:4], in0=bnag[:, 0:1], scalar=-1.0, in1=bnag[:, 2:3],
                                                             op0=ALU.mult, op1=ALU.mult), r=[f"bnag{k}", f"bnagb{k}"], w=[f"bnagc{k}"])
                P.op("act", lambda e: e.activation(out=rr, in_=rr, func=AF.Identity, bias=bnag[:, 3:4], scale=bnag[:, 2:3]),
                     r=[f"xr{k}", f"bnagb{k}", f"bnagc{k}"], w=[f"xr{k}"])
                P.op("sp", lambda e: e.dma_start(out=z_d[tt * 128:(tt + 1) * 128, :], in_=rr), r=[f"xr{k}"], w=[f"z_d{tt}"], dma=True)
                return P.end()


            out = []
            P.rec()
            xr_load(0)
            xr_load(1)
            out += P.end()
            out += merge(convln_chain(0) + convln_chain(1), attn_chain(0), attn_chain(1))
            out += merge(convln_chain(2) + convln_chain(3), attn_chain(2), attn_chain(3))
            out += merge(epi_chain(0, False), epi_chain(1, False))
            out += merge(epi_chain(2, True), epi_chain(3, True))
            return out

        P.play(stage1(0))
        for mt in range(NMT):
            nxt = stage1(mt + 1) if mt + 1 < NMT else []
            P.play(merge(stage2(mt), nxt))

        P.fence()
        if dbg and dbg["what"] == "z":
            nr = _CACHE.get('nmt_run', NMT) * 512
            P.op("sp", lambda e: e.dma_start(out=dbg_d[0:nr, :], in_=z_d[0:nr, :]), r=[f"z_d{i}" for i in range(nr // 128)], w=["dbg"], dma=True)
            P.fence()
            P.emit()
            return nc

        try:
            build_phase2(nc, P, A, ps, nps, dr, out_d, z_d, xs_d, ys_d, dbg, dbg_d, mark_persist,
                     dict(cols=cols, identf=identf, identb=identb, onesb=onesb, Ub=Ub, modc=modc, mod_d=mod_d,
                              EPSC=EPSC, psb=psb))
        except _StopEmit:
            pass
        P.fence()
        P.emit()
    return nc


def build_phase2(nc, P, A, ps, nps, dr, out_d, z_d, xs_d, ys_d, dbg, dbg_d, mark, K):
    cols, identf, identb, onesb, Ub, mod_d, EPSC, psb = (K[k] for k in ("cols", "identf", "identb", "onesb", "Ub", "mod_d", "EPSC", "psb"))
    rows_d, wr_d, wg_d, wu_d, wd_d = dr["rows"], dr["w_r"], dr["wg"], dr["wu"], dr["wd"]
    A.off = mark
    gbc = A.alloc([4, D], F32)
    P.op("sp", lambda e: e.dma_start(out=gbc[:, 1:4, :].rearrange("p a d -> p (a d)"), in_=mod_d[0:1, :].partition_broadcast(128)),
         r=["mod_d"], w=["gbc1_0", "gbc1_1", "gbc2_0", "gbc2_1", "gbc3_0", "gbc3_1"], dma=True)
    lnr = A.alloc([4, D], F32)
    misc = A.alloc([36 + 64], F32)
    A2 = A.alloc([D], F32)
    B2 = A.alloc([D], F32)
    GA = A.alloc([D], F32)
    BA = A.alloc([D], F32)
    wr = A.alloc([8, 36], F32)
    pos_i = A.alloc([2, NT], I32)
    wts = A.alloc([2, NT], F32)
    widx_i = A.alloc([64], I32)
    yidx_i = A.alloc([NSUB, NS], I32)
    mark2 = A.off
    br_bc = misc[:, 0:36]
    slot_bc = misc[:, 36:36 + NS]
    P.op("sp", lambda e: e.dma_start(out=lnr.rearrange("p a d -> p (a d)"), in_=rows_d[0:1, R_L1G:R_L1G + 4 * D].partition_broadcast(128)),
         w=["lnr"], dma=True)
    P.op("sp", lambda e: e.dma_start(out=misc, in_=rows_d[0:1, R_BR:R_BR + 100].partition_broadcast(128)), w=["misc"], dma=True)
    P.op("sp", lambda e: e.dma_start(out=wr, in_=wr_d.rearrange("(c p) n -> p c n", p=128)), w=["wr"], dma=True)
    G2H = ["gbc1_0", "gbc1_1", "gbc2_0", "gbc2_1", "gbc3_0", "gbc3_1"]
    P.op("dve", lambda e: e.scalar_tensor_tensor(out=A2, in0=gbc[:, 2, :], scalar=1.0, in1=lnr[:, 0, :], op0=ALU.add, op1=ALU.mult),
         r=["lnr"] + G2H, w=["A2"])
    P.op("dve", lambda e: e.scalar_tensor_tensor(out=B2, in0=gbc[:, 2, :], scalar=1.0, in1=lnr[:, 1, :], op0=ALU.add, op1=ALU.mult),
         r=["lnr"] + G2H, w=["B2"])
    P.op("dve", lambda e: e.tensor_tensor(out=B2, in0=B2, in1=gbc[:, 1, :], op=ALU.add), r=["B2"] + G2H, w=["B2"])
    P.op("dve", lambda e: e.tensor_scalar(out=GA, in0=lnr[:, 0, :], scalar1=ALPHA, scalar2=None, op0=ALU.mult), r=["lnr"], w=["GA"])
    P.op("dve", lambda e: e.tensor_scalar(out=BA, in0=lnr[:, 1, :], scalar1=ALPHA, scalar2=None, op0=ALU.mult), r=["lnr"], w=["BA"])

    def mk_nps(ids):
        st_ = [0]

        def f():
            i = ids[st_[0] % len(ids)]
            st_[0] += 1
            return ps[i], f"ps{i}"
        return f

    t1_d = nc.dram_tensor("t1_scr", [S, D], F32, kind="Internal").ap()
    h2b = A.alloc([NT, D], BF16)
    logits = A.alloc([NT, 36], F32)
    mark2a = A.off
    NB2A = 4
    zt = [A.alloc([D], F32) for _ in range(NB2A)]
    h2f = [A.alloc([D], F32) for _ in range(NB2A)]
    h2T = [A.alloc([8, 128], F32) for _ in range(NB2A)]
    t1s = [A.alloc([D], F32) for _ in range(NB2A)]
    nps2a_f = [mk_nps([0, 1]), mk_nps([2, 3])]
    nps2a_b = [mk_nps([4]), mk_nps([5])]

    def front_ew(j):
        b = j % NB2A
        z_, hz = zt[b], f"zt{b}"
        hf, hhf = h2f[b], f"h2f{b}"
        t1_, ht1 = t1s[b], f"t1s{b}"
        P.rec()
        P.op("dve", lambda e: e.tensor_tensor(out=hf, in0=z_, in1=A2, op=ALU.mult), r=[hz, "A2"], w=[hhf])
        P.op("dve", lambda e: e.tensor_tensor(out=hf, in0=hf, in1=B2, op=ALU.add), r=[hhf, "B2"], w=[hhf])
        P.op("pool", lambda e: e.tensor_tensor(out=t1_, in0=z_, in1=GA, op=ALU.mult), r=[hz, "GA"], w=[ht1])
        P.op("dve", lambda e: e.tensor_tensor(out=t1_, in0=t1_, in1=BA, op=ALU.add), r=[ht1, "BA"], w=[ht1])
        P.op("sp", lambda e: e.dma_start(out=t1_d[j * 128:(j + 1) * 128, :], in_=t1_), r=[ht1], w=[f"t1_d{j}"], dma=True)
        P.op("act", lambda e: e.activation(out=h2b[:, j, :], in_=hf, func=AF.Identity), r=[hhf], w=[f"h2b{j}"])
        return P.end()

    def zload(j):
        b = j % NB2A
        P.op("sp", lambda e: e.dma_start(out=zt[b], in_=z_d[j * 128:(j + 1) * 128, :]), r=[f"z_d{j}"], w=[f"zt{b}"], dma=True)

    def front_tr(j):
        b = j % NB2A
        mynps = nps2a_f[j % 2]
        hf, hhf = h2f[b], f"h2f{b}"
        hT_, hhT = h2T[b], f"h2T{b}"
        P.rec()
        for hh in range(2):
            pt, hp = mynps()
            for c4 in range(4):
                c = hh * 4 + c4
                P.op("pe", lambda e, pt=pt, c=c, c4=c4: e.transpose(out=pt[:, c4 * 128:(c4 + 1) * 128],
                                                                  in_=hf[:, c * 128:(c + 1) * 128], identity=identf),
                     r=[hhf, "cst"], w=[hp])
            P.op("act", lambda e, pt=pt, hh=hh: e.activation(out=hT_[:, hh * 4:(hh + 1) * 4, :].rearrange("p c t -> p (c t)"),
                                                             in_=pt[:], func=AF.Identity), r=[hp], w=[hhT + "ab"[hh]])
        return P.end()

    def back2a(j):
        b = j % NB2A
        hT_, hhT = h2T[b], f"h2T{b}"
        P.rec()
        pl, hpl = nps2a_b[j % 2]()
        for c in range(8):
            P.op("pe", lambda e, c=c: e.matmul(pl[:, 0:36], lhsT=hT_[:, c, :], rhs=wr[:, c, :], start=(c == 0), stop=(c == 7)),
                 r=[hhT + "a", hhT + "b", "wr"], w=[hpl])
        P.op("dve", lambda e: e.tensor_tensor(out=logits[:, j, :], in0=pl[:, 0:36], in1=br_bc, op=ALU.add),
             r=[hpl, "misc"], w=["logits"])
        return P.end()

    NP2A = NT // 2
    for j in range(4):
        zload(j)
    P.play(front_ew(0) + front_ew(1) + merge(front_tr(0), front_tr(1)))
    for k in range(NP2A):
        if k + 2 < NP2A:
            zload(2 * k + 4)
            zload(2 * k + 5)
        blk = []
        if k + 1 < NP2A:
            blk += merge(front_ew(2 * k + 2), front_ew(2 * k + 3))
        bk = merge(back2a(2 * k), back2a(2 * k + 1))
        tail = [o for o in bk if o[0] == "dve"]
        blk += [o for o in bk if o[0] != "dve"]
        if k + 1 < NP2A:
            blk += merge(front_tr(2 * k + 2), front_tr(2 * k + 3))
        blk += tail
        P.play(blk)
    P.fence()
    A.off = mark2a

    def T3(n):
        return A.alloc([NT, n], F32)
    gmax = A.alloc([NT], F32)
    og = T3(4)
    eg = T3(4)
    sgm = A.alloc([NT], F32)
    ptop = A.alloc([NT], F32)
    tmp4 = A.alloc([NT, 4, 8], F32)
    sel = T3(8)
    sel2 = T3(8)
    m1 = A.alloc([NT], F32)
    m2 = A.alloc([NT], F32)
    o1 = T3(8)
    o2 = T3(8)
    e2 = A.alloc([NT], F32)
    r12 = A.alloc([NT], F32)
    O1 = A.alloc([NT, 4, 8], F32)
    O2 = A.alloc([NT, 4, 8], F32)
    Obf = A.alloc([NT * 32], BF16)
    totA = A.alloc([NT, 32], F32)
    totB = A.alloc([NT, 32], F32)
    tot0 = A.alloc([NT, 32], F32)
    base = A.alloc([NT, 32], F32)
    cnt = A.alloc([32], F32)
    cmpc = A.alloc([32, 24], F32)
    pcnt = A.alloc([32], F32)
    oeA = A.alloc([32], F32)
    oeB = A.alloc([32], F32)
    offs = A.alloc([32], F32)
    cmps = A.alloc([NS, 32], F32)
    esl = A.alloc([64], F32)
    used = A.alloc([64], F32)
    posf = A.alloc([2, NT], F32)

    LG = logits[:, :, 0:4]
    LE4 = logits[:, :, 4:36].rearrange("p j (g e) -> p j g e", g=4)

    def dv(fn, r, w):
        P.op("dve", fn, r=r, w=w)

    dv(lambda e: e.tensor_reduce(out=gmax, in_=LG, axis=AX.X, op=ALU.max), ["logits"], ["gmax"])
    dv(lambda e: e.tensor_tensor(out=og, in0=LG, in1=gmax.unsqueeze(2).to_broadcast([128, NT, 4]), op=ALU.is_equal), ["logits", "gmax"], ["og"])
    dv(lambda e: e.tensor_tensor(out=eg, in0=LG, in1=gmax.unsqueeze(2).to_broadcast([128, NT, 4]), op=ALU.subtract), ["logits", "gmax"], ["eg"])
    P.op("act", lambda e: e.activation(out=eg, in_=eg, func=AF.Exp), r=["eg"], w=["eg"])
    dv(lambda e: e.tensor_reduce(out=sgm, in_=eg, axis=AX.X, op=ALU.add), ["eg"], ["sgm"])
    dv(lambda e: e.reciprocal(out=ptop, in_=sgm), ["sgm"], ["ptop"])
    dv(lambda e: e.tensor_tensor(out=tmp4, in0=LE4, in1=og.unsqueeze(3).to_broadcast([128, NT, 4, 8]), op=ALU.mult), ["logits", "og"], ["tmp4"])
    dv(lambda e: e.tensor_reduce(out=sel, in_=tmp4.rearrange("p j g e -> p j e g"), axis=AX.X, op=ALU.add), ["tmp4"], ["sel"])
    dv(lambda e: e.tensor_reduce(out=m1, in_=sel, axis=AX.X, op=ALU.max), ["sel"], ["m1"])
    dv(lambda e: e.tensor_tensor(out=o1, in0=sel, in1=m1.unsqueeze(2).to_broadcast([128, NT, 8]), op=ALU.is_equal), ["sel", "m1"], ["o1"])
    dv(lambda e: e.scalar_tensor_tensor(out=sel2.rearrange("p j e -> p (j e)"), in0=o1.rearrange("p j e -> p (j e)"), scalar=-1.0e9,
                                        in1=sel.rearrange("p j e -> p (j e)"), op0=ALU.mult, op1=ALU.add), ["o1", "sel"], ["sel2"])
    dv(lambda e: e.tensor_reduce(out=m2, in_=sel2, axis=AX.X, op=ALU.max), ["sel2"], ["m2"])
    dv(lambda e: e.tensor_tensor(out=o2, in0=sel2, in1=m2.unsqueeze(2).to_broadcast([128, NT, 8]), op=ALU.is_equal), ["sel2", "m2"], ["o2"])
    dv(lambda e: e.tensor_tensor(out=e2, in0=m2, in1=m1, op=ALU.subtract), ["m1", "m2"], ["e2"])
    P.op("act", lambda e: e.activation(out=e2, in_=e2, func=AF.Exp), r=["e2"], w=["e2"])
    dv(lambda e: e.tensor_scalar(out=r12, in0=e2, scalar1=1.0, scalar2=None, op0=ALU.add), ["e2"], ["r12"])
    dv(lambda e: e.reciprocal(out=r12, in_=r12), ["r12"], ["r12"])
    dv(lambda e: e.tensor_tensor(out=wts[:, 0, :], in0=r12, in1=ptop, op=ALU.mult), ["r12", "ptop"], ["wts"])
    dv(lambda e: e.tensor_tensor(out=wts[:, 1, :], in0=wts[:, 0, :], in1=e2, op=ALU.mult), ["wts", "e2"], ["wts"])
    dv(lambda e: e.tensor_tensor(out=O1, in0=og.unsqueeze(3).to_broadcast([128, NT, 4, 8]),
                                 in1=o1.unsqueeze(2).to_broadcast([128, NT, 4, 8]), op=ALU.mult), ["og", "o1"], ["O1"])
    dv(lambda e: e.tensor_tensor(out=O2, in0=og.unsqueeze(3).to_broadcast([128, NT, 4, 8]),
                                 in1=o2.unsqueeze(2).to_broadcast([128, NT, 4, 8]), op=ALU.mult), ["og", "o2"], ["O2"])
    O1f = O1.rearrange("p j g e -> p (j g e)")
    O2f = O2.rearrange("p j g e -> p (j g e)")
    dv(lambda e: e.tensor_tensor(out=Obf, in0=O1f, in1=O2f, op=ALU.add), ["O1", "O2"], ["Obf"])
    pcs, pts = [], []
    for h in range(2):
        pc_, hpc = nps()
        P.op("pe", lambda e, pc_=pc_, h=h: e.matmul(pc_[:], lhsT=Ub, rhs=Obf[:, h * 512:(h + 1) * 512], start=True, stop=True),
             r=["Ub", "Obf"], w=[hpc])
        pcs.append((pc_, hpc))
        pt_, hpt = nps()
        P.op("pe", lambda e, pt_=pt_, h=h: e.matmul(pt_[:], lhsT=onesb, rhs=Obf[:, h * 512:(h + 1) * 512], start=True, stop=True),
             r=["onesb", "Obf"], w=[hpt])
        pts.append((pt_, hpt))
    tot0f = tot0.rearrange("p j e -> p (j e)")
    for h in range(2):
        dv(lambda e, h=h: e.tensor_copy(out=tot0f[:, h * 512:(h + 1) * 512], in_=pts[h][0][:]), [pts[h][1]], ["tot0"])
    cur, hc = tot0, "tot0"
    for i_, sft in enumerate((1, 2, 4, 8, 16)):
        nxt, hn = (totA, "totA") if i_ % 2 == 0 else (totB, "totB")
        dv(lambda e, cur=cur, nxt=nxt, sft=sft: e.tensor_tensor(out=nxt[:, sft:, :], in0=cur[:, sft:, :], in1=cur[:, :NT - sft, :], op=ALU.add),
           [hc], [hn])
        dv(lambda e, cur=cur, nxt=nxt, sft=sft: e.tensor_copy(out=nxt[:, :sft, :], in_=cur[:, :sft, :]), [hc, hn], [hn])
        cur, hc = nxt, hn
    incl, hincl = cur, hc
    dv(lambda e: e.tensor_copy(out=cnt, in_=incl[:, NT - 1, :]), [hincl], ["cnt"])
    dv(lambda e: e.tensor_tensor(out=cmpc, in0=cnt.unsqueeze(2).to_broadcast([128, 32, 24]),
                                 in1=slot_bc[:, 0:24].unsqueeze(1).to_broadcast([128, 32, 24]), op=ALU.is_gt), ["cnt", "misc"], ["cmpc"])
    dv(lambda e: e.tensor_reduce(out=pcnt, in_=cmpc, axis=AX.X, op=ALU.add), ["cmpc"], ["pcnt"])
    dv(lambda e: e.tensor_scalar(out=pcnt, in0=pcnt, scalar1=float(T), scalar2=None, op0=ALU.mult), ["pcnt"], ["pcnt"])
    cur, hc = pcnt, "pcnt"
    for i_, sft in enumerate((1, 2, 4, 8, 16)):
        nxt, hn = (oeA, "oeA") if i_ % 2 == 0 else (oeB, "oeB")
        dv(lambda e, cur=cur, nxt=nxt, sft=sft: e.tensor_tensor(out=nxt[:, sft:], in0=cur[:, sft:], in1=cur[:, :32 - sft], op=ALU.add), [hc], [hn])
        dv(lambda e, cur=cur, nxt=nxt, sft=sft: e.tensor_copy(out=nxt[:, :sft], in_=cur[:, :sft]), [hc, hn], [hn])
        cur, hc = nxt, hn
    oend, hoend = cur, hc
    dv(lambda e: e.tensor_tensor(out=offs, in0=oend, in1=pcnt, op=ALU.subtract), [hoend, "pcnt"], ["offs"])
    dv(lambda e: e.tensor_tensor(out=base, in0=incl, in1=tot0, op=ALU.subtract), [hincl, "tot0"], ["base"])
    dv(lambda e: e.tensor_tensor(out=base, in0=base, in1=offs.unsqueeze(1).to_broadcast([128, NT, 32]), op=ALU.add), ["base", "offs"], ["base"])
    basef = base.rearrange("p j e -> p (j e)")
    for h in range(2):
        dv(lambda e, h=h: e.tensor_tensor(out=basef[:, h * 512:(h + 1) * 512], in0=pcs[h][0][:], in1=basef[:, h * 512:(h + 1) * 512], op=ALU.add),
           [pcs[h][1], "base"], ["base"])
    for k, (Ok, hO) in enumerate(((O1, "O1"), (O2, "O2"))):
        dv(lambda e, Ok=Ok: e.tensor_tensor(out=Ok.rearrange("p j g e -> p (j g e)"), in0=Ok.rearrange("p j g e -> p (j g e)"), in1=basef, op=ALU.mult),
           [hO, "base"], [hO])
        dv(lambda e, Ok=Ok, k=k: e.tensor_reduce(out=posf[:, k, :], in_=Ok.rearrange("p j g e -> p j (g e)"), axis=AX.X, op=ALU.add), [hO], ["posf"])
    dv(lambda e: e.tensor_copy(out=pos_i, in_=posf), ["posf"], ["pos_i"])
    dv(lambda e: e.tensor_tensor(out=cmps, in0=oend.unsqueeze(1).to_broadcast([128, NS, 32]),
                                 in1=slot_bc.unsqueeze(2).to_broadcast([128, NS, 32]), op=ALU.is_le), [hoend, "misc"], ["cmps"])
    dv(lambda e: e.tensor_reduce(out=esl[:, 0:NS], in_=cmps, axis=AX.X, op=ALU.add), ["cmps"], ["esl"])
    dv(lambda e: e.tensor_scalar(out=esl[:, 0:NS], in0=esl[:, 0:NS], scalar1=float(NE - 1), scalar2=128.0, op0=ALU.min, op1=ALU.mult), ["esl"], ["esl"])
    dv(lambda e: e.tensor_scalar(out=used[:, 0:NS], in0=slot_bc, scalar1=oend[:, 31:32], scalar2=None, op0=ALU.is_lt), ["misc", hoend], ["used"])
    dv(lambda e: e.tensor_scalar(out=used[:, 0:NS], in0=used[:, 0:NS], scalar1=-1.0e6, scalar2=1.0e6, op0=ALU.mult, op1=ALU.add), ["used"], ["used"])
    dv(lambda e: e.tensor_tensor(out=esl[:, 0:NS], in0=esl[:, 0:NS], in1=used[:, 0:NS], op=ALU.add), ["esl", "used"], ["esl"])
    dv(lambda e: e.tensor_scalar(out=esl[:, 0:NS], in0=esl[:, 0:NS], scalar1=cols[:, C_PID:C_PID + 1], scalar2=None, op0=ALU.add), ["esl", "cols"], ["esl"])
    dv(lambda e: e.tensor_copy(out=widx_i[:, 0:NS], in_=esl[:, 0:NS]), ["esl"], ["widx_i"])
    oh = A.alloc([NS, 32], F32)
    endv = A.alloc([32], F32)
    lim = A.alloc([64], F32)
    rrow = A.alloc([64], F32)
    pen = A.alloc([64], F32)
    yidx_f = A.alloc([NSUB, NS], F32)
    dv(lambda e: e.tensor_tensor(out=endv, in0=offs, in1=cnt, op=ALU.add), ["offs", "cnt"], ["endv"])
    dv(lambda e: e.tensor_tensor(out=oh[:, :, 1:32], in0=cmps[:, :, 0:31], in1=cmps[:, :, 1:32], op=ALU.subtract), ["cmps"], ["oh"])
    dv(lambda e: e.tensor_scalar(out=oh[:, :, 0:1], in0=cmps[:, :, 0:1], scalar1=-1.0, scalar2=1.0, op0=ALU.mult, op1=ALU.add), ["cmps", "oh"], ["oh"])
    dv(lambda e: e.tensor_tensor(out=oh, in0=oh, in1=endv.unsqueeze(1).to_broadcast([128, NS, 32]), op=ALU.mult), ["oh", "endv"], ["oh"])
    dv(lambda e: e.tensor_reduce(out=lim[:, 0:NS], in_=oh, axis=AX.X, op=ALU.add), ["oh"], ["lim"])
    for st in range(NSUB):
        dv(lambda e, st=st: e.tensor_scalar(out=rrow[:, 0:NS], in0=slot_bc, scalar1=cols[:, C_PID:C_PID + 1], scalar2=float(st * 128),
                                            op0=ALU.add, op1=ALU.add), ["misc", "cols", "rrow"], ["rrow"])
        dv(lambda e: e.tensor_tensor(out=pen[:, 0:NS], in0=rrow[:, 0:NS], in1=lim[:, 0:NS], op=ALU.is_lt), ["rrow", "lim", "pen"], ["pen"])
        dv(lambda e: e.tensor_scalar(out=pen[:, 0:NS], in0=pen[:, 0:NS], scalar1=-1.0e6, scalar2=1.0e6, op0=ALU.mult, op1=ALU.add), ["pen"], ["pen"])
        dv(lambda e, st=st: e.tensor_tensor(out=yidx_f[:, st, :], in0=rrow[:, 0:NS], in1=pen[:, 0:NS], op=ALU.add), ["rrow", "pen"], ["yidx_f"])
    dv(lambda e: e.tensor_copy(out=yidx_i, in_=yidx_f), ["yidx_f"], ["yidx_i"])

    if dbg and dbg["what"] == "route":
        P.fence()
        P.op("sp", lambda e: e.dma_start(out=dbg_d[0:128, 0:NT * 36], in_=logits.rearrange("p j n -> p (j n)")), r=["logits"], w=["dbg0"], dma=True)
        P.op("sp", lambda e: e.dma_start(out=dbg_d[128:256, 0:2 * NT], in_=posf.rearrange("p k j -> p (k j)")), r=["posf"], w=["dbg1"], dma=True)
        P.op("sp", lambda e: e.dma_start(out=dbg_d[256:384, 0:2 * NT], in_=wts.rearrange("p k j -> p (k j)")), r=["wts"], w=["dbg2"], dma=True)
        P.op("sp", lambda e: e.dma_start(out=dbg_d[384:512, 0:NS], in_=esl[:, 0:NS]), r=["esl"], w=["dbg3"], dma=True)
        P.fence()
        raise _StopEmit()

    for j in range(NT):
        for k in range(2):
            P.op("pool", lambda e, j=j, k=k: e.indirect_dma_start(
                out=xs_d[:, :], out_offset=bass.IndirectOffsetOnAxis(ap=pos_i[:, k, j:j + 1], axis=0),
                in_=h2b[:, j, :], in_offset=None), r=[f"h2b{j}", "pos_i"], w=[f"xs_{j}_{k}"], dma=True)
    P.fence()

    A.off = mark2
    NBW = 4
    wgs = [A.alloc([2048], BF16) for _ in range(NBW)]
    wus = [A.alloc([2048], BF16) for _ in range(NBW)]
    wds = [A.alloc([2048], BF16) for _ in range(NBW)]
    xtok = [A.alloc([NSUB, D], BF16) for _ in range(NBW)]
    XT = [A.alloc([8, T], BF16) for _ in range(2)]
    sgs = [A.alloc([T], F32) for _ in range(2)]
    aT = [A.alloc([2, T], BF16) for _ in range(2)]
    NYO = 4
    yo = [A.alloc([D], F32) for _ in range(NYO)]
    npsB = mk_nps([0, 1, 2])
    npsC = mk_nps([3, 4, 5])
    _bc = {}

    def get_bc(e):
        if "v" not in _bc:
            reg = e.alloc_register("bcreg")
            e.reg_mov(reg, NE * 128 - 1)
            _bc["v"] = e.snap(reg, donate=True)
        return _bc["v"]

    ORDER = []
    for q in range((NS + 1) // 2):
        ORDER.append(q)
        if NS - 1 - q > q:
            ORDER.append(NS - 1 - q)
    assert sorted(ORDER) == list(range(NS))

    _bc2 = {}

    def get_bc2(e):
        if "v" not in _bc2:
            reg = e.alloc_register("bcreg2")
            e.reg_mov(reg, NS * T - 1)
            _bc2["v"] = e.snap(reg, donate=True)
        return _bc2["v"]

    def load_w(i, which):
        s = ORDER[i]
        bw = i % NBW
        for (wsb, wdr, hn) in which(bw):
            P.op("pool", lambda e, wsb=wsb, wdr=wdr, s=s: e.indirect_dma_start(
                out=wsb, out_offset=None, in_=wdr[:, :],
                in_offset=bass.IndirectOffsetOnAxis(ap=widx_i[:, s:s + 1], axis=0),
                bounds_check=get_bc(e), oob_is_err=False), r=["widx_i"], w=[hn], dma=True)

    def w_gu(bw):
        return ((wgs[bw], wg_d, f"wg{bw}"), (wus[bw], wu_d, f"wu{bw}"))

    def w_d(bw):
        return ((wds[bw], wd_d, f"wd{bw}"),)

    def load_x(i):
        s = ORDER[i]
        bw = i % NBW
        for st in range(NSUB):
            r0 = s * T + st * 128
            P.op("sp", lambda e, bw=bw, st=st, r0=r0: e.dma_start(out=xtok[bw][:, st, :], in_=xs_d[r0:r0 + 128, :]),
                 w=[f"xtok{bw}_{st}"], dma=True)

    def stageA(i):
        s = ORDER[i]
        b, bw = i % 2, i % NBW
        P.rec()
        for st in range(NSUB):
            k = (i * NSUB + st) % 2
            pb_, hpb = psb[k], f"psb{k}"
            for c in range(8):
                P.op("pe", lambda e, pb_=pb_, st=st, c=c: e.transpose(out=pb_[:, c * 128:(c + 1) * 128],
                                                                     in_=xtok[bw][:, st, c * 128:(c + 1) * 128], identity=identb),
                     r=[f"xtok{bw}_{st}", "identb"], w=[hpb])
            if True:
                P.op("act", lambda e, pb_=pb_, st=st: e.activation(out=XT[b][:, :, st * 128:(st + 1) * 128],
                                                                   in_=pb_[:].rearrange("p (c t) -> p c t", c=8), func=AF.Identity),
                     r=[hpb], w=[f"XT{b}_{st}"])
            else:
                P.op("dve", lambda e, pb_=pb_, st=st: e.tensor_copy(out=XT[b][:, :, st * 128:(st + 1) * 128],
                                                                    in_=pb_[:].rearrange("p (c t) -> p c t", c=8)),
                     r=[hpb], w=[f"XT{b}_{st}"])
        return P.end()

    def stageB(i):
        s = ORDER[i]
        b, bw = i % 2, i % NBW
        XH = [f"XT{b}_{st}" for st in range(NSUB)]
        P.rec()
        for fch in range(2):
            pg, hpg = npsB()
            for c in range(8):
                P.op("pe", lambda e, pg=pg, c=c, fch=fch: e.matmul(
                    pg[:, 0:T], lhsT=wgs[bw][:, c * 256 + fch * 128:c * 256 + (fch + 1) * 128], rhs=XT[b][:, c, :],
                    start=(c == 0), stop=(c == 7)), r=[f"wg{bw}"] + XH, w=[hpg])
            P.op("act", lambda e, pg=pg: e.activation(out=sgs[b], in_=pg[:, 0:T], func=AF.Silu), r=[hpg], w=[f"sgs{b}"])
            pu, hpu = npsB()
            for c in range(8):
                P.op("pe", lambda e, pu=pu, c=c, fch=fch: e.matmul(
                    pu[:, 0:T], lhsT=wus[bw][:, c * 256 + fch * 128:c * 256 + (fch + 1) * 128], rhs=XT[b][:, c, :],
                    start=(c == 0), stop=(c == 7)), r=[f"wu{bw}"] + XH, w=[hpu])
            P.op("dve", lambda e, pu=pu, fch=fch: e.tensor_tensor(out=aT[b][:, fch, :], in0=pu[:, 0:T], in1=sgs[b], op=ALU.mult),
                 r=[hpu, f"sgs{b}"], w=[f"aT{b}_{fch}"])
        return P.end()

    def stageC(i):
        s = ORDER[i]
        b, bw = i % 2, i % NBW
        P.rec()
        for st in range(NSUB):
            yb = (i * NSUB + st) % NYO
            for half in range(2):
                po, hpo = npsC()
                for fch in range(2):
                    P.op("pe", lambda e, po=po, st=st, fch=fch, half=half: e.matmul(
                        po[:], lhsT=aT[b][:, fch, st * 128:(st + 1) * 128],
                        rhs=wds[bw][:, fch * 1024 + half * 512:fch * 1024 + (half + 1) * 512], start=(fch == 0), stop=(fch == 1)),
                        r=[f"aT{b}_0", f"aT{b}_1", f"wd{bw}"], w=[hpo])
                P.op("dve", lambda e, po=po, yb=yb, half=half: e.tensor_tensor(
                    out=yo[yb][:, half * 512:(half + 1) * 512], in0=po[:], in1=gbc[:, 3, half * 512:(half + 1) * 512], op=ALU.mult),
                    r=[hpo, "gbc3_0", "gbc3_1"], w=[f"yo{yb}{'ab'[half]}"])
            P.op("pool", lambda e, yb=yb, st=st: e.indirect_dma_start(
                out=ys_d[:, :], out_offset=bass.IndirectOffsetOnAxis(ap=yidx_i[:, st, s:s + 1], axis=0),
                in_=yo[yb], in_offset=None, bounds_check=get_bc2(e), oob_is_err=False),
                r=[f"yo{yb}a", f"yo{yb}b", "yidx_i"], w=[f"ys_{s}_{st}"], dma=True)
        return P.end()

    for s in range(min(NBW, NS)):
        load_x(s)
        load_w(s, w_gu)
        load_w(s, w_d)
    for i in range(NS + 2):
        lists = []
        if i < NS:
            lists.append(stageA(i))
        if 0 <= i - 1 < NS:
            lists.append(stageB(i - 1))
        if 0 <= i - 2 < NS:
            lists.append(stageC(i - 2))
        P.play(merge(*lists))
        if i + NBW < NS:
            load_x(i + NBW)
        if i - 1 >= 0 and i - 1 + NBW < NS:
            load_w(i - 1 + NBW, w_gu)
        if i - 2 >= 0 and i - 2 + NBW < NS:
            load_w(i - 2 + NBW, w_d)
    P.fence()

    A.off = mark2
    NB2F = 4
    Y1 = [A.alloc([D], F32) for _ in range(NB2F)]
    Y2 = [A.alloc([D], F32) for _ in range(NB2F)]
    t1 = [A.alloc([D], F32) for _ in range(NB2F)]
    ff = [A.alloc([D], F32) for _ in range(2)]
    r2 = [A.alloc([D], F32) for _ in range(2)]
    qq = [A.alloc([D], F32) for _ in range(2)]
    ob = [A.alloc([D], F32) for _ in range(2)]
    bn2 = [A.alloc([12], F32) for _ in range(2)]
    ag2 = [A.alloc([4], F32) for _ in range(2)]

    def loads2f(j):
        q = j % NB2F
        P.op("pool", lambda e: e.indirect_dma_start(
            out=Y1[q], out_offset=None, in_=ys_d[:, :], in_offset=bass.IndirectOffsetOnAxis(ap=pos_i[:, 0, j:j + 1], axis=0)),
            r=["pos_i"], w=[f"Y1{q}"], dma=True)
        P.op("pool", lambda e: e.indirect_dma_start(
            out=Y2[q], out_offset=None, in_=ys_d[:, :], in_offset=bass.IndirectOffsetOnAxis(ap=pos_i[:, 1, j:j + 1], axis=0)),
            r=["pos_i"], w=[f"Y2{q}"], dma=True)
        P.op("sp", lambda e: e.dma_start(out=t1[q], in_=t1_d[j * 128:(j + 1) * 128, :]), r=[f"t1_d{j}"], w=[f"t1{q}"], dma=True)

    def chain2f(j):
        b = j % 2
        q = j % NB2F
        P.rec()
        P.op("act", lambda e: e.activation(out=ff[b], in_=Y1[q], func=AF.Identity, scale=wts[:, 0, j:j + 1]), r=[f"Y1{q}", "wts"], w=[f"ff{b}"])
        P.op("dve", lambda e: e.scalar_tensor_tensor(out=ff[b], in0=Y2[q], scalar=wts[:, 1, j:j + 1], in1=ff[b], op0=ALU.mult, op1=ALU.add),
             r=[f"Y2{q}", "wts", f"ff{b}"], w=[f"ff{b}"])
        P.op("dve", lambda e: e.tensor_tensor(out=r2[b], in0=ff[b], in1=t1[q], op=ALU.add), r=[f"ff{b}", f"t1{q}"], w=[f"r2{b}"])
        for h in range(2):
            P.op("dve", lambda e, h=h: e.bn_stats(out=bn2[b][:, h * 6:(h + 1) * 6], in_=r2[b][:, h * 512:(h + 1) * 512]), r=[f"r2{b}"], w=[f"bn2{b}"])
        P.op("dve", lambda e: e.bn_aggr(out=ag2[b][:, 0:2], in_=bn2[b]), r=[f"bn2{b}"], w=[f"ag2{b}"])
        P.op("act", lambda e: e.activation(out=ag2[b][:, 2:3], in_=ag2[b][:, 1:2], func=AF.Sqrt, bias=EPSC, scale=1.0), r=[f"ag2{b}", "epsc"], w=[f"ag2b{b}"])
        P.op("dve", lambda e: e.reciprocal(out=ag2[b][:, 2:3], in_=ag2[b][:, 2:3]), r=[f"ag2b{b}"], w=[f"ag2b{b}"])
        P.op("dve", lambda e: e.scalar_tensor_tensor(out=ag2[b][:, 3:4], in0=ag2[b][:, 0:1], scalar=-1.0, in1=ag2[b][:, 2:3], op0=ALU.mult, op1=ALU.mult),
             r=[f"ag2{b}", f"ag2b{b}"], w=[f"ag2c{b}"])
        P.op("act", lambda e: e.activation(out=qq[b], in_=r2[b], func=AF.Identity, bias=ag2[b][:, 3:4], scale=ag2[b][:, 2:3]),
             r=[f"r2{b}", f"ag2b{b}", f"ag2c{b}"], w=[f"qq{b}"])
        P.op("pool", lambda e: e.tensor_tensor(out=qq[b], in0=qq[b], in1=lnr[:, 2, :], op=ALU.mult), r=[f"qq{b}", "lnr"], w=[f"qq{b}"])
        P.op("dve", lambda e: e.tensor_tensor(out=ob[b], in0=qq[b], in1=lnr[:, 3, :], op=ALU.add), r=[f"qq{b}", "lnr"], w=[f"ob{b}"])
        P.op("sp", lambda e: e.dma_start(out=out_d[j * 128:(j + 1) * 128, :], in_=ob[b]), r=[f"ob{b}"], w=[f"out{j}"], dma=True)
        return P.end()

    loads2f(0)
    loads2f(1)
    for j in range(0, NT, 2):
        if j + 2 < NT:
            loads2f(j + 2)
            loads2f(j + 3)
        P.play(merge(chain2f(j), chain2f(j + 1)))


class _StopEmit(Exception):
    pass


def _host_prep(inp, b):
    f = np.float32
    L = 0
    cols = np.zeros((128, NCOL), f)
    cols[:, C_C:C_C + 8] = inp["c"][b].reshape(8, 128).T
    w_in = inp["w_in"][L]
    b_in = inp["b_in"][L]
    qcols = np.concatenate([np.arange(1024 + h * 64, 1024 + (h + 1) * 64) for h in HORDER])
    perm = np.concatenate([np.arange(0, 1024), qcols, np.arange(1536, 1792)])
    w_in_p = np.ascontiguousarray(w_in[:, perm])
    b_in_p = b_in[perm]
    cols[:, C_BIN:C_BIN + 13] = b_in_p[:1664].reshape(13, 128).T
    cols[:, C_CW:C_CW + 124] = inp["conv_w"][L].T.reshape(4, 128, 31).transpose(1, 0, 2).reshape(128, 124)
    cols[:, C_CB:C_CB + 4] = inp["conv_b"][L].reshape(4, 128).T
    cols[:, C_LG:C_LG + 4] = inp["conv_ln_g"][L].reshape(4, 128).T
    cols[:, C_LB:C_LB + 4] = inp["conv_ln_b"][L].reshape(4, 128).T
    cols[:, C_OG:C_OG + 4] = inp["conv_out_g"][L].reshape(4, 128).T
    cols[:, C_PID] = np.arange(128)
    rows = np.zeros((1, NROW), f)
    rows[0, R_BADA:R_BADA + 6144] = inp["b_ada"][L]
    rows[0, R_BV:R_BV + 128] = b_in[1664:1792]
    rows[0, R_SINK:R_SINK + 8] = inp["sinks"][L][HORDER]
    rows[0, R_AOG:R_AOG + 512] = inp["attn_out_g"][L].reshape(8, 64)[HORDER].reshape(-1)
    rows[0, R_BOUT:R_BOUT + D] = inp["b_out"][L]
    rows[0, R_L1G:R_L1G + D] = inp["ln1_g"][L]
    rows[0, R_L1B:R_L1B + D] = inp["ln1_b"][L]
    rows[0, R_L2G:R_L2G + D] = inp["ln2_g"][L]
    rows[0, R_L2B:R_L2B + D] = inp["ln2_b"][L]
    rows[0, R_BR:R_BR + 4] = inp["b_router_group"][L]
    rows[0, R_BR + 4:R_BR + 36] = inp["b_router_expert"][L]
    rows[0, R_SLOT:R_SLOT + NS] = np.arange(NS) * T
    w_out = inp["w_out"][L]
    arows = np.concatenate([np.arange(512 + h * 64, 512 + (h + 1) * 64) for h in HORDER])
    w_out_p = np.ascontiguousarray(np.concatenate([w_out[:512], w_out[arows]], axis=0))
    w_r = np.ascontiguousarray(np.concatenate([inp["w_router_group"][L], inp["w_router_expert"][L]], axis=1))
    return dict(cols=cols, rows=rows, w_in=w_in_p, w_out=w_out_p, w_r=w_r)


def _consts():
    f = np.float32
    cst = np.zeros((128, NK), f)
    cst[:, K_ID:K_ID + 128] = np.eye(128)
    p = np.arange(128)
    cst[:, K_U:K_U + 128] = (p[:, None] < p[None, :])
    cst[:, K_BD:K_BD + 128] = ((p[:, None] // 64) == (p[None, :] // 64)) / 64.0
    m0 = np.where(p[:, None] > p[None, :], 0.0, NEG)
    m1 = np.where(p[:, None] <= p[None, :], 0.0, NEG)
    cst[:, K_M0:K_M0 + 512] = np.tile(m0, (1, 4))
    cst[:, K_M1:K_M1 + 512] = np.tile(m1, (1, 4))
    return cst


def _expert_layout(inp):
    L = 0
    wg = np.ascontiguousarray(inp["w_gate"][L].reshape(NE, 8, 128, DE).transpose(0, 2, 1, 3)).reshape(NE * 128, 2048)
    wu = np.ascontiguousarray(inp["w_up"][L].reshape(NE, 8, 128, DE).transpose(0, 2, 1, 3)).reshape(NE * 128, 2048)
    wd = np.ascontiguousarray(inp["w_down"][L].reshape(NE, 2, 128, D).transpose(0, 2, 1, 3)).reshape(NE * 128, 2048)
    return wg, wu, wd


_CACHE = {}


def kernel(**inputs):
    inp = {k: np.asarray(v) for k, v in inputs.items()}
    dbg = _CACHE.get("dbg")
    nc = build_program(dbg)
    cst = _consts()
    wg, wu, wd = _expert_layout(inp)
    w_ada = np.ascontiguousarray(inp["w_ada"][0])
    in_maps = []
    ncores = _CACHE.get("ncores", 8)
    for b in range(ncores):
        hp = _host_prep(inp, b)
        m = dict(x=np.ascontiguousarray(inp["x"][b]), cols=hp["cols"], rows=hp["rows"], cst=cst, w_ada=w_ada,
                 w_in=hp["w_in"], w_out=hp["w_out"], w_r=hp["w_r"], wg=wg, wu=wu, wd=wd)
        if _CACHE.get("p2only"):
            m["z_in"] = _CACHE["z_in"]
        in_maps.append(m)
    if _CACHE.get("trace"):
        res = run_bass_kernel_spmd(nc, in_maps, core_ids=list(range(ncores)), trace=True)
        print("EXEC_NS", res.exec_time_ns)
    else:
        res = run_bass_kernel_spmd(nc, in_maps, core_ids=list(range(ncores)))
    if dbg:
        return [np.asarray(r["dbg"]) for r in res.results]
    out = np.stack([np.asarray(r["out"]) for r in res.results], axis=0).astype(np.float32)
    return out
```

```python
import os
import numpy as np
import concourse.bass as bass
import concourse.mybir as mybir
from concourse.bass_utils import run_bass_kernel_spmd

F32 = mybir.dt.float32
BF16 = mybir.dt.bfloat16
I32 = mybir.dt.int32
ALU = mybir.AluOpType
AF = mybir.ActivationFunctionType
AX = mybir.AxisListType

D = 1024
S = 4096
NT = S // 128
NMT = S // 512
DIN = 1792
NE = 32
DE = 256
ALPHA = 2.0 ** 0.25
EPS = 1e-5
NEG = -30000.0
T = 384
NS = (2 * S + NE * (T - 1) + T - 1) // T
NSUB = T // 128
HORDER = [0, 4, 1, 5, 2, 6, 3, 7]

C_C = 0
C_BIN = 8
C_CW = 21
C_CB = 145
C_LG = 149
C_LB = 153
C_OG = 157
C_PID = 161
NCOL = 162
R_BADA = 0
R_BV = 6144
R_SINK = 6272
R_AOG = 6280
R_BOUT = 6792
R_L1G = 7816
R_L1B = 8840
R_L2G = 9864
R_L2B = 10888
R_BR = 11912
R_SLOT = 11948
NROW = 12012
K_ID = 0
K_U = 128
K_BD = 256
K_M0 = 384
K_M1 = 896
NK = 1408


class Prog:
    def __init__(self, nc, sems):
        self.nc = nc
        self.ops = []
        self.last_w = {}
        self.readers = {}
        self.eng_sem = {e: sems[i] for i, e in enumerate(["pe", "act", "dve", "pool"])}
        rest = sems[4:]
        n_sp = (len(rest) * 5) // 10
        n_pool = (len(rest) * 4) // 10
        self.dma_pool = {"sp": rest[:n_sp], "pool": rest[n_sp:n_sp + n_pool], "act": rest[n_sp + n_pool:]}
        self._rec = None

    def rec(self):
        assert self._rec is None
        self._rec = []

    def end(self):
        l = self._rec
        self._rec = None
        return l

    def play(self, lst):
        assert self._rec is None
        for o in lst:
            self.op(*o)

    def op(self, eng, fn, r=(), w=(), dma=False):
        if self._rec is not None:
            self._rec.append((eng, fn, list(r), list(w), dma))
            return None
        i = len(self.ops)
        w = list(w) + [h for h in r if h.startswith("ps") and h not in w]
        raw, oth = set(), set()
        for h in r:
            if h in self.last_w:
                raw.add(self.last_w[h])
        for h in w:
            if h in self.last_w:
                oth.add(self.last_w[h])
            for j in self.readers.get(h, ()):
                oth.add(j)
        for h in w:
            self.last_w[h] = i
            self.readers[h] = []
        for h in r:
            self.readers.setdefault(h, []).append(i)
        deps = []
        for j in sorted(raw | oth):
            p = self.ops[j]
            if j == i:
                continue
            if (not p["dma"]) and p["eng"] == eng:
                if eng == "pe" or j not in raw:
                    continue
            deps.append(j)
        self.ops.append(dict(eng=eng, fn=fn, deps=deps, dma=dma, sig=False))
        for j in deps:
            self.ops[j]["sig"] = True
        return i

    def fence(self, engs=("pe", "act", "dve", "pool", "sp")):
        hs = list(self.last_w.keys())
        for e in engs:
            self.op(e, None, r=hs, w=["_fence_" + e])

    def emit(self):
        nc = self.nc
        ticket = {e: 0 for e in self.eng_sem}
        dma_next = {q: 0 for q in self.dma_pool}
        dma_uses = {}
        for o in self.ops:
            if o["dma"]:
                q = o["eng"]
                pool = self.dma_pool[q]
                sem = pool[dma_next[q] % len(pool)]
                dma_next[q] += 1
                u = dma_uses.get(id(sem), 0)
                o["pre"] = (sem, 16 * u)
                dma_uses[id(sem)] = u + 1
                o["ev"] = (sem, 16 * (u + 1))
            elif o["sig"] and o["fn"] is not None:
                ticket[o["eng"]] += 1
                o["ev"] = (self.eng_sem[o["eng"]], ticket[o["eng"]])
        ops = self.ops

        def run(engname, eobj):
            waited = {}

            def wait(sem, val):
                if val <= 0:
                    return
                if waited.get(id(sem), 0) >= val:
                    return
                eobj.wait_ge(sem, val)
                waited[id(sem)] = val

            for o in ops:
                if o["eng"] != engname:
                    continue
                for j in o["deps"]:
                    ev = ops[j].get("ev")
                    if ev is not None:
                        wait(*ev)
                if o["fn"] is None:
                    continue
                if o["dma"]:
                    wait(*o["pre"])
                    ins = o["fn"](eobj)
                    ins.then_inc(o["ev"][0], 16)
                else:
                    ins = o["fn"](eobj)
                    if o["sig"]:
                        ins.then_inc(o["ev"][0], 1)

        with nc.Block() as block:
            @block.tensor
            def _(e):
                run("pe", e)

            @block.scalar
            def _(e):
                run("act", e)

            @block.vector
            def _(e):
                run("dve", e)

            @block.gpsimd
            def _(e):
                run("pool", e)

            @block.sync
            def _(e):
                run("sp", e)


class _Stop(Exception):
    pass


def merge(*lists):
    lists = [l for l in lists if l]
    out = []
    idx = [0] * len(lists)
    total = sum(len(l) for l in lists)
    while len(out) < total:
        best, bv = None, None
        for k, l in enumerate(lists):
            if idx[k] < len(l):
                v = (idx[k] + 0.5) / len(l)
                if bv is None or v < bv:
                    best, bv = k, v
        out.append(lists[best][idx[best]])
        idx[best] += 1
    return out


class Arena:
    def __init__(self, t, nbytes):
        self.t = t
        self.nbytes = nbytes
        self.off = 0

    def alloc(self, shape, dt):
        esz = 4 if dt in (F32, I32) else 2
        n = int(np.prod(shape)) * esz
        n = (n + 63) // 64 * 64
        assert self.off + n <= self.nbytes, ("arena overflow", self.off, n, self.nbytes)
        v = self.t[:, self.off // 4:(self.off + n) // 4]
        self.off += n
        if dt != F32:
            v = v.bitcast(dt)
        v = v[:, 0:int(np.prod(shape))]
        if len(shape) == 2:
            return v.rearrange("p (a b) -> p a b", a=shape[0])
        if len(shape) == 3:
            return v.rearrange("p (a b c) -> p a b c", a=shape[0], b=shape[1])
        return v


def build_program(dbg=None):
    nc = bass.Bass("TRN2", target_bir_lowering=False)
    try:
        return _build_program(nc, dbg)
    except _Stop:
        return nc


def _build_program(nc, dbg=None):
    dr = {}

    def din(name, shape, dt=F32):
        dr[name] = nc.dram_tensor(name, list(shape), dt, kind="ExternalInput").ap()
        return dr[name]

    x_d = din("x", [S, D])
    cols_d = din("cols", [128, NCOL])
    rows_d = din("rows", [1, NROW])
    cst_d = din("cst", [128, NK])
    wada_d = din("w_ada", [D, 6 * D])
    win_d = din("w_in", [D, DIN])
    wout_d = din("w_out", [D, D])
    wr_d = din("w_r", [D, 36])
    wg_d = din("wg", [NE * 128, 2048])
    wu_d = din("wu", [NE * 128, 2048])
    wd_d = din("wd", [NE * 128, 2048])
    out_d = nc.dram_tensor("out", [S, D], F32, kind="ExternalOutput").ap()
    if _CACHE.get("p2only"):
        z_d = din("z_in", [S, D])
    else:
        z_d = nc.dram_tensor("z_scr", [S, D], F32, kind="Internal").ap()
    xs_d = nc.dram_tensor("xs_scr", [NS * T, D], BF16, kind="Internal").ap()
    ys_d = nc.dram_tensor("ys_scr", [NS * T, D], F32, kind="Internal").ap()
    dbg_d = None
    if dbg:
        dbg_d = nc.dram_tensor("dbg", list(dbg["shape"]), F32, kind="ExternalOutput").ap()

    import contextlib
    with contextlib.ExitStack() as st:
        ARENA_BYTES = 206 * 1024
        arena_t = st.enter_context(nc.sbuf_tensor("arena", [128, ARENA_BYTES // 4], F32))
        ps = [st.enter_context(nc.psum_tensor(f"ps{i}", [128, 512], F32)) for i in range(6)]
        psb = [st.enter_context(nc.psum_tensor(f"psb{i}", [128, 1024], BF16)) for i in range(2)]
        sems = [st.enter_context(nc.semaphore(f"s{i}")) for i in range(_CACHE.get("nsem", 48))]
        P = Prog(nc, sems)
        A = Arena(arena_t, ARENA_BYTES)
        psn = [0]

        def nps():
            i = psn[0] % 6
            psn[0] += 1
            return ps[i], f"ps{i}"

        cols = A.alloc([NCOL], F32)
        identf = A.alloc([128], F32)
        identb = A.alloc([128], BF16)
        onesb = A.alloc([128], BF16)
        ones512 = A.alloc([128], BF16)
        bdb = A.alloc([128], BF16)
        Ub = A.alloc([128], BF16)
        maskT = A.alloc([2, 512], BF16)
        modc = A.alloc([32], F32)
        g1bc = A.alloc([D], F32)
        gbrow = A.alloc([D], BF16)
        EPSC = A.alloc([1], F32)
        EPSC2 = A.alloc([1], F32)
        mark_persist = A.off
        cst = A.alloc([NK], F32)
        gbc = A.alloc([4, D], F32)
        mod_d = nc.dram_tensor("mod_scr", [1, 3 * D], F32, kind="Internal").ap()

        P.op("sp", lambda e: e.dma_start(out=cols, in_=cols_d), w=["cols"], dma=True)
        P.op("sp", lambda e: e.dma_start(out=cst, in_=cst_d), w=["cst0"], dma=True)
        P.op("sp", lambda e: e.dma_start(out=identf, in_=cst_d[:, K_ID:K_ID + 128]), w=["cst"], dma=True)
        P.op("dve", lambda e: e.tensor_copy(out=identb, in_=identf), r=["cst"], w=["identb"])
        P.op("dve", lambda e: e.memset(onesb, 1.0), w=["onesb"])
        P.op("dve", lambda e: e.memset(EPSC, EPS), w=["epsc"])
        P.op("dve", lambda e: e.memset(EPSC2, EPS / (ALPHA * ALPHA)), w=["epsc2"])
        P.op("dve", lambda e: e.memset(ones512, 1.0 / 512.0), w=["ones512"])
        P.op("dve", lambda e: e.tensor_copy(out=bdb, in_=cst[:, K_BD:K_BD + 128]), r=["cst0"], w=["bdb"])
        P.op("dve", lambda e: e.tensor_copy(out=Ub, in_=cst[:, K_U:K_U + 128]), r=["cst0"], w=["Ub"])
        P.op("dve", lambda e: e.tensor_copy(out=maskT.rearrange("p a b -> p (a b)"), in_=cst[:, K_M0:K_M0 + 1024]),
             r=["cst0"], w=["maskT"])

        ph0 = A.off
        cact = A.alloc([8], F32)
        cbc = A.alloc([8, 128], BF16)
        wab = [A.alloc([8, 512], BF16) for _ in range(2)]
        badab = A.alloc([512], F32)
        modb = A.alloc([4 * D], F32)
        P.op("act", lambda e: e.activation(out=cact, in_=cols[:, C_C:C_C + 8], func=AF.Silu), r=["cols"], w=["cact"])
        for c in range(8):
            P.op("dve", lambda e, c=c: e.tensor_scalar(out=cbc[:, c, :], in0=onesb, scalar1=cact[:, c:c + 1],
                                                       scalar2=None, op0=ALU.mult),
                 r=["cact", "onesb"], w=["cbc"])
        for blk in range(12):
            wb = wab[blk % 2]
            hw = f"wab{blk % 2}"
            P.op("pool", lambda e, blk=blk, wb=wb: e.dma_start(
                out=wb, in_=wada_d[:, blk * 512:(blk + 1) * 512].rearrange("(c p) n -> p c n", p=128)),
                w=[hw], dma=True)
            P.op("sp", lambda e, blk=blk: e.dma_start(
                out=badab, in_=rows_d[0:1, R_BADA + blk * 512:R_BADA + (blk + 1) * 512].partition_broadcast(128)),
                w=["badab"], dma=True)
            pt, hp = nps()
            for c in range(8):
                P.op("pe", lambda e, c=c, pt=pt, wb=wb: e.matmul(pt[:], lhsT=cbc[:, c, :], rhs=wb[:, c, :],
                                                               start=(c == 0), stop=(c == 7)),
                     r=["cbc", hw], w=[hp])
            if blk < 4:
                dst = modb[:, blk * 512:(blk + 1) * 512]
                hd = f"modb{blk}"
            else:
                gi = (blk - 4) // 2
                dst = gbc[:, gi, ((blk - 4) % 2) * 512:((blk - 4) % 2 + 1) * 512]
                hd = f"gbc{gi}_{blk % 2}"
            P.op("dve", lambda e, pt=pt, dst=dst: e.tensor_tensor(out=dst, in0=pt[:], in1=badab, op=ALU.add),
                 r=[hp, "badab"], w=[hd])
        srcs = []
        for c in range(8):
            srcs.append((modb[:, c * 128:(c + 1) * 128], f"modb{c // 4}", c, 0.0))
        for c in range(8):
            srcs.append((modb[:, 1024 + c * 128:1024 + (c + 1) * 128], f"modb{2 + c // 4}", 8 + c, 1.0))
        for c in range(8):
            srcs.append((gbc[:, 1, c * 128:(c + 1) * 128], f"gbc1_{c // 4}", 16 + c, 0.0))
        for c in range(8):
            srcs.append((gbc[:, 2, c * 128:(c + 1) * 128], f"gbc2_{c // 4}", 24 + c, 1.0))
        for (src, hs, col, add) in srcs:
            pt, hp = nps()
            P.op("pe", lambda e, pt=pt, src=src: e.transpose(out=pt[:, 0:128], in_=src, identity=identf),
                 r=[hs, "cst"], w=[hp])
            P.op("dve", lambda e, pt=pt, col=col, add=add: e.tensor_scalar(
                out=modc[:, col:col + 1], in0=pt[:, 0:1], scalar1=add, scalar2=None, op0=ALU.add),
                r=[hp], w=["modc"])
        G_ALL = [f"gbc{gi}_{h}" for gi in range(4) for h in range(2)]
        P.op("sp", lambda e: e.dma_start(out=mod_d, in_=gbc[0:1, 1:4, :].rearrange("p a d -> p (a d)")), r=G_ALL, w=["mod_d"], dma=True)
        P.op("dve", lambda e: e.tensor_scalar(out=g1bc, in0=gbc[:, 0, :], scalar1=1.0 / ALPHA, scalar2=None, op0=ALU.mult), r=G_ALL, w=["g1bc"])
        boutb = modb[:, 0:D]
        P.op("sp", lambda e: e.dma_start(out=boutb, in_=rows_d[0:1, R_BOUT:R_BOUT + D].partition_broadcast(128)),
             w=["modb0", "modb1"], dma=True)
        P.op("dve", lambda e: e.tensor_tensor(out=boutb, in0=boutb, in1=g1bc, op=ALU.mult), r=["modb0", "modb1", "g1bc"], w=["modb0", "modb1"])
        P.op("dve", lambda e: e.tensor_copy(out=gbrow, in_=boutb), r=["modb0", "modb1"], w=["gbrow"])
        P.fence()
        if _CACHE.get("stop") == 0:
            P.op("sp", lambda e: e.dma_start(out=dbg_d[0:128, 0:32], in_=modc), r=["modc"], w=["dbg"], dma=True)
            P.op("sp", lambda e: e.dma_start(out=dbg_d[128:256, :], in_=gbc[:, 0, :]), r=["gbc0_0", "gbc0_1"], w=["dbg2"], dma=True)
            P.fence()
            P.emit()
            return nc
        A.off = mark_persist

        win = A.alloc([8, DIN], BF16)
        modc2 = A.alloc([32], F32)
        P.op("dve", lambda e: e.tensor_copy(out=modc2, in_=modc), r=["modc"], w=["modc2"])
        wout = A.alloc([8, D], BF16)
        diag = A.alloc([124, 128], BF16)
        xt = A.alloc([4, D], BF16)
        hT = A.alloc([8, 512], BF16)
        vr = [A.alloc([4, 542], BF16) for _ in range(2)]
        kr = [[A.alloc([640], BF16) for _ in range(2)] for _ in range(2)]
        va = [A.alloc([5, 130], BF16) for _ in range(2)]
        qT2 = [A.alloc([4, 512], BF16) for _ in range(2)]
        sig = A.alloc([512], F32)
        ybf2 = [A.alloc([4, 512], BF16) for _ in range(2)]
        y2bf = A.alloc([4, 512], BF16)
        rstd2 = [A.alloc([512], F32) for _ in range(2)]
        nmr2 = [A.alloc([512], F32) for _ in range(2)]
        zc2 = [A.alloc([512], F32) for _ in range(2)]
        sc2_ = [A.alloc([512], F32) for _ in range(2)]
        s2bf2 = [A.alloc([512], BF16) for _ in range(2)]
        r2c2 = [A.alloc([512], F32) for _ in range(2)]
        ycT = A.alloc([8, 512], BF16)
        mx2 = [A.alloc([8], F32) for _ in range(2)]
        mxb2 = [A.alloc([8], BF16) for _ in range(2)]
        nmx2 = [A.alloc([8], F32) for _ in range(2)]
        dcat2 = [A.alloc([2, 512], BF16) for _ in range(2)]
        ET2 = [A.alloc([4, 512], BF16) for _ in range(2)]
        es_t2 = [A.alloc([8], F32) for _ in range(2)]
        den2 = [A.alloc([8], F32) for _ in range(2)]
        osb2 = [A.alloc([8, 64], F32) for _ in range(2)]
        osq2 = [A.alloc([8, 64], F32) for _ in range(2)]
        ssq2 = [A.alloc([8], F32) for _ in range(2)]
        yat2 = [A.alloc([512], BF16) for _ in range(2)]
        rows_sb = A.alloc([128 + 8 + 512], F32)
        xr2 = [A.alloc([D], F32) for _ in range(2)]
        bnst2 = [A.alloc([12], F32) for _ in range(2)]
        bnag2 = [A.alloc([4], F32) for _ in range(2)]
        zrow = A.alloc([D], BF16)
        print("phase1 arena bytes", A.off)
        P.op("pool", lambda e: e.memset(zrow, 0.0), w=["zrow"])

        def mk_nps(ids):
            st_ = [0]

            def f():
                i = ids[st_[0] % len(ids)]
                st_[0] += 1
                return ps[i], f"ps{i}"
            return f

        bv_bc = rows_sb[:, 0:128]
        sink_bc = rows_sb[:, 128:136]
        aog_bc = rows_sb[:, 136:648]
        P.op("sp", lambda e: e.dma_start(out=rows_sb, in_=rows_d[0:1, R_BV:R_BV + 128 + 8 + 512].partition_broadcast(128)),
             w=["rows_sb"], dma=True)
        for c in range(8):
            P.op("pool", lambda e, c=c: e.dma_start(out=win[:, c, :], in_=win_d[c * 128:(c + 1) * 128, :]), w=[f"win{c}"], dma=True)
            P.op("pool", lambda e, c=c: e.dma_start(out=wout[:, c, :], in_=wout_d[c * 128:(c + 1) * 128, :]), w=[f"wout{c}"], dma=True)
        for c in range(8):
            P.op("dve", lambda e, c=c: e.tensor_tensor(out=wout[:, c, :], in0=wout[:, c, :], in1=g1bc, op=ALU.mult),
                 r=[f"wout{c}", "g1bc"], w=[f"wout{c}"])
        WINH = [f"win{c}" for c in range(8)]
        WOUTH = [f"wout{c}" for c in range(8)]
        SK = _CACHE.get("skip", set())
        for c in range(4 if "diag" not in SK else 0):
            P.op("dve", lambda e, c=c: e.tensor_tensor(
                out=diag[:, c * 31:(c + 1) * 31, :], in0=identb.unsqueeze(1).to_broadcast([128, 31, 128]),
                in1=cols[:, C_CW + c * 31:C_CW + (c + 1) * 31].unsqueeze(2).to_broadcast([128, 31, 128]), op=ALU.mult),
                r=["identb", "cols"], w=["diag"])
        if "memset" not in SK:
            P.op("pool", lambda e: e.memset(vr[0], 0.0), w=["vr0"])
            P.op("pool", lambda e: e.memset(vr[1], 0.0), w=["vr1"])
        for par in range(2 if "memset" not in SK else 0):
            for g in range(2):
                P.op("pool", lambda e, par=par, g=g: e.memset(kr[par][g], 0.0), w=[f"kr{par}"])
            P.op("pool", lambda e, par=par: e.memset(va[par], 0.0), w=[f"va{par}"])
            P.op("dve", lambda e, par=par: e.memset(va[par][:, 1:, 64:65], 1.0), r=[f"va{par}"], w=[f"va{par}"])
            P.op("dve", lambda e, par=par: e.memset(va[par][:, 1:, 129:130], 1.0), r=[f"va{par}"], w=[f"va{par}"])

        if _CACHE.get("stop") == 1:
            P.fence()
            P.op("sp", lambda e: e.dma_start(out=dbg_d[0:128, 0:512], in_=g1bc[:, 0:512]), r=["g1bc"], w=["dbg"], dma=True)
            P.fence()
            P.emit()
            return nc
        nps_s1 = mk_nps([0, 1])
        psb1_f32 = psb[1][:, :].bitcast(F32)
        nps_at = [mk_nps([2, 3]), mk_nps([4, 5])]
        nps_ep = [mk_nps([2, 3]), mk_nps([4, 5])]

        def stage1(mt):
            par = mt % 2
            t0 = mt * 512
            vcur, vprev = vr[par], vr[1 - par]
            hv, hvp = f"vr{par}", f"vr{1 - par}"
            kT, kTp = kr[par], kr[1 - par]
            hk, hkp = f"kr{par}", f"kr{1 - par}"
            vaug, vaugp = va[par], va[1 - par]
            hva, hvap = f"va{par}", f"va{1 - par}"
            qT, hq = qT2[par], f"qT{par}"
            ybf, hy = ybf2[par], f"ybf{par}"
            rstd_sb, hrs = rstd2[par], f"rstd{par}"
            nmr_sb, hnm = nmr2[par], f"nmr{par}"
            nps = nps_s1
            P.rec()
            P.op("pool", lambda e: e.dma_start(out=xt, in_=x_d[t0:t0 + 512, :].rearrange("(s p) d -> p s d", p=128)),
                 w=["xt"], dma=True)
            for c in range(8):
                ptx = psb[0][:, 0:512]
                hp = "psb0"
                for s in range(4):
                    P.op("pe", lambda e, s=s, c=c: e.transpose(
                        out=ptx[:, s * 128:(s + 1) * 128], in_=xt[:, s, c * 128:(c + 1) * 128], identity=identb),
                        r=["xt", "identb"], w=[hp])
                P.op("act", lambda e, c=c: e.activation(
                    out=hT[:, c, :], in_=ptx[:, 0:512], func=AF.Identity, bias=modc2[:, c:c + 1], scale=modc2[:, 8 + c:9 + c]),
                    r=[hp, "modc2"], w=["hT"])
            if mt > 0:
                P.op("pool", lambda e: e.tensor_copy(out=vcur[:, :, 0:30], in_=vprev[:, :, 512:542]), r=[hvp], w=[hv])
                for g in range(2):
                    P.op("pool", lambda e, g=g: e.tensor_copy(out=kT[g][:, 0:128], in_=kTp[g][:, 512:640]), r=[hkp], w=[hk])
                P.op("pool", lambda e: e.tensor_copy(out=vaug[:, 0, :], in_=vaugp[:, 4, :]), r=[hvap], w=[hva])
            for c in range(4):
                pb, hpb = nps()
                for k in range(8):
                    P.op("pe", lambda e, pb=pb, k=k, c=c: e.matmul(
                        pb[:], lhsT=win[:, k, 512 + c * 128:512 + (c + 1) * 128], rhs=hT[:, k, :],
                        start=(k == 0), stop=(k == 7)), r=WINH + ["hT"], w=[hpb])
                P.op("act", lambda e, pb=pb, c=c: e.activation(
                    out=sig, in_=pb[:], func=AF.Sigmoid, bias=cols[:, C_BIN + 4 + c:C_BIN + 5 + c], scale=1.0),
                    r=[hpb, "cols"], w=["sig"])
                pa, hpa = nps()
                for k in range(8):
                    P.op("pe", lambda e, pa=pa, k=k, c=c: e.matmul(
                        pa[:], lhsT=win[:, k, c * 128:(c + 1) * 128], rhs=hT[:, k, :],
                        start=(k == 0), stop=(k == 7)), r=WINH + ["hT"], w=[hpa])
                P.op("dve", lambda e, pa=pa, c=c: e.scalar_tensor_tensor(
                    out=vcur[:, c, 30:542], in0=pa[:], scalar=cols[:, C_BIN + c:C_BIN + c + 1], in1=sig,
                    op0=ALU.add, op1=ALU.mult), r=[hpa, "sig", "cols"], w=[hv])
            for i in range(4):
                pq, hpq = nps()
                for k in range(8):
                    P.op("pe", lambda e, pq=pq, k=k, i=i: e.matmul(
                        pq[:], lhsT=win[:, k, 1024 + i * 128:1024 + (i + 1) * 128], rhs=hT[:, k, :],
                        start=(k == 0), stop=(k == 7)), r=WINH + ["hT"], w=[hpq])
                P.op("dve", lambda e, pq=pq, i=i: e.tensor_scalar(
                    out=qT[:, i, :], in0=pq[:], scalar1=cols[:, C_BIN + 8 + i:C_BIN + 9 + i], scalar2=0.125,
                    op0=ALU.add, op1=ALU.mult), r=[hpq, "cols"], w=[hq])
            pk, hpk = nps()
            for k in range(8):
                P.op("pe", lambda e, k=k: e.matmul(
                    pk[:], lhsT=win[:, k, 1536:1664], rhs=hT[:, k, :], start=(k == 0), stop=(k == 7)),
                    r=WINH + ["hT"], w=[hpk])
            for g in range(2):
                P.op("act", lambda e, g=g: e.activation(
                    out=kT[g][g * 64:(g + 1) * 64, 128:640], in_=pk[g * 64:(g + 1) * 64, :],
                    func=AF.Identity, bias=cols[g * 64:(g + 1) * 64, C_BIN + 12:C_BIN + 13], scale=1.0),
                    r=[hpk, "cols"], w=[hk])
            pv, hpv = nps()
            for s in range(4):
                for k in range(8):
                    P.op("pe", lambda e, s=s, k=k: e.matmul(
                        pv[:, s * 128:(s + 1) * 128], lhsT=hT[:, k, s * 128:(s + 1) * 128], rhs=win[:, k, 1664:1792],
                        start=(k == 0), stop=(k == 7)), r=WINH + ["hT"], w=[hpv])
            for s in range(4):
                blk = s + 1
                P.op("dve", lambda e, s=s, blk=blk: e.tensor_tensor(
                    out=vaug[:, blk, :].rearrange("p (g d) -> p g d", g=2)[:, :, 0:64],
                    in0=pv[:, s * 128:(s + 1) * 128].rearrange("p (g d) -> p g d", g=2),
                    in1=bv_bc.rearrange("p (g d) -> p g d", g=2), op=ALU.add),
                    r=[hpv, "rows_sb"], w=[hva])
            for c in range(4):
                py, hpy = nps()
                for j in range(31):
                    P.op("pe", lambda e, py=py, c=c, j=j: e.matmul(
                        py[:], lhsT=diag[:, c * 31 + j, :], rhs=vcur[:, c, j:j + 512], start=(j == 0), stop=(j == 30)),
                        r=["diag", hv], w=[hpy])
                P.op("act", lambda e, py=py, c=c: e.activation(
                    out=ybf[:, c, :], in_=py[:], func=AF.Identity, bias=cols[:, C_CB + c:C_CB + c + 1], scale=1.0),
                    r=[hpy, "cols"], w=[hy])
                P.op("act", lambda e, py=py, c=c: e.activation(
                    out=y2bf[:, c, :], in_=py[:], func=AF.Square, bias=cols[:, C_CB + c:C_CB + c + 1], scale=1.0),
                    r=[hpy, "cols"], w=["y2bf"])
            pm, hpm = nps()
            for c in range(4):
                P.op("pe", lambda e, c=c: e.matmul(pm[:], lhsT=ones512, rhs=ybf[:, c, :], start=(c == 0), stop=(c == 3)),
                     r=["ones512", hy], w=[hpm])
            pe2, hpe2 = nps()
            for c in range(4):
                P.op("pe", lambda e, c=c: e.matmul(pe2[:], lhsT=ones512, rhs=y2bf[:, c, :], start=(c == 0), stop=(c == 3)),
                     r=["ones512", "y2bf"], w=[hpe2])
            P.op("act", lambda e: e.activation(out=nmr_sb, in_=pm[:], func=AF.Identity), r=[hpm], w=[hnm])
            P.op("dve", lambda e: e.tensor_tensor(out=rstd_sb, in0=nmr_sb, in1=nmr_sb, op=ALU.mult), r=[hnm], w=[hrs])
            P.op("dve", lambda e: e.tensor_tensor(out=rstd_sb, in0=pe2[:], in1=rstd_sb, op=ALU.subtract), r=[hpe2, hrs], w=[hrs])
            P.op("act", lambda e: e.activation(out=rstd_sb, in_=rstd_sb, func=AF.Sqrt, bias=EPSC, scale=1.0), r=[hrs, "epsc"], w=[hrs])
            P.op("dve", lambda e: e.reciprocal(out=rstd_sb, in_=rstd_sb), r=[hrs], w=[hrs])
            P.op("dve", lambda e: e.scalar_tensor_tensor(out=nmr_sb, in0=nmr_sb, scalar=-1.0, in1=rstd_sb, op0=ALU.mult, op1=ALU.mult),
                 r=[hnm, hrs], w=[hnm])
            return P.end()

        def stage2(mt):
            par = mt % 2
            kT_l, vaug_l = kr[par], va[par]
            hk, hva = f"kr{par}", f"va{par}"
            qT, hq = qT2[par], f"qT{par}"
            ybf, hy = ybf2[par], f"ybf{par}"
            rstd_sb, hrs = rstd2[par], f"rstd{par}"
            nmr_sb, hnm = nmr2[par], f"nmr{par}"

            def convln_chain(c):
                k = c % 2
                zc, sc_, s2bf, r2c = zc2[k], sc2_[k], s2bf2[k], r2c2[k]
                P.rec()
                P.op("dve", lambda e: e.tensor_tensor(out=zc, in0=ybf[:, c, :], in1=rstd_sb, op=ALU.mult),
                     r=[hy, hrs], w=[f"zc{k}"])
                P.op("dve", lambda e: e.tensor_tensor(out=zc, in0=zc, in1=nmr_sb, op=ALU.add), r=[f"zc{k}", hnm], w=[f"zc{k}"])
                P.op("act", lambda e: e.activation(out=sc_, in_=zc, func=AF.Silu, bias=cols[:, C_LB + c:C_LB + c + 1],
                                                   scale=cols[:, C_LG + c:C_LG + c + 1]), r=[f"zc{k}", "cols"], w=[f"sc{k}"])
                P.op("act", lambda e: e.activation(out=s2bf, in_=sc_, func=AF.Square), r=[f"sc{k}"], w=[f"s2bf{k}"])
                pr, hpr = psb1_f32, "psb1"
                P.op("pe", lambda e: e.matmul(pr[:], lhsT=bdb, rhs=s2bf, start=True, stop=True), r=["bdb", f"s2bf{k}"], w=[hpr])
                P.op("act", lambda e: e.activation(out=r2c, in_=pr[:], func=AF.Sqrt, bias=EPSC, scale=1.0), r=[hpr, "epsc"], w=[f"r2c{k}"])
                P.op("dve", lambda e: e.reciprocal(out=r2c, in_=r2c), r=[f"r2c{k}"], w=[f"r2c{k}"])
                P.op("dve", lambda e: e.scalar_tensor_tensor(out=ycT[:, c, :], in0=sc_, scalar=cols[:, C_OG + c:C_OG + c + 1],
                                                             in1=r2c, op0=ALU.mult, op1=ALU.mult),
                     r=[f"sc{k}", f"r2c{k}", "cols"], w=[f"ycTc{c}"])
                return P.end()

            def attn_chain(s):
                k = s % 2
                mx, mxb, nmx, dcat, ET = mx2[k], mxb2[k], nmx2[k], dcat2[k], ET2[k]
                es_t, den, osb, osq, ssq, yat = es_t2[k], den2[k], osb2[k], osq2[k], ssq2[k], yat2[k]
                mynps = nps_at[k]
                n = mt * 4 + s
                qs = slice(s * 128, (s + 1) * 128)
                P.rec()
                for i in range(4):
                    pS, hpS = mynps()
                    for g in range(2):
                        P.op("pe", lambda e, pS=pS, i=i, g=g: e.matmul(
                            pS[:, g * 256:(g + 1) * 256], lhsT=qT[:, i, qs], rhs=kT_l[g][:, s * 128:s * 128 + 256],
                            start=True, stop=True), r=[hq, hk], w=[hpS])
                    P.op("dve", lambda e, pS=pS, i=i: e.tensor_reduce(
                        out=mx[:, 2 * i:2 * i + 2], in_=pS[:].rearrange("p (g k) -> p g k", g=2), axis=AX.X, op=ALU.max),
                        r=[hpS], w=[f"mx{k}"])
                P.op("dve", lambda e: e.tensor_copy(out=mxb, in_=mx), r=[f"mx{k}"], w=[f"mxb{k}"])
                P.op("dve", lambda e: e.tensor_scalar(out=nmx, in0=mxb, scalar1=-1.0, scalar2=None, op0=ALU.mult), r=[f"mxb{k}"], w=[f"nmx{k}"])
                for g in range(2):
                    P.op("dve", lambda e, g=g: e.tensor_tensor(
                        out=dcat[:, g, :].rearrange("p (i q) -> p i q", i=4), in0=identb.unsqueeze(1).to_broadcast([128, 4, 128]),
                        in1=nmx.rearrange("p (i g) -> p i g", g=2)[:, :, g:g + 1].to_broadcast([128, 4, 128]), op=ALU.mult),
                        r=["identb", f"nmx{k}"], w=[f"dcat{k}_{g}"])
                khs = [1] if n == 0 else [0, 1]
                for g in range(2):
                    for kh in khs:
                        pT, hpT = mynps()
                        kc = slice(s * 128 + kh * 128, s * 128 + kh * 128 + 128)
                        P.op("pe", lambda e, pT=pT, g=g, kc=kc: e.matmul(
                            pT[:].rearrange("p (i q) -> p i q", i=4), lhsT=kT_l[g][:, kc], rhs=qT[:, :, qs], start=True, stop=False),
                            r=[hk, hq], w=[hpT])
                        P.op("pe", lambda e, pT=pT, g=g: e.matmul(pT[:], lhsT=onesb, rhs=dcat[:, g, :], start=False, stop=False),
                             r=["onesb", f"dcat{k}_{g}"], w=[hpT])
                        P.op("pe", lambda e, pT=pT, kh=kh: e.matmul(pT[:], lhsT=identb, rhs=maskT[:, kh, :], start=False, stop=True),
                             r=["identb", "maskT"], w=[hpT])
                        P.op("act", lambda e, pT=pT, g=g, kh=kh: e.activation(out=ET[:, g * 2 + kh, :], in_=pT[:], func=AF.Exp),
                             r=[hpT], w=[f"ET{k}_{g}{kh}"])
                P.op("dve", lambda e: e.tensor_tensor(out=es_t, in0=sink_bc, in1=nmx, op=ALU.add), r=["rows_sb", f"nmx{k}"], w=[f"es_t{k}"])
                P.op("act", lambda e: e.activation(out=es_t, in_=es_t, func=AF.Exp), r=[f"es_t{k}"], w=[f"es_t{k}"])
                for g in range(2):
                    po, hpo = mynps()
                    for i in range(4):
                        for kh in khs:
                            P.op("pe", lambda e, po=po, g=g, i=i, kh=kh: e.matmul(
                                po[:, i * 65:(i + 1) * 65], lhsT=ET[:, g * 2 + kh, i * 128:(i + 1) * 128],
                                rhs=vaug_l[:, s + kh, g * 65:(g + 1) * 65], start=(kh == khs[0]), stop=(kh == 1)),
                                r=[f"ET{k}_{g}{kh}", hva], w=[hpo])
                    P.op("dve", lambda e, po=po, g=g: e.tensor_tensor(
                        out=den.rearrange("p (i g) -> p i g", g=2)[:, :, g:g + 1],
                        in0=po[:, 0:260].rearrange("p (i d) -> p i d", d=65)[:, :, 64:65],
                        in1=es_t.rearrange("p (i g) -> p i g", g=2)[:, :, g:g + 1], op=ALU.add),
                        r=[hpo, f"es_t{k}"], w=[f"den{k}_{g}"])
                    P.op("act", lambda e, po=po, g=g: e.activation(
                        out=osb.rearrange("p (i g) d -> p i g d", g=2)[:, :, g, :],
                        in_=po[:, 0:260].rearrange("p (i d) -> p i d", d=65)[:, :, 0:64], func=AF.Identity),
                        r=[hpo], w=[f"osb{k}_{g}"])
                DH = [f"den{k}_0", f"den{k}_1"]
                OH = [f"osb{k}_0", f"osb{k}_1"]
                P.op("dve", lambda e: e.reciprocal(out=den, in_=den), r=DH, w=DH)
                P.op("dve", lambda e: e.tensor_tensor(out=osb, in0=osb, in1=den.unsqueeze(2).to_broadcast([128, 8, 64]), op=ALU.mult),
                     r=OH + DH, w=OH)
                P.op("act", lambda e: e.activation(out=osq, in_=osb, func=AF.Square), r=OH, w=[f"osq{k}"])
                P.op("dve", lambda e: e.tensor_reduce(out=ssq, in_=osq, axis=AX.X, op=ALU.add), r=[f"osq{k}"], w=[f"ssq{k}"])
                P.op("act", lambda e: e.activation(out=ssq, in_=ssq, func=AF.Sqrt, bias=EPSC, scale=1.0 / 64.0), r=[f"ssq{k}", "epsc"], w=[f"ssq{k}"])
                P.op("dve", lambda e: e.reciprocal(out=ssq, in_=ssq), r=[f"ssq{k}"], w=[f"ssq{k}"])
                P.op("dve", lambda e: e.tensor_tensor(out=osb, in0=osb, in1=ssq.unsqueeze(2).to_broadcast([128, 8, 64]), op=ALU.mult),
                     r=OH + [f"ssq{k}"], w=OH)
                P.op("dve", lambda e: e.tensor_tensor(out=yat, in0=osb.rearrange("p h d -> p (h d)"), in1=aog_bc, op=ALU.mult),
                     r=OH + ["rows_sb"], w=[f"yat{k}"])
                ptb = ps[3 + 2 * k][:, :].bitcast(BF16)[:, 0:512]
                hptr = f"ps{3 + 2 * k}"
                for i in range(4):
                    P.op("pe", lambda e, i=i: e.transpose(out=ptb[:, i * 128:(i + 1) * 128], in_=yat[:, i * 128:(i + 1) * 128],
                                                         identity=identb), r=[f"yat{k}", "identb"], w=[hptr])
                P.op("act", lambda e: e.activation(
                    out=ycT[:, 4:8, qs], in_=ptb[:, 0:512].rearrange("p (i q) -> p i q", i=4), func=AF.Identity),
                    r=[hptr], w=[f"ycTa{s}"])
                return P.end()

            def xr_load(s):
                k = s % 2
                tt = mt * 4 + s
                P.op("sp", lambda e: e.dma_start(out=xr2[k], in_=x_d[tt * 128:(tt + 1) * 128, :]), w=[f"xr{k}"], dma=True)

            def epi_chain(s, with_load):
                k = s % 2
                tt = mt * 4 + s
                xr, bnst, bnag = xr2[k], bnst2[k], bnag2[k]
                rr = xr
                mynps = nps_ep[k]
                YH = [f"ycTc{c}" for c in range(4)] + [f"ycTa{s}"]
                P.rec()
                if with_load:
                    xr_load(s)
                for h in range(2):
                    po, hpo = mynps()
                    for c in range(8):
                        P.op("pe", lambda e, po=po, c=c, h=h: e.matmul(
                            po[:], lhsT=ycT[:, c, s * 128:(s + 1) * 128], rhs=wout[:, c, h * 512:(h + 1) * 512],
                            start=(c == 0), stop=False), r=YH + WOUTH, w=[hpo])
                    P.op("pe", lambda e, po=po, h=h: e.matmul(
                        po[:], lhsT=onesb[0:1, :], rhs=gbrow[0:1, h * 512:(h + 1) * 512], start=False, stop=True),
                        r=["onesb", "gbrow"], w=[hpo])
                    P.op("dve", lambda e, po=po, h=h: e.tensor_tensor(out=rr[:, h * 512:(h + 1) * 512], in0=po[:],
                                                                      in1=xr[:, h * 512:(h + 1) * 512], op=ALU.add),
                         r=[hpo, f"xr{k}"], w=[f"xr{k}"])
                for h in range(2):
                    P.op("dve", lambda e, h=h: e.bn_stats(out=bnst[:, h * 6:(h + 1) * 6], in_=rr[:, h * 512:(h + 1) * 512]),
                         r=[f"xr{k}"], w=[f"bnst{k}"])
                P.op("dve", lambda e: e.bn_aggr(out=bnag[:, 0:2], in_=bnst), r=[f"bnst{k}"], w=[f"bnag{k}"])
                P.op("act", lambda e: e.activation(out=bnag[:, 2:3], in_=bnag[:, 1:2], func=AF.Sqrt, bias=EPSC2, scale=1.0),
                     r=[f"bnag{k}", "epsc2"], w=[f"bnagb{k}"])
                P.op("dve", lambda e: e.reciprocal(out=bnag[:, 2:3], in_=bnag[:, 2:3]), r=[f"bnagb{k}"], w=[f"bnagb{k}"])
                P.op("dve", lambda e: e.scalar_tensor_tensor(out=bnag[:, 3:4], in0=bnag[:, 0:1], scalar=-1.0, in1=bnag[:, 2:3],
                                                             op0=ALU.mult, op1=ALU.mult), r=[f"bnag{k}", f"bnagb{k}"], w=[f"bnagc{k}"])
                P.op("act", lambda e: e.activation(out=rr, in_=rr, func=AF.Identity, bias=bnag[:, 3:4], scale=bnag[:, 2:3]),
                     r=[f"xr{k}", f"bnagb{k}", f"bnagc{k}"], w=[f"xr{k}"])
                P.op("sp", lambda e: e.dma_start(out=z_d[tt * 128:(tt + 1) * 128, :], in_=rr), r=[f"xr{k}"], w=[f"z_d{tt}"], dma=True)
                return P.end()


            out = []
            P.rec()
            xr_load(0)
            xr_load(1)
            out += P.end()
            out += merge(convln_chain(0) + convln_chain(1), attn_chain(0), attn_chain(1))
            out += merge(convln_chain(2) + convln_chain(3), attn_chain(2), attn_chain(3))
            out += merge(epi_chain(0, False), epi_chain(1, False))
            out += merge(epi_chain(2, True), epi_chain(3, True))
            return out

        NZ = NS * T // 128

        def zero_fill(lo, hi):
            P.rec()
            for n in range(lo, hi):
                P.op("sp", lambda e, n=n: e.dma_start(out=xs_d[n * 128:(n + 1) * 128, :], in_=zrow), r=["zrow"], w=[f"xs_z{n}"], dma=True)
            return P.end()

        P.play(stage1(0))
        for mt in range(NMT):
            nxt = stage1(mt + 1) if mt + 1 < NMT else []
            zl = zero_fill(mt * NZ // NMT, (mt + 1) * NZ // NMT)
            P.play(merge(stage2(mt), nxt, zl))

        P.fence()
        if dbg and dbg["what"] == "z":
            nr = _CACHE.get('nmt_run', NMT) * 512
            P.op("sp", lambda e: e.dma_start(out=dbg_d[0:nr, :], in_=z_d[0:nr, :]), r=[f"z_d{i}" for i in range(nr // 128)], w=["dbg"], dma=True)
            P.fence()
            P.emit()
            return nc

        try:
            build_phase2(nc, P, A, ps, nps, dr, out_d, z_d, xs_d, ys_d, dbg, dbg_d, mark_persist,
                     dict(cols=cols, identf=identf, identb=identb, onesb=onesb, Ub=Ub, modc=modc, mod_d=mod_d,
                              EPSC=EPSC, psb=psb))
        except _StopEmit:
            pass
        P.fence()
        P.emit()
    return nc


def build_phase2(nc, P, A, ps, nps, dr, out_d, z_d, xs_d, ys_d, dbg, dbg_d, mark, K):
    cols, identf, identb, onesb, Ub, mod_d, EPSC, psb = (K[k] for k in ("cols", "identf", "identb", "onesb", "Ub", "mod_d", "EPSC", "psb"))
    rows_d, wr_d, wg_d, wu_d, wd_d = dr["rows"], dr["w_r"], dr["wg"], dr["wu"], dr["wd"]
    A.off = mark
    gbc = A.alloc([4, D], F32)
    P.op("sp", lambda e: e.dma_start(out=gbc[:, 1:4, :].rearrange("p a d -> p (a d)"), in_=mod_d[0:1, :].partition_broadcast(128)),
         r=["mod_d"], w=["gbc1_0", "gbc1_1", "gbc2_0", "gbc2_1", "gbc3_0", "gbc3_1"], dma=True)
    lnr = A.alloc([4, D], F32)
    misc = A.alloc([36 + 64], F32)
    A2 = A.alloc([D], F32)
    B2 = A.alloc([D], F32)
    GA = A.alloc([D], F32)
    BA = A.alloc([D], F32)
    wr = A.alloc([8, 36], F32)
    pos_i = A.alloc([2, NT], I32)
    wts = A.alloc([2, NT], F32)
    widx_i = A.alloc([64], I32)
    yidx_i = A.alloc([NSUB, NS], I32)
    mark2 = A.off
    br_bc = misc[:, 0:36]
    slot_bc = misc[:, 36:36 + NS]
    P.op("sp", lambda e: e.dma_start(out=lnr.rearrange("p a d -> p (a d)"), in_=rows_d[0:1, R_L1G:R_L1G + 4 * D].partition_broadcast(128)),
         w=["lnr"], dma=True)
    P.op("sp", lambda e: e.dma_start(out=misc, in_=rows_d[0:1, R_BR:R_BR + 100].partition_broadcast(128)), w=["misc"], dma=True)
    P.op("sp", lambda e: e.dma_start(out=wr, in_=wr_d.rearrange("(c p) n -> p c n", p=128)), w=["wr"], dma=True)
    G2H = ["gbc1_0", "gbc1_1", "gbc2_0", "gbc2_1", "gbc3_0", "gbc3_1"]
    P.op("dve", lambda e: e.scalar_tensor_tensor(out=A2, in0=gbc[:, 2, :], scalar=1.0, in1=lnr[:, 0, :], op0=ALU.add, op1=ALU.mult),
         r=["lnr"] + G2H, w=["A2"])
    P.op("dve", lambda e: e.scalar_tensor_tensor(out=B2, in0=gbc[:, 2, :], scalar=1.0, in1=lnr[:, 1, :], op0=ALU.add, op1=ALU.mult),
         r=["lnr"] + G2H, w=["B2"])
    P.op("dve", lambda e: e.tensor_tensor(out=B2, in0=B2, in1=gbc[:, 1, :], op=ALU.add), r=["B2"] + G2H, w=["B2"])
    P.op("dve", lambda e: e.tensor_scalar(out=GA, in0=lnr[:, 0, :], scalar1=ALPHA, scalar2=None, op0=ALU.mult), r=["lnr"], w=["GA"])
    P.op("dve", lambda e: e.tensor_scalar(out=BA, in0=lnr[:, 1, :], scalar1=ALPHA, scalar2=None, op0=ALU.mult), r=["lnr"], w=["BA"])

    def mk_nps(ids):
        st_ = [0]

        def f():
            i = ids[st_[0] % len(ids)]
            st_[0] += 1
            return ps[i], f"ps{i}"
        return f

    t1_d = nc.dram_tensor("t1_scr", [S, D], F32, kind="Internal").ap()
    h2b = A.alloc([NT, D], BF16)
    logits = A.alloc([NT, 36], F32)
    mark2a = A.off
    NB2A = 4
    zt = [A.alloc([D], F32) for _ in range(NB2A)]
    h2f = [A.alloc([D], F32) for _ in range(NB2A)]
    h2T = [A.alloc([8, 128], F32) for _ in range(NB2A)]
    t1s = [A.alloc([D], F32) for _ in range(NB2A)]
    nps2a_f = [mk_nps([0, 1]), mk_nps([2, 3])]
    nps2a_b = [mk_nps([4]), mk_nps([5])]

    def front_ew(j):
        b = j % NB2A
        z_, hz = zt[b], f"zt{b}"
        hf, hhf = h2f[b], f"h2f{b}"
        t1_, ht1 = t1s[b], f"t1s{b}"
        P.rec()
        P.op("dve", lambda e: e.tensor_tensor(out=hf, in0=z_, in1=A2, op=ALU.mult), r=[hz, "A2"], w=[hhf])
        P.op("dve", lambda e: e.tensor_tensor(out=hf, in0=hf, in1=B2, op=ALU.add), r=[hhf, "B2"], w=[hhf])
        P.op("pool", lambda e: e.tensor_tensor(out=t1_, in0=z_, in1=GA, op=ALU.mult), r=[hz, "GA"], w=[ht1])
        P.op("dve", lambda e: e.tensor_tensor(out=t1_, in0=t1_, in1=BA, op=ALU.add), r=[ht1, "BA"], w=[ht1])
        P.op("sp", lambda e: e.dma_start(out=t1_d[j * 128:(j + 1) * 128, :], in_=t1_), r=[ht1], w=[f"t1_d{j}"], dma=True)
        P.op("act", lambda e: e.activation(out=h2b[:, j, :], in_=hf, func=AF.Identity), r=[hhf], w=[f"h2b{j}"])
        return P.end()

    def zload(j):
        b = j % NB2A
        P.op("sp", lambda e: e.dma_start(out=zt[b], in_=z_d[j * 128:(j + 1) * 128, :]), r=[f"z_d{j}"], w=[f"zt{b}"], dma=True)

    def front_tr(j):
        b = j % NB2A
        mynps = nps2a_f[j % 2]
        hf, hhf = h2f[b], f"h2f{b}"
        hT_, hhT = h2T[b], f"h2T{b}"
        P.rec()
        for hh in range(2):
            pt, hp = mynps()
            for c4 in range(4):
                c = hh * 4 + c4
                P.op("pe", lambda e, pt=pt, c=c, c4=c4: e.transpose(out=pt[:, c4 * 128:(c4 + 1) * 128],
                                                                  in_=hf[:, c * 128:(c + 1) * 128], identity=identf),
                     r=[hhf, "cst"], w=[hp])
            P.op("act", lambda e, pt=pt, hh=hh: e.activation(out=hT_[:, hh * 4:(hh + 1) * 4, :].rearrange("p c t -> p (c t)"),
                                                             in_=pt[:], func=AF.Identity), r=[hp], w=[hhT + "ab"[hh]])
        return P.end()

    def back2a(j):
        b = j % NB2A
        hT_, hhT = h2T[b], f"h2T{b}"
        P.rec()
        pl, hpl = nps2a_b[j % 2]()
        for c in range(8):
            P.op("pe", lambda e, c=c: e.matmul(pl[:, 0:36], lhsT=hT_[:, c, :], rhs=wr[:, c, :], start=(c == 0), stop=(c == 7)),
                 r=[hhT + "a", hhT + "b", "wr"], w=[hpl])
        P.op("dve", lambda e: e.tensor_tensor(out=logits[:, j, :], in0=pl[:, 0:36], in1=br_bc, op=ALU.add),
             r=[hpl, "misc"], w=["logits"])
        return P.end()

    NP2A = NT // 2
    for j in range(4):
        zload(j)
    P.play(front_ew(0) + front_ew(1) + merge(front_tr(0), front_tr(1)))
    for k in range(NP2A):
        if k + 2 < NP2A:
            zload(2 * k + 4)
            zload(2 * k + 5)
        blk = []
        if k + 1 < NP2A:
            blk += merge(front_ew(2 * k + 2), front_ew(2 * k + 3))
        bk = merge(back2a(2 * k), back2a(2 * k + 1))
        tail = [o for o in bk if o[0] == "dve"]
        blk += [o for o in bk if o[0] != "dve"]
        if k + 1 < NP2A:
            blk += merge(front_tr(2 * k + 2), front_tr(2 * k + 3))
        blk += tail
        P.play(blk)
    P.fence()
    A.off = mark2a

    def T3(n):
        return A.alloc([NT, n], F32)
    gmax = A.alloc([NT], F32)
    og = T3(4)
    eg = T3(4)
    sgm = A.alloc([NT], F32)
    ptop = A.alloc([NT], F32)
    tmp4 = A.alloc([NT, 4, 8], F32)
    sel = T3(8)
    sel2 = T3(8)
    m1 = A.alloc([NT], F32)
    m2 = A.alloc([NT], F32)
    o1 = T3(8)
    o2 = T3(8)
    e2 = A.alloc([NT], F32)
    r12 = A.alloc([NT], F32)
    O1 = A.alloc([NT, 4, 8], F32)
    O2 = A.alloc([NT, 4, 8], F32)
    Obf = A.alloc([NT * 32], BF16)
    totA = A.alloc([NT, 32], F32)
    totB = A.alloc([NT, 32], F32)
    tot0 = A.alloc([NT, 32], F32)
    base = A.alloc([NT, 32], F32)
    cnt = A.alloc([32], F32)
    cmpc = A.alloc([32, 24], F32)
    pcnt = A.alloc([32], F32)
    oeA = A.alloc([32], F32)
    oeB = A.alloc([32], F32)
    offs = A.alloc([32], F32)
    cmps = A.alloc([NS, 32], F32)
    esl = A.alloc([64], F32)
    used = A.alloc([64], F32)
    posf = A.alloc([2, NT], F32)

    LG = logits[:, :, 0:4]
    LE4 = logits[:, :, 4:36].rearrange("p j (g e) -> p j g e", g=4)

    def dv(fn, r, w):
        P.op("dve", fn, r=r, w=w)

    dv(lambda e: e.tensor_reduce(out=gmax, in_=LG, axis=AX.X, op=ALU.max), ["logits"], ["gmax"])
    dv(lambda e: e.tensor_tensor(out=og, in0=LG, in1=gmax.unsqueeze(2).to_broadcast([128, NT, 4]), op=ALU.is_equal), ["logits", "gmax"], ["og"])
    dv(lambda e: e.tensor_tensor(out=eg, in0=LG, in1=gmax.unsqueeze(2).to_broadcast([128, NT, 4]), op=ALU.subtract), ["logits", "gmax"], ["eg"])
    P.op("act", lambda e: e.activation(out=eg, in_=eg, func=AF.Exp), r=["eg"], w=["eg"])
    dv(lambda e: e.tensor_reduce(out=sgm, in_=eg, axis=AX.X, op=ALU.add), ["eg"], ["sgm"])
    dv(lambda e: e.reciprocal(out=ptop, in_=sgm), ["sgm"], ["ptop"])
    dv(lambda e: e.tensor_tensor(out=tmp4, in0=LE4, in1=og.unsqueeze(3).to_broadcast([128, NT, 4, 8]), op=ALU.mult), ["logits", "og"], ["tmp4"])
    dv(lambda e: e.tensor_reduce(out=sel, in_=tmp4.rearrange("p j g e -> p j e g"), axis=AX.X, op=ALU.add), ["tmp4"], ["sel"])
    dv(lambda e: e.tensor_reduce(out=m1, in_=sel, axis=AX.X, op=ALU.max), ["sel"], ["m1"])
    dv(lambda e: e.tensor_tensor(out=o1, in0=sel, in1=m1.unsqueeze(2).to_broadcast([128, NT, 8]), op=ALU.is_equal), ["sel", "m1"], ["o1"])
    dv(lambda e: e.scalar_tensor_tensor(out=sel2.rearrange("p j e -> p (j e)"), in0=o1.rearrange("p j e -> p (j e)"), scalar=-1.0e9,
                                        in1=sel.rearrange("p j e -> p (j e)"), op0=ALU.mult, op1=ALU.add), ["o1", "sel"], ["sel2"])
    dv(lambda e: e.tensor_reduce(out=m2, in_=sel2, axis=AX.X, op=ALU.max), ["sel2"], ["m2"])
    dv(lambda e: e.tensor_tensor(out=o2, in0=sel2, in1=m2.unsqueeze(2).to_broadcast([128, NT, 8]), op=ALU.is_equal), ["sel2", "m2"], ["o2"])
    dv(lambda e: e.tensor_tensor(out=e2, in0=m2, in1=m1, op=ALU.subtract), ["m1", "m2"], ["e2"])
    P.op("act", lambda e: e.activation(out=e2, in_=e2, func=AF.Exp), r=["e2"], w=["e2"])
    dv(lambda e: e.tensor_scalar(out=r12, in0=e2, scalar1=1.0, scalar2=None, op0=ALU.add), ["e2"], ["r12"])
    dv(lambda e: e.reciprocal(out=r12, in_=r12), ["r12"], ["r12"])
    dv(lambda e: e.tensor_tensor(out=wts[:, 0, :], in0=r12, in1=ptop, op=ALU.mult), ["r12", "ptop"], ["wts"])
    dv(lambda e: e.tensor_tensor(out=wts[:, 1, :], in0=wts[:, 0, :], in1=e2, op=ALU.mult), ["wts", "e2"], ["wts"])
    dv(lambda e: e.tensor_tensor(out=O1, in0=og.unsqueeze(3).to_broadcast([128, NT, 4, 8]),
                                 in1=o1.unsqueeze(2).to_broadcast([128, NT, 4, 8]), op=ALU.mult), ["og", "o1"], ["O1"])
    dv(lambda e: e.tensor_tensor(out=O2, in0=og.unsqueeze(3).to_broadcast([128, NT, 4, 8]),
                                 in1=o2.unsqueeze(2).to_broadcast([128, NT, 4, 8]), op=ALU.mult), ["og", "o2"], ["O2"])
    O1f = O1.rearrange("p j g e -> p (j g e)")
    O2f = O2.rearrange("p j g e -> p (j g e)")
    dv(lambda e: e.tensor_tensor(out=Obf, in0=O1f, in1=O2f, op=ALU.add), ["O1", "O2"], ["Obf"])
    pcs, pts = [], []
    for h in range(2):
        pc_, hpc = nps()
        P.op("pe", lambda e, pc_=pc_, h=h: e.matmul(pc_[:], lhsT=Ub, rhs=Obf[:, h * 512:(h + 1) * 512], start=True, stop=True),
             r=["Ub", "Obf"], w=[hpc])
        pcs.append((pc_, hpc))
        pt_, hpt = nps()
        P.op("pe", lambda e, pt_=pt_, h=h: e.matmul(pt_[:], lhsT=onesb, rhs=Obf[:, h * 512:(h + 1) * 512], start=True, stop=True),
             r=["onesb", "Obf"], w=[hpt])
        pts.append((pt_, hpt))
    tot0f = tot0.rearrange("p j e -> p (j e)")
    for h in range(2):
        dv(lambda e, h=h: e.tensor_copy(out=tot0f[:, h * 512:(h + 1) * 512], in_=pts[h][0][:]), [pts[h][1]], ["tot0"])
    cur, hc = tot0, "tot0"
    for i_, sft in enumerate((1, 2, 4, 8, 16)):
        nxt, hn = (totA, "totA") if i_ % 2 == 0 else (totB, "totB")
        dv(lambda e, cur=cur, nxt=nxt, sft=sft: e.tensor_tensor(out=nxt[:, sft:, :], in0=cur[:, sft:, :], in1=cur[:, :NT - sft, :], op=ALU.add),
           [hc], [hn])
        dv(lambda e, cur=cur, nxt=nxt, sft=sft: e.tensor_copy(out=nxt[:, :sft, :], in_=cur[:, :sft, :]), [hc, hn], [hn])
        cur, hc = nxt, hn
    incl, hincl = cur, hc
    dv(lambda e: e.tensor_copy(out=cnt, in_=incl[:, NT - 1, :]), [hincl], ["cnt"])
    dv(lambda e: e.tensor_tensor(out=cmpc, in0=cnt.unsqueeze(2).to_broadcast([128, 32, 24]),
                                 in1=slot_bc[:, 0:24].unsqueeze(1).to_broadcast([128, 32, 24]), op=ALU.is_gt), ["cnt", "misc"], ["cmpc"])
    dv(lambda e: e.tensor_reduce(out=pcnt, in_=cmpc, axis=AX.X, op=ALU.add), ["cmpc"], ["pcnt"])
    dv(lambda e: e.tensor_scalar(out=pcnt, in0=pcnt, scalar1=float(T), scalar2=None, op0=ALU.mult), ["pcnt"], ["pcnt"])
    cur, hc = pcnt, "pcnt"
    for i_, sft in enumerate((1, 2, 4, 8, 16)):
        nxt, hn = (oeA, "oeA") if i_ % 2 == 0 else (oeB, "oeB")
        dv(lambda e, cur=cur, nxt=nxt, sft=sft: e.tensor_tensor(out=nxt[:, sft:], in0=cur[:, sft:], in1=cur[:, :32 - sft], op=ALU.add), [hc], [hn])
        dv(lambda e, cur=cur, nxt=nxt, sft=sft: e.tensor_copy(out=nxt[:, :sft], in_=cur[:, :sft]), [hc, hn], [hn])
        cur, hc = nxt, hn
    oend, hoend = cur, hc
    dv(lambda e: e.tensor_tensor(out=offs, in0=oend, in1=pcnt, op=ALU.subtract), [hoend, "pcnt"], ["offs"])
    dv(lambda e: e.tensor_tensor(out=base, in0=incl, in1=tot0, op=ALU.subtract), [hincl, "tot0"], ["base"])
    dv(lambda e: e.tensor_tensor(out=base, in0=base, in1=offs.unsqueeze(1).to_broadcast([128, NT, 32]), op=ALU.add), ["base", "offs"], ["base"])
    basef = base.rearrange("p j e -> p (j e)")
    for h in range(2):
        dv(lambda e, h=h: e.tensor_tensor(out=basef[:, h * 512:(h + 1) * 512], in0=pcs[h][0][:], in1=basef[:, h * 512:(h + 1) * 512], op=ALU.add),
           [pcs[h][1], "base"], ["base"])
    for k, (Ok, hO) in enumerate(((O1, "O1"), (O2, "O2"))):
        dv(lambda e, Ok=Ok: e.tensor_tensor(out=Ok.rearrange("p j g e -> p (j g e)"), in0=Ok.rearrange("p j g e -> p (j g e)"), in1=basef, op=ALU.mult),
           [hO, "base"], [hO])
        dv(lambda e, Ok=Ok, k=k: e.tensor_reduce(out=posf[:, k, :], in_=Ok.rearrange("p j g e -> p j (g e)"), axis=AX.X, op=ALU.add), [hO], ["posf"])
    dv(lambda e: e.tensor_copy(out=pos_i, in_=posf), ["posf"], ["pos_i"])
    dv(lambda e: e.tensor_tensor(out=cmps, in0=oend.unsqueeze(1).to_broadcast([128, NS, 32]),
                                 in1=slot_bc.unsqueeze(2).to_broadcast([128, NS, 32]), op=ALU.is_le), [hoend, "misc"], ["cmps"])
    dv(lambda e: e.tensor_reduce(out=esl[:, 0:NS], in_=cmps, axis=AX.X, op=ALU.add), ["cmps"], ["esl"])
    dv(lambda e: e.tensor_scalar(out=esl[:, 0:NS], in0=esl[:, 0:NS], scalar1=float(NE - 1), scalar2=128.0, op0=ALU.min, op1=ALU.mult), ["esl"], ["esl"])
    dv(lambda e: e.tensor_scalar(out=used[:, 0:NS], in0=slot_bc, scalar1=oend[:, 31:32], scalar2=None, op0=ALU.is_lt), ["misc", hoend], ["used"])
    dv(lambda e: e.tensor_scalar(out=used[:, 0:NS], in0=used[:, 0:NS], scalar1=-1.0e6, scalar2=1.0e6, op0=ALU.mult, op1=ALU.add), ["used"], ["used"])
    dv(lambda e: e.tensor_tensor(out=esl[:, 0:NS], in0=esl[:, 0:NS], in1=used[:, 0:NS], op=ALU.add), ["esl", "used"], ["esl"])
    dv(lambda e: e.tensor_scalar(out=esl[:, 0:NS], in0=esl[:, 0:NS], scalar1=cols[:, C_PID:C_PID + 1], scalar2=None, op0=ALU.add), ["esl", "cols"], ["esl"])
    dv(lambda e: e.tensor_copy(out=widx_i[:, 0:NS], in_=esl[:, 0:NS]), ["esl"], ["widx_i"])
    oh = A.alloc([NS, 32], F32)
    endv = A.alloc([32], F32)
    lim = A.alloc([64], F32)
    rrow = A.alloc([64], F32)
    pen = A.alloc([64], F32)
    yidx_f = A.alloc([NSUB, NS], F32)
    dv(lambda e: e.tensor_tensor(out=endv, in0=offs, in1=cnt, op=ALU.add), ["offs", "cnt"], ["endv"])
    dv(lambda e: e.tensor_tensor(out=oh[:, :, 1:32], in0=cmps[:, :, 0:31], in1=cmps[:, :, 1:32], op=ALU.subtract), ["cmps"], ["oh"])
    dv(lambda e: e.tensor_scalar(out=oh[:, :, 0:1], in0=cmps[:, :, 0:1], scalar1=-1.0, scalar2=1.0, op0=ALU.mult, op1=ALU.add), ["cmps", "oh"], ["oh"])
    dv(lambda e: e.tensor_tensor(out=oh, in0=oh, in1=endv.unsqueeze(1).to_broadcast([128, NS, 32]), op=ALU.mult), ["oh", "endv"], ["oh"])
    dv(lambda e: e.tensor_reduce(out=lim[:, 0:NS], in_=oh, axis=AX.X, op=ALU.add), ["oh"], ["lim"])
    for st in range(NSUB):
        dv(lambda e, st=st: e.tensor_scalar(out=rrow[:, 0:NS], in0=slot_bc, scalar1=cols[:, C_PID:C_PID + 1], scalar2=float(st * 128),
                                            op0=ALU.add, op1=ALU.add), ["misc", "cols", "rrow"], ["rrow"])
        dv(lambda e: e.tensor_tensor(out=pen[:, 0:NS], in0=rrow[:, 0:NS], in1=lim[:, 0:NS], op=ALU.is_lt), ["rrow", "lim", "pen"], ["pen"])
        dv(lambda e: e.tensor_scalar(out=pen[:, 0:NS], in0=pen[:, 0:NS], scalar1=-1.0e6, scalar2=1.0e6, op0=ALU.mult, op1=ALU.add), ["pen"], ["pen"])
        dv(lambda e, st=st: e.tensor_tensor(out=yidx_f[:, st, :], in0=rrow[:, 0:NS], in1=pen[:, 0:NS], op=ALU.add), ["rrow", "pen"], ["yidx_f"])
    dv(lambda e: e.tensor_copy(out=yidx_i, in_=yidx_f), ["yidx_f"], ["yidx_i"])

    if dbg and dbg["what"] == "route":
        P.fence()
        P.op("sp", lambda e: e.dma_start(out=dbg_d[0:128, 0:NT * 36], in_=logits.rearrange("p j n -> p (j n)")), r=["logits"], w=["dbg0"], dma=True)
        P.op("sp", lambda e: e.dma_start(out=dbg_d[128:256, 0:2 * NT], in_=posf.rearrange("p k j -> p (k j)")), r=["posf"], w=["dbg1"], dma=True)
        P.op("sp", lambda e: e.dma_start(out=dbg_d[256:384, 0:2 * NT], in_=wts.rearrange("p k j -> p (k j)")), r=["wts"], w=["dbg2"], dma=True)
        P.op("sp", lambda e: e.dma_start(out=dbg_d[384:512, 0:NS], in_=esl[:, 0:NS]), r=["esl"], w=["dbg3"], dma=True)
        P.fence()
        raise _StopEmit()

    for j in range(NT):
        for k in range(2):
            P.op("pool", lambda e, j=j, k=k: e.indirect_dma_start(
                out=xs_d[:, :], out_offset=bass.IndirectOffsetOnAxis(ap=pos_i[:, k, j:j + 1], axis=0),
                in_=h2b[:, j, :], in_offset=None), r=[f"h2b{j}", "pos_i"], w=[f"xs_{j}_{k}"], dma=True)
    P.fence()

    A.off = mark2
    NBW = 4
    wgs = [A.alloc([2048], BF16) for _ in range(NBW)]
    wus = [A.alloc([2048], BF16) for _ in range(NBW)]
    wds = [A.alloc([2048], BF16) for _ in range(NBW)]
    xtok = [A.alloc([NSUB, D], BF16) for _ in range(NBW)]
    XT = [A.alloc([8, T], BF16) for _ in range(2)]
    sgs = [A.alloc([T], F32) for _ in range(2)]
    aT = [A.alloc([2, T], BF16) for _ in range(2)]
    NYO = 4
    yo = [A.alloc([D], F32) for _ in range(NYO)]
    npsB = mk_nps([0, 1, 2])
    npsC = mk_nps([3, 4, 5])
    _bc = {}

    def get_bc(e):
        if "v" not in _bc:
            reg = e.alloc_register("bcreg")
            e.reg_mov(reg, NE * 128 - 1)
            _bc["v"] = e.snap(reg, donate=True)
        return _bc["v"]

    ORDER = []
    for q in range((NS + 1) // 2):
        ORDER.append(q)
        if NS - 1 - q > q:
            ORDER.append(NS - 1 - q)
    assert sorted(ORDER) == list(range(NS))

    _bc2 = {}

    def get_bc2(e):
        if "v" not in _bc2:
            reg = e.alloc_register("bcreg2")
            e.reg_mov(reg, NS * T - 1)
            _bc2["v"] = e.snap(reg, donate=True)
        return _bc2["v"]

    def load_w(i, which):
        s = ORDER[i]
        bw = i % NBW
        for (wsb, wdr, hn) in which(bw):
            P.op("pool", lambda e, wsb=wsb, wdr=wdr, s=s: e.indirect_dma_start(
                out=wsb, out_offset=None, in_=wdr[:, :],
                in_offset=bass.IndirectOffsetOnAxis(ap=widx_i[:, s:s + 1], axis=0),
                bounds_check=get_bc(e), oob_is_err=False), r=["widx_i"], w=[hn], dma=True)

    def w_gu(bw):
        return ((wgs[bw], wg_d, f"wg{bw}"), (wus[bw], wu_d, f"wu{bw}"))

    def w_d(bw):
        return ((wds[bw], wd_d, f"wd{bw}"),)

    def load_x(i):
        s = ORDER[i]
        bw = i % NBW
        for st in range(NSUB):
            r0 = s * T + st * 128
            P.op("sp", lambda e, bw=bw, st=st, r0=r0: e.dma_start(out=xtok[bw][:, st, :], in_=xs_d[r0:r0 + 128, :]),
                 w=[f"xtok{bw}_{st}"], dma=True)

    def stageA(i):
        s = ORDER[i]
        b, bw = i % 2, i % NBW
        P.rec()
        for st in range(NSUB):
            k = (i * NSUB + st) % 2
            pb_, hpb = psb[k], f"psb{k}"
            for c in range(8):
                P.op("pe", lambda e, pb_=pb_, st=st, c=c: e.transpose(out=pb_[:, c * 128:(c + 1) * 128],
                                                                     in_=xtok[bw][:, st, c * 128:(c + 1) * 128], identity=identb),
                     r=[f"xtok{bw}_{st}", "identb"], w=[hpb])
            if True:
                P.op("act", lambda e, pb_=pb_, st=st: e.activation(out=XT[b][:, :, st * 128:(st + 1) * 128],
                                                                   in_=pb_[:].rearrange("p (c t) -> p c t", c=8), func=AF.Identity),
                     r=[hpb], w=[f"XT{b}_{st}"])
            else:
                P.op("dve", lambda e, pb_=pb_, st=st: e.tensor_copy(out=XT[b][:, :, st * 128:(st + 1) * 128],
                                                                    in_=pb_[:].rearrange("p (c t) -> p c t", c=8)),
                     r=[hpb], w=[f"XT{b}_{st}"])
        return P.end()

    def stageB(i):
        s = ORDER[i]
        b, bw = i % 2, i % NBW
        XH = [f"XT{b}_{st}" for st in range(NSUB)]
        P.rec()
        for fch in range(2):
            pg, hpg = npsB()
            for c in range(8):
                P.op("pe", lambda e, pg=pg, c=c, fch=fch: e.matmul(
                    pg[:, 0:T], lhsT=wgs[bw][:, c * 256 + fch * 128:c * 256 + (fch + 1) * 128], rhs=XT[b][:, c, :],
                    start=(c == 0), stop=(c == 7)), r=[f"wg{bw}"] + XH, w=[hpg])
            P.op("act", lambda e, pg=pg: e.activation(out=sgs[b], in_=pg[:, 0:T], func=AF.Silu), r=[hpg], w=[f"sgs{b}"])
            pu, hpu = npsB()
            for c in range(8):
                P.op("pe", lambda e, pu=pu, c=c, fch=fch: e.matmul(
                    pu[:, 0:T], lhsT=wus[bw][:, c * 256 + fch * 128:c * 256 + (fch + 1) * 128], rhs=XT[b][:, c, :],
                    start=(c == 0), stop=(c == 7)), r=[f"wu{bw}"] + XH, w=[hpu])
            P.op("dve", lambda e, pu=pu, fch=fch: e.tensor_tensor(out=aT[b][:, fch, :], in0=pu[:, 0:T], in1=sgs[b], op=ALU.mult),
                 r=[hpu, f"sgs{b}"], w=[f"aT{b}_{fch}"])
        return P.end()

    def stageC(i):
        s = ORDER[i]
        b, bw = i % 2, i % NBW
        P.rec()
        for st in range(NSUB):
            yb = (i * NSUB + st) % NYO
            for half in range(2):
                po, hpo = npsC()
                for fch in range(2):
                    P.op("pe", lambda e, po=po, st=st, fch=fch, half=half: e.matmul(
                        po[:], lhsT=aT[b][:, fch, st * 128:(st + 1) * 128],
                        rhs=wds[bw][:, fch * 1024 + half * 512:fch * 1024 + (half + 1) * 512], start=(fch == 0), stop=(fch == 1)),
                        r=[f"aT{b}_0", f"aT{b}_1", f"wd{bw}"], w=[hpo])
                P.op("dve", lambda e, po=po, yb=yb, half=half: e.tensor_tensor(
                    out=yo[yb][:, half * 512:(half + 1) * 512], in0=po[:], in1=gbc[:, 3, half * 512:(half + 1) * 512], op=ALU.mult),
                    r=[hpo, "gbc3_0", "gbc3_1"], w=[f"yo{yb}{'ab'[half]}"])
            P.op("pool", lambda e, yb=yb, st=st: e.indirect_dma_start(
                out=ys_d[:, :], out_offset=bass.IndirectOffsetOnAxis(ap=yidx_i[:, st, s:s + 1], axis=0),
                in_=yo[yb], in_offset=None, bounds_check=get_bc2(e), oob_is_err=False),
                r=[f"yo{yb}a", f"yo{yb}b", "yidx_i"], w=[f"ys_{s}_{st}"], dma=True)
        return P.end()

    for s in range(min(NBW, NS)):
        load_x(s)
        load_w(s, w_gu)
        load_w(s, w_d)
    for i in range(NS + 2):
        lists = []
        if i < NS:
            lists.append(stageA(i))
        if 0 <= i - 1 < NS:
            lists.append(stageB(i - 1))
        if 0 <= i - 2 < NS:
            lists.append(stageC(i - 2))
        P.play(merge(*lists))
        if i + NBW < NS:
            load_x(i + NBW)
        if i - 1 >= 0 and i - 1 + NBW < NS:
            load_w(i - 1 + NBW, w_gu)
        if i - 2 >= 0 and i - 2 + NBW < NS:
            load_w(i - 2 + NBW, w_d)
    P.fence()

    A.off = mark2
    NB2F = 4
    Y1 = [A.alloc([D], F32) for _ in range(NB2F)]
    Y2 = [A.alloc([D], F32) for _ in range(NB2F)]
    t1 = [A.alloc([D], F32) for _ in range(NB2F)]
    ff = [A.alloc([D], F32) for _ in range(2)]
    r2 = [A.alloc([D], F32) for _ in range(2)]
    qq = [A.alloc([D], F32) for _ in range(2)]
    ob = [A.alloc([D], F32) for _ in range(2)]
    bn2 = [A.alloc([12], F32) for _ in range(2)]
    ag2 = [A.alloc([4], F32) for _ in range(2)]

    def loads2f(j):
        q = j % NB2F
        P.op("pool", lambda e: e.indirect_dma_start(
            out=Y1[q], out_offset=None, in_=ys_d[:, :], in_offset=bass.IndirectOffsetOnAxis(ap=pos_i[:, 0, j:j + 1], axis=0)),
            r=["pos_i"], w=[f"Y1{q}"], dma=True)
        P.op("pool", lambda e: e.indirect_dma_start(
            out=Y2[q], out_offset=None, in_=ys_d[:, :], in_offset=bass.IndirectOffsetOnAxis(ap=pos_i[:, 1, j:j + 1], axis=0)),
            r=["pos_i"], w=[f"Y2{q}"], dma=True)
        P.op("sp", lambda e: e.dma_start(out=t1[q], in_=t1_d[j * 128:(j + 1) * 128, :]), r=[f"t1_d{j}"], w=[f"t1{q}"], dma=True)

    def chain2f(j):
        b = j % 2
        q = j % NB2F
        P.rec()
        P.op("act", lambda e: e.activation(out=ff[b], in_=Y1[q], func=AF.Identity, scale=wts[:, 0, j:j + 1]), r=[f"Y1{q}", "wts"], w=[f"ff{b}"])
        P.op("dve", lambda e: e.scalar_tensor_tensor(out=ff[b], in0=Y2[q], scalar=wts[:, 1, j:j + 1], in1=ff[b], op0=ALU.mult, op1=ALU.add),
             r=[f"Y2{q}", "wts", f"ff{b}"], w=[f"ff{b}"])
        P.op("dve", lambda e: e.tensor_tensor(out=r2[b], in0=ff[b], in1=t1[q], op=ALU.add), r=[f"ff{b}", f"t1{q}"], w=[f"r2{b}"])
        for h in range(2):
            P.op("dve", lambda e, h=h: e.bn_stats(out=bn2[b][:, h * 6:(h + 1) * 6], in_=r2[b][:, h * 512:(h + 1) * 512]), r=[f"r2{b}"], w=[f"bn2{b}"])
        P.op("dve", lambda e: e.bn_aggr(out=ag2[b][:, 0:2], in_=bn2[b]), r=[f"bn2{b}"], w=[f"ag2{b}"])
        P.op("act", lambda e: e.activation(out=ag2[b][:, 2:3], in_=ag2[b][:, 1:2], func=AF.Sqrt, bias=EPSC, scale=1.0), r=[f"ag2{b}", "epsc"], w=[f"ag2b{b}"])
        P.op("dve", lambda e: e.reciprocal(out=ag2[b][:, 2:3], in_=ag2[b][:, 2:3]), r=[f"ag2b{b}"], w=[f"ag2b{b}"])
        P.op("dve", lambda e: e.scalar_tensor_tensor(out=ag2[b][:, 3:4], in0=ag2[b][:, 0:1], scalar=-1.0, in1=ag2[b][:, 2:3], op0=ALU.mult, op1=ALU.mult),
             r=[f"ag2{b}", f"ag2b{b}"], w=[f"ag2c{b}"])
        P.op("act", lambda e: e.activation(out=qq[b], in_=r2[b], func=AF.Identity, bias=ag2[b][:, 3:4], scale=ag2[b][:, 2:3]),
             r=[f"r2{b}", f"ag2b{b}", f"ag2c{b}"], w=[f"qq{b}"])
        P.op("pool", lambda e: e.tensor_tensor(out=qq[b], in0=qq[b], in1=lnr[:, 2, :], op=ALU.mult), r=[f"qq{b}", "lnr"], w=[f"qq{b}"])
        P.op("dve", lambda e: e.tensor_tensor(out=ob[b], in0=qq[b], in1=lnr[:, 3, :], op=ALU.add), r=[f"qq{b}", "lnr"], w=[f"ob{b}"])
        P.op("sp", lambda e: e.dma_start(out=out_d[j * 128:(j + 1) * 128, :], in_=ob[b]), r=[f"ob{b}"], w=[f"out{j}"], dma=True)
        return P.end()

    loads2f(0)
    loads2f(1)
    for j in range(0, NT, 2):
        if j + 2 < NT:
            loads2f(j + 2)
            loads2f(j + 3)
        P.play(merge(chain2f(j), chain2f(j + 1)))


class _StopEmit(Exception):
    pass


def _host_prep(inp, b):
    f = np.float32
    L = 0
    cols = np.zeros((128, NCOL), f)
    cols[:, C_C:C_C + 8] = inp["c"][b].reshape(8, 128).T
    w_in = inp["w_in"][L]
    b_in = inp["b_in"][L]
    qcols = np.concatenate([np.arange(1024 + h * 64, 1024 + (h + 1) * 64) for h in HORDER])
    perm = np.concatenate([np.arange(0, 1024), qcols, np.arange(1536, 1792)])
    w_in_p = np.ascontiguousarray(w_in[:, perm])
    b_in_p = b_in[perm]
    cols[:, C_BIN:C_BIN + 13] = b_in_p[:1664].reshape(13, 128).T
    cols[:, C_CW:C_CW + 124] = inp["conv_w"][L].T.reshape(4, 128, 31).transpose(1, 0, 2).reshape(128, 124)
    cols[:, C_CB:C_CB + 4] = inp["conv_b"][L].reshape(4, 128).T
    cols[:, C_LG:C_LG + 4] = inp["conv_ln_g"][L].reshape(4, 128).T
    cols[:, C_LB:C_LB + 4] = inp["conv_ln_b"][L].reshape(4, 128).T
    cols[:, C_OG:C_OG + 4] = inp["conv_out_g"][L].reshape(4, 128).T
    cols[:, C_PID] = np.arange(128)
    rows = np.zeros((1, NROW), f)
    rows[0, R_BADA:R_BADA + 6144] = inp["b_ada"][L]
    rows[0, R_BV:R_BV + 128] = b_in[1664:1792]
    rows[0, R_SINK:R_SINK + 8] = inp["sinks"][L][HORDER]
    rows[0, R_AOG:R_AOG + 512] = inp["attn_out_g"][L].reshape(8, 64)[HORDER].reshape(-1)
    rows[0, R_BOUT:R_BOUT + D] = inp["b_out"][L]
    rows[0, R_L1G:R_L1G + D] = inp["ln1_g"][L]
    rows[0, R_L1B:R_L1B + D] = inp["ln1_b"][L]
    rows[0, R_L2G:R_L2G + D] = inp["ln2_g"][L]
    rows[0, R_L2B:R_L2B + D] = inp["ln2_b"][L]
    rows[0, R_BR:R_BR + 4] = inp["b_router_group"][L]
    rows[0, R_BR + 4:R_BR + 36] = inp["b_router_expert"][L]
    rows[0, R_SLOT:R_SLOT + NS] = np.arange(NS) * T
    w_out = inp["w_out"][L]
    arows = np.concatenate([np.arange(512 + h * 64, 512 + (h + 1) * 64) for h in HORDER])
    w_out_p = np.ascontiguousarray(np.concatenate([w_out[:512], w_out[arows]], axis=0))
    w_r = np.ascontiguousarray(np.concatenate([inp["w_router_group"][L], inp["w_router_expert"][L]], axis=1))
    return dict(cols=cols, rows=rows, w_in=w_in_p, w_out=w_out_p, w_r=w_r)


def _consts():
    f = np.float32
    cst = np.zeros((128, NK), f)
    cst[:, K_ID:K_ID + 128] = np.eye(128)
    p = np.arange(128)
    cst[:, K_U:K_U + 128] = (p[:, None] < p[None, :])
    cst[:, K_BD:K_BD + 128] = ((p[:, None] // 64) == (p[None, :] // 64)) / 64.0
    m0 = np.where(p[:, None] > p[None, :], 0.0, NEG)
    m1 = np.where(p[:, None] <= p[None, :], 0.0, NEG)
    cst[:, K_M0:K_M0 + 512] = np.tile(m0, (1, 4))
    cst[:, K_M1:K_M1 + 512] = np.tile(m1, (1, 4))
    return cst


def _expert_layout(inp):
    L = 0
    wg = np.ascontiguousarray(inp["w_gate"][L].reshape(NE, 8, 128, DE).transpose(0, 2, 1, 3)).reshape(NE * 128, 2048)
    wu = np.ascontiguousarray(inp["w_up"][L].reshape(NE, 8, 128, DE).transpose(0, 2, 1, 3)).reshape(NE * 128, 2048)
    wd = np.ascontiguousarray(inp["w_down"][L].reshape(NE, 2, 128, D).transpose(0, 2, 1, 3)).reshape(NE * 128, 2048)
    return wg, wu, wd


_CACHE = {}


def kernel(**inputs):
    inp = {k: np.asarray(v) for k, v in inputs.items()}
    dbg = _CACHE.get("dbg")
    nc = build_program(dbg)
    cst = _consts()
    wg, wu, wd = _expert_layout(inp)
    w_ada = np.ascontiguousarray(inp["w_ada"][0])
    in_maps = []
    ncores = _CACHE.get("ncores", 8)
    for b in range(ncores):
        hp = _host_prep(inp, b)
        m = dict(x=np.ascontiguousarray(inp["x"][b]), cols=hp["cols"], rows=hp["rows"], cst=cst, w_ada=w_ada,
                 w_in=hp["w_in"], w_out=hp["w_out"], w_r=hp["w_r"], wg=wg, wu=wu, wd=wd)
        if _CACHE.get("p2only"):
            m["z_in"] = _CACHE["z_in"]
        in_maps.append(m)
    if _CACHE.get("trace"):
        res = run_bass_kernel_spmd(nc, in_maps, core_ids=list(range(ncores)), trace=True)
        print("EXEC_NS", res.exec_time_ns)
    else:
        res = run_bass_kernel_spmd(nc, in_maps, core_ids=list(range(ncores)))
    if dbg:
        return [np.asarray(r["dbg"]) for r in res.results]
    out = np.stack([np.asarray(r["out"]) for r in res.results], axis=0).astype(np.float32)
    return out
```

```python
import os
import numpy as np
import concourse.bass as bass
import concourse.mybir as mybir
from concourse.bass_utils import run_bass_kernel_spmd

F32 = mybir.dt.float32
BF16 = mybir.dt.bfloat16
I32 = mybir.dt.int32
ALU = mybir.AluOpType
AF = mybir.ActivationFunctionType
AX = mybir.AxisListType

D = 1024
S = 4096
NT = S // 128
NMT = S // 512
DIN = 1792
NE = 32
DE = 256
ALPHA = 2.0 ** 0.25
EPS = 1e-5
NEG = -30000.0
T = 384
NS = (2 * S + NE * (T - 1) + T - 1) // T
NSUB = T // 128
HORDER = [0, 4, 1, 5, 2, 6, 3, 7]

C_C = 0
C_BIN = 8
C_CW = 21
C_CB = 145
C_LG = 149
C_LB = 153
C_OG = 157
C_PID = 161
NCOL = 162
R_BADA = 0
R_BV = 6144
R_SINK = 6272
R_AOG = 6280
R_BOUT = 6792
R_L1G = 7816
R_L1B = 8840
R_L2G = 9864
R_L2B = 10888
R_BR = 11912
R_SLOT = 11948
NROW = 12012
K_ID = 0
K_U = 128
K_BD = 256
K_M0 = 384
K_M1 = 896
NK = 1408


class Prog:
    def __init__(self, nc, sems):
        self.nc = nc
        self.ops = []
        self.last_w = {}
        self.readers = {}
        self.eng_sem = {e: sems[i] for i, e in enumerate(["pe", "act", "dve", "pool"])}
        rest = sems[4:]
        n_sp = (len(rest) * 5) // 10
        n_pool = (len(rest) * 4) // 10
        self.dma_pool = {"sp": rest[:n_sp], "pool": rest[n_sp:n_sp + n_pool], "act": rest[n_sp + n_pool:]}
        self._rec = None

    def rec(self):
        assert self._rec is None
        self._rec = []

    def end(self):
        l = self._rec
        self._rec = None
        return l

    def play(self, lst):
        assert self._rec is None
        for o in lst:
            self.op(*o)

    def op(self, eng, fn, r=(), w=(), dma=False):
        if self._rec is not None:
            self._rec.append((eng, fn, list(r), list(w), dma))
            return None
        i = len(self.ops)
        w = list(w) + [h for h in r if h.startswith("ps") and h not in w]
        raw, oth = set(), set()
        for h in r:
            if h in self.last_w:
                raw.add(self.last_w[h])
        for h in w:
            if h in self.last_w:
                oth.add(self.last_w[h])
            for j in self.readers.get(h, ()):
                oth.add(j)
        for h in w:
            self.last_w[h] = i
            self.readers[h] = []
        for h in r:
            self.readers.setdefault(h, []).append(i)
        deps = []
        for j in sorted(raw | oth):
            p = self.ops[j]
            if j == i:
                continue
            if (not p["dma"]) and p["eng"] == eng:
                if eng == "pe" or j not in raw:
                    continue
            deps.append(j)
        self.ops.append(dict(eng=eng, fn=fn, deps=deps, dma=dma, sig=False))
        for j in deps:
            self.ops[j]["sig"] = True
        return i

    def fence(self, engs=("pe", "act", "dve", "pool", "sp")):
        hs = list(self.last_w.keys())
        for e in engs:
            self.op(e, None, r=hs, w=["_fence_" + e])

    def emit(self):
        nc = self.nc
        ticket = {e: 0 for e in self.eng_sem}
        dma_next = {q: 0 for q in self.dma_pool}
        dma_uses = {}
        for o in self.ops:
            if o["dma"]:
                q = o["eng"]
                pool = self.dma_pool[q]
                sem = pool[dma_next[q] % len(pool)]
                dma_next[q] += 1
                u = dma_uses.get(id(sem), 0)
                o["pre"] = (sem, 16 * u)
                dma_uses[id(sem)] = u + 1
                o["ev"] = (sem, 16 * (u + 1))
            elif o["sig"] and o["fn"] is not None:
                ticket[o["eng"]] += 1
                o["ev"] = (self.eng_sem[o["eng"]], ticket[o["eng"]])
        ops = self.ops

        def run(engname, eobj):
            waited = {}

            def wait(sem, val):
                if val <= 0:
                    return
                if waited.get(id(sem), 0) >= val:
                    return
                eobj.wait_ge(sem, val)
                waited[id(sem)] = val

            for o in ops:
                if o["eng"] != engname:
                    continue
                for j in o["deps"]:
                    ev = ops[j].get("ev")
                    if ev is not None:
                        wait(*ev)
                if o["fn"] is None:
                    continue
                if o["dma"]:
                    wait(*o["pre"])
                    ins = o["fn"](eobj)
                    ins.then_inc(o["ev"][0], 16)
                else:
                    ins = o["fn"](eobj)
                    if o["sig"]:
                        ins.then_inc(o["ev"][0], 1)

        with nc.Block() as block:
            @block.tensor
            def _(e):
                run("pe", e)

            @block.scalar
            def _(e):
                run("act", e)

            @block.vector
            def _(e):
                run("dve", e)

            @block.gpsimd
            def _(e):
                run("pool", e)

            @block.sync
            def _(e):
                run("sp", e)


class _Stop(Exception):
    pass


def merge(*lists):
    lists = [l for l in lists if l]
    out = []
    idx = [0] * len(lists)
    total = sum(len(l) for l in lists)
    while len(out) < total:
        best, bv = None, None
        for k, l in enumerate(lists):
            if idx[k] < len(l):
                v = (idx[k] + 0.5) / len(l)
                if bv is None or v < bv:
                    best, bv = k, v
        out.append(lists[best][idx[best]])
        idx[best] += 1
    return out


class Arena:
    def __init__(self, t, nbytes):
        self.t = t
        self.nbytes = nbytes
        self.off = 0

    def alloc(self, shape, dt):
        esz = 4 if dt in (F32, I32) else 2
        n = int(np.prod(shape)) * esz
        n = (n + 63) // 64 * 64
        assert self.off + n <= self.nbytes, ("arena overflow", self.off, n, self.nbytes)
        v = self.t[:, self.off // 4:(self.off + n) // 4]
        self.off += n
        if dt != F32:
            v = v.bitcast(dt)
        v = v[:, 0:int(np.prod(shape))]
        if len(shape) == 2:
            return v.rearrange("p (a b) -> p a b", a=shape[0])
        if len(shape) == 3:
            return v.rearrange("p (a b c) -> p a b c", a=shape[0], b=shape[1])
        return v


def build_program(dbg=None):
    nc = bass.Bass("TRN2", target_bir_lowering=False)
    try:
        return _build_program(nc, dbg)
    except _Stop:
        return nc


def _build_program(nc, dbg=None):
    dr = {}

    def din(name, shape, dt=F32):
        dr[name] = nc.dram_tensor(name, list(shape), dt, kind="ExternalInput").ap()
        return dr[name]

    x_d = din("x", [S, D])
    cols_d = din("cols", [128, NCOL])
    rows_d = din("rows", [1, NROW])
    cst_d = din("cst", [128, NK])
    wada_d = din("w_ada", [D, 6 * D])
    win_d = din("w_in", [D, DIN])
    wout_d = din("w_out", [D, D])
    wr_d = din("w_r", [D, 36])
    wg_d = din("wg", [NE * 128, 2048])
    wu_d = din("wu", [NE * 128, 2048])
    wd_d = din("wd", [NE * 128, 2048])
    ZR = 1152
    zeros_d = din("zeros", [ZR, D // 2], F32).bitcast(BF16)
    out_d = nc.dram_tensor("out", [S, D], F32, kind="ExternalOutput").ap()
    if _CACHE.get("p2only"):
        z_d = din("z_in", [S, D])
    else:
        z_d = nc.dram_tensor("z_scr", [S, D], F32, kind="Internal").ap()
    xs_d = nc.dram_tensor("xs_scr", [NS * T, D], BF16, kind="Internal").ap()
    ys_d = nc.dram_tensor("ys_scr", [NS * T, D], F32, kind="Internal").ap()
    dbg_d = None
    if dbg:
        dbg_d = nc.dram_tensor("dbg", list(dbg["shape"]), F32, kind="ExternalOutput").ap()

    import contextlib
    with contextlib.ExitStack() as st:
        ARENA_BYTES = 206 * 1024
        arena_t = st.enter_context(nc.sbuf_tensor("arena", [128, ARENA_BYTES // 4], F32))
        ps = [st.enter_context(nc.psum_tensor(f"ps{i}", [128, 512], F32)) for i in range(6)]
        psb = [st.enter_context(nc.psum_tensor(f"psb{i}", [128, 1024], BF16)) for i in range(2)]
        sems = [st.enter_context(nc.semaphore(f"s{i}")) for i in range(_CACHE.get("nsem", 48))]
        P = Prog(nc, sems)
        A = Arena(arena_t, ARENA_BYTES)
        psn = [0]

        def nps():
            i = psn[0] % 6
            psn[0] += 1
            return ps[i], f"ps{i}"

        cols = A.alloc([NCOL], F32)
        identf = A.alloc([128], F32)
        identb = A.alloc([128], BF16)
        onesb = A.alloc([128], BF16)
        ones512 = A.alloc([128], BF16)
        bdb = A.alloc([128], BF16)
        Ub = A.alloc([128], BF16)
        maskT = A.alloc([2, 512], BF16)
        modc = A.alloc([32], F32)
        g1bc = A.alloc([D], F32)
        gbrow = A.alloc([D], BF16)
        EPSC = A.alloc([1], F32)
        EPSC2 = A.alloc([1], F32)
        mark_persist = A.off
        cst = A.alloc([NK], F32)
        gbc = A.alloc([4, D], F32)
        mod_d = nc.dram_tensor("mod_scr", [1, 3 * D], F32, kind="Internal").ap()

        P.op("sp", lambda e: e.dma_start(out=cols, in_=cols_d), w=["cols"], dma=True)
        P.op("sp", lambda e: e.dma_start(out=cst, in_=cst_d), w=["cst0"], dma=True)
        P.op("sp", lambda e: e.dma_start(out=identf, in_=cst_d[:, K_ID:K_ID + 128]), w=["cst"], dma=True)
        P.op("dve", lambda e: e.tensor_copy(out=identb, in_=identf), r=["cst"], w=["identb"])
        P.op("dve", lambda e: e.memset(onesb, 1.0), w=["onesb"])
        P.op("dve", lambda e: e.memset(EPSC, EPS), w=["epsc"])
        P.op("dve", lambda e: e.memset(EPSC2, EPS / (ALPHA * ALPHA)), w=["epsc2"])
        P.op("dve", lambda e: e.memset(ones512, 1.0 / 512.0), w=["ones512"])
        P.op("dve", lambda e: e.tensor_copy(out=bdb, in_=cst[:, K_BD:K_BD + 128]), r=["cst0"], w=["bdb"])
        P.op("dve", lambda e: e.tensor_copy(out=Ub, in_=cst[:, K_U:K_U + 128]), r=["cst0"], w=["Ub"])
        P.op("dve", lambda e: e.tensor_copy(out=maskT.rearrange("p a b -> p (a b)"), in_=cst[:, K_M0:K_M0 + 1024]),
             r=["cst0"], w=["maskT"])

        ph0 = A.off
        cact = A.alloc([8], F32)
        cbc = A.alloc([8, 128], BF16)
        wab = [A.alloc([8, 512], BF16) for _ in range(2)]
        badab = A.alloc([512], F32)
        modb = A.alloc([4 * D], F32)
        P.op("act", lambda e: e.activation(out=cact, in_=cols[:, C_C:C_C + 8], func=AF.Silu), r=["cols"], w=["cact"])
        for c in range(8):
            P.op("dve", lambda e, c=c: e.tensor_scalar(out=cbc[:, c, :], in0=onesb, scalar1=cact[:, c:c + 1],
                                                       scalar2=None, op0=ALU.mult),
                 r=["cact", "onesb"], w=["cbc"])
        for blk in range(12):
            wb = wab[blk % 2]
            hw = f"wab{blk % 2}"
            P.op("pool", lambda e, blk=blk, wb=wb: e.dma_start(
                out=wb, in_=wada_d[:, blk * 512:(blk + 1) * 512].rearrange("(c p) n -> p c n", p=128)),
                w=[hw], dma=True)
            P.op("sp", lambda e, blk=blk: e.dma_start(
                out=badab, in_=rows_d[0:1, R_BADA + blk * 512:R_BADA + (blk + 1) * 512].partition_broadcast(128)),
                w=["badab"], dma=True)
            pt, hp = nps()
            for c in range(8):
                P.op("pe", lambda e, c=c, pt=pt, wb=wb: e.matmul(pt[:], lhsT=cbc[:, c, :], rhs=wb[:, c, :],
                                                               start=(c == 0), stop=(c == 7)),
                     r=["cbc", hw], w=[hp])
            if blk < 4:
                dst = modb[:, blk * 512:(blk + 1) * 512]
                hd = f"modb{blk}"
            else:
                gi = (blk - 4) // 2
                dst = gbc[:, gi, ((blk - 4) % 2) * 512:((blk - 4) % 2 + 1) * 512]
                hd = f"gbc{gi}_{blk % 2}"
            P.op("dve", lambda e, pt=pt, dst=dst: e.tensor_tensor(out=dst, in0=pt[:], in1=badab, op=ALU.add),
                 r=[hp, "badab"], w=[hd])
        srcs = []
        for c in range(8):
            srcs.append((modb[:, c * 128:(c + 1) * 128], f"modb{c // 4}", c, 0.0))
        for c in range(8):
            srcs.append((modb[:, 1024 + c * 128:1024 + (c + 1) * 128], f"modb{2 + c // 4}", 8 + c, 1.0))
        for c in range(8):
            srcs.append((gbc[:, 1, c * 128:(c + 1) * 128], f"gbc1_{c // 4}", 16 + c, 0.0))
        for c in range(8):
            srcs.append((gbc[:, 2, c * 128:(c + 1) * 128], f"gbc2_{c // 4}", 24 + c, 1.0))
        for (src, hs, col, add) in srcs:
            pt, hp = nps()
            P.op("pe", lambda e, pt=pt, src=src: e.transpose(out=pt[:, 0:128], in_=src, identity=identf),
                 r=[hs, "cst"], w=[hp])
            P.op("dve", lambda e, pt=pt, col=col, add=add: e.tensor_scalar(
                out=modc[:, col:col + 1], in0=pt[:, 0:1], scalar1=add, scalar2=None, op0=ALU.add),
                r=[hp], w=["modc"])
        G_ALL = [f"gbc{gi}_{h}" for gi in range(4) for h in range(2)]
        P.op("sp", lambda e: e.dma_start(out=mod_d, in_=gbc[0:1, 1:4, :].rearrange("p a d -> p (a d)")), r=G_ALL, w=["mod_d"], dma=True)
        P.op("dve", lambda e: e.tensor_scalar(out=g1bc, in0=gbc[:, 0, :], scalar1=1.0 / ALPHA, scalar2=None, op0=ALU.mult), r=G_ALL, w=["g1bc"])
        boutb = modb[:, 0:D]
        P.op("sp", lambda e: e.dma_start(out=boutb, in_=rows_d[0:1, R_BOUT:R_BOUT + D].partition_broadcast(128)),
             w=["modb0", "modb1"], dma=True)
        P.op("dve", lambda e: e.tensor_tensor(out=boutb, in0=boutb, in1=g1bc, op=ALU.mult), r=["modb0", "modb1", "g1bc"], w=["modb0", "modb1"])
        P.op("dve", lambda e: e.tensor_copy(out=gbrow, in_=boutb), r=["modb0", "modb1"], w=["gbrow"])
        P.fence()
        if _CACHE.get("stop") == 0:
            P.op("sp", lambda e: e.dma_start(out=dbg_d[0:128, 0:32], in_=modc), r=["modc"], w=["dbg"], dma=True)
            P.op("sp", lambda e: e.dma_start(out=dbg_d[128:256, :], in_=gbc[:, 0, :]), r=["gbc0_0", "gbc0_1"], w=["dbg2"], dma=True)
            P.fence()
            P.emit()
            return nc
        A.off = mark_persist

        win = A.alloc([8, DIN], BF16)
        modc2 = A.alloc([32], F32)
        P.op("dve", lambda e: e.tensor_copy(out=modc2, in_=modc), r=["modc"], w=["modc2"])
        wout = A.alloc([8, D], BF16)
        diag = A.alloc([124, 128], BF16)
        xt = A.alloc([4, D], BF16)
        hT = A.alloc([8, 512], BF16)
        vr = [A.alloc([4, 542], BF16) for _ in range(2)]
        kr = [[A.alloc([640], BF16) for _ in range(2)] for _ in range(2)]
        va = [A.alloc([5, 130], BF16) for _ in range(2)]
        qT2 = [A.alloc([4, 512], BF16) for _ in range(2)]
        sig = A.alloc([512], F32)
        ybf2 = [A.alloc([4, 512], BF16) for _ in range(2)]
        y2bf = A.alloc([4, 512], BF16)
        rstd2 = [A.alloc([512], F32) for _ in range(2)]
        nmr2 = [A.alloc([512], F32) for _ in range(2)]
        zc2 = [A.alloc([512], F32) for _ in range(2)]
        sc2_ = [A.alloc([512], F32) for _ in range(2)]
        s2bf2 = [A.alloc([512], BF16) for _ in range(2)]
        r2c2 = [A.alloc([512], F32) for _ in range(2)]
        ycT = A.alloc([8, 512], BF16)
        mx2 = [A.alloc([8], F32) for _ in range(2)]
        mxb2 = [A.alloc([8], BF16) for _ in range(2)]
        nmx2 = [A.alloc([8], F32) for _ in range(2)]
        dcat2 = [A.alloc([2, 512], BF16) for _ in range(2)]
        ET2 = [A.alloc([4, 512], BF16) for _ in range(2)]
        es_t2 = [A.alloc([8], F32) for _ in range(2)]
        den2 = [A.alloc([8], F32) for _ in range(2)]
        osb2 = [A.alloc([8, 64], F32) for _ in range(2)]
        osq2 = [A.alloc([8, 64], F32) for _ in range(2)]
        ssq2 = [A.alloc([8], F32) for _ in range(2)]
        yat2 = [A.alloc([512], BF16) for _ in range(2)]
        rows_sb = A.alloc([128 + 8 + 512], F32)
        xr2 = [A.alloc([D], F32) for _ in range(2)]
        bnst2 = [A.alloc([12], F32) for _ in range(2)]
        bnag2 = [A.alloc([4], F32) for _ in range(2)]
        print("phase1 arena bytes", A.off)

        def mk_nps(ids):
            st_ = [0]

            def f():
                i = ids[st_[0] % len(ids)]
                st_[0] += 1
                return ps[i], f"ps{i}"
            return f

        bv_bc = rows_sb[:, 0:128]
        sink_bc = rows_sb[:, 128:136]
        aog_bc = rows_sb[:, 136:648]
        P.op("sp", lambda e: e.dma_start(out=rows_sb, in_=rows_d[0:1, R_BV:R_BV + 128 + 8 + 512].partition_broadcast(128)),
             w=["rows_sb"], dma=True)
        for c in range(8):
            P.op("pool", lambda e, c=c: e.dma_start(out=win[:, c, :], in_=win_d[c * 128:(c + 1) * 128, :]), w=[f"win{c}"], dma=True)
            P.op("pool", lambda e, c=c: e.dma_start(out=wout[:, c, :], in_=wout_d[c * 128:(c + 1) * 128, :]), w=[f"wout{c}"], dma=True)
        for c in range(8):
            P.op("dve", lambda e, c=c: e.tensor_tensor(out=wout[:, c, :], in0=wout[:, c, :], in1=g1bc, op=ALU.mult),
                 r=[f"wout{c}", "g1bc"], w=[f"wout{c}"])
        WINH = [f"win{c}" for c in range(8)]
        WOUTH = [f"wout{c}" for c in range(8)]
        SK = _CACHE.get("skip", set())
        for c in range(4 if "diag" not in SK else 0):
            P.op("dve", lambda e, c=c: e.tensor_tensor(
                out=diag[:, c * 31:(c + 1) * 31, :], in0=identb.unsqueeze(1).to_broadcast([128, 31, 128]),
                in1=cols[:, C_CW + c * 31:C_CW + (c + 1) * 31].unsqueeze(2).to_broadcast([128, 31, 128]), op=ALU.mult),
                r=["identb", "cols"], w=["diag"])
        if "memset" not in SK:
            P.op("pool", lambda e: e.memset(vr[0], 0.0), w=["vr0"])
            P.op("pool", lambda e: e.memset(vr[1], 0.0), w=["vr1"])
        for par in range(2 if "memset" not in SK else 0):
            for g in range(2):
                P.op("pool", lambda e, par=par, g=g: e.memset(kr[par][g], 0.0), w=[f"kr{par}"])
            P.op("pool", lambda e, par=par: e.memset(va[par], 0.0), w=[f"va{par}"])
            P.op("dve", lambda e, par=par: e.memset(va[par][:, 1:, 64:65], 1.0), r=[f"va{par}"], w=[f"va{par}"])
            P.op("dve", lambda e, par=par: e.memset(va[par][:, 1:, 129:130], 1.0), r=[f"va{par}"], w=[f"va{par}"])

        if _CACHE.get("stop") == 1:
            P.fence()
            P.op("sp", lambda e: e.dma_start(out=dbg_d[0:128, 0:512], in_=g1bc[:, 0:512]), r=["g1bc"], w=["dbg"], dma=True)
            P.fence()
            P.emit()
            return nc
        nps_s1 = mk_nps([0, 1])
        psb1_f32 = psb[1][:, :].bitcast(F32)
        nps_at = [mk_nps([2, 3]), mk_nps([4, 5])]
        nps_ep = [mk_nps([2, 3]), mk_nps([4, 5])]

        def stage1(mt):
            par = mt % 2
            t0 = mt * 512
            vcur, vprev = vr[par], vr[1 - par]
            hv, hvp = f"vr{par}", f"vr{1 - par}"
            kT, kTp = kr[par], kr[1 - par]
            hk, hkp = f"kr{par}", f"kr{1 - par}"
            vaug, vaugp = va[par], va[1 - par]
            hva, hvap = f"va{par}", f"va{1 - par}"
            qT, hq = qT2[par], f"qT{par}"
            ybf, hy = ybf2[par], f"ybf{par}"
            rstd_sb, hrs = rstd2[par], f"rstd{par}"
            nmr_sb, hnm = nmr2[par], f"nmr{par}"
            nps = nps_s1
            P.rec()
            P.op("pool", lambda e: e.dma_start(out=xt, in_=x_d[t0:t0 + 512, :].rearrange("(s p) d -> p s d", p=128)),
                 w=["xt"], dma=True)
            for c in range(8):
                ptx = psb[0][:, 0:512]
                hp = "psb0"
                for s in range(4):
                    P.op("pe", lambda e, s=s, c=c: e.transpose(
                        out=ptx[:, s * 128:(s + 1) * 128], in_=xt[:, s, c * 128:(c + 1) * 128], identity=identb),
                        r=["xt", "identb"], w=[hp])
                P.op("act", lambda e, c=c: e.activation(
                    out=hT[:, c, :], in_=ptx[:, 0:512], func=AF.Identity, bias=modc2[:, c:c + 1], scale=modc2[:, 8 + c:9 + c]),
                    r=[hp, "modc2"], w=["hT"])
            if mt > 0:
                P.op("pool", lambda e: e.tensor_copy(out=vcur[:, :, 0:30], in_=vprev[:, :, 512:542]), r=[hvp], w=[hv])
                for g in range(2):
                    P.op("pool", lambda e, g=g: e.tensor_copy(out=kT[g][:, 0:128], in_=kTp[g][:, 512:640]), r=[hkp], w=[hk])
                P.op("pool", lambda e: e.tensor_copy(out=vaug[:, 0, :], in_=vaugp[:, 4, :]), r=[hvap], w=[hva])
            for c in range(4):
                pb, hpb = nps()
                for k in range(8):
                    P.op("pe", lambda e, pb=pb, k=k, c=c: e.matmul(
                        pb[:], lhsT=win[:, k, 512 + c * 128:512 + (c + 1) * 128], rhs=hT[:, k, :],
                        start=(k == 0), stop=(k == 7)), r=WINH + ["hT"], w=[hpb])
                P.op("act", lambda e, pb=pb, c=c: e.activation(
                    out=sig, in_=pb[:], func=AF.Sigmoid, bias=cols[:, C_BIN + 4 + c:C_BIN + 5 + c], scale=1.0),
                    r=[hpb, "cols"], w=["sig"])
                pa, hpa = nps()
                for k in range(8):
                    P.op("pe", lambda e, pa=pa, k=k, c=c: e.matmul(
                        pa[:], lhsT=win[:, k, c * 128:(c + 1) * 128], rhs=hT[:, k, :],
                        start=(k == 0), stop=(k == 7)), r=WINH + ["hT"], w=[hpa])
                P.op("dve", lambda e, pa=pa, c=c: e.scalar_tensor_tensor(
                    out=vcur[:, c, 30:542], in0=pa[:], scalar=cols[:, C_BIN + c:C_BIN + c + 1], in1=sig,
                    op0=ALU.add, op1=ALU.mult), r=[hpa, "sig", "cols"], w=[hv])
            for i in range(4):
                pq, hpq = nps()
                for k in range(8):
                    P.op("pe", lambda e, pq=pq, k=k, i=i: e.matmul(
                        pq[:], lhsT=win[:, k, 1024 + i * 128:1024 + (i + 1) * 128], rhs=hT[:, k, :],
                        start=(k == 0), stop=(k == 7)), r=WINH + ["hT"], w=[hpq])
                P.op("dve", lambda e, pq=pq, i=i: e.tensor_scalar(
                    out=qT[:, i, :], in0=pq[:], scalar1=cols[:, C_BIN + 8 + i:C_BIN + 9 + i], scalar2=0.125,
                    op0=ALU.add, op1=ALU.mult), r=[hpq, "cols"], w=[hq])
            pk, hpk = nps()
            for k in range(8):
                P.op("pe", lambda e, k=k: e.matmul(
                    pk[:], lhsT=win[:, k, 1536:1664], rhs=hT[:, k, :], start=(k == 0), stop=(k == 7)),
                    r=WINH + ["hT"], w=[hpk])
            for g in range(2):
                P.op("act", lambda e, g=g: e.activation(
                    out=kT[g][g * 64:(g + 1) * 64, 128:640], in_=pk[g * 64:(g + 1) * 64, :],
                    func=AF.Identity, bias=cols[g * 64:(g + 1) * 64, C_BIN + 12:C_BIN + 13], scale=1.0),
                    r=[hpk, "cols"], w=[hk])
            pv, hpv = nps()
            for s in range(4):
                for k in range(8):
                    P.op("pe", lambda e, s=s, k=k: e.matmul(
                        pv[:, s * 128:(s + 1) * 128], lhsT=hT[:, k, s * 128:(s + 1) * 128], rhs=win[:, k, 1664:1792],
                        start=(k == 0), stop=(k == 7)), r=WINH + ["hT"], w=[hpv])
            for s in range(4):
                blk = s + 1
                P.op("dve", lambda e, s=s, blk=blk: e.tensor_tensor(
                    out=vaug[:, blk, :].rearrange("p (g d) -> p g d", g=2)[:, :, 0:64],
                    in0=pv[:, s * 128:(s + 1) * 128].rearrange("p (g d) -> p g d", g=2),
                    in1=bv_bc.rearrange("p (g d) -> p g d", g=2), op=ALU.add),
                    r=[hpv, "rows_sb"], w=[hva])
            for c in range(4):
                py, hpy = nps()
                for j in range(31):
                    P.op("pe", lambda e, py=py, c=c, j=j: e.matmul(
                        py[:], lhsT=diag[:, c * 31 + j, :], rhs=vcur[:, c, j:j + 512], start=(j == 0), stop=(j == 30)),
                        r=["diag", hv], w=[hpy])
                P.op("act", lambda e, py=py, c=c: e.activation(
                    out=ybf[:, c, :], in_=py[:], func=AF.Identity, bias=cols[:, C_CB + c:C_CB + c + 1], scale=1.0),
                    r=[hpy, "cols"], w=[hy])
                P.op("act", lambda e, py=py, c=c: e.activation(
                    out=y2bf[:, c, :], in_=py[:], func=AF.Square, bias=cols[:, C_CB + c:C_CB + c + 1], scale=1.0),
                    r=[hpy, "cols"], w=["y2bf"])
            pm, hpm = nps()
            for c in range(4):
                P.op("pe", lambda e, c=c: e.matmul(pm[:], lhsT=ones512, rhs=ybf[:, c, :], start=(c == 0), stop=(c == 3)),
                     r=["ones512", hy], w=[hpm])
            pe2, hpe2 = nps()
            for c in range(4):
                P.op("pe", lambda e, c=c: e.matmul(pe2[:], lhsT=ones512, rhs=y2bf[:, c, :], start=(c == 0), stop=(c == 3)),
                     r=["ones512", "y2bf"], w=[hpe2])
            P.op("act", lambda e: e.activation(out=nmr_sb, in_=pm[:], func=AF.Identity), r=[hpm], w=[hnm])
            P.op("dve", lambda e: e.tensor_tensor(out=rstd_sb, in0=nmr_sb, in1=nmr_sb, op=ALU.mult), r=[hnm], w=[hrs])
            P.op("dve", lambda e: e.tensor_tensor(out=rstd_sb, in0=pe2[:], in1=rstd_sb, op=ALU.subtract), r=[hpe2, hrs], w=[hrs])
            P.op("act", lambda e: e.activation(out=rstd_sb, in_=rstd_sb, func=AF.Sqrt, bias=EPSC, scale=1.0), r=[hrs, "epsc"], w=[hrs])
            P.op("dve", lambda e: e.reciprocal(out=rstd_sb, in_=rstd_sb), r=[hrs], w=[hrs])
            P.op("dve", lambda e: e.scalar_tensor_tensor(out=nmr_sb, in0=nmr_sb, scalar=-1.0, in1=rstd_sb, op0=ALU.mult, op1=ALU.mult),
                 r=[hnm, hrs], w=[hnm])
            return P.end()

        def stage2(mt):
            par = mt % 2
            kT_l, vaug_l = kr[par], va[par]
            hk, hva = f"kr{par}", f"va{par}"
            qT, hq = qT2[par], f"qT{par}"
            ybf, hy = ybf2[par], f"ybf{par}"
            rstd_sb, hrs = rstd2[par], f"rstd{par}"
            nmr_sb, hnm = nmr2[par], f"nmr{par}"

            def convln_chain(c):
                k = c % 2
                zc, sc_, s2bf, r2c = zc2[k], sc2_[k], s2bf2[k], r2c2[k]
                P.rec()
                P.op("dve", lambda e: e.tensor_tensor(out=zc, in0=ybf[:, c, :], in1=rstd_sb, op=ALU.mult),
                     r=[hy, hrs], w=[f"zc{k}"])
                P.op("dve", lambda e: e.tensor_tensor(out=zc, in0=zc, in1=nmr_sb, op=ALU.add), r=[f"zc{k}", hnm], w=[f"zc{k}"])
                P.op("act", lambda e: e.activation(out=sc_, in_=zc, func=AF.Silu, bias=cols[:, C_LB + c:C_LB + c + 1],
                                                   scale=cols[:, C_LG + c:C_LG + c + 1]), r=[f"zc{k}", "cols"], w=[f"sc{k}"])
                P.op("act", lambda e: e.activation(out=s2bf, in_=sc_, func=AF.Square), r=[f"sc{k}"], w=[f"s2bf{k}"])
                pr, hpr = psb1_f32, "psb1"
                P.op("pe", lambda e: e.matmul(pr[:], lhsT=bdb, rhs=s2bf, start=True, stop=True), r=["bdb", f"s2bf{k}"], w=[hpr])
                P.op("act", lambda e: e.activation(out=r2c, in_=pr[:], func=AF.Sqrt, bias=EPSC, scale=1.0), r=[hpr, "epsc"], w=[f"r2c{k}"])
                P.op("dve", lambda e: e.reciprocal(out=r2c, in_=r2c), r=[f"r2c{k}"], w=[f"r2c{k}"])
                P.op("dve", lambda e: e.scalar_tensor_tensor(out=ycT[:, c, :], in0=sc_, scalar=cols[:, C_OG + c:C_OG + c + 1],
                                                             in1=r2c, op0=ALU.mult, op1=ALU.mult),
                     r=[f"sc{k}", f"r2c{k}", "cols"], w=[f"ycTc{c}"])
                return P.end()

            def attn_chain(s):
                k = s % 2
                mx, mxb, nmx, dcat, ET = mx2[k], mxb2[k], nmx2[k], dcat2[k], ET2[k]
                es_t, den, osb, osq, ssq, yat = es_t2[k], den2[k], osb2[k], osq2[k], ssq2[k], yat2[k]
                mynps = nps_at[k]
                n = mt * 4 + s
                qs = slice(s * 128, (s + 1) * 128)
                P.rec()
                for i in range(4):
                    pS, hpS = mynps()
                    for g in range(2):
                        P.op("pe", lambda e, pS=pS, i=i, g=g: e.matmul(
                            pS[:, g * 256:(g + 1) * 256], lhsT=qT[:, i, qs], rhs=kT_l[g][:, s * 128:s * 128 + 256],
                            start=True, stop=True), r=[hq, hk], w=[hpS])
                    P.op("dve", lambda e, pS=pS, i=i: e.tensor_reduce(
                        out=mx[:, 2 * i:2 * i + 2], in_=pS[:].rearrange("p (g k) -> p g k", g=2), axis=AX.X, op=ALU.max),
                        r=[hpS], w=[f"mx{k}"])
                P.op("dve", lambda e: e.tensor_copy(out=mxb, in_=mx), r=[f"mx{k}"], w=[f"mxb{k}"])
                P.op("dve", lambda e: e.tensor_scalar(out=nmx, in0=mxb, scalar1=-1.0, scalar2=None, op0=ALU.mult), r=[f"mxb{k}"], w=[f"nmx{k}"])
                for g in range(2):
                    P.op("dve", lambda e, g=g: e.tensor_tensor(
                        out=dcat[:, g, :].rearrange("p (i q) -> p i q", i=4), in0=identb.unsqueeze(1).to_broadcast([128, 4, 128]),
                        in1=nmx.rearrange("p (i g) -> p i g", g=2)[:, :, g:g + 1].to_broadcast([128, 4, 128]), op=ALU.mult),
                        r=["identb", f"nmx{k}"], w=[f"dcat{k}_{g}"])
                khs = [1] if n == 0 else [0, 1]
                for g in range(2):
                    for kh in khs:
                        pT, hpT = mynps()
                        kc = slice(s * 128 + kh * 128, s * 128 + kh * 128 + 128)
                        P.op("pe", lambda e, pT=pT, g=g, kc=kc: e.matmul(
                            pT[:].rearrange("p (i q) -> p i q", i=4), lhsT=kT_l[g][:, kc], rhs=qT[:, :, qs], start=True, stop=False),
                            r=[hk, hq], w=[hpT])
                        P.op("pe", lambda e, pT=pT, g=g: e.matmul(pT[:], lhsT=onesb, rhs=dcat[:, g, :], start=False, stop=False),
                             r=["onesb", f"dcat{k}_{g}"], w=[hpT])
                        P.op("pe", lambda e, pT=pT, kh=kh: e.matmul(pT[:], lhsT=identb, rhs=maskT[:, kh, :], start=False, stop=True),
                             r=["identb", "maskT"], w=[hpT])
                        P.op("act", lambda e, pT=pT, g=g, kh=kh: e.activation(out=ET[:, g * 2 + kh, :], in_=pT[:], func=AF.Exp),
                             r=[hpT], w=[f"ET{k}_{g}{kh}"])
                P.op("dve", lambda e: e.tensor_tensor(out=es_t, in0=sink_bc, in1=nmx, op=ALU.add), r=["rows_sb", f"nmx{k}"], w=[f"es_t{k}"])
                P.op("act", lambda e: e.activation(out=es_t, in_=es_t, func=AF.Exp), r=[f"es_t{k}"], w=[f"es_t{k}"])
                for g in range(2):
                    po, hpo = mynps()
                    for i in range(4):
                        for kh in khs:
                            P.op("pe", lambda e, po=po, g=g, i=i, kh=kh: e.matmul(
                                po[:, i * 65:(i + 1) * 65], lhsT=ET[:, g * 2 + kh, i * 128:(i + 1) * 128],
                                rhs=vaug_l[:, s + kh, g * 65:(g + 1) * 65], start=(kh == khs[0]), stop=(kh == 1)),
                                r=[f"ET{k}_{g}{kh}", hva], w=[hpo])
                    P.op("dve", lambda e, po=po, g=g: e.tensor_tensor(
                        out=den.rearrange("p (i g) -> p i g", g=2)[:, :, g:g + 1],
                        in0=po[:, 0:260].rearrange("p (i d) -> p i d", d=65)[:, :, 64:65],
                        in1=es_t.rearrange("p (i g) -> p i g", g=2)[:, :, g:g + 1], op=ALU.add),
                        r=[hpo, f"es_t{k}"], w=[f"den{k}_{g}"])
                    P.op("act", lambda e, po=po, g=g: e.activation(
                        out=osb.rearrange("p (i g) d -> p i g d", g=2)[:, :, g, :],
                        in_=po[:, 0:260].rearrange("p (i d) -> p i d", d=65)[:, :, 0:64], func=AF.Identity),
                        r=[hpo], w=[f"osb{k}_{g}"])
                DH = [f"den{k}_0", f"den{k}_1"]
                OH = [f"osb{k}_0", f"osb{k}_1"]
                P.op("dve", lambda e: e.reciprocal(out=den, in_=den), r=DH, w=DH)
                P.op("dve", lambda e: e.tensor_tensor(out=osb, in0=osb, in1=den.unsqueeze(2).to_broadcast([128, 8, 64]), op=ALU.mult),
                     r=OH + DH, w=OH)
                P.op("act", lambda e: e.activation(out=osq, in_=osb, func=AF.Square), r=OH, w=[f"osq{k}"])
                P.op("dve", lambda e: e.tensor_reduce(out=ssq, in_=osq, axis=AX.X, op=ALU.add), r=[f"osq{k}"], w=[f"ssq{k}"])
                P.op("act", lambda e: e.activation(out=ssq, in_=ssq, func=AF.Sqrt, bias=EPSC, scale=1.0 / 64.0), r=[f"ssq{k}", "epsc"], w=[f"ssq{k}"])
                P.op("dve", lambda e: e.reciprocal(out=ssq, in_=ssq), r=[f"ssq{k}"], w=[f"ssq{k}"])
                P.op("dve", lambda e: e.tensor_tensor(out=osb, in0=osb, in1=ssq.unsqueeze(2).to_broadcast([128, 8, 64]), op=ALU.mult),
                     r=OH + [f"ssq{k}"], w=OH)
                P.op("dve", lambda e: e.tensor_tensor(out=yat, in0=osb.rearrange("p h d -> p (h d)"), in1=aog_bc, op=ALU.mult),
                     r=OH + ["rows_sb"], w=[f"yat{k}"])
                ptb = ps[3 + 2 * k][:, :].bitcast(BF16)[:, 0:512]
                hptr = f"ps{3 + 2 * k}"
                for i in range(4):
                    P.op("pe", lambda e, i=i: e.transpose(out=ptb[:, i * 128:(i + 1) * 128], in_=yat[:, i * 128:(i + 1) * 128],
                                                         identity=identb), r=[f"yat{k}", "identb"], w=[hptr])
                P.op("act", lambda e: e.activation(
                    out=ycT[:, 4:8, qs], in_=ptb[:, 0:512].rearrange("p (i q) -> p i q", i=4), func=AF.Identity),
                    r=[hptr], w=[f"ycTa{s}"])
                return P.end()

            def xr_load(s):
                k = s % 2
                tt = mt * 4 + s
                P.op("sp", lambda e: e.dma_start(out=xr2[k], in_=x_d[tt * 128:(tt + 1) * 128, :]), w=[f"xr{k}"], dma=True)

            def epi_chain(s, with_load):
                k = s % 2
                tt = mt * 4 + s
                xr, bnst, bnag = xr2[k], bnst2[k], bnag2[k]
                rr = xr
                mynps = nps_ep[k]
                YH = [f"ycTc{c}" for c in range(4)] + [f"ycTa{s}"]
                P.rec()
                if with_load:
                    xr_load(s)
                for h in range(2):
                    po, hpo = mynps()
                    for c in range(8):
                        P.op("pe", lambda e, po=po, c=c, h=h: e.matmul(
                            po[:], lhsT=ycT[:, c, s * 128:(s + 1) * 128], rhs=wout[:, c, h * 512:(h + 1) * 512],
                            start=(c == 0), stop=False), r=YH + WOUTH, w=[hpo])
                    P.op("pe", lambda e, po=po, h=h: e.matmul(
                        po[:], lhsT=onesb[0:1, :], rhs=gbrow[0:1, h * 512:(h + 1) * 512], start=False, stop=True),
                        r=["onesb", "gbrow"], w=[hpo])
                    P.op("dve", lambda e, po=po, h=h: e.tensor_tensor(out=rr[:, h * 512:(h + 1) * 512], in0=po[:],
                                                                      in1=xr[:, h * 512:(h + 1) * 512], op=ALU.add),
                         r=[hpo, f"xr{k}"], w=[f"xr{k}"])
                for h in range(2):
                    P.op("dve", lambda e, h=h: e.bn_stats(out=bnst[:, h * 6:(h + 1) * 6], in_=rr[:, h * 512:(h + 1) * 512]),
                         r=[f"xr{k}"], w=[f"bnst{k}"])
                P.op("dve", lambda e: e.bn_aggr(out=bnag[:, 0:2], in_=bnst), r=[f"bnst{k}"], w=[f"bnag{k}"])
                P.op("act", lambda e: e.activation(out=bnag[:, 2:3], in_=bnag[:, 1:2], func=AF.Sqrt, bias=EPSC2, scale=1.0),
                     r=[f"bnag{k}", "epsc2"], w=[f"bnagb{k}"])
                P.op("dve", lambda e: e.reciprocal(out=bnag[:, 2:3], in_=bnag[:, 2:3]), r=[f"bnagb{k}"], w=[f"bnagb{k}"])
                P.op("dve", lambda e: e.scalar_tensor_tensor(out=bnag[:, 3:4], in0=bnag[:, 0:1], scalar=-1.0, in1=bnag[:, 2:3],
                                                             op0=ALU.mult, op1=ALU.mult), r=[f"bnag{k}", f"bnagb{k}"], w=[f"bnagc{k}"])
                P.op("act", lambda e: e.activation(out=rr, in_=rr, func=AF.Identity, bias=bnag[:, 3:4], scale=bnag[:, 2:3]),
                     r=[f"xr{k}", f"bnagb{k}", f"bnagc{k}"], w=[f"xr{k}"])
                P.op("sp", lambda e: e.dma_start(out=z_d[tt * 128:(tt + 1) * 128, :], in_=rr), r=[f"xr{k}"], w=[f"z_d{tt}"], dma=True)
                return P.end()


            out = []
            P.rec()
            xr_load(0)
            xr_load(1)
            out += P.end()
            out += merge(convln_chain(0) + convln_chain(1), attn_chain(0), attn_chain(1))
            out += merge(convln_chain(2) + convln_chain(3), attn_chain(2), attn_chain(3))
            out += merge(epi_chain(0, False), epi_chain(1, False))
            out += merge(epi_chain(2, True), epi_chain(3, True))
            return out

        NZ = NS * T // ZR
        assert NZ * ZR == NS * T

        def zero_fill(lo, hi):
            P.rec()
            for n in range(lo, hi):
                P.op("sp", lambda e, n=n: e.dma_start(out=xs_d[n * ZR:(n + 1) * ZR, :], in_=zeros_d[:, :]), w=[f"xs_z{n}"], dma=True)
            return P.end()

        P.play(stage1(0))
        for mt in range(NMT):
            nxt = stage1(mt + 1) if mt + 1 < NMT else []
            zl = zero_fill(mt * NZ // NMT, (mt + 1) * NZ // NMT)
            P.play(merge(stage2(mt), nxt, zl))

        P.fence()
        if dbg and dbg["what"] == "z":
            nr = _CACHE.get('nmt_run', NMT) * 512
            P.op("sp", lambda e: e.dma_start(out=dbg_d[0:nr, :], in_=z_d[0:nr, :]), r=[f"z_d{i}" for i in range(nr // 128)], w=["dbg"], dma=True)
            P.fence()
            P.emit()
            return nc

        try:
            build_phase2(nc, P, A, ps, nps, dr, out_d, z_d, xs_d, ys_d, dbg, dbg_d, mark_persist,
                     dict(cols=cols, identf=identf, identb=identb, onesb=onesb, Ub=Ub, modc=modc, mod_d=mod_d,
                              EPSC=EPSC, psb=psb))
        except _StopEmit:
            pass
        P.fence()
        P.emit()
    return nc


def build_phase2(nc, P, A, ps, nps, dr, out_d, z_d, xs_d, ys_d, dbg, dbg_d, mark, K):
    cols, identf, identb, onesb, Ub, mod_d, EPSC, psb = (K[k] for k in ("cols", "identf", "identb", "onesb", "Ub", "mod_d", "EPSC", "psb"))
    rows_d, wr_d, wg_d, wu_d, wd_d = dr["rows"], dr["w_r"], dr["wg"], dr["wu"], dr["wd"]
    A.off = mark
    gbc = A.alloc([4, D], F32)
    P.op("sp", lambda e: e.dma_start(out=gbc[:, 1:4, :].rearrange("p a d -> p (a d)"), in_=mod_d[0:1, :].partition_broadcast(128)),
         r=["mod_d"], w=["gbc1_0", "gbc1_1", "gbc2_0", "gbc2_1", "gbc3_0", "gbc3_1"], dma=True)
    lnr = A.alloc([4, D], F32)
    misc = A.alloc([36 + 64], F32)
    A2 = A.alloc([D], F32)
    B2 = A.alloc([D], F32)
    GA = A.alloc([D], F32)
    BA = A.alloc([D], F32)
    wr = A.alloc([8, 36], F32)
    pos_i = A.alloc([2, NT], I32)
    wts = A.alloc([2, NT], F32)
    widx_i = A.alloc([64], I32)
    yidx_i = A.alloc([NSUB, NS], I32)
    mark2 = A.off
    br_bc = misc[:, 0:36]
    slot_bc = misc[:, 36:36 + NS]
    P.op("sp", lambda e: e.dma_start(out=lnr.rearrange("p a d -> p (a d)"), in_=rows_d[0:1, R_L1G:R_L1G + 4 * D].partition_broadcast(128)),
         w=["lnr"], dma=True)
    P.op("sp", lambda e: e.dma_start(out=misc, in_=rows_d[0:1, R_BR:R_BR + 100].partition_broadcast(128)), w=["misc"], dma=True)
    P.op("sp", lambda e: e.dma_start(out=wr, in_=wr_d.rearrange("(c p) n -> p c n", p=128)), w=["wr"], dma=True)
    G2H = ["gbc1_0", "gbc1_1", "gbc2_0", "gbc2_1", "gbc3_0", "gbc3_1"]
    P.op("dve", lambda e: e.scalar_tensor_tensor(out=A2, in0=gbc[:, 2, :], scalar=1.0, in1=lnr[:, 0, :], op0=ALU.add, op1=ALU.mult),
         r=["lnr"] + G2H, w=["A2"])
    P.op("dve", lambda e: e.scalar_tensor_tensor(out=B2, in0=gbc[:, 2, :], scalar=1.0, in1=lnr[:, 1, :], op0=ALU.add, op1=ALU.mult),
         r=["lnr"] + G2H, w=["B2"])
    P.op("dve", lambda e: e.tensor_tensor(out=B2, in0=B2, in1=gbc[:, 1, :], op=ALU.add), r=["B2"] + G2H, w=["B2"])
    P.op("dve", lambda e: e.tensor_scalar(out=GA, in0=lnr[:, 0, :], scalar1=ALPHA, scalar2=None, op0=ALU.mult), r=["lnr"], w=["GA"])
    P.op("dve", lambda e: e.tensor_scalar(out=BA, in0=lnr[:, 1, :], scalar1=ALPHA, scalar2=None, op0=ALU.mult), r=["lnr"], w=["BA"])

    def mk_nps(ids):
        st_ = [0]

        def f():
            i = ids[st_[0] % len(ids)]
            st_[0] += 1
            return ps[i], f"ps{i}"
        return f

    t1_d = nc.dram_tensor("t1_scr", [S, D], F32, kind="Internal").ap()
    h2b = A.alloc([NT, D], BF16)
    logits = A.alloc([NT, 36], F32)
    mark2a = A.off
    NB2A = 4
    zt = [A.alloc([D], F32) for _ in range(NB2A)]
    h2f = [A.alloc([D], F32) for _ in range(NB2A)]
    h2T = [A.alloc([8, 128], F32) for _ in range(NB2A)]
    t1s = [A.alloc([D], F32) for _ in range(NB2A)]
    nps2a_f = [mk_nps([0, 1]), mk_nps([2, 3])]
    nps2a_b = [mk_nps([4]), mk_nps([5])]

    def front_ew(j):
        b = j % NB2A
        z_, hz = zt[b], f"zt{b}"
        hf, hhf = h2f[b], f"h2f{b}"
        t1_, ht1 = t1s[b], f"t1s{b}"
        P.rec()
        P.op("dve", lambda e: e.tensor_tensor(out=hf, in0=z_, in1=A2, op=ALU.mult), r=[hz, "A2"], w=[hhf])
        P.op("dve", lambda e: e.tensor_tensor(out=hf, in0=hf, in1=B2, op=ALU.add), r=[hhf, "B2"], w=[hhf])
        P.op("pool", lambda e: e.tensor_tensor(out=t1_, in0=z_, in1=GA, op=ALU.mult), r=[hz, "GA"], w=[ht1])
        P.op("dve", lambda e: e.tensor_tensor(out=t1_, in0=t1_, in1=BA, op=ALU.add), r=[ht1, "BA"], w=[ht1])
        P.op("sp", lambda e: e.dma_start(out=t1_d[j * 128:(j + 1) * 128, :], in_=t1_), r=[ht1], w=[f"t1_d{j}"], dma=True)
        P.op("act", lambda e: e.activation(out=h2b[:, j, :], in_=hf, func=AF.Identity), r=[hhf], w=[f"h2b{j}"])
        return P.end()

    def zload(j):
        b = j % NB2A
        P.op("sp", lambda e: e.dma_start(out=zt[b], in_=z_d[j * 128:(j + 1) * 128, :]), r=[f"z_d{j}"], w=[f"zt{b}"], dma=True)

    def front_tr(j):
        b = j % NB2A
        mynps = nps2a_f[j % 2]
        hf, hhf = h2f[b], f"h2f{b}"
        hT_, hhT = h2T[b], f"h2T{b}"
        P.rec()
        for hh in range(2):
            pt, hp = mynps()
            for c4 in range(4):
                c = hh * 4 + c4
                P.op("pe", lambda e, pt=pt, c=c, c4=c4: e.transpose(out=pt[:, c4 * 128:(c4 + 1) * 128],
                                                                  in_=hf[:, c * 128:(c + 1) * 128], identity=identf),
                     r=[hhf, "cst"], w=[hp])
            P.op("act", lambda e, pt=pt, hh=hh: e.activation(out=hT_[:, hh * 4:(hh + 1) * 4, :].rearrange("p c t -> p (c t)"),
                                                             in_=pt[:], func=AF.Identity), r=[hp], w=[hhT + "ab"[hh]])
        return P.end()

    def back2a(j):
        b = j % NB2A
        hT_, hhT = h2T[b], f"h2T{b}"
        P.rec()
        pl, hpl = nps2a_b[j % 2]()
        for c in range(8):
            P.op("pe", lambda e, c=c: e.matmul(pl[:, 0:36], lhsT=hT_[:, c, :], rhs=wr[:, c, :], start=(c == 0), stop=(c == 7)),
                 r=[hhT + "a", hhT + "b", "wr"], w=[hpl])
        P.op("dve", lambda e: e.tensor_tensor(out=logits[:, j, :], in0=pl[:, 0:36], in1=br_bc, op=ALU.add),
             r=[hpl, "misc"], w=["logits"])
        return P.end()

    NP2A = NT // 2
    for j in range(4):
        zload(j)
    P.play(front_ew(0) + front_ew(1) + merge(front_tr(0), front_tr(1)))
    for k in range(NP2A):
        if k + 2 < NP2A:
            zload(2 * k + 4)
            zload(2 * k + 5)
        blk = []
        if k + 1 < NP2A:
            blk += merge(front_ew(2 * k + 2), front_ew(2 * k + 3))
        bk = merge(back2a(2 * k), back2a(2 * k + 1))
        tail = [o for o in bk if o[0] == "dve"]
        blk += [o for o in bk if o[0] != "dve"]
        if k + 1 < NP2A:
            blk += merge(front_tr(2 * k + 2), front_tr(2 * k + 3))
        blk += tail
        P.play(blk)
    P.fence()
    A.off = mark2a

    def T3(n):
        return A.alloc([NT, n], F32)
    gmax = A.alloc([NT], F32)
    og = T3(4)
    eg = T3(4)
    sgm = A.alloc([NT], F32)
    ptop = A.alloc([NT], F32)
    tmp4 = A.alloc([NT, 4, 8], F32)
    sel = T3(8)
    sel2 = T3(8)
    m1 = A.alloc([NT], F32)
    m2 = A.alloc([NT], F32)
    o1 = T3(8)
    o2 = T3(8)
    e2 = A.alloc([NT], F32)
    r12 = A.alloc([NT], F32)
    O1 = A.alloc([NT, 4, 8], F32)
    O2 = A.alloc([NT, 4, 8], F32)
    Obf = A.alloc([NT * 32], BF16)
    totA = A.alloc([NT, 32], F32)
    totB = A.alloc([NT, 32], F32)
    tot0 = A.alloc([NT, 32], F32)
    base = A.alloc([NT, 32], F32)
    cnt = A.alloc([32], F32)
    cmpc = A.alloc([32, 24], F32)
    pcnt = A.alloc([32], F32)
    oeA = A.alloc([32], F32)
    oeB = A.alloc([32], F32)
    offs = A.alloc([32], F32)
    cmps = A.alloc([NS, 32], F32)
    esl = A.alloc([64], F32)
    used = A.alloc([64], F32)
    posf = A.alloc([2, NT], F32)

    LG = logits[:, :, 0:4]
    LE4 = logits[:, :, 4:36].rearrange("p j (g e) -> p j g e", g=4)

    def dv(fn, r, w):
        P.op("dve", fn, r=r, w=w)

    dv(lambda e: e.tensor_reduce(out=gmax, in_=LG, axis=AX.X, op=ALU.max), ["logits"], ["gmax"])
    dv(lambda e: e.tensor_tensor(out=og, in0=LG, in1=gmax.unsqueeze(2).to_broadcast([128, NT, 4]), op=ALU.is_equal), ["logits", "gmax"], ["og"])
    dv(lambda e: e.tensor_tensor(out=eg, in0=LG, in1=gmax.unsqueeze(2).to_broadcast([128, NT, 4]), op=ALU.subtract), ["logits", "gmax"], ["eg"])
    P.op("act", lambda e: e.activation(out=eg, in_=eg, func=AF.Exp), r=["eg"], w=["eg"])
    dv(lambda e: e.tensor_reduce(out=sgm, in_=eg, axis=AX.X, op=ALU.add), ["eg"], ["sgm"])
    dv(lambda e: e.reciprocal(out=ptop, in_=sgm), ["sgm"], ["ptop"])
    dv(lambda e: e.tensor_tensor(out=tmp4, in0=LE4, in1=og.unsqueeze(3).to_broadcast([128, NT, 4, 8]), op=ALU.mult), ["logits", "og"], ["tmp4"])
    dv(lambda e: e.tensor_reduce(out=sel, in_=tmp4.rearrange("p j g e -> p j e g"), axis=AX.X, op=ALU.add), ["tmp4"], ["sel"])
    dv(lambda e: e.tensor_reduce(out=m1, in_=sel, axis=AX.X, op=ALU.max), ["sel"], ["m1"])
    dv(lambda e: e.tensor_tensor(out=o1, in0=sel, in1=m1.unsqueeze(2).to_broadcast([128, NT, 8]), op=ALU.is_equal), ["sel", "m1"], ["o1"])
    dv(lambda e: e.scalar_tensor_tensor(out=sel2.rearrange("p j e -> p (j e)"), in0=o1.rearrange("p j e -> p (j e)"), scalar=-1.0e9,
                                        in1=sel.rearrange("p j e -> p (j e)"), op0=ALU.mult, op1=ALU.add), ["o1", "sel"], ["sel2"])
    dv(lambda e: e.tensor_reduce(out=m2, in_=sel2, axis=AX.X, op=ALU.max), ["sel2"], ["m2"])
    dv(lambda e: e.tensor_tensor(out=o2, in0=sel2, in1=m2.unsqueeze(2).to_broadcast([128, NT, 8]), op=ALU.is_equal), ["sel2", "m2"], ["o2"])
    dv(lambda e: e.tensor_tensor(out=e2, in0=m2, in1=m1, op=ALU.subtract), ["m1", "m2"], ["e2"])
    P.op("act", lambda e: e.activation(out=e2, in_=e2, func=AF.Exp), r=["e2"], w=["e2"])
    dv(lambda e: e.tensor_scalar(out=r12, in0=e2, scalar1=1.0, scalar2=None, op0=ALU.add), ["e2"], ["r12"])
    dv(lambda e: e.reciprocal(out=r12, in_=r12), ["r12"], ["r12"])
    dv(lambda e: e.tensor_tensor(out=wts[:, 0, :], in0=r12, in1=ptop, op=ALU.mult), ["r12", "ptop"], ["wts"])
    dv(lambda e: e.tensor_tensor(out=wts[:, 1, :], in0=wts[:, 0, :], in1=e2, op=ALU.mult), ["wts", "e2"], ["wts"])
    dv(lambda e: e.tensor_tensor(out=O1, in0=og.unsqueeze(3).to_broadcast([128, NT, 4, 8]),
                                 in1=o1.unsqueeze(2).to_broadcast([128, NT, 4, 8]), op=ALU.mult), ["og", "o1"], ["O1"])
    dv(lambda e: e.tensor_tensor(out=O2, in0=og.unsqueeze(3).to_broadcast([128, NT, 4, 8]),
                                 in1=o2.unsqueeze(2).to_broadcast([128, NT, 4, 8]), op=ALU.mult), ["og", "o2"], ["O2"])
    O1f = O1.rearrange("p j g e -> p (j g e)")
    O2f = O2.rearrange("p j g e -> p (j g e)")
    dv(lambda e: e.tensor_tensor(out=Obf, in0=O1f, in1=O2f, op=ALU.add), ["O1", "O2"], ["Obf"])
    pcs, pts = [], []
    for h in range(2):
        pc_, hpc = nps()
        P.op("pe", lambda e, pc_=pc_, h=h: e.matmul(pc_[:], lhsT=Ub, rhs=Obf[:, h * 512:(h + 1) * 512], start=True, stop=True),
             r=["Ub", "Obf"], w=[hpc])
        pcs.append((pc_, hpc))
        pt_, hpt = nps()
        P.op("pe", lambda e, pt_=pt_, h=h: e.matmul(pt_[:], lhsT=onesb, rhs=Obf[:, h * 512:(h + 1) * 512], start=True, stop=True),
             r=["onesb", "Obf"], w=[hpt])
        pts.append((pt_, hpt))
    tot0f = tot0.rearrange("p j e -> p (j e)")
    for h in range(2):
        dv(lambda e, h=h: e.tensor_copy(out=tot0f[:, h * 512:(h + 1) * 512], in_=pts[h][0][:]), [pts[h][1]], ["tot0"])
    cur, hc = tot0, "tot0"
    for i_, sft in enumerate((1, 2, 4, 8, 16)):
        nxt, hn = (totA, "totA") if i_ % 2 == 0 else (totB, "totB")
        dv(lambda e, cur=cur, nxt=nxt, sft=sft: e.tensor_tensor(out=nxt[:, sft:, :], in0=cur[:, sft:, :], in1=cur[:, :NT - sft, :], op=ALU.add),
           [hc], [hn])
        dv(lambda e, cur=cur, nxt=nxt, sft=sft: e.tensor_copy(out=nxt[:, :sft, :], in_=cur[:, :sft, :]), [hc, hn], [hn])
        cur, hc = nxt, hn
    incl, hincl = cur, hc
    dv(lambda e: e.tensor_copy(out=cnt, in_=incl[:, NT - 1, :]), [hincl], ["cnt"])
    dv(lambda e: e.tensor_tensor(out=cmpc, in0=cnt.unsqueeze(2).to_broadcast([128, 32, 24]),
                                 in1=slot_bc[:, 0:24].unsqueeze(1).to_broadcast([128, 32, 24]), op=ALU.is_gt), ["cnt", "misc"], ["cmpc"])
    dv(lambda e: e.tensor_reduce(out=pcnt, in_=cmpc, axis=AX.X, op=ALU.add), ["cmpc"], ["pcnt"])
    dv(lambda e: e.tensor_scalar(out=pcnt, in0=pcnt, scalar1=float(T), scalar2=None, op0=ALU.mult), ["pcnt"], ["pcnt"])
    cur, hc = pcnt, "pcnt"
    for i_, sft in enumerate((1, 2, 4, 8, 16)):
        nxt, hn = (oeA, "oeA") if i_ % 2 == 0 else (oeB, "oeB")
        dv(lambda e, cur=cur, nxt=nxt, sft=sft: e.tensor_tensor(out=nxt[:, sft:], in0=cur[:, sft:], in1=cur[:, :32 - sft], op=ALU.add), [hc], [hn])
        dv(lambda e, cur=cur, nxt=nxt, sft=sft: e.tensor_copy(out=nxt[:, :sft], in_=cur[:, :sft]), [hc, hn], [hn])
        cur, hc = nxt, hn
    oend, hoend = cur, hc
    dv(lambda e: e.tensor_tensor(out=offs, in0=oend, in1=pcnt, op=ALU.subtract), [hoend, "pcnt"], ["offs"])
    dv(lambda e: e.tensor_tensor(out=base, in0=incl, in1=tot0, op=ALU.subtract), [hincl, "tot0"], ["base"])
    dv(lambda e: e.tensor_tensor(out=base, in0=base, in1=offs.unsqueeze(1).to_broadcast([128, NT, 32]), op=ALU.add), ["base", "offs"], ["base"])
    basef = base.rearrange("p j e -> p (j e)")
    for h in range(2):
        dv(lambda e, h=h: e.tensor_tensor(out=basef[:, h * 512:(h + 1) * 512], in0=pcs[h][0][:], in1=basef[:, h * 512:(h + 1) * 512], op=ALU.add),
           [pcs[h][1], "base"], ["base"])
    for k, (Ok, hO) in enumerate(((O1, "O1"), (O2, "O2"))):
        dv(lambda e, Ok=Ok: e.tensor_tensor(out=Ok.rearrange("p j g e -> p (j g e)"), in0=Ok.rearrange("p j g e -> p (j g e)"), in1=basef, op=ALU.mult),
           [hO, "base"], [hO])
        dv(lambda e, Ok=Ok, k=k: e.tensor_reduce(out=posf[:, k, :], in_=Ok.rearrange("p j g e -> p j (g e)"), axis=AX.X, op=ALU.add), [hO], ["posf"])
    dv(lambda e: e.tensor_copy(out=pos_i, in_=posf), ["posf"], ["pos_i"])
    dv(lambda e: e.tensor_tensor(out=cmps, in0=oend.unsqueeze(1).to_broadcast([128, NS, 32]),
                                 in1=slot_bc.unsqueeze(2).to_broadcast([128, NS, 32]), op=ALU.is_le), [hoend, "misc"], ["cmps"])
    dv(lambda e: e.tensor_reduce(out=esl[:, 0:NS], in_=cmps, axis=AX.X, op=ALU.add), ["cmps"], ["esl"])
    dv(lambda e: e.tensor_scalar(out=esl[:, 0:NS], in0=esl[:, 0:NS], scalar1=float(NE - 1), scalar2=128.0, op0=ALU.min, op1=ALU.mult), ["esl"], ["esl"])
    dv(lambda e: e.tensor_scalar(out=used[:, 0:NS], in0=slot_bc, scalar1=oend[:, 31:32], scalar2=None, op0=ALU.is_lt), ["misc", hoend], ["used"])
    dv(lambda e: e.tensor_scalar(out=used[:, 0:NS], in0=used[:, 0:NS], scalar1=-1.0e6, scalar2=1.0e6, op0=ALU.mult, op1=ALU.add), ["used"], ["used"])
    dv(lambda e: e.tensor_tensor(out=esl[:, 0:NS], in0=esl[:, 0:NS], in1=used[:, 0:NS], op=ALU.add), ["esl", "used"], ["esl"])
    dv(lambda e: e.tensor_scalar(out=esl[:, 0:NS], in0=esl[:, 0:NS], scalar1=cols[:, C_PID:C_PID + 1], scalar2=None, op0=ALU.add), ["esl", "cols"], ["esl"])
    dv(lambda e: e.tensor_copy(out=widx_i[:, 0:NS], in_=esl[:, 0:NS]), ["esl"], ["widx_i"])
    oh = A.alloc([NS, 32], F32)
    endv = A.alloc([32], F32)
    lim = A.alloc([64], F32)
    rrow = A.alloc([64], F32)
    pen = A.alloc([64], F32)
    yidx_f = A.alloc([NSUB, NS], F32)
    dv(lambda e: e.tensor_tensor(out=endv, in0=offs, in1=cnt, op=ALU.add), ["offs", "cnt"], ["endv"])
    dv(lambda e: e.tensor_tensor(out=oh[:, :, 1:32], in0=cmps[:, :, 0:31], in1=cmps[:, :, 1:32], op=ALU.subtract), ["cmps"], ["oh"])
    dv(lambda e: e.tensor_scalar(out=oh[:, :, 0:1], in0=cmps[:, :, 0:1], scalar1=-1.0, scalar2=1.0, op0=ALU.mult, op1=ALU.add), ["cmps", "oh"], ["oh"])
    dv(lambda e: e.tensor_tensor(out=oh, in0=oh, in1=endv.unsqueeze(1).to_broadcast([128, NS, 32]), op=ALU.mult), ["oh", "endv"], ["oh"])
    dv(lambda e: e.tensor_reduce(out=lim[:, 0:NS], in_=oh, axis=AX.X, op=ALU.add), ["oh"], ["lim"])
    for st in range(NSUB):
        dv(lambda e, st=st: e.tensor_scalar(out=rrow[:, 0:NS], in0=slot_bc, scalar1=cols[:, C_PID:C_PID + 1], scalar2=float(st * 128),
                                            op0=ALU.add, op1=ALU.add), ["misc", "cols", "rrow"], ["rrow"])
        dv(lambda e: e.tensor_tensor(out=pen[:, 0:NS], in0=rrow[:, 0:NS], in1=lim[:, 0:NS], op=ALU.is_lt), ["rrow", "lim", "pen"], ["pen"])
        dv(lambda e: e.tensor_scalar(out=pen[:, 0:NS], in0=pen[:, 0:NS], scalar1=-1.0e6, scalar2=1.0e6, op0=ALU.mult, op1=ALU.add), ["pen"], ["pen"])
        dv(lambda e, st=st: e.tensor_tensor(out=yidx_f[:, st, :], in0=rrow[:, 0:NS], in1=pen[:, 0:NS], op=ALU.add), ["rrow", "pen"], ["yidx_f"])
    dv(lambda e: e.tensor_copy(out=yidx_i, in_=yidx_f), ["yidx_f"], ["yidx_i"])

    if dbg and dbg["what"] == "route":
        P.fence()
        P.op("sp", lambda e: e.dma_start(out=dbg_d[0:128, 0:NT * 36], in_=logits.rearrange("p j n -> p (j n)")), r=["logits"], w=["dbg0"], dma=True)
        P.op("sp", lambda e: e.dma_start(out=dbg_d[128:256, 0:2 * NT], in_=posf.rearrange("p k j -> p (k j)")), r=["posf"], w=["dbg1"], dma=True)
        P.op("sp", lambda e: e.dma_start(out=dbg_d[256:384, 0:2 * NT], in_=wts.rearrange("p k j -> p (k j)")), r=["wts"], w=["dbg2"], dma=True)
        P.op("sp", lambda e: e.dma_start(out=dbg_d[384:512, 0:NS], in_=esl[:, 0:NS]), r=["esl"], w=["dbg3"], dma=True)
        P.fence()
        raise _StopEmit()

    for j in range(NT):
        for k in range(2):
            P.op("pool", lambda e, j=j, k=k: e.indirect_dma_start(
                out=xs_d[:, :], out_offset=bass.IndirectOffsetOnAxis(ap=pos_i[:, k, j:j + 1], axis=0),
                in_=h2b[:, j, :], in_offset=None), r=[f"h2b{j}", "pos_i"], w=[f"xs_{j}_{k}"], dma=True)
    P.fence()

    A.off = mark2
    NBW = 4
    wgs = [A.alloc([2048], BF16) for _ in range(NBW)]
    wus = [A.alloc([2048], BF16) for _ in range(NBW)]
    wds = [A.alloc([2048], BF16) for _ in range(NBW)]
    xtok = [A.alloc([NSUB, D], BF16) for _ in range(NBW)]
    XT = [A.alloc([8, T], BF16) for _ in range(2)]
    sgs = [A.alloc([T], F32) for _ in range(2)]
    aT = [A.alloc([2, T], BF16) for _ in range(2)]
    NYO = 4
    yo = [A.alloc([D], F32) for _ in range(NYO)]
    npsB = mk_nps([0, 1, 2])
    npsC = mk_nps([3, 4, 5])
    _bc = {}

    def get_bc(e):
        if "v" not in _bc:
            reg = e.alloc_register("bcreg")
            e.reg_mov(reg, NE * 128 - 1)
            _bc["v"] = e.snap(reg, donate=True)
        return _bc["v"]

    ORDER = []
    for q in range((NS + 1) // 2):
        ORDER.append(q)
        if NS - 1 - q > q:
            ORDER.append(NS - 1 - q)
    assert sorted(ORDER) == list(range(NS))

    _bc2 = {}

    def get_bc2(e):
        if "v" not in _bc2:
            reg = e.alloc_register("bcreg2")
            e.reg_mov(reg, NS * T - 1)
            _bc2["v"] = e.snap(reg, donate=True)
        return _bc2["v"]

    def load_w(i, which):
        s = ORDER[i]
        bw = i % NBW
        for (wsb, wdr, hn) in which(bw):
            P.op("pool", lambda e, wsb=wsb, wdr=wdr, s=s: e.indirect_dma_start(
                out=wsb, out_offset=None, in_=wdr[:, :],
                in_offset=bass.IndirectOffsetOnAxis(ap=widx_i[:, s:s + 1], axis=0),
                bounds_check=get_bc(e), oob_is_err=False), r=["widx_i"], w=[hn], dma=True)

    def w_gu(bw):
        return ((wgs[bw], wg_d, f"wg{bw}"), (wus[bw], wu_d, f"wu{bw}"))

    def w_d(bw):
        return ((wds[bw], wd_d, f"wd{bw}"),)

    def load_x(i):
        s = ORDER[i]
        bw = i % NBW
        for st in range(NSUB):
            r0 = s * T + st * 128
            P.op("sp", lambda e, bw=bw, st=st, r0=r0: e.dma_start(out=xtok[bw][:, st, :], in_=xs_d[r0:r0 + 128, :]),
                 w=[f"xtok{bw}_{st}"], dma=True)

    def stageA(i):
        s = ORDER[i]
        b, bw = i % 2, i % NBW
        P.rec()
        for st in range(NSUB):
            k = (i * NSUB + st) % 2
            pb_, hpb = psb[k], f"psb{k}"
            for c in range(8):
                P.op("pe", lambda e, pb_=pb_, st=st, c=c: e.transpose(out=pb_[:, c * 128:(c + 1) * 128],
                                                                     in_=xtok[bw][:, st, c * 128:(c + 1) * 128], identity=identb),
                     r=[f"xtok{bw}_{st}", "identb"], w=[hpb])
            if True:
                P.op("act", lambda e, pb_=pb_, st=st: e.activation(out=XT[b][:, :, st * 128:(st + 1) * 128],
                                                                   in_=pb_[:].rearrange("p (c t) -> p c t", c=8), func=AF.Identity),
                     r=[hpb], w=[f"XT{b}_{st}"])
            else:
                P.op("dve", lambda e, pb_=pb_, st=st: e.tensor_copy(out=XT[b][:, :, st * 128:(st + 1) * 128],
                                                                    in_=pb_[:].rearrange("p (c t) -> p c t", c=8)),
                     r=[hpb], w=[f"XT{b}_{st}"])
        return P.end()

    def stageB(i):
        s = ORDER[i]
        b, bw = i % 2, i % NBW
        XH = [f"XT{b}_{st}" for st in range(NSUB)]
        P.rec()
        for fch in range(2):
            pg, hpg = npsB()
            for c in range(8):
                P.op("pe", lambda e, pg=pg, c=c, fch=fch: e.matmul(
                    pg[:, 0:T], lhsT=wgs[bw][:, c * 256 + fch * 128:c * 256 + (fch + 1) * 128], rhs=XT[b][:, c, :],
                    start=(c == 0), stop=(c == 7)), r=[f"wg{bw}"] + XH, w=[hpg])
            P.op("act", lambda e, pg=pg: e.activation(out=sgs[b], in_=pg[:, 0:T], func=AF.Silu), r=[hpg], w=[f"sgs{b}"])
            pu, hpu = npsB()
            for c in range(8):
                P.op("pe", lambda e, pu=pu, c=c, fch=fch: e.matmul(
                    pu[:, 0:T], lhsT=wus[bw][:, c * 256 + fch * 128:c * 256 + (fch + 1) * 128], rhs=XT[b][:, c, :],
                    start=(c == 0), stop=(c == 7)), r=[f"wu{bw}"] + XH, w=[hpu])
            P.op("dve", lambda e, pu=pu, fch=fch: e.tensor_tensor(out=aT[b][:, fch, :], in0=pu[:, 0:T], in1=sgs[b], op=ALU.mult),
                 r=[hpu, f"sgs{b}"], w=[f"aT{b}_{fch}"])
        return P.end()

    def stageC(i):
        s = ORDER[i]
        b, bw = i % 2, i % NBW
        P.rec()
        for st in range(NSUB):
            yb = (i * NSUB + st) % NYO
            for half in range(2):
                po, hpo = npsC()
                for fch in range(2):
                    P.op("pe", lambda e, po=po, st=st, fch=fch, half=half: e.matmul(
                        po[:], lhsT=aT[b][:, fch, st * 128:(st + 1) * 128],
                        rhs=wds[bw][:, fch * 1024 + half * 512:fch * 1024 + (half + 1) * 512], start=(fch == 0), stop=(fch == 1)),
                        r=[f"aT{b}_0", f"aT{b}_1", f"wd{bw}"], w=[hpo])
                P.op("dve", lambda e, po=po, yb=yb, half=half: e.tensor_tensor(
                    out=yo[yb][:, half * 512:(half + 1) * 512], in0=po[:], in1=gbc[:, 3, half * 512:(half + 1) * 512], op=ALU.mult),
                    r=[hpo, "gbc3_0", "gbc3_1"], w=[f"yo{yb}{'ab'[half]}"])
            P.op("pool", lambda e, yb=yb, st=st: e.indirect_dma_start(
                out=ys_d[:, :], out_offset=bass.IndirectOffsetOnAxis(ap=yidx_i[:, st, s:s + 1], axis=0),
                in_=yo[yb], in_offset=None, bounds_check=get_bc2(e), oob_is_err=False),
                r=[f"yo{yb}a", f"yo{yb}b", "yidx_i"], w=[f"ys_{s}_{st}"], dma=True)
        return P.end()

    for s in range(min(NBW, NS)):
        load_x(s)
        load_w(s, w_gu)
        load_w(s, w_d)
    for i in range(NS + 2):
        lists = []
        if i < NS:
            lists.append(stageA(i))
        if 0 <= i - 1 < NS:
            lists.append(stageB(i - 1))
        if 0 <= i - 2 < NS:
            lists.append(stageC(i - 2))
        P.play(merge(*lists))
        if i + NBW < NS:
            load_x(i + NBW)
        if i - 1 >= 0 and i - 1 + NBW < NS:
            load_w(i - 1 + NBW, w_gu)
        if i - 2 >= 0 and i - 2 + NBW < NS:
            load_w(i - 2 + NBW, w_d)
    P.fence()

    A.off = mark2
    NB2F = 4
    Y1 = [A.alloc([D], F32) for _ in range(NB2F)]
    Y2 = [A.alloc([D], F32) for _ in range(NB2F)]
    t1 = [A.alloc([D], F32) for _ in range(NB2F)]
    ff = [A.alloc([D], F32) for _ in range(2)]
    r2 = [A.alloc([D], F32) for _ in range(2)]
    qq = [A.alloc([D], F32) for _ in range(2)]
    ob = [A.alloc([D], F32) for _ in range(2)]
    bn2 = [A.alloc([12], F32) for _ in range(2)]
    ag2 = [A.alloc([4], F32) for _ in range(2)]

    def loads2f(j):
        q = j % NB2F
        P.op("pool", lambda e: e.indirect_dma_start(
            out=Y1[q], out_offset=None, in_=ys_d[:, :], in_offset=bass.IndirectOffsetOnAxis(ap=pos_i[:, 0, j:j + 1], axis=0)),
            r=["pos_i"], w=[f"Y1{q}"], dma=True)
        P.op("pool", lambda e: e.indirect_dma_start(
            out=Y2[q], out_offset=None, in_=ys_d[:, :], in_offset=bass.IndirectOffsetOnAxis(ap=pos_i[:, 1, j:j + 1], axis=0)),
            r=["pos_i"], w=[f"Y2{q}"], dma=True)
        P.op("sp", lambda e: e.dma_start(out=t1[q], in_=t1_d[j * 128:(j + 1) * 128, :]), r=[f"t1_d{j}"], w=[f"t1{q}"], dma=True)

    def chain2f(j):
        b = j % 2
        q = j % NB2F
        P.rec()
        P.op("act", lambda e: e.activation(out=ff[b], in_=Y1[q], func=AF.Identity, scale=wts[:, 0, j:j + 1]), r=[f"Y1{q}", "wts"], w=[f"ff{b}"])
        P.op("dve", lambda e: e.scalar_tensor_tensor(out=ff[b], in0=Y2[q], scalar=wts[:, 1, j:j + 1], in1=ff[b], op0=ALU.mult, op1=ALU.add),
             r=[f"Y2{q}", "wts", f"ff{b}"], w=[f"ff{b}"])
        P.op("dve", lambda e: e.tensor_tensor(out=r2[b], in0=ff[b], in1=t1[q], op=ALU.add), r=[f"ff{b}", f"t1{q}"], w=[f"r2{b}"])
        for h in range(2):
            P.op("dve", lambda e, h=h: e.bn_stats(out=bn2[b][:, h * 6:(h + 1) * 6], in_=r2[b][:, h * 512:(h + 1) * 512]), r=[f"r2{b}"], w=[f"bn2{b}"])
        P.op("dve", lambda e: e.bn_aggr(out=ag2[b][:, 0:2], in_=bn2[b]), r=[f"bn2{b}"], w=[f"ag2{b}"])
        P.op("act", lambda e: e.activation(out=ag2[b][:, 2:3], in_=ag2[b][:, 1:2], func=AF.Sqrt, bias=EPSC, scale=1.0), r=[f"ag2{b}", "epsc"], w=[f"ag2b{b}"])
        P.op("dve", lambda e: e.reciprocal(out=ag2[b][:, 2:3], in_=ag2[b][:, 2:3]), r=[f"ag2b{b}"], w=[f"ag2b{b}"])
        P.op("dve", lambda e: e.scalar_tensor_tensor(out=ag2[b][:, 3:4], in0=ag2[b][:, 0:1], scalar=-1.0, in1=ag2[b][:, 2:3], op0=ALU.mult, op1=ALU.mult),
             r=[f"ag2{b}", f"ag2b{b}"], w=[f"ag2c{b}"])
        P.op("act", lambda e: e.activation(out=qq[b], in_=r2[b], func=AF.Identity, bias=ag2[b][:, 3:4], scale=ag2[b][:, 2:3]),
             r=[f"r2{b}", f"ag2b{b}", f"ag2c{b}"], w=[f"qq{b}"])
        P.op("pool", lambda e: e.tensor_tensor(out=qq[b], in0=qq[b], in1=lnr[:, 2, :], op=ALU.mult), r=[f"qq{b}", "lnr"], w=[f"qq{b}"])
        P.op("dve", lambda e: e.tensor_tensor(out=ob[b], in0=qq[b], in1=lnr[:, 3, :], op=ALU.add), r=[f"qq{b}", "lnr"], w=[f"ob{b}"])
        P.op("sp", lambda e: e.dma_start(out=out_d[j * 128:(j + 1) * 128, :], in_=ob[b]), r=[f"ob{b}"], w=[f"out{j}"], dma=True)
        return P.end()

    loads2f(0)
    loads2f(1)
    for j in range(0, NT, 2):
        if j + 2 < NT:
            loads2f(j + 2)
            loads2f(j + 3)
        P.play(merge(chain2f(j), chain2f(j + 1)))


class _StopEmit(Exception):
    pass


def _host_prep(inp, b):
    f = np.float32
    L = 0
    cols = np.zeros((128, NCOL), f)
    cols[:, C_C:C_C + 8] = inp["c"][b].reshape(8, 128).T
    w_in = inp["w_in"][L]
    b_in = inp["b_in"][L]
    qcols = np.concatenate([np.arange(1024 + h * 64, 1024 + (h + 1) * 64) for h in HORDER])
    perm = np.concatenate([np.arange(0, 1024), qcols, np.arange(1536, 1792)])
    w_in_p = np.ascontiguousarray(w_in[:, perm])
    b_in_p = b_in[perm]
    cols[:, C_BIN:C_BIN + 13] = b_in_p[:1664].reshape(13, 128).T
    cols[:, C_CW:C_CW + 124] = inp["conv_w"][L].T.reshape(4, 128, 31).transpose(1, 0, 2).reshape(128, 124)
    cols[:, C_CB:C_CB + 4] = inp["conv_b"][L].reshape(4, 128).T
    cols[:, C_LG:C_LG + 4] = inp["conv_ln_g"][L].reshape(4, 128).T
    cols[:, C_LB:C_LB + 4] = inp["conv_ln_b"][L].reshape(4, 128).T
    cols[:, C_OG:C_OG + 4] = inp["conv_out_g"][L].reshape(4, 128).T
    cols[:, C_PID] = np.arange(128)
    rows = np.zeros((1, NROW), f)
    rows[0, R_BADA:R_BADA + 6144] = inp["b_ada"][L]
    rows[0, R_BV:R_BV + 128] = b_in[1664:1792]
    rows[0, R_SINK:R_SINK + 8] = inp["sinks"][L][HORDER]
    rows[0, R_AOG:R_AOG + 512] = inp["attn_out_g"][L].reshape(8, 64)[HORDER].reshape(-1)
    rows[0, R_BOUT:R_BOUT + D] = inp["b_out"][L]
    rows[0, R_L1G:R_L1G + D] = inp["ln1_g"][L]
    rows[0, R_L1B:R_L1B + D] = inp["ln1_b"][L]
    rows[0, R_L2G:R_L2G + D] = inp["ln2_g"][L]
    rows[0, R_L2B:R_L2B + D] = inp["ln2_b"][L]
    rows[0, R_BR:R_BR + 4] = inp["b_router_group"][L]
    rows[0, R_BR + 4:R_BR + 36] = inp["b_router_expert"][L]
    rows[0, R_SLOT:R_SLOT + NS] = np.arange(NS) * T
    w_out = inp["w_out"][L]
    arows = np.concatenate([np.arange(512 + h * 64, 512 + (h + 1) * 64) for h in HORDER])
    w_out_p = np.ascontiguousarray(np.concatenate([w_out[:512], w_out[arows]], axis=0))
    w_r = np.ascontiguousarray(np.concatenate([inp["w_router_group"][L], inp["w_router_expert"][L]], axis=1))
    return dict(cols=cols, rows=rows, w_in=w_in_p, w_out=w_out_p, w_r=w_r)


def _consts():
    f = np.float32
    cst = np.zeros((128, NK), f)
    cst[:, K_ID:K_ID + 128] = np.eye(128)
    p = np.arange(128)
    cst[:, K_U:K_U + 128] = (p[:, None] < p[None, :])
    cst[:, K_BD:K_BD + 128] = ((p[:, None] // 64) == (p[None, :] // 64)) / 64.0
    m0 = np.where(p[:, None] > p[None, :], 0.0, NEG)
    m1 = np.where(p[:, None] <= p[None, :], 0.0, NEG)
    cst[:, K_M0:K_M0 + 512] = np.tile(m0, (1, 4))
    cst[:, K_M1:K_M1 + 512] = np.tile(m1, (1, 4))
    return cst


def _expert_layout(inp):
    L = 0
    wg = np.ascontiguousarray(inp["w_gate"][L].reshape(NE, 8, 128, DE).transpose(0, 2, 1, 3)).reshape(NE * 128, 2048)
    wu = np.ascontiguousarray(inp["w_up"][L].reshape(NE, 8, 128, DE).transpose(0, 2, 1, 3)).reshape(NE * 128, 2048)
    wd = np.ascontiguousarray(inp["w_down"][L].reshape(NE, 2, 128, D).transpose(0, 2, 1, 3)).reshape(NE * 128, 2048)
    return wg, wu, wd


_CACHE = {}


def kernel(**inputs):
    inp = {k: np.asarray(v) for k, v in inputs.items()}
    dbg = _CACHE.get("dbg")
    nc = build_program(dbg)
    cst = _consts()
    wg, wu, wd = _expert_layout(inp)
    w_ada = np.ascontiguousarray(inp["w_ada"][0])
    zeros = np.zeros((1152, D // 2), dtype=np.float32)
    in_maps = []
    ncores = _CACHE.get("ncores", 8)
    for b in range(ncores):
        hp = _host_prep(inp, b)
        m = dict(x=np.ascontiguousarray(inp["x"][b]), cols=hp["cols"], rows=hp["rows"], cst=cst, w_ada=w_ada,
                 w_in=hp["w_in"], w_out=hp["w_out"], w_r=hp["w_r"], wg=wg, wu=wu, wd=wd, zeros=zeros)
        if _CACHE.get("p2only"):
            m["z_in"] = _CACHE["z_in"]
        in_maps.append(m)
    if _CACHE.get("trace"):
        res = run_bass_kernel_spmd(nc, in_maps, core_ids=list(range(ncores)), trace=True)
        print("EXEC_NS", res.exec_time_ns)
    else:
        res = run_bass_kernel_spmd(nc, in_maps, core_ids=list(range(ncores)))
    if dbg:
        return [np.asarray(r["dbg"]) for r in res.results]
    out = np.stack([np.asarray(r["out"]) for r in res.results], axis=0).astype(np.float32)
    return out
```
